# Optimizing a Trainium2 kernel written in Bass

```python
import jax, jax.numpy as jnp
from jax import lax
import numpy as np

D_MODEL = 1024
BATCH = 8
SEQ = 4096
DEPTH = 2

A_HEADS = 8
A_HEAD_DIM = 64
A_WIDTH = A_HEADS * A_HEAD_DIM
A_DECAY_LORA = 64
A_ICLR_LORA = 64
A_GATE_LORA = 128
A_GN_EPS = 64e-5
A_SIZES = (A_WIDTH, A_WIDTH, A_WIDTH, A_DECAY_LORA, A_ICLR_LORA, A_GATE_LORA)
A_COLS = sum(A_SIZES)

B_HEADS = 4
B_KEY_DIM = 64
B_VAL_DIM = 128
B_KEY_WIDTH = B_HEADS * B_KEY_DIM
B_VAL_WIDTH = B_HEADS * B_VAL_DIM
B_GATE_LORA = 16
B_GATE_TAU = 16.0
B_CHUNK = 64
B_NORM_EPS = 1e-5
B_SIZES = (B_KEY_WIDTH, B_KEY_WIDTH, B_VAL_WIDTH, B_VAL_WIDTH, B_GATE_LORA)
B_COLS = sum(B_SIZES)

EVEN_COLS = A_COLS + B_COLS
EVEN_OUT = A_WIDTH + B_VAL_WIDTH

C_HEADS = 8
C_HEAD_DIM = 128
C_WIDTH = C_HEADS * C_HEAD_DIM
C_IDX_HEADS = 4
C_IDX_DIM = 64
C_INDEX_TOPK = 256
C_QBLOCK = 128
C_SIZES = (C_WIDTH, C_HEAD_DIM, C_HEAD_DIM, C_IDX_HEADS * C_IDX_DIM, C_IDX_DIM, C_IDX_HEADS)
ODD_COLS = sum(C_SIZES)
ROPE_THETA = 10000.0

N_EXPERTS = 16
N_GROUPS = 4
EXPERTS_PER_GROUP = N_EXPERTS // N_GROUPS
TOP_K = 2
D_EXPERT = 256

DN_ALPHA = (2 * DEPTH) ** 0.25
DN_BETA = (8 * DEPTH) ** -0.25
LN_EPS = 1e-5
N_EVEN = (DEPTH + 1) // 2
N_ODD = DEPTH // 2

kernel_name = 'hybrid_rwkv7_gla_dsa_moe_deepnorm'


def _split_cols(p, sizes):
    return jnp.split(p, np.cumsum(sizes)[:-1].tolist(), axis=-1)


def _layer_norm(x, g, b, eps=LN_EPS):
    xf = x.astype(jnp.float32)
    mu = jnp.mean(xf, -1, keepdims=True)
    var = jnp.mean(jnp.square(xf - mu), -1, keepdims=True)
    return ((xf - mu) * lax.rsqrt(var + eps) * g + b).astype(x.dtype)


def _token_shift(z):
    return jnp.pad(z, ((0, 0), (1, 0), (0, 0)))[:, :-1]


def _rope(z, pos):
    half = z.shape[-1] // 2
    inv = ROPE_THETA ** (-jnp.arange(half, dtype=jnp.float32) / half)
    ang = pos[:, None] * inv[None, :]
    cos = jnp.cos(ang)[:, None, :].astype(z.dtype)
    sin = jnp.sin(ang)[:, None, :].astype(z.dtype)
    z1, z2 = z[..., :half], z[..., half:]
    return jnp.concatenate([z1 * cos - z2 * sin, z2 * cos + z1 * sin], axis=-1)


def _rwkv7_group(p, mu, w0, w2, a0, a2, g2, kk_scale, ka_scale, r_k, gn_g, gn_b):
    bsz, t, _ = p.shape
    f32 = jnp.float32
    pm = p + (_token_shift(p) - p) * mu
    r, k, v, xw, xa, xg = _split_cols(pm, A_SIZES)
    w_log = -jax.nn.softplus(-(w0 + jnp.tanh(xw) @ w2).astype(f32)) - 0.5
    decay = jnp.exp(-jnp.exp(w_log))
    a = jax.nn.sigmoid((a0 + xa @ a2).astype(f32))
    g = (jax.nn.sigmoid(xg) @ g2).astype(f32)
    heads = lambda z: z.astype(f32).reshape(bsz, t, A_HEADS, A_HEAD_DIM)
    kk = heads(k * kk_scale)
    kk = kk / jnp.maximum(jnp.linalg.norm(kk, axis=-1, keepdims=True), 1e-12)
    kh = heads(k.astype(f32) * (1.0 + (a - 1.0) * ka_scale))
    rh, vh, wh, ah = heads(r), heads(v), heads(decay), heads(a)

    def step(state, inp):
        r_t, w_t, k_t, v_t, kk_t, a_t = inp
        sa = jnp.einsum('bhvk,bhk->bhv', state, kk_t)
        state = (state * w_t[:, :, None, :] - sa[..., None] * (kk_t * a_t)[:, :, None, :]
                 + v_t[..., None] * k_t[:, :, None, :])
        return state, jnp.einsum('bhvk,bhk->bhv', state, r_t)

    xs = tuple(jnp.swapaxes(z, 0, 1) for z in (rh, wh, kh, vh, kk, ah))
    s0 = jnp.zeros((bsz, A_HEADS, A_HEAD_DIM, A_HEAD_DIM), f32)
    _, y = lax.scan(step, s0, xs)
    y = jnp.swapaxes(y, 0, 1)
    ym = jnp.mean(y, -1, keepdims=True)
    yv = jnp.mean(jnp.square(y - ym), -1, keepdims=True)
    yn = ((y - ym) * lax.rsqrt(yv + A_GN_EPS)).reshape(bsz, t, A_WIDTH) * gn_g + gn_b
    bonus = (jnp.sum(rh * kh * r_k, -1, keepdims=True) * vh).reshape(bsz, t, A_WIDTH)
    return ((yn + bonus) * g).astype(p.dtype)


def _gla_group(p, gate_w2, gate_b, norm_g):
    bsz, t, _ = p.shape
    f32 = jnp.float32
    nc = t // B_CHUNK
    q, k, v, g, xa = _split_cols(p, B_SIZES)
    log_a = jax.nn.log_sigmoid((xa @ gate_w2 + gate_b).astype(f32)) / B_GATE_TAU

    def chunks(z, d):
        return z.astype(f32).reshape(bsz, nc, B_CHUNK, B_HEADS, d).transpose(0, 3, 1, 2, 4)

    qc = chunks(q, B_KEY_DIM) * B_KEY_DIM ** -0.5
    kc = chunks(k, B_KEY_DIM)
    vc = chunks(v, B_VAL_DIM)
    bc = jnp.cumsum(chunks(log_a, B_KEY_DIM), axis=3)
    b_last = bc[:, :, :, -1:, :]
    q_dec = qc * jnp.exp(bc)
    k_inv = kc * jnp.exp(-bc)
    k_end = kc * jnp.exp(b_last - bc)
    causal = jnp.tril(jnp.ones((B_CHUNK, B_CHUNK), bool))
    att = jnp.where(causal, jnp.einsum('bhncd,bhnsd->bhncs', q_dec, k_inv), 0.0)
    o_intra = jnp.einsum('bhncs,bhnse->bhnce', att, vc)
    kv = jnp.einsum('bhncd,bhnce->bhnde', k_end, vc)

    def step(state, inp):
        d_n, kv_n = inp
        return d_n[..., None] * state + kv_n, state

    s0 = jnp.zeros((bsz, B_HEADS, B_KEY_DIM, B_VAL_DIM), f32)
    _, s_prev = lax.scan(step, s0, (jnp.moveaxis(jnp.exp(b_last[:, :, :, 0, :]), 2, 0),
                                    jnp.moveaxis(kv, 2, 0)))
    o_inter = jnp.einsum('bhncd,nbhde->bhnce', q_dec, s_prev)
    o = (o_intra + o_inter).transpose(0, 2, 3, 1, 4).reshape(bsz, t, B_HEADS, B_VAL_DIM)
    o = o * lax.rsqrt(jnp.mean(jnp.square(o), -1, keepdims=True) + B_NORM_EPS)
    o = o.reshape(bsz, t, B_VAL_WIDTH) * norm_g * jax.nn.silu(g.astype(f32))
    return o.astype(p.dtype)


def _dsa_mixer(p, ik_g, ik_b, k_top):
    bsz, t, _ = p.shape
    f32 = jnp.float32
    q, k, v, iq, ik, iw = _split_cols(p, C_SIZES)
    pos = jnp.arange(t, dtype=f32)
    q = _rope(q.reshape(bsz, t, C_HEADS, C_HEAD_DIM), pos)
    k = _rope(k.reshape(bsz, t, 1, C_HEAD_DIM), pos)[:, :, 0]
    iq = _rope(iq.reshape(bsz, t, C_IDX_HEADS, C_IDX_DIM), pos)
    ik = _rope(_layer_norm(ik, ik_g, ik_b).reshape(bsz, t, 1, C_IDX_DIM), pos)[:, :, 0]
    iw = iw.astype(f32) * C_IDX_HEADS ** -0.5
    nb = t // C_QBLOCK
    key_pos = jnp.arange(t)
    to_blocks = lambda z: jnp.swapaxes(z.reshape(bsz, nb, C_QBLOCK, *z.shape[2:]), 0, 1)
    gather_rows = jax.vmap(lambda src, idx: src[idx])

    def block(args):
        qb, iqb, iwb, qpos = args
        s = jnp.einsum('bqhd,bsd->bhqs', iqb, ik).astype(f32) * C_IDX_DIM ** -0.5
        score = jnp.einsum('bhqs,bqh->bqs', jax.nn.relu(s), iwb)
        score = jnp.where(key_pos[None, None, :] <= qpos[None, :, None], score, -jnp.inf)
        _, idx = lax.top_k(score, k_top)
        valid = idx <= qpos[None, :, None]
        k_sel = gather_rows(k, idx)
        v_sel = gather_rows(v, idx)
        logits = jnp.einsum('bqhd,bqkd->bqhk', qb, k_sel).astype(f32) * C_HEAD_DIM ** -0.5
        logits = jnp.where(valid[:, :, None, :], logits, -jnp.inf)
        prob = jax.nn.softmax(logits, axis=-1).astype(v_sel.dtype)
        return jnp.einsum('bqhk,bqkd->bqhd', prob, v_sel)

    out = lax.map(block, (to_blocks(q), to_blocks(iq), to_blocks(iw), key_pos.reshape(nb, C_QBLOCK)))
    return jnp.swapaxes(out, 0, 1).reshape(bsz, t, C_WIDTH)


def _moe(h, router_w, router_bias, w_gate, w_up, w_down):
    bsz, t, d = h.shape
    f32 = jnp.float32
    hf = h.reshape(-1, d)
    s = jax.nn.sigmoid((hf @ router_w).astype(f32))
    sel = s + router_bias
    group_score = lax.top_k(sel.reshape(-1, N_GROUPS, EXPERTS_PER_GROUP), TOP_K)[0].sum(-1)
    best = jnp.argmax(group_score, axis=-1)
    in_group = (jnp.arange(N_EXPERTS) // EXPERTS_PER_GROUP)[None, :] == best[:, None]
    _, eidx = lax.top_k(jnp.where(in_group, sel, -jnp.inf), TOP_K)
    gate = jnp.take_along_axis(s, eidx, axis=-1)
    gate = gate / jnp.sum(gate, -1, keepdims=True)
    comb = jnp.sum(jax.nn.one_hot(eidx, N_EXPERTS, dtype=f32) * gate[..., None], axis=1)
    y = jnp.zeros(hf.shape, f32)
    for e in range(N_EXPERTS):
        he = (jax.nn.silu(hf @ w_gate[e]) * (hf @ w_up[e])) @ w_down[e]
        y = y + comb[:, e:e + 1] * he.astype(f32)
    return y.astype(h.dtype).reshape(bsz, t, d)


def setup_inputs(seed: int = 0) -> dict:
    key = jax.random.key(seed)
    ks = list(jax.random.split(key, 32))
    f32 = jnp.float32
    nrm = lambda k, shape, scale: jax.random.normal(k, shape, f32) * scale
    uni = lambda k, shape, lo, hi: jax.random.uniform(k, shape, f32, lo, hi)
    x = nrm(ks[0], (BATCH, SEQ, D_MODEL), 1.0)
    bv0 = A_COLS + 2 * B_KEY_WIDTH
    even_scale = (jnp.ones((EVEN_COLS,), f32).at[2 * A_WIDTH:3 * A_WIDTH].set(DN_BETA)
                  .at[bv0:bv0 + B_VAL_WIDTH].set(DN_BETA))
    w_in_even = nrm(ks[1], (N_EVEN, D_MODEL, EVEN_COLS), D_MODEL ** -0.5) * even_scale
    a_mu = uni(ks[2], (N_EVEN, A_COLS), 0.0, 1.0)
    a_w0 = uni(ks[3], (N_EVEN, A_WIDTH), -6.0, 1.0)
    a_w2 = nrm(ks[4], (N_EVEN, A_DECAY_LORA, A_WIDTH), A_DECAY_LORA ** -0.5)
    a_a0 = nrm(ks[5], (N_EVEN, A_WIDTH), 0.5)
    a_a2 = nrm(ks[6], (N_EVEN, A_ICLR_LORA, A_WIDTH), 0.5 * A_ICLR_LORA ** -0.5)
    a_g2 = nrm(ks[7], (N_EVEN, A_GATE_LORA, A_WIDTH), A_GATE_LORA ** -0.5)
    a_kk_scale = 1.0 + nrm(ks[8], (N_EVEN, A_WIDTH), 0.1)
    a_ka_scale = 1.0 + nrm(ks[9], (N_EVEN, A_WIDTH), 0.1)
    a_r_k = nrm(ks[10], (N_EVEN, A_HEADS, A_HEAD_DIM), 0.1)
    a_gn_g = 1.0 + nrm(ks[11], (N_EVEN, A_WIDTH), 0.02)
    a_gn_b = nrm(ks[12], (N_EVEN, A_WIDTH), 0.02)
    b_gate_w2 = nrm(ks[13], (N_EVEN, B_GATE_LORA, B_KEY_WIDTH), B_GATE_LORA ** -0.5)
    b_gate_b = nrm(ks[14], (N_EVEN, B_KEY_WIDTH), 0.1)
    b_norm_g = 1.0 + nrm(ks[15], (N_EVEN, B_VAL_WIDTH), 0.02)
    w_out_even = nrm(ks[16], (N_EVEN, EVEN_OUT, D_MODEL), DN_BETA * EVEN_OUT ** -0.5)
    cv0 = C_WIDTH + C_HEAD_DIM
    odd_scale = jnp.ones((ODD_COLS,), f32).at[cv0:cv0 + C_HEAD_DIM].set(DN_BETA)
    w_in_odd = nrm(ks[17], (N_ODD, D_MODEL, ODD_COLS), D_MODEL ** -0.5) * odd_scale
    c_ik_ln_g = 1.0 + nrm(ks[18], (N_ODD, C_IDX_DIM), 0.02)
    c_ik_ln_b = nrm(ks[19], (N_ODD, C_IDX_DIM), 0.02)
    w_out_odd = nrm(ks[20], (N_ODD, C_WIDTH, D_MODEL), DN_BETA * C_WIDTH ** -0.5)
    ln1_g = 1.0 + nrm(ks[21], (DEPTH, D_MODEL), 0.02)
    ln1_b = nrm(ks[22], (DEPTH, D_MODEL), 0.02)
    ln2_g = 1.0 + nrm(ks[23], (DEPTH, D_MODEL), 0.02)
    ln2_b = nrm(ks[24], (DEPTH, D_MODEL), 0.02)
    router_w = nrm(ks[25], (D_MODEL, N_EXPERTS), D_MODEL ** -0.5)
    router_bias = nrm(ks[26], (N_EXPERTS,), 0.01)
    exp_w_gate = nrm(ks[27], (DEPTH, N_EXPERTS, D_MODEL, D_EXPERT), DN_BETA * D_MODEL ** -0.5)
    exp_w_up = nrm(ks[28], (DEPTH, N_EXPERTS, D_MODEL, D_EXPERT), DN_BETA * D_MODEL ** -0.5)
    exp_w_down = nrm(ks[29], (DEPTH, N_EXPERTS, D_EXPERT, D_MODEL), DN_BETA * D_EXPERT ** -0.5)
    return {'x': x, 'w_in_even': w_in_even, 'a_mu': a_mu, 'a_w0': a_w0, 'a_w2': a_w2,
            'a_a0': a_a0, 'a_a2': a_a2, 'a_g2': a_g2, 'a_kk_scale': a_kk_scale,
            'a_ka_scale': a_ka_scale, 'a_r_k': a_r_k, 'a_gn_g': a_gn_g, 'a_gn_b': a_gn_b,
            'b_gate_w2': b_gate_w2, 'b_gate_b': b_gate_b, 'b_norm_g': b_norm_g,
            'w_out_even': w_out_even, 'w_in_odd': w_in_odd, 'c_ik_ln_g': c_ik_ln_g,
            'c_ik_ln_b': c_ik_ln_b, 'w_out_odd': w_out_odd, 'ln1_g': ln1_g, 'ln1_b': ln1_b,
            'ln2_g': ln2_g, 'ln2_b': ln2_b, 'router_w': router_w, 'router_bias': router_bias,
            'exp_w_gate': exp_w_gate, 'exp_w_up': exp_w_up, 'exp_w_down': exp_w_down}


def reference(x, w_in_even, a_mu, a_w0, a_w2, a_a0, a_a2, a_g2, a_kk_scale, a_ka_scale, a_r_k,
              a_gn_g, a_gn_b, b_gate_w2, b_gate_b, b_norm_g, w_out_even, w_in_odd, c_ik_ln_g,
              c_ik_ln_b, w_out_odd, ln1_g, ln1_b, ln2_g, ln2_b, router_w, router_bias,
              exp_w_gate, exp_w_up, exp_w_down):
    k_top = min(C_INDEX_TOPK, x.shape[1] // 4)
    for l in range(DEPTH):
        i = l // 2
        if l % 2 == 0:
            p = x @ w_in_even[i]
            ya = _rwkv7_group(p[..., :A_COLS], a_mu[i], a_w0[i], a_w2[i], a_a0[i], a_a2[i],
                              a_g2[i], a_kk_scale[i], a_ka_scale[i], a_r_k[i], a_gn_g[i], a_gn_b[i])
            yb = _gla_group(p[..., A_COLS:], b_gate_w2[i], b_gate_b[i], b_norm_g[i])
            mix = jnp.concatenate([ya, yb], axis=-1) @ w_out_even[i]
        else:
            p = x @ w_in_odd[i]
            mix = _dsa_mixer(p, c_ik_ln_g[i], c_ik_ln_b[i], k_top) @ w_out_odd[i]
        h = _layer_norm(DN_ALPHA * x + mix, ln1_g[l], ln1_b[l])
        ffn = _moe(h, router_w, router_bias, exp_w_gate[l], exp_w_up[l], exp_w_down[l])
        x = _layer_norm(DN_ALPHA * h + ffn, ln2_g[l], ln2_b[l])
    return x
```

```python
import numpy as np
import ml_dtypes
from contextlib import ExitStack
import concourse.bass as bass
import concourse.mybir as mybir
from concourse.bass_utils import run_bass_kernel_spmd

F32 = mybir.dt.float32
BF16 = mybir.dt.bfloat16
AF = mybir.ActivationFunctionType
ALU = mybir.AluOpType
AX = mybir.AxisListType

D = 1024
A_COLS = 1792
B_COLS = 1552
EVEN_COLS = 3344
ODD_COLS = 1604
NE = 16
DE = 256
DN_ALPHA = 4 ** 0.25
LN_EPS = 1e-5
DEC = 0.6065306597126334
DSA_INTERLEAVE = True
RWKV_INTERLEAVE = False


class Tok:
    __slots__ = ("w", "r")

    def __init__(self):
        self.w = None
        self.r = {}


class Eng:
    def __init__(self, name, h, sem):
        self.name = name
        self.h = h
        self.sem = sem
        self.cnt = 0
        self.waited = {}


class Prog:
    NSLOT = 8

    def __init__(self, nc, es):
        self.nc = nc
        self.es = es
        self.E = {}
        for name, h in (("pe", nc.tensor), ("act", nc.scalar), ("dve", nc.vector),
                        ("pool", nc.gpsimd), ("sp", nc.sync)):
            sem = es.enter_context(nc.semaphore("sem_" + name))
            self.E[name] = Eng(name, h, sem)
        self.slots = {}
        self.dn = {}
        for q in ("sp", "pool", "act"):
            self.slots[q] = [[es.enter_context(nc.semaphore("dq_%s%d" % (q, i))), 0] for i in range(self.NSLOT)]
            self.dn[q] = 0
        self.nalloc = 0

    def sb(self, shape, dt=F32, name=None, es=None):
        self.nalloc += 1
        t = (es or self.es).enter_context(self.nc.sbuf_tensor("%s_%d" % (name or "t", self.nalloc), list(shape), dt))
        return t

    def _wait(self, eng, ev):
        sem, val = ev
        key = sem.num
        if eng.waited.get(key, 0) >= val:
            return
        eng.h.wait_ge(sem, val)
        eng.waited[key] = val

    def _deps(self, en, r, w):
        eng = self.E[en]
        for t in r:
            if t.w is not None:
                yield t.w
        for t in w:
            if t.w is not None:
                yield t.w
            for ev in t.r.values():
                yield ev

    def op(self, en, fn, r=(), w=(), inc=True):
        eng = self.E[en]
        for ev in list(self._deps(en, r, w)):
            if en == "pe" and ev[0] is eng.sem:
                continue
            self._wait(eng, ev)
        ins = fn(eng.h)
        myev = (eng.sem, eng.cnt + 1)
        if inc:
            ins.then_inc(eng.sem, 1)
            eng.cnt += 1
        for t in r:
            t.r[en] = myev
        for t in w:
            t.w = myev
            t.r = {}
        return ins

    def dma(self, qn, out, in_, r=(), w=(), **kw):
        q = self.E[qn]
        for ev in list(self._deps(qn, r, w)):
            self._wait(q, ev)
        slot = self.slots[qn][self.dn[qn] % self.NSLOT]
        self.dn[qn] += 1
        if slot[1] > 0:
            self._wait(q, (slot[0], slot[1]))
        ins = q.h.dma_start(out=out, in_=in_, **kw)
        slot[1] += 16
        ins.then_inc(slot[0], 16)
        ev = (slot[0], slot[1])
        key = "d%d" % slot[0].num
        for t in r:
            t.r[key] = ev
        for t in w:
            t.w = ev
            t.r = {}

    def barrier(self):
        evs = []
        for q in self.slots:
            for sem, val in self.slots[q]:
                if val > 0:
                    evs.append((sem, val))
        for name, e in self.E.items():
            if e.cnt > 0:
                evs.append((e.sem, e.cnt))
        for name, e in self.E.items():
            for ev in evs:
                if ev[0] is e.sem and name == "pe":
                    continue
                self._wait(e, ev)

    def finish(self):
        sp = self.E["sp"]
        for q in self.slots:
            for sem, val in self.slots[q]:
                if val > 0:
                    self._wait(sp, (sem, val))
        for name, e in self.E.items():
            if name != "sp" and e.cnt > 0:
                self._wait(sp, (e.sem, e.cnt))


def host_consts(T):
    c = {}
    c["ident"] = np.eye(128, dtype=np.float32)
    j = np.arange(64)[:, None]
    i = np.arange(64)[None, :]
    c["tri64"] = np.stack([(-DEC) * (j <= i), (-DEC) * (j < i), (-DEC) * (j > i)], 1).astype(np.float32)
    c["ncol64"] = np.full((64, 1), -DEC, np.float32)
    su = (j < i).astype(np.float32)
    iu = (j <= i).astype(np.float32)
    sl = (j > i).astype(np.float32)
    mMA = np.concatenate([-su, iu], 1)
    mBB = np.concatenate([su, iu], 1)
    c["mMA"] = np.tile(mMA[:, None, :], (1, 8, 1)).astype(np.float32)
    c["mBB"] = np.tile(mBB[:, None, :], (1, 8, 1)).astype(np.float32)
    c["mNT"] = np.tile((-sl)[:, None, :], (1, 8, 1)).astype(np.float32)
    c["id8"] = np.tile(np.eye(64, dtype=np.float32)[:, None, :], (1, 8, 1))
    j = np.arange(128)[:, None]
    i = np.arange(128)[None, :]
    c["tri128"] = np.stack([(-1 / 16) * (j <= i), (-1 / 16) * (j > i)], 1).astype(np.float32)
    c["ncol128"] = np.full((128, 1), -1 / 16, np.float32)
    c["sel"] = (np.arange(16)[:, None, None] == np.arange(16)[None, :, None]).astype(np.float32) * np.ones((1, 1, 128), np.float32)
    c["iu128"] = np.tile((j <= i).astype(np.float32)[:, None, :], (1, 4, 1))
    return c


class Ctx:
    pass


def build(T, dbg=(), stages=("A", "R", "G", "O0", "M0", "S1", "O1", "M1")):
    nc = bass.Bass("TRN2", target_bir_lowering=False)
    es = ExitStack()
    P = Prog(nc, es)
    NT = T // 128
    NS = T // 512
    C = Ctx()
    C.nc, C.P, C.T, C.NT, C.NS = nc, P, T, NT, NS
    C.dbg = {}
    C.dsa_interleave = DSA_INTERLEAVE

    def din(name, shape, dt=F32):
        return nc.dram_tensor(name, list(shape), dt, kind="ExternalInput").ap()

    def dscr(name, shape, dt=F32, out=False):
        kind = "ExternalOutput" if (out or name in dbg) else "Internal"
        return nc.dram_tensor(name, list(shape), dt, kind=kind).ap()

    I = {}
    I["x"] = din("x", [T, D])
    for name, shape in (("w_in_even", [D, EVEN_COLS]), ("a_mu", [1, A_COLS]), ("a_w0", [1, 512]), ("a_w2", [64, 512]),
                        ("a_a0", [1, 512]), ("a_a2", [64, 512]), ("a_g2", [128, 512]), ("a_kk_scale", [1, 512]),
                        ("a_ka_scale", [1, 512]), ("a_r_k", [1, 512]), ("a_gn_g", [1, 512]), ("a_gn_b", [1, 512]),
                        ("b_gate_w2", [16, 256]), ("b_gate_b", [1, 256]), ("b_norm_g", [1, 512]),
                        ("w_out_even", [D, D]), ("w_in_odd", [D, ODD_COLS]), ("c_ik_ln_g", [1, 64]),
                        ("c_ik_ln_b", [1, 64]), ("w_out_odd", [D, D]), ("ln1_g", [2, D]), ("ln1_b", [2, D]),
                        ("ln2_g", [2, D]), ("ln2_b", [2, D]), ("router_w", [D, NE]), ("router_bias", [1, NE]),
                        ("exp_w_gate", [2, NE, D, DE]), ("exp_w_up", [2, NE, D, DE]), ("exp_w_down", [2, NE, DE, D])):
        I[name] = din(name, shape)
    hc = host_consts(T)
    hc.update(rope_consts(T))
    for k, v in hc.items():
        I["c_" + k] = din("c_" + k, list(v.shape), F32 if v.dtype == np.float32 else BF16)
    C.I = I
    out = dscr("out", [T, D], out=True)
    C.XT0 = dscr("XT0", [D, T + 1], BF16)
    C.XT0_tok = [Tok() for _ in range(NS)]
    C.XT0_z = Tok()
    C.YT = dscr("YT", [D, T], BF16)
    C.YT_tok = [[Tok() for _ in range(NS)] for _ in range(2)]
    C.H0 = dscr("H0", [T, D])
    C.H0_tok = [Tok() for _ in range(NT)]
    C.HT0 = dscr("HT0", [D, T], BF16)
    C.HT0_tok = [Tok() for _ in range(NS)]
    C.X1 = dscr("X1", [T, D])
    C.X1_tok = [Tok() for _ in range(NT)]
    C.XT1 = dscr("XT1", [D, T], BF16)
    C.XT1_tok = [Tok() for _ in range(NS)]

    for nm, shp in (("dbg_ya", [T, 512]), ("dbg_yb", [T, 512]), ("dbg_dsa", [T, 1024]), ("dbg_mask", [T, T]), ("dbg_sc", [T, T])):
        if nm in dbg:
            C.dbg[nm] = dscr(nm, shp, out=True)
    C.WGU16 = [dscr("WGU16_%d" % l, [NE, 128, 2 * 8 * DE], BF16) for l in range(2)]
    C.WGU16_tok = [[Tok() for _ in range(NE)] for l in range(2)]
    C.WD16 = [dscr("WD16_%d" % l, [128, 32 * D], BF16) for l in range(2)]
    C.WD16_tok = [Tok() for l in range(2)]
    C.banks = []
    for b in range(8):
        t = es.enter_context(nc.psum_tensor("psb%d" % b, [128, 512], F32))
        C.banks.append((t, Tok()))
    C.bi = 0

    C.bank_pool = list(range(8))

    def bank():
        b = C.banks[C.bank_pool[C.bi % len(C.bank_pool)]]
        C.bi += 1
        return b
    C.bank = bank

    C.ident = P.sb([128, 128], F32, "ident")
    C.ident_tok = Tok()
    P.dma("sp", C.ident[:], I["c_ident"], w=[C.ident_tok])

    if "M0" in stages:
        cast_weights(C, 0)
    if "A" in stages:
        phase_A(C)
    if "R" in stages:
        phase_rwkv(C)
    if "G" in stages:
        phase_gla(C)
    if "M1" in stages:
        cast_weights(C, 1)
    if "O0" in stages:
        phase_outproj(C, I["w_out_even"], C.YT, lambda s: [C.YT_tok[0][s], C.YT_tok[1][s]], I["x"], lambda i: [],
                      I["ln1_g"][0:1, :], I["ln1_b"][0:1, :], C.H0, C.H0_tok, C.HT0, C.HT0_tok)
    if "M0" in stages:
        phase_moe(C, 0, C.H0, C.H0_tok, C.HT0, C.HT0_tok, C.X1, C.X1_tok, C.XT1, C.XT1_tok)
    if "S1" in stages:
        phase_dsa(C)
    if "O1" in stages:
        C.H1 = dscr("H1", [T, D])
        C.H1_tok = [Tok() for _ in range(NT)]
        C.HT1 = dscr("HT1", [D, T], BF16)
        C.HT1_tok = [Tok() for _ in range(NS)]
        phase_outproj(C, I["w_out_odd"], C.YT, lambda s: [C.YT_tok[0][s], C.YT_tok[1][s]], C.X1, lambda i: [C.X1_tok[i]],
                      I["ln1_g"][1:2, :], I["ln1_b"][1:2, :], C.H1, C.H1_tok, C.HT1, C.HT1_tok)
    if "M1" in stages:
        out_tok = [Tok() for _ in range(NT)]
        phase_moe(C, 1, C.H1, C.H1_tok, C.HT1, C.HT1_tok, out, out_tok, None, None)
    P.finish()
    es.close()
    return nc


def XTv(ap):
    return ap.rearrange("(c p) t -> p c t", p=128)


def cast_weights(C, l):
    P, I = C.P, C.I
    for e in range(NE):
        dst = C.WGU16[l][e].rearrange("p (t c f) -> p t c f", t=2, c=8)
        P.dma("pool", dst[:, 0, :, :], I["exp_w_gate"][l, e].rearrange("(c p) f -> p c f", p=128), w=[C.WGU16_tok[l][e]])
        P.dma("pool", dst[:, 1, :, :], I["exp_w_up"][l, e].rearrange("(c p) f -> p c f", p=128), w=[C.WGU16_tok[l][e]])
    wd_flat = I["exp_w_down"][l].rearrange("e f d -> (e f) d")
    dstd = C.WD16[l].rearrange("p (c d) -> p c d", c=32)
    for c4 in range(8):
        P.dma("pool", dstd[:, c4 * 4:(c4 + 1) * 4, :], wd_flat[c4 * 512:(c4 + 1) * 512, :].rearrange("(c p) d -> p c d", p=128), w=[C.WD16_tok[l]])


def phase_A(C):
    nc, P, T, I = C.nc, C.P, C.T, C.I
    with ExitStack() as es:
        xin = [P.sb([128, D], F32, "xin", es) for _ in range(2)]
        xin_tok = [Tok(), Tok()]
        st = [P.sb([128, 8, 512], BF16, "ast", es) for _ in range(2)]
        st_tok = [Tok(), Tok()]
        z = P.sb([128, 8, 1], BF16, "zc", es)
        zt = Tok()
        P.op("dve", lambda e: e.memset(z[:], 0.0), w=[zt])
        P.dma("sp", XTv(C.XT0)[:, :, 0:1], z[:], r=[zt], w=[C.XT0_z], allow_slow_non_contiguous=True)
        for s in range(C.NS):
            sb_ = st[s % 2]
            for j in range(4):
                i = s * 4 + j
                xb = xin[i % 2]
                xt = xin_tok[i % 2]
                P.dma("sp", xb[:], I["x"][i * 128:(i + 1) * 128, :], w=[xt])
                for half in range(2):
                    bt, bk = C.bank()
                    for c4 in range(4):
                        c = half * 4 + c4
                        P.op("pe", lambda e: e.transpose(bt[:, c4 * 128:(c4 + 1) * 128], xb[:, c * 128:(c + 1) * 128], C.ident[:]),
                             r=[xt, C.ident_tok], w=[bk], inc=(c4 == 3))
                    en = "act" if half == 0 else "dve"
                    src = bt[:, :].rearrange("p (c t) -> p c t", c=4)
                    dst = sb_[:, half * 4:(half + 1) * 4, j * 128:(j + 1) * 128]
                    if en == "act":
                        P.op("act", lambda e: e.copy(dst, src), r=[bk], w=[st_tok[s % 2]])
                    else:
                        P.op("dve", lambda e: e.tensor_copy(dst, src), r=[bk], w=[st_tok[s % 2]])
            P.dma("sp", XTv(C.XT0)[:, :, 1 + s * 512:1 + (s + 1) * 512], sb_[:], r=[st_tok[s % 2]], w=[C.XT0_tok[s]])
        P.barrier()


class TL:
    def __init__(self, t, k=None):
        self.t = t
        self.k = k or Tok()

    def __getitem__(self, idx):
        return self.t[idx]


def mk(P, shape, dt=F32, name=None, es=None):
    return TL(P.sb(shape, dt, name, es))


def bcast_load(C, dst_ap, src_row, np_, tok, q="sp"):
    C.P.dma(q, dst_ap, src_row.partition_broadcast(np_), w=[tok])


def hv(ap, h):
    return ap.rearrange("p (h v) -> p h v", h=h)


def phase_rwkv(C):
    nc, P, T, I = C.nc, C.P, C.T, C.I
    mm = ALU.mult
    with ExitStack() as es:
        W1 = mk(P, [128, 8, A_COLS], BF16, "W1", es)
        W2 = mk(P, [128, 8, A_COLS], BF16, "W2", es)
        with ExitStack() as es2:
            mub = mk(P, [128, A_COLS], F32, "mub", es2)
            omu = mk(P, [128, A_COLS], F32, "omu", es2)
            stg = [mk(P, [128, A_COLS], F32, "wstg", es2) for _ in range(2)]
            bcast_load(C, mub[:], I["a_mu"], 128, mub.k)
            P.op("dve", lambda e: e.tensor_scalar(omu[:], mub[:], -1.0, 1.0, ALU.mult, ALU.add), r=[mub.k], w=[omu.k])
            for c in range(8):
                s_ = stg[c % 2]
                P.dma("sp", s_[:], I["w_in_even"][c * 128:(c + 1) * 128, 0:A_COLS], w=[s_.k])
                P.op("dve", lambda e: e.tensor_tensor(W1[:, c, :], s_[:], omu[:], mm), r=[s_.k, omu.k], w=[W1.k])
                P.op("pool", lambda e: e.tensor_tensor(W2[:, c, :], s_[:], mub[:], mm), r=[s_.k, mub.k], w=[W2.k])
            P.barrier()
        LW = mk(P, [128, 512], BF16, "LW", es)
        G2 = mk(P, [128, 512], BF16, "G2", es)
        P.dma("pool", LW[0:64, :], I["a_w2"], w=[LW.k])
        P.dma("pool", LW[64:128, :], I["a_a2"], w=[LW.k])
        P.dma("pool", G2[:], I["a_g2"], w=[G2.k])
        BV = mk(P, [64, 7, 512], F32, "BV", es)
        for n, name in enumerate(("a_w0", "a_a0", "a_kk_scale", "a_ka_scale", "a_r_k", "a_gn_g", "a_gn_b")):
            bcast_load(C, BV[:, n, :], I[name], 64, BV.k)
        w0b, a0b, kksb, kab, rkb, gngb, gnbb = [BV[:, n, :] for n in range(7)]
        tri = mk(P, [64, 3, 64], F32, "tri", es)
        ncol = mk(P, [64, 1], F32, "ncol", es)
        mMA = mk(P, [64, 8, 128], F32, "mMA", es)
        mBB = mk(P, [64, 8, 128], F32, "mBB", es)
        mNT = mk(P, [64, 8, 64], F32, "mNT", es)
        id8 = mk(P, [64, 8, 64], F32, "id8", es)
        for tl, nm in ((tri, "c_tri64"), (ncol, "c_ncol64"), (mMA, "c_mMA"), (mBB, "c_mBB"), (mNT, "c_mNT"), (id8, "c_id8")):
            P.dma("sp", tl[:], I[nm], w=[tl.k])
        id64 = C.ident[0:64, 0:64]

        def wt(name, shape=(64, 512), dt=F32):
            return mk(P, list(shape), dt, name, es)
        ATs = [mk(P, [128, 8, 513], BF16, "ATs", es) for _ in range(2)]
        TX = wt("TX", (128, 512), BF16)
        SG = wt("SG", (128, 512), BF16)
        r_, k_, v_, sg, a_, kk, be, Bi, Ki, tmp = [wt(n) for n in ("r", "k", "v", "sg", "a", "kk", "be", "Bi", "Ki", "tmp")]
        Ep, Em, Ex, Ee = [wt(n) for n in ("Ep", "Em", "Ex", "Ee")]
        s8 = [wt("s8_%d" % n, (64, 8)) for n in range(4)]
        KRs = [wt("KR", (64, 8, 128), BF16) for _ in range(2)]
        BiT = wt("BiT", (64, 8, 64), BF16)
        KiT = wt("KiT", (64, 8, 64), BF16)
        MAs = [wt("MA", (64, 8, 128), BF16) for _ in range(2)]
        BBs = [wt("BB", (64, 8, 128), BF16) for _ in range(2)]
        Xb = [wt("X%d" % n, (64, 8, 64), BF16) for n in range(2)]
        XTb = [wt("XT%d" % n, (64, 8, 64), BF16) for n in range(2)]
        Qbs = [[wt("Q%d" % n, (64, 8, 64), BF16) for n in range(2)] for _ in range(2)]
        Xs = wt("Xs", (64, 8, 64), BF16)
        nU = wt("nU", (64, 8, 64), BF16)
        Y = wt("Y")
        tmpb = wt("tmpb")
        Hs = [wt("H%d" % n, (64, 8, 64)) for n in range(2)]
        Hbs = [wt("Hb%d" % n, (64, 8, 64), BF16) for n in range(2)]
        vbs = [wt("vb", (64, 512), BF16) for _ in range(2)]
        Ke16s = [wt("Ke16", (64, 512), BF16) for _ in range(2)]
        Be16s = [wt("Be16", (64, 512), BF16) for _ in range(2)]
        PCs = [wt("PC", (64, 8)) for _ in range(2)]
        gs_ = [wt("g", (64, 512)) for _ in range(2)]
        bonuss = [wt("bonus", (64, 512)) for _ in range(2)]
        P.op("pool", lambda e: e.memset(Hbs[0][:], 0.0), w=[Hbs[0].k])
        yst = [mk(P, [128, 4, 512], BF16, "yst", es) for _ in range(2)]
        P.op("dve", lambda e: e.memset(Hs[0][:], 0.0), w=[Hs[0].k])

        def psb():
            t, k = C.bank()
            return TL(t, k)

        def v3(tl_or_ap, h=8):
            return hv(tl_or_ap, h)

        def chunk(s, ci):
          at = ATs[s % 2]
          ys = yst[s % 2]
          if ci == 0:
            rd = [C.XT0_tok[s]] + ([C.XT0_tok[s - 1]] if s > 0 else [C.XT0_z])
            P.dma("sp", at[:], XTv(C.XT0)[:, :, s * 512:s * 512 + 513], r=rd, w=[at.k])
            for which in range(2):
                pb = psb()
                c0 = 1536 + which * 128
                for c in range(8):
                    P.op("pe", lambda e: e.matmul(pb[:, :], lhsT=W1[:, c, c0:c0 + 128], rhs=at[:, c, 1:513], start=(c == 0), stop=False),
                         r=[W1.k, at.k], w=[pb.k], inc=False)
                for c in range(8):
                    P.op("pe", lambda e: e.matmul(pb[:, :], lhsT=W2[:, c, c0:c0 + 128], rhs=at[:, c, 0:512], start=False, stop=(c == 7)),
                         r=[W2.k, at.k], w=[pb.k], inc=(c == 7))
                if which == 0:
                    P.op("act", lambda e: e.activation(out=TX[0:64, :], in_=pb[0:64, :], func=AF.Tanh), r=[pb.k], w=[TX.k])
                    P.op("act", lambda e: e.copy(TX[64:128, :], pb[64:128, :]), r=[pb.k], w=[TX.k])
                else:
                    P.op("act", lambda e: e.activation(out=SG[:, :], in_=pb[:, :], func=AF.Sigmoid), r=[pb.k], w=[SG.k])
          if True:
            if True:
                g = s * 8 + ci
                t0 = ci * 64
                KR, MA, BB, Qb = KRs[g % 2], MAs[g % 2], BBs[g % 2], Qbs[g % 2]
                vb, Ke16, Be16, PC, g_, bonus = vbs[g % 2], Ke16s[g % 2], Be16s[g % 2], PCs[g % 2], gs_[g % 2], bonuss[g % 2]
                pr, pk, pv = psb(), psb(), psb()
                for pb, c0 in ((pr, 0), (pk, 512), (pv, 1024)):
                    for c in range(8):
                        P.op("pe", lambda e: e.matmul(pb[0:64, :], lhsT=at[:, c, 1 + t0:1 + t0 + 64], rhs=W1[:, c, c0:c0 + 512], start=(c == 0), stop=False),
                             r=[W1.k, at.k], w=[pb.k], inc=False)
                    for c in range(8):
                        P.op("pe", lambda e: e.matmul(pb[0:64, :], lhsT=at[:, c, t0:t0 + 64], rhs=W2[:, c, c0:c0 + 512], start=False, stop=(c == 7)),
                             r=[W2.k, at.k], w=[pb.k], inc=(c == 7))
                yield "F"
                pz, pza, pg = psb(), psb(), psb()
                P.op("pe", lambda e: e.matmul(pz[0:64, :], lhsT=TX[0:64, t0:t0 + 64], rhs=LW[0:64, :], start=True, stop=True), r=[TX.k, LW.k], w=[pz.k])
                P.op("pe", lambda e: e.matmul(pza[0:64, :], lhsT=TX[64:128, t0:t0 + 64], rhs=LW[64:128, :], start=True, stop=True), r=[TX.k, LW.k], w=[pza.k])
                P.op("pe", lambda e: e.matmul(pg[0:64, :], lhsT=SG[:, t0:t0 + 64], rhs=G2[:, :], start=True, stop=True), r=[SG.k, G2.k], w=[pg.k])
                P.op("act", lambda e: e.copy(r_[:], pr[0:64, :]), r=[pr.k], w=[r_.k])
                P.op("act", lambda e: e.copy(v_[:], pv[0:64, :]), r=[pv.k], w=[v_.k])
                P.op("act", lambda e: e.copy(vb[:], pv[0:64, :]), r=[pv.k], w=[vb.k])
                P.op("act", lambda e: e.copy(g_[:], pg[0:64, :]), r=[pg.k], w=[g_.k])
                P.op("dve", lambda e: e.tensor_copy(k_[:], pk[0:64, :]), r=[pk.k], w=[k_.k])
                P.op("dve", lambda e: e.tensor_tensor(sg[:], pz[0:64, :], w0b, ALU.add), r=[pz.k, BV.k], w=[sg.k])
                P.op("act", lambda e: e.activation(out=sg[:], in_=sg[:], func=AF.Sigmoid), r=[sg.k], w=[sg.k])
                P.op("dve", lambda e: e.tensor_tensor(a_[:], pza[0:64, :], a0b, ALU.add), r=[pza.k, BV.k], w=[a_.k])
                P.op("act", lambda e: e.activation(out=a_[:], in_=a_[:], func=AF.Sigmoid), r=[a_.k], w=[a_.k])
                yield "F"
                P.op("pool", lambda e: e.tensor_tensor(kk[:], k_[:], kksb, mm), r=[k_.k, BV.k], w=[kk.k])
                P.op("pool", lambda e: e.tensor_tensor(tmp[:], kk[:], kk[:], mm), r=[kk.k], w=[tmp.k])
                P.op("dve", lambda e: e.tensor_reduce(s8[0][:], v3(tmp[:]), AX.X, ALU.add), r=[tmp.k], w=[s8[0].k])
                P.op("dve", lambda e: e.tensor_scalar(s8[0][:], s8[0][:], 1e-24, None, ALU.max), r=[s8[0].k], w=[s8[0].k])
                P.op("act", lambda e: e.activation(out=s8[0][:], in_=s8[0][:], func=AF.Sqrt), r=[s8[0].k], w=[s8[0].k])
                P.op("dve", lambda e: e.reciprocal(s8[0][:], s8[0][:]), r=[s8[0].k], w=[s8[0].k])
                P.op("dve", lambda e: e.tensor_tensor(v3(kk[:]), v3(kk[:]), s8[0][:, :].unsqueeze(2).to_broadcast([64, 8, 64]), mm),
                     r=[kk.k, s8[0].k], w=[kk.k])
                P.op("pool", lambda e: e.tensor_tensor(be[:], kk[:], a_[:], mm), r=[kk.k, a_.k], w=[be.k])
                P.op("dve", lambda e: e.scalar_tensor_tensor(tmp[:], a_[:], -1.0, kab, ALU.add, mm), r=[a_.k, BV.k], w=[tmp.k])
                P.op("dve", lambda e: e.scalar_tensor_tensor(k_[:], tmp[:], 1.0, k_[:], ALU.add, mm), r=[tmp.k, k_.k], w=[k_.k])
                P.op("pool", lambda e: e.tensor_tensor(tmp[:], r_[:], k_[:], mm), r=[r_.k, k_.k], w=[tmp.k])
                P.op("pool", lambda e: e.tensor_tensor(tmp[:], tmp[:], rkb, mm), r=[tmp.k, BV.k], w=[tmp.k])
                P.op("dve", lambda e: e.tensor_reduce(s8[1][:], v3(tmp[:]), AX.X, ALU.add), r=[tmp.k], w=[s8[1].k])
                P.op("dve", lambda e: e.tensor_tensor(v3(bonus[:]), v3(v_[:]), s8[1][:, :].unsqueeze(2).to_broadcast([64, 8, 64]), mm),
                     r=[v_.k, s8[1].k], w=[bonus.k])
                yield "F"
                pcl, pcx, pca, ppc = psb(), psb(), psb(), psb()
                for pb, n in ((pcl, 0), (pcx, 1), (pca, 2)):
                    P.op("pe", lambda e: e.matmul(pb[0:64, :], lhsT=tri[:, n, :], rhs=sg[:], start=True, stop=True), r=[tri.k, sg.k], w=[pb.k])
                for h in range(8):
                    P.op("pe", lambda e: e.matmul(ppc[0:64, h:h + 1], lhsT=sg[:, h * 64:(h + 1) * 64], rhs=ncol[:], start=True, stop=True),
                         r=[sg.k, ncol.k], w=[ppc.k], inc=(h == 7))
                P.op("act", lambda e: e.activation(out=Ep[:], in_=pcl[0:64, :], func=AF.Exp), r=[pcl.k], w=[Ep.k])
                P.op("act", lambda e: e.activation(out=Em[:], in_=pcl[0:64, :], func=AF.Exp, scale=-1.0), r=[pcl.k], w=[Em.k])
                P.op("act", lambda e: e.activation(out=Ex[:], in_=pcx[0:64, :], func=AF.Exp), r=[pcx.k], w=[Ex.k])
                P.op("act", lambda e: e.activation(out=Ee[:], in_=pca[0:64, :], func=AF.Exp), r=[pca.k], w=[Ee.k])
                P.op("act", lambda e: e.activation(out=PC[:], in_=ppc[0:64, 0:8], func=AF.Exp), r=[ppc.k], w=[PC.k])
                P.op("dve", lambda e: e.tensor_tensor(r_[:], r_[:], Ep[:], mm), r=[r_.k, Ep.k], w=[r_.k])
                P.op("pool", lambda e: e.tensor_tensor(kk[:], kk[:], Ex[:], mm), r=[kk.k, Ex.k], w=[kk.k])
                P.op("dve", lambda e: e.tensor_tensor(Bi[:], be[:], Em[:], mm), r=[be.k, Em.k], w=[Bi.k])
                P.op("pool", lambda e: e.tensor_tensor(Ki[:], k_[:], Em[:], mm), r=[k_.k, Em.k], w=[Ki.k])
                P.op("dve", lambda e: e.tensor_tensor(Ke16[:], k_[:], Ee[:], mm), r=[k_.k, Ee.k], w=[Ke16.k])
                P.op("pool", lambda e: e.tensor_tensor(Be16[:], be[:], Ee[:], mm), r=[be.k, Ee.k], w=[Be16.k])
                yield "F"
                for src, dst, off, en in ((kk, KR, 0, "act"), (r_, KR, 64, "dve"), (Bi, BiT, 0, "act"), (Ki, KiT, 0, "dve")):
                    pb = psb()
                    for h in range(8):
                        P.op("pe", lambda e: e.transpose(pb[0:64, h * 64:(h + 1) * 64], src[:, h * 64:(h + 1) * 64], id64),
                             r=[src.k, C.ident_tok], w=[pb.k], inc=(h == 7))
                    d_ = dst[:, :, off:off + 64]
                    s_ = v3(pb[0:64, :])
                    if en == "act":
                        P.op("act", lambda e: e.copy(d_, s_), r=[pb.k], w=[dst.k])
                    else:
                        P.op("dve", lambda e: e.tensor_copy(d_, s_), r=[pb.k], w=[dst.k])
                yield "F"
                pma = [psb(), psb()]
                pbb = [psb(), psb()]
                pnt = psb()
                for h in range(8):
                    hb, hh = h // 4, h % 4
                    P.op("pe", lambda e: e.matmul(pma[hb][0:64, hh * 128:(hh + 1) * 128], lhsT=BiT[:, h, :], rhs=KR[:, h, :], start=True, stop=True),
                         r=[BiT.k, KR.k], w=[pma[hb].k], inc=(hh == 3))
                for h in range(8):
                    hb, hh = h // 4, h % 4
                    P.op("pe", lambda e: e.matmul(pbb[hb][0:64, hh * 128:(hh + 1) * 128], lhsT=KiT[:, h, :], rhs=KR[:, h, :], start=True, stop=True),
                         r=[KiT.k, KR.k], w=[pbb[hb].k], inc=(hh == 3))
                for h in range(8):
                    P.op("pe", lambda e: e.matmul(pnt[0:64, h * 64:(h + 1) * 64], lhsT=KR[:, h, 0:64], rhs=BiT[:, h, :], start=True, stop=True),
                         r=[BiT.k, KR.k], w=[pnt.k], inc=(h == 7))
                for hb in range(2):
                    P.op("dve", lambda e: e.tensor_tensor(MA[:, hb * 4:(hb + 1) * 4, :], hv(pma[hb][0:64, :], 4), mMA[:, hb * 4:(hb + 1) * 4, :], mm),
                         r=[pma[hb].k, mMA.k], w=[MA.k])
                    P.op("dve", lambda e: e.tensor_tensor(BB[:, hb * 4:(hb + 1) * 4, :], hv(pbb[hb][0:64, :], 4), mBB[:, hb * 4:(hb + 1) * 4, :], mm),
                         r=[pbb[hb].k, mBB.k], w=[BB.k])
                X, XT, Q = Xb[0], XTb[0], Qb[0]
                P.op("dve", lambda e: e.tensor_tensor(XT[:], v3(pnt[0:64, :]), mNT[:], mm), r=[pnt.k, mNT.k], w=[XT.k])
                P.op("pool", lambda e: e.tensor_copy(X[:], MA[:, :, 0:64]), r=[MA.k], w=[X.k])
                P.op("pool", lambda e: e.tensor_tensor(Q[:], MA[:, :, 0:64], id8[:], ALU.add), r=[MA.k, id8.k], w=[Q.k])
                for lvl in range(5):
                    Xn, XTn, Qn = Xb[(lvl + 1) % 2], XTb[(lvl + 1) % 2], Qb[(lvl + 1) % 2]
                    pxt = psb()
                    for h in range(8):
                        P.op("pe", lambda e: e.matmul(pxt[0:64, h * 64:(h + 1) * 64], lhsT=X[:, h, :], rhs=XT[:, h, :], start=True, stop=True),
                             r=[X.k, XT.k], w=[pxt.k], inc=(h == 7))
                    if lvl < 4:
                        px = psb()
                        for h in range(8):
                            P.op("pe", lambda e: e.matmul(px[0:64, h * 64:(h + 1) * 64], lhsT=XT[:, h, :], rhs=X[:, h, :], start=True, stop=True),
                                 r=[X.k, XT.k], w=[px.k], inc=(h == 7))
                    P.op("act", lambda e: e.copy(XTn[:], v3(pxt[0:64, :])), r=[pxt.k], w=[XTn.k])
                    if lvl < 4:
                        P.op("dve", lambda e: e.tensor_copy(Xn[:], v3(px[0:64, :])), r=[px.k], w=[Xn.k])
                    pq = psb()
                    for h in range(8):
                        P.op("pe", lambda e: e.matmul(pq[0:64, h * 64:(h + 1) * 64], lhsT=XTn[:, h, :], rhs=Q[:, h, :], start=True, stop=True),
                             r=[XTn.k, Q.k], w=[pq.k], inc=(h == 7))
                    P.op("dve", lambda e: e.tensor_tensor(Qn[:], Q[:], v3(pq[0:64, :]), ALU.add), r=[Q.k, pq.k], w=[Qn.k])
                    X, XT, Q = Xn, XTn, Qn
                    yield "F"
                yield "END_FRONT"
                H, Hn = Hs[g % 2], Hs[(g + 1) % 2]
                Hb, Hbn = Hbs[g % 2], Hbs[(g + 1) % 2]
                pxs = psb()
                for h in range(8):
                    P.op("pe", lambda e: e.matmul(pxs[0:64, h * 64:(h + 1) * 64], lhsT=KR[:, h, 0:64], rhs=Hb[:, h, :], start=True, stop=False),
                         r=[KR.k, Hb.k], w=[pxs.k], inc=False)
                    P.op("pe", lambda e: e.matmul(pxs[0:64, h * 64:(h + 1) * 64], lhsT=BB[:, h, 0:64], rhs=vb[:, h * 64:(h + 1) * 64], start=False, stop=True),
                         r=[BB.k, vb.k], w=[pxs.k], inc=(h == 7))
                P.op("act", lambda e: e.copy(Xs[:], v3(pxs[0:64, :])), r=[pxs.k], w=[Xs.k])
                yield "B"
                pu = psb()
                for h in range(8):
                    P.op("pe", lambda e: e.matmul(pu[0:64, h * 64:(h + 1) * 64], lhsT=Q[:, h, :], rhs=Xs[:, h, :], start=True, stop=True),
                         r=[Q.k, Xs.k], w=[pu.k], inc=(h == 7))
                P.op("act", lambda e: e.mul(nU[:], v3(pu[0:64, :]), -1.0), r=[pu.k], w=[nU.k])
                yield "B"
                py, ph = psb(), psb()
                for h in range(8):
                    sl = slice(h * 64, (h + 1) * 64)
                    P.op("pe", lambda e: e.matmul(py[0:64, sl], lhsT=KR[:, h, 64:128], rhs=Hb[:, h, :], start=True, stop=False), r=[KR.k, Hb.k], w=[py.k], inc=False)
                    P.op("pe", lambda e: e.matmul(py[0:64, sl], lhsT=BB[:, h, 64:128], rhs=vb[:, sl], start=False, stop=False), r=[BB.k, vb.k], w=[py.k], inc=False)
                    P.op("pe", lambda e: e.matmul(py[0:64, sl], lhsT=MA[:, h, 64:128], rhs=nU[:, h, :], start=False, stop=True), r=[MA.k, nU.k], w=[py.k], inc=(h == 7))
                for h in range(8):
                    sl = slice(h * 64, (h + 1) * 64)
                    P.op("pe", lambda e: e.matmul(ph[0:64, sl], lhsT=Ke16[:, sl], rhs=vb[:, sl], start=True, stop=False), r=[Ke16.k, vb.k], w=[ph.k], inc=False)
                    P.op("pe", lambda e: e.matmul(ph[0:64, sl], lhsT=Be16[:, sl], rhs=nU[:, h, :], start=False, stop=True), r=[Be16.k, nU.k], w=[ph.k], inc=(h == 7))
                P.op("pool", lambda e: e.tensor_tensor(Hn[:], H[:], PC[:, :].unsqueeze(2).to_broadcast([64, 8, 64]), mm), r=[H.k, PC.k], w=[Hn.k])
                P.op("dve", lambda e: e.tensor_tensor(Hn[:], Hn[:], v3(ph[0:64, :]), ALU.add), r=[Hn.k, ph.k], w=[Hn.k])
                P.op("act", lambda e: e.copy(Hbn[:], Hn[:]), r=[Hn.k], w=[Hbn.k])
                yield "B"
                P.op("act", lambda e: e.copy(Y[:], py[0:64, :]), r=[py.k], w=[Y.k])
                P.op("dve", lambda e: e.tensor_reduce(s8[2][:], v3(Y[:]), AX.X, ALU.add), r=[Y.k], w=[s8[2].k])
                P.op("dve", lambda e: e.tensor_scalar(s8[2][:], s8[2][:], 1.0 / 64, None, mm), r=[s8[2].k], w=[s8[2].k])
                P.op("dve", lambda e: e.tensor_tensor(v3(Y[:]), v3(Y[:]), s8[2][:, :].unsqueeze(2).to_broadcast([64, 8, 64]), ALU.subtract),
                     r=[Y.k, s8[2].k], w=[Y.k])
                yield "B"
                P.op("pool", lambda e: e.tensor_tensor(tmpb[:], Y[:], Y[:], mm), r=[Y.k], w=[tmpb.k])
                P.op("dve", lambda e: e.tensor_reduce(s8[3][:], v3(tmpb[:]), AX.X, ALU.add), r=[tmpb.k], w=[s8[3].k])
                P.op("act", lambda e: e.activation(out=s8[3][:], in_=s8[3][:], func=AF.Sqrt, bias=64e-5, scale=1.0 / 64), r=[s8[3].k], w=[s8[3].k])
                P.op("dve", lambda e: e.reciprocal(s8[3][:], s8[3][:]), r=[s8[3].k], w=[s8[3].k])
                P.op("dve", lambda e: e.tensor_tensor(v3(Y[:]), v3(Y[:]), s8[3][:, :].unsqueeze(2).to_broadcast([64, 8, 64]), mm),
                     r=[Y.k, s8[3].k], w=[Y.k])
                P.op("pool", lambda e: e.tensor_tensor(Y[:], Y[:], gngb, mm), r=[Y.k, BV.k], w=[Y.k])
                P.op("pool", lambda e: e.tensor_tensor(Y[:], Y[:], gnbb, ALU.add), r=[Y.k, BV.k], w=[Y.k])
                P.op("dve", lambda e: e.tensor_tensor(Y[:], Y[:], bonus[:], ALU.add), r=[Y.k, bonus.k], w=[Y.k])
                P.op("dve", lambda e: e.tensor_tensor(Y[:], Y[:], g_[:], mm), r=[Y.k, g_.k], w=[Y.k])
                if "dbg_ya" in C.dbg:
                    P.dma("sp", C.dbg["dbg_ya"][g * 64:(g + 1) * 64, :], Y[:], r=[Y.k])
                yield "B"
                pb = psb()
                for q in range(4):
                    P.op("pe", lambda e: e.transpose(pb[:, q * 64:(q + 1) * 64], Y[:, q * 128:(q + 1) * 128], id64), r=[Y.k, C.ident_tok], w=[pb.k], inc=(q == 3))
                P.op("act", lambda e: e.copy(ys[:, :, t0:t0 + 64], hv(pb[:, 0:256], 4)), r=[pb.k], w=[ys.k])
                if ci == 7:
                    P.dma("sp", XTv(C.YT)[:, 0:4, s * 512:(s + 1) * 512], ys[:], r=[ys.k], w=[C.YT_tok[0][s]])

        pipeline2([(lambda s=s, ci=ci: chunk(s, ci)) for s in range(C.NS) for ci in range(8)], interleave=RWKV_INTERLEAVE)
        P.barrier()


def pipeline2(makers, interleave=True):
    if not interleave:
        for mk_ in makers:
            for _ in mk_():
                pass
        return
    prevB = None
    for mk_ in makers:
        g = mk_()
        while True:
            r = next(g)
            if prevB is not None:
                try:
                    next(prevB)
                except StopIteration:
                    prevB = None
            if r == "END_FRONT":
                break
        if prevB is not None:
            for _ in prevB:
                pass
        prevB = g
    if prevB is not None:
        for _ in prevB:
            pass


def psb(C):
    t, k = C.bank()
    return TL(t, k)


def phase_gla(C):
    nc, P, T, I = C.nc, C.P, C.T, C.I
    mm = ALU.mult
    with ExitStack() as es:
        WB = mk(P, [128, 8, B_COLS], BF16, "WB", es)
        for c in range(8):
            P.dma("pool", WB[:, c, :], I["w_in_even"][c * 128:(c + 1) * 128, A_COLS:EVEN_COLS], w=[WB.k])
        GW2 = mk(P, [16, 256], BF16, "GW2", es)
        P.dma("pool", GW2[:], I["b_gate_w2"], w=[GW2.k])
        gbb = mk(P, [128, 256], F32, "gbb", es)
        ngb = mk(P, [128, 512], F32, "ngb", es)
        bcast_load(C, gbb[:], I["b_gate_b"], 128, gbb.k)
        bcast_load(C, ngb[:], I["b_norm_g"], 128, ngb.k)
        tri = mk(P, [128, 2, 128], F32, "tri128", es)
        ncol = mk(P, [128, 1], F32, "ncol128", es)
        iu = mk(P, [128, 4, 128], F32, "iu128", es)
        for tl, nm in ((tri, "c_tri128"), (ncol, "c_ncol128"), (iu, "c_iu128")):
            P.dma("sp", tl[:], I[nm], w=[tl.k])
        id64 = C.ident[0:64, 0:64]

        def wt(name, shape, dt=F32):
            return mk(P, list(shape), dt, name, es)
        ATs = [wt("ATg", (128, 8, 512), BF16) for _ in range(2)]
        AL = wt("AL", (16, 512), BF16)
        l_ = wt("l", (128, 256))
        Eq, Ei, Ee = wt("Eq", (128, 256)), wt("Ei", (128, 256)), wt("Ee", (128, 256))
        PCg = wt("PCg", (64, 4))
        qd, ki, ke = wt("qd", (128, 256)), wt("ki", (128, 256)), wt("ke", (128, 256))
        v_ = wt("vg", (128, 512))
        qdT, kiT = wt("qdT", (64, 4, 128)), wt("kiT", (64, 4, 128))
        attT = wt("attT", (128, 4, 128))
        Ss = [wt("S%d" % n, (64, 4, 128)) for n in range(2)]
        o_ = wt("o", (128, 512))
        sq = wt("sqg", (128, 512))
        sl_ = wt("silu", (128, 512))
        m4 = wt("m4", (128, 4))
        yst = [wt("ystg", (128, 4, 512), BF16) for _ in range(2)]
        P.op("dve", lambda e: e.memset(Ss[0][:], 0.0), w=[Ss[0].k])
        for s in range(C.NS):
            at = ATs[s % 2]
            P.dma("sp", at[:], XTv(C.XT0)[:, :, 1 + s * 512:1 + (s + 1) * 512], r=[C.XT0_tok[s]], w=[at.k])
            pb = psb(C)
            for c in range(8):
                P.op("pe", lambda e: e.matmul(pb[0:16, :], lhsT=WB[:, c, 1536:1552], rhs=at[:, c, :], start=(c == 0), stop=(c == 7)),
                     r=[WB.k, at.k], w=[pb.k], inc=(c == 7))
            P.op("act", lambda e: e.copy(AL[:], pb[0:16, :]), r=[pb.k], w=[AL.k])
            ys = yst[s % 2]
            for ci in range(4):
                g = s * 4 + ci
                t0 = ci * 128
                pqk, pv, pg = psb(C), psb(C), psb(C)
                for pb, c0 in ((pqk, 0), (pv, 512), (pg, 1024)):
                    for c in range(8):
                        P.op("pe", lambda e: e.matmul(pb[:, :], lhsT=at[:, c, t0:t0 + 128], rhs=WB[:, c, c0:c0 + 512], start=(c == 0), stop=(c == 7)),
                             r=[WB.k, at.k], w=[pb.k], inc=(c == 7))
                pla = psb(C)
                P.op("pe", lambda e: e.matmul(pla[:, 0:256], lhsT=AL[:, t0:t0 + 128], rhs=GW2[:], start=True, stop=True), r=[AL.k, GW2.k], w=[pla.k])
                P.op("dve", lambda e: e.tensor_tensor(l_[:], pla[:, 0:256], gbb[:], ALU.add), r=[pla.k, gbb.k], w=[l_.k])
                P.op("act", lambda e: e.activation(out=l_[:], in_=l_[:], func=AF.Exp, scale=-1.0), r=[l_.k], w=[l_.k])
                P.op("act", lambda e: e.activation(out=l_[:], in_=l_[:], func=AF.Ln, bias=1.0), r=[l_.k], w=[l_.k])
                pbc, pba, ppc = psb(C), psb(C), psb(C)
                P.op("pe", lambda e: e.matmul(pbc[:, 0:256], lhsT=tri[:, 0, :], rhs=l_[:], start=True, stop=True), r=[tri.k, l_.k], w=[pbc.k])
                P.op("pe", lambda e: e.matmul(pba[:, 0:256], lhsT=tri[:, 1, :], rhs=l_[:], start=True, stop=True), r=[tri.k, l_.k], w=[pba.k])
                for h in range(4):
                    P.op("pe", lambda e: e.matmul(ppc[0:64, h:h + 1], lhsT=l_[:, h * 64:(h + 1) * 64], rhs=ncol[:], start=True, stop=True),
                         r=[l_.k, ncol.k], w=[ppc.k], inc=(h == 3))
                P.op("act", lambda e: e.activation(out=Eq[:], in_=pbc[:, 0:256], func=AF.Exp), r=[pbc.k], w=[Eq.k])
                P.op("act", lambda e: e.activation(out=Ei[:], in_=pbc[:, 0:256], func=AF.Exp, scale=-1.0), r=[pbc.k], w=[Ei.k])
                P.op("act", lambda e: e.activation(out=Ee[:], in_=pba[:, 0:256], func=AF.Exp), r=[pba.k], w=[Ee.k])
                P.op("act", lambda e: e.activation(out=PCg[:], in_=ppc[0:64, 0:4], func=AF.Exp), r=[ppc.k], w=[PCg.k])
                P.op("dve", lambda e: e.scalar_tensor_tensor(qd[:], pqk[:, 0:256], 0.125, Eq[:], mm, mm), r=[pqk.k, Eq.k], w=[qd.k])
                P.op("dve", lambda e: e.tensor_tensor(ki[:], pqk[:, 256:512], Ei[:], mm), r=[pqk.k, Ei.k], w=[ki.k])
                P.op("dve", lambda e: e.tensor_tensor(ke[:], pqk[:, 256:512], Ee[:], mm), r=[pqk.k, Ee.k], w=[ke.k])
                P.op("act", lambda e: e.copy(v_[:], pv[:, :]), r=[pv.k], w=[v_.k])
                P.op("act", lambda e: e.activation(out=sl_[:], in_=pg[:, :], func=AF.Silu), r=[pg.k], w=[sl_.k])
                for src, dst, en in ((qd, qdT, "act"), (ki, kiT, "dve")):
                    pb = psb(C)
                    for h in range(4):
                        P.op("pe", lambda e: e.transpose(pb[0:64, h * 128:(h + 1) * 128], src[:, h * 64:(h + 1) * 64], C.ident[:]),
                             r=[src.k, C.ident_tok], w=[pb.k], inc=(h == 3))
                    if en == "act":
                        P.op("act", lambda e: e.copy(dst[:], hv(pb[0:64, :], 4)), r=[pb.k], w=[dst.k])
                    else:
                        P.op("dve", lambda e: e.tensor_copy(dst[:], hv(pb[0:64, :], 4)), r=[pb.k], w=[dst.k])
                patt = psb(C)
                for h in range(4):
                    P.op("pe", lambda e: e.matmul(patt[:, h * 128:(h + 1) * 128], lhsT=kiT[:, h, :], rhs=qdT[:, h, :], start=True, stop=True),
                         r=[kiT.k, qdT.k], w=[patt.k], inc=(h == 3))
                P.op("dve", lambda e: e.tensor_tensor(attT[:], hv(patt[:, :], 4), iu[:], mm), r=[patt.k, iu.k], w=[attT.k])
                S, Sn = Ss[g % 2], Ss[(g + 1) % 2]
                po, pS = psb(C), psb(C)
                for h in range(4):
                    sl = slice(h * 128, (h + 1) * 128)
                    P.op("pe", lambda e: e.matmul(po[:, sl], lhsT=attT[:, h, :], rhs=v_[:, sl], start=True, stop=False), r=[attT.k, v_.k], w=[po.k], inc=False)
                    P.op("pe", lambda e: e.matmul(po[:, sl], lhsT=qdT[:, h, :], rhs=S[:, h, :], start=False, stop=True), r=[qdT.k, S.k], w=[po.k], inc=(h == 3))
                for h in range(4):
                    sl = slice(h * 128, (h + 1) * 128)
                    P.op("pe", lambda e: e.matmul(pS[0:64, sl], lhsT=ke[:, h * 64:(h + 1) * 64], rhs=v_[:, sl], start=True, stop=True), r=[ke.k, v_.k], w=[pS.k], inc=(h == 3))
                P.op("pool", lambda e: e.tensor_tensor(Sn[:], S[:], PCg[:, :].unsqueeze(2).to_broadcast([64, 4, 128]), mm), r=[S.k, PCg.k], w=[Sn.k])
                P.op("dve", lambda e: e.tensor_tensor(Sn[:], Sn[:], hv(pS[0:64, :], 4), ALU.add), r=[Sn.k, pS.k], w=[Sn.k])
                P.op("act", lambda e: e.copy(o_[:], po[:, :]), r=[po.k], w=[o_.k])
                P.op("pool", lambda e: e.tensor_tensor(sq[:], o_[:], o_[:], mm), r=[o_.k], w=[sq.k])
                P.op("dve", lambda e: e.tensor_reduce(m4[:], hv(sq[:], 4), AX.X, ALU.add), r=[sq.k], w=[m4.k])
                P.op("act", lambda e: e.activation(out=m4[:], in_=m4[:], func=AF.Sqrt, bias=1e-5, scale=1.0 / 128), r=[m4.k], w=[m4.k])
                P.op("dve", lambda e: e.reciprocal(m4[:], m4[:]), r=[m4.k], w=[m4.k])
                P.op("dve", lambda e: e.tensor_tensor(hv(o_[:], 4), hv(o_[:], 4), m4[:, :].unsqueeze(2).to_broadcast([128, 4, 128]), mm), r=[o_.k, m4.k], w=[o_.k])
                P.op("pool", lambda e: e.tensor_tensor(o_[:], o_[:], ngb[:], mm), r=[o_.k, ngb.k], w=[o_.k])
                P.op("dve", lambda e: e.tensor_tensor(o_[:], o_[:], sl_[:], mm), r=[o_.k, sl_.k], w=[o_.k])
                if "dbg_yb" in C.dbg:
                    P.dma("sp", C.dbg["dbg_yb"][g * 128:(g + 1) * 128, :], o_[:], r=[o_.k])
                pb = psb(C)
                for q in range(4):
                    P.op("pe", lambda e: e.transpose(pb[:, q * 128:(q + 1) * 128], o_[:, q * 128:(q + 1) * 128], C.ident[:]), r=[o_.k, C.ident_tok], w=[pb.k], inc=(q == 3))
                P.op("act", lambda e: e.copy(ys[:, :, t0:t0 + 128], hv(pb[:, :], 4)), r=[pb.k], w=[ys.k])
            P.dma("sp", XTv(C.YT)[:, 4:8, s * 512:(s + 1) * 512], ys[:], r=[ys.k], w=[C.YT_tok[1][s]])
        P.barrier()


def ln_inplace(C, xt, gb, bb, st, junk):
    P = C.P
    P.op("dve", lambda e: e.tensor_reduce(st[:, 0:1], xt[:], AX.X, ALU.add), r=[xt.k], w=[st.k])
    P.op("dve", lambda e: e.tensor_scalar(st[:, 0:1], st[:, 0:1], 1.0 / D, None, ALU.mult), r=[st.k], w=[st.k])
    P.op("dve", lambda e: e.tensor_scalar(xt[:], xt[:], st[:, 0:1], None, ALU.subtract), r=[xt.k, st.k], w=[xt.k])
    P.op("act", lambda e: e.activation(out=junk[:], in_=xt[:], func=AF.Square, accum_out=st[:, 1:2]), r=[xt.k], w=[junk.k, st.k])
    P.op("act", lambda e: e.activation(out=st[:, 1:2], in_=st[:, 1:2], func=AF.Sqrt, bias=LN_EPS, scale=1.0 / D), r=[st.k], w=[st.k])
    P.op("dve", lambda e: e.reciprocal(st[:, 1:2], st[:, 1:2]), r=[st.k], w=[st.k])
    P.op("dve", lambda e: e.scalar_tensor_tensor(xt[:], xt[:], st[:, 1:2], gb[:], ALU.mult, ALU.mult), r=[xt.k, st.k, gb.k], w=[xt.k])
    P.op("pool", lambda e: e.tensor_tensor(xt[:], xt[:], bb[:], ALU.add), r=[xt.k, bb.k], w=[xt.k])


def tile_to_stage(C, xt, stage, j):
    P = C.P
    for half in range(2):
        pb = psb(C)
        for c4 in range(4):
            c = half * 4 + c4
            P.op("pe", lambda e: e.transpose(pb[:, c4 * 128:(c4 + 1) * 128], xt[:, c * 128:(c + 1) * 128], C.ident[:]),
                 r=[xt.k, C.ident_tok], w=[pb.k], inc=(c4 == 3))
        dst = stage[:, half * 4:(half + 1) * 4, j * 128:(j + 1) * 128]
        src = hv(pb[:, :], 4)
        if half == 0:
            P.op("act", lambda e: e.copy(dst, src), r=[pb.k], w=[stage.k])
        else:
            P.op("dve", lambda e: e.tensor_copy(dst, src), r=[pb.k], w=[stage.k])


def phase_outproj(C, w_out, srcYT, srcYT_toks, resid, resid_toks, lng, lnb, dstH, dstH_tok, dstHT, dstHT_tok, dbgname=None):
    nc, P, T, I = C.nc, C.P, C.T, C.I
    with ExitStack() as es:
        WO = mk(P, [128, 8, D], BF16, "WO", es)
        for c in range(8):
            P.dma("pool", WO[:, c, :], w_out[c * 128:(c + 1) * 128, :], w=[WO.k])
        gb = mk(P, [128, D], F32, "lng", es)
        bb = mk(P, [128, D], F32, "lnb", es)
        bcast_load(C, gb[:], lng, 128, gb.k)
        bcast_load(C, bb[:], lnb, 128, bb.k)
        yts = [mk(P, [128, 8, 512], BF16, "yt", es) for _ in range(2)]
        xts = [mk(P, [128, D], F32, "xres", es) for _ in range(2)]
        sts = [mk(P, [128, 2], F32, "lnst", es) for _ in range(2)]
        junk = mk(P, [128, D], BF16, "junk", es)
        stg = [mk(P, [128, 8, 512], BF16, "hstg", es) for _ in range(2)]
        for s in range(C.NS):
            yt = yts[s % 2]
            P.dma("sp", yt[:], XTv(srcYT)[:, :, s * 512:(s + 1) * 512], r=srcYT_toks(s), w=[yt.k])
            sg_ = stg[s % 2]
            for j in range(4):
                i = s * 4 + j
                xt = xts[i % 2]
                P.dma("sp", xt[:], resid[i * 128:(i + 1) * 128, :], r=resid_toks(i), w=[xt.k])
                for half in range(2):
                    pb = psb(C)
                    for c in range(8):
                        P.op("pe", lambda e: e.matmul(pb[:, :], lhsT=yt[:, c, j * 128:(j + 1) * 128], rhs=WO[:, c, half * 512:(half + 1) * 512], start=(c == 0), stop=(c == 7)),
                             r=[yt.k, WO.k], w=[pb.k], inc=(c == 7))
                    P.op("dve", lambda e: e.scalar_tensor_tensor(xt[:, half * 512:(half + 1) * 512], xt[:, half * 512:(half + 1) * 512], DN_ALPHA, pb[:, :], ALU.mult, ALU.add),
                         r=[xt.k, pb.k], w=[xt.k])
                ln_inplace(C, xt, gb, bb, sts[i % 2], junk)
                P.dma("sp", dstH[i * 128:(i + 1) * 128, :], xt[:], r=[xt.k], w=[dstH_tok[i]])
                tile_to_stage(C, xt, sg_, j)
            P.dma("sp", XTv(dstHT)[:, :, s * 512:(s + 1) * 512], sg_[:], r=[sg_.k], w=[dstHT_tok[s]])
        P.barrier()


def phase_moe(C, l, srcH, srcH_tok, srcHT, srcHT_tok, dstX, dstX_tok, dstXT, dstXT_tok):
    nc, P, T, I = C.nc, C.P, C.T, C.I
    mm = ALU.mult
    with ExitStack() as es:
        WD = mk(P, [128, 32, D], BF16, "WD", es)
        for c4 in range(4):
            P.dma("sp", WD[:, c4 * 8:(c4 + 1) * 8, :], C.WD16[l].rearrange("p (c d) -> p c d", c=32)[:, c4 * 8:(c4 + 1) * 8, :], r=[C.WD16_tok[l]], w=[WD.k])
        RW = mk(P, [128, 8, NE], BF16, "RW", es)
        P.dma("pool", RW[:], I["router_w"].rearrange("(c p) e -> p c e", p=128), w=[RW.k])
        rbb = mk(P, [128, NE], F32, "rbb", es)
        bcast_load(C, rbb[:], I["router_bias"], 128, rbb.k)
        SEL = mk(P, [16, 16, 128], BF16, "SEL", es)
        P.dma("pool", SEL[:], I["c_sel"], w=[SEL.k])
        gb = mk(P, [128, D], F32, "lng", es)
        bb = mk(P, [128, D], F32, "lnb", es)
        bcast_load(C, gb[:], I["ln2_g"][l:l + 1, :], 128, gb.k)
        bcast_load(C, bb[:], I["ln2_b"][l:l + 1, :], 128, bb.k)
        hts = [mk(P, [128, 8, 512], BF16, "hT", es) for _ in range(2)]
        xts = [mk(P, [128, D], F32, "hres", es) for _ in range(2)]
        sts = [mk(P, [128, 2], F32, "lnst", es) for _ in range(2)]
        junk = mk(P, [128, D], BF16, "junk", es)
        stg = [mk(P, [128, 8, 512], BF16, "xstg", es) for _ in range(2)]
        actT = mk(P, [128, 32, 512], BF16, "actT", es)
        combT = mk(P, [16, 512], BF16, "combT", es)
        WGUs = [mk(P, [128, 2, 8, DE], BF16, "WGU", es) for _ in range(3)]
        sgl = [mk(P, [128, 512], F32, "sgl", es) for _ in range(2)]
        s_ = mk(P, [128, NE], F32, "rs", es)
        sel = mk(P, [128, NE], F32, "rsel", es)
        pr = mk(P, [128, 4, 6], F32, "rpr", es)
        gs = mk(P, [128, 4], F32, "rgs", es)
        t1 = mk(P, [128, 4], F32, "rt1", es)
        m1 = mk(P, [128, 2], F32, "rm1", es)
        selm = mk(P, [128, NE], F32, "rselm", es)
        sel2 = mk(P, [128, NE], F32, "rsel2", es)
        comb = mk(P, [128, NE], F32, "rcomb", es)
        nwl = [0]

        def load_w(e):
            b = nwl[0] % 3
            nwl[0] += 1
            P.dma("sp", WGUs[b][:], C.WGU16[l][e].rearrange("p (t c f) -> p t c f", t=2, c=8), r=[C.WGU16_tok[l][e]], w=[WGUs[b].k])
            return WGUs[b]

        def g4(t):
            return t[:, :].rearrange("p (g e) -> p g e", g=4)

        for s in range(C.NS):
            hT = hts[s % 2]
            P.dma("sp", hT[:], XTv(srcHT)[:, :, s * 512:(s + 1) * 512], r=[srcHT_tok[s]], w=[hT.k])
            for j in range(4):
                plg = psb(C)
                for c in range(8):
                    P.op("pe", lambda e: e.matmul(plg[:, 0:NE], lhsT=hT[:, c, j * 128:(j + 1) * 128], rhs=RW[:, c, :], start=(c == 0), stop=(c == 7)),
                         r=[hT.k, RW.k], w=[plg.k], inc=(c == 7))
                P.op("act", lambda e: e.activation(out=s_[:], in_=plg[:, 0:NE], func=AF.Sigmoid), r=[plg.k], w=[s_.k])
                P.op("dve", lambda e: e.tensor_tensor(sel[:], s_[:], rbb[:], ALU.add), r=[s_.k, rbb.k], w=[sel.k])
                s4 = g4(sel)
                P.op("dve", lambda e: e.tensor_tensor(pr[:, :, 0:3], s4[:, :, 0:3], s4[:, :, 1:4], ALU.add), r=[sel.k], w=[pr.k])
                P.op("dve", lambda e: e.tensor_tensor(pr[:, :, 3:5], s4[:, :, 0:2], s4[:, :, 2:4], ALU.add), r=[sel.k], w=[pr.k])
                P.op("dve", lambda e: e.tensor_tensor(pr[:, :, 5:6], s4[:, :, 0:1], s4[:, :, 3:4], ALU.add), r=[sel.k], w=[pr.k])
                P.op("dve", lambda e: e.tensor_reduce(gs[:], pr[:], AX.X, ALU.max), r=[pr.k], w=[gs.k])
                P.op("dve", lambda e: e.tensor_reduce(m1[:, 0:1], gs[:], AX.X, ALU.max), r=[gs.k], w=[m1.k])
                P.op("dve", lambda e: e.tensor_scalar(gs[:], gs[:], m1[:, 0:1], None, ALU.is_ge), r=[gs.k, m1.k], w=[gs.k])
                P.op("dve", lambda e: e.tensor_scalar(t1[:], gs[:], -1.0, 1e30, ALU.add, ALU.mult), r=[gs.k], w=[t1.k])
                P.op("dve", lambda e: e.tensor_tensor(g4(selm), s4, gs[:, :].unsqueeze(2).to_broadcast([128, 4, 4]), mm), r=[sel.k, gs.k], w=[selm.k])
                P.op("dve", lambda e: e.tensor_tensor(g4(selm), g4(selm), t1[:, :].unsqueeze(2).to_broadcast([128, 4, 4]), ALU.add), r=[selm.k, t1.k], w=[selm.k])
                P.op("dve", lambda e: e.tensor_reduce(m1[:, 0:1], selm[:], AX.X, ALU.max), r=[selm.k], w=[m1.k])
                P.op("dve", lambda e: e.tensor_scalar(sel2[:], selm[:], m1[:, 0:1], None, ALU.is_ge), r=[selm.k, m1.k], w=[sel2.k])
                P.op("dve", lambda e: e.scalar_tensor_tensor(sel2[:], sel2[:], -1e30, selm[:], mm, ALU.add), r=[sel2.k, selm.k], w=[sel2.k])
                P.op("dve", lambda e: e.tensor_reduce(m1[:, 1:2], sel2[:], AX.X, ALU.max), r=[sel2.k], w=[m1.k])
                P.op("dve", lambda e: e.tensor_scalar(sel2[:], selm[:], m1[:, 1:2], None, ALU.is_ge), r=[selm.k, m1.k], w=[sel2.k])
                P.op("dve", lambda e: e.tensor_tensor(comb[:], s_[:], sel2[:], mm), r=[s_.k, sel2.k], w=[comb.k])
                P.op("dve", lambda e: e.tensor_reduce(m1[:, 0:1], comb[:], AX.X, ALU.add), r=[comb.k], w=[m1.k])
                P.op("dve", lambda e: e.reciprocal(m1[:, 0:1], m1[:, 0:1]), r=[m1.k], w=[m1.k])
                P.op("dve", lambda e: e.tensor_scalar(comb[:], comb[:], m1[:, 0:1], None, mm), r=[comb.k, m1.k], w=[comb.k])
                pct = psb(C)
                P.op("pe", lambda e: e.transpose(pct[0:16, 0:128], comb[:, :], C.ident[:]), r=[comb.k, C.ident_tok], w=[pct.k])
                P.op("act", lambda e: e.copy(combT[:, j * 128:(j + 1) * 128], pct[0:16, 0:128]), r=[pct.k], w=[combT.k])
            for ex in range(NE):
                WGU = load_w(ex)
                pcb = psb(C)
                P.op("pe", lambda e: e.matmul(pcb[:, :], lhsT=SEL[:, ex, :], rhs=combT[:, :], start=True, stop=True), r=[SEL.k, combT.k], w=[pcb.k])
                for f in range(2):
                    pG, pU = psb(C), psb(C)
                    for pb, ti in ((pG, 0), (pU, 1)):
                        for c in range(8):
                            P.op("pe", lambda e: e.matmul(pb[:, :], lhsT=WGU[:, ti, c, f * 128:(f + 1) * 128], rhs=hT[:, c, :], start=(c == 0), stop=(c == 7)),
                                 r=[WGU.k, hT.k], w=[pb.k], inc=(c == 7))
                    sg_ = sgl[(ex * 2 + f) % 2]
                    P.op("act", lambda e: e.activation(out=sg_[:], in_=pG[:, :], func=AF.Silu), r=[pG.k], w=[sg_.k])
                    P.op("dve", lambda e: e.tensor_tensor(sg_[:], sg_[:], pU[:, :], mm), r=[sg_.k, pU.k], w=[sg_.k])
                    P.op("dve", lambda e: e.tensor_tensor(actT[:, ex * 2 + f, :], sg_[:], pcb[:, :], mm), r=[sg_.k, pcb.k], w=[actT.k])
            xs_ = stg[s % 2]
            for j in range(4):
                i = s * 4 + j
                xt = xts[i % 2]
                P.dma("sp", xt[:], srcH[i * 128:(i + 1) * 128, :], r=[srcH_tok[i]], w=[xt.k])
                for half in range(2):
                    pb = psb(C)
                    for c in range(32):
                        P.op("pe", lambda e: e.matmul(pb[:, :], lhsT=actT[:, c, j * 128:(j + 1) * 128], rhs=WD[:, c, half * 512:(half + 1) * 512], start=(c == 0), stop=(c == 31)),
                             r=[actT.k, WD.k], w=[pb.k], inc=(c == 31))
                    P.op("dve", lambda e: e.scalar_tensor_tensor(xt[:, half * 512:(half + 1) * 512], xt[:, half * 512:(half + 1) * 512], DN_ALPHA, pb[:, :], ALU.mult, ALU.add),
                         r=[xt.k, pb.k], w=[xt.k])
                ln_inplace(C, xt, gb, bb, sts[i % 2], junk)
                P.dma("sp", dstX[i * 128:(i + 1) * 128, :], xt[:], r=[xt.k], w=[dstX_tok[i]])
                if dstXT is not None:
                    tile_to_stage(C, xt, xs_, j)
            if dstXT is not None:
                P.dma("sp", XTv(dstXT)[:, :, s * 512:(s + 1) * 512], xs_[:], r=[xs_.k], w=[dstXT_tok[s]])
        P.barrier()


def rope_consts(T):
    pos = np.arange(T, dtype=np.float64)
    c = {}
    for name, half in (("k", 64), ("i", 32)):
        inv = 10000.0 ** (-np.arange(half, dtype=np.float64) / half)
        ang = (pos.astype(np.float32)[:, None] * inv.astype(np.float32)[None, :]).astype(np.float32).astype(np.float64)
        cs = np.cos(ang).astype(np.float32).reshape(T // 128, 128, half).transpose(1, 0, 2)
        sn = np.sin(ang).astype(np.float32).reshape(T // 128, 128, half).transpose(1, 0, 2)
        c["cos_" + name] = np.ascontiguousarray(cs)
        c["sin_" + name] = np.ascontiguousarray(sn)
    q = np.arange(128)[:, None]
    s = np.arange(128)[None, :]
    c["cbias"] = np.where(s <= q, 0.0, -1e30).astype(np.float32)
    c["halfpow"] = (0.5 ** np.arange(1, 33, dtype=np.float64)).astype(np.float32).reshape(1, 32)
    c["tiebias"] = (-1e-6 * np.arange(T, dtype=np.float64)).astype(np.float32).reshape(1, T)
    return c


def rope_tm(C, dst_ap, dst_k, src, src_k, cosb, sinb, nh, half, ta_ap, ta_k, tb_ap, tb_k, rdeps):
    P = C.P
    mm = ALU.mult
    n = nh * 2
    s3 = src.rearrange("p (n f) -> p n f", n=n)
    cb = cosb.unsqueeze(1).to_broadcast([128, n, half])
    sb_ = sinb.unsqueeze(1).to_broadcast([128, n, half])
    a3 = ta_ap.rearrange("p (n f) -> p n f", n=n)
    b3 = tb_ap.rearrange("p (n f) -> p n f", n=n)
    P.op("dve", lambda e: e.tensor_tensor(a3, s3, cb, mm), r=[src_k] + rdeps, w=[ta_k])
    P.op("dve", lambda e: e.tensor_tensor(b3, s3, sb_, mm), r=[src_k] + rdeps, w=[tb_k])
    a4 = ta_ap.rearrange("p (h t f) -> p h t f", h=nh, t=2)
    b4 = tb_ap.rearrange("p (h t f) -> p h t f", h=nh, t=2)
    d4 = dst_ap.rearrange("p (h t f) -> p h t f", h=nh, t=2)
    P.op("pool", lambda e: e.tensor_tensor(d4[:, :, 0, :], a4[:, :, 0, :], b4[:, :, 1, :], ALU.subtract), r=[ta_k, tb_k], w=[dst_k])
    P.op("pool", lambda e: e.tensor_tensor(d4[:, :, 1, :], a4[:, :, 1, :], b4[:, :, 0, :], ALU.add), r=[ta_k, tb_k], w=[dst_k])


def phase_dsa(C):
    nc, P, T, I = C.nc, C.P, C.T, C.I
    mm = ALU.mult
    KT = min(256, T // 4)
    NIT = 25
    SCALE = 128 ** -0.5
    C.bank_pool = [0, 1, 2, 3, 4]
    with ExitStack() as es:
        def wt(name, shape, dt=F32):
            return mk(P, list(shape), dt, name, es)
        WQ = wt("WQ", (128, 8, 1024), BF16)
        WR = wt("WR", (128, 8, 580), BF16)
        for c in range(8):
            P.dma("pool", WQ[:, c, :], I["w_in_odd"][c * 128:(c + 1) * 128, 0:1024], w=[WQ.k])
            P.dma("pool", WR[:, c, :], I["w_in_odd"][c * 128:(c + 1) * 128, 1024:1604], w=[WR.k])
        RTs = [[wt("CK", (128, 4, 64)), wt("SK", (128, 4, 64)), wt("CI", (128, 4, 32)), wt("SI", (128, 4, 32))] for _ in range(2)]

        def load_tables(s):
            tl = RTs[s % 2]
            for t_, nm in zip(tl, ("c_cos_k", "c_sin_k", "c_cos_i", "c_sin_i")):
                P.dma("sp", t_[:], I[nm][:, s * 4:(s + 1) * 4, :], w=[t_.k])
            return tl
        BIAS = wt("BIAS", (128, T))
        bcast_load(C, BIAS[:], I["c_tiebias"], 128, BIAS.k)
        CB = wt("CB", (128, 128))
        P.dma("sp", CB[:], I["c_cbias"], w=[CB.k])
        ikg, ikb = wt("ikg", (128, 64)), wt("ikb", (128, 64))
        bcast_load(C, ikg[:], I["c_ik_ln_g"], 128, ikg.k)
        bcast_load(C, ikb[:], I["c_ik_ln_b"], 128, ikb.k)
        kT = wt("kT", (128, T), BF16)
        ikT = wt("ikT", (128, T), BF16)
        Vx = wt("Vx", (128, C.NT, 129), BF16)
        P.op("dve", lambda e: e.memset(Vx[:, :, 128:129], 1.0), w=[Vx.k])
        xTs = [wt("xTd", (128, 8, 512), BF16) for _ in range(2)]
        ta, tb = wt("ropeA", (128, 512)), wt("ropeB", (128, 512))
        kr = wt("kr", (128, 128))
        ikr = wt("ikr", (128, 128))
        st2 = wt("st2", (128, 2))
        for s in range(C.NS):
            xT = xTs[s % 2]
            P.dma("sp", xT[:], XTv(C.XT1)[:, :, s * 512:(s + 1) * 512], r=[C.XT1_tok[s]], w=[xT.k])
            CK, SK, CI, SI = load_tables(s)
            for j in range(4):
                i = s * 4 + j
                pb = psb(C)
                for c in range(8):
                    P.op("pe", lambda e: e.matmul(pb[:, 0:256], lhsT=xT[:, c, j * 128:(j + 1) * 128], rhs=WR[:, c, 0:256], start=(c == 0), stop=(c == 7)),
                         r=[xT.k, WR.k], w=[pb.k], inc=False)
                for c in range(8):
                    P.op("pe", lambda e: e.matmul(pb[:, 256:320], lhsT=xT[:, c, j * 128:(j + 1) * 128], rhs=WR[:, c, 512:576], start=(c == 0), stop=(c == 7)),
                         r=[xT.k, WR.k], w=[pb.k], inc=(c == 7))
                rope_tm(C, kr[:, :], kr.k, pb[:, 0:128], pb.k, CK[:, j, :], SK[:, j, :], 1, 64, ta[:, 0:128], ta.k, tb[:, 0:128], tb.k, [CK.k, SK.k])
                P.op("act", lambda e: e.copy(Vx[:, i, 0:128], pb[:, 128:256]), r=[pb.k], w=[Vx.k])
                P.op("dve", lambda e: e.tensor_reduce(st2[:, 0:1], pb[:, 256:320], AX.X, ALU.add), r=[pb.k], w=[st2.k])
                P.op("dve", lambda e: e.tensor_scalar(st2[:, 0:1], st2[:, 0:1], 1.0 / 64, None, mm), r=[st2.k], w=[st2.k])
                P.op("dve", lambda e: e.tensor_scalar(ikr[:, 0:64], pb[:, 256:320], st2[:, 0:1], None, ALU.subtract), r=[pb.k, st2.k], w=[ikr.k])
                P.op("act", lambda e: e.activation(out=ikr[:, 64:128], in_=ikr[:, 0:64], func=AF.Square, accum_out=st2[:, 1:2]), r=[ikr.k], w=[ikr.k, st2.k])
                P.op("act", lambda e: e.activation(out=st2[:, 1:2], in_=st2[:, 1:2], func=AF.Sqrt, bias=LN_EPS, scale=1.0 / 64), r=[st2.k], w=[st2.k])
                P.op("dve", lambda e: e.reciprocal(st2[:, 1:2], st2[:, 1:2]), r=[st2.k], w=[st2.k])
                P.op("dve", lambda e: e.scalar_tensor_tensor(ikr[:, 0:64], ikr[:, 0:64], st2[:, 1:2], ikg[:], mm, mm), r=[ikr.k, st2.k, ikg.k], w=[ikr.k])
                P.op("dve", lambda e: e.tensor_tensor(ikr[:, 64:128], ikr[:, 0:64], ikb[:], ALU.add), r=[ikr.k, ikb.k], w=[ikr.k])
                ikn = TL(ikr.t, ikr.k)
                rope_src = ikr[:, 64:128]
                n = 2
                s3 = rope_src.rearrange("p (n f) -> p n f", n=n)
                cb = CI[:, j, :].unsqueeze(1).to_broadcast([128, n, 32])
                sb_ = SI[:, j, :].unsqueeze(1).to_broadcast([128, n, 32])
                a3 = ta[:, 0:64].rearrange("p (n f) -> p n f", n=n)
                b3 = tb[:, 0:64].rearrange("p (n f) -> p n f", n=n)
                P.op("dve", lambda e: e.tensor_tensor(a3, s3, cb, mm), r=[ikr.k, CI.k], w=[ta.k])
                P.op("dve", lambda e: e.tensor_tensor(b3, s3, sb_, mm), r=[ikr.k, SI.k], w=[tb.k])
                P.op("pool", lambda e: e.tensor_tensor(ikr[:, 0:32], ta[:, 0:32], tb[:, 32:64], ALU.subtract), r=[ta.k, tb.k], w=[ikr.k])
                P.op("pool", lambda e: e.tensor_tensor(ikr[:, 32:64], ta[:, 32:64], tb[:, 0:32], ALU.add), r=[ta.k, tb.k], w=[ikr.k])
                P.op("pool", lambda e: e.tensor_copy(ikr[:, 64:128], ikr[:, 0:64]), r=[ikr.k], w=[ikr.k])
                pt = psb(C)
                P.op("pe", lambda e: e.transpose(pt[:, 0:128], kr[:, :], C.ident[:]), r=[kr.k, C.ident_tok], w=[pt.k], inc=False)
                P.op("pe", lambda e: e.transpose(pt[:, 128:256], ikr[:, :], C.ident[:]), r=[ikr.k, C.ident_tok], w=[pt.k])
                P.op("act", lambda e: e.copy(kT[:, i * 128:(i + 1) * 128], pt[:, 0:128]), r=[pt.k], w=[kT.k])
                P.op("act", lambda e: e.mul(ikT[:, i * 128:(i + 1) * 128], pt[:, 128:256], 0.125), r=[pt.k], w=[ikT.k])
        SC = wt("SC", (128, T))
        MASKs = [wt("MASK", (128, T)) for _ in range(2)]
        junk = wt("junkd", (128, T), BF16)
        qr = wt("qr", (128, 1024))
        iqr = wt("iqr", (128, 256))
        qTs = [wt("qT", (128, 8, 128), BF16) for _ in range(2)]
        iqT = wt("iqT", (128, 2, 128), BF16)
        iws = wt("iws", (128, 4))
        rl = [wt("rl%d" % n, (128, 512)) for n in range(2)]
        bs = wt("bs", (128, 8))
        Dk = wt("Dk", (128, NIT))
        HK = wt("HK", (128, NIT))
        bcast_load(C, HK[:], I["c_halfpow"][0:1, 0:NIT], 128, HK.k)
        mT4 = [wt("mT4_%d" % n, (128, 4, 128), BF16) for n in range(2)]
        pTs = [wt("pT%d" % n, (128, 4, 128), BF16) for n in range(4)]
        o_ = wt("od", (128, 1024))
        rs8 = wt("rs8", (128, 8))
        ostg = [wt("ostg", (128, 8, 512), BF16) for _ in range(2)]
        accb = [TL(*C.banks[b]) for b in (5, 6, 7)]
        acc_of = [(0, 0), (0, 1), (0, 2), (1, 0), (1, 1), (1, 2), (2, 0), (2, 1)]
        def qblock(s, j):
            if True:
                i = s * 4 + j
                L = (i + 1) * 128
                xT = xTs[s % 2]
                og = ostg[s % 2]
                qT = qTs[i % 2]
                MASK = MASKs[i % 2]
                if j == 0:
                    P.dma("sp", xT[:], XTv(C.XT1)[:, :, s * 512:(s + 1) * 512], r=[C.XT1_tok[s]], w=[xT.k])
                    load_tables(s)
                CK, SK, CI, SI = RTs[s % 2]
                pq = [psb(C), psb(C)]
                for half in range(2):
                    for c in range(8):
                        P.op("pe", lambda e: e.matmul(pq[half][:, :], lhsT=xT[:, c, j * 128:(j + 1) * 128], rhs=WQ[:, c, half * 512:(half + 1) * 512], start=(c == 0), stop=(c == 7)),
                             r=[xT.k, WQ.k], w=[pq[half].k], inc=(c == 7))
                piq = psb(C)
                for c in range(8):
                    P.op("pe", lambda e: e.matmul(piq[:, 0:256], lhsT=xT[:, c, j * 128:(j + 1) * 128], rhs=WR[:, c, 256:512], start=(c == 0), stop=(c == 7)),
                         r=[xT.k, WR.k], w=[piq.k], inc=False)
                for c in range(8):
                    P.op("pe", lambda e: e.matmul(piq[:, 256:260], lhsT=xT[:, c, j * 128:(j + 1) * 128], rhs=WR[:, c, 576:580], start=(c == 0), stop=(c == 7)),
                         r=[xT.k, WR.k], w=[piq.k], inc=(c == 7))
                for half in range(2):
                    hs = slice(half * 512, (half + 1) * 512)
                    rope_tm(C, qr[:, hs], qr.k, pq[half][:, :], pq[half].k, CK[:, j, :], SK[:, j, :], 4, 64,
                            ta[:, 0:512], ta.k, tb[:, 0:512], tb.k, [CK.k, SK.k])
                rope_tm(C, iqr[:, :], iqr.k, piq[:, 0:256], piq.k, CI[:, j, :], SI[:, j, :], 4, 32, ta[:, 0:256], ta.k, tb[:, 0:256], tb.k, [CI.k, SI.k])
                P.op("act", lambda e: e.mul(iws[:], piq[:, 256:260], 0.5), r=[piq.k], w=[iws.k])
                for half in range(2):
                    pb = psb(C)
                    for c4 in range(4):
                        h = half * 4 + c4
                        P.op("pe", lambda e: e.transpose(pb[:, c4 * 128:(c4 + 1) * 128], qr[:, h * 128:(h + 1) * 128], C.ident[:]), r=[qr.k, C.ident_tok], w=[pb.k], inc=(c4 == 3))
                    P.op("act", lambda e: e.copy(qT[:, half * 4:(half + 1) * 4, :], hv(pb[:, :], 4)), r=[pb.k], w=[qT.k])
                pb = psb(C)
                for c2 in range(2):
                    P.op("pe", lambda e: e.transpose(pb[:, c2 * 128:(c2 + 1) * 128], iqr[:, c2 * 128:(c2 + 1) * 128], C.ident[:]), r=[iqr.k, C.ident_tok], w=[pb.k], inc=(c2 == 1))
                P.op("act", lambda e: e.copy(iqT[:], hv(pb[:, 0:256], 2)), r=[pb.k], w=[iqT.k])
                yield "F"
                for k0 in range(0, L, 512):
                    kw = min(512, L - k0)
                    for h in range(4):
                        ph = psb(C)
                        pl = (h % 2) * 64
                        P.op("pe", lambda e: e.matmul(ph[:, 0:kw], lhsT=iqT[pl:pl + 64, h // 2, :], rhs=ikT[pl:pl + 64, k0:k0 + kw], start=True, stop=True),
                             r=[iqT.k, ikT.k], w=[ph.k])
                        r_ = rl[h % 2]
                        P.op("act", lambda e: e.activation(out=r_[:, 0:kw], in_=ph[:, 0:kw], func=AF.Relu), r=[ph.k], w=[r_.k])
                        if h == 0:
                            P.op("dve", lambda e: e.scalar_tensor_tensor(SC[:, k0:k0 + kw], r_[:, 0:kw], iws[:, 0:1], BIAS[:, k0:k0 + kw], mm, ALU.add), r=[r_.k, iws.k, BIAS.k], w=[SC.k])
                        else:
                            P.op("dve", lambda e: e.scalar_tensor_tensor(SC[:, k0:k0 + kw], r_[:, 0:kw], iws[:, h:h + 1], SC[:, k0:k0 + kw], mm, ALU.add),
                                 r=[r_.k, iws.k, SC.k], w=[SC.k])
                    yield "F"
                if L > KT:
                    P.op("dve", lambda e: e.tensor_reduce(bs[:, 1:2], SC[:, 0:L], AX.X, ALU.max, apply_absolute_value=True), r=[SC.k], w=[bs.k])
                    P.op("dve", lambda e: e.tensor_scalar(bs[:, 0:1], bs[:, 1:2], -1.0, -1.0, mm, ALU.add), r=[bs.k], w=[bs.k])
                    P.op("dve", lambda e: e.tensor_scalar(bs[:, 1:2], bs[:, 1:2], 2.0, 2.0, mm, ALU.add), r=[bs.k], w=[bs.k])
                    P.op("dve", lambda e: e.tensor_scalar(Dk[:], HK[:], bs[:, 1:2], None, mm), r=[bs.k, HK.k], w=[Dk.k])
                P.op("pool", lambda e: e.tensor_tensor(SC[:, i * 128:L], SC[:, i * 128:L], CB[:], ALU.add), r=[SC.k, CB.k], w=[SC.k])
                if L > KT:
                    for it in range(NIT):
                        P.op("dve", lambda e: e.tensor_tensor(bs[:, 2:3], bs[:, 0:1], Dk[:, it:it + 1], ALU.add), r=[bs.k, Dk.k], w=[bs.k])
                        P.op("dve", lambda e: e.tensor_scalar(junk[:, 0:L], SC[:, 0:L], bs[:, 2:3], None, ALU.is_gt, ALU.add, accum_out=bs[:, 3:4]),
                             r=[SC.k, bs.k], w=[junk.k, bs.k])
                        P.op("dve", lambda e: e.scalar_tensor_tensor(bs[:, 4:5], bs[:, 3:4], float(KT) - 0.5, Dk[:, it:it + 1], ALU.is_gt, mm), r=[bs.k, Dk.k], w=[bs.k])
                        P.op("dve", lambda e: e.tensor_tensor(bs[:, 0:1], bs[:, 0:1], bs[:, 4:5], ALU.add), r=[bs.k], w=[bs.k])
                        yield "F"
                    P.op("dve", lambda e: e.tensor_scalar(MASK[:, 0:L], SC[:, 0:L], bs[:, 0:1], None, ALU.is_gt), r=[SC.k, bs.k], w=[MASK.k])
                else:
                    P.op("dve", lambda e: e.tensor_scalar(MASK[:, 0:L], SC[:, 0:L], -1e29, None, ALU.is_gt), r=[SC.k], w=[MASK.k])
                if "dbg_mask" in C.dbg:
                    P.dma("sp", C.dbg["dbg_mask"][i * 128:(i + 1) * 128, 0:L], MASK[:, 0:L], r=[MASK.k])
                    P.dma("sp", C.dbg["dbg_sc"][i * 128:(i + 1) * 128, 0:L], SC[:, 0:L], r=[SC.k])
                yield "END_FRONT"
                for st in range(i + 1):
                    if st % 4 == 0:
                        n4 = min(4, i + 1 - st)
                        m4 = mT4[(st // 4) % 2]
                        pb = psb(C)
                        for u in range(n4):
                            P.op("pe", lambda e: e.transpose(pb[:, u * 128:(u + 1) * 128], MASK[:, (st + u) * 128:(st + u + 1) * 128], C.ident[:]),
                                 r=[MASK.k, C.ident_tok], w=[pb.k], inc=(u == n4 - 1))
                        P.op("act", lambda e: e.copy(m4[:, 0:n4, :], hv(pb[:, :], 4)[:, 0:n4, :]), r=[pb.k], w=[m4.k])
                    for hg in range(2):
                        pl_ = psb(C)
                        P.op("pe", lambda e: e.matmul(pl_[:, :], lhsT=kT[:, st * 128:(st + 1) * 128], rhs=qT[:, hg * 4:(hg + 1) * 4, :].rearrange("p h q -> p (h q)"), start=True, stop=True),
                             r=[kT.k, qT.k], w=[pl_.k])
                        pT = pTs[hg * 2 + st % 2]
                        P.op("act", lambda e: e.activation(out=pT[:], in_=hv(pl_[:, :], 4), func=AF.Exp, scale=SCALE), r=[pl_.k], w=[pT.k])
                        P.op("pool", lambda e: e.tensor_tensor(pT[:], pT[:], m4[:, st % 4, :].unsqueeze(1).to_broadcast([128, 4, 128]), mm), r=[pT.k, m4.k], w=[pT.k])
                        for hh in range(4):
                            h = hg * 4 + hh
                            ab, slot = acc_of[h]
                            last = (st == i) and (h == 7 or True)
                            P.op("pe", lambda e: e.matmul(accb[ab][:, slot * 129:(slot + 1) * 129], lhsT=pT[:, hh, :], rhs=Vx[:, st, :], start=(st == 0 and slot == 0), stop=(st == i), skip_group_check=True),
                                 r=[pT.k, Vx.k], w=[accb[ab].k], inc=(hh == 3))
                    yield "B"
                for h in range(8):
                    ab, slot = acc_of[h]
                    P.op("dve", lambda e: e.reciprocal(rs8[:, h:h + 1], accb[ab][:, slot * 129 + 128:slot * 129 + 129]), r=[accb[ab].k], w=[rs8.k])
                    P.op("act", lambda e: e.activation(out=o_[:, h * 128:(h + 1) * 128], in_=accb[ab][:, slot * 129:slot * 129 + 128], func=AF.Copy, scale=rs8[:, h:h + 1]),
                         r=[accb[ab].k, rs8.k], w=[o_.k])
                if "dbg_dsa" in C.dbg:
                    P.dma("sp", C.dbg["dbg_dsa"][i * 128:(i + 1) * 128, :], o_[:], r=[o_.k])
                tile_to_stage(C, o_, og, j)
                if j == 3:
                    P.dma("sp", XTv(C.YT)[:, :, s * 512:(s + 1) * 512], og[:], r=[og.k], w=[C.YT_tok[0][s], C.YT_tok[1][s]])

        pipeline2([(lambda s=s, j=j: qblock(s, j)) for s in range(C.NS) for j in range(4)], interleave=C.dsa_interleave)
        P.barrier()
    C.bank_pool = list(range(8))


class _View:
    def __init__(self, tl, sl):
        self.t = _Sl(tl.t, sl)
        self.k = tl.k

    def __getitem__(self, idx):
        return self.t[idx]


class _Sl:
    def __init__(self, t, sl):
        self.base = t
        self.sl = sl

    def __getitem__(self, idx):
        rows, cols = idx
        assert cols == slice(None)
        return self.base[rows, self.sl]


_NC_CACHE = {}


def _in_map(inputs, b, T, consts):
    m = {"x": np.ascontiguousarray(inputs["x"][b, :T], dtype=np.float32)}
    for k, a in inputs.items():
        if k == "x":
            continue
        a = np.asarray(a, dtype=np.float32)
        if k in ("router_w", "exp_w_gate", "exp_w_up", "exp_w_down", "ln1_g", "ln1_b", "ln2_g", "ln2_b"):
            m[k] = np.ascontiguousarray(a)
        elif k == "router_bias":
            m[k] = np.ascontiguousarray(a.reshape(1, -1))
        elif k == "a_r_k":
            m[k] = np.ascontiguousarray(a.reshape(1, 512))
        elif a.ndim == 3:
            m[k] = np.ascontiguousarray(a[0])
        elif a.ndim == 2:
            m[k] = np.ascontiguousarray(a[0:1])
    m.update(consts)
    return m


def kernel(**inputs):
    x = np.asarray(inputs["x"])
    B, T, _ = x.shape
    if T not in _NC_CACHE:
        _NC_CACHE[T] = build(T)
    nc = _NC_CACHE[T]
    consts = {"c_" + k: v for k, v in host_consts(T).items()}
    consts.update({"c_" + k: v for k, v in rope_consts(T).items()})
    in_maps = [_in_map(inputs, b, T, consts) for b in range(B)]
    res = run_bass_kernel_spmd(nc, in_maps, core_ids=list(range(B)))
    out = np.stack([np.asarray(res.results[b]["out"], dtype=np.float32) for b in range(B)], 0)
    return out
```

```python
import numpy as np
import ml_dtypes
from contextlib import ExitStack
import concourse.bass as bass
import concourse.mybir as mybir
from concourse.bass_utils import run_bass_kernel_spmd

F32 = mybir.dt.float32
BF16 = mybir.dt.bfloat16
AF = mybir.ActivationFunctionType
ALU = mybir.AluOpType
AX = mybir.AxisListType

D = 1024
A_COLS = 1792
B_COLS = 1552
EVEN_COLS = 3344
ODD_COLS = 1604
NE = 16
DE = 256
DN_ALPHA = 4 ** 0.25
LN_EPS = 1e-5
DEC = 0.6065306597126334
DSA_INTERLEAVE = True
RWKV_INTERLEAVE = False


class Tok:
    __slots__ = ("w", "r")

    def __init__(self):
        self.w = None
        self.r = {}


class Eng:
    def __init__(self, name, h, sem):
        self.name = name
        self.h = h
        self.sem = sem
        self.cnt = 0
        self.waited = {}


class Prog:
    NSLOT = 8

    def __init__(self, nc, es):
        self.nc = nc
        self.es = es
        self.E = {}
        for name, h in (("pe", nc.tensor), ("act", nc.scalar), ("dve", nc.vector),
                        ("pool", nc.gpsimd), ("sp", nc.sync)):
            sem = es.enter_context(nc.semaphore("sem_" + name))
            self.E[name] = Eng(name, h, sem)
        self.slots = {}
        self.dn = {}
        for q in ("sp", "pool", "act"):
            self.slots[q] = [[es.enter_context(nc.semaphore("dq_%s%d" % (q, i))), 0] for i in range(self.NSLOT)]
            self.dn[q] = 0
        self.nalloc = 0

    def sb(self, shape, dt=F32, name=None, es=None):
        self.nalloc += 1
        t = (es or self.es).enter_context(self.nc.sbuf_tensor("%s_%d" % (name or "t", self.nalloc), list(shape), dt))
        return t

    def _wait(self, eng, ev):
        sem, val = ev
        key = sem.num
        if eng.waited.get(key, 0) >= val:
            return
        eng.h.wait_ge(sem, val)
        eng.waited[key] = val

    def _deps(self, en, r, w):
        eng = self.E[en]
        for t in r:
            if t.w is not None:
                yield t.w
        for t in w:
            if t.w is not None:
                yield t.w
            for ev in t.r.values():
                yield ev

    def op(self, en, fn, r=(), w=(), inc=True):
        eng = self.E[en]
        for ev in list(self._deps(en, r, w)):
            if en == "pe" and ev[0] is eng.sem:
                continue
            self._wait(eng, ev)
        ins = fn(eng.h)
        myev = (eng.sem, eng.cnt + 1)
        if inc:
            ins.then_inc(eng.sem, 1)
            eng.cnt += 1
        for t in r:
            t.r[en] = myev
        for t in w:
            t.w = myev
            t.r = {}
        return ins

    def dma(self, qn, out, in_, r=(), w=(), **kw):
        q = self.E[qn]
        for ev in list(self._deps(qn, r, w)):
            self._wait(q, ev)
        slot = self.slots[qn][self.dn[qn] % self.NSLOT]
        self.dn[qn] += 1
        if slot[1] > 0:
            self._wait(q, (slot[0], slot[1]))
        ins = q.h.dma_start(out=out, in_=in_, **kw)
        slot[1] += 16
        ins.then_inc(slot[0], 16)
        ev = (slot[0], slot[1])
        key = "d%d" % slot[0].num
        for t in r:
            t.r[key] = ev
        for t in w:
            t.w = ev
            t.r = {}

    def barrier(self):
        evs = []
        for q in self.slots:
            for sem, val in self.slots[q]:
                if val > 0:
                    evs.append((sem, val))
        for name, e in self.E.items():
            if e.cnt > 0:
                evs.append((e.sem, e.cnt))
        for name, e in self.E.items():
            for ev in evs:
                if ev[0] is e.sem and name == "pe":
                    continue
                self._wait(e, ev)

    def finish(self):
        sp = self.E["sp"]
        for q in self.slots:
            for sem, val in self.slots[q]:
                if val > 0:
                    self._wait(sp, (sem, val))
        for name, e in self.E.items():
            if name != "sp" and e.cnt > 0:
                self._wait(sp, (e.sem, e.cnt))


def host_consts(T):
    c = {}
    c["ident"] = np.eye(128, dtype=np.float32)
    j = np.arange(64)[:, None]
    i = np.arange(64)[None, :]
    c["tri64"] = np.stack([(-DEC) * (j <= i), (-DEC) * (j < i), (-DEC) * (j > i)], 1).astype(np.float32)
    c["ncol64"] = np.full((64, 1), -DEC, np.float32)
    su = (j < i).astype(np.float32)
    iu = (j <= i).astype(np.float32)
    sl = (j > i).astype(np.float32)
    mMA = np.concatenate([-su, iu], 1)
    mBB = np.concatenate([su, iu], 1)
    c["mMA"] = np.tile(mMA[:, None, :], (1, 8, 1)).astype(np.float32)
    c["mBB"] = np.tile(mBB[:, None, :], (1, 8, 1)).astype(np.float32)
    c["mNT"] = np.tile((-sl)[:, None, :], (1, 8, 1)).astype(np.float32)
    c["id8"] = np.tile(np.eye(64, dtype=np.float32)[:, None, :], (1, 8, 1))
    j = np.arange(128)[:, None]
    i = np.arange(128)[None, :]
    c["tri128"] = np.stack([(-1 / 16) * (j <= i), (-1 / 16) * (j > i)], 1).astype(np.float32)
    c["ncol128"] = np.full((128, 1), -1 / 16, np.float32)
    c["sel"] = (np.arange(16)[:, None, None] == np.arange(16)[None, :, None]).astype(np.float32) * np.ones((1, 1, 128), np.float32)
    c["iu128"] = np.tile((j <= i).astype(np.float32)[:, None, :], (1, 4, 1))
    return c


class Ctx:
    pass


def build(T, dbg=(), stages=("A", "R", "G", "O0", "M0", "S1", "O1", "M1")):
    nc = bass.Bass("TRN2", target_bir_lowering=False)
    es = ExitStack()
    P = Prog(nc, es)
    NT = T // 128
    NS = T // 512
    C = Ctx()
    C.nc, C.P, C.T, C.NT, C.NS = nc, P, T, NT, NS
    C.dbg = {}
    C.dsa_interleave = DSA_INTERLEAVE

    def din(name, shape, dt=F32):
        return nc.dram_tensor(name, list(shape), dt, kind="ExternalInput").ap()

    def dscr(name, shape, dt=F32, out=False):
        kind = "ExternalOutput" if (out or name in dbg) else "Internal"
        return nc.dram_tensor(name, list(shape), dt, kind=kind).ap()

    I = {}
    I["x"] = din("x", [T, D])
    for name, shape in (("w_in_even", [D, EVEN_COLS]), ("a_mu", [1, A_COLS]), ("a_w0", [1, 512]), ("a_w2", [64, 512]),
                        ("a_a0", [1, 512]), ("a_a2", [64, 512]), ("a_g2", [128, 512]), ("a_kk_scale", [1, 512]),
                        ("a_ka_scale", [1, 512]), ("a_r_k", [1, 512]), ("a_gn_g", [1, 512]), ("a_gn_b", [1, 512]),
                        ("b_gate_w2", [16, 256]), ("b_gate_b", [1, 256]), ("b_norm_g", [1, 512]),
                        ("w_out_even", [D, D]), ("w_in_odd", [D, ODD_COLS]), ("c_ik_ln_g", [1, 64]),
                        ("c_ik_ln_b", [1, 64]), ("w_out_odd", [D, D]), ("ln1_g", [2, D]), ("ln1_b", [2, D]),
                        ("ln2_g", [2, D]), ("ln2_b", [2, D]), ("router_w", [D, NE]), ("router_bias", [1, NE]),
                        ("exp_w_gate", [2, NE, D, DE]), ("exp_w_up", [2, NE, D, DE]), ("exp_w_down", [2, NE, DE, D])):
        I[name] = din(name, shape)
    hc = host_consts(T)
    hc.update(rope_consts(T))
    for k, v in hc.items():
        I["c_" + k] = din("c_" + k, list(v.shape), F32 if v.dtype == np.float32 else BF16)
    C.I = I
    out = dscr("out", [T, D], out=True)
    C.XT0 = dscr("XT0", [D, T + 1], BF16)
    C.XT0_tok = [Tok() for _ in range(NS)]
    C.XT0_z = Tok()
    C.YT = dscr("YT", [D, T], BF16)
    C.YT_tok = [[Tok() for _ in range(NS)] for _ in range(2)]
    C.H0 = dscr("H0", [T, D])
    C.H0_tok = [Tok() for _ in range(NT)]
    C.HT0 = dscr("HT0", [D, T], BF16)
    C.HT0_tok = [Tok() for _ in range(NS)]
    C.X1 = dscr("X1", [T, D])
    C.X1_tok = [Tok() for _ in range(NT)]
    C.XT1 = dscr("XT1", [D, T], BF16)
    C.XT1_tok = [Tok() for _ in range(NS)]

    for nm, shp in (("dbg_ya", [T, 512]), ("dbg_yb", [T, 512]), ("dbg_dsa", [T, 1024]), ("dbg_mask", [T, T]), ("dbg_sc", [T, T])):
        if nm in dbg:
            C.dbg[nm] = dscr(nm, shp, out=True)
    C.WGU16 = [dscr("WGU16_%d" % l, [NE, 128, 2 * 8 * DE], BF16) for l in range(2)]
    C.WGU16_tok = [[Tok() for _ in range(NE)] for l in range(2)]
    C.WD16 = [dscr("WD16_%d" % l, [128, 32 * D], BF16) for l in range(2)]
    C.WD16_tok = [Tok() for l in range(2)]
    C.banks = []
    for b in range(8):
        t = es.enter_context(nc.psum_tensor("psb%d" % b, [128, 512], F32))
        C.banks.append((t, Tok()))
    C.bi = 0

    C.bank_pool = list(range(8))

    def bank():
        b = C.banks[C.bank_pool[C.bi % len(C.bank_pool)]]
        C.bi += 1
        return b
    C.bank = bank

    C.ident = P.sb([128, 128], F32, "ident")
    C.ident_tok = Tok()
    P.dma("sp", C.ident[:], I["c_ident"], w=[C.ident_tok])

    if "M0" in stages:
        cast_weights(C, 0)
    if "A" in stages:
        phase_A(C)
    if "R" in stages:
        phase_rwkv(C)
    if "G" in stages:
        phase_gla(C)
    if "M1" in stages:
        cast_weights(C, 1)
    if "O0" in stages:
        phase_outproj(C, I["w_out_even"], C.YT, lambda s: [C.YT_tok[0][s], C.YT_tok[1][s]], I["x"], lambda i: [],
                      I["ln1_g"][0:1, :], I["ln1_b"][0:1, :], C.H0, C.H0_tok, C.HT0, C.HT0_tok)
    if "M0" in stages:
        phase_moe(C, 0, C.H0, C.H0_tok, C.HT0, C.HT0_tok, C.X1, C.X1_tok, C.XT1, C.XT1_tok)
    if "S1" in stages:
        phase_dsa(C)
    if "O1" in stages:
        C.H1 = dscr("H1", [T, D])
        C.H1_tok = [Tok() for _ in range(NT)]
        C.HT1 = dscr("HT1", [D, T], BF16)
        C.HT1_tok = [Tok() for _ in range(NS)]
        phase_outproj(C, I["w_out_odd"], C.YT, lambda s: [C.YT_tok[0][s], C.YT_tok[1][s]], C.X1, lambda i: [C.X1_tok[i]],
                      I["ln1_g"][1:2, :], I["ln1_b"][1:2, :], C.H1, C.H1_tok, C.HT1, C.HT1_tok)
    if "M1" in stages:
        out_tok = [Tok() for _ in range(NT)]
        phase_moe(C, 1, C.H1, C.H1_tok, C.HT1, C.HT1_tok, out, out_tok, None, None)
    P.finish()
    es.close()
    return nc


def XTv(ap):
    return ap.rearrange("(c p) t -> p c t", p=128)


def cast_weights(C, l):
    P, I = C.P, C.I
    for e in range(NE):
        dst = C.WGU16[l][e].rearrange("p (t c f) -> p t c f", t=2, c=8)
        P.dma("pool", dst[:, 0, :, :], I["exp_w_gate"][l, e].rearrange("(c p) f -> p c f", p=128), w=[C.WGU16_tok[l][e]])
        P.dma("pool", dst[:, 1, :, :], I["exp_w_up"][l, e].rearrange("(c p) f -> p c f", p=128), w=[C.WGU16_tok[l][e]])
    wd_flat = I["exp_w_down"][l].rearrange("e f d -> (e f) d")
    dstd = C.WD16[l].rearrange("p (c d) -> p c d", c=32)
    for c4 in range(8):
        P.dma("pool", dstd[:, c4 * 4:(c4 + 1) * 4, :], wd_flat[c4 * 512:(c4 + 1) * 512, :].rearrange("(c p) d -> p c d", p=128), w=[C.WD16_tok[l]])


def phase_A(C):
    nc, P, T, I = C.nc, C.P, C.T, C.I
    with ExitStack() as es:
        xin = [P.sb([128, D], F32, "xin", es) for _ in range(2)]
        xin_tok = [Tok(), Tok()]
        st = [P.sb([128, 8, 512], BF16, "ast", es) for _ in range(2)]
        st_tok = [Tok(), Tok()]
        z = P.sb([128, 8, 1], BF16, "zc", es)
        zt = Tok()
        P.op("dve", lambda e: e.memset(z[:], 0.0), w=[zt])
        P.dma("sp", XTv(C.XT0)[:, :, 0:1], z[:], r=[zt], w=[C.XT0_z], allow_slow_non_contiguous=True)
        for s in range(C.NS):
            sb_ = st[s % 2]
            for j in range(4):
                i = s * 4 + j
                xb = xin[i % 2]
                xt = xin_tok[i % 2]
                P.dma("sp", xb[:], I["x"][i * 128:(i + 1) * 128, :], w=[xt])
                for half in range(2):
                    bt, bk = C.bank()
                    for c4 in range(4):
                        c = half * 4 + c4
                        P.op("pe", lambda e: e.transpose(bt[:, c4 * 128:(c4 + 1) * 128], xb[:, c * 128:(c + 1) * 128], C.ident[:]),
                             r=[xt, C.ident_tok], w=[bk], inc=(c4 == 3))
                    en = "act" if half == 0 else "dve"
                    src = bt[:, :].rearrange("p (c t) -> p c t", c=4)
                    dst = sb_[:, half * 4:(half + 1) * 4, j * 128:(j + 1) * 128]
                    if en == "act":
                        P.op("act", lambda e: e.copy(dst, src), r=[bk], w=[st_tok[s % 2]])
                    else:
                        P.op("dve", lambda e: e.tensor_copy(dst, src), r=[bk], w=[st_tok[s % 2]])
            P.dma("sp", XTv(C.XT0)[:, :, 1 + s * 512:1 + (s + 1) * 512], sb_[:], r=[st_tok[s % 2]], w=[C.XT0_tok[s]])
        P.barrier()


class TL:
    def __init__(self, t, k=None):
        self.t = t
        self.k = k or Tok()

    def __getitem__(self, idx):
        return self.t[idx]


def mk(P, shape, dt=F32, name=None, es=None):
    return TL(P.sb(shape, dt, name, es))


def bcast_load(C, dst_ap, src_row, np_, tok, q="sp"):
    C.P.dma(q, dst_ap, src_row.partition_broadcast(np_), w=[tok])


def hv(ap, h):
    return ap.rearrange("p (h v) -> p h v", h=h)


def phase_rwkv(C):
    nc, P, T, I = C.nc, C.P, C.T, C.I
    mm = ALU.mult
    with ExitStack() as es:
        W1 = mk(P, [128, 8, A_COLS], BF16, "W1", es)
        W2 = mk(P, [128, 8, A_COLS], BF16, "W2", es)
        with ExitStack() as es2:
            mub = mk(P, [128, A_COLS], F32, "mub", es2)
            omu = mk(P, [128, A_COLS], F32, "omu", es2)
            stg = [mk(P, [128, A_COLS], F32, "wstg", es2) for _ in range(2)]
            bcast_load(C, mub[:], I["a_mu"], 128, mub.k)
            P.op("dve", lambda e: e.tensor_scalar(omu[:], mub[:], -1.0, 1.0, ALU.mult, ALU.add), r=[mub.k], w=[omu.k])
            for c in range(8):
                s_ = stg[c % 2]
                P.dma("sp", s_[:], I["w_in_even"][c * 128:(c + 1) * 128, 0:A_COLS], w=[s_.k])
                P.op("dve", lambda e: e.tensor_tensor(W1[:, c, :], s_[:], omu[:], mm), r=[s_.k, omu.k], w=[W1.k])
                P.op("pool", lambda e: e.tensor_tensor(W2[:, c, :], s_[:], mub[:], mm), r=[s_.k, mub.k], w=[W2.k])
            P.barrier()
        LW = mk(P, [128, 512], BF16, "LW", es)
        G2 = mk(P, [128, 512], BF16, "G2", es)
        P.dma("pool", LW[0:64, :], I["a_w2"], w=[LW.k])
        P.dma("pool", LW[64:128, :], I["a_a2"], w=[LW.k])
        P.dma("pool", G2[:], I["a_g2"], w=[G2.k])
        BV = mk(P, [64, 7, 512], F32, "BV", es)
        for n, name in enumerate(("a_w0", "a_a0", "a_kk_scale", "a_ka_scale", "a_r_k", "a_gn_g", "a_gn_b")):
            bcast_load(C, BV[:, n, :], I[name], 64, BV.k)
        w0b, a0b, kksb, kab, rkb, gngb, gnbb = [BV[:, n, :] for n in range(7)]
        tri = mk(P, [64, 3, 64], F32, "tri", es)
        ncol = mk(P, [64, 1], F32, "ncol", es)
        mMA = mk(P, [64, 8, 128], F32, "mMA", es)
        mBB = mk(P, [64, 8, 128], F32, "mBB", es)
        mNT = mk(P, [64, 8, 64], F32, "mNT", es)
        id8 = mk(P, [64, 8, 64], F32, "id8", es)
        for tl, nm in ((tri, "c_tri64"), (ncol, "c_ncol64"), (mMA, "c_mMA"), (mBB, "c_mBB"), (mNT, "c_mNT"), (id8, "c_id8")):
            P.dma("sp", tl[:], I[nm], w=[tl.k])
        id64 = C.ident[0:64, 0:64]

        def wt(name, shape=(64, 512), dt=F32):
            return mk(P, list(shape), dt, name, es)
        ATs = [mk(P, [128, 8, 513], BF16, "ATs", es) for _ in range(2)]
        TX = wt("TX", (128, 512), BF16)
        SG = wt("SG", (128, 512), BF16)
        r_, k_, v_, sg, a_, kk, be, Bi, Ki, tmp = [wt(n) for n in ("r", "k", "v", "sg", "a", "kk", "be", "Bi", "Ki", "tmp")]
        Ep, Em, Ex, Ee = [wt(n) for n in ("Ep", "Em", "Ex", "Ee")]
        s8 = [wt("s8_%d" % n, (64, 8)) for n in range(4)]
        KRs = [wt("KR", (64, 8, 128), BF16) for _ in range(2)]
        BiT = wt("BiT", (64, 8, 64), BF16)
        KiT = wt("KiT", (64, 8, 64), BF16)
        MAs = [wt("MA", (64, 8, 128), BF16) for _ in range(2)]
        BBs = [wt("BB", (64, 8, 128), BF16) for _ in range(2)]
        Xb = [wt("X%d" % n, (64, 8, 64), BF16) for n in range(2)]
        XTb = [wt("XT%d" % n, (64, 8, 64), BF16) for n in range(2)]
        Qbs = [[wt("Q%d" % n, (64, 8, 64), BF16) for n in range(2)] for _ in range(2)]
        Xs = wt("Xs", (64, 8, 64), BF16)
        nU = wt("nU", (64, 8, 64), BF16)
        Y = wt("Y")
        tmpb = wt("tmpb")
        Hs = [wt("H%d" % n, (64, 8, 64)) for n in range(2)]
        Hbs = [wt("Hb%d" % n, (64, 8, 64), BF16) for n in range(2)]
        vbs = [wt("vb", (64, 512), BF16) for _ in range(2)]
        Ke16s = [wt("Ke16", (64, 512), BF16) for _ in range(2)]
        Be16s = [wt("Be16", (64, 512), BF16) for _ in range(2)]
        PCs = [wt("PC", (64, 8)) for _ in range(2)]
        gs_ = [wt("g", (64, 512)) for _ in range(2)]
        bonuss = [wt("bonus", (64, 512)) for _ in range(2)]
        P.op("pool", lambda e: e.memset(Hbs[0][:], 0.0), w=[Hbs[0].k])
        yst = [mk(P, [128, 4, 512], BF16, "yst", es) for _ in range(2)]
        P.op("dve", lambda e: e.memset(Hs[0][:], 0.0), w=[Hs[0].k])

        def psb():
            t, k = C.bank()
            return TL(t, k)

        def v3(tl_or_ap, h=8):
            return hv(tl_or_ap, h)

        def chunk(s, ci):
          at = ATs[s % 2]
          ys = yst[s % 2]
          if ci == 0:
            rd = [C.XT0_tok[s]] + ([C.XT0_tok[s - 1]] if s > 0 else [C.XT0_z])
            P.dma("sp", at[:], XTv(C.XT0)[:, :, s * 512:s * 512 + 513], r=rd, w=[at.k])
            for which in range(2):
                pb = psb()
                c0 = 1536 + which * 128
                for c in range(8):
                    P.op("pe", lambda e: e.matmul(pb[:, :], lhsT=W1[:, c, c0:c0 + 128], rhs=at[:, c, 1:513], start=(c == 0), stop=False),
                         r=[W1.k, at.k], w=[pb.k], inc=False)
                for c in range(8):
                    P.op("pe", lambda e: e.matmul(pb[:, :], lhsT=W2[:, c, c0:c0 + 128], rhs=at[:, c, 0:512], start=False, stop=(c == 7)),
                         r=[W2.k, at.k], w=[pb.k], inc=(c == 7))
                if which == 0:
                    P.op("act", lambda e: e.activation(out=TX[0:64, :], in_=pb[0:64, :], func=AF.Tanh), r=[pb.k], w=[TX.k])
                    P.op("act", lambda e: e.copy(TX[64:128, :], pb[64:128, :]), r=[pb.k], w=[TX.k])
                else:
                    P.op("act", lambda e: e.activation(out=SG[:, :], in_=pb[:, :], func=AF.Sigmoid), r=[pb.k], w=[SG.k])
          if True:
            if True:
                g = s * 8 + ci
                t0 = ci * 64
                KR, MA, BB, Qb = KRs[g % 2], MAs[g % 2], BBs[g % 2], Qbs[g % 2]
                vb, Ke16, Be16, PC, g_, bonus = vbs[g % 2], Ke16s[g % 2], Be16s[g % 2], PCs[g % 2], gs_[g % 2], bonuss[g % 2]
                pr, pk, pv = psb(), psb(), psb()
                for pb, c0 in ((pr, 0), (pk, 512), (pv, 1024)):
                    for c in range(8):
                        P.op("pe", lambda e: e.matmul(pb[0:64, :], lhsT=at[:, c, 1 + t0:1 + t0 + 64], rhs=W1[:, c, c0:c0 + 512], start=(c == 0), stop=False),
                             r=[W1.k, at.k], w=[pb.k], inc=False)
                    for c in range(8):
                        P.op("pe", lambda e: e.matmul(pb[0:64, :], lhsT=at[:, c, t0:t0 + 64], rhs=W2[:, c, c0:c0 + 512], start=False, stop=(c == 7)),
                             r=[W2.k, at.k], w=[pb.k], inc=(c == 7))
                yield "F"
                pz, pza, pg = psb(), psb(), psb()
                P.op("pe", lambda e: e.matmul(pz[0:64, :], lhsT=TX[0:64, t0:t0 + 64], rhs=LW[0:64, :], start=True, stop=True), r=[TX.k, LW.k], w=[pz.k])
                P.op("pe", lambda e: e.matmul(pza[0:64, :], lhsT=TX[64:128, t0:t0 + 64], rhs=LW[64:128, :], start=True, stop=True), r=[TX.k, LW.k], w=[pza.k])
                P.op("pe", lambda e: e.matmul(pg[0:64, :], lhsT=SG[:, t0:t0 + 64], rhs=G2[:, :], start=True, stop=True), r=[SG.k, G2.k], w=[pg.k])
                P.op("act", lambda e: e.copy(r_[:], pr[0:64, :]), r=[pr.k], w=[r_.k])
                P.op("act", lambda e: e.copy(v_[:], pv[0:64, :]), r=[pv.k], w=[v_.k])
                P.op("act", lambda e: e.copy(vb[:], pv[0:64, :]), r=[pv.k], w=[vb.k])
                P.op("act", lambda e: e.copy(g_[:], pg[0:64, :]), r=[pg.k], w=[g_.k])
                P.op("dve", lambda e: e.tensor_copy(k_[:], pk[0:64, :]), r=[pk.k], w=[k_.k])
                P.op("dve", lambda e: e.tensor_tensor(sg[:], pz[0:64, :], w0b, ALU.add), r=[pz.k, BV.k], w=[sg.k])
                P.op("act", lambda e: e.activation(out=sg[:], in_=sg[:], func=AF.Sigmoid), r=[sg.k], w=[sg.k])
                P.op("dve", lambda e: e.tensor_tensor(a_[:], pza[0:64, :], a0b, ALU.add), r=[pza.k, BV.k], w=[a_.k])
                P.op("act", lambda e: e.activation(out=a_[:], in_=a_[:], func=AF.Sigmoid), r=[a_.k], w=[a_.k])
                yield "F"
                P.op("pool", lambda e: e.tensor_tensor(kk[:], k_[:], kksb, mm), r=[k_.k, BV.k], w=[kk.k])
                P.op("pool", lambda e: e.tensor_tensor(tmp[:], kk[:], kk[:], mm), r=[kk.k], w=[tmp.k])
                P.op("dve", lambda e: e.tensor_reduce(s8[0][:], v3(tmp[:]), AX.X, ALU.add), r=[tmp.k], w=[s8[0].k])
                P.op("dve", lambda e: e.tensor_scalar(s8[0][:], s8[0][:], 1e-24, None, ALU.max), r=[s8[0].k], w=[s8[0].k])
                P.op("act", lambda e: e.activation(out=s8[0][:], in_=s8[0][:], func=AF.Sqrt), r=[s8[0].k], w=[s8[0].k])
                P.op("dve", lambda e: e.reciprocal(s8[0][:], s8[0][:]), r=[s8[0].k], w=[s8[0].k])
                P.op("dve", lambda e: e.tensor_tensor(v3(kk[:]), v3(kk[:]), s8[0][:, :].unsqueeze(2).to_broadcast([64, 8, 64]), mm),
                     r=[kk.k, s8[0].k], w=[kk.k])
                P.op("pool", lambda e: e.tensor_tensor(be[:], kk[:], a_[:], mm), r=[kk.k, a_.k], w=[be.k])
                P.op("dve", lambda e: e.scalar_tensor_tensor(tmp[:], a_[:], -1.0, kab, ALU.add, mm), r=[a_.k, BV.k], w=[tmp.k])
                P.op("dve", lambda e: e.scalar_tensor_tensor(k_[:], tmp[:], 1.0, k_[:], ALU.add, mm), r=[tmp.k, k_.k], w=[k_.k])
                P.op("pool", lambda e: e.tensor_tensor(tmp[:], r_[:], k_[:], mm), r=[r_.k, k_.k], w=[tmp.k])
                P.op("pool", lambda e: e.tensor_tensor(tmp[:], tmp[:], rkb, mm), r=[tmp.k, BV.k], w=[tmp.k])
                P.op("dve", lambda e: e.tensor_reduce(s8[1][:], v3(tmp[:]), AX.X, ALU.add), r=[tmp.k], w=[s8[1].k])
                P.op("dve", lambda e: e.tensor_tensor(v3(bonus[:]), v3(v_[:]), s8[1][:, :].unsqueeze(2).to_broadcast([64, 8, 64]), mm),
                     r=[v_.k, s8[1].k], w=[bonus.k])
                yield "F"
                pcl, pcx, pca, ppc = psb(), psb(), psb(), psb()
                for pb, n in ((pcl, 0), (pcx, 1), (pca, 2)):
                    P.op("pe", lambda e: e.matmul(pb[0:64, :], lhsT=tri[:, n, :], rhs=sg[:], start=True, stop=True), r=[tri.k, sg.k], w=[pb.k])
                for h in range(8):
                    P.op("pe", lambda e: e.matmul(ppc[0:64, h:h + 1], lhsT=sg[:, h * 64:(h + 1) * 64], rhs=ncol[:], start=True, stop=True),
                         r=[sg.k, ncol.k], w=[ppc.k], inc=(h == 7))
                P.op("act", lambda e: e.activation(out=Ep[:], in_=pcl[0:64, :], func=AF.Exp), r=[pcl.k], w=[Ep.k])
                P.op("act", lambda e: e.activation(out=Em[:], in_=pcl[0:64, :], func=AF.Exp, scale=-1.0), r=[pcl.k], w=[Em.k])
                P.op("act", lambda e: e.activation(out=Ex[:], in_=pcx[0:64, :], func=AF.Exp), r=[pcx.k], w=[Ex.k])
                P.op("act", lambda e: e.activation(out=Ee[:], in_=pca[0:64, :], func=AF.Exp), r=[pca.k], w=[Ee.k])
                P.op("act", lambda e: e.activation(out=PC[:], in_=ppc[0:64, 0:8], func=AF.Exp), r=[ppc.k], w=[PC.k])
                P.op("dve", lambda e: e.tensor_tensor(r_[:], r_[:], Ep[:], mm), r=[r_.k, Ep.k], w=[r_.k])
                P.op("pool", lambda e: e.tensor_tensor(kk[:], kk[:], Ex[:], mm), r=[kk.k, Ex.k], w=[kk.k])
                P.op("dve", lambda e: e.tensor_tensor(Bi[:], be[:], Em[:], mm), r=[be.k, Em.k], w=[Bi.k])
                P.op("pool", lambda e: e.tensor_tensor(Ki[:], k_[:], Em[:], mm), r=[k_.k, Em.k], w=[Ki.k])
                P.op("dve", lambda e: e.tensor_tensor(Ke16[:], k_[:], Ee[:], mm), r=[k_.k, Ee.k], w=[Ke16.k])
                P.op("pool", lambda e: e.tensor_tensor(Be16[:], be[:], Ee[:], mm), r=[be.k, Ee.k], w=[Be16.k])
                yield "F"
                for src, dst, off, en in ((kk, KR, 0, "act"), (r_, KR, 64, "dve"), (Bi, BiT, 0, "act"), (Ki, KiT, 0, "dve")):
                    pb = psb()
                    for h in range(8):
                        P.op("pe", lambda e: e.transpose(pb[0:64, h * 64:(h + 1) * 64], src[:, h * 64:(h + 1) * 64], id64),
                             r=[src.k, C.ident_tok], w=[pb.k], inc=(h == 7))
                    d_ = dst[:, :, off:off + 64]
                    s_ = v3(pb[0:64, :])
                    if en == "act":
                        P.op("act", lambda e: e.copy(d_, s_), r=[pb.k], w=[dst.k])
                    else:
                        P.op("dve", lambda e: e.tensor_copy(d_, s_), r=[pb.k], w=[dst.k])
                yield "F"
                pma = [psb(), psb()]
                pbb = [psb(), psb()]
                pnt = psb()
                for h in range(8):
                    hb, hh = h // 4, h % 4
                    P.op("pe", lambda e: e.matmul(pma[hb][0:64, hh * 128:(hh + 1) * 128], lhsT=BiT[:, h, :], rhs=KR[:, h, :], start=True, stop=True),
                         r=[BiT.k, KR.k], w=[pma[hb].k], inc=(hh == 3))
                for h in range(8):
                    hb, hh = h // 4, h % 4
                    P.op("pe", lambda e: e.matmul(pbb[hb][0:64, hh * 128:(hh + 1) * 128], lhsT=KiT[:, h, :], rhs=KR[:, h, :], start=True, stop=True),
                         r=[KiT.k, KR.k], w=[pbb[hb].k], inc=(hh == 3))
                for h in range(8):
                    P.op("pe", lambda e: e.matmul(pnt[0:64, h * 64:(h + 1) * 64], lhsT=KR[:, h, 0:64], rhs=BiT[:, h, :], start=True, stop=True),
                         r=[BiT.k, KR.k], w=[pnt.k], inc=(h == 7))
                for hb in range(2):
                    P.op("dve", lambda e: e.tensor_tensor(MA[:, hb * 4:(hb + 1) * 4, :], hv(pma[hb][0:64, :], 4), mMA[:, hb * 4:(hb + 1) * 4, :], mm),
                         r=[pma[hb].k, mMA.k], w=[MA.k])
                    P.op("dve", lambda e: e.tensor_tensor(BB[:, hb * 4:(hb + 1) * 4, :], hv(pbb[hb][0:64, :], 4), mBB[:, hb * 4:(hb + 1) * 4, :], mm),
                         r=[pbb[hb].k, mBB.k], w=[BB.k])
                X, XT, Q = Xb[0], XTb[0], Qb[0]
                P.op("dve", lambda e: e.tensor_tensor(XT[:], v3(pnt[0:64, :]), mNT[:], mm), r=[pnt.k, mNT.k], w=[XT.k])
                P.op("pool", lambda e: e.tensor_copy(X[:], MA[:, :, 0:64]), r=[MA.k], w=[X.k])
                P.op("pool", lambda e: e.tensor_tensor(Q[:], MA[:, :, 0:64], id8[:], ALU.add), r=[MA.k, id8.k], w=[Q.k])
                for lvl in range(5):
                    Xn, XTn, Qn = Xb[(lvl + 1) % 2], XTb[(lvl + 1) % 2], Qb[(lvl + 1) % 2]
                    pxt = psb()
                    for h in range(8):
                        P.op("pe", lambda e: e.matmul(pxt[0:64, h * 64:(h + 1) * 64], lhsT=X[:, h, :], rhs=XT[:, h, :], start=True, stop=True),
                             r=[X.k, XT.k], w=[pxt.k], inc=(h == 7))
                    if lvl < 4:
                        px = psb()
                        for h in range(8):
                            P.op("pe", lambda e: e.matmul(px[0:64, h * 64:(h + 1) * 64], lhsT=XT[:, h, :], rhs=X[:, h, :], start=True, stop=True),
                                 r=[X.k, XT.k], w=[px.k], inc=(h == 7))
                    P.op("act", lambda e: e.copy(XTn[:], v3(pxt[0:64, :])), r=[pxt.k], w=[XTn.k])
                    if lvl < 4:
                        P.op("dve", lambda e: e.tensor_copy(Xn[:], v3(px[0:64, :])), r=[px.k], w=[Xn.k])
                    pq = psb()
                    for h in range(8):
                        P.op("pe", lambda e: e.matmul(pq[0:64, h * 64:(h + 1) * 64], lhsT=XTn[:, h, :], rhs=Q[:, h, :], start=True, stop=True),
                             r=[XTn.k, Q.k], w=[pq.k], inc=(h == 7))
                    P.op("dve", lambda e: e.tensor_tensor(Qn[:], Q[:], v3(pq[0:64, :]), ALU.add), r=[Q.k, pq.k], w=[Qn.k])
                    X, XT, Q = Xn, XTn, Qn
                    yield "F"
                yield "END_FRONT"
                H, Hn = Hs[g % 2], Hs[(g + 1) % 2]
                Hb, Hbn = Hbs[g % 2], Hbs[(g + 1) % 2]
                pxs = psb()
                for h in range(8):
                    P.op("pe", lambda e: e.matmul(pxs[0:64, h * 64:(h + 1) * 64], lhsT=KR[:, h, 0:64], rhs=Hb[:, h, :], start=True, stop=False),
                         r=[KR.k, Hb.k], w=[pxs.k], inc=False)
                    P.op("pe", lambda e: e.matmul(pxs[0:64, h * 64:(h + 1) * 64], lhsT=BB[:, h, 0:64], rhs=vb[:, h * 64:(h + 1) * 64], start=False, stop=True),
                         r=[BB.k, vb.k], w=[pxs.k], inc=(h == 7))
                P.op("act", lambda e: e.copy(Xs[:], v3(pxs[0:64, :])), r=[pxs.k], w=[Xs.k])
                yield "B"
                pu = psb()
                for h in range(8):
                    P.op("pe", lambda e: e.matmul(pu[0:64, h * 64:(h + 1) * 64], lhsT=Q[:, h, :], rhs=Xs[:, h, :], start=True, stop=True),
                         r=[Q.k, Xs.k], w=[pu.k], inc=(h == 7))
                P.op("act", lambda e: e.mul(nU[:], v3(pu[0:64, :]), -1.0), r=[pu.k], w=[nU.k])
                yield "B"
                py, ph = psb(), psb()
                for h in range(8):
                    sl = slice(h * 64, (h + 1) * 64)
                    P.op("pe", lambda e: e.matmul(py[0:64, sl], lhsT=KR[:, h, 64:128], rhs=Hb[:, h, :], start=True, stop=False), r=[KR.k, Hb.k], w=[py.k], inc=False)
                    P.op("pe", lambda e: e.matmul(py[0:64, sl], lhsT=BB[:, h, 64:128], rhs=vb[:, sl], start=False, stop=False), r=[BB.k, vb.k], w=[py.k], inc=False)
                    P.op("pe", lambda e: e.matmul(py[0:64, sl], lhsT=MA[:, h, 64:128], rhs=nU[:, h, :], start=False, stop=True), r=[MA.k, nU.k], w=[py.k], inc=(h == 7))
                for h in range(8):
                    sl = slice(h * 64, (h + 1) * 64)
                    P.op("pe", lambda e: e.matmul(ph[0:64, sl], lhsT=Ke16[:, sl], rhs=vb[:, sl], start=True, stop=False), r=[Ke16.k, vb.k], w=[ph.k], inc=False)
                    P.op("pe", lambda e: e.matmul(ph[0:64, sl], lhsT=Be16[:, sl], rhs=nU[:, h, :], start=False, stop=True), r=[Be16.k, nU.k], w=[ph.k], inc=(h == 7))
                P.op("pool", lambda e: e.tensor_tensor(Hn[:], H[:], PC[:, :].unsqueeze(2).to_broadcast([64, 8, 64]), mm), r=[H.k, PC.k], w=[Hn.k])
                P.op("dve", lambda e: e.tensor_tensor(Hn[:], Hn[:], v3(ph[0:64, :]), ALU.add), r=[Hn.k, ph.k], w=[Hn.k])
                P.op("act", lambda e: e.copy(Hbn[:], Hn[:]), r=[Hn.k], w=[Hbn.k])
                yield "B"
                P.op("act", lambda e: e.copy(Y[:], py[0:64, :]), r=[py.k], w=[Y.k])
                P.op("dve", lambda e: e.tensor_reduce(s8[2][:], v3(Y[:]), AX.X, ALU.add), r=[Y.k], w=[s8[2].k])
                P.op("dve", lambda e: e.tensor_scalar(s8[2][:], s8[2][:], 1.0 / 64, None, mm), r=[s8[2].k], w=[s8[2].k])
                P.op("dve", lambda e: e.tensor_tensor(v3(Y[:]), v3(Y[:]), s8[2][:, :].unsqueeze(2).to_broadcast([64, 8, 64]), ALU.subtract),
                     r=[Y.k, s8[2].k], w=[Y.k])
                yield "B"
                P.op("pool", lambda e: e.tensor_tensor(tmpb[:], Y[:], Y[:], mm), r=[Y.k], w=[tmpb.k])
                P.op("dve", lambda e: e.tensor_reduce(s8[3][:], v3(tmpb[:]), AX.X, ALU.add), r=[tmpb.k], w=[s8[3].k])
                P.op("act", lambda e: e.activation(out=s8[3][:], in_=s8[3][:], func=AF.Sqrt, bias=64e-5, scale=1.0 / 64), r=[s8[3].k], w=[s8[3].k])
                P.op("dve", lambda e: e.reciprocal(s8[3][:], s8[3][:]), r=[s8[3].k], w=[s8[3].k])
                P.op("dve", lambda e: e.tensor_tensor(v3(Y[:]), v3(Y[:]), s8[3][:, :].unsqueeze(2).to_broadcast([64, 8, 64]), mm),
                     r=[Y.k, s8[3].k], w=[Y.k])
                P.op("pool", lambda e: e.tensor_tensor(Y[:], Y[:], gngb, mm), r=[Y.k, BV.k], w=[Y.k])
                P.op("pool", lambda e: e.tensor_tensor(Y[:], Y[:], gnbb, ALU.add), r=[Y.k, BV.k], w=[Y.k])
                P.op("dve", lambda e: e.tensor_tensor(Y[:], Y[:], bonus[:], ALU.add), r=[Y.k, bonus.k], w=[Y.k])
                P.op("dve", lambda e: e.tensor_tensor(Y[:], Y[:], g_[:], mm), r=[Y.k, g_.k], w=[Y.k])
                if "dbg_ya" in C.dbg:
                    P.dma("sp", C.dbg["dbg_ya"][g * 64:(g + 1) * 64, :], Y[:], r=[Y.k])
                yield "B"
                pb = psb()
                for q in range(4):
                    P.op("pe", lambda e: e.transpose(pb[:, q * 64:(q + 1) * 64], Y[:, q * 128:(q + 1) * 128], id64), r=[Y.k, C.ident_tok], w=[pb.k], inc=(q == 3))
                P.op("act", lambda e: e.copy(ys[:, :, t0:t0 + 64], hv(pb[:, 0:256], 4)), r=[pb.k], w=[ys.k])
                if ci == 7:
                    P.dma("sp", XTv(C.YT)[:, 0:4, s * 512:(s + 1) * 512], ys[:], r=[ys.k], w=[C.YT_tok[0][s]])

        pipeline2([(lambda s=s, ci=ci: chunk(s, ci)) for s in range(C.NS) for ci in range(8)], interleave=RWKV_INTERLEAVE)
        P.barrier()


def pipeline2(makers, interleave=True):
    if not interleave:
        for mk_ in makers:
            for _ in mk_():
                pass
        return
    prevB = None
    for mk_ in makers:
        g = mk_()
        while True:
            r = next(g)
            if prevB is not None:
                try:
                    next(prevB)
                except StopIteration:
                    prevB = None
            if r == "END_FRONT":
                break
        if prevB is not None:
            for _ in prevB:
                pass
        prevB = g
    if prevB is not None:
        for _ in prevB:
            pass


def psb(C):
    t, k = C.bank()
    return TL(t, k)


def phase_gla(C):
    nc, P, T, I = C.nc, C.P, C.T, C.I
    mm = ALU.mult
    with ExitStack() as es:
        WB = mk(P, [128, 8, B_COLS], BF16, "WB", es)
        for c in range(8):
            P.dma("pool", WB[:, c, :], I["w_in_even"][c * 128:(c + 1) * 128, A_COLS:EVEN_COLS], w=[WB.k])
        GW2 = mk(P, [16, 256], BF16, "GW2", es)
        P.dma("pool", GW2[:], I["b_gate_w2"], w=[GW2.k])
        gbb = mk(P, [128, 256], F32, "gbb", es)
        ngb = mk(P, [128, 512], F32, "ngb", es)
        bcast_load(C, gbb[:], I["b_gate_b"], 128, gbb.k)
        bcast_load(C, ngb[:], I["b_norm_g"], 128, ngb.k)
        tri = mk(P, [128, 2, 128], F32, "tri128", es)
        ncol = mk(P, [128, 1], F32, "ncol128", es)
        iu = mk(P, [128, 4, 128], F32, "iu128", es)
        for tl, nm in ((tri, "c_tri128"), (ncol, "c_ncol128"), (iu, "c_iu128")):
            P.dma("sp", tl[:], I[nm], w=[tl.k])
        id64 = C.ident[0:64, 0:64]

        def wt(name, shape, dt=F32):
            return mk(P, list(shape), dt, name, es)
        ATs = [wt("ATg", (128, 8, 512), BF16) for _ in range(2)]
        AL = wt("AL", (16, 512), BF16)
        l_ = wt("l", (128, 256))
        Eq, Ei, Ee = wt("Eq", (128, 256)), wt("Ei", (128, 256)), wt("Ee", (128, 256))
        PCg = wt("PCg", (64, 4))
        qd, ki, ke = wt("qd", (128, 256)), wt("ki", (128, 256)), wt("ke", (128, 256))
        v_ = wt("vg", (128, 512))
        qdT, kiT = wt("qdT", (64, 4, 128)), wt("kiT", (64, 4, 128))
        attT = wt("attT", (128, 4, 128))
        Ss = [wt("S%d" % n, (64, 4, 128)) for n in range(2)]
        o_ = wt("o", (128, 512))
        sq = wt("sqg", (128, 512))
        sl_ = wt("silu", (128, 512))
        m4 = wt("m4", (128, 4))
        yst = [wt("ystg", (128, 4, 512), BF16) for _ in range(2)]
        P.op("dve", lambda e: e.memset(Ss[0][:], 0.0), w=[Ss[0].k])
        for s in range(C.NS):
            at = ATs[s % 2]
            P.dma("sp", at[:], XTv(C.XT0)[:, :, 1 + s * 512:1 + (s + 1) * 512], r=[C.XT0_tok[s]], w=[at.k])
            pb = psb(C)
            for c in range(8):
                P.op("pe", lambda e: e.matmul(pb[0:16, :], lhsT=WB[:, c, 1536:1552], rhs=at[:, c, :], start=(c == 0), stop=(c == 7)),
                     r=[WB.k, at.k], w=[pb.k], inc=(c == 7))
            P.op("act", lambda e: e.copy(AL[:], pb[0:16, :]), r=[pb.k], w=[AL.k])
            ys = yst[s % 2]
            for ci in range(4):
                g = s * 4 + ci
                t0 = ci * 128
                pqk, pv, pg = psb(C), psb(C), psb(C)
                for pb, c0 in ((pqk, 0), (pv, 512), (pg, 1024)):
                    for c in range(8):
                        P.op("pe", lambda e: e.matmul(pb[:, :], lhsT=at[:, c, t0:t0 + 128], rhs=WB[:, c, c0:c0 + 512], start=(c == 0), stop=(c == 7)),
                             r=[WB.k, at.k], w=[pb.k], inc=(c == 7))
                pla = psb(C)
                P.op("pe", lambda e: e.matmul(pla[:, 0:256], lhsT=AL[:, t0:t0 + 128], rhs=GW2[:], start=True, stop=True), r=[AL.k, GW2.k], w=[pla.k])
                P.op("dve", lambda e: e.tensor_tensor(l_[:], pla[:, 0:256], gbb[:], ALU.add), r=[pla.k, gbb.k], w=[l_.k])
                P.op("act", lambda e: e.activation(out=l_[:], in_=l_[:], func=AF.Exp, scale=-1.0), r=[l_.k], w=[l_.k])
                P.op("act", lambda e: e.activation(out=l_[:], in_=l_[:], func=AF.Ln, bias=1.0), r=[l_.k], w=[l_.k])
                pbc, pba, ppc = psb(C), psb(C), psb(C)
                P.op("pe", lambda e: e.matmul(pbc[:, 0:256], lhsT=tri[:, 0, :], rhs=l_[:], start=True, stop=True), r=[tri.k, l_.k], w=[pbc.k])
                P.op("pe", lambda e: e.matmul(pba[:, 0:256], lhsT=tri[:, 1, :], rhs=l_[:], start=True, stop=True), r=[tri.k, l_.k], w=[pba.k])
                for h in range(4):
                    P.op("pe", lambda e: e.matmul(ppc[0:64, h:h + 1], lhsT=l_[:, h * 64:(h + 1) * 64], rhs=ncol[:], start=True, stop=True),
                         r=[l_.k, ncol.k], w=[ppc.k], inc=(h == 3))
                P.op("act", lambda e: e.activation(out=Eq[:], in_=pbc[:, 0:256], func=AF.Exp), r=[pbc.k], w=[Eq.k])
                P.op("act", lambda e: e.activation(out=Ei[:], in_=pbc[:, 0:256], func=AF.Exp, scale=-1.0), r=[pbc.k], w=[Ei.k])
                P.op("act", lambda e: e.activation(out=Ee[:], in_=pba[:, 0:256], func=AF.Exp), r=[pba.k], w=[Ee.k])
                P.op("act", lambda e: e.activation(out=PCg[:], in_=ppc[0:64, 0:4], func=AF.Exp), r=[ppc.k], w=[PCg.k])
                P.op("dve", lambda e: e.scalar_tensor_tensor(qd[:], pqk[:, 0:256], 0.125, Eq[:], mm, mm), r=[pqk.k, Eq.k], w=[qd.k])
                P.op("dve", lambda e: e.tensor_tensor(ki[:], pqk[:, 256:512], Ei[:], mm), r=[pqk.k, Ei.k], w=[ki.k])
                P.op("dve", lambda e: e.tensor_tensor(ke[:], pqk[:, 256:512], Ee[:], mm), r=[pqk.k, Ee.k], w=[ke.k])
                P.op("act", lambda e: e.copy(v_[:], pv[:, :]), r=[pv.k], w=[v_.k])
                P.op("act", lambda e: e.activation(out=sl_[:], in_=pg[:, :], func=AF.Silu), r=[pg.k], w=[sl_.k])
                for src, dst, en in ((qd, qdT, "act"), (ki, kiT, "dve")):
                    pb = psb(C)
                    for h in range(4):
                        P.op("pe", lambda e: e.transpose(pb[0:64, h * 128:(h + 1) * 128], src[:, h * 64:(h + 1) * 64], C.ident[:]),
                             r=[src.k, C.ident_tok], w=[pb.k], inc=(h == 3))
                    if en == "act":
                        P.op("act", lambda e: e.copy(dst[:], hv(pb[0:64, :], 4)), r=[pb.k], w=[dst.k])
                    else:
                        P.op("dve", lambda e: e.tensor_copy(dst[:], hv(pb[0:64, :], 4)), r=[pb.k], w=[dst.k])
                patt = psb(C)
                for h in range(4):
                    P.op("pe", lambda e: e.matmul(patt[:, h * 128:(h + 1) * 128], lhsT=kiT[:, h, :], rhs=qdT[:, h, :], start=True, stop=True),
                         r=[kiT.k, qdT.k], w=[patt.k], inc=(h == 3))
                P.op("dve", lambda e: e.tensor_tensor(attT[:], hv(patt[:, :], 4), iu[:], mm), r=[patt.k, iu.k], w=[attT.k])
                S, Sn = Ss[g % 2], Ss[(g + 1) % 2]
                po, pS = psb(C), psb(C)
                for h in range(4):
                    sl = slice(h * 128, (h + 1) * 128)
                    P.op("pe", lambda e: e.matmul(po[:, sl], lhsT=attT[:, h, :], rhs=v_[:, sl], start=True, stop=False), r=[attT.k, v_.k], w=[po.k], inc=False)
                    P.op("pe", lambda e: e.matmul(po[:, sl], lhsT=qdT[:, h, :], rhs=S[:, h, :], start=False, stop=True), r=[qdT.k, S.k], w=[po.k], inc=(h == 3))
                for h in range(4):
                    sl = slice(h * 128, (h + 1) * 128)
                    P.op("pe", lambda e: e.matmul(pS[0:64, sl], lhsT=ke[:, h * 64:(h + 1) * 64], rhs=v_[:, sl], start=True, stop=True), r=[ke.k, v_.k], w=[pS.k], inc=(h == 3))
                P.op("pool", lambda e: e.tensor_tensor(Sn[:], S[:], PCg[:, :].unsqueeze(2).to_broadcast([64, 4, 128]), mm), r=[S.k, PCg.k], w=[Sn.k])
                P.op("dve", lambda e: e.tensor_tensor(Sn[:], Sn[:], hv(pS[0:64, :], 4), ALU.add), r=[Sn.k, pS.k], w=[Sn.k])
                P.op("act", lambda e: e.copy(o_[:], po[:, :]), r=[po.k], w=[o_.k])
                P.op("pool", lambda e: e.tensor_tensor(sq[:], o_[:], o_[:], mm), r=[o_.k], w=[sq.k])
                P.op("dve", lambda e: e.tensor_reduce(m4[:], hv(sq[:], 4), AX.X, ALU.add), r=[sq.k], w=[m4.k])
                P.op("act", lambda e: e.activation(out=m4[:], in_=m4[:], func=AF.Sqrt, bias=1e-5, scale=1.0 / 128), r=[m4.k], w=[m4.k])
                P.op("dve", lambda e: e.reciprocal(m4[:], m4[:]), r=[m4.k], w=[m4.k])
                P.op("dve", lambda e: e.tensor_tensor(hv(o_[:], 4), hv(o_[:], 4), m4[:, :].unsqueeze(2).to_broadcast([128, 4, 128]), mm), r=[o_.k, m4.k], w=[o_.k])
                P.op("pool", lambda e: e.tensor_tensor(o_[:], o_[:], ngb[:], mm), r=[o_.k, ngb.k], w=[o_.k])
                P.op("dve", lambda e: e.tensor_tensor(o_[:], o_[:], sl_[:], mm), r=[o_.k, sl_.k], w=[o_.k])
                if "dbg_yb" in C.dbg:
                    P.dma("sp", C.dbg["dbg_yb"][g * 128:(g + 1) * 128, :], o_[:], r=[o_.k])
                pb = psb(C)
                for q in range(4):
                    P.op("pe", lambda e: e.transpose(pb[:, q * 128:(q + 1) * 128], o_[:, q * 128:(q + 1) * 128], C.ident[:]), r=[o_.k, C.ident_tok], w=[pb.k], inc=(q == 3))
                P.op("act", lambda e: e.copy(ys[:, :, t0:t0 + 128], hv(pb[:, :], 4)), r=[pb.k], w=[ys.k])
            P.dma("sp", XTv(C.YT)[:, 4:8, s * 512:(s + 1) * 512], ys[:], r=[ys.k], w=[C.YT_tok[1][s]])
        P.barrier()


def ln_inplace(C, xt, gb, bb, st, junk):
    P = C.P
    P.op("dve", lambda e: e.tensor_reduce(st[:, 0:1], xt[:], AX.X, ALU.add), r=[xt.k], w=[st.k])
    P.op("dve", lambda e: e.tensor_scalar(st[:, 0:1], st[:, 0:1], 1.0 / D, None, ALU.mult), r=[st.k], w=[st.k])
    P.op("dve", lambda e: e.tensor_scalar(xt[:], xt[:], st[:, 0:1], None, ALU.subtract), r=[xt.k, st.k], w=[xt.k])
    P.op("act", lambda e: e.activation(out=junk[:], in_=xt[:], func=AF.Square, accum_out=st[:, 1:2]), r=[xt.k], w=[junk.k, st.k])
    P.op("act", lambda e: e.activation(out=st[:, 1:2], in_=st[:, 1:2], func=AF.Sqrt, bias=LN_EPS, scale=1.0 / D), r=[st.k], w=[st.k])
    P.op("dve", lambda e: e.reciprocal(st[:, 1:2], st[:, 1:2]), r=[st.k], w=[st.k])
    P.op("dve", lambda e: e.scalar_tensor_tensor(xt[:], xt[:], st[:, 1:2], gb[:], ALU.mult, ALU.mult), r=[xt.k, st.k, gb.k], w=[xt.k])
    P.op("pool", lambda e: e.tensor_tensor(xt[:], xt[:], bb[:], ALU.add), r=[xt.k, bb.k], w=[xt.k])


def tile_to_stage(C, xt, stage, j):
    P = C.P
    for half in range(2):
        pb = psb(C)
        for c4 in range(4):
            c = half * 4 + c4
            P.op("pe", lambda e: e.transpose(pb[:, c4 * 128:(c4 + 1) * 128], xt[:, c * 128:(c + 1) * 128], C.ident[:]),
                 r=[xt.k, C.ident_tok], w=[pb.k], inc=(c4 == 3))
        dst = stage[:, half * 4:(half + 1) * 4, j * 128:(j + 1) * 128]
        src = hv(pb[:, :], 4)
        if half == 0:
            P.op("act", lambda e: e.copy(dst, src), r=[pb.k], w=[stage.k])
        else:
            P.op("dve", lambda e: e.tensor_copy(dst, src), r=[pb.k], w=[stage.k])


def phase_outproj(C, w_out, srcYT, srcYT_toks, resid, resid_toks, lng, lnb, dstH, dstH_tok, dstHT, dstHT_tok, dbgname=None):
    nc, P, T, I = C.nc, C.P, C.T, C.I
    with ExitStack() as es:
        WO = mk(P, [128, 8, D], BF16, "WO", es)
        for c in range(8):
            P.dma("pool", WO[:, c, :], w_out[c * 128:(c + 1) * 128, :], w=[WO.k])
        gb = mk(P, [128, D], F32, "lng", es)
        bb = mk(P, [128, D], F32, "lnb", es)
        bcast_load(C, gb[:], lng, 128, gb.k)
        bcast_load(C, bb[:], lnb, 128, bb.k)
        yts = [mk(P, [128, 8, 512], BF16, "yt", es) for _ in range(2)]
        xts = [mk(P, [128, D], F32, "xres", es) for _ in range(2)]
        sts = [mk(P, [128, 2], F32, "lnst", es) for _ in range(2)]
        junk = mk(P, [128, D], BF16, "junk", es)
        stg = [mk(P, [128, 8, 512], BF16, "hstg", es) for _ in range(2)]
        for s in range(C.NS):
            yt = yts[s % 2]
            P.dma("sp", yt[:], XTv(srcYT)[:, :, s * 512:(s + 1) * 512], r=srcYT_toks(s), w=[yt.k])
            sg_ = stg[s % 2]
            for j in range(4):
                i = s * 4 + j
                xt = xts[i % 2]
                P.dma("sp", xt[:], resid[i * 128:(i + 1) * 128, :], r=resid_toks(i), w=[xt.k])
                for half in range(2):
                    pb = psb(C)
                    for c in range(8):
                        P.op("pe", lambda e: e.matmul(pb[:, :], lhsT=yt[:, c, j * 128:(j + 1) * 128], rhs=WO[:, c, half * 512:(half + 1) * 512], start=(c == 0), stop=(c == 7)),
                             r=[yt.k, WO.k], w=[pb.k], inc=(c == 7))
                    P.op("dve", lambda e: e.scalar_tensor_tensor(xt[:, half * 512:(half + 1) * 512], xt[:, half * 512:(half + 1) * 512], DN_ALPHA, pb[:, :], ALU.mult, ALU.add),
                         r=[xt.k, pb.k], w=[xt.k])
                ln_inplace(C, xt, gb, bb, sts[i % 2], junk)
                P.dma("sp", dstH[i * 128:(i + 1) * 128, :], xt[:], r=[xt.k], w=[dstH_tok[i]])
                tile_to_stage(C, xt, sg_, j)
            P.dma("sp", XTv(dstHT)[:, :, s * 512:(s + 1) * 512], sg_[:], r=[sg_.k], w=[dstHT_tok[s]])
        P.barrier()


def phase_moe(C, l, srcH, srcH_tok, srcHT, srcHT_tok, dstX, dstX_tok, dstXT, dstXT_tok):
    nc, P, T, I = C.nc, C.P, C.T, C.I
    mm = ALU.mult
    with ExitStack() as es:
        WD = mk(P, [128, 32, D], BF16, "WD", es)
        for c4 in range(4):
            P.dma("sp", WD[:, c4 * 8:(c4 + 1) * 8, :], C.WD16[l].rearrange("p (c d) -> p c d", c=32)[:, c4 * 8:(c4 + 1) * 8, :], r=[C.WD16_tok[l]], w=[WD.k])
        RW = mk(P, [128, 8, NE], BF16, "RW", es)
        P.dma("pool", RW[:], I["router_w"].rearrange("(c p) e -> p c e", p=128), w=[RW.k])
        rbb = mk(P, [128, NE], F32, "rbb", es)
        bcast_load(C, rbb[:], I["router_bias"], 128, rbb.k)
        SEL = mk(P, [16, 16, 128], BF16, "SEL", es)
        P.dma("pool", SEL[:], I["c_sel"], w=[SEL.k])
        gb = mk(P, [128, D], F32, "lng", es)
        bb = mk(P, [128, D], F32, "lnb", es)
        bcast_load(C, gb[:], I["ln2_g"][l:l + 1, :], 128, gb.k)
        bcast_load(C, bb[:], I["ln2_b"][l:l + 1, :], 128, bb.k)
        hts = [mk(P, [128, 8, 512], BF16, "hT", es) for _ in range(2)]
        xts = [mk(P, [128, D], F32, "hres", es) for _ in range(2)]
        sts = [mk(P, [128, 2], F32, "lnst", es) for _ in range(2)]
        junk = mk(P, [128, D], BF16, "junk", es)
        stg = [mk(P, [128, 8, 512], BF16, "xstg", es) for _ in range(2)]
        actT = mk(P, [128, 32, 512], BF16, "actT", es)
        combT = mk(P, [16, 512], BF16, "combT", es)
        WGUs = [mk(P, [128, 2, 8, DE], BF16, "WGU", es) for _ in range(3)]
        sgl = [mk(P, [128, 512], F32, "sgl", es) for _ in range(2)]
        s_ = mk(P, [128, NE], F32, "rs", es)
        sel = mk(P, [128, NE], F32, "rsel", es)
        pr = mk(P, [128, 4, 6], F32, "rpr", es)
        gs = mk(P, [128, 4], F32, "rgs", es)
        t1 = mk(P, [128, 4], F32, "rt1", es)
        m1 = mk(P, [128, 2], F32, "rm1", es)
        selm = mk(P, [128, NE], F32, "rselm", es)
        sel2 = mk(P, [128, NE], F32, "rsel2", es)
        comb = mk(P, [128, NE], F32, "rcomb", es)
        nwl = [0]

        def load_w(e):
            b = nwl[0] % 3
            nwl[0] += 1
            P.dma("sp", WGUs[b][:], C.WGU16[l][e].rearrange("p (t c f) -> p t c f", t=2, c=8), r=[C.WGU16_tok[l][e]], w=[WGUs[b].k])
            return WGUs[b]

        def g4(t):
            return t[:, :].rearrange("p (g e) -> p g e", g=4)

        for s in range(C.NS):
            hT = hts[s % 2]
            P.dma("sp", hT[:], XTv(srcHT)[:, :, s * 512:(s + 1) * 512], r=[srcHT_tok[s]], w=[hT.k])
            for j in range(4):
                plg = psb(C)
                for c in range(8):
                    P.op("pe", lambda e: e.matmul(plg[:, 0:NE], lhsT=hT[:, c, j * 128:(j + 1) * 128], rhs=RW[:, c, :], start=(c == 0), stop=(c == 7)),
                         r=[hT.k, RW.k], w=[plg.k], inc=(c == 7))
                P.op("act", lambda e: e.activation(out=s_[:], in_=plg[:, 0:NE], func=AF.Sigmoid), r=[plg.k], w=[s_.k])
                P.op("dve", lambda e: e.tensor_tensor(sel[:], s_[:], rbb[:], ALU.add), r=[s_.k, rbb.k], w=[sel.k])
                s4 = g4(sel)
                P.op("dve", lambda e: e.tensor_tensor(pr[:, :, 0:3], s4[:, :, 0:3], s4[:, :, 1:4], ALU.add), r=[sel.k], w=[pr.k])
                P.op("dve", lambda e: e.tensor_tensor(pr[:, :, 3:5], s4[:, :, 0:2], s4[:, :, 2:4], ALU.add), r=[sel.k], w=[pr.k])
                P.op("dve", lambda e: e.tensor_tensor(pr[:, :, 5:6], s4[:, :, 0:1], s4[:, :, 3:4], ALU.add), r=[sel.k], w=[pr.k])
                P.op("dve", lambda e: e.tensor_reduce(gs[:], pr[:], AX.X, ALU.max), r=[pr.k], w=[gs.k])
                P.op("dve", lambda e: e.tensor_reduce(m1[:, 0:1], gs[:], AX.X, ALU.max), r=[gs.k], w=[m1.k])
                P.op("dve", lambda e: e.tensor_scalar(gs[:], gs[:], m1[:, 0:1], None, ALU.is_ge), r=[gs.k, m1.k], w=[gs.k])
                P.op("dve", lambda e: e.tensor_scalar(t1[:], gs[:], -1.0, 1e30, ALU.add, ALU.mult), r=[gs.k], w=[t1.k])
                P.op("dve", lambda e: e.tensor_tensor(g4(selm), s4, gs[:, :].unsqueeze(2).to_broadcast([128, 4, 4]), mm), r=[sel.k, gs.k], w=[selm.k])
                P.op("dve", lambda e: e.tensor_tensor(g4(selm), g4(selm), t1[:, :].unsqueeze(2).to_broadcast([128, 4, 4]), ALU.add), r=[selm.k, t1.k], w=[selm.k])
                P.op("dve", lambda e: e.tensor_reduce(m1[:, 0:1], selm[:], AX.X, ALU.max), r=[selm.k], w=[m1.k])
                P.op("dve", lambda e: e.tensor_scalar(sel2[:], selm[:], m1[:, 0:1], None, ALU.is_ge), r=[selm.k, m1.k], w=[sel2.k])
                P.op("dve", lambda e: e.scalar_tensor_tensor(sel2[:], sel2[:], -1e30, selm[:], mm, ALU.add), r=[sel2.k, selm.k], w=[sel2.k])
                P.op("dve", lambda e: e.tensor_reduce(m1[:, 1:2], sel2[:], AX.X, ALU.max), r=[sel2.k], w=[m1.k])
                P.op("dve", lambda e: e.tensor_scalar(sel2[:], selm[:], m1[:, 1:2], None, ALU.is_ge), r=[selm.k, m1.k], w=[sel2.k])
                P.op("dve", lambda e: e.tensor_tensor(comb[:], s_[:], sel2[:], mm), r=[s_.k, sel2.k], w=[comb.k])
                P.op("dve", lambda e: e.tensor_reduce(m1[:, 0:1], comb[:], AX.X, ALU.add), r=[comb.k], w=[m1.k])
                P.op("dve", lambda e: e.reciprocal(m1[:, 0:1], m1[:, 0:1]), r=[m1.k], w=[m1.k])
                P.op("dve", lambda e: e.tensor_scalar(comb[:], comb[:], m1[:, 0:1], None, mm), r=[comb.k, m1.k], w=[comb.k])
                pct = psb(C)
                P.op("pe", lambda e: e.transpose(pct[0:16, 0:128], comb[:, :], C.ident[:]), r=[comb.k, C.ident_tok], w=[pct.k])
                P.op("act", lambda e: e.copy(combT[:, j * 128:(j + 1) * 128], pct[0:16, 0:128]), r=[pct.k], w=[combT.k])
            for ex in range(NE):
                WGU = load_w(ex)
                pcb = psb(C)
                P.op("pe", lambda e: e.matmul(pcb[:, :], lhsT=SEL[:, ex, :], rhs=combT[:, :], start=True, stop=True), r=[SEL.k, combT.k], w=[pcb.k])
                for f in range(2):
                    pG, pU = psb(C), psb(C)
                    for pb, ti in ((pG, 0), (pU, 1)):
                        for c in range(8):
                            P.op("pe", lambda e: e.matmul(pb[:, :], lhsT=WGU[:, ti, c, f * 128:(f + 1) * 128], rhs=hT[:, c, :], start=(c == 0), stop=(c == 7)),
                                 r=[WGU.k, hT.k], w=[pb.k], inc=(c == 7))
                    sg_ = sgl[(ex * 2 + f) % 2]
                    P.op("act", lambda e: e.activation(out=sg_[:], in_=pG[:, :], func=AF.Silu), r=[pG.k], w=[sg_.k])
                    P.op("dve", lambda e: e.tensor_tensor(sg_[:], sg_[:], pU[:, :], mm), r=[sg_.k, pU.k], w=[sg_.k])
                    P.op("dve", lambda e: e.tensor_tensor(actT[:, ex * 2 + f, :], sg_[:], pcb[:, :], mm), r=[sg_.k, pcb.k], w=[actT.k])
            xs_ = stg[s % 2]
            for j in range(4):
                i = s * 4 + j
                xt = xts[i % 2]
                P.dma("sp", xt[:], srcH[i * 128:(i + 1) * 128, :], r=[srcH_tok[i]], w=[xt.k])
                for half in range(2):
                    pb = psb(C)
                    for c in range(32):
                        P.op("pe", lambda e: e.matmul(pb[:, :], lhsT=actT[:, c, j * 128:(j + 1) * 128], rhs=WD[:, c, half * 512:(half + 1) * 512], start=(c == 0), stop=(c == 31)),
                             r=[actT.k, WD.k], w=[pb.k], inc=(c == 31))
                    P.op("dve", lambda e: e.scalar_tensor_tensor(xt[:, half * 512:(half + 1) * 512], xt[:, half * 512:(half + 1) * 512], DN_ALPHA, pb[:, :], ALU.mult, ALU.add),
                         r=[xt.k, pb.k], w=[xt.k])
                ln_inplace(C, xt, gb, bb, sts[i % 2], junk)
                P.dma("sp", dstX[i * 128:(i + 1) * 128, :], xt[:], r=[xt.k], w=[dstX_tok[i]])
                if dstXT is not None:
                    tile_to_stage(C, xt, xs_, j)
            if dstXT is not None:
                P.dma("sp", XTv(dstXT)[:, :, s * 512:(s + 1) * 512], xs_[:], r=[xs_.k], w=[dstXT_tok[s]])
        P.barrier()


def rope_consts(T):
    pos = np.arange(T, dtype=np.float64)
    c = {}
    for name, half in (("k", 64), ("i", 32)):
        inv = 10000.0 ** (-np.arange(half, dtype=np.float64) / half)
        ang = (pos.astype(np.float32)[:, None] * inv.astype(np.float32)[None, :]).astype(np.float32).astype(np.float64)
        cs = np.cos(ang).astype(np.float32).reshape(T // 128, 128, half).transpose(1, 0, 2)
        sn = np.sin(ang).astype(np.float32).reshape(T // 128, 128, half).transpose(1, 0, 2)
        c["cos_" + name] = np.ascontiguousarray(cs)
        c["sin_" + name] = np.ascontiguousarray(sn)
    q = np.arange(128)[:, None]
    s = np.arange(128)[None, :]
    c["cbias"] = np.where(s <= q, 0.0, -1e30).astype(np.float32)
    c["halfpow"] = (0.5 ** np.arange(1, 33, dtype=np.float64)).astype(np.float32).reshape(1, 32)
    c["tiebias"] = (-1e-6 * np.arange(T, dtype=np.float64)).astype(np.float32).reshape(1, T)
    return c


def rope_tm(C, dst_ap, dst_k, src, src_k, cosb, sinb, nh, half, ta_ap, ta_k, tb_ap, tb_k, rdeps):
    P = C.P
    mm = ALU.mult
    n = nh * 2
    s3 = src.rearrange("p (n f) -> p n f", n=n)
    cb = cosb.unsqueeze(1).to_broadcast([128, n, half])
    sb_ = sinb.unsqueeze(1).to_broadcast([128, n, half])
    a3 = ta_ap.rearrange("p (n f) -> p n f", n=n)
    b3 = tb_ap.rearrange("p (n f) -> p n f", n=n)
    P.op("dve", lambda e: e.tensor_tensor(a3, s3, cb, mm), r=[src_k] + rdeps, w=[ta_k])
    P.op("dve", lambda e: e.tensor_tensor(b3, s3, sb_, mm), r=[src_k] + rdeps, w=[tb_k])
    a4 = ta_ap.rearrange("p (h t f) -> p h t f", h=nh, t=2)
    b4 = tb_ap.rearrange("p (h t f) -> p h t f", h=nh, t=2)
    d4 = dst_ap.rearrange("p (h t f) -> p h t f", h=nh, t=2)
    P.op("pool", lambda e: e.tensor_tensor(d4[:, :, 0, :], a4[:, :, 0, :], b4[:, :, 1, :], ALU.subtract), r=[ta_k, tb_k], w=[dst_k])
    P.op("pool", lambda e: e.tensor_tensor(d4[:, :, 1, :], a4[:, :, 1, :], b4[:, :, 0, :], ALU.add), r=[ta_k, tb_k], w=[dst_k])


def phase_dsa(C):
    nc, P, T, I = C.nc, C.P, C.T, C.I
    mm = ALU.mult
    KT = min(256, T // 4)
    NIT = 25
    SCALE = 128 ** -0.5
    C.bank_pool = [0, 1, 2, 3, 4]
    with ExitStack() as es:
        def wt(name, shape, dt=F32):
            return mk(P, list(shape), dt, name, es)
        WQ = wt("WQ", (128, 8, 1024), BF16)
        WR = wt("WR", (128, 8, 580), BF16)
        for c in range(8):
            P.dma("pool", WQ[:, c, :], I["w_in_odd"][c * 128:(c + 1) * 128, 0:1024], w=[WQ.k])
            P.dma("pool", WR[:, c, :], I["w_in_odd"][c * 128:(c + 1) * 128, 1024:1604], w=[WR.k])
        RTs = [[wt("CK", (128, 4, 64)), wt("SK", (128, 4, 64)), wt("CI", (128, 4, 32)), wt("SI", (128, 4, 32))] for _ in range(2)]

        def load_tables(s):
            tl = RTs[s % 2]
            for t_, nm in zip(tl, ("c_cos_k", "c_sin_k", "c_cos_i", "c_sin_i")):
                P.dma("sp", t_[:], I[nm][:, s * 4:(s + 1) * 4, :], w=[t_.k])
            return tl
        BIAS = wt("BIAS", (128, T))
        bcast_load(C, BIAS[:], I["c_tiebias"], 128, BIAS.k)
        CB = wt("CB", (128, 128))
        P.dma("sp", CB[:], I["c_cbias"], w=[CB.k])
        ikg, ikb = wt("ikg", (128, 64)), wt("ikb", (128, 64))
        bcast_load(C, ikg[:], I["c_ik_ln_g"], 128, ikg.k)
        bcast_load(C, ikb[:], I["c_ik_ln_b"], 128, ikb.k)
        kT = wt("kT", (128, T), BF16)
        ikT = wt("ikT", (128, T), BF16)
        Vx = wt("Vx", (128, C.NT, 129), BF16)
        P.op("dve", lambda e: e.memset(Vx[:, :, 128:129], 1.0), w=[Vx.k])
        xTs = [wt("xTd", (128, 8, 512), BF16) for _ in range(2)]
        ta, tb = wt("ropeA", (128, 512)), wt("ropeB", (128, 512))
        kr = wt("kr", (128, 128))
        ikr = wt("ikr", (128, 128))
        st2 = wt("st2", (128, 2))
        for s in range(C.NS):
            xT = xTs[s % 2]
            P.dma("sp", xT[:], XTv(C.XT1)[:, :, s * 512:(s + 1) * 512], r=[C.XT1_tok[s]], w=[xT.k])
            CK, SK, CI, SI = load_tables(s)
            for j in range(4):
                i = s * 4 + j
                pb = psb(C)
                for c in range(8):
                    P.op("pe", lambda e: e.matmul(pb[:, 0:256], lhsT=xT[:, c, j * 128:(j + 1) * 128], rhs=WR[:, c, 0:256], start=(c == 0), stop=(c == 7)),
                         r=[xT.k, WR.k], w=[pb.k], inc=False)
                for c in range(8):
                    P.op("pe", lambda e: e.matmul(pb[:, 256:320], lhsT=xT[:, c, j * 128:(j + 1) * 128], rhs=WR[:, c, 512:576], start=(c == 0), stop=(c == 7)),
                         r=[xT.k, WR.k], w=[pb.k], inc=(c == 7))
                rope_tm(C, kr[:, :], kr.k, pb[:, 0:128], pb.k, CK[:, j, :], SK[:, j, :], 1, 64, ta[:, 0:128], ta.k, tb[:, 0:128], tb.k, [CK.k, SK.k])
                P.op("act", lambda e: e.copy(Vx[:, i, 0:128], pb[:, 128:256]), r=[pb.k], w=[Vx.k])
                P.op("dve", lambda e: e.tensor_reduce(st2[:, 0:1], pb[:, 256:320], AX.X, ALU.add), r=[pb.k], w=[st2.k])
                P.op("dve", lambda e: e.tensor_scalar(st2[:, 0:1], st2[:, 0:1], 1.0 / 64, None, mm), r=[st2.k], w=[st2.k])
                P.op("dve", lambda e: e.tensor_scalar(ikr[:, 0:64], pb[:, 256:320], st2[:, 0:1], None, ALU.subtract), r=[pb.k, st2.k], w=[ikr.k])
                P.op("act", lambda e: e.activation(out=ikr[:, 64:128], in_=ikr[:, 0:64], func=AF.Square, accum_out=st2[:, 1:2]), r=[ikr.k], w=[ikr.k, st2.k])
                P.op("act", lambda e: e.activation(out=st2[:, 1:2], in_=st2[:, 1:2], func=AF.Sqrt, bias=LN_EPS, scale=1.0 / 64), r=[st2.k], w=[st2.k])
                P.op("dve", lambda e: e.reciprocal(st2[:, 1:2], st2[:, 1:2]), r=[st2.k], w=[st2.k])
                P.op("dve", lambda e: e.scalar_tensor_tensor(ikr[:, 0:64], ikr[:, 0:64], st2[:, 1:2], ikg[:], mm, mm), r=[ikr.k, st2.k, ikg.k], w=[ikr.k])
                P.op("dve", lambda e: e.tensor_tensor(ikr[:, 64:128], ikr[:, 0:64], ikb[:], ALU.add), r=[ikr.k, ikb.k], w=[ikr.k])
                ikn = TL(ikr.t, ikr.k)
                rope_src = ikr[:, 64:128]
                n = 2
                s3 = rope_src.rearrange("p (n f) -> p n f", n=n)
                cb = CI[:, j, :].unsqueeze(1).to_broadcast([128, n, 32])
                sb_ = SI[:, j, :].unsqueeze(1).to_broadcast([128, n, 32])
                a3 = ta[:, 0:64].rearrange("p (n f) -> p n f", n=n)
                b3 = tb[:, 0:64].rearrange("p (n f) -> p n f", n=n)
                P.op("dve", lambda e: e.tensor_tensor(a3, s3, cb, mm), r=[ikr.k, CI.k], w=[ta.k])
                P.op("dve", lambda e: e.tensor_tensor(b3, s3, sb_, mm), r=[ikr.k, SI.k], w=[tb.k])
                P.op("pool", lambda e: e.tensor_tensor(ikr[:, 0:32], ta[:, 0:32], tb[:, 32:64], ALU.subtract), r=[ta.k, tb.k], w=[ikr.k])
                P.op("pool", lambda e: e.tensor_tensor(ikr[:, 32:64], ta[:, 32:64], tb[:, 0:32], ALU.add), r=[ta.k, tb.k], w=[ikr.k])
                P.op("pool", lambda e: e.tensor_copy(ikr[:, 64:128], ikr[:, 0:64]), r=[ikr.k], w=[ikr.k])
                pt = psb(C)
                P.op("pe", lambda e: e.transpose(pt[:, 0:128], kr[:, :], C.ident[:]), r=[kr.k, C.ident_tok], w=[pt.k], inc=False)
                P.op("pe", lambda e: e.transpose(pt[:, 128:256], ikr[:, :], C.ident[:]), r=[ikr.k, C.ident_tok], w=[pt.k])
                P.op("act", lambda e: e.copy(kT[:, i * 128:(i + 1) * 128], pt[:, 0:128]), r=[pt.k], w=[kT.k])
                P.op("act", lambda e: e.mul(ikT[:, i * 128:(i + 1) * 128], pt[:, 128:256], 0.125), r=[pt.k], w=[ikT.k])
        SC = wt("SC", (128, T))
        MASKs = [wt("MASK", (128, T)) for _ in range(2)]
        junk = wt("junkd", (128, T), BF16)
        qr = wt("qr", (128, 1024))
        iqr = wt("iqr", (128, 256))
        qTs = [wt("qT", (128, 8, 128), BF16) for _ in range(2)]
        iqT = wt("iqT", (128, 2, 128), BF16)
        iws = wt("iws", (128, 4))
        rl = [wt("rl%d" % n, (128, 512)) for n in range(2)]
        bs = wt("bs", (128, 8))
        Dk = wt("Dk", (128, NIT))
        HK = wt("HK", (128, NIT))
        bcast_load(C, HK[:], I["c_halfpow"][0:1, 0:NIT], 128, HK.k)
        mT4 = [wt("mT4_%d" % n, (128, 4, 128), BF16) for n in range(2)]
        pTs = [wt("pT%d" % n, (128, 4, 128), BF16) for n in range(4)]
        o_ = wt("od", (128, 1024))
        rs8 = wt("rs8", (128, 8))
        ostg = [wt("ostg", (128, 8, 512), BF16) for _ in range(2)]
        accb = [TL(*C.banks[b]) for b in (5, 6, 7)]
        MBIG = 30000.0
        identb = wt("identb", (128, 128), BF16)
        P.op("dve", lambda e: e.tensor_copy(identb[:], C.ident[:]), r=[C.ident_tok], w=[identb.k])
        acc_of = [(0, 0), (0, 1), (0, 2), (1, 0), (1, 1), (1, 2), (2, 0), (2, 1)]
        def qblock(s, j):
            if True:
                i = s * 4 + j
                L = (i + 1) * 128
                xT = xTs[s % 2]
                og = ostg[s % 2]
                qT = qTs[i % 2]
                MASK = MASKs[i % 2]
                if j == 0:
                    P.dma("sp", xT[:], XTv(C.XT1)[:, :, s * 512:(s + 1) * 512], r=[C.XT1_tok[s]], w=[xT.k])
                    load_tables(s)
                CK, SK, CI, SI = RTs[s % 2]
                pq = [psb(C), psb(C)]
                for half in range(2):
                    for c in range(8):
                        P.op("pe", lambda e: e.matmul(pq[half][:, :], lhsT=xT[:, c, j * 128:(j + 1) * 128], rhs=WQ[:, c, half * 512:(half + 1) * 512], start=(c == 0), stop=(c == 7)),
                             r=[xT.k, WQ.k], w=[pq[half].k], inc=(c == 7))
                piq = psb(C)
                for c in range(8):
                    P.op("pe", lambda e: e.matmul(piq[:, 0:256], lhsT=xT[:, c, j * 128:(j + 1) * 128], rhs=WR[:, c, 256:512], start=(c == 0), stop=(c == 7)),
                         r=[xT.k, WR.k], w=[piq.k], inc=False)
                for c in range(8):
                    P.op("pe", lambda e: e.matmul(piq[:, 256:260], lhsT=xT[:, c, j * 128:(j + 1) * 128], rhs=WR[:, c, 576:580], start=(c == 0), stop=(c == 7)),
                         r=[xT.k, WR.k], w=[piq.k], inc=(c == 7))
                for half in range(2):
                    hs = slice(half * 512, (half + 1) * 512)
                    rope_tm(C, qr[:, hs], qr.k, pq[half][:, :], pq[half].k, CK[:, j, :], SK[:, j, :], 4, 64,
                            ta[:, 0:512], ta.k, tb[:, 0:512], tb.k, [CK.k, SK.k])
                rope_tm(C, iqr[:, :], iqr.k, piq[:, 0:256], piq.k, CI[:, j, :], SI[:, j, :], 4, 32, ta[:, 0:256], ta.k, tb[:, 0:256], tb.k, [CI.k, SI.k])
                P.op("act", lambda e: e.mul(iws[:], piq[:, 256:260], 0.5), r=[piq.k], w=[iws.k])
                for half in range(2):
                    pb = psb(C)
                    for c4 in range(4):
                        h = half * 4 + c4
                        P.op("pe", lambda e: e.transpose(pb[:, c4 * 128:(c4 + 1) * 128], qr[:, h * 128:(h + 1) * 128], C.ident[:]), r=[qr.k, C.ident_tok], w=[pb.k], inc=(c4 == 3))
                    P.op("act", lambda e: e.copy(qT[:, half * 4:(half + 1) * 4, :], hv(pb[:, :], 4)), r=[pb.k], w=[qT.k])
                pb = psb(C)
                for c2 in range(2):
                    P.op("pe", lambda e: e.transpose(pb[:, c2 * 128:(c2 + 1) * 128], iqr[:, c2 * 128:(c2 + 1) * 128], C.ident[:]), r=[iqr.k, C.ident_tok], w=[pb.k], inc=(c2 == 1))
                P.op("act", lambda e: e.copy(iqT[:], hv(pb[:, 0:256], 2)), r=[pb.k], w=[iqT.k])
                yield "F"
                for k0 in range(0, L, 512):
                    kw = min(512, L - k0)
                    for h in range(4):
                        ph = psb(C)
                        pl = (h % 2) * 64
                        P.op("pe", lambda e: e.matmul(ph[:, 0:kw], lhsT=iqT[pl:pl + 64, h // 2, :], rhs=ikT[pl:pl + 64, k0:k0 + kw], start=True, stop=True),
                             r=[iqT.k, ikT.k], w=[ph.k])
                        r_ = rl[h % 2]
                        P.op("act", lambda e: e.activation(out=r_[:, 0:kw], in_=ph[:, 0:kw], func=AF.Relu), r=[ph.k], w=[r_.k])
                        if h == 0:
                            P.op("dve", lambda e: e.scalar_tensor_tensor(SC[:, k0:k0 + kw], r_[:, 0:kw], iws[:, 0:1], BIAS[:, k0:k0 + kw], mm, ALU.add), r=[r_.k, iws.k, BIAS.k], w=[SC.k])
                        else:
                            P.op("dve", lambda e: e.scalar_tensor_tensor(SC[:, k0:k0 + kw], r_[:, 0:kw], iws[:, h:h + 1], SC[:, k0:k0 + kw], mm, ALU.add),
                                 r=[r_.k, iws.k, SC.k], w=[SC.k])
                    yield "F"
                if L > KT:
                    P.op("dve", lambda e: e.tensor_reduce(bs[:, 1:2], SC[:, 0:L], AX.X, ALU.max, apply_absolute_value=True), r=[SC.k], w=[bs.k])
                    P.op("dve", lambda e: e.tensor_scalar(bs[:, 0:1], bs[:, 1:2], -1.0, -1.0, mm, ALU.add), r=[bs.k], w=[bs.k])
                    P.op("dve", lambda e: e.tensor_scalar(bs[:, 1:2], bs[:, 1:2], 2.0, 2.0, mm, ALU.add), r=[bs.k], w=[bs.k])
                    P.op("dve", lambda e: e.tensor_scalar(Dk[:], HK[:], bs[:, 1:2], None, mm), r=[bs.k, HK.k], w=[Dk.k])
                P.op("pool", lambda e: e.tensor_tensor(SC[:, i * 128:L], SC[:, i * 128:L], CB[:], ALU.add), r=[SC.k, CB.k], w=[SC.k])
                if L > KT:
                    for it in range(NIT):
                        P.op("dve", lambda e: e.tensor_tensor(bs[:, 2:3], bs[:, 0:1], Dk[:, it:it + 1], ALU.add), r=[bs.k, Dk.k], w=[bs.k])
                        P.op("dve", lambda e: e.tensor_scalar(junk[:, 0:L], SC[:, 0:L], bs[:, 2:3], None, ALU.is_gt, ALU.add, accum_out=bs[:, 3:4]),
                             r=[SC.k, bs.k], w=[junk.k, bs.k])
                        P.op("dve", lambda e: e.scalar_tensor_tensor(bs[:, 4:5], bs[:, 3:4], float(KT) - 0.5, Dk[:, it:it + 1], ALU.is_gt, mm), r=[bs.k, Dk.k], w=[bs.k])
                        P.op("dve", lambda e: e.tensor_tensor(bs[:, 0:1], bs[:, 0:1], bs[:, 4:5], ALU.add), r=[bs.k], w=[bs.k])
                        yield "F"
                    P.op("dve", lambda e: e.tensor_scalar(MASK[:, 0:L], SC[:, 0:L], bs[:, 0:1], None, ALU.is_le), r=[SC.k, bs.k], w=[MASK.k])
                else:
                    P.op("dve", lambda e: e.tensor_scalar(MASK[:, 0:L], SC[:, 0:L], -1e29, None, ALU.is_le), r=[SC.k], w=[MASK.k])
                if "dbg_mask" in C.dbg:
                    P.dma("sp", C.dbg["dbg_mask"][i * 128:(i + 1) * 128, 0:L], MASK[:, 0:L], r=[MASK.k])
                    P.dma("sp", C.dbg["dbg_sc"][i * 128:(i + 1) * 128, 0:L], SC[:, 0:L], r=[SC.k])
                yield "END_FRONT"
                units = [(st, hg) for st in range(i + 1) for hg in range(2)]

                def stage1(st, hg):
                    if hg == 0 and st % 4 == 0:
                        n4 = min(4, i + 1 - st)
                        m4 = mT4[(st // 4) % 2]
                        pb = psb(C)
                        for u in range(n4):
                            P.op("pe", lambda e: e.transpose(pb[:, u * 128:(u + 1) * 128], MASK[:, (st + u) * 128:(st + u + 1) * 128], C.ident[:]),
                                 r=[MASK.k, C.ident_tok], w=[pb.k], inc=(u == n4 - 1))
                        P.op("act", lambda e: e.mul(m4[:, 0:n4, :], hv(pb[:, :], 4)[:, 0:n4, :], -MBIG), r=[pb.k], w=[m4.k])
                    m4 = mT4[(st // 4) % 2]
                    pl_ = psb(C)
                    P.op("pe", lambda e: e.matmul(pl_[:, :], lhsT=identb[:, :], rhs=m4[:, st % 4, :].unsqueeze(1).to_broadcast([128, 4, 128]), start=True, stop=False),
                         r=[identb.k, m4.k], w=[pl_.k], inc=False)
                    P.op("pe", lambda e: e.matmul(pl_[:, :], lhsT=kT[:, st * 128:(st + 1) * 128], rhs=qT[:, hg * 4:(hg + 1) * 4, :].rearrange("p h q -> p (h q)"), start=False, stop=True),
                         r=[kT.k, qT.k], w=[pl_.k])
                    pT = pTs[hg * 2 + st % 2]
                    P.op("act", lambda e: e.activation(out=pT[:], in_=hv(pl_[:, :], 4), func=AF.Exp, scale=SCALE), r=[pl_.k], w=[pT.k])

                def stage2(st, hg):
                    pT = pTs[hg * 2 + st % 2]
                    for hh in range(4):
                        h = hg * 4 + hh
                        ab, slot = acc_of[h]
                        P.op("pe", lambda e: e.matmul(accb[ab][:, slot * 129:(slot + 1) * 129], lhsT=pT[:, hh, :], rhs=Vx[:, st, :], start=(st == 0 and slot == 0), stop=(st == i), skip_group_check=True),
                             r=[pT.k, Vx.k], w=[accb[ab].k], inc=(hh == 3))

                stage1(*units[0])
                for n in range(len(units)):
                    if n + 1 < len(units):
                        stage1(*units[n + 1])
                    stage2(*units[n])
                    if units[n][1] == 1:
                        yield "B"
                for h in range(8):
                    ab, slot = acc_of[h]
                    P.op("dve", lambda e: e.reciprocal(rs8[:, h:h + 1], accb[ab][:, slot * 129 + 128:slot * 129 + 129]), r=[accb[ab].k], w=[rs8.k])
                    P.op("act", lambda e: e.activation(out=o_[:, h * 128:(h + 1) * 128], in_=accb[ab][:, slot * 129:slot * 129 + 128], func=AF.Copy, scale=rs8[:, h:h + 1]),
                         r=[accb[ab].k, rs8.k], w=[o_.k])
                if "dbg_dsa" in C.dbg:
                    P.dma("sp", C.dbg["dbg_dsa"][i * 128:(i + 1) * 128, :], o_[:], r=[o_.k])
                tile_to_stage(C, o_, og, j)
                if j == 3:
                    P.dma("sp", XTv(C.YT)[:, :, s * 512:(s + 1) * 512], og[:], r=[og.k], w=[C.YT_tok[0][s], C.YT_tok[1][s]])

        pipeline2([(lambda s=s, j=j: qblock(s, j)) for s in range(C.NS) for j in range(4)], interleave=C.dsa_interleave)
        P.barrier()
    C.bank_pool = list(range(8))


class _View:
    def __init__(self, tl, sl):
        self.t = _Sl(tl.t, sl)
        self.k = tl.k

    def __getitem__(self, idx):
        return self.t[idx]


class _Sl:
    def __init__(self, t, sl):
        self.base = t
        self.sl = sl

    def __getitem__(self, idx):
        rows, cols = idx
        assert cols == slice(None)
        return self.base[rows, self.sl]


_NC_CACHE = {}


def _in_map(inputs, b, T, consts):
    m = {"x": np.ascontiguousarray(inputs["x"][b, :T], dtype=np.float32)}
    for k, a in inputs.items():
        if k == "x":
            continue
        a = np.asarray(a, dtype=np.float32)
        if k in ("router_w", "exp_w_gate", "exp_w_up", "exp_w_down", "ln1_g", "ln1_b", "ln2_g", "ln2_b"):
            m[k] = np.ascontiguousarray(a)
        elif k == "router_bias":
            m[k] = np.ascontiguousarray(a.reshape(1, -1))
        elif k == "a_r_k":
            m[k] = np.ascontiguousarray(a.reshape(1, 512))
        elif a.ndim == 3:
            m[k] = np.ascontiguousarray(a[0])
        elif a.ndim == 2:
            m[k] = np.ascontiguousarray(a[0:1])
    m.update(consts)
    return m


def kernel(**inputs):
    x = np.asarray(inputs["x"])
    B, T, _ = x.shape
    if T not in _NC_CACHE:
        _NC_CACHE[T] = build(T)
    nc = _NC_CACHE[T]
    consts = {"c_" + k: v for k, v in host_consts(T).items()}
    consts.update({"c_" + k: v for k, v in rope_consts(T).items()})
    in_maps = [_in_map(inputs, b, T, consts) for b in range(B)]
    res = run_bass_kernel_spmd(nc, in_maps, core_ids=list(range(B)))
    out = np.stack([np.asarray(res.results[b]["out"], dtype=np.float32) for b in range(B)], 0)
    return out
```

```python
import numpy as np
import ml_dtypes
from contextlib import ExitStack
import concourse.bass as bass
import concourse.mybir as mybir
from concourse.bass_utils import run_bass_kernel_spmd

F32 = mybir.dt.float32
BF16 = mybir.dt.bfloat16
AF = mybir.ActivationFunctionType
ALU = mybir.AluOpType
AX = mybir.AxisListType

D = 1024
A_COLS = 1792
B_COLS = 1552
EVEN_COLS = 3344
ODD_COLS = 1604
NE = 16
DE = 256
DN_ALPHA = 4 ** 0.25
LN_EPS = 1e-5
DEC = 0.6065306597126334
DSA_INTERLEAVE = True
RWKV_INTERLEAVE = False


class Tok:
    __slots__ = ("w", "r")

    def __init__(self):
        self.w = None
        self.r = {}


class Eng:
    def __init__(self, name, h, sem):
        self.name = name
        self.h = h
        self.sem = sem
        self.cnt = 0
        self.waited = {}


class Prog:
    NSLOT = 8

    def __init__(self, nc, es):
        self.nc = nc
        self.es = es
        self.E = {}
        for name, h in (("pe", nc.tensor), ("act", nc.scalar), ("dve", nc.vector),
                        ("pool", nc.gpsimd), ("sp", nc.sync)):
            sem = es.enter_context(nc.semaphore("sem_" + name))
            self.E[name] = Eng(name, h, sem)
        self.slots = {}
        self.dn = {}
        for q in ("sp", "pool", "act"):
            self.slots[q] = [[es.enter_context(nc.semaphore("dq_%s%d" % (q, i))), 0] for i in range(self.NSLOT)]
            self.dn[q] = 0
        self.nalloc = 0

    def sb(self, shape, dt=F32, name=None, es=None):
        self.nalloc += 1
        t = (es or self.es).enter_context(self.nc.sbuf_tensor("%s_%d" % (name or "t", self.nalloc), list(shape), dt))
        return t

    def _wait(self, eng, ev):
        sem, val = ev
        key = sem.num
        if eng.waited.get(key, 0) >= val:
            return
        eng.h.wait_ge(sem, val)
        eng.waited[key] = val

    def _deps(self, en, r, w):
        eng = self.E[en]
        for t in r:
            if t.w is not None:
                yield t.w
        for t in w:
            if t.w is not None:
                yield t.w
            for ev in t.r.values():
                yield ev

    def op(self, en, fn, r=(), w=(), inc=True):
        eng = self.E[en]
        for ev in list(self._deps(en, r, w)):
            if en == "pe" and ev[0] is eng.sem:
                continue
            self._wait(eng, ev)
        ins = fn(eng.h)
        myev = (eng.sem, eng.cnt + 1)
        if inc:
            ins.then_inc(eng.sem, 1)
            eng.cnt += 1
        for t in r:
            t.r[en] = myev
        for t in w:
            t.w = myev
            t.r = {}
        return ins

    def dma(self, qn, out, in_, r=(), w=(), **kw):
        q = self.E[qn]
        for ev in list(self._deps(qn, r, w)):
            self._wait(q, ev)
        slot = self.slots[qn][self.dn[qn] % self.NSLOT]
        self.dn[qn] += 1
        if slot[1] > 0:
            self._wait(q, (slot[0], slot[1]))
        ins = q.h.dma_start(out=out, in_=in_, **kw)
        slot[1] += 16
        ins.then_inc(slot[0], 16)
        ev = (slot[0], slot[1])
        key = "d%d" % slot[0].num
        for t in r:
            t.r[key] = ev
        for t in w:
            t.w = ev
            t.r = {}

    def barrier(self):
        evs = []
        for q in self.slots:
            for sem, val in self.slots[q]:
                if val > 0:
                    evs.append((sem, val))
        for name, e in self.E.items():
            if e.cnt > 0:
                evs.append((e.sem, e.cnt))
        for name, e in self.E.items():
            for ev in evs:
                if ev[0] is e.sem and name == "pe":
                    continue
                self._wait(e, ev)

    def finish(self):
        sp = self.E["sp"]
        for q in self.slots:
            for sem, val in self.slots[q]:
                if val > 0:
                    self._wait(sp, (sem, val))
        for name, e in self.E.items():
            if name != "sp" and e.cnt > 0:
                self._wait(sp, (e.sem, e.cnt))


def host_consts(T):
    c = {}
    c["ident"] = np.eye(128, dtype=np.float32)
    j = np.arange(64)[:, None]
    i = np.arange(64)[None, :]
    c["tri64"] = np.stack([(-DEC) * (j <= i), (-DEC) * (j < i), (-DEC) * (j > i)], 1).astype(np.float32)
    c["ncol64"] = np.full((64, 1), -DEC, np.float32)
    su = (j < i).astype(np.float32)
    iu = (j <= i).astype(np.float32)
    sl = (j > i).astype(np.float32)
    mMA = np.concatenate([-su, iu], 1)
    mBB = np.concatenate([su, iu], 1)
    c["mMA"] = np.tile(mMA[:, None, :], (1, 8, 1)).astype(np.float32)
    c["mBB"] = np.tile(mBB[:, None, :], (1, 8, 1)).astype(np.float32)
    c["mNT"] = np.tile((-sl)[:, None, :], (1, 8, 1)).astype(np.float32)
    c["id8"] = np.tile(np.eye(64, dtype=np.float32)[:, None, :], (1, 8, 1))
    j = np.arange(128)[:, None]
    i = np.arange(128)[None, :]
    c["tri128"] = np.stack([(-1 / 16) * (j <= i), (-1 / 16) * (j > i)], 1).astype(np.float32)
    c["ncol128"] = np.full((128, 1), -1 / 16, np.float32)
    c["sel"] = (np.arange(16)[:, None, None] == np.arange(16)[None, :, None]).astype(np.float32) * np.ones((1, 1, 128), np.float32)
    c["iu128"] = np.tile((j <= i).astype(np.float32)[:, None, :], (1, 4, 1))
    return c


class Ctx:
    pass


def build(T, dbg=(), stages=("A", "R", "G", "O0", "M0", "S1", "O1", "M1")):
    nc = bass.Bass("TRN2", target_bir_lowering=False)
    es = ExitStack()
    P = Prog(nc, es)
    NT = T // 128
    NS = T // 512
    C = Ctx()
    C.nc, C.P, C.T, C.NT, C.NS = nc, P, T, NT, NS
    C.dbg = {}
    C.dsa_interleave = DSA_INTERLEAVE

    def din(name, shape, dt=F32):
        return nc.dram_tensor(name, list(shape), dt, kind="ExternalInput").ap()

    def dscr(name, shape, dt=F32, out=False):
        kind = "ExternalOutput" if (out or name in dbg) else "Internal"
        return nc.dram_tensor(name, list(shape), dt, kind=kind).ap()

    I = {}
    I["x"] = din("x", [T, D])
    for name, shape in (("w_in_even", [D, EVEN_COLS]), ("a_mu", [1, A_COLS]), ("a_w0", [1, 512]), ("a_w2", [64, 512]),
                        ("a_a0", [1, 512]), ("a_a2", [64, 512]), ("a_g2", [128, 512]), ("a_kk_scale", [1, 512]),
                        ("a_ka_scale", [1, 512]), ("a_r_k", [1, 512]), ("a_gn_g", [1, 512]), ("a_gn_b", [1, 512]),
                        ("b_gate_w2", [16, 256]), ("b_gate_b", [1, 256]), ("b_norm_g", [1, 512]),
                        ("w_out_even", [D, D]), ("w_in_odd", [D, ODD_COLS]), ("c_ik_ln_g", [1, 64]),
                        ("c_ik_ln_b", [1, 64]), ("w_out_odd", [D, D]), ("ln1_g", [2, D]), ("ln1_b", [2, D]),
                        ("ln2_g", [2, D]), ("ln2_b", [2, D]), ("router_w", [D, NE]), ("router_bias", [1, NE]),
                        ("exp_w_gate", [2, NE, D, DE]), ("exp_w_up", [2, NE, D, DE]), ("exp_w_down", [2, NE, DE, D])):
        I[name] = din(name, shape)
    hc = host_consts(T)
    hc.update(rope_consts(T))
    for k, v in hc.items():
        I["c_" + k] = din("c_" + k, list(v.shape), F32 if v.dtype == np.float32 else BF16)
    C.I = I
    out = dscr("out", [T, D], out=True)
    C.XT0 = dscr("XT0", [D, T + 1], BF16)
    C.XT0_tok = [Tok() for _ in range(NS)]
    C.XT0_z = Tok()
    C.YT = dscr("YT", [D, T], BF16)
    C.YT_tok = [[Tok() for _ in range(NS)] for _ in range(2)]
    C.H0 = dscr("H0", [T, D])
    C.H0_tok = [Tok() for _ in range(NT)]
    C.HT0 = dscr("HT0", [D, T], BF16)
    C.HT0_tok = [Tok() for _ in range(NS)]
    C.X1 = dscr("X1", [T, D])
    C.X1_tok = [Tok() for _ in range(NT)]
    C.XT1 = dscr("XT1", [D, T], BF16)
    C.XT1_tok = [Tok() for _ in range(NS)]

    for nm, shp in (("dbg_ya", [T, 512]), ("dbg_yb", [T, 512]), ("dbg_dsa", [T, 1024]), ("dbg_mask", [T, T]), ("dbg_sc", [T, T])):
        if nm in dbg:
            C.dbg[nm] = dscr(nm, shp, out=True)
    C.WGU16 = [dscr("WGU16_%d" % l, [NE, 128, 2 * 8 * DE], BF16) for l in range(2)]
    C.WGU16_tok = [[Tok() for _ in range(NE)] for l in range(2)]
    C.WD16 = [dscr("WD16_%d" % l, [128, 32 * D], BF16) for l in range(2)]
    C.WD16_tok = [Tok() for l in range(2)]
    C.banks = []
    for b in range(8):
        t = es.enter_context(nc.psum_tensor("psb%d" % b, [128, 512], F32))
        C.banks.append((t, Tok()))
    C.bi = 0

    C.bank_pool = list(range(8))

    def bank():
        b = C.banks[C.bank_pool[C.bi % len(C.bank_pool)]]
        C.bi += 1
        return b
    C.bank = bank

    C.ident = P.sb([128, 128], F32, "ident")
    C.ident_tok = Tok()
    P.dma("sp", C.ident[:], I["c_ident"], w=[C.ident_tok])

    if "M0" in stages:
        cast_weights(C, 0)
    if "A" in stages:
        phase_A(C)
    if "R" in stages:
        phase_rwkv(C)
    if "G" in stages:
        phase_gla(C)
    if "M1" in stages:
        cast_weights(C, 1)
    if "O0" in stages:
        phase_outproj(C, I["w_out_even"], C.YT, lambda s: [C.YT_tok[0][s], C.YT_tok[1][s]], I["x"], lambda i: [],
                      I["ln1_g"][0:1, :], I["ln1_b"][0:1, :], C.H0, C.H0_tok, C.HT0, C.HT0_tok)
    if "M0" in stages:
        phase_moe(C, 0, C.H0, C.H0_tok, C.HT0, C.HT0_tok, C.X1, C.X1_tok, C.XT1, C.XT1_tok)
    if "S1" in stages:
        phase_dsa(C)
    if "O1" in stages:
        C.H1 = dscr("H1", [T, D])
        C.H1_tok = [Tok() for _ in range(NT)]
        C.HT1 = dscr("HT1", [D, T], BF16)
        C.HT1_tok = [Tok() for _ in range(NS)]
        phase_outproj(C, I["w_out_odd"], C.YT, lambda s: [C.YT_tok[0][s], C.YT_tok[1][s]], C.X1, lambda i: [C.X1_tok[i]],
                      I["ln1_g"][1:2, :], I["ln1_b"][1:2, :], C.H1, C.H1_tok, C.HT1, C.HT1_tok)
    if "M1" in stages:
        out_tok = [Tok() for _ in range(NT)]
        phase_moe(C, 1, C.H1, C.H1_tok, C.HT1, C.HT1_tok, out, out_tok, None, None)
    P.finish()
    es.close()
    return nc


def XTv(ap):
    return ap.rearrange("(c p) t -> p c t", p=128)


def cast_weights(C, l):
    P, I = C.P, C.I
    for e in range(NE):
        dst = C.WGU16[l][e].rearrange("p (t c f) -> p t c f", t=2, c=8)
        P.dma("pool", dst[:, 0, :, :], I["exp_w_gate"][l, e].rearrange("(c p) f -> p c f", p=128), w=[C.WGU16_tok[l][e]])
        P.dma("pool", dst[:, 1, :, :], I["exp_w_up"][l, e].rearrange("(c p) f -> p c f", p=128), w=[C.WGU16_tok[l][e]])
    wd_flat = I["exp_w_down"][l].rearrange("e f d -> (e f) d")
    dstd = C.WD16[l].rearrange("p (c d) -> p c d", c=32)
    for c4 in range(8):
        P.dma("pool", dstd[:, c4 * 4:(c4 + 1) * 4, :], wd_flat[c4 * 512:(c4 + 1) * 512, :].rearrange("(c p) d -> p c d", p=128), w=[C.WD16_tok[l]])


def phase_A(C):
    nc, P, T, I = C.nc, C.P, C.T, C.I
    with ExitStack() as es:
        xin = [P.sb([128, D], F32, "xin", es) for _ in range(2)]
        xin_tok = [Tok(), Tok()]
        st = [P.sb([128, 8, 512], BF16, "ast", es) for _ in range(2)]
        st_tok = [Tok(), Tok()]
        z = P.sb([128, 8, 1], BF16, "zc", es)
        zt = Tok()
        P.op("dve", lambda e: e.memset(z[:], 0.0), w=[zt])
        P.dma("sp", XTv(C.XT0)[:, :, 0:1], z[:], r=[zt], w=[C.XT0_z], allow_slow_non_contiguous=True)
        for s in range(C.NS):
            sb_ = st[s % 2]
            for j in range(4):
                i = s * 4 + j
                xb = xin[i % 2]
                xt = xin_tok[i % 2]
                P.dma("sp", xb[:], I["x"][i * 128:(i + 1) * 128, :], w=[xt])
                for half in range(2):
                    bt, bk = C.bank()
                    for c4 in range(4):
                        c = half * 4 + c4
                        P.op("pe", lambda e: e.transpose(bt[:, c4 * 128:(c4 + 1) * 128], xb[:, c * 128:(c + 1) * 128], C.ident[:]),
                             r=[xt, C.ident_tok], w=[bk], inc=(c4 == 3))
                    en = "act" if half == 0 else "dve"
                    src = bt[:, :].rearrange("p (c t) -> p c t", c=4)
                    dst = sb_[:, half * 4:(half + 1) * 4, j * 128:(j + 1) * 128]
                    if en == "act":
                        P.op("act", lambda e: e.copy(dst, src), r=[bk], w=[st_tok[s % 2]])
                    else:
                        P.op("dve", lambda e: e.tensor_copy(dst, src), r=[bk], w=[st_tok[s % 2]])
            P.dma("sp", XTv(C.XT0)[:, :, 1 + s * 512:1 + (s + 1) * 512], sb_[:], r=[st_tok[s % 2]], w=[C.XT0_tok[s]])
        P.barrier()


class TL:
    def __init__(self, t, k=None):
        self.t = t
        self.k = k or Tok()

    def __getitem__(self, idx):
        return self.t[idx]


def mk(P, shape, dt=F32, name=None, es=None):
    return TL(P.sb(shape, dt, name, es))


def bcast_load(C, dst_ap, src_row, np_, tok, q="sp"):
    C.P.dma(q, dst_ap, src_row.partition_broadcast(np_), w=[tok])


def hv(ap, h):
    return ap.rearrange("p (h v) -> p h v", h=h)


def phase_rwkv(C):
    nc, P, T, I = C.nc, C.P, C.T, C.I
    mm = ALU.mult
    with ExitStack() as es:
        W1 = mk(P, [128, 8, A_COLS], BF16, "W1", es)
        W2 = mk(P, [128, 8, A_COLS], BF16, "W2", es)
        with ExitStack() as es2:
            mub = mk(P, [128, A_COLS], F32, "mub", es2)
            omu = mk(P, [128, A_COLS], F32, "omu", es2)
            stg = [mk(P, [128, A_COLS], F32, "wstg", es2) for _ in range(2)]
            bcast_load(C, mub[:], I["a_mu"], 128, mub.k)
            P.op("dve", lambda e: e.tensor_scalar(omu[:], mub[:], -1.0, 1.0, ALU.mult, ALU.add), r=[mub.k], w=[omu.k])
            for c in range(8):
                s_ = stg[c % 2]
                P.dma("sp", s_[:], I["w_in_even"][c * 128:(c + 1) * 128, 0:A_COLS], w=[s_.k])
                P.op("dve", lambda e: e.tensor_tensor(W1[:, c, :], s_[:], omu[:], mm), r=[s_.k, omu.k], w=[W1.k])
                P.op("pool", lambda e: e.tensor_tensor(W2[:, c, :], s_[:], mub[:], mm), r=[s_.k, mub.k], w=[W2.k])
            P.barrier()
        LW = mk(P, [128, 512], BF16, "LW", es)
        G2 = mk(P, [128, 512], BF16, "G2", es)
        P.dma("pool", LW[0:64, :], I["a_w2"], w=[LW.k])
        P.dma("pool", LW[64:128, :], I["a_a2"], w=[LW.k])
        P.dma("pool", G2[:], I["a_g2"], w=[G2.k])
        BV = mk(P, [64, 7, 512], F32, "BV", es)
        for n, name in enumerate(("a_w0", "a_a0", "a_kk_scale", "a_ka_scale", "a_r_k", "a_gn_g", "a_gn_b")):
            bcast_load(C, BV[:, n, :], I[name], 64, BV.k)
        w0b, a0b, kksb, kab, rkb, gngb, gnbb = [BV[:, n, :] for n in range(7)]
        tri = mk(P, [64, 3, 64], F32, "tri", es)
        ncol = mk(P, [64, 1], F32, "ncol", es)
        mMA = mk(P, [64, 8, 128], F32, "mMA", es)
        mBB = mk(P, [64, 8, 128], F32, "mBB", es)
        mNT = mk(P, [64, 8, 64], F32, "mNT", es)
        id8 = mk(P, [64, 8, 64], F32, "id8", es)
        for tl, nm in ((tri, "c_tri64"), (ncol, "c_ncol64"), (mMA, "c_mMA"), (mBB, "c_mBB"), (mNT, "c_mNT"), (id8, "c_id8")):
            P.dma("sp", tl[:], I[nm], w=[tl.k])
        id64 = C.ident[0:64, 0:64]

        def wt(name, shape=(64, 512), dt=F32):
            return mk(P, list(shape), dt, name, es)
        ATs = [mk(P, [128, 8, 513], BF16, "ATs", es) for _ in range(2)]
        TX = wt("TX", (128, 512), BF16)
        SG = wt("SG", (128, 512), BF16)
        r_, k_, v_, sg, a_, kk, be, Bi, Ki, tmp = [wt(n) for n in ("r", "k", "v", "sg", "a", "kk", "be", "Bi", "Ki", "tmp")]
        Ep, Em, Ex, Ee = [wt(n) for n in ("Ep", "Em", "Ex", "Ee")]
        s8 = [wt("s8_%d" % n, (64, 8)) for n in range(4)]
        KRs = [wt("KR", (64, 8, 128), BF16) for _ in range(2)]
        BiT = wt("BiT", (64, 8, 64), BF16)
        KiT = wt("KiT", (64, 8, 64), BF16)
        MAs = [wt("MA", (64, 8, 128), BF16) for _ in range(2)]
        BBs = [wt("BB", (64, 8, 128), BF16) for _ in range(2)]
        Xb = [wt("X%d" % n, (64, 8, 64), BF16) for n in range(2)]
        XTb = [wt("XT%d" % n, (64, 8, 64), BF16) for n in range(2)]
        Qbs = [[wt("Q%d" % n, (64, 8, 64), BF16) for n in range(2)] for _ in range(2)]
        Xs = wt("Xs", (64, 8, 64), BF16)
        nU = wt("nU", (64, 8, 64), BF16)
        Y = wt("Y")
        tmpb = wt("tmpb")
        Hs = [wt("H%d" % n, (64, 8, 64)) for n in range(2)]
        Hbs = [wt("Hb%d" % n, (64, 8, 64), BF16) for n in range(2)]
        vbs = [wt("vb", (64, 512), BF16) for _ in range(2)]
        Ke16s = [wt("Ke16", (64, 512), BF16) for _ in range(2)]
        Be16s = [wt("Be16", (64, 512), BF16) for _ in range(2)]
        PCs = [wt("PC", (64, 8)) for _ in range(2)]
        gs_ = [wt("g", (64, 512)) for _ in range(2)]
        bonuss = [wt("bonus", (64, 512)) for _ in range(2)]
        P.op("pool", lambda e: e.memset(Hbs[0][:], 0.0), w=[Hbs[0].k])
        yst = [mk(P, [128, 4, 512], BF16, "yst", es) for _ in range(2)]
        P.op("dve", lambda e: e.memset(Hs[0][:], 0.0), w=[Hs[0].k])

        def psb():
            t, k = C.bank()
            return TL(t, k)

        def v3(tl_or_ap, h=8):
            return hv(tl_or_ap, h)

        def chunk(s, ci):
          at = ATs[s % 2]
          ys = yst[s % 2]
          if ci == 0:
            rd = [C.XT0_tok[s]] + ([C.XT0_tok[s - 1]] if s > 0 else [C.XT0_z])
            P.dma("sp", at[:], XTv(C.XT0)[:, :, s * 512:s * 512 + 513], r=rd, w=[at.k])
            for which in range(2):
                pb = psb()
                c0 = 1536 + which * 128
                for c in range(8):
                    P.op("pe", lambda e: e.matmul(pb[:, :], lhsT=W1[:, c, c0:c0 + 128], rhs=at[:, c, 1:513], start=(c == 0), stop=False),
                         r=[W1.k, at.k], w=[pb.k], inc=False)
                for c in range(8):
                    P.op("pe", lambda e: e.matmul(pb[:, :], lhsT=W2[:, c, c0:c0 + 128], rhs=at[:, c, 0:512], start=False, stop=(c == 7)),
                         r=[W2.k, at.k], w=[pb.k], inc=(c == 7))
                if which == 0:
                    P.op("act", lambda e: e.activation(out=TX[0:64, :], in_=pb[0:64, :], func=AF.Tanh), r=[pb.k], w=[TX.k])
                    P.op("act", lambda e: e.copy(TX[64:128, :], pb[64:128, :]), r=[pb.k], w=[TX.k])
                else:
                    P.op("act", lambda e: e.activation(out=SG[:, :], in_=pb[:, :], func=AF.Sigmoid), r=[pb.k], w=[SG.k])
          if True:
            if True:
                g = s * 8 + ci
                t0 = ci * 64
                KR, MA, BB, Qb = KRs[g % 2], MAs[g % 2], BBs[g % 2], Qbs[g % 2]
                vb, Ke16, Be16, PC, g_, bonus = vbs[g % 2], Ke16s[g % 2], Be16s[g % 2], PCs[g % 2], gs_[g % 2], bonuss[g % 2]
                pr, pk, pv = psb(), psb(), psb()
                for pb, c0 in ((pr, 0), (pk, 512), (pv, 1024)):
                    for c in range(8):
                        P.op("pe", lambda e: e.matmul(pb[0:64, :], lhsT=at[:, c, 1 + t0:1 + t0 + 64], rhs=W1[:, c, c0:c0 + 512], start=(c == 0), stop=False),
                             r=[W1.k, at.k], w=[pb.k], inc=False)
                    for c in range(8):
                        P.op("pe", lambda e: e.matmul(pb[0:64, :], lhsT=at[:, c, t0:t0 + 64], rhs=W2[:, c, c0:c0 + 512], start=False, stop=(c == 7)),
                             r=[W2.k, at.k], w=[pb.k], inc=(c == 7))
                yield "F"
                pz, pza, pg = psb(), psb(), psb()
                P.op("pe", lambda e: e.matmul(pz[0:64, :], lhsT=TX[0:64, t0:t0 + 64], rhs=LW[0:64, :], start=True, stop=True), r=[TX.k, LW.k], w=[pz.k])
                P.op("pe", lambda e: e.matmul(pza[0:64, :], lhsT=TX[64:128, t0:t0 + 64], rhs=LW[64:128, :], start=True, stop=True), r=[TX.k, LW.k], w=[pza.k])
                P.op("pe", lambda e: e.matmul(pg[0:64, :], lhsT=SG[:, t0:t0 + 64], rhs=G2[:, :], start=True, stop=True), r=[SG.k, G2.k], w=[pg.k])
                P.op("act", lambda e: e.copy(r_[:], pr[0:64, :]), r=[pr.k], w=[r_.k])
                P.op("act", lambda e: e.copy(v_[:], pv[0:64, :]), r=[pv.k], w=[v_.k])
                P.op("act", lambda e: e.copy(vb[:], pv[0:64, :]), r=[pv.k], w=[vb.k])
                P.op("act", lambda e: e.copy(g_[:], pg[0:64, :]), r=[pg.k], w=[g_.k])
                P.op("dve", lambda e: e.tensor_copy(k_[:], pk[0:64, :]), r=[pk.k], w=[k_.k])
                P.op("dve", lambda e: e.tensor_tensor(sg[:], pz[0:64, :], w0b, ALU.add), r=[pz.k, BV.k], w=[sg.k])
                P.op("act", lambda e: e.activation(out=sg[:], in_=sg[:], func=AF.Sigmoid), r=[sg.k], w=[sg.k])
                P.op("dve", lambda e: e.tensor_tensor(a_[:], pza[0:64, :], a0b, ALU.add), r=[pza.k, BV.k], w=[a_.k])
                P.op("act", lambda e: e.activation(out=a_[:], in_=a_[:], func=AF.Sigmoid), r=[a_.k], w=[a_.k])
                yield "F"
                P.op("pool", lambda e: e.tensor_tensor(kk[:], k_[:], kksb, mm), r=[k_.k, BV.k], w=[kk.k])
                P.op("pool", lambda e: e.tensor_tensor(tmp[:], kk[:], kk[:], mm), r=[kk.k], w=[tmp.k])
                P.op("dve", lambda e: e.tensor_reduce(s8[0][:], v3(tmp[:]), AX.X, ALU.add), r=[tmp.k], w=[s8[0].k])
                P.op("dve", lambda e: e.tensor_scalar(s8[0][:], s8[0][:], 1e-24, None, ALU.max), r=[s8[0].k], w=[s8[0].k])
                P.op("act", lambda e: e.activation(out=s8[0][:], in_=s8[0][:], func=AF.Sqrt), r=[s8[0].k], w=[s8[0].k])
                P.op("dve", lambda e: e.reciprocal(s8[0][:], s8[0][:]), r=[s8[0].k], w=[s8[0].k])
                P.op("dve", lambda e: e.tensor_tensor(v3(kk[:]), v3(kk[:]), s8[0][:, :].unsqueeze(2).to_broadcast([64, 8, 64]), mm),
                     r=[kk.k, s8[0].k], w=[kk.k])
                P.op("pool", lambda e: e.tensor_tensor(be[:], kk[:], a_[:], mm), r=[kk.k, a_.k], w=[be.k])
                P.op("dve", lambda e: e.scalar_tensor_tensor(tmp[:], a_[:], -1.0, kab, ALU.add, mm), r=[a_.k, BV.k], w=[tmp.k])
                P.op("dve", lambda e: e.scalar_tensor_tensor(k_[:], tmp[:], 1.0, k_[:], ALU.add, mm), r=[tmp.k, k_.k], w=[k_.k])
                P.op("pool", lambda e: e.tensor_tensor(tmp[:], r_[:], k_[:], mm), r=[r_.k, k_.k], w=[tmp.k])
                P.op("pool", lambda e: e.tensor_tensor(tmp[:], tmp[:], rkb, mm), r=[tmp.k, BV.k], w=[tmp.k])
                P.op("dve", lambda e: e.tensor_reduce(s8[1][:], v3(tmp[:]), AX.X, ALU.add), r=[tmp.k], w=[s8[1].k])
                P.op("dve", lambda e: e.tensor_tensor(v3(bonus[:]), v3(v_[:]), s8[1][:, :].unsqueeze(2).to_broadcast([64, 8, 64]), mm),
                     r=[v_.k, s8[1].k], w=[bonus.k])
                yield "F"
                pcl, pcx, pca, ppc = psb(), psb(), psb(), psb()
                for pb, n in ((pcl, 0), (pcx, 1), (pca, 2)):
                    P.op("pe", lambda e: e.matmul(pb[0:64, :], lhsT=tri[:, n, :], rhs=sg[:], start=True, stop=True), r=[tri.k, sg.k], w=[pb.k])
                for h in range(8):
                    P.op("pe", lambda e: e.matmul(ppc[0:64, h:h + 1], lhsT=sg[:, h * 64:(h + 1) * 64], rhs=ncol[:], start=True, stop=True),
                         r=[sg.k, ncol.k], w=[ppc.k], inc=(h == 7))
                P.op("act", lambda e: e.activation(out=Ep[:], in_=pcl[0:64, :], func=AF.Exp), r=[pcl.k], w=[Ep.k])
                P.op("act", lambda e: e.activation(out=Em[:], in_=pcl[0:64, :], func=AF.Exp, scale=-1.0), r=[pcl.k], w=[Em.k])
                P.op("act", lambda e: e.activation(out=Ex[:], in_=pcx[0:64, :], func=AF.Exp), r=[pcx.k], w=[Ex.k])
                P.op("act", lambda e: e.activation(out=Ee[:], in_=pca[0:64, :], func=AF.Exp), r=[pca.k], w=[Ee.k])
                P.op("act", lambda e: e.activation(out=PC[:], in_=ppc[0:64, 0:8], func=AF.Exp), r=[ppc.k], w=[PC.k])
                P.op("dve", lambda e: e.tensor_tensor(r_[:], r_[:], Ep[:], mm), r=[r_.k, Ep.k], w=[r_.k])
                P.op("pool", lambda e: e.tensor_tensor(kk[:], kk[:], Ex[:], mm), r=[kk.k, Ex.k], w=[kk.k])
                P.op("dve", lambda e: e.tensor_tensor(Bi[:], be[:], Em[:], mm), r=[be.k, Em.k], w=[Bi.k])
                P.op("pool", lambda e: e.tensor_tensor(Ki[:], k_[:], Em[:], mm), r=[k_.k, Em.k], w=[Ki.k])
                P.op("dve", lambda e: e.tensor_tensor(Ke16[:], k_[:], Ee[:], mm), r=[k_.k, Ee.k], w=[Ke16.k])
                P.op("pool", lambda e: e.tensor_tensor(Be16[:], be[:], Ee[:], mm), r=[be.k, Ee.k], w=[Be16.k])
                yield "F"
                for src, dst, off, en in ((kk, KR, 0, "act"), (r_, KR, 64, "dve"), (Bi, BiT, 0, "act"), (Ki, KiT, 0, "dve")):
                    pb = psb()
                    for h in range(8):
                        P.op("pe", lambda e: e.transpose(pb[0:64, h * 64:(h + 1) * 64], src[:, h * 64:(h + 1) * 64], id64),
                             r=[src.k, C.ident_tok], w=[pb.k], inc=(h == 7))
                    d_ = dst[:, :, off:off + 64]
                    s_ = v3(pb[0:64, :])
                    if en == "act":
                        P.op("act", lambda e: e.copy(d_, s_), r=[pb.k], w=[dst.k])
                    else:
                        P.op("dve", lambda e: e.tensor_copy(d_, s_), r=[pb.k], w=[dst.k])
                yield "F"
                pma = [psb(), psb()]
                pbb = [psb(), psb()]
                pnt = psb()
                for h in range(8):
                    hb, hh = h // 4, h % 4
                    P.op("pe", lambda e: e.matmul(pma[hb][0:64, hh * 128:(hh + 1) * 128], lhsT=BiT[:, h, :], rhs=KR[:, h, :], start=True, stop=True),
                         r=[BiT.k, KR.k], w=[pma[hb].k], inc=(hh == 3))
                for h in range(8):
                    hb, hh = h // 4, h % 4
                    P.op("pe", lambda e: e.matmul(pbb[hb][0:64, hh * 128:(hh + 1) * 128], lhsT=KiT[:, h, :], rhs=KR[:, h, :], start=True, stop=True),
                         r=[KiT.k, KR.k], w=[pbb[hb].k], inc=(hh == 3))
                for h in range(8):
                    P.op("pe", lambda e: e.matmul(pnt[0:64, h * 64:(h + 1) * 64], lhsT=KR[:, h, 0:64], rhs=BiT[:, h, :], start=True, stop=True),
                         r=[BiT.k, KR.k], w=[pnt.k], inc=(h == 7))
                for hb in range(2):
                    P.op("dve", lambda e: e.tensor_tensor(MA[:, hb * 4:(hb + 1) * 4, :], hv(pma[hb][0:64, :], 4), mMA[:, hb * 4:(hb + 1) * 4, :], mm),
                         r=[pma[hb].k, mMA.k], w=[MA.k])
                    P.op("dve", lambda e: e.tensor_tensor(BB[:, hb * 4:(hb + 1) * 4, :], hv(pbb[hb][0:64, :], 4), mBB[:, hb * 4:(hb + 1) * 4, :], mm),
                         r=[pbb[hb].k, mBB.k], w=[BB.k])
                X, XT, Q = Xb[0], XTb[0], Qb[0]
                P.op("dve", lambda e: e.tensor_tensor(XT[:], v3(pnt[0:64, :]), mNT[:], mm), r=[pnt.k, mNT.k], w=[XT.k])
                P.op("pool", lambda e: e.tensor_copy(X[:], MA[:, :, 0:64]), r=[MA.k], w=[X.k])
                P.op("pool", lambda e: e.tensor_tensor(Q[:], MA[:, :, 0:64], id8[:], ALU.add), r=[MA.k, id8.k], w=[Q.k])
                for lvl in range(5):
                    Xn, XTn, Qn = Xb[(lvl + 1) % 2], XTb[(lvl + 1) % 2], Qb[(lvl + 1) % 2]
                    pxt = psb()
                    for h in range(8):
                        P.op("pe", lambda e: e.matmul(pxt[0:64, h * 64:(h + 1) * 64], lhsT=X[:, h, :], rhs=XT[:, h, :], start=True, stop=True),
                             r=[X.k, XT.k], w=[pxt.k], inc=(h == 7))
                    if lvl < 4:
                        px = psb()
                        for h in range(8):
                            P.op("pe", lambda e: e.matmul(px[0:64, h * 64:(h + 1) * 64], lhsT=XT[:, h, :], rhs=X[:, h, :], start=True, stop=True),
                                 r=[X.k, XT.k], w=[px.k], inc=(h == 7))
                    P.op("act", lambda e: e.copy(XTn[:], v3(pxt[0:64, :])), r=[pxt.k], w=[XTn.k])
                    if lvl < 4:
                        P.op("dve", lambda e: e.tensor_copy(Xn[:], v3(px[0:64, :])), r=[px.k], w=[Xn.k])
                    pq = psb()
                    for h in range(8):
                        P.op("pe", lambda e: e.matmul(pq[0:64, h * 64:(h + 1) * 64], lhsT=XTn[:, h, :], rhs=Q[:, h, :], start=True, stop=True),
                             r=[XTn.k, Q.k], w=[pq.k], inc=(h == 7))
                    P.op("dve", lambda e: e.tensor_tensor(Qn[:], Q[:], v3(pq[0:64, :]), ALU.add), r=[Q.k, pq.k], w=[Qn.k])
                    X, XT, Q = Xn, XTn, Qn
                    yield "F"
                yield "END_FRONT"
                H, Hn = Hs[g % 2], Hs[(g + 1) % 2]
                Hb, Hbn = Hbs[g % 2], Hbs[(g + 1) % 2]
                pxs = psb()
                for h in range(8):
                    P.op("pe", lambda e: e.matmul(pxs[0:64, h * 64:(h + 1) * 64], lhsT=KR[:, h, 0:64], rhs=Hb[:, h, :], start=True, stop=False),
                         r=[KR.k, Hb.k], w=[pxs.k], inc=False)
                    P.op("pe", lambda e: e.matmul(pxs[0:64, h * 64:(h + 1) * 64], lhsT=BB[:, h, 0:64], rhs=vb[:, h * 64:(h + 1) * 64], start=False, stop=True),
                         r=[BB.k, vb.k], w=[pxs.k], inc=(h == 7))
                P.op("act", lambda e: e.copy(Xs[:], v3(pxs[0:64, :])), r=[pxs.k], w=[Xs.k])
                yield "B"
                pu = psb()
                for h in range(8):
                    P.op("pe", lambda e: e.matmul(pu[0:64, h * 64:(h + 1) * 64], lhsT=Q[:, h, :], rhs=Xs[:, h, :], start=True, stop=True),
                         r=[Q.k, Xs.k], w=[pu.k], inc=(h == 7))
                P.op("act", lambda e: e.mul(nU[:], v3(pu[0:64, :]), -1.0), r=[pu.k], w=[nU.k])
                yield "B"
                py, ph = psb(), psb()
                for h in range(8):
                    sl = slice(h * 64, (h + 1) * 64)
                    P.op("pe", lambda e: e.matmul(py[0:64, sl], lhsT=KR[:, h, 64:128], rhs=Hb[:, h, :], start=True, stop=False), r=[KR.k, Hb.k], w=[py.k], inc=False)
                    P.op("pe", lambda e: e.matmul(py[0:64, sl], lhsT=BB[:, h, 64:128], rhs=vb[:, sl], start=False, stop=False), r=[BB.k, vb.k], w=[py.k], inc=False)
                    P.op("pe", lambda e: e.matmul(py[0:64, sl], lhsT=MA[:, h, 64:128], rhs=nU[:, h, :], start=False, stop=True), r=[MA.k, nU.k], w=[py.k], inc=(h == 7))
                for h in range(8):
                    sl = slice(h * 64, (h + 1) * 64)
                    P.op("pe", lambda e: e.matmul(ph[0:64, sl], lhsT=Ke16[:, sl], rhs=vb[:, sl], start=True, stop=False), r=[Ke16.k, vb.k], w=[ph.k], inc=False)
                    P.op("pe", lambda e: e.matmul(ph[0:64, sl], lhsT=Be16[:, sl], rhs=nU[:, h, :], start=False, stop=True), r=[Be16.k, nU.k], w=[ph.k], inc=(h == 7))
                P.op("pool", lambda e: e.tensor_tensor(Hn[:], H[:], PC[:, :].unsqueeze(2).to_broadcast([64, 8, 64]), mm), r=[H.k, PC.k], w=[Hn.k])
                P.op("dve", lambda e: e.tensor_tensor(Hn[:], Hn[:], v3(ph[0:64, :]), ALU.add), r=[Hn.k, ph.k], w=[Hn.k])
                P.op("act", lambda e: e.copy(Hbn[:], Hn[:]), r=[Hn.k], w=[Hbn.k])
                yield "B"
                P.op("act", lambda e: e.copy(Y[:], py[0:64, :]), r=[py.k], w=[Y.k])
                P.op("dve", lambda e: e.tensor_reduce(s8[2][:], v3(Y[:]), AX.X, ALU.add), r=[Y.k], w=[s8[2].k])
                P.op("dve", lambda e: e.tensor_scalar(s8[2][:], s8[2][:], 1.0 / 64, None, mm), r=[s8[2].k], w=[s8[2].k])
                P.op("dve", lambda e: e.tensor_tensor(v3(Y[:]), v3(Y[:]), s8[2][:, :].unsqueeze(2).to_broadcast([64, 8, 64]), ALU.subtract),
                     r=[Y.k, s8[2].k], w=[Y.k])
                yield "B"
                P.op("pool", lambda e: e.tensor_tensor(tmpb[:], Y[:], Y[:], mm), r=[Y.k], w=[tmpb.k])
                P.op("dve", lambda e: e.tensor_reduce(s8[3][:], v3(tmpb[:]), AX.X, ALU.add), r=[tmpb.k], w=[s8[3].k])
                P.op("act", lambda e: e.activation(out=s8[3][:], in_=s8[3][:], func=AF.Sqrt, bias=64e-5, scale=1.0 / 64), r=[s8[3].k], w=[s8[3].k])
                P.op("dve", lambda e: e.reciprocal(s8[3][:], s8[3][:]), r=[s8[3].k], w=[s8[3].k])
                P.op("dve", lambda e: e.tensor_tensor(v3(Y[:]), v3(Y[:]), s8[3][:, :].unsqueeze(2).to_broadcast([64, 8, 64]), mm),
                     r=[Y.k, s8[3].k], w=[Y.k])
                P.op("pool", lambda e: e.tensor_tensor(Y[:], Y[:], gngb, mm), r=[Y.k, BV.k], w=[Y.k])
                P.op("pool", lambda e: e.tensor_tensor(Y[:], Y[:], gnbb, ALU.add), r=[Y.k, BV.k], w=[Y.k])
                P.op("dve", lambda e: e.tensor_tensor(Y[:], Y[:], bonus[:], ALU.add), r=[Y.k, bonus.k], w=[Y.k])
                P.op("dve", lambda e: e.tensor_tensor(Y[:], Y[:], g_[:], mm), r=[Y.k, g_.k], w=[Y.k])
                if "dbg_ya" in C.dbg:
                    P.dma("sp", C.dbg["dbg_ya"][g * 64:(g + 1) * 64, :], Y[:], r=[Y.k])
                yield "B"
                pb = psb()
                for q in range(4):
                    P.op("pe", lambda e: e.transpose(pb[:, q * 64:(q + 1) * 64], Y[:, q * 128:(q + 1) * 128], id64), r=[Y.k, C.ident_tok], w=[pb.k], inc=(q == 3))
                P.op("act", lambda e: e.copy(ys[:, :, t0:t0 + 64], hv(pb[:, 0:256], 4)), r=[pb.k], w=[ys.k])
                if ci == 7:
                    P.dma("sp", XTv(C.YT)[:, 0:4, s * 512:(s + 1) * 512], ys[:], r=[ys.k], w=[C.YT_tok[0][s]])

        pipeline2([(lambda s=s, ci=ci: chunk(s, ci)) for s in range(C.NS) for ci in range(8)], interleave=RWKV_INTERLEAVE)
        P.barrier()


def pipeline2(makers, interleave=True):
    if not interleave:
        for mk_ in makers:
            for _ in mk_():
                pass
        return
    prevB = None
    for mk_ in makers:
        g = mk_()
        while True:
            r = next(g)
            if prevB is not None:
                try:
                    next(prevB)
                except StopIteration:
                    prevB = None
            if r == "END_FRONT":
                break
        if prevB is not None:
            for _ in prevB:
                pass
        prevB = g
    if prevB is not None:
        for _ in prevB:
            pass


def psb(C):
    t, k = C.bank()
    return TL(t, k)


def phase_gla(C):
    nc, P, T, I = C.nc, C.P, C.T, C.I
    mm = ALU.mult
    with ExitStack() as es:
        WB = mk(P, [128, 8, B_COLS], BF16, "WB", es)
        for c in range(8):
            P.dma("pool", WB[:, c, :], I["w_in_even"][c * 128:(c + 1) * 128, A_COLS:EVEN_COLS], w=[WB.k])
        GW2 = mk(P, [16, 256], BF16, "GW2", es)
        P.dma("pool", GW2[:], I["b_gate_w2"], w=[GW2.k])
        gbb = mk(P, [128, 256], F32, "gbb", es)
        ngb = mk(P, [128, 512], F32, "ngb", es)
        bcast_load(C, gbb[:], I["b_gate_b"], 128, gbb.k)
        bcast_load(C, ngb[:], I["b_norm_g"], 128, ngb.k)
        tri = mk(P, [128, 2, 128], F32, "tri128", es)
        ncol = mk(P, [128, 1], F32, "ncol128", es)
        iu = mk(P, [128, 4, 128], F32, "iu128", es)
        for tl, nm in ((tri, "c_tri128"), (ncol, "c_ncol128"), (iu, "c_iu128")):
            P.dma("sp", tl[:], I[nm], w=[tl.k])
        id64 = C.ident[0:64, 0:64]

        def wt(name, shape, dt=F32):
            return mk(P, list(shape), dt, name, es)
        ATs = [wt("ATg", (128, 8, 512), BF16) for _ in range(2)]
        AL = wt("AL", (16, 512), BF16)
        l_ = wt("l", (128, 256))
        Eq, Ei, Ee = wt("Eq", (128, 256)), wt("Ei", (128, 256)), wt("Ee", (128, 256))
        PCg = wt("PCg", (64, 4))
        qd, ki, ke = wt("qd", (128, 256)), wt("ki", (128, 256)), wt("ke", (128, 256))
        v_ = wt("vg", (128, 512))
        qdT, kiT = wt("qdT", (64, 4, 128)), wt("kiT", (64, 4, 128))
        attT = wt("attT", (128, 4, 128))
        Ss = [wt("S%d" % n, (64, 4, 128)) for n in range(2)]
        o_ = wt("o", (128, 512))
        sq = wt("sqg", (128, 512))
        sl_ = wt("silu", (128, 512))
        m4 = wt("m4", (128, 4))
        yst = [wt("ystg", (128, 4, 512), BF16) for _ in range(2)]
        P.op("dve", lambda e: e.memset(Ss[0][:], 0.0), w=[Ss[0].k])
        for s in range(C.NS):
            at = ATs[s % 2]
            P.dma("sp", at[:], XTv(C.XT0)[:, :, 1 + s * 512:1 + (s + 1) * 512], r=[C.XT0_tok[s]], w=[at.k])
            pb = psb(C)
            for c in range(8):
                P.op("pe", lambda e: e.matmul(pb[0:16, :], lhsT=WB[:, c, 1536:1552], rhs=at[:, c, :], start=(c == 0), stop=(c == 7)),
                     r=[WB.k, at.k], w=[pb.k], inc=(c == 7))
            P.op("act", lambda e: e.copy(AL[:], pb[0:16, :]), r=[pb.k], w=[AL.k])
            ys = yst[s % 2]
            for ci in range(4):
                g = s * 4 + ci
                t0 = ci * 128
                pqk, pv, pg = psb(C), psb(C), psb(C)
                for pb, c0 in ((pqk, 0), (pv, 512), (pg, 1024)):
                    for c in range(8):
                        P.op("pe", lambda e: e.matmul(pb[:, :], lhsT=at[:, c, t0:t0 + 128], rhs=WB[:, c, c0:c0 + 512], start=(c == 0), stop=(c == 7)),
                             r=[WB.k, at.k], w=[pb.k], inc=(c == 7))
                pla = psb(C)
                P.op("pe", lambda e: e.matmul(pla[:, 0:256], lhsT=AL[:, t0:t0 + 128], rhs=GW2[:], start=True, stop=True), r=[AL.k, GW2.k], w=[pla.k])
                P.op("dve", lambda e: e.tensor_tensor(l_[:], pla[:, 0:256], gbb[:], ALU.add), r=[pla.k, gbb.k], w=[l_.k])
                P.op("act", lambda e: e.activation(out=l_[:], in_=l_[:], func=AF.Exp, scale=-1.0), r=[l_.k], w=[l_.k])
                P.op("act", lambda e: e.activation(out=l_[:], in_=l_[:], func=AF.Ln, bias=1.0), r=[l_.k], w=[l_.k])
                pbc, pba, ppc = psb(C), psb(C), psb(C)
                P.op("pe", lambda e: e.matmul(pbc[:, 0:256], lhsT=tri[:, 0, :], rhs=l_[:], start=True, stop=True), r=[tri.k, l_.k], w=[pbc.k])
                P.op("pe", lambda e: e.matmul(pba[:, 0:256], lhsT=tri[:, 1, :], rhs=l_[:], start=True, stop=True), r=[tri.k, l_.k], w=[pba.k])
                for h in range(4):
                    P.op("pe", lambda e: e.matmul(ppc[0:64, h:h + 1], lhsT=l_[:, h * 64:(h + 1) * 64], rhs=ncol[:], start=True, stop=True),
                         r=[l_.k, ncol.k], w=[ppc.k], inc=(h == 3))
                P.op("act", lambda e: e.activation(out=Eq[:], in_=pbc[:, 0:256], func=AF.Exp), r=[pbc.k], w=[Eq.k])
                P.op("act", lambda e: e.activation(out=Ei[:], in_=pbc[:, 0:256], func=AF.Exp, scale=-1.0), r=[pbc.k], w=[Ei.k])
                P.op("act", lambda e: e.activation(out=Ee[:], in_=pba[:, 0:256], func=AF.Exp), r=[pba.k], w=[Ee.k])
                P.op("act", lambda e: e.activation(out=PCg[:], in_=ppc[0:64, 0:4], func=AF.Exp), r=[ppc.k], w=[PCg.k])
                P.op("dve", lambda e: e.scalar_tensor_tensor(qd[:], pqk[:, 0:256], 0.125, Eq[:], mm, mm), r=[pqk.k, Eq.k], w=[qd.k])
                P.op("dve", lambda e: e.tensor_tensor(ki[:], pqk[:, 256:512], Ei[:], mm), r=[pqk.k, Ei.k], w=[ki.k])
                P.op("dve", lambda e: e.tensor_tensor(ke[:], pqk[:, 256:512], Ee[:], mm), r=[pqk.k, Ee.k], w=[ke.k])
                P.op("act", lambda e: e.copy(v_[:], pv[:, :]), r=[pv.k], w=[v_.k])
                P.op("act", lambda e: e.activation(out=sl_[:], in_=pg[:, :], func=AF.Silu), r=[pg.k], w=[sl_.k])
                for src, dst, en in ((qd, qdT, "act"), (ki, kiT, "dve")):
                    pb = psb(C)
                    for h in range(4):
                        P.op("pe", lambda e: e.transpose(pb[0:64, h * 128:(h + 1) * 128], src[:, h * 64:(h + 1) * 64], C.ident[:]),
                             r=[src.k, C.ident_tok], w=[pb.k], inc=(h == 3))
                    if en == "act":
                        P.op("act", lambda e: e.copy(dst[:], hv(pb[0:64, :], 4)), r=[pb.k], w=[dst.k])
                    else:
                        P.op("dve", lambda e: e.tensor_copy(dst[:], hv(pb[0:64, :], 4)), r=[pb.k], w=[dst.k])
                patt = psb(C)
                for h in range(4):
                    P.op("pe", lambda e: e.matmul(patt[:, h * 128:(h + 1) * 128], lhsT=kiT[:, h, :], rhs=qdT[:, h, :], start=True, stop=True),
                         r=[kiT.k, qdT.k], w=[patt.k], inc=(h == 3))
                P.op("dve", lambda e: e.tensor_tensor(attT[:], hv(patt[:, :], 4), iu[:], mm), r=[patt.k, iu.k], w=[attT.k])
                S, Sn = Ss[g % 2], Ss[(g + 1) % 2]
                po, pS = psb(C), psb(C)
                for h in range(4):
                    sl = slice(h * 128, (h + 1) * 128)
                    P.op("pe", lambda e: e.matmul(po[:, sl], lhsT=attT[:, h, :], rhs=v_[:, sl], start=True, stop=False), r=[attT.k, v_.k], w=[po.k], inc=False)
                    P.op("pe", lambda e: e.matmul(po[:, sl], lhsT=qdT[:, h, :], rhs=S[:, h, :], start=False, stop=True), r=[qdT.k, S.k], w=[po.k], inc=(h == 3))
                for h in range(4):
                    sl = slice(h * 128, (h + 1) * 128)
                    P.op("pe", lambda e: e.matmul(pS[0:64, sl], lhsT=ke[:, h * 64:(h + 1) * 64], rhs=v_[:, sl], start=True, stop=True), r=[ke.k, v_.k], w=[pS.k], inc=(h == 3))
                P.op("pool", lambda e: e.tensor_tensor(Sn[:], S[:], PCg[:, :].unsqueeze(2).to_broadcast([64, 4, 128]), mm), r=[S.k, PCg.k], w=[Sn.k])
                P.op("dve", lambda e: e.tensor_tensor(Sn[:], Sn[:], hv(pS[0:64, :], 4), ALU.add), r=[Sn.k, pS.k], w=[Sn.k])
                P.op("act", lambda e: e.copy(o_[:], po[:, :]), r=[po.k], w=[o_.k])
                P.op("pool", lambda e: e.tensor_tensor(sq[:], o_[:], o_[:], mm), r=[o_.k], w=[sq.k])
                P.op("dve", lambda e: e.tensor_reduce(m4[:], hv(sq[:], 4), AX.X, ALU.add), r=[sq.k], w=[m4.k])
                P.op("act", lambda e: e.activation(out=m4[:], in_=m4[:], func=AF.Sqrt, bias=1e-5, scale=1.0 / 128), r=[m4.k], w=[m4.k])
                P.op("dve", lambda e: e.reciprocal(m4[:], m4[:]), r=[m4.k], w=[m4.k])
                P.op("dve", lambda e: e.tensor_tensor(hv(o_[:], 4), hv(o_[:], 4), m4[:, :].unsqueeze(2).to_broadcast([128, 4, 128]), mm), r=[o_.k, m4.k], w=[o_.k])
                P.op("pool", lambda e: e.tensor_tensor(o_[:], o_[:], ngb[:], mm), r=[o_.k, ngb.k], w=[o_.k])
                P.op("dve", lambda e: e.tensor_tensor(o_[:], o_[:], sl_[:], mm), r=[o_.k, sl_.k], w=[o_.k])
                if "dbg_yb" in C.dbg:
                    P.dma("sp", C.dbg["dbg_yb"][g * 128:(g + 1) * 128, :], o_[:], r=[o_.k])
                pb = psb(C)
                for q in range(4):
                    P.op("pe", lambda e: e.transpose(pb[:, q * 128:(q + 1) * 128], o_[:, q * 128:(q + 1) * 128], C.ident[:]), r=[o_.k, C.ident_tok], w=[pb.k], inc=(q == 3))
                P.op("act", lambda e: e.copy(ys[:, :, t0:t0 + 128], hv(pb[:, :], 4)), r=[pb.k], w=[ys.k])
            P.dma("sp", XTv(C.YT)[:, 4:8, s * 512:(s + 1) * 512], ys[:], r=[ys.k], w=[C.YT_tok[1][s]])
        P.barrier()


def ln_inplace(C, xt, gb, bb, st, junk):
    P = C.P
    P.op("dve", lambda e: e.tensor_reduce(st[:, 0:1], xt[:], AX.X, ALU.add), r=[xt.k], w=[st.k])
    P.op("dve", lambda e: e.tensor_scalar(st[:, 0:1], st[:, 0:1], 1.0 / D, None, ALU.mult), r=[st.k], w=[st.k])
    P.op("dve", lambda e: e.tensor_scalar(xt[:], xt[:], st[:, 0:1], None, ALU.subtract), r=[xt.k, st.k], w=[xt.k])
    P.op("act", lambda e: e.activation(out=junk[:], in_=xt[:], func=AF.Square, accum_out=st[:, 1:2]), r=[xt.k], w=[junk.k, st.k])
    P.op("act", lambda e: e.activation(out=st[:, 1:2], in_=st[:, 1:2], func=AF.Sqrt, bias=LN_EPS, scale=1.0 / D), r=[st.k], w=[st.k])
    P.op("dve", lambda e: e.reciprocal(st[:, 1:2], st[:, 1:2]), r=[st.k], w=[st.k])
    P.op("dve", lambda e: e.scalar_tensor_tensor(xt[:], xt[:], st[:, 1:2], gb[:], ALU.mult, ALU.mult), r=[xt.k, st.k, gb.k], w=[xt.k])
    P.op("pool", lambda e: e.tensor_tensor(xt[:], xt[:], bb[:], ALU.add), r=[xt.k, bb.k], w=[xt.k])


def ln_lockstep(C, xs, gb, bb, sts, junk):
    P = C.P
    n = len(xs)
    for k in range(n):
        xt, st = xs[k], sts[k]
        P.op("dve", lambda e: e.tensor_reduce(st[:, 0:1], xt[:], AX.X, ALU.add), r=[xt.k], w=[st.k])
    for k in range(n):
        xt, st = xs[k], sts[k]
        P.op("dve", lambda e: e.tensor_scalar(st[:, 0:1], st[:, 0:1], 1.0 / D, None, ALU.mult), r=[st.k], w=[st.k])
    for k in range(n):
        xt, st = xs[k], sts[k]
        P.op("dve", lambda e: e.tensor_scalar(xt[:], xt[:], st[:, 0:1], None, ALU.subtract), r=[xt.k, st.k], w=[xt.k])
        P.op("act", lambda e: e.activation(out=junk[:], in_=xt[:], func=AF.Square, accum_out=st[:, 1:2]), r=[xt.k], w=[junk.k, st.k])
    for k in range(n):
        xt, st = xs[k], sts[k]
        P.op("act", lambda e: e.activation(out=st[:, 1:2], in_=st[:, 1:2], func=AF.Sqrt, bias=LN_EPS, scale=1.0 / D), r=[st.k], w=[st.k])
    for k in range(n):
        xt, st = xs[k], sts[k]
        P.op("dve", lambda e: e.reciprocal(st[:, 1:2], st[:, 1:2]), r=[st.k], w=[st.k])
    for k in range(n):
        xt, st = xs[k], sts[k]
        P.op("dve", lambda e: e.scalar_tensor_tensor(xt[:], xt[:], st[:, 1:2], gb[:], ALU.mult, ALU.mult), r=[xt.k, st.k, gb.k], w=[xt.k])
        P.op("pool", lambda e: e.tensor_tensor(xt[:], xt[:], bb[:], ALU.add), r=[xt.k, bb.k], w=[xt.k])


def tile_to_stage(C, xt, stage, j):
    P = C.P
    for half in range(2):
        pb = psb(C)
        for c4 in range(4):
            c = half * 4 + c4
            P.op("pe", lambda e: e.transpose(pb[:, c4 * 128:(c4 + 1) * 128], xt[:, c * 128:(c + 1) * 128], C.ident[:]),
                 r=[xt.k, C.ident_tok], w=[pb.k], inc=(c4 == 3))
        dst = stage[:, half * 4:(half + 1) * 4, j * 128:(j + 1) * 128]
        src = hv(pb[:, :], 4)
        if half == 0:
            P.op("act", lambda e: e.copy(dst, src), r=[pb.k], w=[stage.k])
        else:
            P.op("dve", lambda e: e.tensor_copy(dst, src), r=[pb.k], w=[stage.k])


def phase_outproj(C, w_out, srcYT, srcYT_toks, resid, resid_toks, lng, lnb, dstH, dstH_tok, dstHT, dstHT_tok, dbgname=None):
    nc, P, T, I = C.nc, C.P, C.T, C.I
    with ExitStack() as es:
        WO = mk(P, [128, 8, D], BF16, "WO", es)
        for c in range(8):
            P.dma("pool", WO[:, c, :], w_out[c * 128:(c + 1) * 128, :], w=[WO.k])
        gb = mk(P, [128, D], F32, "lng", es)
        bb = mk(P, [128, D], F32, "lnb", es)
        bcast_load(C, gb[:], lng, 128, gb.k)
        bcast_load(C, bb[:], lnb, 128, bb.k)
        yts = [mk(P, [128, 8, 512], BF16, "yt", es) for _ in range(2)]
        xts = [mk(P, [128, D], F32, "xres", es) for _ in range(8)]
        sts = [mk(P, [128, 2], F32, "lnst", es) for _ in range(8)]
        junk = mk(P, [128, D], BF16, "junk", es)
        stg = [mk(P, [128, 8, 512], BF16, "hstg", es) for _ in range(2)]
        pend = None

        def loads(s):
            P.dma("sp", yts[s % 2][:], XTv(srcYT)[:, :, s * 512:(s + 1) * 512], r=srcYT_toks(s), w=[yts[s % 2].k])
            for j in range(4):
                i = s * 4 + j
                xt = xts[(s % 2) * 4 + j]
                P.dma("sp", xt[:], resid[i * 128:(i + 1) * 128, :], r=resid_toks(i), w=[xt.k])

        loads(0)
        for s in range(C.NS):
            yt = yts[s % 2]
            sg_ = stg[s % 2]
            X = xts[(s % 2) * 4:(s % 2) * 4 + 4]
            S = sts[(s % 2) * 4:(s % 2) * 4 + 4]
            for j in range(4):
                xt = X[j]
                for half in range(2):
                    pb = psb(C)
                    for c in range(8):
                        P.op("pe", lambda e: e.matmul(pb[:, :], lhsT=yt[:, c, j * 128:(j + 1) * 128], rhs=WO[:, c, half * 512:(half + 1) * 512], start=(c == 0), stop=(c == 7)),
                             r=[yt.k, WO.k], w=[pb.k], inc=(c == 7))
                    P.op("dve", lambda e: e.scalar_tensor_tensor(xt[:, half * 512:(half + 1) * 512], xt[:, half * 512:(half + 1) * 512], DN_ALPHA, pb[:, :], ALU.mult, ALU.add),
                         r=[xt.k, pb.k], w=[xt.k])
            if pend is not None:
                pend()
            if s + 1 < C.NS:
                loads(s + 1)
            ln_lockstep(C, X, gb, bb, S, junk)
            for j in range(4):
                i = s * 4 + j
                P.dma("sp", dstH[i * 128:(i + 1) * 128, :], X[j][:], r=[X[j].k], w=[dstH_tok[i]])

            def pend(s=s, X=X, sg_=sg_):
                for j in range(4):
                    tile_to_stage(C, X[j], sg_, j)
                P.dma("sp", XTv(dstHT)[:, :, s * 512:(s + 1) * 512], sg_[:], r=[sg_.k], w=[dstHT_tok[s]])
        pend()
        P.barrier()


def phase_moe(C, l, srcH, srcH_tok, srcHT, srcHT_tok, dstX, dstX_tok, dstXT, dstXT_tok):
    nc, P, T, I = C.nc, C.P, C.T, C.I
    mm = ALU.mult
    with ExitStack() as es:
        WD = mk(P, [128, 32, D], BF16, "WD", es)
        for c4 in range(4):
            P.dma("sp", WD[:, c4 * 8:(c4 + 1) * 8, :], C.WD16[l].rearrange("p (c d) -> p c d", c=32)[:, c4 * 8:(c4 + 1) * 8, :], r=[C.WD16_tok[l]], w=[WD.k])
        RW = mk(P, [128, 8, NE], BF16, "RW", es)
        P.dma("pool", RW[:], I["router_w"].rearrange("(c p) e -> p c e", p=128), w=[RW.k])
        rbb = mk(P, [128, NE], F32, "rbb", es)
        bcast_load(C, rbb[:], I["router_bias"], 128, rbb.k)
        SEL = mk(P, [16, 16, 128], BF16, "SEL", es)
        P.dma("pool", SEL[:], I["c_sel"], w=[SEL.k])
        gb = mk(P, [128, D], F32, "lng", es)
        bb = mk(P, [128, D], F32, "lnb", es)
        bcast_load(C, gb[:], I["ln2_g"][l:l + 1, :], 128, gb.k)
        bcast_load(C, bb[:], I["ln2_b"][l:l + 1, :], 128, bb.k)
        hts = [mk(P, [128, 8, 512], BF16, "hT", es) for _ in range(2)]
        xts = [mk(P, [128, D], F32, "hres", es) for _ in range(4)]
        sts = [mk(P, [128, 2], F32, "lnst", es) for _ in range(4)]
        junk = mk(P, [128, D], BF16, "junk", es)
        stg = [mk(P, [128, 8, 512], BF16, "xstg", es) for _ in range(2)]
        actT = mk(P, [128, 32, 512], BF16, "actT", es)
        combT = mk(P, [16, 512], BF16, "combT", es)
        WGUs = [mk(P, [128, 2, 8, DE], BF16, "WGU", es) for _ in range(3)]
        sgl = [mk(P, [128, 512], F32, "sgl", es) for _ in range(2)]
        R4 = range(4)
        s_l = [mk(P, [128, NE], F32, "rs", es) for _ in R4]
        sel_l = [mk(P, [128, NE], F32, "rsel", es) for _ in R4]
        pr_l = [mk(P, [128, 4, 6], F32, "rpr", es) for _ in R4]
        gs_l = [mk(P, [128, 4], F32, "rgs", es) for _ in R4]
        t1_l = [mk(P, [128, 4], F32, "rt1", es) for _ in R4]
        m1_l = [mk(P, [128, 2], F32, "rm1", es) for _ in R4]
        selm_l = [mk(P, [128, NE], F32, "rselm", es) for _ in R4]
        sel2_l = [mk(P, [128, NE], F32, "rsel2", es) for _ in R4]
        comb_l = [mk(P, [128, NE], F32, "rcomb", es) for _ in R4]
        nwl = [0]

        def load_w(e):
            b = nwl[0] % 3
            nwl[0] += 1
            P.dma("sp", WGUs[b][:], C.WGU16[l][e].rearrange("p (t c f) -> p t c f", t=2, c=8), r=[C.WGU16_tok[l][e]], w=[WGUs[b].k])
            return WGUs[b]

        def g4(t):
            return t[:, :].rearrange("p (g e) -> p g e", g=4)

        def load_hT(s):
            P.dma("sp", hts[s % 2][:], XTv(srcHT)[:, :, s * 512:(s + 1) * 512], r=[srcHT_tok[s]], w=[hts[s % 2].k])

        def router_front(s):
            hT = hts[s % 2]
            for j in R4:
                plg = psb(C)
                s_ = s_l[j]
                for c in range(8):
                    P.op("pe", lambda e: e.matmul(plg[:, 0:NE], lhsT=hT[:, c, j * 128:(j + 1) * 128], rhs=RW[:, c, :], start=(c == 0), stop=(c == 7)),
                         r=[hT.k, RW.k], w=[plg.k], inc=(c == 7))
                P.op("act", lambda e: e.activation(out=s_[:], in_=plg[:, 0:NE], func=AF.Sigmoid), r=[plg.k], w=[s_.k])

            def step(fn):
                for j in R4:
                    fn(s_l[j], sel_l[j], pr_l[j], gs_l[j], t1_l[j], m1_l[j], selm_l[j], sel2_l[j], comb_l[j])
            step(lambda s_, sel, pr, gs, t1, m1, selm, sel2, comb: P.op("dve", lambda e: e.tensor_tensor(sel[:], s_[:], rbb[:], ALU.add), r=[s_.k, rbb.k], w=[sel.k]))
            step(lambda s_, sel, pr, gs, t1, m1, selm, sel2, comb: P.op("dve", lambda e: e.tensor_tensor(pr[:, :, 0:3], g4(sel)[:, :, 0:3], g4(sel)[:, :, 1:4], ALU.add), r=[sel.k], w=[pr.k]))
            step(lambda s_, sel, pr, gs, t1, m1, selm, sel2, comb: P.op("dve", lambda e: e.tensor_tensor(pr[:, :, 3:5], g4(sel)[:, :, 0:2], g4(sel)[:, :, 2:4], ALU.add), r=[sel.k], w=[pr.k]))
            step(lambda s_, sel, pr, gs, t1, m1, selm, sel2, comb: P.op("dve", lambda e: e.tensor_tensor(pr[:, :, 5:6], g4(sel)[:, :, 0:1], g4(sel)[:, :, 3:4], ALU.add), r=[sel.k], w=[pr.k]))
            step(lambda s_, sel, pr, gs, t1, m1, selm, sel2, comb: P.op("dve", lambda e: e.tensor_reduce(gs[:], pr[:], AX.X, ALU.max), r=[pr.k], w=[gs.k]))
            step(lambda s_, sel, pr, gs, t1, m1, selm, sel2, comb: P.op("dve", lambda e: e.tensor_reduce(m1[:, 0:1], gs[:], AX.X, ALU.max), r=[gs.k], w=[m1.k]))
            step(lambda s_, sel, pr, gs, t1, m1, selm, sel2, comb: P.op("dve", lambda e: e.tensor_scalar(gs[:], gs[:], m1[:, 0:1], None, ALU.is_ge), r=[gs.k, m1.k], w=[gs.k]))
            step(lambda s_, sel, pr, gs, t1, m1, selm, sel2, comb: P.op("dve", lambda e: e.tensor_scalar(t1[:], gs[:], -1.0, 1e30, ALU.add, ALU.mult), r=[gs.k], w=[t1.k]))
            step(lambda s_, sel, pr, gs, t1, m1, selm, sel2, comb: P.op("dve", lambda e: e.tensor_tensor(g4(selm), g4(sel), gs[:, :].unsqueeze(2).to_broadcast([128, 4, 4]), mm), r=[sel.k, gs.k], w=[selm.k]))
            step(lambda s_, sel, pr, gs, t1, m1, selm, sel2, comb: P.op("dve", lambda e: e.tensor_tensor(g4(selm), g4(selm), t1[:, :].unsqueeze(2).to_broadcast([128, 4, 4]), ALU.add), r=[selm.k, t1.k], w=[selm.k]))
            step(lambda s_, sel, pr, gs, t1, m1, selm, sel2, comb: P.op("dve", lambda e: e.tensor_reduce(m1[:, 0:1], selm[:], AX.X, ALU.max), r=[selm.k], w=[m1.k]))
            step(lambda s_, sel, pr, gs, t1, m1, selm, sel2, comb: P.op("dve", lambda e: e.tensor_scalar(sel2[:], selm[:], m1[:, 0:1], None, ALU.is_ge), r=[selm.k, m1.k], w=[sel2.k]))
            step(lambda s_, sel, pr, gs, t1, m1, selm, sel2, comb: P.op("dve", lambda e: e.scalar_tensor_tensor(sel2[:], sel2[:], -1e30, selm[:], mm, ALU.add), r=[sel2.k, selm.k], w=[sel2.k]))
            step(lambda s_, sel, pr, gs, t1, m1, selm, sel2, comb: P.op("dve", lambda e: e.tensor_reduce(m1[:, 1:2], sel2[:], AX.X, ALU.max), r=[sel2.k], w=[m1.k]))
            step(lambda s_, sel, pr, gs, t1, m1, selm, sel2, comb: P.op("dve", lambda e: e.tensor_scalar(sel2[:], selm[:], m1[:, 1:2], None, ALU.is_ge), r=[selm.k, m1.k], w=[sel2.k]))
            step(lambda s_, sel, pr, gs, t1, m1, selm, sel2, comb: P.op("dve", lambda e: e.tensor_tensor(comb[:], s_[:], sel2[:], mm), r=[s_.k, sel2.k], w=[comb.k]))
            step(lambda s_, sel, pr, gs, t1, m1, selm, sel2, comb: P.op("dve", lambda e: e.tensor_reduce(m1[:, 0:1], comb[:], AX.X, ALU.add), r=[comb.k], w=[m1.k]))
            step(lambda s_, sel, pr, gs, t1, m1, selm, sel2, comb: P.op("dve", lambda e: e.reciprocal(m1[:, 0:1], m1[:, 0:1]), r=[m1.k], w=[m1.k]))
            step(lambda s_, sel, pr, gs, t1, m1, selm, sel2, comb: P.op("dve", lambda e: e.tensor_scalar(comb[:], comb[:], m1[:, 0:1], None, mm), r=[comb.k, m1.k], w=[comb.k]))

        def router_back(s):
            for j in R4:
                comb = comb_l[j]
                pct = psb(C)
                P.op("pe", lambda e: e.transpose(pct[0:16, 0:128], comb[:, :], C.ident[:]), r=[comb.k, C.ident_tok], w=[pct.k])
                P.op("act", lambda e: e.copy(combT[:, j * 128:(j + 1) * 128], pct[0:16, 0:128]), r=[pct.k], w=[combT.k])

        def load_x(s):
            for j in R4:
                i = s * 4 + j
                P.dma("sp", xts[j][:], srcH[i * 128:(i + 1) * 128, :], r=[srcH_tok[i]], w=[xts[j].k])

        def make_pend(s):
            xs_ = stg[s % 2]

            def pend():
                for j in R4:
                    tile_to_stage(C, xts[j], xs_, j)
                P.dma("sp", XTv(dstXT)[:, :, s * 512:(s + 1) * 512], xs_[:], r=[xs_.k], w=[dstXT_tok[s]])
            return pend

        pend = None
        load_hT(0)
        router_front(0)
        router_back(0)
        for s in range(C.NS):
            hT = hts[s % 2]
            if s + 1 < C.NS:
                load_hT(s + 1)
            for ex in range(NE):
                WGU = load_w(ex)
                pcb = psb(C)
                P.op("pe", lambda e: e.matmul(pcb[:, :], lhsT=SEL[:, ex, :], rhs=combT[:, :], start=True, stop=True), r=[SEL.k, combT.k], w=[pcb.k])
                for f in range(2):
                    pG, pU = psb(C), psb(C)
                    for pb, ti in ((pG, 0), (pU, 1)):
                        for c in range(8):
                            P.op("pe", lambda e: e.matmul(pb[:, :], lhsT=WGU[:, ti, c, f * 128:(f + 1) * 128], rhs=hT[:, c, :], start=(c == 0), stop=(c == 7)),
                                 r=[WGU.k, hT.k], w=[pb.k], inc=(c == 7))
                    sg_ = sgl[(ex * 2 + f) % 2]
                    P.op("act", lambda e: e.activation(out=sg_[:], in_=pG[:, :], func=AF.Silu), r=[pG.k], w=[sg_.k])
                    P.op("dve", lambda e: e.tensor_tensor(sg_[:], sg_[:], pU[:, :], mm), r=[sg_.k, pU.k], w=[sg_.k])
                    P.op("dve", lambda e: e.tensor_tensor(actT[:, ex * 2 + f, :], sg_[:], pcb[:, :], mm), r=[sg_.k, pcb.k], w=[actT.k])
                if ex == 3:
                    if pend is not None:
                        pend()
                        pend = None
                    load_x(s)
            if s + 1 < C.NS:
                router_front(s + 1)
            for j in R4:
                xt = xts[j]
                for half in range(2):
                    pb = psb(C)
                    for c in range(32):
                        P.op("pe", lambda e: e.matmul(pb[:, :], lhsT=actT[:, c, j * 128:(j + 1) * 128], rhs=WD[:, c, half * 512:(half + 1) * 512], start=(c == 0), stop=(c == 31)),
                             r=[actT.k, WD.k], w=[pb.k], inc=(c == 31))
                    P.op("dve", lambda e: e.scalar_tensor_tensor(xt[:, half * 512:(half + 1) * 512], xt[:, half * 512:(half + 1) * 512], DN_ALPHA, pb[:, :], ALU.mult, ALU.add),
                         r=[xt.k, pb.k], w=[xt.k])
            if s + 1 < C.NS:
                router_back(s + 1)
            ln_lockstep(C, xts, gb, bb, sts, junk)
            for j in R4:
                i = s * 4 + j
                P.dma("sp", dstX[i * 128:(i + 1) * 128, :], xts[j][:], r=[xts[j].k], w=[dstX_tok[i]])
            if dstXT is not None:
                pend = make_pend(s)
        if pend is not None:
            pend()
        P.barrier()


def rope_consts(T):
    pos = np.arange(T, dtype=np.float64)
    c = {}
    for name, half in (("k", 64), ("i", 32)):
        inv = 10000.0 ** (-np.arange(half, dtype=np.float64) / half)
        ang = (pos.astype(np.float32)[:, None] * inv.astype(np.float32)[None, :]).astype(np.float32).astype(np.float64)
        cs = np.cos(ang).astype(np.float32).reshape(T // 128, 128, half).transpose(1, 0, 2)
        sn = np.sin(ang).astype(np.float32).reshape(T // 128, 128, half).transpose(1, 0, 2)
        c["cos_" + name] = np.ascontiguousarray(cs)
        c["sin_" + name] = np.ascontiguousarray(sn)
    q = np.arange(128)[:, None]
    s = np.arange(128)[None, :]
    c["cbias"] = np.where(s <= q, 0.0, -1e30).astype(np.float32)
    c["halfpow"] = (0.5 ** np.arange(1, 33, dtype=np.float64)).astype(np.float32).reshape(1, 32)
    c["tiebias"] = (-1e-6 * np.arange(T, dtype=np.float64)).astype(np.float32).reshape(1, T)
    return c


def rope_tm(C, dst_ap, dst_k, src, src_k, cosb, sinb, nh, half, ta_ap, ta_k, tb_ap, tb_k, rdeps):
    P = C.P
    mm = ALU.mult
    n = nh * 2
    s3 = src.rearrange("p (n f) -> p n f", n=n)
    cb = cosb.unsqueeze(1).to_broadcast([128, n, half])
    sb_ = sinb.unsqueeze(1).to_broadcast([128, n, half])
    a3 = ta_ap.rearrange("p (n f) -> p n f", n=n)
    b3 = tb_ap.rearrange("p (n f) -> p n f", n=n)
    P.op("dve", lambda e: e.tensor_tensor(a3, s3, cb, mm), r=[src_k] + rdeps, w=[ta_k])
    P.op("dve", lambda e: e.tensor_tensor(b3, s3, sb_, mm), r=[src_k] + rdeps, w=[tb_k])
    a4 = ta_ap.rearrange("p (h t f) -> p h t f", h=nh, t=2)
    b4 = tb_ap.rearrange("p (h t f) -> p h t f", h=nh, t=2)
    d4 = dst_ap.rearrange("p (h t f) -> p h t f", h=nh, t=2)
    P.op("pool", lambda e: e.tensor_tensor(d4[:, :, 0, :], a4[:, :, 0, :], b4[:, :, 1, :], ALU.subtract), r=[ta_k, tb_k], w=[dst_k])
    P.op("pool", lambda e: e.tensor_tensor(d4[:, :, 1, :], a4[:, :, 1, :], b4[:, :, 0, :], ALU.add), r=[ta_k, tb_k], w=[dst_k])


def phase_dsa(C):
    nc, P, T, I = C.nc, C.P, C.T, C.I
    mm = ALU.mult
    KT = min(256, T // 4)
    NIT = 25
    SCALE = 128 ** -0.5
    C.bank_pool = [0, 1, 2, 3, 4]
    with ExitStack() as es:
        def wt(name, shape, dt=F32):
            return mk(P, list(shape), dt, name, es)
        WQ = wt("WQ", (128, 8, 1024), BF16)
        WR = wt("WR", (128, 8, 580), BF16)
        for c in range(8):
            P.dma("pool", WQ[:, c, :], I["w_in_odd"][c * 128:(c + 1) * 128, 0:1024], w=[WQ.k])
            P.dma("pool", WR[:, c, :], I["w_in_odd"][c * 128:(c + 1) * 128, 1024:1604], w=[WR.k])
        RTs = [[wt("CK", (128, 4, 64)), wt("SK", (128, 4, 64)), wt("CI", (128, 4, 32)), wt("SI", (128, 4, 32))] for _ in range(2)]

        def load_tables(s):
            tl = RTs[s % 2]
            for t_, nm in zip(tl, ("c_cos_k", "c_sin_k", "c_cos_i", "c_sin_i")):
                P.dma("sp", t_[:], I[nm][:, s * 4:(s + 1) * 4, :], w=[t_.k])
            return tl
        BIAS = wt("BIAS", (128, T))
        bcast_load(C, BIAS[:], I["c_tiebias"], 128, BIAS.k)
        CB = wt("CB", (128, 128))
        P.dma("sp", CB[:], I["c_cbias"], w=[CB.k])
        ikg, ikb = wt("ikg", (128, 64)), wt("ikb", (128, 64))
        bcast_load(C, ikg[:], I["c_ik_ln_g"], 128, ikg.k)
        bcast_load(C, ikb[:], I["c_ik_ln_b"], 128, ikb.k)
        kT = wt("kT", (128, T), BF16)
        ikT = wt("ikT", (128, T), BF16)
        Vx = wt("Vx", (128, C.NT, 129), BF16)
        P.op("dve", lambda e: e.memset(Vx[:, :, 128:129], 1.0), w=[Vx.k])
        xTs = [wt("xTd", (128, 8, 512), BF16) for _ in range(2)]
        ta, tb = wt("ropeA", (128, 512)), wt("ropeB", (128, 512))
        kr = wt("kr", (128, 128))
        ikr = wt("ikr", (128, 128))
        st2 = wt("st2", (128, 2))
        for s in range(C.NS):
            xT = xTs[s % 2]
            P.dma("sp", xT[:], XTv(C.XT1)[:, :, s * 512:(s + 1) * 512], r=[C.XT1_tok[s]], w=[xT.k])
            CK, SK, CI, SI = load_tables(s)
            for j in range(4):
                i = s * 4 + j
                pb = psb(C)
                for c in range(8):
                    P.op("pe", lambda e: e.matmul(pb[:, 0:256], lhsT=xT[:, c, j * 128:(j + 1) * 128], rhs=WR[:, c, 0:256], start=(c == 0), stop=(c == 7)),
                         r=[xT.k, WR.k], w=[pb.k], inc=False)
                for c in range(8):
                    P.op("pe", lambda e: e.matmul(pb[:, 256:320], lhsT=xT[:, c, j * 128:(j + 1) * 128], rhs=WR[:, c, 512:576], start=(c == 0), stop=(c == 7)),
                         r=[xT.k, WR.k], w=[pb.k], inc=(c == 7))
                rope_tm(C, kr[:, :], kr.k, pb[:, 0:128], pb.k, CK[:, j, :], SK[:, j, :], 1, 64, ta[:, 0:128], ta.k, tb[:, 0:128], tb.k, [CK.k, SK.k])
                P.op("act", lambda e: e.copy(Vx[:, i, 0:128], pb[:, 128:256]), r=[pb.k], w=[Vx.k])
                P.op("dve", lambda e: e.tensor_reduce(st2[:, 0:1], pb[:, 256:320], AX.X, ALU.add), r=[pb.k], w=[st2.k])
                P.op("dve", lambda e: e.tensor_scalar(st2[:, 0:1], st2[:, 0:1], 1.0 / 64, None, mm), r=[st2.k], w=[st2.k])
                P.op("dve", lambda e: e.tensor_scalar(ikr[:, 0:64], pb[:, 256:320], st2[:, 0:1], None, ALU.subtract), r=[pb.k, st2.k], w=[ikr.k])
                P.op("act", lambda e: e.activation(out=ikr[:, 64:128], in_=ikr[:, 0:64], func=AF.Square, accum_out=st2[:, 1:2]), r=[ikr.k], w=[ikr.k, st2.k])
                P.op("act", lambda e: e.activation(out=st2[:, 1:2], in_=st2[:, 1:2], func=AF.Sqrt, bias=LN_EPS, scale=1.0 / 64), r=[st2.k], w=[st2.k])
                P.op("dve", lambda e: e.reciprocal(st2[:, 1:2], st2[:, 1:2]), r=[st2.k], w=[st2.k])
                P.op("dve", lambda e: e.scalar_tensor_tensor(ikr[:, 0:64], ikr[:, 0:64], st2[:, 1:2], ikg[:], mm, mm), r=[ikr.k, st2.k, ikg.k], w=[ikr.k])
                P.op("dve", lambda e: e.tensor_tensor(ikr[:, 64:128], ikr[:, 0:64], ikb[:], ALU.add), r=[ikr.k, ikb.k], w=[ikr.k])
                ikn = TL(ikr.t, ikr.k)
                rope_src = ikr[:, 64:128]
                n = 2
                s3 = rope_src.rearrange("p (n f) -> p n f", n=n)
                cb = CI[:, j, :].unsqueeze(1).to_broadcast([128, n, 32])
                sb_ = SI[:, j, :].unsqueeze(1).to_broadcast([128, n, 32])
                a3 = ta[:, 0:64].rearrange("p (n f) -> p n f", n=n)
                b3 = tb[:, 0:64].rearrange("p (n f) -> p n f", n=n)
                P.op("dve", lambda e: e.tensor_tensor(a3, s3, cb, mm), r=[ikr.k, CI.k], w=[ta.k])
                P.op("dve", lambda e: e.tensor_tensor(b3, s3, sb_, mm), r=[ikr.k, SI.k], w=[tb.k])
                P.op("pool", lambda e: e.tensor_tensor(ikr[:, 0:32], ta[:, 0:32], tb[:, 32:64], ALU.subtract), r=[ta.k, tb.k], w=[ikr.k])
                P.op("pool", lambda e: e.tensor_tensor(ikr[:, 32:64], ta[:, 32:64], tb[:, 0:32], ALU.add), r=[ta.k, tb.k], w=[ikr.k])
                P.op("pool", lambda e: e.tensor_copy(ikr[:, 64:128], ikr[:, 0:64]), r=[ikr.k], w=[ikr.k])
                pt = psb(C)
                P.op("pe", lambda e: e.transpose(pt[:, 0:128], kr[:, :], C.ident[:]), r=[kr.k, C.ident_tok], w=[pt.k], inc=False)
                P.op("pe", lambda e: e.transpose(pt[:, 128:256], ikr[:, :], C.ident[:]), r=[ikr.k, C.ident_tok], w=[pt.k])
                P.op("act", lambda e: e.copy(kT[:, i * 128:(i + 1) * 128], pt[:, 0:128]), r=[pt.k], w=[kT.k])
                P.op("act", lambda e: e.mul(ikT[:, i * 128:(i + 1) * 128], pt[:, 128:256], 0.125), r=[pt.k], w=[ikT.k])
        SC = wt("SC", (128, T))
        MASKs = [wt("MASK", (128, T)) for _ in range(2)]
        junk = wt("junkd", (128, T), BF16)
        qr = wt("qr", (128, 1024))
        iqr = wt("iqr", (128, 256))
        qTs = [wt("qT", (128, 8, 128), BF16) for _ in range(2)]
        iqT = wt("iqT", (128, 2, 128), BF16)
        iws = wt("iws", (128, 4))
        rl = [wt("rl%d" % n, (128, 512)) for n in range(2)]
        bs = wt("bs", (128, 8))
        Dk = wt("Dk", (128, NIT))
        HK = wt("HK", (128, NIT))
        bcast_load(C, HK[:], I["c_halfpow"][0:1, 0:NIT], 128, HK.k)
        mT4 = [wt("mT4_%d" % n, (128, 4, 128), BF16) for n in range(2)]
        pTs = [wt("pT%d" % n, (128, 4, 128), BF16) for n in range(4)]
        o_ = wt("od", (128, 1024))
        rs8 = wt("rs8", (128, 8))
        ostg = [wt("ostg", (128, 8, 512), BF16) for _ in range(2)]
        accb = [TL(*C.banks[b]) for b in (5, 6, 7)]
        MBIG = 30000.0
        identb = wt("identb", (128, 128), BF16)
        P.op("dve", lambda e: e.tensor_copy(identb[:], C.ident[:]), r=[C.ident_tok], w=[identb.k])
        acc_of = [(0, 0), (0, 1), (0, 2), (1, 0), (1, 1), (1, 2), (2, 0), (2, 1)]
        def qblock(s, j):
            if True:
                i = s * 4 + j
                L = (i + 1) * 128
                xT = xTs[s % 2]
                og = ostg[s % 2]
                qT = qTs[i % 2]
                MASK = MASKs[i % 2]
                if j == 0:
                    P.dma("sp", xT[:], XTv(C.XT1)[:, :, s * 512:(s + 1) * 512], r=[C.XT1_tok[s]], w=[xT.k])
                    load_tables(s)
                CK, SK, CI, SI = RTs[s % 2]
                pq = [psb(C), psb(C)]
                for half in range(2):
                    for c in range(8):
                        P.op("pe", lambda e: e.matmul(pq[half][:, :], lhsT=xT[:, c, j * 128:(j + 1) * 128], rhs=WQ[:, c, half * 512:(half + 1) * 512], start=(c == 0), stop=(c == 7)),
                             r=[xT.k, WQ.k], w=[pq[half].k], inc=(c == 7))
                piq = psb(C)
                for c in range(8):
                    P.op("pe", lambda e: e.matmul(piq[:, 0:256], lhsT=xT[:, c, j * 128:(j + 1) * 128], rhs=WR[:, c, 256:512], start=(c == 0), stop=(c == 7)),
                         r=[xT.k, WR.k], w=[piq.k], inc=False)
                for c in range(8):
                    P.op("pe", lambda e: e.matmul(piq[:, 256:260], lhsT=xT[:, c, j * 128:(j + 1) * 128], rhs=WR[:, c, 576:580], start=(c == 0), stop=(c == 7)),
                         r=[xT.k, WR.k], w=[piq.k], inc=(c == 7))
                for half in range(2):
                    hs = slice(half * 512, (half + 1) * 512)
                    rope_tm(C, qr[:, hs], qr.k, pq[half][:, :], pq[half].k, CK[:, j, :], SK[:, j, :], 4, 64,
                            ta[:, 0:512], ta.k, tb[:, 0:512], tb.k, [CK.k, SK.k])
                rope_tm(C, iqr[:, :], iqr.k, piq[:, 0:256], piq.k, CI[:, j, :], SI[:, j, :], 4, 32, ta[:, 0:256], ta.k, tb[:, 0:256], tb.k, [CI.k, SI.k])
                P.op("act", lambda e: e.mul(iws[:], piq[:, 256:260], 0.5), r=[piq.k], w=[iws.k])
                for half in range(2):
                    pb = psb(C)
                    for c4 in range(4):
                        h = half * 4 + c4
                        P.op("pe", lambda e: e.transpose(pb[:, c4 * 128:(c4 + 1) * 128], qr[:, h * 128:(h + 1) * 128], C.ident[:]), r=[qr.k, C.ident_tok], w=[pb.k], inc=(c4 == 3))
                    P.op("act", lambda e: e.copy(qT[:, half * 4:(half + 1) * 4, :], hv(pb[:, :], 4)), r=[pb.k], w=[qT.k])
                pb = psb(C)
                for c2 in range(2):
                    P.op("pe", lambda e: e.transpose(pb[:, c2 * 128:(c2 + 1) * 128], iqr[:, c2 * 128:(c2 + 1) * 128], C.ident[:]), r=[iqr.k, C.ident_tok], w=[pb.k], inc=(c2 == 1))
                P.op("act", lambda e: e.copy(iqT[:], hv(pb[:, 0:256], 2)), r=[pb.k], w=[iqT.k])
                yield "F"
                for k0 in range(0, L, 512):
                    kw = min(512, L - k0)
                    for h in range(4):
                        ph = psb(C)
                        pl = (h % 2) * 64
                        P.op("pe", lambda e: e.matmul(ph[:, 0:kw], lhsT=iqT[pl:pl + 64, h // 2, :], rhs=ikT[pl:pl + 64, k0:k0 + kw], start=True, stop=True),
                             r=[iqT.k, ikT.k], w=[ph.k])
                        r_ = rl[h % 2]
                        P.op("act", lambda e: e.activation(out=r_[:, 0:kw], in_=ph[:, 0:kw], func=AF.Relu), r=[ph.k], w=[r_.k])
                        if h == 0:
                            P.op("dve", lambda e: e.scalar_tensor_tensor(SC[:, k0:k0 + kw], r_[:, 0:kw], iws[:, 0:1], BIAS[:, k0:k0 + kw], mm, ALU.add), r=[r_.k, iws.k, BIAS.k], w=[SC.k])
                        else:
                            P.op("dve", lambda e: e.scalar_tensor_tensor(SC[:, k0:k0 + kw], r_[:, 0:kw], iws[:, h:h + 1], SC[:, k0:k0 + kw], mm, ALU.add),
                                 r=[r_.k, iws.k, SC.k], w=[SC.k])
                    yield "F"
                if L > KT:
                    P.op("dve", lambda e: e.tensor_reduce(bs[:, 1:2], SC[:, 0:L], AX.X, ALU.max, apply_absolute_value=True), r=[SC.k], w=[bs.k])
                    P.op("dve", lambda e: e.tensor_scalar(bs[:, 0:1], bs[:, 1:2], -1.0, -1.0, mm, ALU.add), r=[bs.k], w=[bs.k])
                    P.op("dve", lambda e: e.tensor_scalar(bs[:, 1:2], bs[:, 1:2], 2.0, 2.0, mm, ALU.add), r=[bs.k], w=[bs.k])
                    P.op("dve", lambda e: e.tensor_scalar(Dk[:], HK[:], bs[:, 1:2], None, mm), r=[bs.k, HK.k], w=[Dk.k])
                P.op("pool", lambda e: e.tensor_tensor(SC[:, i * 128:L], SC[:, i * 128:L], CB[:], ALU.add), r=[SC.k, CB.k], w=[SC.k])
                if L > KT:
                    for it in range(NIT):
                        P.op("dve", lambda e: e.tensor_tensor(bs[:, 2:3], bs[:, 0:1], Dk[:, it:it + 1], ALU.add), r=[bs.k, Dk.k], w=[bs.k])
                        P.op("dve", lambda e: e.tensor_scalar(junk[:, 0:L], SC[:, 0:L], bs[:, 2:3], None, ALU.is_gt, ALU.add, accum_out=bs[:, 3:4]),
                             r=[SC.k, bs.k], w=[junk.k, bs.k])
                        P.op("dve", lambda e: e.scalar_tensor_tensor(bs[:, 4:5], bs[:, 3:4], float(KT) - 0.5, Dk[:, it:it + 1], ALU.is_gt, mm), r=[bs.k, Dk.k], w=[bs.k])
                        P.op("dve", lambda e: e.tensor_tensor(bs[:, 0:1], bs[:, 0:1], bs[:, 4:5], ALU.add), r=[bs.k], w=[bs.k])
                        yield "F"
                    P.op("dve", lambda e: e.tensor_scalar(MASK[:, 0:L], SC[:, 0:L], bs[:, 0:1], None, ALU.is_le), r=[SC.k, bs.k], w=[MASK.k])
                else:
                    P.op("dve", lambda e: e.tensor_scalar(MASK[:, 0:L], SC[:, 0:L], -1e29, None, ALU.is_le), r=[SC.k], w=[MASK.k])
                if "dbg_mask" in C.dbg:
                    P.dma("sp", C.dbg["dbg_mask"][i * 128:(i + 1) * 128, 0:L], MASK[:, 0:L], r=[MASK.k])
                    P.dma("sp", C.dbg["dbg_sc"][i * 128:(i + 1) * 128, 0:L], SC[:, 0:L], r=[SC.k])
                yield "END_FRONT"
                units = [(st, hg) for st in range(i + 1) for hg in range(2)]

                def stage1(st, hg):
                    if hg == 0 and st % 4 == 0:
                        n4 = min(4, i + 1 - st)
                        m4 = mT4[(st // 4) % 2]
                        pb = psb(C)
                        for u in range(n4):
                            P.op("pe", lambda e: e.transpose(pb[:, u * 128:(u + 1) * 128], MASK[:, (st + u) * 128:(st + u + 1) * 128], C.ident[:]),
                                 r=[MASK.k, C.ident_tok], w=[pb.k], inc=(u == n4 - 1))
                        P.op("act", lambda e: e.mul(m4[:, 0:n4, :], hv(pb[:, :], 4)[:, 0:n4, :], -MBIG), r=[pb.k], w=[m4.k])
                    m4 = mT4[(st // 4) % 2]
                    pl_ = psb(C)
                    P.op("pe", lambda e: e.matmul(pl_[:, :], lhsT=identb[:, :], rhs=m4[:, st % 4, :].unsqueeze(1).to_broadcast([128, 4, 128]), start=True, stop=False),
                         r=[identb.k, m4.k], w=[pl_.k], inc=False)
                    P.op("pe", lambda e: e.matmul(pl_[:, :], lhsT=kT[:, st * 128:(st + 1) * 128], rhs=qT[:, hg * 4:(hg + 1) * 4, :].rearrange("p h q -> p (h q)"), start=False, stop=True),
                         r=[kT.k, qT.k], w=[pl_.k])
                    pT = pTs[hg * 2 + st % 2]
                    P.op("act", lambda e: e.activation(out=pT[:], in_=hv(pl_[:, :], 4), func=AF.Exp, scale=SCALE), r=[pl_.k], w=[pT.k])

                def stage2(st, hg):
                    pT = pTs[hg * 2 + st % 2]
                    for hh in range(4):
                        h = hg * 4 + hh
                        ab, slot = acc_of[h]
                        P.op("pe", lambda e: e.matmul(accb[ab][:, slot * 129:(slot + 1) * 129], lhsT=pT[:, hh, :], rhs=Vx[:, st, :], start=(st == 0 and slot == 0), stop=(st == i), skip_group_check=True),
                             r=[pT.k, Vx.k], w=[accb[ab].k], inc=(hh == 3))

                stage1(*units[0])
                for n in range(len(units)):
                    if n + 1 < len(units):
                        stage1(*units[n + 1])
                    stage2(*units[n])
                    if units[n][1] == 1:
                        yield "B"
                for h in range(8):
                    ab, slot = acc_of[h]
                    P.op("dve", lambda e: e.reciprocal(rs8[:, h:h + 1], accb[ab][:, slot * 129 + 128:slot * 129 + 129]), r=[accb[ab].k], w=[rs8.k])
                    P.op("act", lambda e: e.activation(out=o_[:, h * 128:(h + 1) * 128], in_=accb[ab][:, slot * 129:slot * 129 + 128], func=AF.Copy, scale=rs8[:, h:h + 1]),
                         r=[accb[ab].k, rs8.k], w=[o_.k])
                if "dbg_dsa" in C.dbg:
                    P.dma("sp", C.dbg["dbg_dsa"][i * 128:(i + 1) * 128, :], o_[:], r=[o_.k])
                tile_to_stage(C, o_, og, j)
                if j == 3:
                    P.dma("sp", XTv(C.YT)[:, :, s * 512:(s + 1) * 512], og[:], r=[og.k], w=[C.YT_tok[0][s], C.YT_tok[1][s]])

        pipeline2([(lambda s=s, j=j: qblock(s, j)) for s in range(C.NS) for j in range(4)], interleave=C.dsa_interleave)
        P.barrier()
    C.bank_pool = list(range(8))


class _View:
    def __init__(self, tl, sl):
        self.t = _Sl(tl.t, sl)
        self.k = tl.k

    def __getitem__(self, idx):
        return self.t[idx]


class _Sl:
    def __init__(self, t, sl):
        self.base = t
        self.sl = sl

    def __getitem__(self, idx):
        rows, cols = idx
        assert cols == slice(None)
        return self.base[rows, self.sl]


_NC_CACHE = {}


def _in_map(inputs, b, T, consts):
    m = {"x": np.ascontiguousarray(inputs["x"][b, :T], dtype=np.float32)}
    for k, a in inputs.items():
        if k == "x":
            continue
        a = np.asarray(a, dtype=np.float32)
        if k in ("router_w", "exp_w_gate", "exp_w_up", "exp_w_down", "ln1_g", "ln1_b", "ln2_g", "ln2_b"):
            m[k] = np.ascontiguousarray(a)
        elif k == "router_bias":
            m[k] = np.ascontiguousarray(a.reshape(1, -1))
        elif k == "a_r_k":
            m[k] = np.ascontiguousarray(a.reshape(1, 512))
        elif a.ndim == 3:
            m[k] = np.ascontiguousarray(a[0])
        elif a.ndim == 2:
            m[k] = np.ascontiguousarray(a[0:1])
    m.update(consts)
    return m


def kernel(**inputs):
    x = np.asarray(inputs["x"])
    B, T, _ = x.shape
    if T not in _NC_CACHE:
        _NC_CACHE[T] = build(T)
    nc = _NC_CACHE[T]
    consts = {"c_" + k: v for k, v in host_consts(T).items()}
    consts.update({"c_" + k: v for k, v in rope_consts(T).items()})
    in_maps = [_in_map(inputs, b, T, consts) for b in range(B)]
    res = run_bass_kernel_spmd(nc, in_maps, core_ids=list(range(B)))
    out = np.stack([np.asarray(res.results[b]["out"], dtype=np.float32) for b in range(B)], 0)
    return out
```

```python
import numpy as np
import ml_dtypes
from contextlib import ExitStack
import concourse.bass as bass
import concourse.mybir as mybir
from concourse.bass_utils import run_bass_kernel_spmd

F32 = mybir.dt.float32
BF16 = mybir.dt.bfloat16
AF = mybir.ActivationFunctionType
ALU = mybir.AluOpType
AX = mybir.AxisListType

D = 1024
A_COLS = 1792
B_COLS = 1552
EVEN_COLS = 3344
ODD_COLS = 1604
NE = 16
DE = 256
DN_ALPHA = 4 ** 0.25
LN_EPS = 1e-5
DEC = 0.6065306597126334
DSA_INTERLEAVE = True
RWKV_INTERLEAVE = False


class Tok:
    __slots__ = ("w", "r")

    def __init__(self):
        self.w = None
        self.r = {}


class Eng:
    def __init__(self, name, h, sem):
        self.name = name
        self.h = h
        self.sem = sem
        self.cnt = 0
        self.waited = {}


class Prog:
    NSLOT = 8

    def __init__(self, nc, es):
        self.nc = nc
        self.es = es
        self.E = {}
        for name, h in (("pe", nc.tensor), ("act", nc.scalar), ("dve", nc.vector),
                        ("pool", nc.gpsimd), ("sp", nc.sync)):
            sem = es.enter_context(nc.semaphore("sem_" + name))
            self.E[name] = Eng(name, h, sem)
        self.slots = {}
        self.dn = {}
        for q in ("sp", "pool", "act"):
            self.slots[q] = [[es.enter_context(nc.semaphore("dq_%s%d" % (q, i))), 0] for i in range(self.NSLOT)]
            self.dn[q] = 0
        self.nalloc = 0

    def sb(self, shape, dt=F32, name=None, es=None):
        self.nalloc += 1
        t = (es or self.es).enter_context(self.nc.sbuf_tensor("%s_%d" % (name or "t", self.nalloc), list(shape), dt))
        return t

    def _wait(self, eng, ev):
        sem, val = ev
        key = sem.num
        if eng.waited.get(key, 0) >= val:
            return
        eng.h.wait_ge(sem, val)
        eng.waited[key] = val

    def _deps(self, en, r, w):
        eng = self.E[en]
        for t in r:
            if t.w is not None:
                yield t.w
        for t in w:
            if t.w is not None:
                yield t.w
            for ev in t.r.values():
                yield ev

    def op(self, en, fn, r=(), w=(), inc=True):
        eng = self.E[en]
        for ev in list(self._deps(en, r, w)):
            if en == "pe" and ev[0] is eng.sem:
                continue
            self._wait(eng, ev)
        ins = fn(eng.h)
        myev = (eng.sem, eng.cnt + 1)
        if inc:
            ins.then_inc(eng.sem, 1)
            eng.cnt += 1
        for t in r:
            t.r[en] = myev
        for t in w:
            t.w = myev
            t.r = {}
        return ins

    def dma(self, qn, out, in_, r=(), w=(), **kw):
        q = self.E[qn]
        for ev in list(self._deps(qn, r, w)):
            self._wait(q, ev)
        slot = self.slots[qn][self.dn[qn] % self.NSLOT]
        self.dn[qn] += 1
        if slot[1] > 0:
            self._wait(q, (slot[0], slot[1]))
        ins = q.h.dma_start(out=out, in_=in_, **kw)
        slot[1] += 16
        ins.then_inc(slot[0], 16)
        ev = (slot[0], slot[1])
        key = "d%d" % slot[0].num
        for t in r:
            t.r[key] = ev
        for t in w:
            t.w = ev
            t.r = {}

    def barrier(self):
        evs = []
        for q in self.slots:
            for sem, val in self.slots[q]:
                if val > 0:
                    evs.append((sem, val))
        for name, e in self.E.items():
            if e.cnt > 0:
                evs.append((e.sem, e.cnt))
        for name, e in self.E.items():
            for ev in evs:
                if ev[0] is e.sem and name == "pe":
                    continue
                self._wait(e, ev)

    def finish(self):
        sp = self.E["sp"]
        for q in self.slots:
            for sem, val in self.slots[q]:
                if val > 0:
                    self._wait(sp, (sem, val))
        for name, e in self.E.items():
            if name != "sp" and e.cnt > 0:
                self._wait(sp, (e.sem, e.cnt))


def host_consts(T):
    c = {}
    c["ident"] = np.eye(128, dtype=np.float32)
    j = np.arange(64)[:, None]
    i = np.arange(64)[None, :]
    c["tri64"] = np.stack([(-DEC) * (j <= i), (-DEC) * (j < i), (-DEC) * (j > i)], 1).astype(np.float32)
    c["ncol64"] = np.full((64, 1), -DEC, np.float32)
    su = (j < i).astype(np.float32)
    iu = (j <= i).astype(np.float32)
    sl = (j > i).astype(np.float32)
    mMA = np.concatenate([-su, iu], 1)
    mBB = np.concatenate([su, iu], 1)
    c["mMA"] = np.tile(mMA[:, None, :], (1, 8, 1)).astype(np.float32)
    c["mBB"] = np.tile(mBB[:, None, :], (1, 8, 1)).astype(np.float32)
    c["mNT"] = np.tile((-sl)[:, None, :], (1, 8, 1)).astype(np.float32)
    c["id8"] = np.tile(np.eye(64, dtype=np.float32)[:, None, :], (1, 8, 1))
    j = np.arange(128)[:, None]
    i = np.arange(128)[None, :]
    c["tri128"] = np.stack([(-1 / 16) * (j <= i), (-1 / 16) * (j > i)], 1).astype(np.float32)
    c["ncol128"] = np.full((128, 1), -1 / 16, np.float32)
    c["sel"] = (np.arange(16)[:, None, None] == np.arange(16)[None, :, None]).astype(np.float32) * np.ones((1, 1, 128), np.float32)
    c["iu128"] = np.tile((j <= i).astype(np.float32)[:, None, :], (1, 4, 1))
    return c


class Ctx:
    pass


def build(T, dbg=(), stages=("A", "R", "G", "O0", "M0", "S1", "O1", "M1")):
    nc = bass.Bass("TRN2", target_bir_lowering=False)
    es = ExitStack()
    P = Prog(nc, es)
    NT = T // 128
    NS = T // 512
    C = Ctx()
    C.nc, C.P, C.T, C.NT, C.NS = nc, P, T, NT, NS
    C.dbg = {}
    C.dsa_interleave = DSA_INTERLEAVE

    def din(name, shape, dt=F32):
        return nc.dram_tensor(name, list(shape), dt, kind="ExternalInput").ap()

    def dscr(name, shape, dt=F32, out=False):
        kind = "ExternalOutput" if (out or name in dbg) else "Internal"
        return nc.dram_tensor(name, list(shape), dt, kind=kind).ap()

    I = {}
    I["x"] = din("x", [T, D])
    for name, shape in (("w_in_even", [D, EVEN_COLS]), ("a_mu", [1, A_COLS]), ("a_w0", [1, 512]), ("a_w2", [64, 512]),
                        ("a_a0", [1, 512]), ("a_a2", [64, 512]), ("a_g2", [128, 512]), ("a_kk_scale", [1, 512]),
                        ("a_ka_scale", [1, 512]), ("a_r_k", [1, 512]), ("a_gn_g", [1, 512]), ("a_gn_b", [1, 512]),
                        ("b_gate_w2", [16, 256]), ("b_gate_b", [1, 256]), ("b_norm_g", [1, 512]),
                        ("w_out_even", [D, D]), ("w_in_odd", [D, ODD_COLS]), ("c_ik_ln_g", [1, 64]),
                        ("c_ik_ln_b", [1, 64]), ("w_out_odd", [D, D]), ("ln1_g", [2, D]), ("ln1_b", [2, D]),
                        ("ln2_g", [2, D]), ("ln2_b", [2, D]), ("router_w", [D, NE]), ("router_bias", [1, NE]),
                        ("exp_w_gate", [2, NE, D, DE]), ("exp_w_up", [2, NE, D, DE]), ("exp_w_down", [2, NE, DE, D])):
        I[name] = din(name, shape)
    hc = host_consts(T)
    hc.update(rope_consts(T))
    for k, v in hc.items():
        I["c_" + k] = din("c_" + k, list(v.shape), F32 if v.dtype == np.float32 else BF16)
    C.I = I
    out = dscr("out", [T, D], out=True)
    C.XT0 = dscr("XT0", [D, T + 1], BF16)
    C.XT0_tok = [Tok() for _ in range(NS)]
    C.XT0_z = Tok()
    C.YT = dscr("YT", [D, T], BF16)
    C.YT_tok = [[Tok() for _ in range(NS)] for _ in range(2)]
    C.H0 = dscr("H0", [T, D])
    C.H0_tok = [Tok() for _ in range(NT)]
    C.HT0 = dscr("HT0", [D, T], BF16)
    C.HT0_tok = [Tok() for _ in range(NS)]
    C.X1 = dscr("X1", [T, D])
    C.X1_tok = [Tok() for _ in range(NT)]
    C.XT1 = dscr("XT1", [D, T], BF16)
    C.XT1_tok = [Tok() for _ in range(NS)]

    for nm, shp in (("dbg_ya", [T, 512]), ("dbg_yb", [T, 512]), ("dbg_dsa", [T, 1024]), ("dbg_mask", [T, T]), ("dbg_sc", [T, T])):
        if nm in dbg:
            C.dbg[nm] = dscr(nm, shp, out=True)
    C.WGU16 = [dscr("WGU16_%d" % l, [NE, 128, 2 * 8 * DE], BF16) for l in range(2)]
    C.WGU16_tok = [[Tok() for _ in range(NE)] for l in range(2)]
    C.WD16 = [dscr("WD16_%d" % l, [128, 32 * D], BF16) for l in range(2)]
    C.WD16_tok = [Tok() for l in range(2)]
    C.banks = []
    for b in range(8):
        t = es.enter_context(nc.psum_tensor("psb%d" % b, [128, 512], F32))
        C.banks.append((t, Tok()))
    C.bi = 0

    C.bank_pool = list(range(8))

    def bank():
        b = C.banks[C.bank_pool[C.bi % len(C.bank_pool)]]
        C.bi += 1
        return b
    C.bank = bank

    C.ident = P.sb([128, 128], F32, "ident")
    C.ident_tok = Tok()
    P.dma("sp", C.ident[:], I["c_ident"], w=[C.ident_tok])

    C.cast_todo = {}
    if "M0" in stages:
        cast_weights(C, 0)
    if "A" in stages:
        phase_A(C)
    if "R" in stages:
        phase_rwkv(C)
    if "G" in stages:
        phase_gla(C)
    if "M1" in stages:
        cast_weights(C, 1)
    if "O0" in stages:
        phase_outproj(C, I["w_out_even"], C.YT, lambda s: [C.YT_tok[0][s], C.YT_tok[1][s]], I["x"], lambda i: [],
                      I["ln1_g"][0:1, :], I["ln1_b"][0:1, :], C.H0, C.H0_tok, C.HT0, C.HT0_tok)
    if "M0" in stages:
        cast_some(C, 0, 999)
        phase_moe(C, 0, C.H0, C.H0_tok, C.HT0, C.HT0_tok, C.X1, C.X1_tok, C.XT1, C.XT1_tok)
    if "S1" in stages:
        phase_dsa(C)
    if "O1" in stages:
        C.H1 = dscr("H1", [T, D])
        C.H1_tok = [Tok() for _ in range(NT)]
        C.HT1 = dscr("HT1", [D, T], BF16)
        C.HT1_tok = [Tok() for _ in range(NS)]
        phase_outproj(C, I["w_out_odd"], C.YT, lambda s: [C.YT_tok[0][s], C.YT_tok[1][s]], C.X1, lambda i: [C.X1_tok[i]],
                      I["ln1_g"][1:2, :], I["ln1_b"][1:2, :], C.H1, C.H1_tok, C.HT1, C.HT1_tok)
    if "M1" in stages:
        cast_some(C, 1, 999)
        out_tok = [Tok() for _ in range(NT)]
        phase_moe(C, 1, C.H1, C.H1_tok, C.HT1, C.HT1_tok, out, out_tok, None, None)
    P.finish()
    es.close()
    return nc


def XTv(ap):
    return ap.rearrange("(c p) t -> p c t", p=128)


def cast_weights(C, l):
    P, I = C.P, C.I
    th = []
    for e in range(NE):
        dst = C.WGU16[l][e].rearrange("p (t c f) -> p t c f", t=2, c=8)
        th.append(lambda e=e, dst=dst: P.dma("pool", dst[:, 0, :, :], I["exp_w_gate"][l, e].rearrange("(c p) f -> p c f", p=128), w=[C.WGU16_tok[l][e]]))
        th.append(lambda e=e, dst=dst: P.dma("pool", dst[:, 1, :, :], I["exp_w_up"][l, e].rearrange("(c p) f -> p c f", p=128), w=[C.WGU16_tok[l][e]]))
    wd_flat = I["exp_w_down"][l].rearrange("e f d -> (e f) d")
    dstd = C.WD16[l].rearrange("p (c d) -> p c d", c=32)
    for c4 in range(8):
        th.append(lambda c4=c4: P.dma("pool", dstd[:, c4 * 4:(c4 + 1) * 4, :], wd_flat[c4 * 512:(c4 + 1) * 512, :].rearrange("(c p) d -> p c d", p=128), w=[C.WD16_tok[l]]))
    C.cast_todo[l] = th


def cast_some(C, l, n):
    th = C.cast_todo.get(l, [])
    for _ in range(min(n, len(th))):
        th.pop(0)()


def phase_A(C):
    nc, P, T, I = C.nc, C.P, C.T, C.I
    with ExitStack() as es:
        xin = [P.sb([128, D], F32, "xin", es) for _ in range(2)]
        xin_tok = [Tok(), Tok()]
        st = [P.sb([128, 8, 512], BF16, "ast", es) for _ in range(2)]
        st_tok = [Tok(), Tok()]
        z = P.sb([128, 8, 1], BF16, "zc", es)
        zt = Tok()
        P.op("dve", lambda e: e.memset(z[:], 0.0), w=[zt])
        P.dma("sp", XTv(C.XT0)[:, :, 0:1], z[:], r=[zt], w=[C.XT0_z], allow_slow_non_contiguous=True)
        for s in range(C.NS):
            sb_ = st[s % 2]
            for j in range(4):
                i = s * 4 + j
                xb = xin[i % 2]
                xt = xin_tok[i % 2]
                P.dma("sp", xb[:], I["x"][i * 128:(i + 1) * 128, :], w=[xt])
                for half in range(2):
                    bt, bk = C.bank()
                    for c4 in range(4):
                        c = half * 4 + c4
                        P.op("pe", lambda e: e.transpose(bt[:, c4 * 128:(c4 + 1) * 128], xb[:, c * 128:(c + 1) * 128], C.ident[:]),
                             r=[xt, C.ident_tok], w=[bk], inc=(c4 == 3))
                    en = "act" if half == 0 else "dve"
                    src = bt[:, :].rearrange("p (c t) -> p c t", c=4)
                    dst = sb_[:, half * 4:(half + 1) * 4, j * 128:(j + 1) * 128]
                    if en == "act":
                        P.op("act", lambda e: e.copy(dst, src), r=[bk], w=[st_tok[s % 2]])
                    else:
                        P.op("dve", lambda e: e.tensor_copy(dst, src), r=[bk], w=[st_tok[s % 2]])
            P.dma("sp", XTv(C.XT0)[:, :, 1 + s * 512:1 + (s + 1) * 512], sb_[:], r=[st_tok[s % 2]], w=[C.XT0_tok[s]])
        P.barrier()


class TL:
    def __init__(self, t, k=None):
        self.t = t
        self.k = k or Tok()

    def __getitem__(self, idx):
        return self.t[idx]


def mk(P, shape, dt=F32, name=None, es=None):
    return TL(P.sb(shape, dt, name, es))


def bcast_load(C, dst_ap, src_row, np_, tok, q="sp"):
    C.P.dma(q, dst_ap, src_row.partition_broadcast(np_), w=[tok])


def hv(ap, h):
    return ap.rearrange("p (h v) -> p h v", h=h)


def phase_rwkv(C):
    nc, P, T, I = C.nc, C.P, C.T, C.I
    mm = ALU.mult
    with ExitStack() as es:
        W1 = mk(P, [128, 8, A_COLS], BF16, "W1", es)
        W2 = mk(P, [128, 8, A_COLS], BF16, "W2", es)
        with ExitStack() as es2:
            mub = mk(P, [128, A_COLS], F32, "mub", es2)
            omu = mk(P, [128, A_COLS], F32, "omu", es2)
            stg = [mk(P, [128, A_COLS], F32, "wstg", es2) for _ in range(2)]
            bcast_load(C, mub[:], I["a_mu"], 128, mub.k)
            P.op("dve", lambda e: e.tensor_scalar(omu[:], mub[:], -1.0, 1.0, ALU.mult, ALU.add), r=[mub.k], w=[omu.k])
            for c in range(8):
                s_ = stg[c % 2]
                P.dma("sp", s_[:], I["w_in_even"][c * 128:(c + 1) * 128, 0:A_COLS], w=[s_.k])
                P.op("dve", lambda e: e.tensor_tensor(W1[:, c, :], s_[:], omu[:], mm), r=[s_.k, omu.k], w=[W1.k])
                P.op("pool", lambda e: e.tensor_tensor(W2[:, c, :], s_[:], mub[:], mm), r=[s_.k, mub.k], w=[W2.k])
            P.barrier()
        LW = mk(P, [128, 512], BF16, "LW", es)
        G2 = mk(P, [128, 512], BF16, "G2", es)
        P.dma("pool", LW[0:64, :], I["a_w2"], w=[LW.k])
        P.dma("pool", LW[64:128, :], I["a_a2"], w=[LW.k])
        P.dma("pool", G2[:], I["a_g2"], w=[G2.k])
        BV = mk(P, [64, 7, 512], F32, "BV", es)
        for n, name in enumerate(("a_w0", "a_a0", "a_kk_scale", "a_ka_scale", "a_r_k", "a_gn_g", "a_gn_b")):
            bcast_load(C, BV[:, n, :], I[name], 64, BV.k)
        w0b, a0b, kksb, kab, rkb, gngb, gnbb = [BV[:, n, :] for n in range(7)]
        tri = mk(P, [64, 3, 64], F32, "tri", es)
        ncol = mk(P, [64, 1], F32, "ncol", es)
        mMA = mk(P, [64, 8, 128], F32, "mMA", es)
        mBB = mk(P, [64, 8, 128], F32, "mBB", es)
        mNT = mk(P, [64, 8, 64], F32, "mNT", es)
        id8 = mk(P, [64, 8, 64], F32, "id8", es)
        for tl, nm in ((tri, "c_tri64"), (ncol, "c_ncol64"), (mMA, "c_mMA"), (mBB, "c_mBB"), (mNT, "c_mNT"), (id8, "c_id8")):
            P.dma("sp", tl[:], I[nm], w=[tl.k])
        id64 = C.ident[0:64, 0:64]

        def wt(name, shape=(64, 512), dt=F32):
            return mk(P, list(shape), dt, name, es)
        ATs = [mk(P, [128, 8, 513], BF16, "ATs", es) for _ in range(2)]
        TX = wt("TX", (128, 512), BF16)
        SG = wt("SG", (128, 512), BF16)
        r_, k_, v_, sg, a_, kk, be, Bi, Ki, tmp = [wt(n) for n in ("r", "k", "v", "sg", "a", "kk", "be", "Bi", "Ki", "tmp")]
        Ep, Em, Ex, Ee = [wt(n) for n in ("Ep", "Em", "Ex", "Ee")]
        s8 = [wt("s8_%d" % n, (64, 8)) for n in range(4)]
        KRs = [wt("KR", (64, 8, 128), BF16) for _ in range(2)]
        BiT = wt("BiT", (64, 8, 64), BF16)
        KiT = wt("KiT", (64, 8, 64), BF16)
        MAs = [wt("MA", (64, 8, 128), BF16) for _ in range(2)]
        BBs = [wt("BB", (64, 8, 128), BF16) for _ in range(2)]
        Xb = [wt("X%d" % n, (64, 8, 64), BF16) for n in range(2)]
        XTb = [wt("XT%d" % n, (64, 8, 64), BF16) for n in range(2)]
        Qbs = [[wt("Q%d" % n, (64, 8, 64), BF16) for n in range(2)] for _ in range(2)]
        Xs = wt("Xs", (64, 8, 64), BF16)
        nU = wt("nU", (64, 8, 64), BF16)
        Y = wt("Y")
        tmpb = wt("tmpb")
        Hs = [wt("H%d" % n, (64, 8, 64)) for n in range(2)]
        Hbs = [wt("Hb%d" % n, (64, 8, 64), BF16) for n in range(2)]
        vbs = [wt("vb", (64, 512), BF16) for _ in range(2)]
        Ke16s = [wt("Ke16", (64, 512), BF16) for _ in range(2)]
        Be16s = [wt("Be16", (64, 512), BF16) for _ in range(2)]
        PCs = [wt("PC", (64, 8)) for _ in range(2)]
        gs_ = [wt("g", (64, 512)) for _ in range(2)]
        bonuss = [wt("bonus", (64, 512)) for _ in range(2)]
        P.op("pool", lambda e: e.memset(Hbs[0][:], 0.0), w=[Hbs[0].k])
        yst = [mk(P, [128, 4, 512], BF16, "yst", es) for _ in range(2)]
        P.op("dve", lambda e: e.memset(Hs[0][:], 0.0), w=[Hs[0].k])

        def psb():
            t, k = C.bank()
            return TL(t, k)

        def v3(tl_or_ap, h=8):
            return hv(tl_or_ap, h)

        def chunk(s, ci):
          at = ATs[s % 2]
          ys = yst[s % 2]
          cast_some(C, 0, 1)
          if ci == 0:
            rd = [C.XT0_tok[s]] + ([C.XT0_tok[s - 1]] if s > 0 else [C.XT0_z])
            P.dma("sp", at[:], XTv(C.XT0)[:, :, s * 512:s * 512 + 513], r=rd, w=[at.k])
            for which in range(2):
                pb = psb()
                c0 = 1536 + which * 128
                for c in range(8):
                    P.op("pe", lambda e: e.matmul(pb[:, :], lhsT=W1[:, c, c0:c0 + 128], rhs=at[:, c, 1:513], start=(c == 0), stop=False),
                         r=[W1.k, at.k], w=[pb.k], inc=False)
                for c in range(8):
                    P.op("pe", lambda e: e.matmul(pb[:, :], lhsT=W2[:, c, c0:c0 + 128], rhs=at[:, c, 0:512], start=False, stop=(c == 7)),
                         r=[W2.k, at.k], w=[pb.k], inc=(c == 7))
                if which == 0:
                    P.op("act", lambda e: e.activation(out=TX[0:64, :], in_=pb[0:64, :], func=AF.Tanh), r=[pb.k], w=[TX.k])
                    P.op("act", lambda e: e.copy(TX[64:128, :], pb[64:128, :]), r=[pb.k], w=[TX.k])
                else:
                    P.op("act", lambda e: e.activation(out=SG[:, :], in_=pb[:, :], func=AF.Sigmoid), r=[pb.k], w=[SG.k])
          if True:
            if True:
                g = s * 8 + ci
                t0 = ci * 64
                KR, MA, BB, Qb = KRs[g % 2], MAs[g % 2], BBs[g % 2], Qbs[g % 2]
                vb, Ke16, Be16, PC, g_, bonus = vbs[g % 2], Ke16s[g % 2], Be16s[g % 2], PCs[g % 2], gs_[g % 2], bonuss[g % 2]
                pr, pk, pv = psb(), psb(), psb()
                for pb, c0 in ((pr, 0), (pk, 512), (pv, 1024)):
                    for c in range(8):
                        P.op("pe", lambda e: e.matmul(pb[0:64, :], lhsT=at[:, c, 1 + t0:1 + t0 + 64], rhs=W1[:, c, c0:c0 + 512], start=(c == 0), stop=False),
                             r=[W1.k, at.k], w=[pb.k], inc=False)
                    for c in range(8):
                        P.op("pe", lambda e: e.matmul(pb[0:64, :], lhsT=at[:, c, t0:t0 + 64], rhs=W2[:, c, c0:c0 + 512], start=False, stop=(c == 7)),
                             r=[W2.k, at.k], w=[pb.k], inc=(c == 7))
                yield "F"
                pz, pza, pg = psb(), psb(), psb()
                P.op("pe", lambda e: e.matmul(pz[0:64, :], lhsT=TX[0:64, t0:t0 + 64], rhs=LW[0:64, :], start=True, stop=True), r=[TX.k, LW.k], w=[pz.k])
                P.op("pe", lambda e: e.matmul(pza[0:64, :], lhsT=TX[64:128, t0:t0 + 64], rhs=LW[64:128, :], start=True, stop=True), r=[TX.k, LW.k], w=[pza.k])
                P.op("pe", lambda e: e.matmul(pg[0:64, :], lhsT=SG[:, t0:t0 + 64], rhs=G2[:, :], start=True, stop=True), r=[SG.k, G2.k], w=[pg.k])
                P.op("act", lambda e: e.copy(r_[:], pr[0:64, :]), r=[pr.k], w=[r_.k])
                P.op("act", lambda e: e.copy(v_[:], pv[0:64, :]), r=[pv.k], w=[v_.k])
                P.op("act", lambda e: e.copy(vb[:], pv[0:64, :]), r=[pv.k], w=[vb.k])
                P.op("act", lambda e: e.copy(g_[:], pg[0:64, :]), r=[pg.k], w=[g_.k])
                P.op("dve", lambda e: e.tensor_copy(k_[:], pk[0:64, :]), r=[pk.k], w=[k_.k])
                P.op("dve", lambda e: e.tensor_tensor(sg[:], pz[0:64, :], w0b, ALU.add), r=[pz.k, BV.k], w=[sg.k])
                P.op("act", lambda e: e.activation(out=sg[:], in_=sg[:], func=AF.Sigmoid), r=[sg.k], w=[sg.k])
                P.op("dve", lambda e: e.tensor_tensor(a_[:], pza[0:64, :], a0b, ALU.add), r=[pza.k, BV.k], w=[a_.k])
                P.op("act", lambda e: e.activation(out=a_[:], in_=a_[:], func=AF.Sigmoid), r=[a_.k], w=[a_.k])
                yield "F"
                P.op("pool", lambda e: e.tensor_tensor(kk[:], k_[:], kksb, mm), r=[k_.k, BV.k], w=[kk.k])
                P.op("pool", lambda e: e.tensor_tensor(tmp[:], kk[:], kk[:], mm), r=[kk.k], w=[tmp.k])
                P.op("dve", lambda e: e.tensor_reduce(s8[0][:], v3(tmp[:]), AX.X, ALU.add), r=[tmp.k], w=[s8[0].k])
                P.op("dve", lambda e: e.tensor_scalar(s8[0][:], s8[0][:], 1e-24, None, ALU.max), r=[s8[0].k], w=[s8[0].k])
                P.op("act", lambda e: e.activation(out=s8[0][:], in_=s8[0][:], func=AF.Sqrt), r=[s8[0].k], w=[s8[0].k])
                P.op("dve", lambda e: e.reciprocal(s8[0][:], s8[0][:]), r=[s8[0].k], w=[s8[0].k])
                P.op("dve", lambda e: e.tensor_tensor(v3(kk[:]), v3(kk[:]), s8[0][:, :].unsqueeze(2).to_broadcast([64, 8, 64]), mm),
                     r=[kk.k, s8[0].k], w=[kk.k])
                P.op("pool", lambda e: e.tensor_tensor(be[:], kk[:], a_[:], mm), r=[kk.k, a_.k], w=[be.k])
                P.op("dve", lambda e: e.scalar_tensor_tensor(tmp[:], a_[:], -1.0, kab, ALU.add, mm), r=[a_.k, BV.k], w=[tmp.k])
                P.op("dve", lambda e: e.scalar_tensor_tensor(k_[:], tmp[:], 1.0, k_[:], ALU.add, mm), r=[tmp.k, k_.k], w=[k_.k])
                P.op("pool", lambda e: e.tensor_tensor(tmp[:], r_[:], k_[:], mm), r=[r_.k, k_.k], w=[tmp.k])
                P.op("pool", lambda e: e.tensor_tensor(tmp[:], tmp[:], rkb, mm), r=[tmp.k, BV.k], w=[tmp.k])
                P.op("dve", lambda e: e.tensor_reduce(s8[1][:], v3(tmp[:]), AX.X, ALU.add), r=[tmp.k], w=[s8[1].k])
                P.op("dve", lambda e: e.tensor_tensor(v3(bonus[:]), v3(v_[:]), s8[1][:, :].unsqueeze(2).to_broadcast([64, 8, 64]), mm),
                     r=[v_.k, s8[1].k], w=[bonus.k])
                yield "F"
                pcl, pcx, pca, ppc = psb(), psb(), psb(), psb()
                for pb, n in ((pcl, 0), (pcx, 1), (pca, 2)):
                    P.op("pe", lambda e: e.matmul(pb[0:64, :], lhsT=tri[:, n, :], rhs=sg[:], start=True, stop=True), r=[tri.k, sg.k], w=[pb.k])
                for h in range(8):
                    P.op("pe", lambda e: e.matmul(ppc[0:64, h:h + 1], lhsT=sg[:, h * 64:(h + 1) * 64], rhs=ncol[:], start=True, stop=True),
                         r=[sg.k, ncol.k], w=[ppc.k], inc=(h == 7))
                P.op("act", lambda e: e.activation(out=Ep[:], in_=pcl[0:64, :], func=AF.Exp), r=[pcl.k], w=[Ep.k])
                P.op("act", lambda e: e.activation(out=Em[:], in_=pcl[0:64, :], func=AF.Exp, scale=-1.0), r=[pcl.k], w=[Em.k])
                P.op("act", lambda e: e.activation(out=Ex[:], in_=pcx[0:64, :], func=AF.Exp), r=[pcx.k], w=[Ex.k])
                P.op("act", lambda e: e.activation(out=Ee[:], in_=pca[0:64, :], func=AF.Exp), r=[pca.k], w=[Ee.k])
                P.op("act", lambda e: e.activation(out=PC[:], in_=ppc[0:64, 0:8], func=AF.Exp), r=[ppc.k], w=[PC.k])
                P.op("dve", lambda e: e.tensor_tensor(r_[:], r_[:], Ep[:], mm), r=[r_.k, Ep.k], w=[r_.k])
                P.op("pool", lambda e: e.tensor_tensor(kk[:], kk[:], Ex[:], mm), r=[kk.k, Ex.k], w=[kk.k])
                P.op("dve", lambda e: e.tensor_tensor(Bi[:], be[:], Em[:], mm), r=[be.k, Em.k], w=[Bi.k])
                P.op("pool", lambda e: e.tensor_tensor(Ki[:], k_[:], Em[:], mm), r=[k_.k, Em.k], w=[Ki.k])
                P.op("dve", lambda e: e.tensor_tensor(Ke16[:], k_[:], Ee[:], mm), r=[k_.k, Ee.k], w=[Ke16.k])
                P.op("pool", lambda e: e.tensor_tensor(Be16[:], be[:], Ee[:], mm), r=[be.k, Ee.k], w=[Be16.k])
                yield "F"
                for src, dst, off, en in ((kk, KR, 0, "act"), (r_, KR, 64, "dve"), (Bi, BiT, 0, "act"), (Ki, KiT, 0, "dve")):
                    pb = psb()
                    for h in range(8):
                        P.op("pe", lambda e: e.transpose(pb[0:64, h * 64:(h + 1) * 64], src[:, h * 64:(h + 1) * 64], id64),
                             r=[src.k, C.ident_tok], w=[pb.k], inc=(h == 7))
                    d_ = dst[:, :, off:off + 64]
                    s_ = v3(pb[0:64, :])
                    if en == "act":
                        P.op("act", lambda e: e.copy(d_, s_), r=[pb.k], w=[dst.k])
                    else:
                        P.op("dve", lambda e: e.tensor_copy(d_, s_), r=[pb.k], w=[dst.k])
                yield "F"
                pma = [psb(), psb()]
                pbb = [psb(), psb()]
                pnt = psb()
                for h in range(8):
                    hb, hh = h // 4, h % 4
                    P.op("pe", lambda e: e.matmul(pma[hb][0:64, hh * 128:(hh + 1) * 128], lhsT=BiT[:, h, :], rhs=KR[:, h, :], start=True, stop=True),
                         r=[BiT.k, KR.k], w=[pma[hb].k], inc=(hh == 3))
                for h in range(8):
                    hb, hh = h // 4, h % 4
                    P.op("pe", lambda e: e.matmul(pbb[hb][0:64, hh * 128:(hh + 1) * 128], lhsT=KiT[:, h, :], rhs=KR[:, h, :], start=True, stop=True),
                         r=[KiT.k, KR.k], w=[pbb[hb].k], inc=(hh == 3))
                for h in range(8):
                    P.op("pe", lambda e: e.matmul(pnt[0:64, h * 64:(h + 1) * 64], lhsT=KR[:, h, 0:64], rhs=BiT[:, h, :], start=True, stop=True),
                         r=[BiT.k, KR.k], w=[pnt.k], inc=(h == 7))
                for hb in range(2):
                    P.op("dve", lambda e: e.tensor_tensor(MA[:, hb * 4:(hb + 1) * 4, :], hv(pma[hb][0:64, :], 4), mMA[:, hb * 4:(hb + 1) * 4, :], mm),
                         r=[pma[hb].k, mMA.k], w=[MA.k])
                    P.op("dve", lambda e: e.tensor_tensor(BB[:, hb * 4:(hb + 1) * 4, :], hv(pbb[hb][0:64, :], 4), mBB[:, hb * 4:(hb + 1) * 4, :], mm),
                         r=[pbb[hb].k, mBB.k], w=[BB.k])
                X, XT, Q = Xb[0], XTb[0], Qb[0]
                P.op("dve", lambda e: e.tensor_tensor(XT[:], v3(pnt[0:64, :]), mNT[:], mm), r=[pnt.k, mNT.k], w=[XT.k])
                P.op("pool", lambda e: e.tensor_copy(X[:], MA[:, :, 0:64]), r=[MA.k], w=[X.k])
                P.op("pool", lambda e: e.tensor_tensor(Q[:], MA[:, :, 0:64], id8[:], ALU.add), r=[MA.k, id8.k], w=[Q.k])
                for lvl in range(5):
                    Xn, XTn, Qn = Xb[(lvl + 1) % 2], XTb[(lvl + 1) % 2], Qb[(lvl + 1) % 2]
                    pxt = psb()
                    for h in range(8):
                        P.op("pe", lambda e: e.matmul(pxt[0:64, h * 64:(h + 1) * 64], lhsT=X[:, h, :], rhs=XT[:, h, :], start=True, stop=True),
                             r=[X.k, XT.k], w=[pxt.k], inc=(h == 7))
                    if lvl < 4:
                        px = psb()
                        for h in range(8):
                            P.op("pe", lambda e: e.matmul(px[0:64, h * 64:(h + 1) * 64], lhsT=XT[:, h, :], rhs=X[:, h, :], start=True, stop=True),
                                 r=[X.k, XT.k], w=[px.k], inc=(h == 7))
                    P.op("act", lambda e: e.copy(XTn[:], v3(pxt[0:64, :])), r=[pxt.k], w=[XTn.k])
                    if lvl < 4:
                        P.op("dve", lambda e: e.tensor_copy(Xn[:], v3(px[0:64, :])), r=[px.k], w=[Xn.k])
                    pq = psb()
                    for h in range(8):
                        P.op("pe", lambda e: e.matmul(pq[0:64, h * 64:(h + 1) * 64], lhsT=XTn[:, h, :], rhs=Q[:, h, :], start=True, stop=True),
                             r=[XTn.k, Q.k], w=[pq.k], inc=(h == 7))
                    P.op("dve", lambda e: e.tensor_tensor(Qn[:], Q[:], v3(pq[0:64, :]), ALU.add), r=[Q.k, pq.k], w=[Qn.k])
                    X, XT, Q = Xn, XTn, Qn
                    yield "F"
                yield "END_FRONT"
                H, Hn = Hs[g % 2], Hs[(g + 1) % 2]
                Hb, Hbn = Hbs[g % 2], Hbs[(g + 1) % 2]
                pxs = psb()
                for h in range(8):
                    P.op("pe", lambda e: e.matmul(pxs[0:64, h * 64:(h + 1) * 64], lhsT=KR[:, h, 0:64], rhs=Hb[:, h, :], start=True, stop=False),
                         r=[KR.k, Hb.k], w=[pxs.k], inc=False)
                    P.op("pe", lambda e: e.matmul(pxs[0:64, h * 64:(h + 1) * 64], lhsT=BB[:, h, 0:64], rhs=vb[:, h * 64:(h + 1) * 64], start=False, stop=True),
                         r=[BB.k, vb.k], w=[pxs.k], inc=(h == 7))
                P.op("act", lambda e: e.copy(Xs[:], v3(pxs[0:64, :])), r=[pxs.k], w=[Xs.k])
                yield "B"
                pu = psb()
                for h in range(8):
                    P.op("pe", lambda e: e.matmul(pu[0:64, h * 64:(h + 1) * 64], lhsT=Q[:, h, :], rhs=Xs[:, h, :], start=True, stop=True),
                         r=[Q.k, Xs.k], w=[pu.k], inc=(h == 7))
                P.op("act", lambda e: e.mul(nU[:], v3(pu[0:64, :]), -1.0), r=[pu.k], w=[nU.k])
                yield "B"
                py, ph = psb(), psb()
                for h in range(8):
                    sl = slice(h * 64, (h + 1) * 64)
                    P.op("pe", lambda e: e.matmul(py[0:64, sl], lhsT=KR[:, h, 64:128], rhs=Hb[:, h, :], start=True, stop=False), r=[KR.k, Hb.k], w=[py.k], inc=False)
                    P.op("pe", lambda e: e.matmul(py[0:64, sl], lhsT=BB[:, h, 64:128], rhs=vb[:, sl], start=False, stop=False), r=[BB.k, vb.k], w=[py.k], inc=False)
                    P.op("pe", lambda e: e.matmul(py[0:64, sl], lhsT=MA[:, h, 64:128], rhs=nU[:, h, :], start=False, stop=True), r=[MA.k, nU.k], w=[py.k], inc=(h == 7))
                for h in range(8):
                    sl = slice(h * 64, (h + 1) * 64)
                    P.op("pe", lambda e: e.matmul(ph[0:64, sl], lhsT=Ke16[:, sl], rhs=vb[:, sl], start=True, stop=False), r=[Ke16.k, vb.k], w=[ph.k], inc=False)
                    P.op("pe", lambda e: e.matmul(ph[0:64, sl], lhsT=Be16[:, sl], rhs=nU[:, h, :], start=False, stop=True), r=[Be16.k, nU.k], w=[ph.k], inc=(h == 7))
                P.op("pool", lambda e: e.tensor_tensor(Hn[:], H[:], PC[:, :].unsqueeze(2).to_broadcast([64, 8, 64]), mm), r=[H.k, PC.k], w=[Hn.k])
                P.op("dve", lambda e: e.tensor_tensor(Hn[:], Hn[:], v3(ph[0:64, :]), ALU.add), r=[Hn.k, ph.k], w=[Hn.k])
                P.op("act", lambda e: e.copy(Hbn[:], Hn[:]), r=[Hn.k], w=[Hbn.k])
                yield "B"
                P.op("act", lambda e: e.copy(Y[:], py[0:64, :]), r=[py.k], w=[Y.k])
                P.op("dve", lambda e: e.tensor_reduce(s8[2][:], v3(Y[:]), AX.X, ALU.add), r=[Y.k], w=[s8[2].k])
                P.op("dve", lambda e: e.tensor_scalar(s8[2][:], s8[2][:], 1.0 / 64, None, mm), r=[s8[2].k], w=[s8[2].k])
                P.op("dve", lambda e: e.tensor_tensor(v3(Y[:]), v3(Y[:]), s8[2][:, :].unsqueeze(2).to_broadcast([64, 8, 64]), ALU.subtract),
                     r=[Y.k, s8[2].k], w=[Y.k])
                yield "B"
                P.op("pool", lambda e: e.tensor_tensor(tmpb[:], Y[:], Y[:], mm), r=[Y.k], w=[tmpb.k])
                P.op("dve", lambda e: e.tensor_reduce(s8[3][:], v3(tmpb[:]), AX.X, ALU.add), r=[tmpb.k], w=[s8[3].k])
                P.op("act", lambda e: e.activation(out=s8[3][:], in_=s8[3][:], func=AF.Sqrt, bias=64e-5, scale=1.0 / 64), r=[s8[3].k], w=[s8[3].k])
                P.op("dve", lambda e: e.reciprocal(s8[3][:], s8[3][:]), r=[s8[3].k], w=[s8[3].k])
                P.op("dve", lambda e: e.tensor_tensor(v3(Y[:]), v3(Y[:]), s8[3][:, :].unsqueeze(2).to_broadcast([64, 8, 64]), mm),
                     r=[Y.k, s8[3].k], w=[Y.k])
                P.op("pool", lambda e: e.tensor_tensor(Y[:], Y[:], gngb, mm), r=[Y.k, BV.k], w=[Y.k])
                P.op("pool", lambda e: e.tensor_tensor(Y[:], Y[:], gnbb, ALU.add), r=[Y.k, BV.k], w=[Y.k])
                P.op("dve", lambda e: e.tensor_tensor(Y[:], Y[:], bonus[:], ALU.add), r=[Y.k, bonus.k], w=[Y.k])
                P.op("dve", lambda e: e.tensor_tensor(Y[:], Y[:], g_[:], mm), r=[Y.k, g_.k], w=[Y.k])
                if "dbg_ya" in C.dbg:
                    P.dma("sp", C.dbg["dbg_ya"][g * 64:(g + 1) * 64, :], Y[:], r=[Y.k])
                yield "B"
                pb = psb()
                for q in range(4):
                    P.op("pe", lambda e: e.transpose(pb[:, q * 64:(q + 1) * 64], Y[:, q * 128:(q + 1) * 128], id64), r=[Y.k, C.ident_tok], w=[pb.k], inc=(q == 3))
                P.op("act", lambda e: e.copy(ys[:, :, t0:t0 + 64], hv(pb[:, 0:256], 4)), r=[pb.k], w=[ys.k])
                if ci == 7:
                    P.dma("sp", XTv(C.YT)[:, 0:4, s * 512:(s + 1) * 512], ys[:], r=[ys.k], w=[C.YT_tok[0][s]])

        pipeline2([(lambda s=s, ci=ci: chunk(s, ci)) for s in range(C.NS) for ci in range(8)], interleave=RWKV_INTERLEAVE)
        P.barrier()


def pipeline2(makers, interleave=True):
    if not interleave:
        for mk_ in makers:
            for _ in mk_():
                pass
        return
    prevB = None
    for mk_ in makers:
        g = mk_()
        while True:
            r = next(g)
            if prevB is not None:
                try:
                    next(prevB)
                except StopIteration:
                    prevB = None
            if r == "END_FRONT":
                break
        if prevB is not None:
            for _ in prevB:
                pass
        prevB = g
    if prevB is not None:
        for _ in prevB:
            pass


def psb(C):
    t, k = C.bank()
    return TL(t, k)


def phase_gla(C):
    nc, P, T, I = C.nc, C.P, C.T, C.I
    mm = ALU.mult
    with ExitStack() as es:
        WB = mk(P, [128, 8, B_COLS], BF16, "WB", es)
        for c in range(8):
            P.dma("pool", WB[:, c, :], I["w_in_even"][c * 128:(c + 1) * 128, A_COLS:EVEN_COLS], w=[WB.k])
        GW2 = mk(P, [16, 256], BF16, "GW2", es)
        P.dma("pool", GW2[:], I["b_gate_w2"], w=[GW2.k])
        gbb = mk(P, [128, 256], F32, "gbb", es)
        ngb = mk(P, [128, 512], F32, "ngb", es)
        bcast_load(C, gbb[:], I["b_gate_b"], 128, gbb.k)
        bcast_load(C, ngb[:], I["b_norm_g"], 128, ngb.k)
        tri = mk(P, [128, 2, 128], F32, "tri128", es)
        ncol = mk(P, [128, 1], F32, "ncol128", es)
        iu = mk(P, [128, 4, 128], F32, "iu128", es)
        for tl, nm in ((tri, "c_tri128"), (ncol, "c_ncol128"), (iu, "c_iu128")):
            P.dma("sp", tl[:], I[nm], w=[tl.k])
        id64 = C.ident[0:64, 0:64]

        def wt(name, shape, dt=F32):
            return mk(P, list(shape), dt, name, es)
        ATs = [wt("ATg", (128, 8, 512), BF16) for _ in range(2)]
        AL = wt("AL", (16, 512), BF16)
        l_ = wt("l", (128, 256))
        Eq, Ei, Ee = wt("Eq", (128, 256)), wt("Ei", (128, 256)), wt("Ee", (128, 256))
        PCg = wt("PCg", (64, 4))
        qd, ki, ke = wt("qd", (128, 256)), wt("ki", (128, 256)), wt("ke", (128, 256))
        v_ = wt("vg", (128, 512))
        qdT, kiT = wt("qdT", (64, 4, 128)), wt("kiT", (64, 4, 128))
        attT = wt("attT", (128, 4, 128))
        Ss = [wt("S%d" % n, (64, 4, 128)) for n in range(2)]
        o_ = wt("o", (128, 512))
        sq = wt("sqg", (128, 512))
        sl_ = wt("silu", (128, 512))
        m4 = wt("m4", (128, 4))
        yst = [wt("ystg", (128, 4, 512), BF16) for _ in range(2)]
        P.op("dve", lambda e: e.memset(Ss[0][:], 0.0), w=[Ss[0].k])
        for s in range(C.NS):
            at = ATs[s % 2]
            P.dma("sp", at[:], XTv(C.XT0)[:, :, 1 + s * 512:1 + (s + 1) * 512], r=[C.XT0_tok[s]], w=[at.k])
            pb = psb(C)
            for c in range(8):
                P.op("pe", lambda e: e.matmul(pb[0:16, :], lhsT=WB[:, c, 1536:1552], rhs=at[:, c, :], start=(c == 0), stop=(c == 7)),
                     r=[WB.k, at.k], w=[pb.k], inc=(c == 7))
            P.op("act", lambda e: e.copy(AL[:], pb[0:16, :]), r=[pb.k], w=[AL.k])
            ys = yst[s % 2]
            for ci in range(4):
                g = s * 4 + ci
                t0 = ci * 128
                pqk, pv, pg = psb(C), psb(C), psb(C)
                for pb, c0 in ((pqk, 0), (pv, 512), (pg, 1024)):
                    for c in range(8):
                        P.op("pe", lambda e: e.matmul(pb[:, :], lhsT=at[:, c, t0:t0 + 128], rhs=WB[:, c, c0:c0 + 512], start=(c == 0), stop=(c == 7)),
                             r=[WB.k, at.k], w=[pb.k], inc=(c == 7))
                pla = psb(C)
                P.op("pe", lambda e: e.matmul(pla[:, 0:256], lhsT=AL[:, t0:t0 + 128], rhs=GW2[:], start=True, stop=True), r=[AL.k, GW2.k], w=[pla.k])
                P.op("dve", lambda e: e.tensor_tensor(l_[:], pla[:, 0:256], gbb[:], ALU.add), r=[pla.k, gbb.k], w=[l_.k])
                P.op("act", lambda e: e.activation(out=l_[:], in_=l_[:], func=AF.Exp, scale=-1.0), r=[l_.k], w=[l_.k])
                P.op("act", lambda e: e.activation(out=l_[:], in_=l_[:], func=AF.Ln, bias=1.0), r=[l_.k], w=[l_.k])
                pbc, pba, ppc = psb(C), psb(C), psb(C)
                P.op("pe", lambda e: e.matmul(pbc[:, 0:256], lhsT=tri[:, 0, :], rhs=l_[:], start=True, stop=True), r=[tri.k, l_.k], w=[pbc.k])
                P.op("pe", lambda e: e.matmul(pba[:, 0:256], lhsT=tri[:, 1, :], rhs=l_[:], start=True, stop=True), r=[tri.k, l_.k], w=[pba.k])
                for h in range(4):
                    P.op("pe", lambda e: e.matmul(ppc[0:64, h:h + 1], lhsT=l_[:, h * 64:(h + 1) * 64], rhs=ncol[:], start=True, stop=True),
                         r=[l_.k, ncol.k], w=[ppc.k], inc=(h == 3))
                P.op("act", lambda e: e.activation(out=Eq[:], in_=pbc[:, 0:256], func=AF.Exp), r=[pbc.k], w=[Eq.k])
                P.op("act", lambda e: e.activation(out=Ei[:], in_=pbc[:, 0:256], func=AF.Exp, scale=-1.0), r=[pbc.k], w=[Ei.k])
                P.op("act", lambda e: e.activation(out=Ee[:], in_=pba[:, 0:256], func=AF.Exp), r=[pba.k], w=[Ee.k])
                P.op("act", lambda e: e.activation(out=PCg[:], in_=ppc[0:64, 0:4], func=AF.Exp), r=[ppc.k], w=[PCg.k])
                P.op("dve", lambda e: e.scalar_tensor_tensor(qd[:], pqk[:, 0:256], 0.125, Eq[:], mm, mm), r=[pqk.k, Eq.k], w=[qd.k])
                P.op("dve", lambda e: e.tensor_tensor(ki[:], pqk[:, 256:512], Ei[:], mm), r=[pqk.k, Ei.k], w=[ki.k])
                P.op("dve", lambda e: e.tensor_tensor(ke[:], pqk[:, 256:512], Ee[:], mm), r=[pqk.k, Ee.k], w=[ke.k])
                P.op("act", lambda e: e.copy(v_[:], pv[:, :]), r=[pv.k], w=[v_.k])
                P.op("act", lambda e: e.activation(out=sl_[:], in_=pg[:, :], func=AF.Silu), r=[pg.k], w=[sl_.k])
                for src, dst, en in ((qd, qdT, "act"), (ki, kiT, "dve")):
                    pb = psb(C)
                    for h in range(4):
                        P.op("pe", lambda e: e.transpose(pb[0:64, h * 128:(h + 1) * 128], src[:, h * 64:(h + 1) * 64], C.ident[:]),
                             r=[src.k, C.ident_tok], w=[pb.k], inc=(h == 3))
                    if en == "act":
                        P.op("act", lambda e: e.copy(dst[:], hv(pb[0:64, :], 4)), r=[pb.k], w=[dst.k])
                    else:
                        P.op("dve", lambda e: e.tensor_copy(dst[:], hv(pb[0:64, :], 4)), r=[pb.k], w=[dst.k])
                patt = psb(C)
                for h in range(4):
                    P.op("pe", lambda e: e.matmul(patt[:, h * 128:(h + 1) * 128], lhsT=kiT[:, h, :], rhs=qdT[:, h, :], start=True, stop=True),
                         r=[kiT.k, qdT.k], w=[patt.k], inc=(h == 3))
                P.op("dve", lambda e: e.tensor_tensor(attT[:], hv(patt[:, :], 4), iu[:], mm), r=[patt.k, iu.k], w=[attT.k])
                S, Sn = Ss[g % 2], Ss[(g + 1) % 2]
                po, pS = psb(C), psb(C)
                for h in range(4):
                    sl = slice(h * 128, (h + 1) * 128)
                    P.op("pe", lambda e: e.matmul(po[:, sl], lhsT=attT[:, h, :], rhs=v_[:, sl], start=True, stop=False), r=[attT.k, v_.k], w=[po.k], inc=False)
                    P.op("pe", lambda e: e.matmul(po[:, sl], lhsT=qdT[:, h, :], rhs=S[:, h, :], start=False, stop=True), r=[qdT.k, S.k], w=[po.k], inc=(h == 3))
                for h in range(4):
                    sl = slice(h * 128, (h + 1) * 128)
                    P.op("pe", lambda e: e.matmul(pS[0:64, sl], lhsT=ke[:, h * 64:(h + 1) * 64], rhs=v_[:, sl], start=True, stop=True), r=[ke.k, v_.k], w=[pS.k], inc=(h == 3))
                P.op("pool", lambda e: e.tensor_tensor(Sn[:], S[:], PCg[:, :].unsqueeze(2).to_broadcast([64, 4, 128]), mm), r=[S.k, PCg.k], w=[Sn.k])
                P.op("dve", lambda e: e.tensor_tensor(Sn[:], Sn[:], hv(pS[0:64, :], 4), ALU.add), r=[Sn.k, pS.k], w=[Sn.k])
                P.op("act", lambda e: e.copy(o_[:], po[:, :]), r=[po.k], w=[o_.k])
                P.op("pool", lambda e: e.tensor_tensor(sq[:], o_[:], o_[:], mm), r=[o_.k], w=[sq.k])
                P.op("dve", lambda e: e.tensor_reduce(m4[:], hv(sq[:], 4), AX.X, ALU.add), r=[sq.k], w=[m4.k])
                P.op("act", lambda e: e.activation(out=m4[:], in_=m4[:], func=AF.Sqrt, bias=1e-5, scale=1.0 / 128), r=[m4.k], w=[m4.k])
                P.op("dve", lambda e: e.reciprocal(m4[:], m4[:]), r=[m4.k], w=[m4.k])
                P.op("dve", lambda e: e.tensor_tensor(hv(o_[:], 4), hv(o_[:], 4), m4[:, :].unsqueeze(2).to_broadcast([128, 4, 128]), mm), r=[o_.k, m4.k], w=[o_.k])
                P.op("pool", lambda e: e.tensor_tensor(o_[:], o_[:], ngb[:], mm), r=[o_.k, ngb.k], w=[o_.k])
                P.op("dve", lambda e: e.tensor_tensor(o_[:], o_[:], sl_[:], mm), r=[o_.k, sl_.k], w=[o_.k])
                if "dbg_yb" in C.dbg:
                    P.dma("sp", C.dbg["dbg_yb"][g * 128:(g + 1) * 128, :], o_[:], r=[o_.k])
                pb = psb(C)
                for q in range(4):
                    P.op("pe", lambda e: e.transpose(pb[:, q * 128:(q + 1) * 128], o_[:, q * 128:(q + 1) * 128], C.ident[:]), r=[o_.k, C.ident_tok], w=[pb.k], inc=(q == 3))
                P.op("act", lambda e: e.copy(ys[:, :, t0:t0 + 128], hv(pb[:, :], 4)), r=[pb.k], w=[ys.k])
            P.dma("sp", XTv(C.YT)[:, 4:8, s * 512:(s + 1) * 512], ys[:], r=[ys.k], w=[C.YT_tok[1][s]])
        P.barrier()


def ln_inplace(C, xt, gb, bb, st, junk):
    P = C.P
    P.op("dve", lambda e: e.tensor_reduce(st[:, 0:1], xt[:], AX.X, ALU.add), r=[xt.k], w=[st.k])
    P.op("dve", lambda e: e.tensor_scalar(st[:, 0:1], st[:, 0:1], 1.0 / D, None, ALU.mult), r=[st.k], w=[st.k])
    P.op("dve", lambda e: e.tensor_scalar(xt[:], xt[:], st[:, 0:1], None, ALU.subtract), r=[xt.k, st.k], w=[xt.k])
    P.op("act", lambda e: e.activation(out=junk[:], in_=xt[:], func=AF.Square, accum_out=st[:, 1:2]), r=[xt.k], w=[junk.k, st.k])
    P.op("act", lambda e: e.activation(out=st[:, 1:2], in_=st[:, 1:2], func=AF.Sqrt, bias=LN_EPS, scale=1.0 / D), r=[st.k], w=[st.k])
    P.op("dve", lambda e: e.reciprocal(st[:, 1:2], st[:, 1:2]), r=[st.k], w=[st.k])
    P.op("dve", lambda e: e.scalar_tensor_tensor(xt[:], xt[:], st[:, 1:2], gb[:], ALU.mult, ALU.mult), r=[xt.k, st.k, gb.k], w=[xt.k])
    P.op("pool", lambda e: e.tensor_tensor(xt[:], xt[:], bb[:], ALU.add), r=[xt.k, bb.k], w=[xt.k])


def ln_lockstep(C, xs, gb, bb, sts, junk):
    P = C.P
    n = len(xs)
    for k in range(n):
        xt, st = xs[k], sts[k]
        P.op("dve", lambda e: e.tensor_reduce(st[:, 0:1], xt[:], AX.X, ALU.add), r=[xt.k], w=[st.k])
    for k in range(n):
        xt, st = xs[k], sts[k]
        P.op("dve", lambda e: e.tensor_scalar(st[:, 0:1], st[:, 0:1], 1.0 / D, None, ALU.mult), r=[st.k], w=[st.k])
    for k in range(n):
        xt, st = xs[k], sts[k]
        P.op("dve", lambda e: e.tensor_scalar(xt[:], xt[:], st[:, 0:1], None, ALU.subtract), r=[xt.k, st.k], w=[xt.k])
        P.op("act", lambda e: e.activation(out=junk[:], in_=xt[:], func=AF.Square, accum_out=st[:, 1:2]), r=[xt.k], w=[junk.k, st.k])
    for k in range(n):
        xt, st = xs[k], sts[k]
        P.op("act", lambda e: e.activation(out=st[:, 1:2], in_=st[:, 1:2], func=AF.Sqrt, bias=LN_EPS, scale=1.0 / D), r=[st.k], w=[st.k])
    for k in range(n):
        xt, st = xs[k], sts[k]
        P.op("dve", lambda e: e.reciprocal(st[:, 1:2], st[:, 1:2]), r=[st.k], w=[st.k])
    for k in range(n):
        xt, st = xs[k], sts[k]
        P.op("dve", lambda e: e.scalar_tensor_tensor(xt[:], xt[:], st[:, 1:2], gb[:], ALU.mult, ALU.mult), r=[xt.k, st.k, gb.k], w=[xt.k])
        P.op("pool", lambda e: e.tensor_tensor(xt[:], xt[:], bb[:], ALU.add), r=[xt.k, bb.k], w=[xt.k])


def tile_to_stage(C, xt, stage, j):
    P = C.P
    for half in range(2):
        pb = psb(C)
        for c4 in range(4):
            c = half * 4 + c4
            P.op("pe", lambda e: e.transpose(pb[:, c4 * 128:(c4 + 1) * 128], xt[:, c * 128:(c + 1) * 128], C.ident[:]),
                 r=[xt.k, C.ident_tok], w=[pb.k], inc=(c4 == 3))
        dst = stage[:, half * 4:(half + 1) * 4, j * 128:(j + 1) * 128]
        src = hv(pb[:, :], 4)
        if half == 0:
            P.op("act", lambda e: e.copy(dst, src), r=[pb.k], w=[stage.k])
        else:
            P.op("dve", lambda e: e.tensor_copy(dst, src), r=[pb.k], w=[stage.k])


def phase_outproj(C, w_out, srcYT, srcYT_toks, resid, resid_toks, lng, lnb, dstH, dstH_tok, dstHT, dstHT_tok, dbgname=None):
    nc, P, T, I = C.nc, C.P, C.T, C.I
    with ExitStack() as es:
        WO = mk(P, [128, 8, D], BF16, "WO", es)
        for c in range(8):
            P.dma("pool", WO[:, c, :], w_out[c * 128:(c + 1) * 128, :], w=[WO.k])
        gb = mk(P, [128, D], F32, "lng", es)
        bb = mk(P, [128, D], F32, "lnb", es)
        bcast_load(C, gb[:], lng, 128, gb.k)
        bcast_load(C, bb[:], lnb, 128, bb.k)
        yts = [mk(P, [128, 8, 512], BF16, "yt", es) for _ in range(2)]
        xts = [mk(P, [128, D], F32, "xres", es) for _ in range(8)]
        sts = [mk(P, [128, 2], F32, "lnst", es) for _ in range(8)]
        junk = mk(P, [128, D], BF16, "junk", es)
        stg = [mk(P, [128, 8, 512], BF16, "hstg", es) for _ in range(2)]
        pend = None

        def loads(s):
            P.dma("sp", yts[s % 2][:], XTv(srcYT)[:, :, s * 512:(s + 1) * 512], r=srcYT_toks(s), w=[yts[s % 2].k])
            for j in range(4):
                i = s * 4 + j
                xt = xts[(s % 2) * 4 + j]
                P.dma("sp", xt[:], resid[i * 128:(i + 1) * 128, :], r=resid_toks(i), w=[xt.k])

        loads(0)
        for s in range(C.NS):
            yt = yts[s % 2]
            sg_ = stg[s % 2]
            X = xts[(s % 2) * 4:(s % 2) * 4 + 4]
            S = sts[(s % 2) * 4:(s % 2) * 4 + 4]
            for j in range(4):
                xt = X[j]
                for half in range(2):
                    pb = psb(C)
                    for c in range(8):
                        P.op("pe", lambda e: e.matmul(pb[:, :], lhsT=yt[:, c, j * 128:(j + 1) * 128], rhs=WO[:, c, half * 512:(half + 1) * 512], start=(c == 0), stop=(c == 7)),
                             r=[yt.k, WO.k], w=[pb.k], inc=(c == 7))
                    P.op("dve", lambda e: e.scalar_tensor_tensor(xt[:, half * 512:(half + 1) * 512], xt[:, half * 512:(half + 1) * 512], DN_ALPHA, pb[:, :], ALU.mult, ALU.add),
                         r=[xt.k, pb.k], w=[xt.k])
            if pend is not None:
                pend()
            if s + 1 < C.NS:
                loads(s + 1)
            ln_lockstep(C, X, gb, bb, S, junk)
            for j in range(4):
                i = s * 4 + j
                P.dma("sp", dstH[i * 128:(i + 1) * 128, :], X[j][:], r=[X[j].k], w=[dstH_tok[i]])

            def pend(s=s, X=X, sg_=sg_):
                for j in range(4):
                    tile_to_stage(C, X[j], sg_, j)
                P.dma("sp", XTv(dstHT)[:, :, s * 512:(s + 1) * 512], sg_[:], r=[sg_.k], w=[dstHT_tok[s]])
        pend()
        P.barrier()


def phase_moe(C, l, srcH, srcH_tok, srcHT, srcHT_tok, dstX, dstX_tok, dstXT, dstXT_tok):
    nc, P, T, I = C.nc, C.P, C.T, C.I
    mm = ALU.mult
    with ExitStack() as es:
        WD = mk(P, [128, 32, D], BF16, "WD", es)
        for c4 in range(4):
            P.dma("sp", WD[:, c4 * 8:(c4 + 1) * 8, :], C.WD16[l].rearrange("p (c d) -> p c d", c=32)[:, c4 * 8:(c4 + 1) * 8, :], r=[C.WD16_tok[l]], w=[WD.k])
        RW = mk(P, [128, 8, NE], BF16, "RW", es)
        P.dma("pool", RW[:], I["router_w"].rearrange("(c p) e -> p c e", p=128), w=[RW.k])
        rbb = mk(P, [128, NE], F32, "rbb", es)
        bcast_load(C, rbb[:], I["router_bias"], 128, rbb.k)
        SEL = mk(P, [16, 16, 128], BF16, "SEL", es)
        P.dma("pool", SEL[:], I["c_sel"], w=[SEL.k])
        gb = mk(P, [128, D], F32, "lng", es)
        bb = mk(P, [128, D], F32, "lnb", es)
        bcast_load(C, gb[:], I["ln2_g"][l:l + 1, :], 128, gb.k)
        bcast_load(C, bb[:], I["ln2_b"][l:l + 1, :], 128, bb.k)
        hts = [mk(P, [128, 8, 512], BF16, "hT", es) for _ in range(2)]
        xts = [mk(P, [128, D], F32, "hres", es) for _ in range(4)]
        sts = [mk(P, [128, 2], F32, "lnst", es) for _ in range(4)]
        junk = mk(P, [128, D], BF16, "junk", es)
        stg = [mk(P, [128, 8, 512], BF16, "xstg", es) for _ in range(2)]
        actT = mk(P, [128, 32, 512], BF16, "actT", es)
        combT = mk(P, [16, 512], BF16, "combT", es)
        WGUs = [mk(P, [128, 2, 8, DE], BF16, "WGU", es) for _ in range(3)]
        sgl = [mk(P, [128, 512], F32, "sgl", es) for _ in range(2)]
        R4 = range(4)
        s_l = [mk(P, [128, NE], F32, "rs", es) for _ in R4]
        sel_l = [mk(P, [128, NE], F32, "rsel", es) for _ in R4]
        pr_l = [mk(P, [128, 4, 6], F32, "rpr", es) for _ in R4]
        gs_l = [mk(P, [128, 4], F32, "rgs", es) for _ in R4]
        t1_l = [mk(P, [128, 4], F32, "rt1", es) for _ in R4]
        m1_l = [mk(P, [128, 2], F32, "rm1", es) for _ in R4]
        selm_l = [mk(P, [128, NE], F32, "rselm", es) for _ in R4]
        sel2_l = [mk(P, [128, NE], F32, "rsel2", es) for _ in R4]
        comb_l = [mk(P, [128, NE], F32, "rcomb", es) for _ in R4]
        nwl = [0]

        def load_w(e):
            b = nwl[0] % 3
            nwl[0] += 1
            P.dma("sp", WGUs[b][:], C.WGU16[l][e].rearrange("p (t c f) -> p t c f", t=2, c=8), r=[C.WGU16_tok[l][e]], w=[WGUs[b].k])
            return WGUs[b]

        def g4(t):
            return t[:, :].rearrange("p (g e) -> p g e", g=4)

        def load_hT(s):
            P.dma("sp", hts[s % 2][:], XTv(srcHT)[:, :, s * 512:(s + 1) * 512], r=[srcHT_tok[s]], w=[hts[s % 2].k])

        def router_front(s):
            hT = hts[s % 2]
            for j in R4:
                plg = psb(C)
                s_ = s_l[j]
                for c in range(8):
                    P.op("pe", lambda e: e.matmul(plg[:, 0:NE], lhsT=hT[:, c, j * 128:(j + 1) * 128], rhs=RW[:, c, :], start=(c == 0), stop=(c == 7)),
                         r=[hT.k, RW.k], w=[plg.k], inc=(c == 7))
                P.op("act", lambda e: e.activation(out=s_[:], in_=plg[:, 0:NE], func=AF.Sigmoid), r=[plg.k], w=[s_.k])

            def step(fn):
                for j in R4:
                    fn(s_l[j], sel_l[j], pr_l[j], gs_l[j], t1_l[j], m1_l[j], selm_l[j], sel2_l[j], comb_l[j])
            step(lambda s_, sel, pr, gs, t1, m1, selm, sel2, comb: P.op("dve", lambda e: e.tensor_tensor(sel[:], s_[:], rbb[:], ALU.add), r=[s_.k, rbb.k], w=[sel.k]))
            step(lambda s_, sel, pr, gs, t1, m1, selm, sel2, comb: P.op("dve", lambda e: e.tensor_tensor(pr[:, :, 0:3], g4(sel)[:, :, 0:3], g4(sel)[:, :, 1:4], ALU.add), r=[sel.k], w=[pr.k]))
            step(lambda s_, sel, pr, gs, t1, m1, selm, sel2, comb: P.op("dve", lambda e: e.tensor_tensor(pr[:, :, 3:5], g4(sel)[:, :, 0:2], g4(sel)[:, :, 2:4], ALU.add), r=[sel.k], w=[pr.k]))
            step(lambda s_, sel, pr, gs, t1, m1, selm, sel2, comb: P.op("dve", lambda e: e.tensor_tensor(pr[:, :, 5:6], g4(sel)[:, :, 0:1], g4(sel)[:, :, 3:4], ALU.add), r=[sel.k], w=[pr.k]))
            step(lambda s_, sel, pr, gs, t1, m1, selm, sel2, comb: P.op("dve", lambda e: e.tensor_reduce(gs[:], pr[:], AX.X, ALU.max), r=[pr.k], w=[gs.k]))
            step(lambda s_, sel, pr, gs, t1, m1, selm, sel2, comb: P.op("dve", lambda e: e.tensor_reduce(m1[:, 0:1], gs[:], AX.X, ALU.max), r=[gs.k], w=[m1.k]))
            step(lambda s_, sel, pr, gs, t1, m1, selm, sel2, comb: P.op("dve", lambda e: e.tensor_scalar(gs[:], gs[:], m1[:, 0:1], None, ALU.is_ge), r=[gs.k, m1.k], w=[gs.k]))
            step(lambda s_, sel, pr, gs, t1, m1, selm, sel2, comb: P.op("dve", lambda e: e.tensor_scalar(t1[:], gs[:], -1.0, 1e30, ALU.add, ALU.mult), r=[gs.k], w=[t1.k]))
            step(lambda s_, sel, pr, gs, t1, m1, selm, sel2, comb: P.op("dve", lambda e: e.tensor_tensor(g4(selm), g4(sel), gs[:, :].unsqueeze(2).to_broadcast([128, 4, 4]), mm), r=[sel.k, gs.k], w=[selm.k]))
            step(lambda s_, sel, pr, gs, t1, m1, selm, sel2, comb: P.op("dve", lambda e: e.tensor_tensor(g4(selm), g4(selm), t1[:, :].unsqueeze(2).to_broadcast([128, 4, 4]), ALU.add), r=[selm.k, t1.k], w=[selm.k]))
            step(lambda s_, sel, pr, gs, t1, m1, selm, sel2, comb: P.op("dve", lambda e: e.tensor_reduce(m1[:, 0:1], selm[:], AX.X, ALU.max), r=[selm.k], w=[m1.k]))
            step(lambda s_, sel, pr, gs, t1, m1, selm, sel2, comb: P.op("dve", lambda e: e.tensor_scalar(sel2[:], selm[:], m1[:, 0:1], None, ALU.is_ge), r=[selm.k, m1.k], w=[sel2.k]))
            step(lambda s_, sel, pr, gs, t1, m1, selm, sel2, comb: P.op("dve", lambda e: e.scalar_tensor_tensor(sel2[:], sel2[:], -1e30, selm[:], mm, ALU.add), r=[sel2.k, selm.k], w=[sel2.k]))
            step(lambda s_, sel, pr, gs, t1, m1, selm, sel2, comb: P.op("dve", lambda e: e.tensor_reduce(m1[:, 1:2], sel2[:], AX.X, ALU.max), r=[sel2.k], w=[m1.k]))
            step(lambda s_, sel, pr, gs, t1, m1, selm, sel2, comb: P.op("dve", lambda e: e.tensor_scalar(sel2[:], selm[:], m1[:, 1:2], None, ALU.is_ge), r=[selm.k, m1.k], w=[sel2.k]))
            step(lambda s_, sel, pr, gs, t1, m1, selm, sel2, comb: P.op("dve", lambda e: e.tensor_tensor(comb[:], s_[:], sel2[:], mm), r=[s_.k, sel2.k], w=[comb.k]))
            step(lambda s_, sel, pr, gs, t1, m1, selm, sel2, comb: P.op("dve", lambda e: e.tensor_reduce(m1[:, 0:1], comb[:], AX.X, ALU.add), r=[comb.k], w=[m1.k]))
            step(lambda s_, sel, pr, gs, t1, m1, selm, sel2, comb: P.op("dve", lambda e: e.reciprocal(m1[:, 0:1], m1[:, 0:1]), r=[m1.k], w=[m1.k]))
            step(lambda s_, sel, pr, gs, t1, m1, selm, sel2, comb: P.op("dve", lambda e: e.tensor_scalar(comb[:], comb[:], m1[:, 0:1], None, mm), r=[comb.k, m1.k], w=[comb.k]))

        def router_back(s):
            for j in R4:
                comb = comb_l[j]
                pct = psb(C)
                P.op("pe", lambda e: e.transpose(pct[0:16, 0:128], comb[:, :], C.ident[:]), r=[comb.k, C.ident_tok], w=[pct.k])
                P.op("act", lambda e: e.copy(combT[:, j * 128:(j + 1) * 128], pct[0:16, 0:128]), r=[pct.k], w=[combT.k])

        def load_x(s):
            for j in R4:
                i = s * 4 + j
                P.dma("sp", xts[j][:], srcH[i * 128:(i + 1) * 128, :], r=[srcH_tok[i]], w=[xts[j].k])

        def make_pend(s):
            xs_ = stg[s % 2]

            def pend():
                for j in R4:
                    tile_to_stage(C, xts[j], xs_, j)
                P.dma("sp", XTv(dstXT)[:, :, s * 512:(s + 1) * 512], xs_[:], r=[xs_.k], w=[dstXT_tok[s]])
            return pend

        pend = None
        load_hT(0)
        router_front(0)
        router_back(0)
        for s in range(C.NS):
            hT = hts[s % 2]
            if s + 1 < C.NS:
                load_hT(s + 1)
            for ex in range(NE):
                WGU = load_w(ex)
                pcb = psb(C)
                P.op("pe", lambda e: e.matmul(pcb[:, :], lhsT=SEL[:, ex, :], rhs=combT[:, :], start=True, stop=True), r=[SEL.k, combT.k], w=[pcb.k])
                for f in range(2):
                    pG, pU = psb(C), psb(C)
                    for pb, ti in ((pG, 0), (pU, 1)):
                        for c in range(8):
                            P.op("pe", lambda e: e.matmul(pb[:, :], lhsT=WGU[:, ti, c, f * 128:(f + 1) * 128], rhs=hT[:, c, :], start=(c == 0), stop=(c == 7)),
                                 r=[WGU.k, hT.k], w=[pb.k], inc=(c == 7))
                    sg_ = sgl[(ex * 2 + f) % 2]
                    P.op("act", lambda e: e.activation(out=sg_[:], in_=pG[:, :], func=AF.Silu), r=[pG.k], w=[sg_.k])
                    P.op("dve", lambda e: e.tensor_tensor(sg_[:], sg_[:], pU[:, :], mm), r=[sg_.k, pU.k], w=[sg_.k])
                    P.op("dve", lambda e: e.tensor_tensor(actT[:, ex * 2 + f, :], sg_[:], pcb[:, :], mm), r=[sg_.k, pcb.k], w=[actT.k])
                if ex == 3:
                    if pend is not None:
                        pend()
                        pend = None
                    load_x(s)
            if s + 1 < C.NS:
                router_front(s + 1)
            for j in R4:
                xt = xts[j]
                for half in range(2):
                    pb = psb(C)
                    for c in range(32):
                        P.op("pe", lambda e: e.matmul(pb[:, :], lhsT=actT[:, c, j * 128:(j + 1) * 128], rhs=WD[:, c, half * 512:(half + 1) * 512], start=(c == 0), stop=(c == 31)),
                             r=[actT.k, WD.k], w=[pb.k], inc=(c == 31))
                    P.op("dve", lambda e: e.scalar_tensor_tensor(xt[:, half * 512:(half + 1) * 512], xt[:, half * 512:(half + 1) * 512], DN_ALPHA, pb[:, :], ALU.mult, ALU.add),
                         r=[xt.k, pb.k], w=[xt.k])
            if s + 1 < C.NS:
                router_back(s + 1)
            ln_lockstep(C, xts, gb, bb, sts, junk)
            for j in R4:
                i = s * 4 + j
                P.dma("sp", dstX[i * 128:(i + 1) * 128, :], xts[j][:], r=[xts[j].k], w=[dstX_tok[i]])
            if dstXT is not None:
                pend = make_pend(s)
        if pend is not None:
            pend()
        P.barrier()


def rope_consts(T):
    pos = np.arange(T, dtype=np.float64)
    c = {}
    for name, half in (("k", 64), ("i", 32)):
        inv = 10000.0 ** (-np.arange(half, dtype=np.float64) / half)
        ang = (pos.astype(np.float32)[:, None] * inv.astype(np.float32)[None, :]).astype(np.float32).astype(np.float64)
        cs = np.cos(ang).astype(np.float32).reshape(T // 128, 128, half).transpose(1, 0, 2)
        sn = np.sin(ang).astype(np.float32).reshape(T // 128, 128, half).transpose(1, 0, 2)
        c["cos_" + name] = np.ascontiguousarray(cs)
        c["sin_" + name] = np.ascontiguousarray(sn)
    q = np.arange(128)[:, None]
    s = np.arange(128)[None, :]
    c["cbias"] = np.where(s <= q, 0.0, -1e30).astype(np.float32)
    c["halfpow"] = (0.5 ** np.arange(1, 33, dtype=np.float64)).astype(np.float32).reshape(1, 32)
    c["tiebias"] = (-1e-6 * np.arange(T, dtype=np.float64)).astype(np.float32).reshape(1, T)
    return c


def rope_tm(C, dst_ap, dst_k, src, src_k, cosb, sinb, nh, half, ta_ap, ta_k, tb_ap, tb_k, rdeps):
    P = C.P
    mm = ALU.mult
    n = nh * 2
    s3 = src.rearrange("p (n f) -> p n f", n=n)
    cb = cosb.unsqueeze(1).to_broadcast([128, n, half])
    sb_ = sinb.unsqueeze(1).to_broadcast([128, n, half])
    a3 = ta_ap.rearrange("p (n f) -> p n f", n=n)
    b3 = tb_ap.rearrange("p (n f) -> p n f", n=n)
    P.op("dve", lambda e: e.tensor_tensor(a3, s3, cb, mm), r=[src_k] + rdeps, w=[ta_k])
    P.op("dve", lambda e: e.tensor_tensor(b3, s3, sb_, mm), r=[src_k] + rdeps, w=[tb_k])
    a4 = ta_ap.rearrange("p (h t f) -> p h t f", h=nh, t=2)
    b4 = tb_ap.rearrange("p (h t f) -> p h t f", h=nh, t=2)
    d4 = dst_ap.rearrange("p (h t f) -> p h t f", h=nh, t=2)
    P.op("pool", lambda e: e.tensor_tensor(d4[:, :, 0, :], a4[:, :, 0, :], b4[:, :, 1, :], ALU.subtract), r=[ta_k, tb_k], w=[dst_k])
    P.op("pool", lambda e: e.tensor_tensor(d4[:, :, 1, :], a4[:, :, 1, :], b4[:, :, 0, :], ALU.add), r=[ta_k, tb_k], w=[dst_k])


def phase_dsa(C):
    nc, P, T, I = C.nc, C.P, C.T, C.I
    mm = ALU.mult
    KT = min(256, T // 4)
    NIT = 25
    SCALE = 128 ** -0.5
    C.bank_pool = [0, 1, 2, 3, 4]
    with ExitStack() as es:
        def wt(name, shape, dt=F32):
            return mk(P, list(shape), dt, name, es)
        WQ = wt("WQ", (128, 8, 1024), BF16)
        WR = wt("WR", (128, 8, 580), BF16)
        for c in range(8):
            P.dma("pool", WQ[:, c, :], I["w_in_odd"][c * 128:(c + 1) * 128, 0:1024], w=[WQ.k])
            P.dma("pool", WR[:, c, :], I["w_in_odd"][c * 128:(c + 1) * 128, 1024:1604], w=[WR.k])
        RTs = [[wt("CK", (128, 4, 64)), wt("SK", (128, 4, 64)), wt("CI", (128, 4, 32)), wt("SI", (128, 4, 32))] for _ in range(2)]

        def load_tables(s):
            tl = RTs[s % 2]
            for t_, nm in zip(tl, ("c_cos_k", "c_sin_k", "c_cos_i", "c_sin_i")):
                P.dma("sp", t_[:], I[nm][:, s * 4:(s + 1) * 4, :], w=[t_.k])
            return tl
        BIAS = wt("BIAS", (128, T))
        bcast_load(C, BIAS[:], I["c_tiebias"], 128, BIAS.k)
        CB = wt("CB", (128, 128))
        P.dma("sp", CB[:], I["c_cbias"], w=[CB.k])
        ikg, ikb = wt("ikg", (128, 64)), wt("ikb", (128, 64))
        bcast_load(C, ikg[:], I["c_ik_ln_g"], 128, ikg.k)
        bcast_load(C, ikb[:], I["c_ik_ln_b"], 128, ikb.k)
        kT = wt("kT", (128, T), BF16)
        ikT = wt("ikT", (128, T), BF16)
        Vx = wt("Vx", (128, C.NT, 129), BF16)
        P.op("dve", lambda e: e.memset(Vx[:, :, 128:129], 1.0), w=[Vx.k])
        xTs = [wt("xTd", (128, 8, 512), BF16) for _ in range(2)]
        ta, tb = wt("ropeA", (128, 512)), wt("ropeB", (128, 512))
        kr = wt("kr", (128, 128))
        ikr = wt("ikr", (128, 128))
        st2 = wt("st2", (128, 2))
        for s in range(C.NS):
            xT = xTs[s % 2]
            P.dma("sp", xT[:], XTv(C.XT1)[:, :, s * 512:(s + 1) * 512], r=[C.XT1_tok[s]], w=[xT.k])
            CK, SK, CI, SI = load_tables(s)
            for j in range(4):
                i = s * 4 + j
                pb = psb(C)
                for c in range(8):
                    P.op("pe", lambda e: e.matmul(pb[:, 0:256], lhsT=xT[:, c, j * 128:(j + 1) * 128], rhs=WR[:, c, 0:256], start=(c == 0), stop=(c == 7)),
                         r=[xT.k, WR.k], w=[pb.k], inc=False)
                for c in range(8):
                    P.op("pe", lambda e: e.matmul(pb[:, 256:320], lhsT=xT[:, c, j * 128:(j + 1) * 128], rhs=WR[:, c, 512:576], start=(c == 0), stop=(c == 7)),
                         r=[xT.k, WR.k], w=[pb.k], inc=(c == 7))
                rope_tm(C, kr[:, :], kr.k, pb[:, 0:128], pb.k, CK[:, j, :], SK[:, j, :], 1, 64, ta[:, 0:128], ta.k, tb[:, 0:128], tb.k, [CK.k, SK.k])
                P.op("act", lambda e: e.copy(Vx[:, i, 0:128], pb[:, 128:256]), r=[pb.k], w=[Vx.k])
                P.op("dve", lambda e: e.tensor_reduce(st2[:, 0:1], pb[:, 256:320], AX.X, ALU.add), r=[pb.k], w=[st2.k])
                P.op("dve", lambda e: e.tensor_scalar(st2[:, 0:1], st2[:, 0:1], 1.0 / 64, None, mm), r=[st2.k], w=[st2.k])
                P.op("dve", lambda e: e.tensor_scalar(ikr[:, 0:64], pb[:, 256:320], st2[:, 0:1], None, ALU.subtract), r=[pb.k, st2.k], w=[ikr.k])
                P.op("act", lambda e: e.activation(out=ikr[:, 64:128], in_=ikr[:, 0:64], func=AF.Square, accum_out=st2[:, 1:2]), r=[ikr.k], w=[ikr.k, st2.k])
                P.op("act", lambda e: e.activation(out=st2[:, 1:2], in_=st2[:, 1:2], func=AF.Sqrt, bias=LN_EPS, scale=1.0 / 64), r=[st2.k], w=[st2.k])
                P.op("dve", lambda e: e.reciprocal(st2[:, 1:2], st2[:, 1:2]), r=[st2.k], w=[st2.k])
                P.op("dve", lambda e: e.scalar_tensor_tensor(ikr[:, 0:64], ikr[:, 0:64], st2[:, 1:2], ikg[:], mm, mm), r=[ikr.k, st2.k, ikg.k], w=[ikr.k])
                P.op("dve", lambda e: e.tensor_tensor(ikr[:, 64:128], ikr[:, 0:64], ikb[:], ALU.add), r=[ikr.k, ikb.k], w=[ikr.k])
                ikn = TL(ikr.t, ikr.k)
                rope_src = ikr[:, 64:128]
                n = 2
                s3 = rope_src.rearrange("p (n f) -> p n f", n=n)
                cb = CI[:, j, :].unsqueeze(1).to_broadcast([128, n, 32])
                sb_ = SI[:, j, :].unsqueeze(1).to_broadcast([128, n, 32])
                a3 = ta[:, 0:64].rearrange("p (n f) -> p n f", n=n)
                b3 = tb[:, 0:64].rearrange("p (n f) -> p n f", n=n)
                P.op("dve", lambda e: e.tensor_tensor(a3, s3, cb, mm), r=[ikr.k, CI.k], w=[ta.k])
                P.op("dve", lambda e: e.tensor_tensor(b3, s3, sb_, mm), r=[ikr.k, SI.k], w=[tb.k])
                P.op("pool", lambda e: e.tensor_tensor(ikr[:, 0:32], ta[:, 0:32], tb[:, 32:64], ALU.subtract), r=[ta.k, tb.k], w=[ikr.k])
                P.op("pool", lambda e: e.tensor_tensor(ikr[:, 32:64], ta[:, 32:64], tb[:, 0:32], ALU.add), r=[ta.k, tb.k], w=[ikr.k])
                P.op("pool", lambda e: e.tensor_copy(ikr[:, 64:128], ikr[:, 0:64]), r=[ikr.k], w=[ikr.k])
                pt = psb(C)
                P.op("pe", lambda e: e.transpose(pt[:, 0:128], kr[:, :], C.ident[:]), r=[kr.k, C.ident_tok], w=[pt.k], inc=False)
                P.op("pe", lambda e: e.transpose(pt[:, 128:256], ikr[:, :], C.ident[:]), r=[ikr.k, C.ident_tok], w=[pt.k])
                P.op("act", lambda e: e.copy(kT[:, i * 128:(i + 1) * 128], pt[:, 0:128]), r=[pt.k], w=[kT.k])
                P.op("act", lambda e: e.mul(ikT[:, i * 128:(i + 1) * 128], pt[:, 128:256], 0.125), r=[pt.k], w=[ikT.k])
        SC = wt("SC", (128, T))
        MASKs = [wt("MASK", (128, T)) for _ in range(2)]
        junk = wt("junkd", (128, T), BF16)
        qr = wt("qr", (128, 1024))
        iqr = wt("iqr", (128, 256))
        qTs = [wt("qT", (128, 8, 128), BF16) for _ in range(2)]
        iqT = wt("iqT", (128, 2, 128), BF16)
        iws = wt("iws", (128, 4))
        rl = [wt("rl%d" % n, (128, 512)) for n in range(2)]
        bs = wt("bs", (128, 8))
        Dk = wt("Dk", (128, NIT))
        HK = wt("HK", (128, NIT))
        bcast_load(C, HK[:], I["c_halfpow"][0:1, 0:NIT], 128, HK.k)
        mT4 = [wt("mT4_%d" % n, (128, 4, 128), BF16) for n in range(2)]
        pTs = [wt("pT%d" % n, (128, 4, 128), BF16) for n in range(4)]
        o_ = wt("od", (128, 1024))
        rs8 = wt("rs8", (128, 8))
        ostg = [wt("ostg", (128, 8, 512), BF16) for _ in range(2)]
        accb = [TL(*C.banks[b]) for b in (5, 6, 7)]
        MBIG = 30000.0
        identb = wt("identb", (128, 128), BF16)
        P.op("dve", lambda e: e.tensor_copy(identb[:], C.ident[:]), r=[C.ident_tok], w=[identb.k])
        acc_of = [(0, 0), (0, 1), (0, 2), (1, 0), (1, 1), (1, 2), (2, 0), (2, 1)]
        def qblock(s, j):
            if True:
                i = s * 4 + j
                L = (i + 1) * 128
                xT = xTs[s % 2]
                og = ostg[s % 2]
                qT = qTs[i % 2]
                MASK = MASKs[i % 2]
                if j == 0:
                    P.dma("sp", xT[:], XTv(C.XT1)[:, :, s * 512:(s + 1) * 512], r=[C.XT1_tok[s]], w=[xT.k])
                    load_tables(s)
                cast_some(C, 1, 2)
                CK, SK, CI, SI = RTs[s % 2]
                pq = [psb(C), psb(C)]
                for half in range(2):
                    for c in range(8):
                        P.op("pe", lambda e: e.matmul(pq[half][:, :], lhsT=xT[:, c, j * 128:(j + 1) * 128], rhs=WQ[:, c, half * 512:(half + 1) * 512], start=(c == 0), stop=(c == 7)),
                             r=[xT.k, WQ.k], w=[pq[half].k], inc=(c == 7))
                piq = psb(C)
                for c in range(8):
                    P.op("pe", lambda e: e.matmul(piq[:, 0:256], lhsT=xT[:, c, j * 128:(j + 1) * 128], rhs=WR[:, c, 256:512], start=(c == 0), stop=(c == 7)),
                         r=[xT.k, WR.k], w=[piq.k], inc=False)
                for c in range(8):
                    P.op("pe", lambda e: e.matmul(piq[:, 256:260], lhsT=xT[:, c, j * 128:(j + 1) * 128], rhs=WR[:, c, 576:580], start=(c == 0), stop=(c == 7)),
                         r=[xT.k, WR.k], w=[piq.k], inc=(c == 7))
                for half in range(2):
                    hs = slice(half * 512, (half + 1) * 512)
                    rope_tm(C, qr[:, hs], qr.k, pq[half][:, :], pq[half].k, CK[:, j, :], SK[:, j, :], 4, 64,
                            ta[:, 0:512], ta.k, tb[:, 0:512], tb.k, [CK.k, SK.k])
                rope_tm(C, iqr[:, :], iqr.k, piq[:, 0:256], piq.k, CI[:, j, :], SI[:, j, :], 4, 32, ta[:, 0:256], ta.k, tb[:, 0:256], tb.k, [CI.k, SI.k])
                P.op("act", lambda e: e.mul(iws[:], piq[:, 256:260], 0.5), r=[piq.k], w=[iws.k])
                for half in range(2):
                    pb = psb(C)
                    for c4 in range(4):
                        h = half * 4 + c4
                        P.op("pe", lambda e: e.transpose(pb[:, c4 * 128:(c4 + 1) * 128], qr[:, h * 128:(h + 1) * 128], C.ident[:]), r=[qr.k, C.ident_tok], w=[pb.k], inc=(c4 == 3))
                    P.op("act", lambda e: e.copy(qT[:, half * 4:(half + 1) * 4, :], hv(pb[:, :], 4)), r=[pb.k], w=[qT.k])
                pb = psb(C)
                for c2 in range(2):
                    P.op("pe", lambda e: e.transpose(pb[:, c2 * 128:(c2 + 1) * 128], iqr[:, c2 * 128:(c2 + 1) * 128], C.ident[:]), r=[iqr.k, C.ident_tok], w=[pb.k], inc=(c2 == 1))
                P.op("act", lambda e: e.copy(iqT[:], hv(pb[:, 0:256], 2)), r=[pb.k], w=[iqT.k])
                yield "F"
                for k0 in range(0, L, 512):
                    kw = min(512, L - k0)
                    for h in range(4):
                        ph = psb(C)
                        pl = (h % 2) * 64
                        P.op("pe", lambda e: e.matmul(ph[:, 0:kw], lhsT=iqT[pl:pl + 64, h // 2, :], rhs=ikT[pl:pl + 64, k0:k0 + kw], start=True, stop=True),
                             r=[iqT.k, ikT.k], w=[ph.k])
                        r_ = rl[h % 2]
                        P.op("act", lambda e: e.activation(out=r_[:, 0:kw], in_=ph[:, 0:kw], func=AF.Relu), r=[ph.k], w=[r_.k])
                        if h == 0:
                            P.op("dve", lambda e: e.scalar_tensor_tensor(SC[:, k0:k0 + kw], r_[:, 0:kw], iws[:, 0:1], BIAS[:, k0:k0 + kw], mm, ALU.add), r=[r_.k, iws.k, BIAS.k], w=[SC.k])
                        else:
                            P.op("dve", lambda e: e.scalar_tensor_tensor(SC[:, k0:k0 + kw], r_[:, 0:kw], iws[:, h:h + 1], SC[:, k0:k0 + kw], mm, ALU.add),
                                 r=[r_.k, iws.k, SC.k], w=[SC.k])
                    yield "F"
                if L > KT:
                    P.op("dve", lambda e: e.tensor_reduce(bs[:, 1:2], SC[:, 0:L], AX.X, ALU.max, apply_absolute_value=True), r=[SC.k], w=[bs.k])
                    P.op("dve", lambda e: e.tensor_scalar(bs[:, 0:1], bs[:, 1:2], -1.0, -1.0, mm, ALU.add), r=[bs.k], w=[bs.k])
                    P.op("dve", lambda e: e.tensor_scalar(bs[:, 1:2], bs[:, 1:2], 2.0, 2.0, mm, ALU.add), r=[bs.k], w=[bs.k])
                    P.op("dve", lambda e: e.tensor_scalar(Dk[:], HK[:], bs[:, 1:2], None, mm), r=[bs.k, HK.k], w=[Dk.k])
                P.op("pool", lambda e: e.tensor_tensor(SC[:, i * 128:L], SC[:, i * 128:L], CB[:], ALU.add), r=[SC.k, CB.k], w=[SC.k])
                if L > KT:
                    for it in range(NIT):
                        P.op("dve", lambda e: e.tensor_tensor(bs[:, 2:3], bs[:, 0:1], Dk[:, it:it + 1], ALU.add), r=[bs.k, Dk.k], w=[bs.k])
                        P.op("dve", lambda e: e.tensor_scalar(junk[:, 0:L], SC[:, 0:L], bs[:, 2:3], None, ALU.is_gt, ALU.add, accum_out=bs[:, 3:4]),
                             r=[SC.k, bs.k], w=[junk.k, bs.k])
                        P.op("dve", lambda e: e.scalar_tensor_tensor(bs[:, 4:5], bs[:, 3:4], float(KT) - 0.5, Dk[:, it:it + 1], ALU.is_gt, mm), r=[bs.k, Dk.k], w=[bs.k])
                        P.op("dve", lambda e: e.tensor_tensor(bs[:, 0:1], bs[:, 0:1], bs[:, 4:5], ALU.add), r=[bs.k], w=[bs.k])
                        yield "F"
                    P.op("dve", lambda e: e.tensor_scalar(MASK[:, 0:L], SC[:, 0:L], bs[:, 0:1], None, ALU.is_le), r=[SC.k, bs.k], w=[MASK.k])
                else:
                    P.op("dve", lambda e: e.tensor_scalar(MASK[:, 0:L], SC[:, 0:L], -1e29, None, ALU.is_le), r=[SC.k], w=[MASK.k])
                if "dbg_mask" in C.dbg:
                    P.dma("sp", C.dbg["dbg_mask"][i * 128:(i + 1) * 128, 0:L], MASK[:, 0:L], r=[MASK.k])
                    P.dma("sp", C.dbg["dbg_sc"][i * 128:(i + 1) * 128, 0:L], SC[:, 0:L], r=[SC.k])
                yield "END_FRONT"
                units = [(st, hg) for st in range(i + 1) for hg in range(2)]

                def stage1(st, hg):
                    if hg == 0 and st % 4 == 0:
                        n4 = min(4, i + 1 - st)
                        m4 = mT4[(st // 4) % 2]
                        pb = psb(C)
                        for u in range(n4):
                            P.op("pe", lambda e: e.transpose(pb[:, u * 128:(u + 1) * 128], MASK[:, (st + u) * 128:(st + u + 1) * 128], C.ident[:]),
                                 r=[MASK.k, C.ident_tok], w=[pb.k], inc=(u == n4 - 1))
                        P.op("act", lambda e: e.mul(m4[:, 0:n4, :], hv(pb[:, :], 4)[:, 0:n4, :], -MBIG), r=[pb.k], w=[m4.k])
                    m4 = mT4[(st // 4) % 2]
                    pl_ = psb(C)
                    P.op("pe", lambda e: e.matmul(pl_[:, :], lhsT=identb[:, :], rhs=m4[:, st % 4, :].unsqueeze(1).to_broadcast([128, 4, 128]), start=True, stop=False),
                         r=[identb.k, m4.k], w=[pl_.k], inc=False)
                    P.op("pe", lambda e: e.matmul(pl_[:, :], lhsT=kT[:, st * 128:(st + 1) * 128], rhs=qT[:, hg * 4:(hg + 1) * 4, :].rearrange("p h q -> p (h q)"), start=False, stop=True),
                         r=[kT.k, qT.k], w=[pl_.k])
                    pT = pTs[hg * 2 + st % 2]
                    P.op("act", lambda e: e.activation(out=pT[:], in_=hv(pl_[:, :], 4), func=AF.Exp, scale=SCALE), r=[pl_.k], w=[pT.k])

                def stage2(st, hg):
                    pT = pTs[hg * 2 + st % 2]
                    for hh in range(4):
                        h = hg * 4 + hh
                        ab, slot = acc_of[h]
                        P.op("pe", lambda e: e.matmul(accb[ab][:, slot * 129:(slot + 1) * 129], lhsT=pT[:, hh, :], rhs=Vx[:, st, :], start=(st == 0 and slot == 0), stop=(st == i), skip_group_check=True),
                             r=[pT.k, Vx.k], w=[accb[ab].k], inc=(hh == 3))

                stage1(*units[0])
                for n in range(len(units)):
                    if n + 1 < len(units):
                        stage1(*units[n + 1])
                    stage2(*units[n])
                    if units[n][1] == 1:
                        yield "B"
                for h in range(8):
                    ab, slot = acc_of[h]
                    P.op("dve", lambda e: e.reciprocal(rs8[:, h:h + 1], accb[ab][:, slot * 129 + 128:slot * 129 + 129]), r=[accb[ab].k], w=[rs8.k])
                    P.op("act", lambda e: e.activation(out=o_[:, h * 128:(h + 1) * 128], in_=accb[ab][:, slot * 129:slot * 129 + 128], func=AF.Copy, scale=rs8[:, h:h + 1]),
                         r=[accb[ab].k, rs8.k], w=[o_.k])
                if "dbg_dsa" in C.dbg:
                    P.dma("sp", C.dbg["dbg_dsa"][i * 128:(i + 1) * 128, :], o_[:], r=[o_.k])
                tile_to_stage(C, o_, og, j)
                if j == 3:
                    P.dma("sp", XTv(C.YT)[:, :, s * 512:(s + 1) * 512], og[:], r=[og.k], w=[C.YT_tok[0][s], C.YT_tok[1][s]])

        pipeline2([(lambda s=s, j=j: qblock(s, j)) for s in range(C.NS) for j in range(4)], interleave=C.dsa_interleave)
        P.barrier()
    C.bank_pool = list(range(8))


class _View:
    def __init__(self, tl, sl):
        self.t = _Sl(tl.t, sl)
        self.k = tl.k

    def __getitem__(self, idx):
        return self.t[idx]


class _Sl:
    def __init__(self, t, sl):
        self.base = t
        self.sl = sl

    def __getitem__(self, idx):
        rows, cols = idx
        assert cols == slice(None)
        return self.base[rows, self.sl]


_NC_CACHE = {}


def _in_map(inputs, b, T, consts):
    m = {"x": np.ascontiguousarray(inputs["x"][b, :T], dtype=np.float32)}
    for k, a in inputs.items():
        if k == "x":
            continue
        a = np.asarray(a, dtype=np.float32)
        if k in ("router_w", "exp_w_gate", "exp_w_up", "exp_w_down", "ln1_g", "ln1_b", "ln2_g", "ln2_b"):
            m[k] = np.ascontiguousarray(a)
        elif k == "router_bias":
            m[k] = np.ascontiguousarray(a.reshape(1, -1))
        elif k == "a_r_k":
            m[k] = np.ascontiguousarray(a.reshape(1, 512))
        elif a.ndim == 3:
            m[k] = np.ascontiguousarray(a[0])
        elif a.ndim == 2:
            m[k] = np.ascontiguousarray(a[0:1])
    m.update(consts)
    return m


def kernel(**inputs):
    x = np.asarray(inputs["x"])
    B, T, _ = x.shape
    if T not in _NC_CACHE:
        _NC_CACHE[T] = build(T)
    nc = _NC_CACHE[T]
    consts = {"c_" + k: v for k, v in host_consts(T).items()}
    consts.update({"c_" + k: v for k, v in rope_consts(T).items()})
    in_maps = [_in_map(inputs, b, T, consts) for b in range(B)]
    res = run_bass_kernel_spmd(nc, in_maps, core_ids=list(range(B)))
    out = np.stack([np.asarray(res.results[b]["out"], dtype=np.float32) for b in range(B)], 0)
    return out
```

```python
import numpy as np
import ml_dtypes
from contextlib import ExitStack
import concourse.bass as bass
import concourse.mybir as mybir
from concourse.bass_utils import run_bass_kernel_spmd

F32 = mybir.dt.float32
BF16 = mybir.dt.bfloat16
AF = mybir.ActivationFunctionType
ALU = mybir.AluOpType
AX = mybir.AxisListType

D = 1024
A_COLS = 1792
B_COLS = 1552
EVEN_COLS = 3344
ODD_COLS = 1604
NE = 16
DE = 256
DN_ALPHA = 4 ** 0.25
LN_EPS = 1e-5
DEC = 0.6065306597126334
DSA_INTERLEAVE = True
RWKV_INTERLEAVE = False


class Tok:
    __slots__ = ("w", "r")

    def __init__(self):
        self.w = None
        self.r = {}


class Eng:
    def __init__(self, name, h, sem):
        self.name = name
        self.h = h
        self.sem = sem
        self.cnt = 0
        self.waited = {}


class Prog:
    NSLOT = 8

    def __init__(self, nc, es):
        self.nc = nc
        self.es = es
        self.E = {}
        for name, h in (("pe", nc.tensor), ("act", nc.scalar), ("dve", nc.vector),
                        ("pool", nc.gpsimd), ("sp", nc.sync)):
            sem = es.enter_context(nc.semaphore("sem_" + name))
            self.E[name] = Eng(name, h, sem)
        self.slots = {}
        self.dn = {}
        for q in ("sp", "pool", "act"):
            self.slots[q] = [[es.enter_context(nc.semaphore("dq_%s%d" % (q, i))), 0] for i in range(self.NSLOT)]
            self.dn[q] = 0
        self.nalloc = 0

    def sb(self, shape, dt=F32, name=None, es=None):
        self.nalloc += 1
        t = (es or self.es).enter_context(self.nc.sbuf_tensor("%s_%d" % (name or "t", self.nalloc), list(shape), dt))
        return t

    def _wait(self, eng, ev):
        sem, val = ev
        key = sem.num
        if eng.waited.get(key, 0) >= val:
            return
        eng.h.wait_ge(sem, val)
        eng.waited[key] = val

    def _deps(self, en, r, w):
        eng = self.E[en]
        for t in r:
            if t.w is not None:
                yield t.w
        for t in w:
            if t.w is not None:
                yield t.w
            for ev in t.r.values():
                yield ev

    def op(self, en, fn, r=(), w=(), inc=True):
        eng = self.E[en]
        for ev in list(self._deps(en, r, w)):
            if en == "pe" and ev[0] is eng.sem:
                continue
            self._wait(eng, ev)
        ins = fn(eng.h)
        myev = (eng.sem, eng.cnt + 1)
        if inc:
            ins.then_inc(eng.sem, 1)
            eng.cnt += 1
        for t in r:
            t.r[en] = myev
        for t in w:
            t.w = myev
            t.r = {}
        return ins

    def dma(self, qn, out, in_, r=(), w=(), **kw):
        q = self.E[qn]
        for ev in list(self._deps(qn, r, w)):
            self._wait(q, ev)
        slot = self.slots[qn][self.dn[qn] % self.NSLOT]
        self.dn[qn] += 1
        if slot[1] > 0:
            self._wait(q, (slot[0], slot[1]))
        ins = q.h.dma_start(out=out, in_=in_, **kw)
        slot[1] += 16
        ins.then_inc(slot[0], 16)
        ev = (slot[0], slot[1])
        key = "d%d" % slot[0].num
        for t in r:
            t.r[key] = ev
        for t in w:
            t.w = ev
            t.r = {}

    def barrier(self):
        evs = []
        for q in self.slots:
            for sem, val in self.slots[q]:
                if val > 0:
                    evs.append((sem, val))
        for name, e in self.E.items():
            if e.cnt > 0:
                evs.append((e.sem, e.cnt))
        for name, e in self.E.items():
            for ev in evs:
                if ev[0] is e.sem and name == "pe":
                    continue
                self._wait(e, ev)

    def finish(self):
        sp = self.E["sp"]
        for q in self.slots:
            for sem, val in self.slots[q]:
                if val > 0:
                    self._wait(sp, (sem, val))
        for name, e in self.E.items():
            if name != "sp" and e.cnt > 0:
                self._wait(sp, (e.sem, e.cnt))


def host_consts(T):
    c = {}
    c["ident"] = np.eye(128, dtype=np.float32)
    j = np.arange(64)[:, None]
    i = np.arange(64)[None, :]
    c["tri64"] = np.stack([(-DEC) * (j <= i), (-DEC) * (j < i), (-DEC) * (j > i)], 1).astype(np.float32)
    c["ncol64"] = np.full((64, 1), -DEC, np.float32)
    su = (j < i).astype(np.float32)
    iu = (j <= i).astype(np.float32)
    sl = (j > i).astype(np.float32)
    mMA = np.concatenate([-su, iu], 1)
    mBB = np.concatenate([su, iu], 1)
    c["mMA"] = np.tile(mMA[:, None, :], (1, 8, 1)).astype(np.float32)
    c["mBB"] = np.tile(mBB[:, None, :], (1, 8, 1)).astype(np.float32)
    c["mNT"] = np.tile((-sl)[:, None, :], (1, 8, 1)).astype(np.float32)
    c["id8"] = np.tile(np.eye(64, dtype=np.float32)[:, None, :], (1, 8, 1))
    j = np.arange(128)[:, None]
    i = np.arange(128)[None, :]
    c["tri128"] = np.stack([(-1 / 16) * (j <= i), (-1 / 16) * (j > i)], 1).astype(np.float32)
    c["ncol128"] = np.full((128, 1), -1 / 16, np.float32)
    c["sel"] = (np.arange(16)[:, None, None] == np.arange(16)[None, :, None]).astype(np.float32) * np.ones((1, 1, 128), np.float32)
    c["iu128"] = np.tile((j <= i).astype(np.float32)[:, None, :], (1, 4, 1))
    return c


class Ctx:
    pass


def build(T, dbg=(), stages=("A", "R", "G", "O0", "M0", "S1", "O1", "M1")):
    nc = bass.Bass("TRN2", target_bir_lowering=False)
    es = ExitStack()
    P = Prog(nc, es)
    NT = T // 128
    NS = T // 512
    C = Ctx()
    C.nc, C.P, C.T, C.NT, C.NS = nc, P, T, NT, NS
    C.dbg = {}
    C.dsa_interleave = DSA_INTERLEAVE

    def din(name, shape, dt=F32):
        return nc.dram_tensor(name, list(shape), dt, kind="ExternalInput").ap()

    def dscr(name, shape, dt=F32, out=False):
        kind = "ExternalOutput" if (out or name in dbg) else "Internal"
        return nc.dram_tensor(name, list(shape), dt, kind=kind).ap()

    I = {}
    I["x"] = din("x", [T, D])
    for name, shape in (("w_in_even", [D, EVEN_COLS]), ("a_mu", [1, A_COLS]), ("a_w0", [1, 512]), ("a_w2", [64, 512]),
                        ("a_a0", [1, 512]), ("a_a2", [64, 512]), ("a_g2", [128, 512]), ("a_kk_scale", [1, 512]),
                        ("a_ka_scale", [1, 512]), ("a_r_k", [1, 512]), ("a_gn_g", [1, 512]), ("a_gn_b", [1, 512]),
                        ("b_gate_w2", [16, 256]), ("b_gate_b", [1, 256]), ("b_norm_g", [1, 512]),
                        ("w_out_even", [D, D]), ("w_in_odd", [D, ODD_COLS]), ("c_ik_ln_g", [1, 64]),
                        ("c_ik_ln_b", [1, 64]), ("w_out_odd", [D, D]), ("ln1_g", [2, D]), ("ln1_b", [2, D]),
                        ("ln2_g", [2, D]), ("ln2_b", [2, D]), ("router_w", [D, NE]), ("router_bias", [1, NE]),
                        ("exp_w_gate", [2, NE, D, DE]), ("exp_w_up", [2, NE, D, DE]), ("exp_w_down", [2, NE, DE, D])):
        I[name] = din(name, shape)
    hc = host_consts(T)
    hc.update(rope_consts(T))
    for k, v in hc.items():
        I["c_" + k] = din("c_" + k, list(v.shape), F32 if v.dtype == np.float32 else BF16)
    C.I = I
    out = dscr("out", [T, D], out=True)
    C.XT0 = dscr("XT0", [D, T + 1], BF16)
    C.XT0_tok = [Tok() for _ in range(NS)]
    C.XT0_z = Tok()
    C.YT = dscr("YT", [D, T], BF16)
    C.YT_tok = [[Tok() for _ in range(NS)] for _ in range(2)]
    C.H0 = dscr("H0", [T, D])
    C.H0_tok = [Tok() for _ in range(NT)]
    C.HT0 = dscr("HT0", [D, T], BF16)
    C.HT0_tok = [Tok() for _ in range(NS)]
    C.X1 = dscr("X1", [T, D])
    C.X1_tok = [Tok() for _ in range(NT)]
    C.XT1 = dscr("XT1", [D, T], BF16)
    C.XT1_tok = [Tok() for _ in range(NS)]

    for nm, shp in (("dbg_ya", [T, 512]), ("dbg_yb", [T, 512]), ("dbg_dsa", [T, 1024]), ("dbg_mask", [T, T]), ("dbg_sc", [T, T])):
        if nm in dbg:
            C.dbg[nm] = dscr(nm, shp, out=True)
    C.WGU16 = [dscr("WGU16_%d" % l, [NE, 128, 2 * 8 * DE], BF16) for l in range(2)]
    C.WGU16_tok = [[Tok() for _ in range(NE)] for l in range(2)]
    C.WD16 = [dscr("WD16_%d" % l, [128, 32 * D], BF16) for l in range(2)]
    C.WD16_tok = [Tok() for l in range(2)]
    C.banks = []
    for b in range(8):
        t = es.enter_context(nc.psum_tensor("psb%d" % b, [128, 512], F32))
        C.banks.append((t, Tok()))
    C.bi = 0

    C.bank_pool = list(range(8))

    def bank():
        b = C.banks[C.bank_pool[C.bi % len(C.bank_pool)]]
        C.bi += 1
        return b
    C.bank = bank

    C.ident = P.sb([128, 128], F32, "ident")
    C.ident_tok = Tok()
    P.dma("sp", C.ident[:], I["c_ident"], w=[C.ident_tok])

    C.cast_todo = {}
    if "M0" in stages:
        cast_weights(C, 0)
    if "A" in stages:
        phase_A(C)
    if "R" in stages:
        phase_rwkv(C)
    if "G" in stages:
        phase_gla(C)
    if "M1" in stages:
        cast_weights(C, 1)
    if "O0" in stages:
        phase_outproj(C, I["w_out_even"], C.YT, lambda s: [C.YT_tok[0][s], C.YT_tok[1][s]], I["x"], lambda i: [],
                      I["ln1_g"][0:1, :], I["ln1_b"][0:1, :], C.H0, C.H0_tok, C.HT0, C.HT0_tok)
    if "M0" in stages:
        cast_some(C, 0, 999)
        phase_moe(C, 0, C.H0, C.H0_tok, C.HT0, C.HT0_tok, C.X1, C.X1_tok, C.XT1, C.XT1_tok)
    if "S1" in stages:
        phase_dsa(C)
    if "O1" in stages:
        C.H1 = dscr("H1", [T, D])
        C.H1_tok = [Tok() for _ in range(NT)]
        C.HT1 = dscr("HT1", [D, T], BF16)
        C.HT1_tok = [Tok() for _ in range(NS)]
        phase_outproj(C, I["w_out_odd"], C.YT, lambda s: [C.YT_tok[0][s], C.YT_tok[1][s]], C.X1, lambda i: [C.X1_tok[i]],
                      I["ln1_g"][1:2, :], I["ln1_b"][1:2, :], C.H1, C.H1_tok, C.HT1, C.HT1_tok)
    if "M1" in stages:
        cast_some(C, 1, 999)
        out_tok = [Tok() for _ in range(NT)]
        phase_moe(C, 1, C.H1, C.H1_tok, C.HT1, C.HT1_tok, out, out_tok, None, None)
    P.finish()
    es.close()
    return nc


def XTv(ap):
    return ap.rearrange("(c p) t -> p c t", p=128)


def cast_weights(C, l):
    P, I = C.P, C.I
    th = []
    for e in range(NE):
        dst = C.WGU16[l][e].rearrange("p (t c f) -> p t c f", t=2, c=8)
        th.append(lambda e=e, dst=dst: P.dma("pool", dst[:, 0, :, :], I["exp_w_gate"][l, e].rearrange("(c p) f -> p c f", p=128), w=[C.WGU16_tok[l][e]]))
        th.append(lambda e=e, dst=dst: P.dma("pool", dst[:, 1, :, :], I["exp_w_up"][l, e].rearrange("(c p) f -> p c f", p=128), w=[C.WGU16_tok[l][e]]))
    wd_flat = I["exp_w_down"][l].rearrange("e f d -> (e f) d")
    dstd = C.WD16[l].rearrange("p (c d) -> p c d", c=32)
    for c4 in range(8):
        th.append(lambda c4=c4: P.dma("pool", dstd[:, c4 * 4:(c4 + 1) * 4, :], wd_flat[c4 * 512:(c4 + 1) * 512, :].rearrange("(c p) d -> p c d", p=128), w=[C.WD16_tok[l]]))
    C.cast_todo[l] = th


def cast_some(C, l, n):
    th = C.cast_todo.get(l, [])
    for _ in range(min(n, len(th))):
        th.pop(0)()


def phase_A(C):
    nc, P, T, I = C.nc, C.P, C.T, C.I
    with ExitStack() as es:
        xin = [P.sb([128, D], F32, "xin", es) for _ in range(2)]
        xin_tok = [Tok(), Tok()]
        st = [P.sb([128, 8, 512], BF16, "ast", es) for _ in range(2)]
        st_tok = [Tok(), Tok()]
        z = P.sb([128, 8, 1], BF16, "zc", es)
        zt = Tok()
        P.op("dve", lambda e: e.memset(z[:], 0.0), w=[zt])
        P.dma("sp", XTv(C.XT0)[:, :, 0:1], z[:], r=[zt], w=[C.XT0_z], allow_slow_non_contiguous=True)
        for s in range(C.NS):
            sb_ = st[s % 2]
            for j in range(4):
                i = s * 4 + j
                xb = xin[i % 2]
                xt = xin_tok[i % 2]
                P.dma("sp", xb[:], I["x"][i * 128:(i + 1) * 128, :], w=[xt])
                for half in range(2):
                    bt, bk = C.bank()
                    for c4 in range(4):
                        c = half * 4 + c4
                        P.op("pe", lambda e: e.transpose(bt[:, c4 * 128:(c4 + 1) * 128], xb[:, c * 128:(c + 1) * 128], C.ident[:]),
                             r=[xt, C.ident_tok], w=[bk], inc=(c4 == 3))
                    en = "act" if half == 0 else "dve"
                    src = bt[:, :].rearrange("p (c t) -> p c t", c=4)
                    dst = sb_[:, half * 4:(half + 1) * 4, j * 128:(j + 1) * 128]
                    if en == "act":
                        P.op("act", lambda e: e.copy(dst, src), r=[bk], w=[st_tok[s % 2]])
                    else:
                        P.op("dve", lambda e: e.tensor_copy(dst, src), r=[bk], w=[st_tok[s % 2]])
            P.dma("sp", XTv(C.XT0)[:, :, 1 + s * 512:1 + (s + 1) * 512], sb_[:], r=[st_tok[s % 2]], w=[C.XT0_tok[s]])
        P.barrier()


class TL:
    def __init__(self, t, k=None):
        self.t = t
        self.k = k or Tok()

    def __getitem__(self, idx):
        return self.t[idx]


def mk(P, shape, dt=F32, name=None, es=None):
    return TL(P.sb(shape, dt, name, es))


def bcast_load(C, dst_ap, src_row, np_, tok, q="sp"):
    C.P.dma(q, dst_ap, src_row.partition_broadcast(np_), w=[tok])


def hv(ap, h):
    return ap.rearrange("p (h v) -> p h v", h=h)


def phase_rwkv(C):
    nc, P, T, I = C.nc, C.P, C.T, C.I
    mm = ALU.mult
    with ExitStack() as es:
        W1 = mk(P, [128, 8, A_COLS], BF16, "W1", es)
        W2 = mk(P, [128, 8, A_COLS], BF16, "W2", es)
        with ExitStack() as es2:
            mub = mk(P, [128, A_COLS], F32, "mub", es2)
            omu = mk(P, [128, A_COLS], F32, "omu", es2)
            stg = [mk(P, [128, A_COLS], F32, "wstg", es2) for _ in range(2)]
            bcast_load(C, mub[:], I["a_mu"], 128, mub.k)
            P.op("dve", lambda e: e.tensor_scalar(omu[:], mub[:], -1.0, 1.0, ALU.mult, ALU.add), r=[mub.k], w=[omu.k])
            for c in range(8):
                s_ = stg[c % 2]
                P.dma("sp", s_[:], I["w_in_even"][c * 128:(c + 1) * 128, 0:A_COLS], w=[s_.k])
                P.op("dve", lambda e: e.tensor_tensor(W1[:, c, :], s_[:], omu[:], mm), r=[s_.k, omu.k], w=[W1.k])
                P.op("pool", lambda e: e.tensor_tensor(W2[:, c, :], s_[:], mub[:], mm), r=[s_.k, mub.k], w=[W2.k])
            P.barrier()
        LW = mk(P, [128, 512], BF16, "LW", es)
        G2 = mk(P, [128, 512], BF16, "G2", es)
        P.dma("pool", LW[0:64, :], I["a_w2"], w=[LW.k])
        P.dma("pool", LW[64:128, :], I["a_a2"], w=[LW.k])
        P.dma("pool", G2[:], I["a_g2"], w=[G2.k])
        BV = mk(P, [64, 7, 512], F32, "BV", es)
        for n, name in enumerate(("a_w0", "a_a0", "a_kk_scale", "a_ka_scale", "a_r_k", "a_gn_g", "a_gn_b")):
            bcast_load(C, BV[:, n, :], I[name], 64, BV.k)
        w0b, a0b, kksb, kab, rkb, gngb, gnbb = [BV[:, n, :] for n in range(7)]
        tri = mk(P, [64, 3, 64], F32, "tri", es)
        ncol = mk(P, [64, 1], F32, "ncol", es)
        mMA = mk(P, [64, 8, 128], F32, "mMA", es)
        mBB = mk(P, [64, 8, 128], F32, "mBB", es)
        mNT = mk(P, [64, 8, 64], F32, "mNT", es)
        id8 = mk(P, [64, 8, 64], F32, "id8", es)
        for tl, nm in ((tri, "c_tri64"), (ncol, "c_ncol64"), (mMA, "c_mMA"), (mBB, "c_mBB"), (mNT, "c_mNT"), (id8, "c_id8")):
            P.dma("sp", tl[:], I[nm], w=[tl.k])
        id64 = C.ident[0:64, 0:64]

        def wt(name, shape=(64, 512), dt=F32):
            return mk(P, list(shape), dt, name, es)
        ATs = [mk(P, [128, 8, 513], BF16, "ATs", es) for _ in range(2)]
        TX = wt("TX", (128, 512), BF16)
        SG = wt("SG", (128, 512), BF16)
        r_, k_, v_, sg, a_, kk, be, Bi, Ki, tmp = [wt(n) for n in ("r", "k", "v", "sg", "a", "kk", "be", "Bi", "Ki", "tmp")]
        Ep, Em, Ex, Ee = [wt(n) for n in ("Ep", "Em", "Ex", "Ee")]
        s8 = [wt("s8_%d" % n, (64, 8)) for n in range(4)]
        KRs = [wt("KR", (64, 8, 128), BF16) for _ in range(2)]
        BiT = wt("BiT", (64, 8, 64), BF16)
        KiT = wt("KiT", (64, 8, 64), BF16)
        MAs = [wt("MA", (64, 8, 128), BF16) for _ in range(2)]
        BBs = [wt("BB", (64, 8, 128), BF16) for _ in range(2)]
        Xb = [wt("X%d" % n, (64, 8, 64), BF16) for n in range(2)]
        XTb = [wt("XT%d" % n, (64, 8, 64), BF16) for n in range(2)]
        Qbs = [[wt("Q%d" % n, (64, 8, 64), BF16) for n in range(2)] for _ in range(2)]
        Xs = wt("Xs", (64, 8, 64), BF16)
        nU = wt("nU", (64, 8, 64), BF16)
        Y = wt("Y")
        tmpb = wt("tmpb")
        Hs = [wt("H%d" % n, (64, 8, 64)) for n in range(2)]
        Hbs = [wt("Hb%d" % n, (64, 8, 64), BF16) for n in range(2)]
        vbs = [wt("vb", (64, 512), BF16) for _ in range(2)]
        Ke16s = [wt("Ke16", (64, 512), BF16) for _ in range(2)]
        Be16s = [wt("Be16", (64, 512), BF16) for _ in range(2)]
        PCs = [wt("PC", (64, 8)) for _ in range(2)]
        gs_ = [wt("g", (64, 512)) for _ in range(2)]
        bonuss = [wt("bonus", (64, 512)) for _ in range(2)]
        P.op("pool", lambda e: e.memset(Hbs[0][:], 0.0), w=[Hbs[0].k])
        yst = [mk(P, [128, 4, 512], BF16, "yst", es) for _ in range(2)]
        P.op("dve", lambda e: e.memset(Hs[0][:], 0.0), w=[Hs[0].k])

        def psb():
            t, k = C.bank()
            return TL(t, k)

        def v3(tl_or_ap, h=8):
            return hv(tl_or_ap, h)

        def chunk(s, ci):
          at = ATs[s % 2]
          ys = yst[s % 2]
          cast_some(C, 0, 1)
          if ci == 0:
            rd = [C.XT0_tok[s]] + ([C.XT0_tok[s - 1]] if s > 0 else [C.XT0_z])
            P.dma("sp", at[:], XTv(C.XT0)[:, :, s * 512:s * 512 + 513], r=rd, w=[at.k])
            for which in range(2):
                pb = psb()
                c0 = 1536 + which * 128
                for c in range(8):
                    P.op("pe", lambda e: e.matmul(pb[:, :], lhsT=W1[:, c, c0:c0 + 128], rhs=at[:, c, 1:513], start=(c == 0), stop=False),
                         r=[W1.k, at.k], w=[pb.k], inc=False)
                for c in range(8):
                    P.op("pe", lambda e: e.matmul(pb[:, :], lhsT=W2[:, c, c0:c0 + 128], rhs=at[:, c, 0:512], start=False, stop=(c == 7)),
                         r=[W2.k, at.k], w=[pb.k], inc=(c == 7))
                if which == 0:
                    P.op("act", lambda e: e.activation(out=TX[0:64, :], in_=pb[0:64, :], func=AF.Tanh), r=[pb.k], w=[TX.k])
                    P.op("act", lambda e: e.copy(TX[64:128, :], pb[64:128, :]), r=[pb.k], w=[TX.k])
                else:
                    P.op("act", lambda e: e.activation(out=SG[:, :], in_=pb[:, :], func=AF.Sigmoid), r=[pb.k], w=[SG.k])
          if True:
            if True:
                g = s * 8 + ci
                t0 = ci * 64
                KR, MA, BB, Qb = KRs[g % 2], MAs[g % 2], BBs[g % 2], Qbs[g % 2]
                vb, Ke16, Be16, PC, g_, bonus = vbs[g % 2], Ke16s[g % 2], Be16s[g % 2], PCs[g % 2], gs_[g % 2], bonuss[g % 2]
                pr, pk, pv = psb(), psb(), psb()
                for pb, c0 in ((pr, 0), (pk, 512), (pv, 1024)):
                    for c in range(8):
                        P.op("pe", lambda e: e.matmul(pb[0:64, :], lhsT=at[:, c, 1 + t0:1 + t0 + 64], rhs=W1[:, c, c0:c0 + 512], start=(c == 0), stop=False),
                             r=[W1.k, at.k], w=[pb.k], inc=False)
                    for c in range(8):
                        P.op("pe", lambda e: e.matmul(pb[0:64, :], lhsT=at[:, c, t0:t0 + 64], rhs=W2[:, c, c0:c0 + 512], start=False, stop=(c == 7)),
                             r=[W2.k, at.k], w=[pb.k], inc=(c == 7))
                yield "F"
                pz, pza, pg = psb(), psb(), psb()
                P.op("pe", lambda e: e.matmul(pz[0:64, :], lhsT=TX[0:64, t0:t0 + 64], rhs=LW[0:64, :], start=True, stop=True), r=[TX.k, LW.k], w=[pz.k])
                P.op("pe", lambda e: e.matmul(pza[0:64, :], lhsT=TX[64:128, t0:t0 + 64], rhs=LW[64:128, :], start=True, stop=True), r=[TX.k, LW.k], w=[pza.k])
                P.op("pe", lambda e: e.matmul(pg[0:64, :], lhsT=SG[:, t0:t0 + 64], rhs=G2[:, :], start=True, stop=True), r=[SG.k, G2.k], w=[pg.k])
                P.op("act", lambda e: e.copy(r_[:], pr[0:64, :]), r=[pr.k], w=[r_.k])
                P.op("act", lambda e: e.copy(v_[:], pv[0:64, :]), r=[pv.k], w=[v_.k])
                P.op("act", lambda e: e.copy(vb[:], pv[0:64, :]), r=[pv.k], w=[vb.k])
                P.op("act", lambda e: e.copy(g_[:], pg[0:64, :]), r=[pg.k], w=[g_.k])
                P.op("dve", lambda e: e.tensor_copy(k_[:], pk[0:64, :]), r=[pk.k], w=[k_.k])
                P.op("dve", lambda e: e.tensor_tensor(sg[:], pz[0:64, :], w0b, ALU.add), r=[pz.k, BV.k], w=[sg.k])
                P.op("act", lambda e: e.activation(out=sg[:], in_=sg[:], func=AF.Sigmoid), r=[sg.k], w=[sg.k])
                P.op("dve", lambda e: e.tensor_tensor(a_[:], pza[0:64, :], a0b, ALU.add), r=[pza.k, BV.k], w=[a_.k])
                P.op("act", lambda e: e.activation(out=a_[:], in_=a_[:], func=AF.Sigmoid), r=[a_.k], w=[a_.k])
                yield "F"
                P.op("pool", lambda e: e.tensor_tensor(kk[:], k_[:], kksb, mm), r=[k_.k, BV.k], w=[kk.k])
                P.op("pool", lambda e: e.tensor_tensor(tmp[:], kk[:], kk[:], mm), r=[kk.k], w=[tmp.k])
                P.op("dve", lambda e: e.tensor_reduce(s8[0][:], v3(tmp[:]), AX.X, ALU.add), r=[tmp.k], w=[s8[0].k])
                P.op("dve", lambda e: e.tensor_scalar(s8[0][:], s8[0][:], 1e-24, None, ALU.max), r=[s8[0].k], w=[s8[0].k])
                P.op("act", lambda e: e.activation(out=s8[0][:], in_=s8[0][:], func=AF.Sqrt), r=[s8[0].k], w=[s8[0].k])
                P.op("dve", lambda e: e.reciprocal(s8[0][:], s8[0][:]), r=[s8[0].k], w=[s8[0].k])
                P.op("dve", lambda e: e.tensor_tensor(v3(kk[:]), v3(kk[:]), s8[0][:, :].unsqueeze(2).to_broadcast([64, 8, 64]), mm),
                     r=[kk.k, s8[0].k], w=[kk.k])
                P.op("pool", lambda e: e.tensor_tensor(be[:], kk[:], a_[:], mm), r=[kk.k, a_.k], w=[be.k])
                P.op("dve", lambda e: e.scalar_tensor_tensor(tmp[:], a_[:], -1.0, kab, ALU.add, mm), r=[a_.k, BV.k], w=[tmp.k])
                P.op("dve", lambda e: e.scalar_tensor_tensor(k_[:], tmp[:], 1.0, k_[:], ALU.add, mm), r=[tmp.k, k_.k], w=[k_.k])
                P.op("pool", lambda e: e.tensor_tensor(tmp[:], r_[:], k_[:], mm), r=[r_.k, k_.k], w=[tmp.k])
                P.op("pool", lambda e: e.tensor_tensor(tmp[:], tmp[:], rkb, mm), r=[tmp.k, BV.k], w=[tmp.k])
                P.op("dve", lambda e: e.tensor_reduce(s8[1][:], v3(tmp[:]), AX.X, ALU.add), r=[tmp.k], w=[s8[1].k])
                P.op("dve", lambda e: e.tensor_tensor(v3(bonus[:]), v3(v_[:]), s8[1][:, :].unsqueeze(2).to_broadcast([64, 8, 64]), mm),
                     r=[v_.k, s8[1].k], w=[bonus.k])
                yield "F"
                pcl, pcx, pca, ppc = psb(), psb(), psb(), psb()
                for pb, n in ((pcl, 0), (pcx, 1), (pca, 2)):
                    P.op("pe", lambda e: e.matmul(pb[0:64, :], lhsT=tri[:, n, :], rhs=sg[:], start=True, stop=True), r=[tri.k, sg.k], w=[pb.k])
                for h in range(8):
                    P.op("pe", lambda e: e.matmul(ppc[0:64, h:h + 1], lhsT=sg[:, h * 64:(h + 1) * 64], rhs=ncol[:], start=True, stop=True),
                         r=[sg.k, ncol.k], w=[ppc.k], inc=(h == 7))
                P.op("act", lambda e: e.activation(out=Ep[:], in_=pcl[0:64, :], func=AF.Exp), r=[pcl.k], w=[Ep.k])
                P.op("act", lambda e: e.activation(out=Em[:], in_=pcl[0:64, :], func=AF.Exp, scale=-1.0), r=[pcl.k], w=[Em.k])
                P.op("act", lambda e: e.activation(out=Ex[:], in_=pcx[0:64, :], func=AF.Exp), r=[pcx.k], w=[Ex.k])
                P.op("act", lambda e: e.activation(out=Ee[:], in_=pca[0:64, :], func=AF.Exp), r=[pca.k], w=[Ee.k])
                P.op("act", lambda e: e.activation(out=PC[:], in_=ppc[0:64, 0:8], func=AF.Exp), r=[ppc.k], w=[PC.k])
                P.op("dve", lambda e: e.tensor_tensor(r_[:], r_[:], Ep[:], mm), r=[r_.k, Ep.k], w=[r_.k])
                P.op("pool", lambda e: e.tensor_tensor(kk[:], kk[:], Ex[:], mm), r=[kk.k, Ex.k], w=[kk.k])
                P.op("dve", lambda e: e.tensor_tensor(Bi[:], be[:], Em[:], mm), r=[be.k, Em.k], w=[Bi.k])
                P.op("pool", lambda e: e.tensor_tensor(Ki[:], k_[:], Em[:], mm), r=[k_.k, Em.k], w=[Ki.k])
                P.op("dve", lambda e: e.tensor_tensor(Ke16[:], k_[:], Ee[:], mm), r=[k_.k, Ee.k], w=[Ke16.k])
                P.op("pool", lambda e: e.tensor_tensor(Be16[:], be[:], Ee[:], mm), r=[be.k, Ee.k], w=[Be16.k])
                yield "F"
                for src, dst, off, en in ((kk, KR, 0, "act"), (r_, KR, 64, "dve"), (Bi, BiT, 0, "act"), (Ki, KiT, 0, "dve")):
                    pb = psb()
                    for h in range(8):
                        P.op("pe", lambda e: e.transpose(pb[0:64, h * 64:(h + 1) * 64], src[:, h * 64:(h + 1) * 64], id64),
                             r=[src.k, C.ident_tok], w=[pb.k], inc=(h == 7))
                    d_ = dst[:, :, off:off + 64]
                    s_ = v3(pb[0:64, :])
                    if en == "act":
                        P.op("act", lambda e: e.copy(d_, s_), r=[pb.k], w=[dst.k])
                    else:
                        P.op("dve", lambda e: e.tensor_copy(d_, s_), r=[pb.k], w=[dst.k])
                yield "F"
                pma = [psb(), psb()]
                pbb = [psb(), psb()]
                pnt = psb()
                for h in range(8):
                    hb, hh = h // 4, h % 4
                    P.op("pe", lambda e: e.matmul(pma[hb][0:64, hh * 128:(hh + 1) * 128], lhsT=BiT[:, h, :], rhs=KR[:, h, :], start=True, stop=True),
                         r=[BiT.k, KR.k], w=[pma[hb].k], inc=(hh == 3))
                for h in range(8):
                    hb, hh = h // 4, h % 4
                    P.op("pe", lambda e: e.matmul(pbb[hb][0:64, hh * 128:(hh + 1) * 128], lhsT=KiT[:, h, :], rhs=KR[:, h, :], start=True, stop=True),
                         r=[KiT.k, KR.k], w=[pbb[hb].k], inc=(hh == 3))
                for h in range(8):
                    P.op("pe", lambda e: e.matmul(pnt[0:64, h * 64:(h + 1) * 64], lhsT=KR[:, h, 0:64], rhs=BiT[:, h, :], start=True, stop=True),
                         r=[BiT.k, KR.k], w=[pnt.k], inc=(h == 7))
                for hb in range(2):
                    P.op("dve", lambda e: e.tensor_tensor(MA[:, hb * 4:(hb + 1) * 4, :], hv(pma[hb][0:64, :], 4), mMA[:, hb * 4:(hb + 1) * 4, :], mm),
                         r=[pma[hb].k, mMA.k], w=[MA.k])
                    P.op("dve", lambda e: e.tensor_tensor(BB[:, hb * 4:(hb + 1) * 4, :], hv(pbb[hb][0:64, :], 4), mBB[:, hb * 4:(hb + 1) * 4, :], mm),
                         r=[pbb[hb].k, mBB.k], w=[BB.k])
                X, XT, Q = Xb[0], XTb[0], Qb[0]
                P.op("dve", lambda e: e.tensor_tensor(XT[:], v3(pnt[0:64, :]), mNT[:], mm), r=[pnt.k, mNT.k], w=[XT.k])
                P.op("pool", lambda e: e.tensor_copy(X[:], MA[:, :, 0:64]), r=[MA.k], w=[X.k])
                P.op("pool", lambda e: e.tensor_tensor(Q[:], MA[:, :, 0:64], id8[:], ALU.add), r=[MA.k, id8.k], w=[Q.k])
                for lvl in range(5):
                    Xn, XTn, Qn = Xb[(lvl + 1) % 2], XTb[(lvl + 1) % 2], Qb[(lvl + 1) % 2]
                    pxt = psb()
                    for h in range(8):
                        P.op("pe", lambda e: e.matmul(pxt[0:64, h * 64:(h + 1) * 64], lhsT=X[:, h, :], rhs=XT[:, h, :], start=True, stop=True),
                             r=[X.k, XT.k], w=[pxt.k], inc=(h == 7))
                    if lvl < 4:
                        px = psb()
                        for h in range(8):
                            P.op("pe", lambda e: e.matmul(px[0:64, h * 64:(h + 1) * 64], lhsT=XT[:, h, :], rhs=X[:, h, :], start=True, stop=True),
                                 r=[X.k, XT.k], w=[px.k], inc=(h == 7))
                    P.op("act", lambda e: e.copy(XTn[:], v3(pxt[0:64, :])), r=[pxt.k], w=[XTn.k])
                    if lvl < 4:
                        P.op("dve", lambda e: e.tensor_copy(Xn[:], v3(px[0:64, :])), r=[px.k], w=[Xn.k])
                    pq = psb()
                    for h in range(8):
                        P.op("pe", lambda e: e.matmul(pq[0:64, h * 64:(h + 1) * 64], lhsT=XTn[:, h, :], rhs=Q[:, h, :], start=True, stop=True),
                             r=[XTn.k, Q.k], w=[pq.k], inc=(h == 7))
                    P.op("dve", lambda e: e.tensor_tensor(Qn[:], Q[:], v3(pq[0:64, :]), ALU.add), r=[Q.k, pq.k], w=[Qn.k])
                    X, XT, Q = Xn, XTn, Qn
                    yield "F"
                yield "END_FRONT"
                H, Hn = Hs[g % 2], Hs[(g + 1) % 2]
                Hb, Hbn = Hbs[g % 2], Hbs[(g + 1) % 2]
                pxs = psb()
                for h in range(8):
                    P.op("pe", lambda e: e.matmul(pxs[0:64, h * 64:(h + 1) * 64], lhsT=KR[:, h, 0:64], rhs=Hb[:, h, :], start=True, stop=False),
                         r=[KR.k, Hb.k], w=[pxs.k], inc=False)
                    P.op("pe", lambda e: e.matmul(pxs[0:64, h * 64:(h + 1) * 64], lhsT=BB[:, h, 0:64], rhs=vb[:, h * 64:(h + 1) * 64], start=False, stop=True),
                         r=[BB.k, vb.k], w=[pxs.k], inc=(h == 7))
                P.op("act", lambda e: e.copy(Xs[:], v3(pxs[0:64, :])), r=[pxs.k], w=[Xs.k])
                yield "B"
                pu = psb()
                for h in range(8):
                    P.op("pe", lambda e: e.matmul(pu[0:64, h * 64:(h + 1) * 64], lhsT=Q[:, h, :], rhs=Xs[:, h, :], start=True, stop=True),
                         r=[Q.k, Xs.k], w=[pu.k], inc=(h == 7))
                P.op("act", lambda e: e.mul(nU[:], v3(pu[0:64, :]), -1.0), r=[pu.k], w=[nU.k])
                yield "B"
                py, ph = psb(), psb()
                for h in range(8):
                    sl = slice(h * 64, (h + 1) * 64)
                    P.op("pe", lambda e: e.matmul(py[0:64, sl], lhsT=KR[:, h, 64:128], rhs=Hb[:, h, :], start=True, stop=False), r=[KR.k, Hb.k], w=[py.k], inc=False)
                    P.op("pe", lambda e: e.matmul(py[0:64, sl], lhsT=BB[:, h, 64:128], rhs=vb[:, sl], start=False, stop=False), r=[BB.k, vb.k], w=[py.k], inc=False)
                    P.op("pe", lambda e: e.matmul(py[0:64, sl], lhsT=MA[:, h, 64:128], rhs=nU[:, h, :], start=False, stop=True), r=[MA.k, nU.k], w=[py.k], inc=(h == 7))
                for h in range(8):
                    sl = slice(h * 64, (h + 1) * 64)
                    P.op("pe", lambda e: e.matmul(ph[0:64, sl], lhsT=Ke16[:, sl], rhs=vb[:, sl], start=True, stop=False), r=[Ke16.k, vb.k], w=[ph.k], inc=False)
                    P.op("pe", lambda e: e.matmul(ph[0:64, sl], lhsT=Be16[:, sl], rhs=nU[:, h, :], start=False, stop=True), r=[Be16.k, nU.k], w=[ph.k], inc=(h == 7))
                P.op("pool", lambda e: e.tensor_tensor(Hn[:], H[:], PC[:, :].unsqueeze(2).to_broadcast([64, 8, 64]), mm), r=[H.k, PC.k], w=[Hn.k])
                P.op("dve", lambda e: e.tensor_tensor(Hn[:], Hn[:], v3(ph[0:64, :]), ALU.add), r=[Hn.k, ph.k], w=[Hn.k])
                P.op("act", lambda e: e.copy(Hbn[:], Hn[:]), r=[Hn.k], w=[Hbn.k])
                yield "B"
                P.op("act", lambda e: e.copy(Y[:], py[0:64, :]), r=[py.k], w=[Y.k])
                P.op("dve", lambda e: e.tensor_reduce(s8[2][:], v3(Y[:]), AX.X, ALU.add), r=[Y.k], w=[s8[2].k])
                P.op("dve", lambda e: e.tensor_scalar(s8[2][:], s8[2][:], 1.0 / 64, None, mm), r=[s8[2].k], w=[s8[2].k])
                P.op("dve", lambda e: e.tensor_tensor(v3(Y[:]), v3(Y[:]), s8[2][:, :].unsqueeze(2).to_broadcast([64, 8, 64]), ALU.subtract),
                     r=[Y.k, s8[2].k], w=[Y.k])
                yield "B"
                P.op("pool", lambda e: e.tensor_tensor(tmpb[:], Y[:], Y[:], mm), r=[Y.k], w=[tmpb.k])
                P.op("dve", lambda e: e.tensor_reduce(s8[3][:], v3(tmpb[:]), AX.X, ALU.add), r=[tmpb.k], w=[s8[3].k])
                P.op("act", lambda e: e.activation(out=s8[3][:], in_=s8[3][:], func=AF.Sqrt, bias=64e-5, scale=1.0 / 64), r=[s8[3].k], w=[s8[3].k])
                P.op("dve", lambda e: e.reciprocal(s8[3][:], s8[3][:]), r=[s8[3].k], w=[s8[3].k])
                P.op("dve", lambda e: e.tensor_tensor(v3(Y[:]), v3(Y[:]), s8[3][:, :].unsqueeze(2).to_broadcast([64, 8, 64]), mm),
                     r=[Y.k, s8[3].k], w=[Y.k])
                P.op("pool", lambda e: e.tensor_tensor(Y[:], Y[:], gngb, mm), r=[Y.k, BV.k], w=[Y.k])
                P.op("pool", lambda e: e.tensor_tensor(Y[:], Y[:], gnbb, ALU.add), r=[Y.k, BV.k], w=[Y.k])
                P.op("dve", lambda e: e.tensor_tensor(Y[:], Y[:], bonus[:], ALU.add), r=[Y.k, bonus.k], w=[Y.k])
                P.op("dve", lambda e: e.tensor_tensor(Y[:], Y[:], g_[:], mm), r=[Y.k, g_.k], w=[Y.k])
                if "dbg_ya" in C.dbg:
                    P.dma("sp", C.dbg["dbg_ya"][g * 64:(g + 1) * 64, :], Y[:], r=[Y.k])
                yield "B"
                pb = psb()
                for q in range(4):
                    P.op("pe", lambda e: e.transpose(pb[:, q * 64:(q + 1) * 64], Y[:, q * 128:(q + 1) * 128], id64), r=[Y.k, C.ident_tok], w=[pb.k], inc=(q == 3))
                P.op("act", lambda e: e.copy(ys[:, :, t0:t0 + 64], hv(pb[:, 0:256], 4)), r=[pb.k], w=[ys.k])
                if ci == 7:
                    P.dma("sp", XTv(C.YT)[:, 0:4, s * 512:(s + 1) * 512], ys[:], r=[ys.k], w=[C.YT_tok[0][s]])

        makers = [(lambda s=s, ci=ci: chunk(s, ci)) for s in range(C.NS) for ci in range(8)]
        if RWKV_INTERLEAVE:
            pipeline2(makers, interleave=True)
        else:
            def to_end_front(g_):
                while next(g_) != "END_FRONT":
                    pass
            g = makers[0]()
            to_end_front(g)
            for n in range(len(makers)):
                for _ in range(3):
                    next(g)
                g2 = None
                if n + 1 < len(makers):
                    g2 = makers[n + 1]()
                    next(g2)
                for _ in g:
                    pass
                if g2 is not None:
                    to_end_front(g2)
                g = g2
        P.barrier()


def pipeline2(makers, interleave=True):
    if not interleave:
        for mk_ in makers:
            for _ in mk_():
                pass
        return
    prevB = None
    for mk_ in makers:
        g = mk_()
        while True:
            r = next(g)
            if prevB is not None:
                try:
                    next(prevB)
                except StopIteration:
                    prevB = None
            if r == "END_FRONT":
                break
        if prevB is not None:
            for _ in prevB:
                pass
        prevB = g
    if prevB is not None:
        for _ in prevB:
            pass


def psb(C):
    t, k = C.bank()
    return TL(t, k)


def phase_gla(C):
    nc, P, T, I = C.nc, C.P, C.T, C.I
    mm = ALU.mult
    with ExitStack() as es:
        WB = mk(P, [128, 8, B_COLS], BF16, "WB", es)
        for c in range(8):
            P.dma("pool", WB[:, c, :], I["w_in_even"][c * 128:(c + 1) * 128, A_COLS:EVEN_COLS], w=[WB.k])
        GW2 = mk(P, [16, 256], BF16, "GW2", es)
        P.dma("pool", GW2[:], I["b_gate_w2"], w=[GW2.k])
        gbb = mk(P, [128, 256], F32, "gbb", es)
        ngb = mk(P, [128, 512], F32, "ngb", es)
        bcast_load(C, gbb[:], I["b_gate_b"], 128, gbb.k)
        bcast_load(C, ngb[:], I["b_norm_g"], 128, ngb.k)
        tri = mk(P, [128, 2, 128], F32, "tri128", es)
        ncol = mk(P, [128, 1], F32, "ncol128", es)
        iu = mk(P, [128, 4, 128], F32, "iu128", es)
        for tl, nm in ((tri, "c_tri128"), (ncol, "c_ncol128"), (iu, "c_iu128")):
            P.dma("sp", tl[:], I[nm], w=[tl.k])
        id64 = C.ident[0:64, 0:64]

        def wt(name, shape, dt=F32):
            return mk(P, list(shape), dt, name, es)
        ATs = [wt("ATg", (128, 8, 512), BF16) for _ in range(2)]
        AL = wt("AL", (16, 512), BF16)
        l_ = wt("l", (128, 256))
        Eq, Ei, Ee = wt("Eq", (128, 256)), wt("Ei", (128, 256)), wt("Ee", (128, 256))
        PCg = wt("PCg", (64, 4))
        qd, ki, ke = wt("qd", (128, 256)), wt("ki", (128, 256)), wt("ke", (128, 256))
        v_ = wt("vg", (128, 512))
        qdT, kiT = wt("qdT", (64, 4, 128)), wt("kiT", (64, 4, 128))
        attT = wt("attT", (128, 4, 128))
        Ss = [wt("S%d" % n, (64, 4, 128)) for n in range(2)]
        o_ = wt("o", (128, 512))
        sq = wt("sqg", (128, 512))
        sl_ = wt("silu", (128, 512))
        m4 = wt("m4", (128, 4))
        yst = [wt("ystg", (128, 4, 512), BF16) for _ in range(2)]
        P.op("dve", lambda e: e.memset(Ss[0][:], 0.0), w=[Ss[0].k])
        for s in range(C.NS):
            at = ATs[s % 2]
            P.dma("sp", at[:], XTv(C.XT0)[:, :, 1 + s * 512:1 + (s + 1) * 512], r=[C.XT0_tok[s]], w=[at.k])
            pb = psb(C)
            for c in range(8):
                P.op("pe", lambda e: e.matmul(pb[0:16, :], lhsT=WB[:, c, 1536:1552], rhs=at[:, c, :], start=(c == 0), stop=(c == 7)),
                     r=[WB.k, at.k], w=[pb.k], inc=(c == 7))
            P.op("act", lambda e: e.copy(AL[:], pb[0:16, :]), r=[pb.k], w=[AL.k])
            ys = yst[s % 2]
            for ci in range(4):
                g = s * 4 + ci
                t0 = ci * 128
                pqk, pv, pg = psb(C), psb(C), psb(C)
                for pb, c0 in ((pqk, 0), (pv, 512), (pg, 1024)):
                    for c in range(8):
                        P.op("pe", lambda e: e.matmul(pb[:, :], lhsT=at[:, c, t0:t0 + 128], rhs=WB[:, c, c0:c0 + 512], start=(c == 0), stop=(c == 7)),
                             r=[WB.k, at.k], w=[pb.k], inc=(c == 7))
                pla = psb(C)
                P.op("pe", lambda e: e.matmul(pla[:, 0:256], lhsT=AL[:, t0:t0 + 128], rhs=GW2[:], start=True, stop=True), r=[AL.k, GW2.k], w=[pla.k])
                P.op("dve", lambda e: e.tensor_tensor(l_[:], pla[:, 0:256], gbb[:], ALU.add), r=[pla.k, gbb.k], w=[l_.k])
                P.op("act", lambda e: e.activation(out=l_[:], in_=l_[:], func=AF.Exp, scale=-1.0), r=[l_.k], w=[l_.k])
                P.op("act", lambda e: e.activation(out=l_[:], in_=l_[:], func=AF.Ln, bias=1.0), r=[l_.k], w=[l_.k])
                pbc, pba, ppc = psb(C), psb(C), psb(C)
                P.op("pe", lambda e: e.matmul(pbc[:, 0:256], lhsT=tri[:, 0, :], rhs=l_[:], start=True, stop=True), r=[tri.k, l_.k], w=[pbc.k])
                P.op("pe", lambda e: e.matmul(pba[:, 0:256], lhsT=tri[:, 1, :], rhs=l_[:], start=True, stop=True), r=[tri.k, l_.k], w=[pba.k])
                for h in range(4):
                    P.op("pe", lambda e: e.matmul(ppc[0:64, h:h + 1], lhsT=l_[:, h * 64:(h + 1) * 64], rhs=ncol[:], start=True, stop=True),
                         r=[l_.k, ncol.k], w=[ppc.k], inc=(h == 3))
                P.op("act", lambda e: e.activation(out=Eq[:], in_=pbc[:, 0:256], func=AF.Exp), r=[pbc.k], w=[Eq.k])
                P.op("act", lambda e: e.activation(out=Ei[:], in_=pbc[:, 0:256], func=AF.Exp, scale=-1.0), r=[pbc.k], w=[Ei.k])
                P.op("act", lambda e: e.activation(out=Ee[:], in_=pba[:, 0:256], func=AF.Exp), r=[pba.k], w=[Ee.k])
                P.op("act", lambda e: e.activation(out=PCg[:], in_=ppc[0:64, 0:4], func=AF.Exp), r=[ppc.k], w=[PCg.k])
                P.op("dve", lambda e: e.scalar_tensor_tensor(qd[:], pqk[:, 0:256], 0.125, Eq[:], mm, mm), r=[pqk.k, Eq.k], w=[qd.k])
                P.op("dve", lambda e: e.tensor_tensor(ki[:], pqk[:, 256:512], Ei[:], mm), r=[pqk.k, Ei.k], w=[ki.k])
                P.op("dve", lambda e: e.tensor_tensor(ke[:], pqk[:, 256:512], Ee[:], mm), r=[pqk.k, Ee.k], w=[ke.k])
                P.op("act", lambda e: e.copy(v_[:], pv[:, :]), r=[pv.k], w=[v_.k])
                P.op("act", lambda e: e.activation(out=sl_[:], in_=pg[:, :], func=AF.Silu), r=[pg.k], w=[sl_.k])
                for src, dst, en in ((qd, qdT, "act"), (ki, kiT, "dve")):
                    pb = psb(C)
                    for h in range(4):
                        P.op("pe", lambda e: e.transpose(pb[0:64, h * 128:(h + 1) * 128], src[:, h * 64:(h + 1) * 64], C.ident[:]),
                             r=[src.k, C.ident_tok], w=[pb.k], inc=(h == 3))
                    if en == "act":
                        P.op("act", lambda e: e.copy(dst[:], hv(pb[0:64, :], 4)), r=[pb.k], w=[dst.k])
                    else:
                        P.op("dve", lambda e: e.tensor_copy(dst[:], hv(pb[0:64, :], 4)), r=[pb.k], w=[dst.k])
                patt = psb(C)
                for h in range(4):
                    P.op("pe", lambda e: e.matmul(patt[:, h * 128:(h + 1) * 128], lhsT=kiT[:, h, :], rhs=qdT[:, h, :], start=True, stop=True),
                         r=[kiT.k, qdT.k], w=[patt.k], inc=(h == 3))
                P.op("dve", lambda e: e.tensor_tensor(attT[:], hv(patt[:, :], 4), iu[:], mm), r=[patt.k, iu.k], w=[attT.k])
                S, Sn = Ss[g % 2], Ss[(g + 1) % 2]
                po, pS = psb(C), psb(C)
                for h in range(4):
                    sl = slice(h * 128, (h + 1) * 128)
                    P.op("pe", lambda e: e.matmul(po[:, sl], lhsT=attT[:, h, :], rhs=v_[:, sl], start=True, stop=False), r=[attT.k, v_.k], w=[po.k], inc=False)
                    P.op("pe", lambda e: e.matmul(po[:, sl], lhsT=qdT[:, h, :], rhs=S[:, h, :], start=False, stop=True), r=[qdT.k, S.k], w=[po.k], inc=(h == 3))
                for h in range(4):
                    sl = slice(h * 128, (h + 1) * 128)
                    P.op("pe", lambda e: e.matmul(pS[0:64, sl], lhsT=ke[:, h * 64:(h + 1) * 64], rhs=v_[:, sl], start=True, stop=True), r=[ke.k, v_.k], w=[pS.k], inc=(h == 3))
                P.op("pool", lambda e: e.tensor_tensor(Sn[:], S[:], PCg[:, :].unsqueeze(2).to_broadcast([64, 4, 128]), mm), r=[S.k, PCg.k], w=[Sn.k])
                P.op("dve", lambda e: e.tensor_tensor(Sn[:], Sn[:], hv(pS[0:64, :], 4), ALU.add), r=[Sn.k, pS.k], w=[Sn.k])
                P.op("act", lambda e: e.copy(o_[:], po[:, :]), r=[po.k], w=[o_.k])
                P.op("pool", lambda e: e.tensor_tensor(sq[:], o_[:], o_[:], mm), r=[o_.k], w=[sq.k])
                P.op("dve", lambda e: e.tensor_reduce(m4[:], hv(sq[:], 4), AX.X, ALU.add), r=[sq.k], w=[m4.k])
                P.op("act", lambda e: e.activation(out=m4[:], in_=m4[:], func=AF.Sqrt, bias=1e-5, scale=1.0 / 128), r=[m4.k], w=[m4.k])
                P.op("dve", lambda e: e.reciprocal(m4[:], m4[:]), r=[m4.k], w=[m4.k])
                P.op("dve", lambda e: e.tensor_tensor(hv(o_[:], 4), hv(o_[:], 4), m4[:, :].unsqueeze(2).to_broadcast([128, 4, 128]), mm), r=[o_.k, m4.k], w=[o_.k])
                P.op("pool", lambda e: e.tensor_tensor(o_[:], o_[:], ngb[:], mm), r=[o_.k, ngb.k], w=[o_.k])
                P.op("dve", lambda e: e.tensor_tensor(o_[:], o_[:], sl_[:], mm), r=[o_.k, sl_.k], w=[o_.k])
                if "dbg_yb" in C.dbg:
                    P.dma("sp", C.dbg["dbg_yb"][g * 128:(g + 1) * 128, :], o_[:], r=[o_.k])
                pb = psb(C)
                for q in range(4):
                    P.op("pe", lambda e: e.transpose(pb[:, q * 128:(q + 1) * 128], o_[:, q * 128:(q + 1) * 128], C.ident[:]), r=[o_.k, C.ident_tok], w=[pb.k], inc=(q == 3))
                P.op("act", lambda e: e.copy(ys[:, :, t0:t0 + 128], hv(pb[:, :], 4)), r=[pb.k], w=[ys.k])
            P.dma("sp", XTv(C.YT)[:, 4:8, s * 512:(s + 1) * 512], ys[:], r=[ys.k], w=[C.YT_tok[1][s]])
        P.barrier()


def ln_inplace(C, xt, gb, bb, st, junk):
    P = C.P
    P.op("dve", lambda e: e.tensor_reduce(st[:, 0:1], xt[:], AX.X, ALU.add), r=[xt.k], w=[st.k])
    P.op("dve", lambda e: e.tensor_scalar(st[:, 0:1], st[:, 0:1], 1.0 / D, None, ALU.mult), r=[st.k], w=[st.k])
    P.op("dve", lambda e: e.tensor_scalar(xt[:], xt[:], st[:, 0:1], None, ALU.subtract), r=[xt.k, st.k], w=[xt.k])
    P.op("act", lambda e: e.activation(out=junk[:], in_=xt[:], func=AF.Square, accum_out=st[:, 1:2]), r=[xt.k], w=[junk.k, st.k])
    P.op("act", lambda e: e.activation(out=st[:, 1:2], in_=st[:, 1:2], func=AF.Sqrt, bias=LN_EPS, scale=1.0 / D), r=[st.k], w=[st.k])
    P.op("dve", lambda e: e.reciprocal(st[:, 1:2], st[:, 1:2]), r=[st.k], w=[st.k])
    P.op("dve", lambda e: e.scalar_tensor_tensor(xt[:], xt[:], st[:, 1:2], gb[:], ALU.mult, ALU.mult), r=[xt.k, st.k, gb.k], w=[xt.k])
    P.op("pool", lambda e: e.tensor_tensor(xt[:], xt[:], bb[:], ALU.add), r=[xt.k, bb.k], w=[xt.k])


def ln_lockstep(C, xs, gb, bb, sts, junk):
    P = C.P
    n = len(xs)
    for k in range(n):
        xt, st = xs[k], sts[k]
        P.op("dve", lambda e: e.tensor_reduce(st[:, 0:1], xt[:], AX.X, ALU.add), r=[xt.k], w=[st.k])
    for k in range(n):
        xt, st = xs[k], sts[k]
        P.op("dve", lambda e: e.tensor_scalar(st[:, 0:1], st[:, 0:1], 1.0 / D, None, ALU.mult), r=[st.k], w=[st.k])
    for k in range(n):
        xt, st = xs[k], sts[k]
        P.op("dve", lambda e: e.tensor_scalar(xt[:], xt[:], st[:, 0:1], None, ALU.subtract), r=[xt.k, st.k], w=[xt.k])
        P.op("act", lambda e: e.activation(out=junk[:], in_=xt[:], func=AF.Square, accum_out=st[:, 1:2]), r=[xt.k], w=[junk.k, st.k])
    for k in range(n):
        xt, st = xs[k], sts[k]
        P.op("act", lambda e: e.activation(out=st[:, 1:2], in_=st[:, 1:2], func=AF.Sqrt, bias=LN_EPS, scale=1.0 / D), r=[st.k], w=[st.k])
    for k in range(n):
        xt, st = xs[k], sts[k]
        P.op("dve", lambda e: e.reciprocal(st[:, 1:2], st[:, 1:2]), r=[st.k], w=[st.k])
    for k in range(n):
        xt, st = xs[k], sts[k]
        P.op("dve", lambda e: e.scalar_tensor_tensor(xt[:], xt[:], st[:, 1:2], gb[:], ALU.mult, ALU.mult), r=[xt.k, st.k, gb.k], w=[xt.k])
        P.op("pool", lambda e: e.tensor_tensor(xt[:], xt[:], bb[:], ALU.add), r=[xt.k, bb.k], w=[xt.k])


def tile_to_stage(C, xt, stage, j):
    P = C.P
    for half in range(2):
        pb = psb(C)
        for c4 in range(4):
            c = half * 4 + c4
            P.op("pe", lambda e: e.transpose(pb[:, c4 * 128:(c4 + 1) * 128], xt[:, c * 128:(c + 1) * 128], C.ident[:]),
                 r=[xt.k, C.ident_tok], w=[pb.k], inc=(c4 == 3))
        dst = stage[:, half * 4:(half + 1) * 4, j * 128:(j + 1) * 128]
        src = hv(pb[:, :], 4)
        if half == 0:
            P.op("act", lambda e: e.copy(dst, src), r=[pb.k], w=[stage.k])
        else:
            P.op("dve", lambda e: e.tensor_copy(dst, src), r=[pb.k], w=[stage.k])


def phase_outproj(C, w_out, srcYT, srcYT_toks, resid, resid_toks, lng, lnb, dstH, dstH_tok, dstHT, dstHT_tok, dbgname=None):
    nc, P, T, I = C.nc, C.P, C.T, C.I
    with ExitStack() as es:
        WO = mk(P, [128, 8, D], BF16, "WO", es)
        for c in range(8):
            P.dma("pool", WO[:, c, :], w_out[c * 128:(c + 1) * 128, :], w=[WO.k])
        gb = mk(P, [128, D], F32, "lng", es)
        bb = mk(P, [128, D], F32, "lnb", es)
        bcast_load(C, gb[:], lng, 128, gb.k)
        bcast_load(C, bb[:], lnb, 128, bb.k)
        yts = [mk(P, [128, 8, 512], BF16, "yt", es) for _ in range(2)]
        xts = [mk(P, [128, D], F32, "xres", es) for _ in range(8)]
        sts = [mk(P, [128, 2], F32, "lnst", es) for _ in range(8)]
        junk = mk(P, [128, D], BF16, "junk", es)
        stg = [mk(P, [128, 8, 512], BF16, "hstg", es) for _ in range(2)]
        pend = None

        def loads(s):
            P.dma("sp", yts[s % 2][:], XTv(srcYT)[:, :, s * 512:(s + 1) * 512], r=srcYT_toks(s), w=[yts[s % 2].k])
            for j in range(4):
                i = s * 4 + j
                xt = xts[(s % 2) * 4 + j]
                P.dma("sp", xt[:], resid[i * 128:(i + 1) * 128, :], r=resid_toks(i), w=[xt.k])

        loads(0)
        for s in range(C.NS):
            yt = yts[s % 2]
            sg_ = stg[s % 2]
            X = xts[(s % 2) * 4:(s % 2) * 4 + 4]
            S = sts[(s % 2) * 4:(s % 2) * 4 + 4]
            for j in range(4):
                xt = X[j]
                for half in range(2):
                    pb = psb(C)
                    for c in range(8):
                        P.op("pe", lambda e: e.matmul(pb[:, :], lhsT=yt[:, c, j * 128:(j + 1) * 128], rhs=WO[:, c, half * 512:(half + 1) * 512], start=(c == 0), stop=(c == 7)),
                             r=[yt.k, WO.k], w=[pb.k], inc=(c == 7))
                    P.op("dve", lambda e: e.scalar_tensor_tensor(xt[:, half * 512:(half + 1) * 512], xt[:, half * 512:(half + 1) * 512], DN_ALPHA, pb[:, :], ALU.mult, ALU.add),
                         r=[xt.k, pb.k], w=[xt.k])
            if pend is not None:
                pend()
            if s + 1 < C.NS:
                loads(s + 1)
            ln_lockstep(C, X, gb, bb, S, junk)
            for j in range(4):
                i = s * 4 + j
                P.dma("sp", dstH[i * 128:(i + 1) * 128, :], X[j][:], r=[X[j].k], w=[dstH_tok[i]])

            def pend(s=s, X=X, sg_=sg_):
                for j in range(4):
                    tile_to_stage(C, X[j], sg_, j)
                P.dma("sp", XTv(dstHT)[:, :, s * 512:(s + 1) * 512], sg_[:], r=[sg_.k], w=[dstHT_tok[s]])
        pend()
        P.barrier()


def phase_moe(C, l, srcH, srcH_tok, srcHT, srcHT_tok, dstX, dstX_tok, dstXT, dstXT_tok):
    nc, P, T, I = C.nc, C.P, C.T, C.I
    mm = ALU.mult
    with ExitStack() as es:
        WD = mk(P, [128, 32, D], BF16, "WD", es)
        for c4 in range(4):
            P.dma("sp", WD[:, c4 * 8:(c4 + 1) * 8, :], C.WD16[l].rearrange("p (c d) -> p c d", c=32)[:, c4 * 8:(c4 + 1) * 8, :], r=[C.WD16_tok[l]], w=[WD.k])
        RW = mk(P, [128, 8, NE], BF16, "RW", es)
        P.dma("pool", RW[:], I["router_w"].rearrange("(c p) e -> p c e", p=128), w=[RW.k])
        rbb = mk(P, [128, NE], F32, "rbb", es)
        bcast_load(C, rbb[:], I["router_bias"], 128, rbb.k)
        SEL = mk(P, [16, 16, 128], BF16, "SEL", es)
        P.dma("pool", SEL[:], I["c_sel"], w=[SEL.k])
        gb = mk(P, [128, D], F32, "lng", es)
        bb = mk(P, [128, D], F32, "lnb", es)
        bcast_load(C, gb[:], I["ln2_g"][l:l + 1, :], 128, gb.k)
        bcast_load(C, bb[:], I["ln2_b"][l:l + 1, :], 128, bb.k)
        hts = [mk(P, [128, 8, 512], BF16, "hT", es) for _ in range(2)]
        xts = [mk(P, [128, D], F32, "hres", es) for _ in range(4)]
        sts = [mk(P, [128, 2], F32, "lnst", es) for _ in range(4)]
        junk = mk(P, [128, D], BF16, "junk", es)
        stg = [mk(P, [128, 8, 512], BF16, "xstg", es) for _ in range(2)]
        actT = mk(P, [128, 32, 512], BF16, "actT", es)
        combT = mk(P, [16, 512], BF16, "combT", es)
        WGUs = [mk(P, [128, 2, 8, DE], BF16, "WGU", es) for _ in range(3)]
        sgl = [mk(P, [128, 512], F32, "sgl", es) for _ in range(2)]
        R4 = range(4)
        s_l = [mk(P, [128, NE], F32, "rs", es) for _ in R4]
        sel_l = [mk(P, [128, NE], F32, "rsel", es) for _ in R4]
        pr_l = [mk(P, [128, 4, 6], F32, "rpr", es) for _ in R4]
        gs_l = [mk(P, [128, 4], F32, "rgs", es) for _ in R4]
        t1_l = [mk(P, [128, 4], F32, "rt1", es) for _ in R4]
        m1_l = [mk(P, [128, 2], F32, "rm1", es) for _ in R4]
        selm_l = [mk(P, [128, NE], F32, "rselm", es) for _ in R4]
        sel2_l = [mk(P, [128, NE], F32, "rsel2", es) for _ in R4]
        comb_l = [mk(P, [128, NE], F32, "rcomb", es) for _ in R4]
        nwl = [0]

        def load_w(e):
            b = nwl[0] % 3
            nwl[0] += 1
            P.dma("sp", WGUs[b][:], C.WGU16[l][e].rearrange("p (t c f) -> p t c f", t=2, c=8), r=[C.WGU16_tok[l][e]], w=[WGUs[b].k])
            return WGUs[b]

        def g4(t):
            return t[:, :].rearrange("p (g e) -> p g e", g=4)

        def load_hT(s):
            P.dma("sp", hts[s % 2][:], XTv(srcHT)[:, :, s * 512:(s + 1) * 512], r=[srcHT_tok[s]], w=[hts[s % 2].k])

        def router_front(s):
            hT = hts[s % 2]
            for j in R4:
                plg = psb(C)
                s_ = s_l[j]
                for c in range(8):
                    P.op("pe", lambda e: e.matmul(plg[:, 0:NE], lhsT=hT[:, c, j * 128:(j + 1) * 128], rhs=RW[:, c, :], start=(c == 0), stop=(c == 7)),
                         r=[hT.k, RW.k], w=[plg.k], inc=(c == 7))
                P.op("act", lambda e: e.activation(out=s_[:], in_=plg[:, 0:NE], func=AF.Sigmoid), r=[plg.k], w=[s_.k])

            def step(fn):
                for j in R4:
                    fn(s_l[j], sel_l[j], pr_l[j], gs_l[j], t1_l[j], m1_l[j], selm_l[j], sel2_l[j], comb_l[j])
            step(lambda s_, sel, pr, gs, t1, m1, selm, sel2, comb: P.op("dve", lambda e: e.tensor_tensor(sel[:], s_[:], rbb[:], ALU.add), r=[s_.k, rbb.k], w=[sel.k]))
            step(lambda s_, sel, pr, gs, t1, m1, selm, sel2, comb: P.op("dve", lambda e: e.tensor_tensor(pr[:, :, 0:3], g4(sel)[:, :, 0:3], g4(sel)[:, :, 1:4], ALU.add), r=[sel.k], w=[pr.k]))
            step(lambda s_, sel, pr, gs, t1, m1, selm, sel2, comb: P.op("dve", lambda e: e.tensor_tensor(pr[:, :, 3:5], g4(sel)[:, :, 0:2], g4(sel)[:, :, 2:4], ALU.add), r=[sel.k], w=[pr.k]))
            step(lambda s_, sel, pr, gs, t1, m1, selm, sel2, comb: P.op("dve", lambda e: e.tensor_tensor(pr[:, :, 5:6], g4(sel)[:, :, 0:1], g4(sel)[:, :, 3:4], ALU.add), r=[sel.k], w=[pr.k]))
            step(lambda s_, sel, pr, gs, t1, m1, selm, sel2, comb: P.op("dve", lambda e: e.tensor_reduce(gs[:], pr[:], AX.X, ALU.max), r=[pr.k], w=[gs.k]))
            step(lambda s_, sel, pr, gs, t1, m1, selm, sel2, comb: P.op("dve", lambda e: e.tensor_reduce(m1[:, 0:1], gs[:], AX.X, ALU.max), r=[gs.k], w=[m1.k]))
            step(lambda s_, sel, pr, gs, t1, m1, selm, sel2, comb: P.op("dve", lambda e: e.tensor_scalar(gs[:], gs[:], m1[:, 0:1], None, ALU.is_ge), r=[gs.k, m1.k], w=[gs.k]))
            step(lambda s_, sel, pr, gs, t1, m1, selm, sel2, comb: P.op("dve", lambda e: e.tensor_scalar(t1[:], gs[:], -1.0, 1e30, ALU.add, ALU.mult), r=[gs.k], w=[t1.k]))
            step(lambda s_, sel, pr, gs, t1, m1, selm, sel2, comb: P.op("dve", lambda e: e.tensor_tensor(g4(selm), g4(sel), gs[:, :].unsqueeze(2).to_broadcast([128, 4, 4]), mm), r=[sel.k, gs.k], w=[selm.k]))
            step(lambda s_, sel, pr, gs, t1, m1, selm, sel2, comb: P.op("dve", lambda e: e.tensor_tensor(g4(selm), g4(selm), t1[:, :].unsqueeze(2).to_broadcast([128, 4, 4]), ALU.add), r=[selm.k, t1.k], w=[selm.k]))
            step(lambda s_, sel, pr, gs, t1, m1, selm, sel2, comb: P.op("dve", lambda e: e.tensor_reduce(m1[:, 0:1], selm[:], AX.X, ALU.max), r=[selm.k], w=[m1.k]))
            step(lambda s_, sel, pr, gs, t1, m1, selm, sel2, comb: P.op("dve", lambda e: e.tensor_scalar(sel2[:], selm[:], m1[:, 0:1], None, ALU.is_ge), r=[selm.k, m1.k], w=[sel2.k]))
            step(lambda s_, sel, pr, gs, t1, m1, selm, sel2, comb: P.op("dve", lambda e: e.scalar_tensor_tensor(sel2[:], sel2[:], -1e30, selm[:], mm, ALU.add), r=[sel2.k, selm.k], w=[sel2.k]))
            step(lambda s_, sel, pr, gs, t1, m1, selm, sel2, comb: P.op("dve", lambda e: e.tensor_reduce(m1[:, 1:2], sel2[:], AX.X, ALU.max), r=[sel2.k], w=[m1.k]))
            step(lambda s_, sel, pr, gs, t1, m1, selm, sel2, comb: P.op("dve", lambda e: e.tensor_scalar(sel2[:], selm[:], m1[:, 1:2], None, ALU.is_ge), r=[selm.k, m1.k], w=[sel2.k]))
            step(lambda s_, sel, pr, gs, t1, m1, selm, sel2, comb: P.op("dve", lambda e: e.tensor_tensor(comb[:], s_[:], sel2[:], mm), r=[s_.k, sel2.k], w=[comb.k]))
            step(lambda s_, sel, pr, gs, t1, m1, selm, sel2, comb: P.op("dve", lambda e: e.tensor_reduce(m1[:, 0:1], comb[:], AX.X, ALU.add), r=[comb.k], w=[m1.k]))
            step(lambda s_, sel, pr, gs, t1, m1, selm, sel2, comb: P.op("dve", lambda e: e.reciprocal(m1[:, 0:1], m1[:, 0:1]), r=[m1.k], w=[m1.k]))
            step(lambda s_, sel, pr, gs, t1, m1, selm, sel2, comb: P.op("dve", lambda e: e.tensor_scalar(comb[:], comb[:], m1[:, 0:1], None, mm), r=[comb.k, m1.k], w=[comb.k]))

        def router_back(s):
            for j in R4:
                comb = comb_l[j]
                pct = psb(C)
                P.op("pe", lambda e: e.transpose(pct[0:16, 0:128], comb[:, :], C.ident[:]), r=[comb.k, C.ident_tok], w=[pct.k])
                P.op("act", lambda e: e.copy(combT[:, j * 128:(j + 1) * 128], pct[0:16, 0:128]), r=[pct.k], w=[combT.k])

        def load_x(s):
            for j in R4:
                i = s * 4 + j
                P.dma("sp", xts[j][:], srcH[i * 128:(i + 1) * 128, :], r=[srcH_tok[i]], w=[xts[j].k])

        def make_pend(s):
            xs_ = stg[s % 2]

            def pend():
                for j in R4:
                    tile_to_stage(C, xts[j], xs_, j)
                P.dma("sp", XTv(dstXT)[:, :, s * 512:(s + 1) * 512], xs_[:], r=[xs_.k], w=[dstXT_tok[s]])
            return pend

        pend = None
        load_hT(0)
        router_front(0)
        router_back(0)
        for s in range(C.NS):
            hT = hts[s % 2]
            if s + 1 < C.NS:
                load_hT(s + 1)
            for ex in range(NE):
                WGU = load_w(ex)
                pcb = psb(C)
                P.op("pe", lambda e: e.matmul(pcb[:, :], lhsT=SEL[:, ex, :], rhs=combT[:, :], start=True, stop=True), r=[SEL.k, combT.k], w=[pcb.k])
                for f in range(2):
                    pG, pU = psb(C), psb(C)
                    for pb, ti in ((pG, 0), (pU, 1)):
                        for c in range(8):
                            P.op("pe", lambda e: e.matmul(pb[:, :], lhsT=WGU[:, ti, c, f * 128:(f + 1) * 128], rhs=hT[:, c, :], start=(c == 0), stop=(c == 7)),
                                 r=[WGU.k, hT.k], w=[pb.k], inc=(c == 7))
                    sg_ = sgl[(ex * 2 + f) % 2]
                    P.op("act", lambda e: e.activation(out=sg_[:], in_=pG[:, :], func=AF.Silu), r=[pG.k], w=[sg_.k])
                    P.op("dve", lambda e: e.tensor_tensor(sg_[:], sg_[:], pU[:, :], mm), r=[sg_.k, pU.k], w=[sg_.k])
                    P.op("dve", lambda e: e.tensor_tensor(actT[:, ex * 2 + f, :], sg_[:], pcb[:, :], mm), r=[sg_.k, pcb.k], w=[actT.k])
                if ex == 3:
                    if pend is not None:
                        pend()
                        pend = None
                    load_x(s)
            if s + 1 < C.NS:
                router_front(s + 1)
            for j in R4:
                xt = xts[j]
                for half in range(2):
                    pb = psb(C)
                    for c in range(32):
                        P.op("pe", lambda e: e.matmul(pb[:, :], lhsT=actT[:, c, j * 128:(j + 1) * 128], rhs=WD[:, c, half * 512:(half + 1) * 512], start=(c == 0), stop=(c == 31)),
                             r=[actT.k, WD.k], w=[pb.k], inc=(c == 31))
                    P.op("dve", lambda e: e.scalar_tensor_tensor(xt[:, half * 512:(half + 1) * 512], xt[:, half * 512:(half + 1) * 512], DN_ALPHA, pb[:, :], ALU.mult, ALU.add),
                         r=[xt.k, pb.k], w=[xt.k])
            if s + 1 < C.NS:
                router_back(s + 1)
            ln_lockstep(C, xts, gb, bb, sts, junk)
            for j in R4:
                i = s * 4 + j
                P.dma("sp", dstX[i * 128:(i + 1) * 128, :], xts[j][:], r=[xts[j].k], w=[dstX_tok[i]])
            if dstXT is not None:
                pend = make_pend(s)
        if pend is not None:
            pend()
        P.barrier()


def rope_consts(T):
    pos = np.arange(T, dtype=np.float64)
    c = {}
    for name, half in (("k", 64), ("i", 32)):
        inv = 10000.0 ** (-np.arange(half, dtype=np.float64) / half)
        ang = (pos.astype(np.float32)[:, None] * inv.astype(np.float32)[None, :]).astype(np.float32).astype(np.float64)
        cs = np.cos(ang).astype(np.float32).reshape(T // 128, 128, half).transpose(1, 0, 2)
        sn = np.sin(ang).astype(np.float32).reshape(T // 128, 128, half).transpose(1, 0, 2)
        c["cos_" + name] = np.ascontiguousarray(cs)
        c["sin_" + name] = np.ascontiguousarray(sn)
    q = np.arange(128)[:, None]
    s = np.arange(128)[None, :]
    c["cbias"] = np.where(s <= q, 0.0, -1e30).astype(np.float32)
    c["halfpow"] = (0.5 ** np.arange(1, 33, dtype=np.float64)).astype(np.float32).reshape(1, 32)
    c["tiebias"] = (-1e-6 * np.arange(T, dtype=np.float64)).astype(np.float32).reshape(1, T)
    return c


def rope_tm(C, dst_ap, dst_k, src, src_k, cosb, sinb, nh, half, ta_ap, ta_k, tb_ap, tb_k, rdeps):
    P = C.P
    mm = ALU.mult
    n = nh * 2
    s3 = src.rearrange("p (n f) -> p n f", n=n)
    cb = cosb.unsqueeze(1).to_broadcast([128, n, half])
    sb_ = sinb.unsqueeze(1).to_broadcast([128, n, half])
    a3 = ta_ap.rearrange("p (n f) -> p n f", n=n)
    b3 = tb_ap.rearrange("p (n f) -> p n f", n=n)
    P.op("dve", lambda e: e.tensor_tensor(a3, s3, cb, mm), r=[src_k] + rdeps, w=[ta_k])
    P.op("dve", lambda e: e.tensor_tensor(b3, s3, sb_, mm), r=[src_k] + rdeps, w=[tb_k])
    a4 = ta_ap.rearrange("p (h t f) -> p h t f", h=nh, t=2)
    b4 = tb_ap.rearrange("p (h t f) -> p h t f", h=nh, t=2)
    d4 = dst_ap.rearrange("p (h t f) -> p h t f", h=nh, t=2)
    P.op("pool", lambda e: e.tensor_tensor(d4[:, :, 0, :], a4[:, :, 0, :], b4[:, :, 1, :], ALU.subtract), r=[ta_k, tb_k], w=[dst_k])
    P.op("pool", lambda e: e.tensor_tensor(d4[:, :, 1, :], a4[:, :, 1, :], b4[:, :, 0, :], ALU.add), r=[ta_k, tb_k], w=[dst_k])


def phase_dsa(C):
    nc, P, T, I = C.nc, C.P, C.T, C.I
    mm = ALU.mult
    KT = min(256, T // 4)
    NIT = 25
    SCALE = 128 ** -0.5
    C.bank_pool = [0, 1, 2, 3, 4]
    with ExitStack() as es:
        def wt(name, shape, dt=F32):
            return mk(P, list(shape), dt, name, es)
        WQ = wt("WQ", (128, 8, 1024), BF16)
        WR = wt("WR", (128, 8, 580), BF16)
        for c in range(8):
            P.dma("pool", WQ[:, c, :], I["w_in_odd"][c * 128:(c + 1) * 128, 0:1024], w=[WQ.k])
            P.dma("pool", WR[:, c, :], I["w_in_odd"][c * 128:(c + 1) * 128, 1024:1604], w=[WR.k])
        RTs = [[wt("CK", (128, 4, 64)), wt("SK", (128, 4, 64)), wt("CI", (128, 4, 32)), wt("SI", (128, 4, 32))] for _ in range(2)]

        def load_tables(s):
            tl = RTs[s % 2]
            for t_, nm in zip(tl, ("c_cos_k", "c_sin_k", "c_cos_i", "c_sin_i")):
                P.dma("sp", t_[:], I[nm][:, s * 4:(s + 1) * 4, :], w=[t_.k])
            return tl
        BIAS = wt("BIAS", (128, T))
        bcast_load(C, BIAS[:], I["c_tiebias"], 128, BIAS.k)
        CB = wt("CB", (128, 128))
        P.dma("sp", CB[:], I["c_cbias"], w=[CB.k])
        ikg, ikb = wt("ikg", (128, 64)), wt("ikb", (128, 64))
        bcast_load(C, ikg[:], I["c_ik_ln_g"], 128, ikg.k)
        bcast_load(C, ikb[:], I["c_ik_ln_b"], 128, ikb.k)
        kT = wt("kT", (128, T), BF16)
        ikT = wt("ikT", (128, T), BF16)
        Vx = wt("Vx", (128, C.NT, 129), BF16)
        P.op("dve", lambda e: e.memset(Vx[:, :, 128:129], 1.0), w=[Vx.k])
        xTs = [wt("xTd", (128, 8, 512), BF16) for _ in range(2)]
        ta, tb = wt("ropeA", (128, 512)), wt("ropeB", (128, 512))
        kr = wt("kr", (128, 128))
        ikr = wt("ikr", (128, 128))
        st2 = wt("st2", (128, 2))
        for s in range(C.NS):
            xT = xTs[s % 2]
            P.dma("sp", xT[:], XTv(C.XT1)[:, :, s * 512:(s + 1) * 512], r=[C.XT1_tok[s]], w=[xT.k])
            CK, SK, CI, SI = load_tables(s)
            for j in range(4):
                i = s * 4 + j
                pb = psb(C)
                for c in range(8):
                    P.op("pe", lambda e: e.matmul(pb[:, 0:256], lhsT=xT[:, c, j * 128:(j + 1) * 128], rhs=WR[:, c, 0:256], start=(c == 0), stop=(c == 7)),
                         r=[xT.k, WR.k], w=[pb.k], inc=False)
                for c in range(8):
                    P.op("pe", lambda e: e.matmul(pb[:, 256:320], lhsT=xT[:, c, j * 128:(j + 1) * 128], rhs=WR[:, c, 512:576], start=(c == 0), stop=(c == 7)),
                         r=[xT.k, WR.k], w=[pb.k], inc=(c == 7))
                rope_tm(C, kr[:, :], kr.k, pb[:, 0:128], pb.k, CK[:, j, :], SK[:, j, :], 1, 64, ta[:, 0:128], ta.k, tb[:, 0:128], tb.k, [CK.k, SK.k])
                P.op("act", lambda e: e.copy(Vx[:, i, 0:128], pb[:, 128:256]), r=[pb.k], w=[Vx.k])
                P.op("dve", lambda e: e.tensor_reduce(st2[:, 0:1], pb[:, 256:320], AX.X, ALU.add), r=[pb.k], w=[st2.k])
                P.op("dve", lambda e: e.tensor_scalar(st2[:, 0:1], st2[:, 0:1], 1.0 / 64, None, mm), r=[st2.k], w=[st2.k])
                P.op("dve", lambda e: e.tensor_scalar(ikr[:, 0:64], pb[:, 256:320], st2[:, 0:1], None, ALU.subtract), r=[pb.k, st2.k], w=[ikr.k])
                P.op("act", lambda e: e.activation(out=ikr[:, 64:128], in_=ikr[:, 0:64], func=AF.Square, accum_out=st2[:, 1:2]), r=[ikr.k], w=[ikr.k, st2.k])
                P.op("act", lambda e: e.activation(out=st2[:, 1:2], in_=st2[:, 1:2], func=AF.Sqrt, bias=LN_EPS, scale=1.0 / 64), r=[st2.k], w=[st2.k])
                P.op("dve", lambda e: e.reciprocal(st2[:, 1:2], st2[:, 1:2]), r=[st2.k], w=[st2.k])
                P.op("dve", lambda e: e.scalar_tensor_tensor(ikr[:, 0:64], ikr[:, 0:64], st2[:, 1:2], ikg[:], mm, mm), r=[ikr.k, st2.k, ikg.k], w=[ikr.k])
                P.op("dve", lambda e: e.tensor_tensor(ikr[:, 64:128], ikr[:, 0:64], ikb[:], ALU.add), r=[ikr.k, ikb.k], w=[ikr.k])
                ikn = TL(ikr.t, ikr.k)
                rope_src = ikr[:, 64:128]
                n = 2
                s3 = rope_src.rearrange("p (n f) -> p n f", n=n)
                cb = CI[:, j, :].unsqueeze(1).to_broadcast([128, n, 32])
                sb_ = SI[:, j, :].unsqueeze(1).to_broadcast([128, n, 32])
                a3 = ta[:, 0:64].rearrange("p (n f) -> p n f", n=n)
                b3 = tb[:, 0:64].rearrange("p (n f) -> p n f", n=n)
                P.op("dve", lambda e: e.tensor_tensor(a3, s3, cb, mm), r=[ikr.k, CI.k], w=[ta.k])
                P.op("dve", lambda e: e.tensor_tensor(b3, s3, sb_, mm), r=[ikr.k, SI.k], w=[tb.k])
                P.op("pool", lambda e: e.tensor_tensor(ikr[:, 0:32], ta[:, 0:32], tb[:, 32:64], ALU.subtract), r=[ta.k, tb.k], w=[ikr.k])
                P.op("pool", lambda e: e.tensor_tensor(ikr[:, 32:64], ta[:, 32:64], tb[:, 0:32], ALU.add), r=[ta.k, tb.k], w=[ikr.k])
                P.op("pool", lambda e: e.tensor_copy(ikr[:, 64:128], ikr[:, 0:64]), r=[ikr.k], w=[ikr.k])
                pt = psb(C)
                P.op("pe", lambda e: e.transpose(pt[:, 0:128], kr[:, :], C.ident[:]), r=[kr.k, C.ident_tok], w=[pt.k], inc=False)
                P.op("pe", lambda e: e.transpose(pt[:, 128:256], ikr[:, :], C.ident[:]), r=[ikr.k, C.ident_tok], w=[pt.k])
                P.op("act", lambda e: e.copy(kT[:, i * 128:(i + 1) * 128], pt[:, 0:128]), r=[pt.k], w=[kT.k])
                P.op("act", lambda e: e.mul(ikT[:, i * 128:(i + 1) * 128], pt[:, 128:256], 0.125), r=[pt.k], w=[ikT.k])
        SC = wt("SC", (128, T))
        MASKs = [wt("MASK", (128, T)) for _ in range(2)]
        junk = wt("junkd", (128, T), BF16)
        qr = wt("qr", (128, 1024))
        iqr = wt("iqr", (128, 256))
        qTs = [wt("qT", (128, 8, 128), BF16) for _ in range(2)]
        iqT = wt("iqT", (128, 2, 128), BF16)
        iws = wt("iws", (128, 4))
        rl = [wt("rl%d" % n, (128, 512)) for n in range(2)]
        bs = wt("bs", (128, 8))
        Dk = wt("Dk", (128, NIT))
        HK = wt("HK", (128, NIT))
        bcast_load(C, HK[:], I["c_halfpow"][0:1, 0:NIT], 128, HK.k)
        mT4 = [wt("mT4_%d" % n, (128, 4, 128), BF16) for n in range(2)]
        pTs = [wt("pT%d" % n, (128, 4, 128), BF16) for n in range(4)]
        o_ = wt("od", (128, 1024))
        rs8 = wt("rs8", (128, 8))
        ostg = [wt("ostg", (128, 8, 512), BF16) for _ in range(2)]
        accb = [TL(*C.banks[b]) for b in (5, 6, 7)]
        MBIG = 30000.0
        identb = wt("identb", (128, 128), BF16)
        P.op("dve", lambda e: e.tensor_copy(identb[:], C.ident[:]), r=[C.ident_tok], w=[identb.k])
        acc_of = [(0, 0), (0, 1), (0, 2), (1, 0), (1, 1), (1, 2), (2, 0), (2, 1)]
        def qblock(s, j):
            if True:
                i = s * 4 + j
                L = (i + 1) * 128
                xT = xTs[s % 2]
                og = ostg[s % 2]
                qT = qTs[i % 2]
                MASK = MASKs[i % 2]
                if j == 0:
                    P.dma("sp", xT[:], XTv(C.XT1)[:, :, s * 512:(s + 1) * 512], r=[C.XT1_tok[s]], w=[xT.k])
                    load_tables(s)
                cast_some(C, 1, 2)
                CK, SK, CI, SI = RTs[s % 2]
                pq = [psb(C), psb(C)]
                for half in range(2):
                    for c in range(8):
                        P.op("pe", lambda e: e.matmul(pq[half][:, :], lhsT=xT[:, c, j * 128:(j + 1) * 128], rhs=WQ[:, c, half * 512:(half + 1) * 512], start=(c == 0), stop=(c == 7)),
                             r=[xT.k, WQ.k], w=[pq[half].k], inc=(c == 7))
                piq = psb(C)
                for c in range(8):
                    P.op("pe", lambda e: e.matmul(piq[:, 0:256], lhsT=xT[:, c, j * 128:(j + 1) * 128], rhs=WR[:, c, 256:512], start=(c == 0), stop=(c == 7)),
                         r=[xT.k, WR.k], w=[piq.k], inc=False)
                for c in range(8):
                    P.op("pe", lambda e: e.matmul(piq[:, 256:260], lhsT=xT[:, c, j * 128:(j + 1) * 128], rhs=WR[:, c, 576:580], start=(c == 0), stop=(c == 7)),
                         r=[xT.k, WR.k], w=[piq.k], inc=(c == 7))
                for half in range(2):
                    hs = slice(half * 512, (half + 1) * 512)
                    rope_tm(C, qr[:, hs], qr.k, pq[half][:, :], pq[half].k, CK[:, j, :], SK[:, j, :], 4, 64,
                            ta[:, 0:512], ta.k, tb[:, 0:512], tb.k, [CK.k, SK.k])
                rope_tm(C, iqr[:, :], iqr.k, piq[:, 0:256], piq.k, CI[:, j, :], SI[:, j, :], 4, 32, ta[:, 0:256], ta.k, tb[:, 0:256], tb.k, [CI.k, SI.k])
                P.op("act", lambda e: e.mul(iws[:], piq[:, 256:260], 0.5), r=[piq.k], w=[iws.k])
                for half in range(2):
                    pb = psb(C)
                    for c4 in range(4):
                        h = half * 4 + c4
                        P.op("pe", lambda e: e.transpose(pb[:, c4 * 128:(c4 + 1) * 128], qr[:, h * 128:(h + 1) * 128], C.ident[:]), r=[qr.k, C.ident_tok], w=[pb.k], inc=(c4 == 3))
                    P.op("act", lambda e: e.copy(qT[:, half * 4:(half + 1) * 4, :], hv(pb[:, :], 4)), r=[pb.k], w=[qT.k])
                pb = psb(C)
                for c2 in range(2):
                    P.op("pe", lambda e: e.transpose(pb[:, c2 * 128:(c2 + 1) * 128], iqr[:, c2 * 128:(c2 + 1) * 128], C.ident[:]), r=[iqr.k, C.ident_tok], w=[pb.k], inc=(c2 == 1))
                P.op("act", lambda e: e.copy(iqT[:], hv(pb[:, 0:256], 2)), r=[pb.k], w=[iqT.k])
                yield "F"
                for k0 in range(0, L, 512):
                    kw = min(512, L - k0)
                    for h in range(4):
                        ph = psb(C)
                        pl = (h % 2) * 64
                        P.op("pe", lambda e: e.matmul(ph[:, 0:kw], lhsT=iqT[pl:pl + 64, h // 2, :], rhs=ikT[pl:pl + 64, k0:k0 + kw], start=True, stop=True),
                             r=[iqT.k, ikT.k], w=[ph.k])
                        r_ = rl[h % 2]
                        P.op("act", lambda e: e.activation(out=r_[:, 0:kw], in_=ph[:, 0:kw], func=AF.Relu), r=[ph.k], w=[r_.k])
                        if h == 0:
                            P.op("dve", lambda e: e.scalar_tensor_tensor(SC[:, k0:k0 + kw], r_[:, 0:kw], iws[:, 0:1], BIAS[:, k0:k0 + kw], mm, ALU.add), r=[r_.k, iws.k, BIAS.k], w=[SC.k])
                        else:
                            P.op("dve", lambda e: e.scalar_tensor_tensor(SC[:, k0:k0 + kw], r_[:, 0:kw], iws[:, h:h + 1], SC[:, k0:k0 + kw], mm, ALU.add),
                                 r=[r_.k, iws.k, SC.k], w=[SC.k])
                    yield "F"
                if L > KT:
                    P.op("dve", lambda e: e.tensor_reduce(bs[:, 1:2], SC[:, 0:L], AX.X, ALU.max, apply_absolute_value=True), r=[SC.k], w=[bs.k])
                    P.op("dve", lambda e: e.tensor_scalar(bs[:, 0:1], bs[:, 1:2], -1.0, -1.0, mm, ALU.add), r=[bs.k], w=[bs.k])
                    P.op("dve", lambda e: e.tensor_scalar(bs[:, 1:2], bs[:, 1:2], 2.0, 2.0, mm, ALU.add), r=[bs.k], w=[bs.k])
                    P.op("dve", lambda e: e.tensor_scalar(Dk[:], HK[:], bs[:, 1:2], None, mm), r=[bs.k, HK.k], w=[Dk.k])
                P.op("pool", lambda e: e.tensor_tensor(SC[:, i * 128:L], SC[:, i * 128:L], CB[:], ALU.add), r=[SC.k, CB.k], w=[SC.k])
                if L > KT:
                    for it in range(NIT):
                        P.op("dve", lambda e: e.tensor_tensor(bs[:, 2:3], bs[:, 0:1], Dk[:, it:it + 1], ALU.add), r=[bs.k, Dk.k], w=[bs.k])
                        P.op("dve", lambda e: e.tensor_scalar(junk[:, 0:L], SC[:, 0:L], bs[:, 2:3], None, ALU.is_gt, ALU.add, accum_out=bs[:, 3:4]),
                             r=[SC.k, bs.k], w=[junk.k, bs.k])
                        P.op("dve", lambda e: e.scalar_tensor_tensor(bs[:, 4:5], bs[:, 3:4], float(KT) - 0.5, Dk[:, it:it + 1], ALU.is_gt, mm), r=[bs.k, Dk.k], w=[bs.k])
                        P.op("dve", lambda e: e.tensor_tensor(bs[:, 0:1], bs[:, 0:1], bs[:, 4:5], ALU.add), r=[bs.k], w=[bs.k])
                        yield "F"
                    P.op("dve", lambda e: e.tensor_scalar(MASK[:, 0:L], SC[:, 0:L], bs[:, 0:1], None, ALU.is_le), r=[SC.k, bs.k], w=[MASK.k])
                else:
                    P.op("dve", lambda e: e.tensor_scalar(MASK[:, 0:L], SC[:, 0:L], -1e29, None, ALU.is_le), r=[SC.k], w=[MASK.k])
                if "dbg_mask" in C.dbg:
                    P.dma("sp", C.dbg["dbg_mask"][i * 128:(i + 1) * 128, 0:L], MASK[:, 0:L], r=[MASK.k])
                    P.dma("sp", C.dbg["dbg_sc"][i * 128:(i + 1) * 128, 0:L], SC[:, 0:L], r=[SC.k])
                yield "END_FRONT"
                units = [(st, hg) for st in range(i + 1) for hg in range(2)]

                def stage1(st, hg):
                    if hg == 0 and st % 4 == 0:
                        n4 = min(4, i + 1 - st)
                        m4 = mT4[(st // 4) % 2]
                        pb = psb(C)
                        for u in range(n4):
                            P.op("pe", lambda e: e.transpose(pb[:, u * 128:(u + 1) * 128], MASK[:, (st + u) * 128:(st + u + 1) * 128], C.ident[:]),
                                 r=[MASK.k, C.ident_tok], w=[pb.k], inc=(u == n4 - 1))
                        P.op("act", lambda e: e.mul(m4[:, 0:n4, :], hv(pb[:, :], 4)[:, 0:n4, :], -MBIG), r=[pb.k], w=[m4.k])
                    m4 = mT4[(st // 4) % 2]
                    pl_ = psb(C)
                    P.op("pe", lambda e: e.matmul(pl_[:, :], lhsT=identb[:, :], rhs=m4[:, st % 4, :].unsqueeze(1).to_broadcast([128, 4, 128]), start=True, stop=False),
                         r=[identb.k, m4.k], w=[pl_.k], inc=False)
                    P.op("pe", lambda e: e.matmul(pl_[:, :], lhsT=kT[:, st * 128:(st + 1) * 128], rhs=qT[:, hg * 4:(hg + 1) * 4, :].rearrange("p h q -> p (h q)"), start=False, stop=True),
                         r=[kT.k, qT.k], w=[pl_.k])
                    pT = pTs[hg * 2 + st % 2]
                    P.op("act", lambda e: e.activation(out=pT[:], in_=hv(pl_[:, :], 4), func=AF.Exp, scale=SCALE), r=[pl_.k], w=[pT.k])

                def stage2(st, hg):
                    pT = pTs[hg * 2 + st % 2]
                    for hh in range(4):
                        h = hg * 4 + hh
                        ab, slot = acc_of[h]
                        P.op("pe", lambda e: e.matmul(accb[ab][:, slot * 129:(slot + 1) * 129], lhsT=pT[:, hh, :], rhs=Vx[:, st, :], start=(st == 0 and slot == 0), stop=(st == i), skip_group_check=True),
                             r=[pT.k, Vx.k], w=[accb[ab].k], inc=(hh == 3))

                stage1(*units[0])
                for n in range(len(units)):
                    if n + 1 < len(units):
                        stage1(*units[n + 1])
                    stage2(*units[n])
                    if units[n][1] == 1:
                        yield "B"
                for h in range(8):
                    ab, slot = acc_of[h]
                    P.op("dve", lambda e: e.reciprocal(rs8[:, h:h + 1], accb[ab][:, slot * 129 + 128:slot * 129 + 129]), r=[accb[ab].k], w=[rs8.k])
                    P.op("act", lambda e: e.activation(out=o_[:, h * 128:(h + 1) * 128], in_=accb[ab][:, slot * 129:slot * 129 + 128], func=AF.Copy, scale=rs8[:, h:h + 1]),
                         r=[accb[ab].k, rs8.k], w=[o_.k])
                if "dbg_dsa" in C.dbg:
                    P.dma("sp", C.dbg["dbg_dsa"][i * 128:(i + 1) * 128, :], o_[:], r=[o_.k])
                tile_to_stage(C, o_, og, j)
                if j == 3:
                    P.dma("sp", XTv(C.YT)[:, :, s * 512:(s + 1) * 512], og[:], r=[og.k], w=[C.YT_tok[0][s], C.YT_tok[1][s]])

        pipeline2([(lambda s=s, j=j: qblock(s, j)) for s in range(C.NS) for j in range(4)], interleave=C.dsa_interleave)
        P.barrier()
    C.bank_pool = list(range(8))


class _View:
    def __init__(self, tl, sl):
        self.t = _Sl(tl.t, sl)
        self.k = tl.k

    def __getitem__(self, idx):
        return self.t[idx]


class _Sl:
    def __init__(self, t, sl):
        self.base = t
        self.sl = sl

    def __getitem__(self, idx):
        rows, cols = idx
        assert cols == slice(None)
        return self.base[rows, self.sl]


_NC_CACHE = {}


def _in_map(inputs, b, T, consts):
    m = {"x": np.ascontiguousarray(inputs["x"][b, :T], dtype=np.float32)}
    for k, a in inputs.items():
        if k == "x":
            continue
        a = np.asarray(a, dtype=np.float32)
        if k in ("router_w", "exp_w_gate", "exp_w_up", "exp_w_down", "ln1_g", "ln1_b", "ln2_g", "ln2_b"):
            m[k] = np.ascontiguousarray(a)
        elif k == "router_bias":
            m[k] = np.ascontiguousarray(a.reshape(1, -1))
        elif k == "a_r_k":
            m[k] = np.ascontiguousarray(a.reshape(1, 512))
        elif a.ndim == 3:
            m[k] = np.ascontiguousarray(a[0])
        elif a.ndim == 2:
            m[k] = np.ascontiguousarray(a[0:1])
    m.update(consts)
    return m


def kernel(**inputs):
    x = np.asarray(inputs["x"])
    B, T, _ = x.shape
    if T not in _NC_CACHE:
        _NC_CACHE[T] = build(T)
    nc = _NC_CACHE[T]
    consts = {"c_" + k: v for k, v in host_consts(T).items()}
    consts.update({"c_" + k: v for k, v in rope_consts(T).items()})
    in_maps = [_in_map(inputs, b, T, consts) for b in range(B)]
    res = run_bass_kernel_spmd(nc, in_maps, core_ids=list(range(B)))
    out = np.stack([np.asarray(res.results[b]["out"], dtype=np.float32) for b in range(B)], 0)
    return out
```

```python
import numpy as np
import ml_dtypes
from contextlib import ExitStack
import concourse.bass as bass
import concourse.mybir as mybir
from concourse.bass_utils import run_bass_kernel_spmd

F32 = mybir.dt.float32
BF16 = mybir.dt.bfloat16
AF = mybir.ActivationFunctionType
ALU = mybir.AluOpType
AX = mybir.AxisListType

D = 1024
A_COLS = 1792
B_COLS = 1552
EVEN_COLS = 3344
ODD_COLS = 1604
NE = 16
DE = 256
DN_ALPHA = 4 ** 0.25
LN_EPS = 1e-5
DEC = 0.6065306597126334
DSA_INTERLEAVE = True
RWKV_INTERLEAVE = False


class Tok:
    __slots__ = ("w", "r")

    def __init__(self):
        self.w = None
        self.r = {}


class Eng:
    def __init__(self, name, h, sem):
        self.name = name
        self.h = h
        self.sem = sem
        self.cnt = 0
        self.waited = {}


class Prog:
    NSLOT = 8

    def __init__(self, nc, es):
        self.nc = nc
        self.es = es
        self.E = {}
        for name, h in (("pe", nc.tensor), ("act", nc.scalar), ("dve", nc.vector),
                        ("pool", nc.gpsimd), ("sp", nc.sync)):
            sem = es.enter_context(nc.semaphore("sem_" + name))
            self.E[name] = Eng(name, h, sem)
        self.slots = {}
        self.dn = {}
        for q in ("sp", "pool", "act"):
            self.slots[q] = [[es.enter_context(nc.semaphore("dq_%s%d" % (q, i))), 0] for i in range(self.NSLOT)]
            self.dn[q] = 0
        self.nalloc = 0

    def sb(self, shape, dt=F32, name=None, es=None):
        self.nalloc += 1
        t = (es or self.es).enter_context(self.nc.sbuf_tensor("%s_%d" % (name or "t", self.nalloc), list(shape), dt))
        return t

    def _wait(self, eng, ev):
        sem, val = ev
        key = sem.num
        if eng.waited.get(key, 0) >= val:
            return
        eng.h.wait_ge(sem, val)
        eng.waited[key] = val

    def _deps(self, en, r, w):
        eng = self.E[en]
        for t in r:
            if t.w is not None:
                yield t.w
        for t in w:
            if t.w is not None:
                yield t.w
            for ev in t.r.values():
                yield ev

    def op(self, en, fn, r=(), w=(), inc=True):
        eng = self.E[en]
        for ev in list(self._deps(en, r, w)):
            if en == "pe" and ev[0] is eng.sem:
                continue
            self._wait(eng, ev)
        ins = fn(eng.h)
        myev = (eng.sem, eng.cnt + 1)
        if inc:
            ins.then_inc(eng.sem, 1)
            eng.cnt += 1
        for t in r:
            t.r[en] = myev
        for t in w:
            t.w = myev
            t.r = {}
        return ins

    def dma(self, qn, out, in_, r=(), w=(), **kw):
        q = self.E[qn]
        for ev in list(self._deps(qn, r, w)):
            self._wait(q, ev)
        slot = self.slots[qn][self.dn[qn] % self.NSLOT]
        self.dn[qn] += 1
        if slot[1] > 0:
            self._wait(q, (slot[0], slot[1]))
        ins = q.h.dma_start(out=out, in_=in_, **kw)
        slot[1] += 16
        ins.then_inc(slot[0], 16)
        ev = (slot[0], slot[1])
        key = "d%d" % slot[0].num
        for t in r:
            t.r[key] = ev
        for t in w:
            t.w = ev
            t.r = {}

    def barrier(self):
        evs = []
        for q in self.slots:
            for sem, val in self.slots[q]:
                if val > 0:
                    evs.append((sem, val))
        for name, e in self.E.items():
            if e.cnt > 0:
                evs.append((e.sem, e.cnt))
        for name, e in self.E.items():
            for ev in evs:
                if ev[0] is e.sem and name == "pe":
                    continue
                self._wait(e, ev)

    def finish(self):
        sp = self.E["sp"]
        for q in self.slots:
            for sem, val in self.slots[q]:
                if val > 0:
                    self._wait(sp, (sem, val))
        for name, e in self.E.items():
            if name != "sp" and e.cnt > 0:
                self._wait(sp, (e.sem, e.cnt))


def host_consts(T):
    c = {}
    c["ident"] = np.eye(128, dtype=np.float32)
    j = np.arange(64)[:, None]
    i = np.arange(64)[None, :]
    c["tri64"] = np.stack([(-DEC) * (j <= i), (-DEC) * (j < i), (-DEC) * (j > i)], 1).astype(np.float32)
    c["ncol64"] = np.full((64, 1), -DEC, np.float32)
    su = (j < i).astype(np.float32)
    iu = (j <= i).astype(np.float32)
    sl = (j > i).astype(np.float32)
    mMA = np.concatenate([-su, iu], 1)
    mBB = np.concatenate([su, iu], 1)
    c["mMA"] = np.tile(mMA[:, None, :], (1, 8, 1)).astype(np.float32)
    c["mBB"] = np.tile(mBB[:, None, :], (1, 8, 1)).astype(np.float32)
    c["mNT"] = np.tile((-sl)[:, None, :], (1, 8, 1)).astype(np.float32)
    c["id8"] = np.tile(np.eye(64, dtype=np.float32)[:, None, :], (1, 8, 1))
    j = np.arange(128)[:, None]
    i = np.arange(128)[None, :]
    c["tri128"] = np.stack([(-1 / 16) * (j <= i), (-1 / 16) * (j > i)], 1).astype(np.float32)
    c["ncol128"] = np.full((128, 1), -1 / 16, np.float32)
    c["sel"] = (np.arange(16)[:, None, None] == np.arange(16)[None, :, None]).astype(np.float32) * np.ones((1, 1, 128), np.float32)
    c["iu128"] = np.tile((j <= i).astype(np.float32)[:, None, :], (1, 4, 1))
    return c


class Ctx:
    pass


def build(T, dbg=(), stages=("A", "R", "G", "O0", "M0", "S1", "O1", "M1")):
    nc = bass.Bass("TRN2", target_bir_lowering=False)
    es = ExitStack()
    P = Prog(nc, es)
    NT = T // 128
    NS = T // 512
    C = Ctx()
    C.nc, C.P, C.T, C.NT, C.NS = nc, P, T, NT, NS
    C.dbg = {}
    C.dsa_interleave = DSA_INTERLEAVE

    def din(name, shape, dt=F32):
        return nc.dram_tensor(name, list(shape), dt, kind="ExternalInput").ap()

    def dscr(name, shape, dt=F32, out=False):
        kind = "ExternalOutput" if (out or name in dbg) else "Internal"
        return nc.dram_tensor(name, list(shape), dt, kind=kind).ap()

    I = {}
    I["x"] = din("x", [T, D])
    for name, shape in (("w_in_even", [D, EVEN_COLS]), ("a_mu", [1, A_COLS]), ("a_w0", [1, 512]), ("a_w2", [64, 512]),
                        ("a_a0", [1, 512]), ("a_a2", [64, 512]), ("a_g2", [128, 512]), ("a_kk_scale", [1, 512]),
                        ("a_ka_scale", [1, 512]), ("a_r_k", [1, 512]), ("a_gn_g", [1, 512]), ("a_gn_b", [1, 512]),
                        ("b_gate_w2", [16, 256]), ("b_gate_b", [1, 256]), ("b_norm_g", [1, 512]),
                        ("w_out_even", [D, D]), ("w_in_odd", [D, ODD_COLS]), ("c_ik_ln_g", [1, 64]),
                        ("c_ik_ln_b", [1, 64]), ("w_out_odd", [D, D]), ("ln1_g", [2, D]), ("ln1_b", [2, D]),
                        ("ln2_g", [2, D]), ("ln2_b", [2, D]), ("router_w", [D, NE]), ("router_bias", [1, NE]),
                        ("exp_w_gate", [2, NE, D, DE]), ("exp_w_up", [2, NE, D, DE]), ("exp_w_down", [2, NE, DE, D])):
        I[name] = din(name, shape)
    hc = host_consts(T)
    hc.update(rope_consts(T))
    for k, v in hc.items():
        I["c_" + k] = din("c_" + k, list(v.shape), F32 if v.dtype == np.float32 else BF16)
    C.I = I
    out = dscr("out", [T, D], out=True)
    C.XT0 = dscr("XT0", [D, T + 1], BF16)
    C.XT0_tok = [Tok() for _ in range(NS)]
    C.XT0_z = Tok()
    C.YT = dscr("YT", [D, T], BF16)
    C.YT_tok = [[Tok() for _ in range(NS)] for _ in range(2)]
    C.H0 = dscr("H0", [T, D])
    C.H0_tok = [Tok() for _ in range(NT)]
    C.HT0 = dscr("HT0", [D, T], BF16)
    C.HT0_tok = [Tok() for _ in range(NS)]
    C.X1 = dscr("X1", [T, D])
    C.X1_tok = [Tok() for _ in range(NT)]
    C.XT1 = dscr("XT1", [D, T], BF16)
    C.XT1_tok = [Tok() for _ in range(NS)]

    for nm, shp in (("dbg_ya", [T, 512]), ("dbg_yb", [T, 512]), ("dbg_dsa", [T, 1024]), ("dbg_mask", [T, T]), ("dbg_sc", [T, T])):
        if nm in dbg:
            C.dbg[nm] = dscr(nm, shp, out=True)
    C.WGU16 = [dscr("WGU16_%d" % l, [NE, 128, 2 * 8 * DE], BF16) for l in range(2)]
    C.WGU16_tok = [[Tok() for _ in range(NE)] for l in range(2)]
    C.WD16 = [dscr("WD16_%d" % l, [128, 32 * D], BF16) for l in range(2)]
    C.WD16_tok = [Tok() for l in range(2)]
    C.banks = []
    for b in range(8):
        t = es.enter_context(nc.psum_tensor("psb%d" % b, [128, 512], F32))
        C.banks.append((t, Tok()))
    C.bi = 0

    C.bank_pool = list(range(8))

    def bank():
        b = C.banks[C.bank_pool[C.bi % len(C.bank_pool)]]
        C.bi += 1
        return b
    C.bank = bank

    C.ident = P.sb([128, 128], F32, "ident")
    C.ident_tok = Tok()
    P.dma("sp", C.ident[:], I["c_ident"], w=[C.ident_tok])

    C.cast_todo = {}
    if "M0" in stages:
        cast_weights(C, 0)
    if "A" in stages:
        phase_A(C)
    if "R" in stages:
        phase_rwkv(C)
    if "G" in stages:
        phase_gla(C)
    if "M1" in stages:
        cast_weights(C, 1)
    if "O0" in stages:
        phase_outproj(C, I["w_out_even"], C.YT, lambda s: [C.YT_tok[0][s], C.YT_tok[1][s]], I["x"], lambda i: [],
                      I["ln1_g"][0:1, :], I["ln1_b"][0:1, :], C.H0, C.H0_tok, C.HT0, C.HT0_tok)
    if "M0" in stages:
        cast_some(C, 0, 999)
        phase_moe(C, 0, C.H0, C.H0_tok, C.HT0, C.HT0_tok, C.X1, C.X1_tok, C.XT1, C.XT1_tok)
    if "S1" in stages:
        phase_dsa(C)
    if "O1" in stages:
        C.H1 = dscr("H1", [T, D])
        C.H1_tok = [Tok() for _ in range(NT)]
        C.HT1 = dscr("HT1", [D, T], BF16)
        C.HT1_tok = [Tok() for _ in range(NS)]
        phase_outproj(C, I["w_out_odd"], C.YT, lambda s: [C.YT_tok[0][s], C.YT_tok[1][s]], C.X1, lambda i: [C.X1_tok[i]],
                      I["ln1_g"][1:2, :], I["ln1_b"][1:2, :], C.H1, C.H1_tok, C.HT1, C.HT1_tok)
    if "M1" in stages:
        cast_some(C, 1, 999)
        out_tok = [Tok() for _ in range(NT)]
        phase_moe(C, 1, C.H1, C.H1_tok, C.HT1, C.HT1_tok, out, out_tok, None, None)
    P.finish()
    es.close()
    return nc


def XTv(ap):
    return ap.rearrange("(c p) t -> p c t", p=128)


def cast_weights(C, l):
    P, I = C.P, C.I
    th = []
    for e in range(NE):
        dst = C.WGU16[l][e].rearrange("p (t c f) -> p t c f", t=2, c=8)
        th.append(lambda e=e, dst=dst: P.dma("pool", dst[:, 0, :, :], I["exp_w_gate"][l, e].rearrange("(c p) f -> p c f", p=128), w=[C.WGU16_tok[l][e]]))
        th.append(lambda e=e, dst=dst: P.dma("pool", dst[:, 1, :, :], I["exp_w_up"][l, e].rearrange("(c p) f -> p c f", p=128), w=[C.WGU16_tok[l][e]]))
    wd_flat = I["exp_w_down"][l].rearrange("e f d -> (e f) d")
    dstd = C.WD16[l].rearrange("p (c d) -> p c d", c=32)
    for c4 in range(8):
        th.append(lambda c4=c4: P.dma("pool", dstd[:, c4 * 4:(c4 + 1) * 4, :], wd_flat[c4 * 512:(c4 + 1) * 512, :].rearrange("(c p) d -> p c d", p=128), w=[C.WD16_tok[l]]))
    C.cast_todo[l] = th


def cast_some(C, l, n):
    th = C.cast_todo.get(l, [])
    for _ in range(min(n, len(th))):
        th.pop(0)()


def phase_A(C):
    nc, P, T, I = C.nc, C.P, C.T, C.I
    with ExitStack() as es:
        xin = [P.sb([128, D], F32, "xin", es) for _ in range(2)]
        xin_tok = [Tok(), Tok()]
        st = [P.sb([128, 8, 512], BF16, "ast", es) for _ in range(2)]
        st_tok = [Tok(), Tok()]
        z = P.sb([128, 8, 1], BF16, "zc", es)
        zt = Tok()
        P.op("dve", lambda e: e.memset(z[:], 0.0), w=[zt])
        P.dma("sp", XTv(C.XT0)[:, :, 0:1], z[:], r=[zt], w=[C.XT0_z], allow_slow_non_contiguous=True)
        for s in range(C.NS):
            sb_ = st[s % 2]
            for j in range(4):
                i = s * 4 + j
                xb = xin[i % 2]
                xt = xin_tok[i % 2]
                P.dma("sp", xb[:], I["x"][i * 128:(i + 1) * 128, :], w=[xt])
                for half in range(2):
                    bt, bk = C.bank()
                    for c4 in range(4):
                        c = half * 4 + c4
                        P.op("pe", lambda e: e.transpose(bt[:, c4 * 128:(c4 + 1) * 128], xb[:, c * 128:(c + 1) * 128], C.ident[:]),
                             r=[xt, C.ident_tok], w=[bk], inc=(c4 == 3))
                    en = "act" if half == 0 else "dve"
                    src = bt[:, :].rearrange("p (c t) -> p c t", c=4)
                    dst = sb_[:, half * 4:(half + 1) * 4, j * 128:(j + 1) * 128]
                    if en == "act":
                        P.op("act", lambda e: e.copy(dst, src), r=[bk], w=[st_tok[s % 2]])
                    else:
                        P.op("dve", lambda e: e.tensor_copy(dst, src), r=[bk], w=[st_tok[s % 2]])
            P.dma("sp", XTv(C.XT0)[:, :, 1 + s * 512:1 + (s + 1) * 512], sb_[:], r=[st_tok[s % 2]], w=[C.XT0_tok[s]])
        P.barrier()


class TL:
    def __init__(self, t, k=None):
        self.t = t
        self.k = k or Tok()

    def __getitem__(self, idx):
        return self.t[idx]


def mk(P, shape, dt=F32, name=None, es=None):
    return TL(P.sb(shape, dt, name, es))


def bcast_load(C, dst_ap, src_row, np_, tok, q="sp"):
    C.P.dma(q, dst_ap, src_row.partition_broadcast(np_), w=[tok])


def hv(ap, h):
    return ap.rearrange("p (h v) -> p h v", h=h)


def phase_rwkv(C):
    nc, P, T, I = C.nc, C.P, C.T, C.I
    mm = ALU.mult
    with ExitStack() as es:
        W1 = mk(P, [128, 8, A_COLS], BF16, "W1", es)
        W2 = mk(P, [128, 8, A_COLS], BF16, "W2", es)
        with ExitStack() as es2:
            mub = mk(P, [128, A_COLS], F32, "mub", es2)
            omu = mk(P, [128, A_COLS], F32, "omu", es2)
            stg = [mk(P, [128, A_COLS], F32, "wstg", es2) for _ in range(2)]
            bcast_load(C, mub[:], I["a_mu"], 128, mub.k)
            P.op("dve", lambda e: e.tensor_scalar(omu[:], mub[:], -1.0, 1.0, ALU.mult, ALU.add), r=[mub.k], w=[omu.k])
            for c in range(8):
                s_ = stg[c % 2]
                P.dma("sp", s_[:], I["w_in_even"][c * 128:(c + 1) * 128, 0:A_COLS], w=[s_.k])
                P.op("dve", lambda e: e.tensor_tensor(W1[:, c, :], s_[:], omu[:], mm), r=[s_.k, omu.k], w=[W1.k])
                P.op("pool", lambda e: e.tensor_tensor(W2[:, c, :], s_[:], mub[:], mm), r=[s_.k, mub.k], w=[W2.k])
            P.barrier()
        LW = mk(P, [128, 512], BF16, "LW", es)
        G2 = mk(P, [128, 512], BF16, "G2", es)
        P.dma("pool", LW[0:64, :], I["a_w2"], w=[LW.k])
        P.dma("pool", LW[64:128, :], I["a_a2"], w=[LW.k])
        P.dma("pool", G2[:], I["a_g2"], w=[G2.k])
        BV = mk(P, [64, 7, 512], F32, "BV", es)
        for n, name in enumerate(("a_w0", "a_a0", "a_kk_scale", "a_ka_scale", "a_r_k", "a_gn_g", "a_gn_b")):
            bcast_load(C, BV[:, n, :], I[name], 64, BV.k)
        w0b, a0b, kksb, kab, rkb, gngb, gnbb = [BV[:, n, :] for n in range(7)]
        tri = mk(P, [64, 3, 64], F32, "tri", es)
        ncol = mk(P, [64, 1], F32, "ncol", es)
        mMA = mk(P, [64, 8, 128], F32, "mMA", es)
        mBB = mk(P, [64, 8, 128], F32, "mBB", es)
        mNT = mk(P, [64, 8, 64], F32, "mNT", es)
        id8 = mk(P, [64, 8, 64], F32, "id8", es)
        for tl, nm in ((tri, "c_tri64"), (ncol, "c_ncol64"), (mMA, "c_mMA"), (mBB, "c_mBB"), (mNT, "c_mNT"), (id8, "c_id8")):
            P.dma("sp", tl[:], I[nm], w=[tl.k])
        id64 = C.ident[0:64, 0:64]

        def wt(name, shape=(64, 512), dt=F32):
            return mk(P, list(shape), dt, name, es)
        ATs = [mk(P, [128, 8, 513], BF16, "ATs", es) for _ in range(2)]
        TX = wt("TX", (128, 512), BF16)
        SG = wt("SG", (128, 512), BF16)
        r_, k_, v_, sg, a_, kk, be, Bi, Ki, tmp = [wt(n) for n in ("r", "k", "v", "sg", "a", "kk", "be", "Bi", "Ki", "tmp")]
        Ep, Em, Ex, Ee = [wt(n) for n in ("Ep", "Em", "Ex", "Ee")]
        s8 = [wt("s8_%d" % n, (64, 8)) for n in range(4)]
        KRs = [wt("KR", (64, 8, 128), BF16) for _ in range(2)]
        BiT = wt("BiT", (64, 8, 64), BF16)
        KiT = wt("KiT", (64, 8, 64), BF16)
        MAs = [wt("MA", (64, 8, 128), BF16) for _ in range(2)]
        BBs = [wt("BB", (64, 8, 128), BF16) for _ in range(2)]
        Xb = [wt("X%d" % n, (64, 8, 64), BF16) for n in range(2)]
        XTb = [wt("XT%d" % n, (64, 8, 64), BF16) for n in range(2)]
        Qbs = [[wt("Q%d" % n, (64, 8, 64), BF16) for n in range(2)] for _ in range(2)]
        Xs = wt("Xs", (64, 8, 64), BF16)
        nU = wt("nU", (64, 8, 64), BF16)
        Y = wt("Y")
        tmpb = wt("tmpb")
        Hs = [wt("H%d" % n, (64, 8, 64)) for n in range(2)]
        Hbs = [wt("Hb%d" % n, (64, 8, 64), BF16) for n in range(2)]
        vbs = [wt("vb", (64, 512), BF16) for _ in range(2)]
        Ke16s = [wt("Ke16", (64, 512), BF16) for _ in range(2)]
        Be16s = [wt("Be16", (64, 512), BF16) for _ in range(2)]
        PCs = [wt("PC", (64, 8)) for _ in range(2)]
        gs_ = [wt("g", (64, 512)) for _ in range(2)]
        bonuss = [wt("bonus", (64, 512)) for _ in range(2)]
        P.op("pool", lambda e: e.memset(Hbs[0][:], 0.0), w=[Hbs[0].k])
        yst = [mk(P, [128, 4, 512], BF16, "yst", es) for _ in range(2)]
        P.op("dve", lambda e: e.memset(Hs[0][:], 0.0), w=[Hs[0].k])

        def psb():
            t, k = C.bank()
            return TL(t, k)

        def v3(tl_or_ap, h=8):
            return hv(tl_or_ap, h)

        def chunk(s, ci):
          at = ATs[s % 2]
          ys = yst[s % 2]
          cast_some(C, 0, 1)
          if ci == 0:
            rd = [C.XT0_tok[s]] + ([C.XT0_tok[s - 1]] if s > 0 else [C.XT0_z])
            P.dma("sp", at[:], XTv(C.XT0)[:, :, s * 512:s * 512 + 513], r=rd, w=[at.k])
            for which in range(2):
                pb = psb()
                c0 = 1536 + which * 128
                for c in range(8):
                    P.op("pe", lambda e: e.matmul(pb[:, :], lhsT=W1[:, c, c0:c0 + 128], rhs=at[:, c, 1:513], start=(c == 0), stop=False),
                         r=[W1.k, at.k], w=[pb.k], inc=False)
                for c in range(8):
                    P.op("pe", lambda e: e.matmul(pb[:, :], lhsT=W2[:, c, c0:c0 + 128], rhs=at[:, c, 0:512], start=False, stop=(c == 7)),
                         r=[W2.k, at.k], w=[pb.k], inc=(c == 7))
                if which == 0:
                    P.op("act", lambda e: e.activation(out=TX[0:64, :], in_=pb[0:64, :], func=AF.Tanh), r=[pb.k], w=[TX.k])
                    P.op("act", lambda e: e.copy(TX[64:128, :], pb[64:128, :]), r=[pb.k], w=[TX.k])
                else:
                    P.op("act", lambda e: e.activation(out=SG[:, :], in_=pb[:, :], func=AF.Sigmoid), r=[pb.k], w=[SG.k])
          if True:
            if True:
                g = s * 8 + ci
                t0 = ci * 64
                KR, MA, BB, Qb = KRs[g % 2], MAs[g % 2], BBs[g % 2], Qbs[g % 2]
                vb, Ke16, Be16, PC, g_, bonus = vbs[g % 2], Ke16s[g % 2], Be16s[g % 2], PCs[g % 2], gs_[g % 2], bonuss[g % 2]
                pr, pk, pv = psb(), psb(), psb()
                for pb, c0 in ((pr, 0), (pk, 512), (pv, 1024)):
                    for c in range(8):
                        P.op("pe", lambda e: e.matmul(pb[0:64, :], lhsT=at[:, c, 1 + t0:1 + t0 + 64], rhs=W1[:, c, c0:c0 + 512], start=(c == 0), stop=False),
                             r=[W1.k, at.k], w=[pb.k], inc=False)
                    for c in range(8):
                        P.op("pe", lambda e: e.matmul(pb[0:64, :], lhsT=at[:, c, t0:t0 + 64], rhs=W2[:, c, c0:c0 + 512], start=False, stop=(c == 7)),
                             r=[W2.k, at.k], w=[pb.k], inc=(c == 7))
                yield "F"
                pz, pza, pg = psb(), psb(), psb()
                P.op("pe", lambda e: e.matmul(pz[0:64, :], lhsT=TX[0:64, t0:t0 + 64], rhs=LW[0:64, :], start=True, stop=True), r=[TX.k, LW.k], w=[pz.k])
                P.op("pe", lambda e: e.matmul(pza[0:64, :], lhsT=TX[64:128, t0:t0 + 64], rhs=LW[64:128, :], start=True, stop=True), r=[TX.k, LW.k], w=[pza.k])
                P.op("pe", lambda e: e.matmul(pg[0:64, :], lhsT=SG[:, t0:t0 + 64], rhs=G2[:, :], start=True, stop=True), r=[SG.k, G2.k], w=[pg.k])
                P.op("act", lambda e: e.copy(r_[:], pr[0:64, :]), r=[pr.k], w=[r_.k])
                P.op("act", lambda e: e.copy(v_[:], pv[0:64, :]), r=[pv.k], w=[v_.k])
                P.op("act", lambda e: e.copy(vb[:], pv[0:64, :]), r=[pv.k], w=[vb.k])
                P.op("act", lambda e: e.copy(g_[:], pg[0:64, :]), r=[pg.k], w=[g_.k])
                P.op("dve", lambda e: e.tensor_copy(k_[:], pk[0:64, :]), r=[pk.k], w=[k_.k])
                P.op("dve", lambda e: e.tensor_tensor(sg[:], pz[0:64, :], w0b, ALU.add), r=[pz.k, BV.k], w=[sg.k])
                P.op("act", lambda e: e.activation(out=sg[:], in_=sg[:], func=AF.Sigmoid), r=[sg.k], w=[sg.k])
                P.op("dve", lambda e: e.tensor_tensor(a_[:], pza[0:64, :], a0b, ALU.add), r=[pza.k, BV.k], w=[a_.k])
                P.op("act", lambda e: e.activation(out=a_[:], in_=a_[:], func=AF.Sigmoid), r=[a_.k], w=[a_.k])
                yield "F"
                P.op("pool", lambda e: e.tensor_tensor(kk[:], k_[:], kksb, mm), r=[k_.k, BV.k], w=[kk.k])
                P.op("pool", lambda e: e.tensor_tensor(tmp[:], kk[:], kk[:], mm), r=[kk.k], w=[tmp.k])
                P.op("dve", lambda e: e.tensor_reduce(s8[0][:], v3(tmp[:]), AX.X, ALU.add), r=[tmp.k], w=[s8[0].k])
                P.op("dve", lambda e: e.tensor_scalar(s8[0][:], s8[0][:], 1e-24, None, ALU.max), r=[s8[0].k], w=[s8[0].k])
                P.op("act", lambda e: e.activation(out=s8[0][:], in_=s8[0][:], func=AF.Ln), r=[s8[0].k], w=[s8[0].k])
                P.op("act", lambda e: e.activation(out=s8[0][:], in_=s8[0][:], func=AF.Exp, scale=-0.5), r=[s8[0].k], w=[s8[0].k])
                P.op("dve", lambda e: e.tensor_tensor(v3(kk[:]), v3(kk[:]), s8[0][:, :].unsqueeze(2).to_broadcast([64, 8, 64]), mm),
                     r=[kk.k, s8[0].k], w=[kk.k])
                P.op("pool", lambda e: e.tensor_tensor(be[:], kk[:], a_[:], mm), r=[kk.k, a_.k], w=[be.k])
                P.op("dve", lambda e: e.scalar_tensor_tensor(tmp[:], a_[:], -1.0, kab, ALU.add, mm), r=[a_.k, BV.k], w=[tmp.k])
                P.op("dve", lambda e: e.scalar_tensor_tensor(k_[:], tmp[:], 1.0, k_[:], ALU.add, mm), r=[tmp.k, k_.k], w=[k_.k])
                P.op("pool", lambda e: e.tensor_tensor(tmp[:], r_[:], k_[:], mm), r=[r_.k, k_.k], w=[tmp.k])
                P.op("pool", lambda e: e.tensor_tensor(tmp[:], tmp[:], rkb, mm), r=[tmp.k, BV.k], w=[tmp.k])
                P.op("dve", lambda e: e.tensor_reduce(s8[1][:], v3(tmp[:]), AX.X, ALU.add), r=[tmp.k], w=[s8[1].k])
                P.op("dve", lambda e: e.tensor_tensor(v3(bonus[:]), v3(v_[:]), s8[1][:, :].unsqueeze(2).to_broadcast([64, 8, 64]), mm),
                     r=[v_.k, s8[1].k], w=[bonus.k])
                yield "F"
                pcl, pcx, pca, ppc = psb(), psb(), psb(), psb()
                for pb, n in ((pcl, 0), (pcx, 1), (pca, 2)):
                    P.op("pe", lambda e: e.matmul(pb[0:64, :], lhsT=tri[:, n, :], rhs=sg[:], start=True, stop=True), r=[tri.k, sg.k], w=[pb.k])
                for h in range(8):
                    P.op("pe", lambda e: e.matmul(ppc[0:64, h:h + 1], lhsT=sg[:, h * 64:(h + 1) * 64], rhs=ncol[:], start=True, stop=True),
                         r=[sg.k, ncol.k], w=[ppc.k], inc=(h == 7))
                P.op("act", lambda e: e.activation(out=Ep[:], in_=pcl[0:64, :], func=AF.Exp), r=[pcl.k], w=[Ep.k])
                P.op("act", lambda e: e.activation(out=Em[:], in_=pcl[0:64, :], func=AF.Exp, scale=-1.0), r=[pcl.k], w=[Em.k])
                P.op("act", lambda e: e.activation(out=Ex[:], in_=pcx[0:64, :], func=AF.Exp), r=[pcx.k], w=[Ex.k])
                P.op("act", lambda e: e.activation(out=Ee[:], in_=pca[0:64, :], func=AF.Exp), r=[pca.k], w=[Ee.k])
                P.op("act", lambda e: e.activation(out=PC[:], in_=ppc[0:64, 0:8], func=AF.Exp), r=[ppc.k], w=[PC.k])
                P.op("dve", lambda e: e.tensor_tensor(r_[:], r_[:], Ep[:], mm), r=[r_.k, Ep.k], w=[r_.k])
                P.op("pool", lambda e: e.tensor_tensor(kk[:], kk[:], Ex[:], mm), r=[kk.k, Ex.k], w=[kk.k])
                P.op("dve", lambda e: e.tensor_tensor(Bi[:], be[:], Em[:], mm), r=[be.k, Em.k], w=[Bi.k])
                P.op("pool", lambda e: e.tensor_tensor(Ki[:], k_[:], Em[:], mm), r=[k_.k, Em.k], w=[Ki.k])
                P.op("dve", lambda e: e.tensor_tensor(Ke16[:], k_[:], Ee[:], mm), r=[k_.k, Ee.k], w=[Ke16.k])
                P.op("pool", lambda e: e.tensor_tensor(Be16[:], be[:], Ee[:], mm), r=[be.k, Ee.k], w=[Be16.k])
                yield "F"
                for src, dst, off, en in ((kk, KR, 0, "act"), (r_, KR, 64, "dve"), (Bi, BiT, 0, "act"), (Ki, KiT, 0, "dve")):
                    pb = psb()
                    for h in range(8):
                        P.op("pe", lambda e: e.transpose(pb[0:64, h * 64:(h + 1) * 64], src[:, h * 64:(h + 1) * 64], id64),
                             r=[src.k, C.ident_tok], w=[pb.k], inc=(h == 7))
                    d_ = dst[:, :, off:off + 64]
                    s_ = v3(pb[0:64, :])
                    if en == "act":
                        P.op("act", lambda e: e.copy(d_, s_), r=[pb.k], w=[dst.k])
                    else:
                        P.op("dve", lambda e: e.tensor_copy(d_, s_), r=[pb.k], w=[dst.k])
                yield "F"
                pma = [psb(), psb()]
                pbb = [psb(), psb()]
                pnt = psb()
                for h in range(8):
                    hb, hh = h // 4, h % 4
                    P.op("pe", lambda e: e.matmul(pma[hb][0:64, hh * 128:(hh + 1) * 128], lhsT=BiT[:, h, :], rhs=KR[:, h, :], start=True, stop=True),
                         r=[BiT.k, KR.k], w=[pma[hb].k], inc=(hh == 3))
                for h in range(8):
                    hb, hh = h // 4, h % 4
                    P.op("pe", lambda e: e.matmul(pbb[hb][0:64, hh * 128:(hh + 1) * 128], lhsT=KiT[:, h, :], rhs=KR[:, h, :], start=True, stop=True),
                         r=[KiT.k, KR.k], w=[pbb[hb].k], inc=(hh == 3))
                for h in range(8):
                    P.op("pe", lambda e: e.matmul(pnt[0:64, h * 64:(h + 1) * 64], lhsT=KR[:, h, 0:64], rhs=BiT[:, h, :], start=True, stop=True),
                         r=[BiT.k, KR.k], w=[pnt.k], inc=(h == 7))
                for hb in range(2):
                    P.op("dve", lambda e: e.tensor_tensor(MA[:, hb * 4:(hb + 1) * 4, :], hv(pma[hb][0:64, :], 4), mMA[:, hb * 4:(hb + 1) * 4, :], mm),
                         r=[pma[hb].k, mMA.k], w=[MA.k])
                    P.op("dve", lambda e: e.tensor_tensor(BB[:, hb * 4:(hb + 1) * 4, :], hv(pbb[hb][0:64, :], 4), mBB[:, hb * 4:(hb + 1) * 4, :], mm),
                         r=[pbb[hb].k, mBB.k], w=[BB.k])
                X, XT, Q = Xb[0], XTb[0], Qb[0]
                P.op("dve", lambda e: e.tensor_tensor(XT[:], v3(pnt[0:64, :]), mNT[:], mm), r=[pnt.k, mNT.k], w=[XT.k])
                P.op("pool", lambda e: e.tensor_copy(X[:], MA[:, :, 0:64]), r=[MA.k], w=[X.k])
                P.op("pool", lambda e: e.tensor_tensor(Q[:], MA[:, :, 0:64], id8[:], ALU.add), r=[MA.k, id8.k], w=[Q.k])
                for lvl in range(5):
                    Xn, XTn, Qn = Xb[(lvl + 1) % 2], XTb[(lvl + 1) % 2], Qb[(lvl + 1) % 2]
                    pxt = psb()
                    for h in range(8):
                        P.op("pe", lambda e: e.matmul(pxt[0:64, h * 64:(h + 1) * 64], lhsT=X[:, h, :], rhs=XT[:, h, :], start=True, stop=True),
                             r=[X.k, XT.k], w=[pxt.k], inc=(h == 7))
                    if lvl < 4:
                        px = psb()
                        for h in range(8):
                            P.op("pe", lambda e: e.matmul(px[0:64, h * 64:(h + 1) * 64], lhsT=XT[:, h, :], rhs=X[:, h, :], start=True, stop=True),
                                 r=[X.k, XT.k], w=[px.k], inc=(h == 7))
                    P.op("act", lambda e: e.copy(XTn[:], v3(pxt[0:64, :])), r=[pxt.k], w=[XTn.k])
                    if lvl < 4:
                        P.op("dve", lambda e: e.tensor_copy(Xn[:], v3(px[0:64, :])), r=[px.k], w=[Xn.k])
                    pq = psb()
                    for h in range(8):
                        P.op("pe", lambda e: e.matmul(pq[0:64, h * 64:(h + 1) * 64], lhsT=XTn[:, h, :], rhs=Q[:, h, :], start=True, stop=True),
                             r=[XTn.k, Q.k], w=[pq.k], inc=(h == 7))
                    P.op("dve", lambda e: e.tensor_tensor(Qn[:], Q[:], v3(pq[0:64, :]), ALU.add), r=[Q.k, pq.k], w=[Qn.k])
                    X, XT, Q = Xn, XTn, Qn
                    yield "F"
                yield "END_FRONT"
                H, Hn = Hs[g % 2], Hs[(g + 1) % 2]
                Hb, Hbn = Hbs[g % 2], Hbs[(g + 1) % 2]
                pxs = psb()
                for h in range(8):
                    P.op("pe", lambda e: e.matmul(pxs[0:64, h * 64:(h + 1) * 64], lhsT=KR[:, h, 0:64], rhs=Hb[:, h, :], start=True, stop=False),
                         r=[KR.k, Hb.k], w=[pxs.k], inc=False)
                    P.op("pe", lambda e: e.matmul(pxs[0:64, h * 64:(h + 1) * 64], lhsT=BB[:, h, 0:64], rhs=vb[:, h * 64:(h + 1) * 64], start=False, stop=True),
                         r=[BB.k, vb.k], w=[pxs.k], inc=(h == 7))
                P.op("act", lambda e: e.copy(Xs[:], v3(pxs[0:64, :])), r=[pxs.k], w=[Xs.k])
                yield "B"
                pu = psb()
                for h in range(8):
                    P.op("pe", lambda e: e.matmul(pu[0:64, h * 64:(h + 1) * 64], lhsT=Q[:, h, :], rhs=Xs[:, h, :], start=True, stop=True),
                         r=[Q.k, Xs.k], w=[pu.k], inc=(h == 7))
                P.op("act", lambda e: e.mul(nU[:], v3(pu[0:64, :]), -1.0), r=[pu.k], w=[nU.k])
                yield "B"
                py, ph = psb(), psb()
                for h in range(8):
                    sl = slice(h * 64, (h + 1) * 64)
                    P.op("pe", lambda e: e.matmul(py[0:64, sl], lhsT=KR[:, h, 64:128], rhs=Hb[:, h, :], start=True, stop=False), r=[KR.k, Hb.k], w=[py.k], inc=False)
                    P.op("pe", lambda e: e.matmul(py[0:64, sl], lhsT=BB[:, h, 64:128], rhs=vb[:, sl], start=False, stop=False), r=[BB.k, vb.k], w=[py.k], inc=False)
                    P.op("pe", lambda e: e.matmul(py[0:64, sl], lhsT=MA[:, h, 64:128], rhs=nU[:, h, :], start=False, stop=True), r=[MA.k, nU.k], w=[py.k], inc=(h == 7))
                for h in range(8):
                    sl = slice(h * 64, (h + 1) * 64)
                    P.op("pe", lambda e: e.matmul(ph[0:64, sl], lhsT=Ke16[:, sl], rhs=vb[:, sl], start=True, stop=False), r=[Ke16.k, vb.k], w=[ph.k], inc=False)
                    P.op("pe", lambda e: e.matmul(ph[0:64, sl], lhsT=Be16[:, sl], rhs=nU[:, h, :], start=False, stop=True), r=[Be16.k, nU.k], w=[ph.k], inc=(h == 7))
                P.op("pool", lambda e: e.tensor_tensor(Hn[:], H[:], PC[:, :].unsqueeze(2).to_broadcast([64, 8, 64]), mm), r=[H.k, PC.k], w=[Hn.k])
                P.op("dve", lambda e: e.tensor_tensor(Hn[:], Hn[:], v3(ph[0:64, :]), ALU.add), r=[Hn.k, ph.k], w=[Hn.k])
                P.op("act", lambda e: e.copy(Hbn[:], Hn[:]), r=[Hn.k], w=[Hbn.k])
                yield "B"
                P.op("act", lambda e: e.copy(Y[:], py[0:64, :]), r=[py.k], w=[Y.k])
                P.op("dve", lambda e: e.tensor_reduce(s8[2][:], v3(Y[:]), AX.X, ALU.add), r=[Y.k], w=[s8[2].k])
                P.op("dve", lambda e: e.tensor_scalar(s8[2][:], s8[2][:], 1.0 / 64, None, mm), r=[s8[2].k], w=[s8[2].k])
                P.op("dve", lambda e: e.tensor_tensor(v3(Y[:]), v3(Y[:]), s8[2][:, :].unsqueeze(2).to_broadcast([64, 8, 64]), ALU.subtract),
                     r=[Y.k, s8[2].k], w=[Y.k])
                yield "B"
                P.op("pool", lambda e: e.tensor_tensor(tmpb[:], Y[:], Y[:], mm), r=[Y.k], w=[tmpb.k])
                P.op("dve", lambda e: e.tensor_reduce(s8[3][:], v3(tmpb[:]), AX.X, ALU.add), r=[tmpb.k], w=[s8[3].k])
                P.op("act", lambda e: e.activation(out=s8[3][:], in_=s8[3][:], func=AF.Ln, bias=64e-5, scale=1.0 / 64), r=[s8[3].k], w=[s8[3].k])
                P.op("act", lambda e: e.activation(out=s8[3][:], in_=s8[3][:], func=AF.Exp, scale=-0.5), r=[s8[3].k], w=[s8[3].k])
                P.op("dve", lambda e: e.tensor_tensor(v3(Y[:]), v3(Y[:]), s8[3][:, :].unsqueeze(2).to_broadcast([64, 8, 64]), mm),
                     r=[Y.k, s8[3].k], w=[Y.k])
                P.op("pool", lambda e: e.tensor_tensor(Y[:], Y[:], gngb, mm), r=[Y.k, BV.k], w=[Y.k])
                P.op("pool", lambda e: e.tensor_tensor(Y[:], Y[:], gnbb, ALU.add), r=[Y.k, BV.k], w=[Y.k])
                P.op("dve", lambda e: e.tensor_tensor(Y[:], Y[:], bonus[:], ALU.add), r=[Y.k, bonus.k], w=[Y.k])
                P.op("dve", lambda e: e.tensor_tensor(Y[:], Y[:], g_[:], mm), r=[Y.k, g_.k], w=[Y.k])
                if "dbg_ya" in C.dbg:
                    P.dma("sp", C.dbg["dbg_ya"][g * 64:(g + 1) * 64, :], Y[:], r=[Y.k])
                yield "B"
                pb = psb()
                for q in range(4):
                    P.op("pe", lambda e: e.transpose(pb[:, q * 64:(q + 1) * 64], Y[:, q * 128:(q + 1) * 128], id64), r=[Y.k, C.ident_tok], w=[pb.k], inc=(q == 3))
                P.op("act", lambda e: e.copy(ys[:, :, t0:t0 + 64], hv(pb[:, 0:256], 4)), r=[pb.k], w=[ys.k])
                if ci == 7:
                    P.dma("sp", XTv(C.YT)[:, 0:4, s * 512:(s + 1) * 512], ys[:], r=[ys.k], w=[C.YT_tok[0][s]])

        makers = [(lambda s=s, ci=ci: chunk(s, ci)) for s in range(C.NS) for ci in range(8)]
        if RWKV_INTERLEAVE:
            pipeline2(makers, interleave=True)
        else:
            def to_end_front(g_):
                while next(g_) != "END_FRONT":
                    pass
            g = makers[0]()
            to_end_front(g)
            for n in range(len(makers)):
                for _ in range(3):
                    next(g)
                g2 = None
                if n + 1 < len(makers):
                    g2 = makers[n + 1]()
                    next(g2)
                for _ in g:
                    pass
                if g2 is not None:
                    to_end_front(g2)
                g = g2
        P.barrier()


def pipeline2(makers, interleave=True):
    if not interleave:
        for mk_ in makers:
            for _ in mk_():
                pass
        return
    prevB = None
    for mk_ in makers:
        g = mk_()
        while True:
            r = next(g)
            if prevB is not None:
                try:
                    next(prevB)
                except StopIteration:
                    prevB = None
            if r == "END_FRONT":
                break
        if prevB is not None:
            for _ in prevB:
                pass
        prevB = g
    if prevB is not None:
        for _ in prevB:
            pass


def psb(C):
    t, k = C.bank()
    return TL(t, k)


def phase_gla(C):
    nc, P, T, I = C.nc, C.P, C.T, C.I
    mm = ALU.mult
    with ExitStack() as es:
        WB = mk(P, [128, 8, B_COLS], BF16, "WB", es)
        for c in range(8):
            P.dma("pool", WB[:, c, :], I["w_in_even"][c * 128:(c + 1) * 128, A_COLS:EVEN_COLS], w=[WB.k])
        GW2 = mk(P, [16, 256], BF16, "GW2", es)
        P.dma("pool", GW2[:], I["b_gate_w2"], w=[GW2.k])
        gbb = mk(P, [128, 256], F32, "gbb", es)
        ngb = mk(P, [128, 512], F32, "ngb", es)
        bcast_load(C, gbb[:], I["b_gate_b"], 128, gbb.k)
        bcast_load(C, ngb[:], I["b_norm_g"], 128, ngb.k)
        tri = mk(P, [128, 2, 128], F32, "tri128", es)
        ncol = mk(P, [128, 1], F32, "ncol128", es)
        iu = mk(P, [128, 4, 128], F32, "iu128", es)
        for tl, nm in ((tri, "c_tri128"), (ncol, "c_ncol128"), (iu, "c_iu128")):
            P.dma("sp", tl[:], I[nm], w=[tl.k])
        id64 = C.ident[0:64, 0:64]

        def wt(name, shape, dt=F32):
            return mk(P, list(shape), dt, name, es)
        ATs = [wt("ATg", (128, 8, 512), BF16) for _ in range(2)]
        AL = wt("AL", (16, 512), BF16)
        l_ = wt("l", (128, 256))
        Eq, Ei, Ee = wt("Eq", (128, 256)), wt("Ei", (128, 256)), wt("Ee", (128, 256))
        PCg = wt("PCg", (64, 4))
        qd, ki, ke = wt("qd", (128, 256)), wt("ki", (128, 256)), wt("ke", (128, 256))
        v_ = wt("vg", (128, 512))
        qdT, kiT = wt("qdT", (64, 4, 128)), wt("kiT", (64, 4, 128))
        attT = wt("attT", (128, 4, 128))
        Ss = [wt("S%d" % n, (64, 4, 128)) for n in range(2)]
        o_ = wt("o", (128, 512))
        sq = wt("sqg", (128, 512))
        sl_ = wt("silu", (128, 512))
        m4 = wt("m4", (128, 4))
        yst = [wt("ystg", (128, 4, 512), BF16) for _ in range(2)]
        P.op("dve", lambda e: e.memset(Ss[0][:], 0.0), w=[Ss[0].k])
        for s in range(C.NS):
            at = ATs[s % 2]
            P.dma("sp", at[:], XTv(C.XT0)[:, :, 1 + s * 512:1 + (s + 1) * 512], r=[C.XT0_tok[s]], w=[at.k])
            pb = psb(C)
            for c in range(8):
                P.op("pe", lambda e: e.matmul(pb[0:16, :], lhsT=WB[:, c, 1536:1552], rhs=at[:, c, :], start=(c == 0), stop=(c == 7)),
                     r=[WB.k, at.k], w=[pb.k], inc=(c == 7))
            P.op("act", lambda e: e.copy(AL[:], pb[0:16, :]), r=[pb.k], w=[AL.k])
            ys = yst[s % 2]
            for ci in range(4):
                g = s * 4 + ci
                t0 = ci * 128
                pqk, pv, pg = psb(C), psb(C), psb(C)
                for pb, c0 in ((pqk, 0), (pv, 512), (pg, 1024)):
                    for c in range(8):
                        P.op("pe", lambda e: e.matmul(pb[:, :], lhsT=at[:, c, t0:t0 + 128], rhs=WB[:, c, c0:c0 + 512], start=(c == 0), stop=(c == 7)),
                             r=[WB.k, at.k], w=[pb.k], inc=(c == 7))
                pla = psb(C)
                P.op("pe", lambda e: e.matmul(pla[:, 0:256], lhsT=AL[:, t0:t0 + 128], rhs=GW2[:], start=True, stop=True), r=[AL.k, GW2.k], w=[pla.k])
                P.op("dve", lambda e: e.tensor_tensor(l_[:], pla[:, 0:256], gbb[:], ALU.add), r=[pla.k, gbb.k], w=[l_.k])
                P.op("act", lambda e: e.activation(out=l_[:], in_=l_[:], func=AF.Exp, scale=-1.0), r=[l_.k], w=[l_.k])
                P.op("act", lambda e: e.activation(out=l_[:], in_=l_[:], func=AF.Ln, bias=1.0), r=[l_.k], w=[l_.k])
                pbc, pba, ppc = psb(C), psb(C), psb(C)
                P.op("pe", lambda e: e.matmul(pbc[:, 0:256], lhsT=tri[:, 0, :], rhs=l_[:], start=True, stop=True), r=[tri.k, l_.k], w=[pbc.k])
                P.op("pe", lambda e: e.matmul(pba[:, 0:256], lhsT=tri[:, 1, :], rhs=l_[:], start=True, stop=True), r=[tri.k, l_.k], w=[pba.k])
                for h in range(4):
                    P.op("pe", lambda e: e.matmul(ppc[0:64, h:h + 1], lhsT=l_[:, h * 64:(h + 1) * 64], rhs=ncol[:], start=True, stop=True),
                         r=[l_.k, ncol.k], w=[ppc.k], inc=(h == 3))
                P.op("act", lambda e: e.activation(out=Eq[:], in_=pbc[:, 0:256], func=AF.Exp), r=[pbc.k], w=[Eq.k])
                P.op("act", lambda e: e.activation(out=Ei[:], in_=pbc[:, 0:256], func=AF.Exp, scale=-1.0), r=[pbc.k], w=[Ei.k])
                P.op("act", lambda e: e.activation(out=Ee[:], in_=pba[:, 0:256], func=AF.Exp), r=[pba.k], w=[Ee.k])
                P.op("act", lambda e: e.activation(out=PCg[:], in_=ppc[0:64, 0:4], func=AF.Exp), r=[ppc.k], w=[PCg.k])
                P.op("dve", lambda e: e.scalar_tensor_tensor(qd[:], pqk[:, 0:256], 0.125, Eq[:], mm, mm), r=[pqk.k, Eq.k], w=[qd.k])
                P.op("dve", lambda e: e.tensor_tensor(ki[:], pqk[:, 256:512], Ei[:], mm), r=[pqk.k, Ei.k], w=[ki.k])
                P.op("dve", lambda e: e.tensor_tensor(ke[:], pqk[:, 256:512], Ee[:], mm), r=[pqk.k, Ee.k], w=[ke.k])
                P.op("act", lambda e: e.copy(v_[:], pv[:, :]), r=[pv.k], w=[v_.k])
                P.op("act", lambda e: e.activation(out=sl_[:], in_=pg[:, :], func=AF.Silu), r=[pg.k], w=[sl_.k])
                for src, dst, en in ((qd, qdT, "act"), (ki, kiT, "dve")):
                    pb = psb(C)
                    for h in range(4):
                        P.op("pe", lambda e: e.transpose(pb[0:64, h * 128:(h + 1) * 128], src[:, h * 64:(h + 1) * 64], C.ident[:]),
                             r=[src.k, C.ident_tok], w=[pb.k], inc=(h == 3))
                    if en == "act":
                        P.op("act", lambda e: e.copy(dst[:], hv(pb[0:64, :], 4)), r=[pb.k], w=[dst.k])
                    else:
                        P.op("dve", lambda e: e.tensor_copy(dst[:], hv(pb[0:64, :], 4)), r=[pb.k], w=[dst.k])
                patt = psb(C)
                for h in range(4):
                    P.op("pe", lambda e: e.matmul(patt[:, h * 128:(h + 1) * 128], lhsT=kiT[:, h, :], rhs=qdT[:, h, :], start=True, stop=True),
                         r=[kiT.k, qdT.k], w=[patt.k], inc=(h == 3))
                P.op("dve", lambda e: e.tensor_tensor(attT[:], hv(patt[:, :], 4), iu[:], mm), r=[patt.k, iu.k], w=[attT.k])
                S, Sn = Ss[g % 2], Ss[(g + 1) % 2]
                po, pS = psb(C), psb(C)
                for h in range(4):
                    sl = slice(h * 128, (h + 1) * 128)
                    P.op("pe", lambda e: e.matmul(po[:, sl], lhsT=attT[:, h, :], rhs=v_[:, sl], start=True, stop=False), r=[attT.k, v_.k], w=[po.k], inc=False)
                    P.op("pe", lambda e: e.matmul(po[:, sl], lhsT=qdT[:, h, :], rhs=S[:, h, :], start=False, stop=True), r=[qdT.k, S.k], w=[po.k], inc=(h == 3))
                for h in range(4):
                    sl = slice(h * 128, (h + 1) * 128)
                    P.op("pe", lambda e: e.matmul(pS[0:64, sl], lhsT=ke[:, h * 64:(h + 1) * 64], rhs=v_[:, sl], start=True, stop=True), r=[ke.k, v_.k], w=[pS.k], inc=(h == 3))
                P.op("pool", lambda e: e.tensor_tensor(Sn[:], S[:], PCg[:, :].unsqueeze(2).to_broadcast([64, 4, 128]), mm), r=[S.k, PCg.k], w=[Sn.k])
                P.op("dve", lambda e: e.tensor_tensor(Sn[:], Sn[:], hv(pS[0:64, :], 4), ALU.add), r=[Sn.k, pS.k], w=[Sn.k])
                P.op("act", lambda e: e.copy(o_[:], po[:, :]), r=[po.k], w=[o_.k])
                P.op("pool", lambda e: e.tensor_tensor(sq[:], o_[:], o_[:], mm), r=[o_.k], w=[sq.k])
                P.op("dve", lambda e: e.tensor_reduce(m4[:], hv(sq[:], 4), AX.X, ALU.add), r=[sq.k], w=[m4.k])
                P.op("act", lambda e: e.activation(out=m4[:], in_=m4[:], func=AF.Ln, bias=1e-5, scale=1.0 / 128), r=[m4.k], w=[m4.k])
                P.op("act", lambda e: e.activation(out=m4[:], in_=m4[:], func=AF.Exp, scale=-0.5), r=[m4.k], w=[m4.k])
                P.op("dve", lambda e: e.tensor_tensor(hv(o_[:], 4), hv(o_[:], 4), m4[:, :].unsqueeze(2).to_broadcast([128, 4, 128]), mm), r=[o_.k, m4.k], w=[o_.k])
                P.op("pool", lambda e: e.tensor_tensor(o_[:], o_[:], ngb[:], mm), r=[o_.k, ngb.k], w=[o_.k])
                P.op("dve", lambda e: e.tensor_tensor(o_[:], o_[:], sl_[:], mm), r=[o_.k, sl_.k], w=[o_.k])
                if "dbg_yb" in C.dbg:
                    P.dma("sp", C.dbg["dbg_yb"][g * 128:(g + 1) * 128, :], o_[:], r=[o_.k])
                pb = psb(C)
                for q in range(4):
                    P.op("pe", lambda e: e.transpose(pb[:, q * 128:(q + 1) * 128], o_[:, q * 128:(q + 1) * 128], C.ident[:]), r=[o_.k, C.ident_tok], w=[pb.k], inc=(q == 3))
                P.op("act", lambda e: e.copy(ys[:, :, t0:t0 + 128], hv(pb[:, :], 4)), r=[pb.k], w=[ys.k])
            P.dma("sp", XTv(C.YT)[:, 4:8, s * 512:(s + 1) * 512], ys[:], r=[ys.k], w=[C.YT_tok[1][s]])
        P.barrier()


def ln_inplace(C, xt, gb, bb, st, junk):
    P = C.P
    P.op("dve", lambda e: e.tensor_reduce(st[:, 0:1], xt[:], AX.X, ALU.add), r=[xt.k], w=[st.k])
    P.op("dve", lambda e: e.tensor_scalar(st[:, 0:1], st[:, 0:1], 1.0 / D, None, ALU.mult), r=[st.k], w=[st.k])
    P.op("dve", lambda e: e.tensor_scalar(xt[:], xt[:], st[:, 0:1], None, ALU.subtract), r=[xt.k, st.k], w=[xt.k])
    P.op("act", lambda e: e.activation(out=junk[:], in_=xt[:], func=AF.Square, accum_out=st[:, 1:2]), r=[xt.k], w=[junk.k, st.k])
    P.op("act", lambda e: e.activation(out=st[:, 1:2], in_=st[:, 1:2], func=AF.Sqrt, bias=LN_EPS, scale=1.0 / D), r=[st.k], w=[st.k])
    P.op("dve", lambda e: e.reciprocal(st[:, 1:2], st[:, 1:2]), r=[st.k], w=[st.k])
    P.op("dve", lambda e: e.scalar_tensor_tensor(xt[:], xt[:], st[:, 1:2], gb[:], ALU.mult, ALU.mult), r=[xt.k, st.k, gb.k], w=[xt.k])
    P.op("pool", lambda e: e.tensor_tensor(xt[:], xt[:], bb[:], ALU.add), r=[xt.k, bb.k], w=[xt.k])


def ln_lockstep(C, xs, gb, bb, sts, junk):
    P = C.P
    n = len(xs)
    for k in range(n):
        xt, st = xs[k], sts[k]
        P.op("dve", lambda e: e.tensor_reduce(st[:, 0:1], xt[:], AX.X, ALU.add), r=[xt.k], w=[st.k])
    for k in range(n):
        xt, st = xs[k], sts[k]
        P.op("dve", lambda e: e.tensor_scalar(st[:, 0:1], st[:, 0:1], 1.0 / D, None, ALU.mult), r=[st.k], w=[st.k])
    for k in range(n):
        xt, st = xs[k], sts[k]
        P.op("dve", lambda e: e.tensor_scalar(xt[:], xt[:], st[:, 0:1], None, ALU.subtract), r=[xt.k, st.k], w=[xt.k])
        P.op("act", lambda e: e.activation(out=junk[:], in_=xt[:], func=AF.Square, accum_out=st[:, 1:2]), r=[xt.k], w=[junk.k, st.k])
    for k in range(n):
        xt, st = xs[k], sts[k]
        P.op("act", lambda e: e.activation(out=st[:, 1:2], in_=st[:, 1:2], func=AF.Sqrt, bias=LN_EPS, scale=1.0 / D), r=[st.k], w=[st.k])
    for k in range(n):
        xt, st = xs[k], sts[k]
        P.op("dve", lambda e: e.reciprocal(st[:, 1:2], st[:, 1:2]), r=[st.k], w=[st.k])
    for k in range(n):
        xt, st = xs[k], sts[k]
        P.op("dve", lambda e: e.scalar_tensor_tensor(xt[:], xt[:], st[:, 1:2], gb[:], ALU.mult, ALU.mult), r=[xt.k, st.k, gb.k], w=[xt.k])
        P.op("pool", lambda e: e.tensor_tensor(xt[:], xt[:], bb[:], ALU.add), r=[xt.k, bb.k], w=[xt.k])


def tile_to_stage(C, xt, stage, j):
    P = C.P
    for half in range(2):
        pb = psb(C)
        for c4 in range(4):
            c = half * 4 + c4
            P.op("pe", lambda e: e.transpose(pb[:, c4 * 128:(c4 + 1) * 128], xt[:, c * 128:(c + 1) * 128], C.ident[:]),
                 r=[xt.k, C.ident_tok], w=[pb.k], inc=(c4 == 3))
        dst = stage[:, half * 4:(half + 1) * 4, j * 128:(j + 1) * 128]
        src = hv(pb[:, :], 4)
        if half == 0:
            P.op("act", lambda e: e.copy(dst, src), r=[pb.k], w=[stage.k])
        else:
            P.op("dve", lambda e: e.tensor_copy(dst, src), r=[pb.k], w=[stage.k])


def phase_outproj(C, w_out, srcYT, srcYT_toks, resid, resid_toks, lng, lnb, dstH, dstH_tok, dstHT, dstHT_tok, dbgname=None):
    nc, P, T, I = C.nc, C.P, C.T, C.I
    with ExitStack() as es:
        WO = mk(P, [128, 8, D], BF16, "WO", es)
        for c in range(8):
            P.dma("pool", WO[:, c, :], w_out[c * 128:(c + 1) * 128, :], w=[WO.k])
        gb = mk(P, [128, D], F32, "lng", es)
        bb = mk(P, [128, D], F32, "lnb", es)
        bcast_load(C, gb[:], lng, 128, gb.k)
        bcast_load(C, bb[:], lnb, 128, bb.k)
        yts = [mk(P, [128, 8, 512], BF16, "yt", es) for _ in range(2)]
        xts = [mk(P, [128, D], F32, "xres", es) for _ in range(8)]
        sts = [mk(P, [128, 2], F32, "lnst", es) for _ in range(8)]
        junk = mk(P, [128, D], BF16, "junk", es)
        stg = [mk(P, [128, 8, 512], BF16, "hstg", es) for _ in range(2)]
        pend = None

        def loads(s):
            P.dma("sp", yts[s % 2][:], XTv(srcYT)[:, :, s * 512:(s + 1) * 512], r=srcYT_toks(s), w=[yts[s % 2].k])
            for j in range(4):
                i = s * 4 + j
                xt = xts[(s % 2) * 4 + j]
                P.dma("sp", xt[:], resid[i * 128:(i + 1) * 128, :], r=resid_toks(i), w=[xt.k])

        loads(0)
        for s in range(C.NS):
            yt = yts[s % 2]
            sg_ = stg[s % 2]
            X = xts[(s % 2) * 4:(s % 2) * 4 + 4]
            S = sts[(s % 2) * 4:(s % 2) * 4 + 4]
            for j in range(4):
                xt = X[j]
                for half in range(2):
                    pb = psb(C)
                    for c in range(8):
                        P.op("pe", lambda e: e.matmul(pb[:, :], lhsT=yt[:, c, j * 128:(j + 1) * 128], rhs=WO[:, c, half * 512:(half + 1) * 512], start=(c == 0), stop=(c == 7)),
                             r=[yt.k, WO.k], w=[pb.k], inc=(c == 7))
                    P.op("dve", lambda e: e.scalar_tensor_tensor(xt[:, half * 512:(half + 1) * 512], xt[:, half * 512:(half + 1) * 512], DN_ALPHA, pb[:, :], ALU.mult, ALU.add),
                         r=[xt.k, pb.k], w=[xt.k])
            if pend is not None:
                pend()
            if s + 1 < C.NS:
                loads(s + 1)
            ln_lockstep(C, X, gb, bb, S, junk)
            for j in range(4):
                i = s * 4 + j
                P.dma("sp", dstH[i * 128:(i + 1) * 128, :], X[j][:], r=[X[j].k], w=[dstH_tok[i]])

            def pend(s=s, X=X, sg_=sg_):
                for j in range(4):
                    tile_to_stage(C, X[j], sg_, j)
                P.dma("sp", XTv(dstHT)[:, :, s * 512:(s + 1) * 512], sg_[:], r=[sg_.k], w=[dstHT_tok[s]])
        pend()
        P.barrier()


def phase_moe(C, l, srcH, srcH_tok, srcHT, srcHT_tok, dstX, dstX_tok, dstXT, dstXT_tok):
    nc, P, T, I = C.nc, C.P, C.T, C.I
    mm = ALU.mult
    with ExitStack() as es:
        WD = mk(P, [128, 32, D], BF16, "WD", es)
        for c4 in range(4):
            P.dma("sp", WD[:, c4 * 8:(c4 + 1) * 8, :], C.WD16[l].rearrange("p (c d) -> p c d", c=32)[:, c4 * 8:(c4 + 1) * 8, :], r=[C.WD16_tok[l]], w=[WD.k])
        RW = mk(P, [128, 8, NE], BF16, "RW", es)
        P.dma("pool", RW[:], I["router_w"].rearrange("(c p) e -> p c e", p=128), w=[RW.k])
        rbb = mk(P, [128, NE], F32, "rbb", es)
        bcast_load(C, rbb[:], I["router_bias"], 128, rbb.k)
        SEL = mk(P, [16, 16, 128], BF16, "SEL", es)
        P.dma("pool", SEL[:], I["c_sel"], w=[SEL.k])
        gb = mk(P, [128, D], F32, "lng", es)
        bb = mk(P, [128, D], F32, "lnb", es)
        bcast_load(C, gb[:], I["ln2_g"][l:l + 1, :], 128, gb.k)
        bcast_load(C, bb[:], I["ln2_b"][l:l + 1, :], 128, bb.k)
        hts = [mk(P, [128, 8, 512], BF16, "hT", es) for _ in range(2)]
        xts = [mk(P, [128, D], F32, "hres", es) for _ in range(4)]
        sts = [mk(P, [128, 2], F32, "lnst", es) for _ in range(4)]
        junk = mk(P, [128, D], BF16, "junk", es)
        stg = [mk(P, [128, 8, 512], BF16, "xstg", es) for _ in range(2)]
        actT = mk(P, [128, 32, 512], BF16, "actT", es)
        combT = mk(P, [16, 512], BF16, "combT", es)
        WGUs = [mk(P, [128, 2, 8, DE], BF16, "WGU", es) for _ in range(3)]
        sgl = [mk(P, [128, 512], F32, "sgl", es) for _ in range(2)]
        R4 = range(4)
        s_l = [mk(P, [128, NE], F32, "rs", es) for _ in R4]
        sel_l = [mk(P, [128, NE], F32, "rsel", es) for _ in R4]
        pr_l = [mk(P, [128, 4, 6], F32, "rpr", es) for _ in R4]
        gs_l = [mk(P, [128, 4], F32, "rgs", es) for _ in R4]
        t1_l = [mk(P, [128, 4], F32, "rt1", es) for _ in R4]
        m1_l = [mk(P, [128, 2], F32, "rm1", es) for _ in R4]
        selm_l = [mk(P, [128, NE], F32, "rselm", es) for _ in R4]
        sel2_l = [mk(P, [128, NE], F32, "rsel2", es) for _ in R4]
        comb_l = [mk(P, [128, NE], F32, "rcomb", es) for _ in R4]
        nwl = [0]

        def load_w(e):
            b = nwl[0] % 3
            nwl[0] += 1
            P.dma("sp", WGUs[b][:], C.WGU16[l][e].rearrange("p (t c f) -> p t c f", t=2, c=8), r=[C.WGU16_tok[l][e]], w=[WGUs[b].k])
            return WGUs[b]

        def g4(t):
            return t[:, :].rearrange("p (g e) -> p g e", g=4)

        def load_hT(s):
            P.dma("sp", hts[s % 2][:], XTv(srcHT)[:, :, s * 512:(s + 1) * 512], r=[srcHT_tok[s]], w=[hts[s % 2].k])

        def router_front(s):
            hT = hts[s % 2]
            for j in R4:
                plg = psb(C)
                s_ = s_l[j]
                for c in range(8):
                    P.op("pe", lambda e: e.matmul(plg[:, 0:NE], lhsT=hT[:, c, j * 128:(j + 1) * 128], rhs=RW[:, c, :], start=(c == 0), stop=(c == 7)),
                         r=[hT.k, RW.k], w=[plg.k], inc=(c == 7))
                P.op("act", lambda e: e.activation(out=s_[:], in_=plg[:, 0:NE], func=AF.Sigmoid), r=[plg.k], w=[s_.k])

            def step(fn):
                for j in R4:
                    fn(s_l[j], sel_l[j], pr_l[j], gs_l[j], t1_l[j], m1_l[j], selm_l[j], sel2_l[j], comb_l[j])
            step(lambda s_, sel, pr, gs, t1, m1, selm, sel2, comb: P.op("dve", lambda e: e.tensor_tensor(sel[:], s_[:], rbb[:], ALU.add), r=[s_.k, rbb.k], w=[sel.k]))
            step(lambda s_, sel, pr, gs, t1, m1, selm, sel2, comb: P.op("dve", lambda e: e.tensor_tensor(pr[:, :, 0:3], g4(sel)[:, :, 0:3], g4(sel)[:, :, 1:4], ALU.add), r=[sel.k], w=[pr.k]))
            step(lambda s_, sel, pr, gs, t1, m1, selm, sel2, comb: P.op("dve", lambda e: e.tensor_tensor(pr[:, :, 3:5], g4(sel)[:, :, 0:2], g4(sel)[:, :, 2:4], ALU.add), r=[sel.k], w=[pr.k]))
            step(lambda s_, sel, pr, gs, t1, m1, selm, sel2, comb: P.op("dve", lambda e: e.tensor_tensor(pr[:, :, 5:6], g4(sel)[:, :, 0:1], g4(sel)[:, :, 3:4], ALU.add), r=[sel.k], w=[pr.k]))
            step(lambda s_, sel, pr, gs, t1, m1, selm, sel2, comb: P.op("dve", lambda e: e.tensor_reduce(gs[:], pr[:], AX.X, ALU.max), r=[pr.k], w=[gs.k]))
            step(lambda s_, sel, pr, gs, t1, m1, selm, sel2, comb: P.op("dve", lambda e: e.tensor_reduce(m1[:, 0:1], gs[:], AX.X, ALU.max), r=[gs.k], w=[m1.k]))
            step(lambda s_, sel, pr, gs, t1, m1, selm, sel2, comb: P.op("dve", lambda e: e.tensor_scalar(gs[:], gs[:], m1[:, 0:1], None, ALU.is_ge), r=[gs.k, m1.k], w=[gs.k]))
            step(lambda s_, sel, pr, gs, t1, m1, selm, sel2, comb: P.op("dve", lambda e: e.tensor_scalar(t1[:], gs[:], -1.0, 1e30, ALU.add, ALU.mult), r=[gs.k], w=[t1.k]))
            step(lambda s_, sel, pr, gs, t1, m1, selm, sel2, comb: P.op("dve", lambda e: e.tensor_tensor(g4(selm), g4(sel), gs[:, :].unsqueeze(2).to_broadcast([128, 4, 4]), mm), r=[sel.k, gs.k], w=[selm.k]))
            step(lambda s_, sel, pr, gs, t1, m1, selm, sel2, comb: P.op("dve", lambda e: e.tensor_tensor(g4(selm), g4(selm), t1[:, :].unsqueeze(2).to_broadcast([128, 4, 4]), ALU.add), r=[selm.k, t1.k], w=[selm.k]))
            step(lambda s_, sel, pr, gs, t1, m1, selm, sel2, comb: P.op("dve", lambda e: e.tensor_reduce(m1[:, 0:1], selm[:], AX.X, ALU.max), r=[selm.k], w=[m1.k]))
            step(lambda s_, sel, pr, gs, t1, m1, selm, sel2, comb: P.op("dve", lambda e: e.tensor_scalar(sel2[:], selm[:], m1[:, 0:1], None, ALU.is_ge), r=[selm.k, m1.k], w=[sel2.k]))
            step(lambda s_, sel, pr, gs, t1, m1, selm, sel2, comb: P.op("dve", lambda e: e.scalar_tensor_tensor(sel2[:], sel2[:], -1e30, selm[:], mm, ALU.add), r=[sel2.k, selm.k], w=[sel2.k]))
            step(lambda s_, sel, pr, gs, t1, m1, selm, sel2, comb: P.op("dve", lambda e: e.tensor_reduce(m1[:, 1:2], sel2[:], AX.X, ALU.max), r=[sel2.k], w=[m1.k]))
            step(lambda s_, sel, pr, gs, t1, m1, selm, sel2, comb: P.op("dve", lambda e: e.tensor_scalar(sel2[:], selm[:], m1[:, 1:2], None, ALU.is_ge), r=[selm.k, m1.k], w=[sel2.k]))
            step(lambda s_, sel, pr, gs, t1, m1, selm, sel2, comb: P.op("dve", lambda e: e.tensor_tensor(comb[:], s_[:], sel2[:], mm), r=[s_.k, sel2.k], w=[comb.k]))
            step(lambda s_, sel, pr, gs, t1, m1, selm, sel2, comb: P.op("dve", lambda e: e.tensor_reduce(m1[:, 0:1], comb[:], AX.X, ALU.add), r=[comb.k], w=[m1.k]))
            step(lambda s_, sel, pr, gs, t1, m1, selm, sel2, comb: P.op("dve", lambda e: e.reciprocal(m1[:, 0:1], m1[:, 0:1]), r=[m1.k], w=[m1.k]))
            step(lambda s_, sel, pr, gs, t1, m1, selm, sel2, comb: P.op("dve", lambda e: e.tensor_scalar(comb[:], comb[:], m1[:, 0:1], None, mm), r=[comb.k, m1.k], w=[comb.k]))

        def router_back(s):
            for j in R4:
                comb = comb_l[j]
                pct = psb(C)
                P.op("pe", lambda e: e.transpose(pct[0:16, 0:128], comb[:, :], C.ident[:]), r=[comb.k, C.ident_tok], w=[pct.k])
                P.op("act", lambda e: e.copy(combT[:, j * 128:(j + 1) * 128], pct[0:16, 0:128]), r=[pct.k], w=[combT.k])

        def load_x(s):
            for j in R4:
                i = s * 4 + j
                P.dma("sp", xts[j][:], srcH[i * 128:(i + 1) * 128, :], r=[srcH_tok[i]], w=[xts[j].k])

        def make_pend(s):
            xs_ = stg[s % 2]

            def pend():
                for j in R4:
                    tile_to_stage(C, xts[j], xs_, j)
                P.dma("sp", XTv(dstXT)[:, :, s * 512:(s + 1) * 512], xs_[:], r=[xs_.k], w=[dstXT_tok[s]])
            return pend

        pend = None
        pre = []
        load_hT(0)
        router_front(0)
        router_back(0)
        for s in range(C.NS):
            hT = hts[s % 2]
            if s + 1 < C.NS:
                load_hT(s + 1)
            for ex in range(NE):
                WGU = pre.pop(0) if pre else load_w(ex)
                pcb = psb(C)
                P.op("pe", lambda e: e.matmul(pcb[:, :], lhsT=SEL[:, ex, :], rhs=combT[:, :], start=True, stop=True), r=[SEL.k, combT.k], w=[pcb.k])
                for f in range(2):
                    pG, pU = psb(C), psb(C)
                    for pb, ti in ((pG, 0), (pU, 1)):
                        for c in range(8):
                            P.op("pe", lambda e: e.matmul(pb[:, :], lhsT=WGU[:, ti, c, f * 128:(f + 1) * 128], rhs=hT[:, c, :], start=(c == 0), stop=(c == 7)),
                                 r=[WGU.k, hT.k], w=[pb.k], inc=(c == 7))
                    sg_ = sgl[(ex * 2 + f) % 2]
                    P.op("act", lambda e: e.activation(out=sg_[:], in_=pG[:, :], func=AF.Silu), r=[pG.k], w=[sg_.k])
                    P.op("dve", lambda e: e.tensor_tensor(sg_[:], sg_[:], pU[:, :], mm), r=[sg_.k, pU.k], w=[sg_.k])
                    P.op("dve", lambda e: e.tensor_tensor(actT[:, ex * 2 + f, :], sg_[:], pcb[:, :], mm), r=[sg_.k, pcb.k], w=[actT.k])
                if ex == 3:
                    if pend is not None:
                        pend()
                        pend = None
                    load_x(s)
            if s + 1 < C.NS:
                pre.extend(load_w(e_) for e_ in range(3))
            if s + 1 < C.NS:
                router_front(s + 1)
            for j in R4:
                xt = xts[j]
                for half in range(2):
                    pb = psb(C)
                    for c in range(32):
                        P.op("pe", lambda e: e.matmul(pb[:, :], lhsT=actT[:, c, j * 128:(j + 1) * 128], rhs=WD[:, c, half * 512:(half + 1) * 512], start=(c == 0), stop=(c == 31)),
                             r=[actT.k, WD.k], w=[pb.k], inc=(c == 31))
                    P.op("dve", lambda e: e.scalar_tensor_tensor(xt[:, half * 512:(half + 1) * 512], xt[:, half * 512:(half + 1) * 512], DN_ALPHA, pb[:, :], ALU.mult, ALU.add),
                         r=[xt.k, pb.k], w=[xt.k])
            if s + 1 < C.NS:
                router_back(s + 1)
            ln_lockstep(C, xts, gb, bb, sts, junk)
            for j in R4:
                i = s * 4 + j
                P.dma("sp", dstX[i * 128:(i + 1) * 128, :], xts[j][:], r=[xts[j].k], w=[dstX_tok[i]])
            if dstXT is not None:
                pend = make_pend(s)
        if pend is not None:
            pend()
        P.barrier()


def rope_consts(T):
    pos = np.arange(T, dtype=np.float64)
    c = {}
    for name, half in (("k", 64), ("i", 32)):
        inv = 10000.0 ** (-np.arange(half, dtype=np.float64) / half)
        ang = (pos.astype(np.float32)[:, None] * inv.astype(np.float32)[None, :]).astype(np.float32).astype(np.float64)
        cs = np.cos(ang).astype(np.float32).reshape(T // 128, 128, half).transpose(1, 0, 2)
        sn = np.sin(ang).astype(np.float32).reshape(T // 128, 128, half).transpose(1, 0, 2)
        c["cos_" + name] = np.ascontiguousarray(cs)
        c["sin_" + name] = np.ascontiguousarray(sn)
    q = np.arange(128)[:, None]
    s = np.arange(128)[None, :]
    c["cbias"] = np.where(s <= q, 0.0, -1e30).astype(np.float32)
    c["halfpow"] = (0.5 ** np.arange(1, 33, dtype=np.float64)).astype(np.float32).reshape(1, 32)
    c["tiebias"] = (-1e-6 * np.arange(T, dtype=np.float64)).astype(np.float32).reshape(1, T)
    return c


def rope_tm(C, dst_ap, dst_k, src, src_k, cosb, sinb, nh, half, ta_ap, ta_k, tb_ap, tb_k, rdeps):
    P = C.P
    mm = ALU.mult
    n = nh * 2
    s3 = src.rearrange("p (n f) -> p n f", n=n)
    cb = cosb.unsqueeze(1).to_broadcast([128, n, half])
    sb_ = sinb.unsqueeze(1).to_broadcast([128, n, half])
    a3 = ta_ap.rearrange("p (n f) -> p n f", n=n)
    b3 = tb_ap.rearrange("p (n f) -> p n f", n=n)
    P.op("dve", lambda e: e.tensor_tensor(a3, s3, cb, mm), r=[src_k] + rdeps, w=[ta_k])
    P.op("dve", lambda e: e.tensor_tensor(b3, s3, sb_, mm), r=[src_k] + rdeps, w=[tb_k])
    a4 = ta_ap.rearrange("p (h t f) -> p h t f", h=nh, t=2)
    b4 = tb_ap.rearrange("p (h t f) -> p h t f", h=nh, t=2)
    d4 = dst_ap.rearrange("p (h t f) -> p h t f", h=nh, t=2)
    P.op("pool", lambda e: e.tensor_tensor(d4[:, :, 0, :], a4[:, :, 0, :], b4[:, :, 1, :], ALU.subtract), r=[ta_k, tb_k], w=[dst_k])
    P.op("pool", lambda e: e.tensor_tensor(d4[:, :, 1, :], a4[:, :, 1, :], b4[:, :, 0, :], ALU.add), r=[ta_k, tb_k], w=[dst_k])


def phase_dsa(C):
    nc, P, T, I = C.nc, C.P, C.T, C.I
    mm = ALU.mult
    KT = min(256, T // 4)
    NIT = 25
    SCALE = 128 ** -0.5
    C.bank_pool = [0, 1, 2, 3, 4]
    with ExitStack() as es:
        def wt(name, shape, dt=F32):
            return mk(P, list(shape), dt, name, es)
        WQ = wt("WQ", (128, 8, 1024), BF16)
        WR = wt("WR", (128, 8, 580), BF16)
        for c in range(8):
            P.dma("pool", WQ[:, c, :], I["w_in_odd"][c * 128:(c + 1) * 128, 0:1024], w=[WQ.k])
            P.dma("pool", WR[:, c, :], I["w_in_odd"][c * 128:(c + 1) * 128, 1024:1604], w=[WR.k])
        RTs = [[wt("CK", (128, 4, 64)), wt("SK", (128, 4, 64)), wt("CI", (128, 4, 32)), wt("SI", (128, 4, 32))] for _ in range(2)]

        def load_tables(s):
            tl = RTs[s % 2]
            for t_, nm in zip(tl, ("c_cos_k", "c_sin_k", "c_cos_i", "c_sin_i")):
                P.dma("sp", t_[:], I[nm][:, s * 4:(s + 1) * 4, :], w=[t_.k])
            return tl
        BIAS = wt("BIAS", (128, T))
        bcast_load(C, BIAS[:], I["c_tiebias"], 128, BIAS.k)
        CB = wt("CB", (128, 128))
        P.dma("sp", CB[:], I["c_cbias"], w=[CB.k])
        ikg, ikb = wt("ikg", (128, 64)), wt("ikb", (128, 64))
        bcast_load(C, ikg[:], I["c_ik_ln_g"], 128, ikg.k)
        bcast_load(C, ikb[:], I["c_ik_ln_b"], 128, ikb.k)
        kT = wt("kT", (128, T), BF16)
        ikT = wt("ikT", (128, T), BF16)
        Vx = wt("Vx", (128, C.NT, 129), BF16)
        P.op("dve", lambda e: e.memset(Vx[:, :, 128:129], 1.0), w=[Vx.k])
        xTs = [wt("xTd", (128, 8, 512), BF16) for _ in range(2)]
        ta, tb = wt("ropeA", (128, 512)), wt("ropeB", (128, 512))
        kr = wt("kr", (128, 128))
        ikr = wt("ikr", (128, 128))
        st2 = wt("st2", (128, 2))
        for s in range(C.NS):
            xT = xTs[s % 2]
            P.dma("sp", xT[:], XTv(C.XT1)[:, :, s * 512:(s + 1) * 512], r=[C.XT1_tok[s]], w=[xT.k])
            CK, SK, CI, SI = load_tables(s)
            for j in range(4):
                i = s * 4 + j
                pb = psb(C)
                for c in range(8):
                    P.op("pe", lambda e: e.matmul(pb[:, 0:256], lhsT=xT[:, c, j * 128:(j + 1) * 128], rhs=WR[:, c, 0:256], start=(c == 0), stop=(c == 7)),
                         r=[xT.k, WR.k], w=[pb.k], inc=False)
                for c in range(8):
                    P.op("pe", lambda e: e.matmul(pb[:, 256:320], lhsT=xT[:, c, j * 128:(j + 1) * 128], rhs=WR[:, c, 512:576], start=(c == 0), stop=(c == 7)),
                         r=[xT.k, WR.k], w=[pb.k], inc=(c == 7))
                rope_tm(C, kr[:, :], kr.k, pb[:, 0:128], pb.k, CK[:, j, :], SK[:, j, :], 1, 64, ta[:, 0:128], ta.k, tb[:, 0:128], tb.k, [CK.k, SK.k])
                P.op("act", lambda e: e.copy(Vx[:, i, 0:128], pb[:, 128:256]), r=[pb.k], w=[Vx.k])
                P.op("dve", lambda e: e.tensor_reduce(st2[:, 0:1], pb[:, 256:320], AX.X, ALU.add), r=[pb.k], w=[st2.k])
                P.op("dve", lambda e: e.tensor_scalar(st2[:, 0:1], st2[:, 0:1], 1.0 / 64, None, mm), r=[st2.k], w=[st2.k])
                P.op("dve", lambda e: e.tensor_scalar(ikr[:, 0:64], pb[:, 256:320], st2[:, 0:1], None, ALU.subtract), r=[pb.k, st2.k], w=[ikr.k])
                P.op("act", lambda e: e.activation(out=ikr[:, 64:128], in_=ikr[:, 0:64], func=AF.Square, accum_out=st2[:, 1:2]), r=[ikr.k], w=[ikr.k, st2.k])
                P.op("act", lambda e: e.activation(out=st2[:, 1:2], in_=st2[:, 1:2], func=AF.Sqrt, bias=LN_EPS, scale=1.0 / 64), r=[st2.k], w=[st2.k])
                P.op("dve", lambda e: e.reciprocal(st2[:, 1:2], st2[:, 1:2]), r=[st2.k], w=[st2.k])
                P.op("dve", lambda e: e.scalar_tensor_tensor(ikr[:, 0:64], ikr[:, 0:64], st2[:, 1:2], ikg[:], mm, mm), r=[ikr.k, st2.k, ikg.k], w=[ikr.k])
                P.op("dve", lambda e: e.tensor_tensor(ikr[:, 64:128], ikr[:, 0:64], ikb[:], ALU.add), r=[ikr.k, ikb.k], w=[ikr.k])
                ikn = TL(ikr.t, ikr.k)
                rope_src = ikr[:, 64:128]
                n = 2
                s3 = rope_src.rearrange("p (n f) -> p n f", n=n)
                cb = CI[:, j, :].unsqueeze(1).to_broadcast([128, n, 32])
                sb_ = SI[:, j, :].unsqueeze(1).to_broadcast([128, n, 32])
                a3 = ta[:, 0:64].rearrange("p (n f) -> p n f", n=n)
                b3 = tb[:, 0:64].rearrange("p (n f) -> p n f", n=n)
                P.op("dve", lambda e: e.tensor_tensor(a3, s3, cb, mm), r=[ikr.k, CI.k], w=[ta.k])
                P.op("dve", lambda e: e.tensor_tensor(b3, s3, sb_, mm), r=[ikr.k, SI.k], w=[tb.k])
                P.op("pool", lambda e: e.tensor_tensor(ikr[:, 0:32], ta[:, 0:32], tb[:, 32:64], ALU.subtract), r=[ta.k, tb.k], w=[ikr.k])
                P.op("pool", lambda e: e.tensor_tensor(ikr[:, 32:64], ta[:, 32:64], tb[:, 0:32], ALU.add), r=[ta.k, tb.k], w=[ikr.k])
                P.op("pool", lambda e: e.tensor_copy(ikr[:, 64:128], ikr[:, 0:64]), r=[ikr.k], w=[ikr.k])
                pt = psb(C)
                P.op("pe", lambda e: e.transpose(pt[:, 0:128], kr[:, :], C.ident[:]), r=[kr.k, C.ident_tok], w=[pt.k], inc=False)
                P.op("pe", lambda e: e.transpose(pt[:, 128:256], ikr[:, :], C.ident[:]), r=[ikr.k, C.ident_tok], w=[pt.k])
                P.op("act", lambda e: e.copy(kT[:, i * 128:(i + 1) * 128], pt[:, 0:128]), r=[pt.k], w=[kT.k])
                P.op("act", lambda e: e.mul(ikT[:, i * 128:(i + 1) * 128], pt[:, 128:256], 0.125), r=[pt.k], w=[ikT.k])
        SC = wt("SC", (128, T))
        MASKs = [wt("MASK", (128, T)) for _ in range(2)]
        junk = wt("junkd", (128, T), BF16)
        qr = wt("qr", (128, 1024))
        iqr = wt("iqr", (128, 256))
        qTs = [wt("qT", (128, 8, 128), BF16) for _ in range(3)]
        iqT = wt("iqT", (128, 2, 128), BF16)
        iws = wt("iws", (128, 4))
        rl = [wt("rl%d" % n, (128, 512)) for n in range(2)]
        bs = wt("bs", (128, 8))
        Dk = wt("Dk", (128, NIT))
        HK = wt("HK", (128, NIT))
        bcast_load(C, HK[:], I["c_halfpow"][0:1, 0:NIT], 128, HK.k)
        mT4 = [wt("mT4_%d" % n, (128, 4, 128), BF16) for n in range(2)]
        pTs = [wt("pT%d" % n, (128, 4, 128), BF16) for n in range(4)]
        o_ = wt("od", (128, 1024))
        rs8 = wt("rs8", (128, 8))
        ostg = [wt("ostg", (128, 8, 512), BF16) for _ in range(2)]
        accb = [TL(*C.banks[b]) for b in (5, 6, 7)]
        MBIG = 30000.0
        identb = wt("identb", (128, 128), BF16)
        P.op("dve", lambda e: e.tensor_copy(identb[:], C.ident[:]), r=[C.ident_tok], w=[identb.k])
        acc_of = [(0, 0), (0, 1), (0, 2), (1, 0), (1, 1), (1, 2), (2, 0), (2, 1)]
        def qproj(s, j):
            i = s * 4 + j
            xT = xTs[s % 2]
            qT = qTs[i % 3]
            if j == 0:
                P.dma("sp", xT[:], XTv(C.XT1)[:, :, s * 512:(s + 1) * 512], r=[C.XT1_tok[s]], w=[xT.k])
                load_tables(s)
            CK, SK, CI, SI = RTs[s % 2]
            pq = [psb(C), psb(C)]
            for half in range(2):
                for c in range(8):
                    P.op("pe", lambda e: e.matmul(pq[half][:, :], lhsT=xT[:, c, j * 128:(j + 1) * 128], rhs=WQ[:, c, half * 512:(half + 1) * 512], start=(c == 0), stop=(c == 7)),
                         r=[xT.k, WQ.k], w=[pq[half].k], inc=(c == 7))
            piq = psb(C)
            for c in range(8):
                P.op("pe", lambda e: e.matmul(piq[:, 0:256], lhsT=xT[:, c, j * 128:(j + 1) * 128], rhs=WR[:, c, 256:512], start=(c == 0), stop=(c == 7)),
                     r=[xT.k, WR.k], w=[piq.k], inc=False)
            for c in range(8):
                P.op("pe", lambda e: e.matmul(piq[:, 256:260], lhsT=xT[:, c, j * 128:(j + 1) * 128], rhs=WR[:, c, 576:580], start=(c == 0), stop=(c == 7)),
                     r=[xT.k, WR.k], w=[piq.k], inc=(c == 7))
            for half in range(2):
                hs = slice(half * 512, (half + 1) * 512)
                rope_tm(C, qr[:, hs], qr.k, pq[half][:, :], pq[half].k, CK[:, j, :], SK[:, j, :], 4, 64,
                        ta[:, 0:512], ta.k, tb[:, 0:512], tb.k, [CK.k, SK.k])
            rope_tm(C, iqr[:, :], iqr.k, piq[:, 0:256], piq.k, CI[:, j, :], SI[:, j, :], 4, 32, ta[:, 0:256], ta.k, tb[:, 0:256], tb.k, [CI.k, SI.k])
            P.op("act", lambda e: e.mul(iws[:], piq[:, 256:260], 0.5), r=[piq.k], w=[iws.k])
            for half in range(2):
                pb = psb(C)
                for c4 in range(4):
                    h = half * 4 + c4
                    P.op("pe", lambda e: e.transpose(pb[:, c4 * 128:(c4 + 1) * 128], qr[:, h * 128:(h + 1) * 128], C.ident[:]), r=[qr.k, C.ident_tok], w=[pb.k], inc=(c4 == 3))
                P.op("act", lambda e: e.copy(qT[:, half * 4:(half + 1) * 4, :], hv(pb[:, :], 4)), r=[pb.k], w=[qT.k])
            pb = psb(C)
            for c2 in range(2):
                P.op("pe", lambda e: e.transpose(pb[:, c2 * 128:(c2 + 1) * 128], iqr[:, c2 * 128:(c2 + 1) * 128], C.ident[:]), r=[iqr.k, C.ident_tok], w=[pb.k], inc=(c2 == 1))
            P.op("act", lambda e: e.copy(iqT[:], hv(pb[:, 0:256], 2)), r=[pb.k], w=[iqT.k])

        NB = C.NS * 4

        def qproj_next(i):
            if i + 1 < NB:
                qproj((i + 1) // 4, (i + 1) % 4)

        def qblock(s, j):
            if True:
                i = s * 4 + j
                L = (i + 1) * 128
                og = ostg[s % 2]
                qT = qTs[i % 3]
                MASK = MASKs[i % 2]
                cast_some(C, 1, 2)
                if i == 0:
                    qproj(0, 0)
                yield "F"
                for k0 in range(0, L, 512):
                    kw = min(512, L - k0)
                    for h in range(4):
                        ph = psb(C)
                        pl = (h % 2) * 64
                        P.op("pe", lambda e: e.matmul(ph[:, 0:kw], lhsT=iqT[pl:pl + 64, h // 2, :], rhs=ikT[pl:pl + 64, k0:k0 + kw], start=True, stop=True),
                             r=[iqT.k, ikT.k], w=[ph.k])
                        r_ = rl[h % 2]
                        P.op("act", lambda e: e.activation(out=r_[:, 0:kw], in_=ph[:, 0:kw], func=AF.Relu), r=[ph.k], w=[r_.k])
                        if h == 0:
                            P.op("dve", lambda e: e.scalar_tensor_tensor(SC[:, k0:k0 + kw], r_[:, 0:kw], iws[:, 0:1], BIAS[:, k0:k0 + kw], mm, ALU.add), r=[r_.k, iws.k, BIAS.k], w=[SC.k])
                        else:
                            P.op("dve", lambda e: e.scalar_tensor_tensor(SC[:, k0:k0 + kw], r_[:, 0:kw], iws[:, h:h + 1], SC[:, k0:k0 + kw], mm, ALU.add),
                                 r=[r_.k, iws.k, SC.k], w=[SC.k])
                    yield "F"
                if L > KT:
                    P.op("dve", lambda e: e.tensor_reduce(bs[:, 1:2], SC[:, 0:L], AX.X, ALU.max, apply_absolute_value=True), r=[SC.k], w=[bs.k])
                    P.op("dve", lambda e: e.tensor_scalar(bs[:, 0:1], bs[:, 1:2], -1.0, -1.0, mm, ALU.add), r=[bs.k], w=[bs.k])
                    P.op("dve", lambda e: e.tensor_scalar(bs[:, 1:2], bs[:, 1:2], 2.0, 2.0, mm, ALU.add), r=[bs.k], w=[bs.k])
                    P.op("dve", lambda e: e.tensor_scalar(Dk[:], HK[:], bs[:, 1:2], None, mm), r=[bs.k, HK.k], w=[Dk.k])
                P.op("pool", lambda e: e.tensor_tensor(SC[:, i * 128:L], SC[:, i * 128:L], CB[:], ALU.add), r=[SC.k, CB.k], w=[SC.k])
                if L > KT:
                    for it in range(NIT):
                        P.op("dve", lambda e: e.tensor_tensor(bs[:, 2:3], bs[:, 0:1], Dk[:, it:it + 1], ALU.add), r=[bs.k, Dk.k], w=[bs.k])
                        P.op("dve", lambda e: e.tensor_scalar(junk[:, 0:L], SC[:, 0:L], bs[:, 2:3], None, ALU.is_gt, ALU.add, accum_out=bs[:, 3:4]),
                             r=[SC.k, bs.k], w=[junk.k, bs.k])
                        P.op("dve", lambda e: e.scalar_tensor_tensor(bs[:, 4:5], bs[:, 3:4], float(KT) - 0.5, Dk[:, it:it + 1], ALU.is_gt, mm), r=[bs.k, Dk.k], w=[bs.k])
                        P.op("dve", lambda e: e.tensor_tensor(bs[:, 0:1], bs[:, 0:1], bs[:, 4:5], ALU.add), r=[bs.k], w=[bs.k])
                        if it == 4:
                            qproj_next(i)
                        yield "F"
                    P.op("dve", lambda e: e.tensor_scalar(MASK[:, 0:L], SC[:, 0:L], bs[:, 0:1], None, ALU.is_le), r=[SC.k, bs.k], w=[MASK.k])
                else:
                    P.op("dve", lambda e: e.tensor_scalar(MASK[:, 0:L], SC[:, 0:L], -1e29, None, ALU.is_le), r=[SC.k], w=[MASK.k])
                    qproj_next(i)
                if "dbg_mask" in C.dbg:
                    P.dma("sp", C.dbg["dbg_mask"][i * 128:(i + 1) * 128, 0:L], MASK[:, 0:L], r=[MASK.k])
                    P.dma("sp", C.dbg["dbg_sc"][i * 128:(i + 1) * 128, 0:L], SC[:, 0:L], r=[SC.k])
                yield "END_FRONT"
                units = [(st, hg) for st in range(i + 1) for hg in range(2)]

                def stage1(st, hg):
                    if hg == 0 and st % 4 == 0:
                        n4 = min(4, i + 1 - st)
                        m4 = mT4[(st // 4) % 2]
                        pb = psb(C)
                        for u in range(n4):
                            P.op("pe", lambda e: e.transpose(pb[:, u * 128:(u + 1) * 128], MASK[:, (st + u) * 128:(st + u + 1) * 128], C.ident[:]),
                                 r=[MASK.k, C.ident_tok], w=[pb.k], inc=(u == n4 - 1))
                        P.op("act", lambda e: e.mul(m4[:, 0:n4, :], hv(pb[:, :], 4)[:, 0:n4, :], -MBIG), r=[pb.k], w=[m4.k])
                    m4 = mT4[(st // 4) % 2]
                    pl_ = psb(C)
                    P.op("pe", lambda e: e.matmul(pl_[:, :], lhsT=identb[:, :], rhs=m4[:, st % 4, :].unsqueeze(1).to_broadcast([128, 4, 128]), start=True, stop=False),
                         r=[identb.k, m4.k], w=[pl_.k], inc=False)
                    P.op("pe", lambda e: e.matmul(pl_[:, :], lhsT=kT[:, st * 128:(st + 1) * 128], rhs=qT[:, hg * 4:(hg + 1) * 4, :].rearrange("p h q -> p (h q)"), start=False, stop=True),
                         r=[kT.k, qT.k], w=[pl_.k])
                    pT = pTs[hg * 2 + st % 2]
                    P.op("act", lambda e: e.activation(out=pT[:], in_=hv(pl_[:, :], 4), func=AF.Exp, scale=SCALE), r=[pl_.k], w=[pT.k])

                def stage2(st, hg):
                    pT = pTs[hg * 2 + st % 2]
                    for hh in range(4):
                        h = hg * 4 + hh
                        ab, slot = acc_of[h]
                        P.op("pe", lambda e: e.matmul(accb[ab][:, slot * 129:(slot + 1) * 129], lhsT=pT[:, hh, :], rhs=Vx[:, st, :], start=(st == 0 and slot == 0), stop=(st == i), skip_group_check=True),
                             r=[pT.k, Vx.k], w=[accb[ab].k], inc=(hh == 3))

                stage1(*units[0])
                for n in range(len(units)):
                    if n + 1 < len(units):
                        stage1(*units[n + 1])
                    stage2(*units[n])
                    if units[n][1] == 1:
                        yield "B"
                for h in range(8):
                    ab, slot = acc_of[h]
                    P.op("dve", lambda e: e.reciprocal(rs8[:, h:h + 1], accb[ab][:, slot * 129 + 128:slot * 129 + 129]), r=[accb[ab].k], w=[rs8.k])
                    P.op("act", lambda e: e.activation(out=o_[:, h * 128:(h + 1) * 128], in_=accb[ab][:, slot * 129:slot * 129 + 128], func=AF.Copy, scale=rs8[:, h:h + 1]),
                         r=[accb[ab].k, rs8.k], w=[o_.k])
                if "dbg_dsa" in C.dbg:
                    P.dma("sp", C.dbg["dbg_dsa"][i * 128:(i + 1) * 128, :], o_[:], r=[o_.k])
                tile_to_stage(C, o_, og, j)
                if j == 3:
                    P.dma("sp", XTv(C.YT)[:, :, s * 512:(s + 1) * 512], og[:], r=[og.k], w=[C.YT_tok[0][s], C.YT_tok[1][s]])

        pipeline2([(lambda s=s, j=j: qblock(s, j)) for s in range(C.NS) for j in range(4)], interleave=C.dsa_interleave)
        P.barrier()
    C.bank_pool = list(range(8))


class _View:
    def __init__(self, tl, sl):
        self.t = _Sl(tl.t, sl)
        self.k = tl.k

    def __getitem__(self, idx):
        return self.t[idx]


class _Sl:
    def __init__(self, t, sl):
        self.base = t
        self.sl = sl

    def __getitem__(self, idx):
        rows, cols = idx
        assert cols == slice(None)
        return self.base[rows, self.sl]


_NC_CACHE = {}


def _in_map(inputs, b, T, consts):
    m = {"x": np.ascontiguousarray(inputs["x"][b, :T], dtype=np.float32)}
    for k, a in inputs.items():
        if k == "x":
            continue
        a = np.asarray(a, dtype=np.float32)
        if k in ("router_w", "exp_w_gate", "exp_w_up", "exp_w_down", "ln1_g", "ln1_b", "ln2_g", "ln2_b"):
            m[k] = np.ascontiguousarray(a)
        elif k == "router_bias":
            m[k] = np.ascontiguousarray(a.reshape(1, -1))
        elif k == "a_r_k":
            m[k] = np.ascontiguousarray(a.reshape(1, 512))
        elif a.ndim == 3:
            m[k] = np.ascontiguousarray(a[0])
        elif a.ndim == 2:
            m[k] = np.ascontiguousarray(a[0:1])
    m.update(consts)
    return m


def kernel(**inputs):
    x = np.asarray(inputs["x"])
    B, T, _ = x.shape
    if T not in _NC_CACHE:
        _NC_CACHE[T] = build(T)
    nc = _NC_CACHE[T]
    consts = {"c_" + k: v for k, v in host_consts(T).items()}
    consts.update({"c_" + k: v for k, v in rope_consts(T).items()})
    in_maps = [_in_map(inputs, b, T, consts) for b in range(B)]
    res = run_bass_kernel_spmd(nc, in_maps, core_ids=list(range(B)))
    out = np.stack([np.asarray(res.results[b]["out"], dtype=np.float32) for b in range(B)], 0)
    return out
```

```python
import numpy as np
import ml_dtypes
from contextlib import ExitStack
import concourse.bass as bass
import concourse.mybir as mybir
from concourse.bass_utils import run_bass_kernel_spmd

F32 = mybir.dt.float32
BF16 = mybir.dt.bfloat16
AF = mybir.ActivationFunctionType
ALU = mybir.AluOpType
AX = mybir.AxisListType

D = 1024
A_COLS = 1792
B_COLS = 1552
EVEN_COLS = 3344
ODD_COLS = 1604
NE = 16
DE = 256
DN_ALPHA = 4 ** 0.25
LN_EPS = 1e-5
DEC = 0.6065306597126334
DSA_INTERLEAVE = True
RWKV_INTERLEAVE = False


class Tok:
    __slots__ = ("w", "r")

    def __init__(self):
        self.w = None
        self.r = {}


class Eng:
    def __init__(self, name, h, sem):
        self.name = name
        self.h = h
        self.sem = sem
        self.cnt = 0
        self.waited = {}


class Prog:
    NSLOT = 8

    def __init__(self, nc, es):
        self.nc = nc
        self.es = es
        self.E = {}
        for name, h in (("pe", nc.tensor), ("act", nc.scalar), ("dve", nc.vector),
                        ("pool", nc.gpsimd), ("sp", nc.sync)):
            sem = es.enter_context(nc.semaphore("sem_" + name))
            self.E[name] = Eng(name, h, sem)
        self.slots = {}
        self.dn = {}
        for q in ("sp", "pool", "act"):
            self.slots[q] = [[es.enter_context(nc.semaphore("dq_%s%d" % (q, i))), 0] for i in range(self.NSLOT)]
            self.dn[q] = 0
        self.nalloc = 0

    def sb(self, shape, dt=F32, name=None, es=None):
        self.nalloc += 1
        t = (es or self.es).enter_context(self.nc.sbuf_tensor("%s_%d" % (name or "t", self.nalloc), list(shape), dt))
        return t

    def _wait(self, eng, ev):
        sem, val = ev
        key = sem.num
        if eng.waited.get(key, 0) >= val:
            return
        eng.h.wait_ge(sem, val)
        eng.waited[key] = val

    def _deps(self, en, r, w):
        eng = self.E[en]
        for t in r:
            if t.w is not None:
                yield t.w
        for t in w:
            if t.w is not None:
                yield t.w
            for ev in t.r.values():
                yield ev

    def op(self, en, fn, r=(), w=(), inc=True):
        eng = self.E[en]
        for ev in list(self._deps(en, r, w)):
            if en == "pe" and ev[0] is eng.sem:
                continue
            self._wait(eng, ev)
        ins = fn(eng.h)
        myev = (eng.sem, eng.cnt + 1)
        if inc:
            ins.then_inc(eng.sem, 1)
            eng.cnt += 1
        for t in r:
            t.r[en] = myev
        for t in w:
            t.w = myev
            t.r = {}
        return ins

    def dma(self, qn, out, in_, r=(), w=(), **kw):
        q = self.E[qn]
        for ev in list(self._deps(qn, r, w)):
            self._wait(q, ev)
        slot = self.slots[qn][self.dn[qn] % self.NSLOT]
        self.dn[qn] += 1
        if slot[1] > 0:
            self._wait(q, (slot[0], slot[1]))
        ins = q.h.dma_start(out=out, in_=in_, **kw)
        slot[1] += 16
        ins.then_inc(slot[0], 16)
        ev = (slot[0], slot[1])
        key = "d%d" % slot[0].num
        for t in r:
            t.r[key] = ev
        for t in w:
            t.w = ev
            t.r = {}

    def barrier(self):
        evs = []
        for q in self.slots:
            for sem, val in self.slots[q]:
                if val > 0:
                    evs.append((sem, val))
        for name, e in self.E.items():
            if e.cnt > 0:
                evs.append((e.sem, e.cnt))
        for name, e in self.E.items():
            for ev in evs:
                if ev[0] is e.sem and name == "pe":
                    continue
                self._wait(e, ev)

    def finish(self):
        sp = self.E["sp"]
        for q in self.slots:
            for sem, val in self.slots[q]:
                if val > 0:
                    self._wait(sp, (sem, val))
        for name, e in self.E.items():
            if name != "sp" and e.cnt > 0:
                self._wait(sp, (e.sem, e.cnt))


def host_consts(T):
    c = {}
    c["ident"] = np.eye(128, dtype=np.float32)
    j = np.arange(64)[:, None]
    i = np.arange(64)[None, :]
    c["tri64"] = np.stack([(-DEC) * (j <= i), (-DEC) * (j < i), (-DEC) * (j > i)], 1).astype(np.float32)
    c["ncol64"] = np.full((64, 1), -DEC, np.float32)
    su = (j < i).astype(np.float32)
    iu = (j <= i).astype(np.float32)
    sl = (j > i).astype(np.float32)
    mMA = np.concatenate([-su, iu], 1)
    mBB = np.concatenate([su, iu], 1)
    c["mMA"] = np.tile(mMA[:, None, :], (1, 8, 1)).astype(np.float32)
    c["mBB"] = np.tile(mBB[:, None, :], (1, 8, 1)).astype(np.float32)
    c["mNT"] = np.tile((-sl)[:, None, :], (1, 8, 1)).astype(np.float32)
    c["id8"] = np.tile(np.eye(64, dtype=np.float32)[:, None, :], (1, 8, 1))
    j = np.arange(128)[:, None]
    i = np.arange(128)[None, :]
    c["tri128"] = np.stack([(-1 / 16) * (j <= i), (-1 / 16) * (j > i)], 1).astype(np.float32)
    c["ncol128"] = np.full((128, 1), -1 / 16, np.float32)
    c["sel"] = (np.arange(16)[:, None, None] == np.arange(16)[None, :, None]).astype(np.float32) * np.ones((1, 1, 128), np.float32)
    c["iu128"] = np.tile((j <= i).astype(np.float32)[:, None, :], (1, 4, 1))
    return c


class Ctx:
    pass


def build(T, dbg=(), stages=("A", "R", "G", "O0", "M0", "S1", "O1", "M1")):
    nc = bass.Bass("TRN2", target_bir_lowering=False)
    es = ExitStack()
    P = Prog(nc, es)
    NT = T // 128
    NS = T // 512
    C = Ctx()
    C.nc, C.P, C.T, C.NT, C.NS = nc, P, T, NT, NS
    C.dbg = {}
    C.dsa_interleave = DSA_INTERLEAVE

    def din(name, shape, dt=F32):
        return nc.dram_tensor(name, list(shape), dt, kind="ExternalInput").ap()

    def dscr(name, shape, dt=F32, out=False):
        kind = "ExternalOutput" if (out or name in dbg) else "Internal"
        return nc.dram_tensor(name, list(shape), dt, kind=kind).ap()

    I = {}
    I["x"] = din("x", [T, D])
    for name, shape in (("w_in_even", [D, EVEN_COLS]), ("a_mu", [1, A_COLS]), ("a_w0", [1, 512]), ("a_w2", [64, 512]),
                        ("a_a0", [1, 512]), ("a_a2", [64, 512]), ("a_g2", [128, 512]), ("a_kk_scale", [1, 512]),
                        ("a_ka_scale", [1, 512]), ("a_r_k", [1, 512]), ("a_gn_g", [1, 512]), ("a_gn_b", [1, 512]),
                        ("b_gate_w2", [16, 256]), ("b_gate_b", [1, 256]), ("b_norm_g", [1, 512]),
                        ("w_out_even", [D, D]), ("w_in_odd", [D, ODD_COLS]), ("c_ik_ln_g", [1, 64]),
                        ("c_ik_ln_b", [1, 64]), ("w_out_odd", [D, D]), ("ln1_g", [2, D]), ("ln1_b", [2, D]),
                        ("ln2_g", [2, D]), ("ln2_b", [2, D]), ("router_w", [D, NE]), ("router_bias", [1, NE]),
                        ("exp_w_gate", [2, NE, D, DE]), ("exp_w_up", [2, NE, D, DE]), ("exp_w_down", [2, NE, DE, D])):
        I[name] = din(name, shape)
    hc = host_consts(T)
    hc.update(rope_consts(T))
    for k, v in hc.items():
        I["c_" + k] = din("c_" + k, list(v.shape), F32 if v.dtype == np.float32 else BF16)
    C.I = I
    out = dscr("out", [T, D], out=True)
    C.XT0 = dscr("XT0", [D, T + 1], BF16)
    C.XT0_tok = [Tok() for _ in range(NS)]
    C.XT0_z = Tok()
    C.YT = dscr("YT", [D, T], BF16)
    C.YT_tok = [[Tok() for _ in range(NS)] for _ in range(2)]
    C.H0 = dscr("H0", [T, D])
    C.H0_tok = [Tok() for _ in range(NT)]
    C.HT0 = dscr("HT0", [D, T], BF16)
    C.HT0_tok = [Tok() for _ in range(NS)]
    C.X1 = dscr("X1", [T, D])
    C.X1_tok = [Tok() for _ in range(NT)]
    C.XT1 = dscr("XT1", [D, T], BF16)
    C.XT1_tok = [Tok() for _ in range(NS)]

    for nm, shp in (("dbg_ya", [T, 512]), ("dbg_yb", [T, 512]), ("dbg_dsa", [T, 1024]), ("dbg_mask", [T, T]), ("dbg_sc", [T, T])):
        if nm in dbg:
            C.dbg[nm] = dscr(nm, shp, out=True)
    C.WGU16 = [dscr("WGU16_%d" % l, [NE, 128, 2 * 8 * DE], BF16) for l in range(2)]
    C.WGU16_tok = [[Tok() for _ in range(NE)] for l in range(2)]
    C.WD16 = [dscr("WD16_%d" % l, [128, 32 * D], BF16) for l in range(2)]
    C.WD16_tok = [Tok() for l in range(2)]
    C.banks = []
    for b in range(8):
        t = es.enter_context(nc.psum_tensor("psb%d" % b, [128, 512], F32))
        C.banks.append((t, Tok()))
    C.bi = 0

    C.bank_pool = list(range(8))

    def bank():
        b = C.banks[C.bank_pool[C.bi % len(C.bank_pool)]]
        C.bi += 1
        return b
    C.bank = bank

    C.ident = P.sb([128, 128], F32, "ident")
    C.ident_tok = Tok()
    P.dma("sp", C.ident[:], I["c_ident"], w=[C.ident_tok])

    C.cast_todo = {}
    if "M0" in stages:
        cast_weights(C, 0)
    if "A" in stages:
        phase_A(C)
    if "R" in stages:
        phase_rwkv(C)
    if "G" in stages:
        phase_gla(C)
    if "M1" in stages:
        cast_weights(C, 1)
    if "O0" in stages:
        phase_outproj(C, I["w_out_even"], C.YT, lambda s: [C.YT_tok[0][s], C.YT_tok[1][s]], I["x"], lambda i: [],
                      I["ln1_g"][0:1, :], I["ln1_b"][0:1, :], C.H0, C.H0_tok, C.HT0, C.HT0_tok)
    if "M0" in stages:
        cast_some(C, 0, 999)
        phase_moe(C, 0, C.H0, C.H0_tok, C.HT0, C.HT0_tok, C.X1, C.X1_tok, C.XT1, C.XT1_tok)
    if "S1" in stages:
        phase_dsa(C)
    if "O1" in stages:
        C.H1 = dscr("H1", [T, D])
        C.H1_tok = [Tok() for _ in range(NT)]
        C.HT1 = dscr("HT1", [D, T], BF16)
        C.HT1_tok = [Tok() for _ in range(NS)]
        phase_outproj(C, I["w_out_odd"], C.YT, lambda s: [C.YT_tok[0][s], C.YT_tok[1][s]], C.X1, lambda i: [C.X1_tok[i]],
                      I["ln1_g"][1:2, :], I["ln1_b"][1:2, :], C.H1, C.H1_tok, C.HT1, C.HT1_tok)
    if "M1" in stages:
        cast_some(C, 1, 999)
        out_tok = [Tok() for _ in range(NT)]
        phase_moe(C, 1, C.H1, C.H1_tok, C.HT1, C.HT1_tok, out, out_tok, None, None)
    P.finish()
    es.close()
    return nc


def XTv(ap):
    return ap.rearrange("(c p) t -> p c t", p=128)


def cast_weights(C, l):
    P, I = C.P, C.I
    th = []
    for e in range(NE):
        dst = C.WGU16[l][e].rearrange("p (t c f) -> p t c f", t=2, c=8)
        th.append(lambda e=e, dst=dst: P.dma("pool", dst[:, 0, :, :], I["exp_w_gate"][l, e].rearrange("(c p) f -> p c f", p=128), w=[C.WGU16_tok[l][e]]))
        th.append(lambda e=e, dst=dst: P.dma("pool", dst[:, 1, :, :], I["exp_w_up"][l, e].rearrange("(c p) f -> p c f", p=128), w=[C.WGU16_tok[l][e]]))
    wd_flat = I["exp_w_down"][l].rearrange("e f d -> (e f) d")
    dstd = C.WD16[l].rearrange("p (c d) -> p c d", c=32)
    for c4 in range(8):
        th.append(lambda c4=c4: P.dma("pool", dstd[:, c4 * 4:(c4 + 1) * 4, :], wd_flat[c4 * 512:(c4 + 1) * 512, :].rearrange("(c p) d -> p c d", p=128), w=[C.WD16_tok[l]]))
    C.cast_todo[l] = th


def cast_some(C, l, n):
    th = C.cast_todo.get(l, [])
    for _ in range(min(n, len(th))):
        th.pop(0)()


def phase_A(C):
    nc, P, T, I = C.nc, C.P, C.T, C.I
    with ExitStack() as es:
        xin = [P.sb([128, D], F32, "xin", es) for _ in range(2)]
        xin_tok = [Tok(), Tok()]
        st = [P.sb([128, 8, 512], BF16, "ast", es) for _ in range(2)]
        st_tok = [Tok(), Tok()]
        z = P.sb([128, 8, 1], BF16, "zc", es)
        zt = Tok()
        P.op("dve", lambda e: e.memset(z[:], 0.0), w=[zt])
        P.dma("sp", XTv(C.XT0)[:, :, 0:1], z[:], r=[zt], w=[C.XT0_z], allow_slow_non_contiguous=True)
        for s in range(C.NS):
            sb_ = st[s % 2]
            for j in range(4):
                i = s * 4 + j
                xb = xin[i % 2]
                xt = xin_tok[i % 2]
                P.dma("sp", xb[:], I["x"][i * 128:(i + 1) * 128, :], w=[xt])
                for half in range(2):
                    bt, bk = C.bank()
                    for c4 in range(4):
                        c = half * 4 + c4
                        P.op("pe", lambda e: e.transpose(bt[:, c4 * 128:(c4 + 1) * 128], xb[:, c * 128:(c + 1) * 128], C.ident[:]),
                             r=[xt, C.ident_tok], w=[bk], inc=(c4 == 3))
                    en = "act" if half == 0 else "dve"
                    src = bt[:, :].rearrange("p (c t) -> p c t", c=4)
                    dst = sb_[:, half * 4:(half + 1) * 4, j * 128:(j + 1) * 128]
                    if en == "act":
                        P.op("act", lambda e: e.copy(dst, src), r=[bk], w=[st_tok[s % 2]])
                    else:
                        P.op("dve", lambda e: e.tensor_copy(dst, src), r=[bk], w=[st_tok[s % 2]])
            P.dma("sp", XTv(C.XT0)[:, :, 1 + s * 512:1 + (s + 1) * 512], sb_[:], r=[st_tok[s % 2]], w=[C.XT0_tok[s]])
        P.barrier()


class TL:
    def __init__(self, t, k=None):
        self.t = t
        self.k = k or Tok()

    def __getitem__(self, idx):
        return self.t[idx]


def mk(P, shape, dt=F32, name=None, es=None):
    return TL(P.sb(shape, dt, name, es))


def bcast_load(C, dst_ap, src_row, np_, tok, q="sp"):
    C.P.dma(q, dst_ap, src_row.partition_broadcast(np_), w=[tok])


def hv(ap, h):
    return ap.rearrange("p (h v) -> p h v", h=h)


def phase_rwkv(C):
    nc, P, T, I = C.nc, C.P, C.T, C.I
    mm = ALU.mult
    with ExitStack() as es:
        W1 = mk(P, [128, 8, A_COLS], BF16, "W1", es)
        W2 = mk(P, [128, 8, A_COLS], BF16, "W2", es)
        with ExitStack() as es2:
            mub = mk(P, [128, A_COLS], F32, "mub", es2)
            omu = mk(P, [128, A_COLS], F32, "omu", es2)
            stg = [mk(P, [128, A_COLS], F32, "wstg", es2) for _ in range(2)]
            bcast_load(C, mub[:], I["a_mu"], 128, mub.k)
            P.op("dve", lambda e: e.tensor_scalar(omu[:], mub[:], -1.0, 1.0, ALU.mult, ALU.add), r=[mub.k], w=[omu.k])
            for c in range(8):
                s_ = stg[c % 2]
                P.dma("sp", s_[:], I["w_in_even"][c * 128:(c + 1) * 128, 0:A_COLS], w=[s_.k])
                P.op("dve", lambda e: e.tensor_tensor(W1[:, c, :], s_[:], omu[:], mm), r=[s_.k, omu.k], w=[W1.k])
                P.op("pool", lambda e: e.tensor_tensor(W2[:, c, :], s_[:], mub[:], mm), r=[s_.k, mub.k], w=[W2.k])
            P.barrier()
        LW = mk(P, [128, 512], BF16, "LW", es)
        G2 = mk(P, [128, 512], BF16, "G2", es)
        P.dma("pool", LW[0:64, :], I["a_w2"], w=[LW.k])
        P.dma("pool", LW[64:128, :], I["a_a2"], w=[LW.k])
        P.dma("pool", G2[:], I["a_g2"], w=[G2.k])
        BV = mk(P, [64, 7, 512], F32, "BV", es)
        for n, name in enumerate(("a_w0", "a_a0", "a_kk_scale", "a_ka_scale", "a_r_k", "a_gn_g", "a_gn_b")):
            bcast_load(C, BV[:, n, :], I[name], 64, BV.k)
        w0b, a0b, kksb, kab, rkb, gngb, gnbb = [BV[:, n, :] for n in range(7)]
        tri = mk(P, [64, 3, 64], F32, "tri", es)
        ncol = mk(P, [64, 1], F32, "ncol", es)
        mMA = mk(P, [64, 8, 128], F32, "mMA", es)
        mBB = mk(P, [64, 8, 128], F32, "mBB", es)
        mNT = mk(P, [64, 8, 64], F32, "mNT", es)
        id8 = mk(P, [64, 8, 64], F32, "id8", es)
        for tl, nm in ((tri, "c_tri64"), (ncol, "c_ncol64"), (mMA, "c_mMA"), (mBB, "c_mBB"), (mNT, "c_mNT"), (id8, "c_id8")):
            P.dma("sp", tl[:], I[nm], w=[tl.k])
        id64 = C.ident[0:64, 0:64]

        def wt(name, shape=(64, 512), dt=F32):
            return mk(P, list(shape), dt, name, es)
        ATs = [mk(P, [128, 8, 513], BF16, "ATs", es) for _ in range(2)]
        TX = wt("TX", (128, 512), BF16)
        SG = wt("SG", (128, 512), BF16)
        r_, k_, v_, sg, a_, kk, be, Bi, Ki, tmp = [wt(n) for n in ("r", "k", "v", "sg", "a", "kk", "be", "Bi", "Ki", "tmp")]
        Ep, Em, Ex, Ee = [wt(n) for n in ("Ep", "Em", "Ex", "Ee")]
        s8 = [wt("s8_%d" % n, (64, 8)) for n in range(4)]
        KRs = [wt("KR", (64, 8, 128), BF16) for _ in range(2)]
        BiT = wt("BiT", (64, 8, 64), BF16)
        KiT = wt("KiT", (64, 8, 64), BF16)
        MAs = [wt("MA", (64, 8, 128), BF16) for _ in range(2)]
        BBs = [wt("BB", (64, 8, 128), BF16) for _ in range(2)]
        Xb = [wt("X%d" % n, (64, 8, 64), BF16) for n in range(2)]
        XTb = [wt("XT%d" % n, (64, 8, 64), BF16) for n in range(2)]
        Qbs = [[wt("Q%d" % n, (64, 8, 64), BF16) for n in range(2)] for _ in range(2)]
        Xs = wt("Xs", (64, 8, 64), BF16)
        nU = wt("nU", (64, 8, 64), BF16)
        Y = wt("Y")
        tmpb = wt("tmpb")
        Hs = [wt("H%d" % n, (64, 8, 64)) for n in range(2)]
        Hbs = [wt("Hb%d" % n, (64, 8, 64), BF16) for n in range(2)]
        vbs = [wt("vb", (64, 512), BF16) for _ in range(2)]
        Ke16s = [wt("Ke16", (64, 512), BF16) for _ in range(2)]
        Be16s = [wt("Be16", (64, 512), BF16) for _ in range(2)]
        PCs = [wt("PC", (64, 8)) for _ in range(2)]
        gs_ = [wt("g", (64, 512)) for _ in range(2)]
        bonuss = [wt("bonus", (64, 512)) for _ in range(2)]
        P.op("pool", lambda e: e.memset(Hbs[0][:], 0.0), w=[Hbs[0].k])
        yst = [mk(P, [128, 4, 512], BF16, "yst", es) for _ in range(2)]
        P.op("dve", lambda e: e.memset(Hs[0][:], 0.0), w=[Hs[0].k])

        def psb():
            t, k = C.bank()
            return TL(t, k)

        def v3(tl_or_ap, h=8):
            return hv(tl_or_ap, h)

        def chunk(s, ci):
          at = ATs[s % 2]
          ys = yst[s % 2]
          cast_some(C, 0, 1)
          if ci == 0:
            rd = [C.XT0_tok[s]] + ([C.XT0_tok[s - 1]] if s > 0 else [C.XT0_z])
            P.dma("sp", at[:], XTv(C.XT0)[:, :, s * 512:s * 512 + 513], r=rd, w=[at.k])
            for which in range(2):
                pb = psb()
                c0 = 1536 + which * 128
                for c in range(8):
                    P.op("pe", lambda e: e.matmul(pb[:, :], lhsT=W1[:, c, c0:c0 + 128], rhs=at[:, c, 1:513], start=(c == 0), stop=False),
                         r=[W1.k, at.k], w=[pb.k], inc=False)
                for c in range(8):
                    P.op("pe", lambda e: e.matmul(pb[:, :], lhsT=W2[:, c, c0:c0 + 128], rhs=at[:, c, 0:512], start=False, stop=(c == 7)),
                         r=[W2.k, at.k], w=[pb.k], inc=(c == 7))
                if which == 0:
                    P.op("act", lambda e: e.activation(out=TX[0:64, :], in_=pb[0:64, :], func=AF.Tanh), r=[pb.k], w=[TX.k])
                    P.op("act", lambda e: e.copy(TX[64:128, :], pb[64:128, :]), r=[pb.k], w=[TX.k])
                else:
                    P.op("act", lambda e: e.activation(out=SG[:, :], in_=pb[:, :], func=AF.Sigmoid), r=[pb.k], w=[SG.k])
          if True:
            if True:
                g = s * 8 + ci
                t0 = ci * 64
                KR, MA, BB, Qb = KRs[g % 2], MAs[g % 2], BBs[g % 2], Qbs[g % 2]
                vb, Ke16, Be16, PC, g_, bonus = vbs[g % 2], Ke16s[g % 2], Be16s[g % 2], PCs[g % 2], gs_[g % 2], bonuss[g % 2]
                pr, pk, pv = psb(), psb(), psb()
                for pb, c0 in ((pr, 0), (pk, 512), (pv, 1024)):
                    for c in range(8):
                        P.op("pe", lambda e: e.matmul(pb[0:64, :], lhsT=at[:, c, 1 + t0:1 + t0 + 64], rhs=W1[:, c, c0:c0 + 512], start=(c == 0), stop=False),
                             r=[W1.k, at.k], w=[pb.k], inc=False)
                    for c in range(8):
                        P.op("pe", lambda e: e.matmul(pb[0:64, :], lhsT=at[:, c, t0:t0 + 64], rhs=W2[:, c, c0:c0 + 512], start=False, stop=(c == 7)),
                             r=[W2.k, at.k], w=[pb.k], inc=(c == 7))
                yield "F"
                pz, pza, pg = psb(), psb(), psb()
                P.op("pe", lambda e: e.matmul(pz[0:64, :], lhsT=TX[0:64, t0:t0 + 64], rhs=LW[0:64, :], start=True, stop=True), r=[TX.k, LW.k], w=[pz.k])
                P.op("pe", lambda e: e.matmul(pza[0:64, :], lhsT=TX[64:128, t0:t0 + 64], rhs=LW[64:128, :], start=True, stop=True), r=[TX.k, LW.k], w=[pza.k])
                P.op("pe", lambda e: e.matmul(pg[0:64, :], lhsT=SG[:, t0:t0 + 64], rhs=G2[:, :], start=True, stop=True), r=[SG.k, G2.k], w=[pg.k])
                P.op("act", lambda e: e.copy(r_[:], pr[0:64, :]), r=[pr.k], w=[r_.k])
                P.op("act", lambda e: e.copy(v_[:], pv[0:64, :]), r=[pv.k], w=[v_.k])
                P.op("act", lambda e: e.copy(vb[:], pv[0:64, :]), r=[pv.k], w=[vb.k])
                P.op("act", lambda e: e.copy(g_[:], pg[0:64, :]), r=[pg.k], w=[g_.k])
                P.op("dve", lambda e: e.tensor_copy(k_[:], pk[0:64, :]), r=[pk.k], w=[k_.k])
                P.op("dve", lambda e: e.tensor_tensor(sg[:], pz[0:64, :], w0b, ALU.add), r=[pz.k, BV.k], w=[sg.k])
                P.op("act", lambda e: e.activation(out=sg[:], in_=sg[:], func=AF.Sigmoid), r=[sg.k], w=[sg.k])
                P.op("dve", lambda e: e.tensor_tensor(a_[:], pza[0:64, :], a0b, ALU.add), r=[pza.k, BV.k], w=[a_.k])
                P.op("act", lambda e: e.activation(out=a_[:], in_=a_[:], func=AF.Sigmoid), r=[a_.k], w=[a_.k])
                yield "F"
                P.op("pool", lambda e: e.tensor_tensor(kk[:], k_[:], kksb, mm), r=[k_.k, BV.k], w=[kk.k])
                P.op("pool", lambda e: e.tensor_tensor(tmp[:], kk[:], kk[:], mm), r=[kk.k], w=[tmp.k])
                P.op("dve", lambda e: e.tensor_reduce(s8[0][:], v3(tmp[:]), AX.X, ALU.add), r=[tmp.k], w=[s8[0].k])
                P.op("dve", lambda e: e.tensor_scalar(s8[0][:], s8[0][:], 1e-24, None, ALU.max), r=[s8[0].k], w=[s8[0].k])
                P.op("act", lambda e: e.activation(out=s8[0][:], in_=s8[0][:], func=AF.Ln), r=[s8[0].k], w=[s8[0].k])
                P.op("act", lambda e: e.activation(out=s8[0][:], in_=s8[0][:], func=AF.Exp, scale=-0.5), r=[s8[0].k], w=[s8[0].k])
                P.op("dve", lambda e: e.tensor_tensor(v3(kk[:]), v3(kk[:]), s8[0][:, :].unsqueeze(2).to_broadcast([64, 8, 64]), mm),
                     r=[kk.k, s8[0].k], w=[kk.k])
                P.op("pool", lambda e: e.tensor_tensor(be[:], kk[:], a_[:], mm), r=[kk.k, a_.k], w=[be.k])
                P.op("dve", lambda e: e.scalar_tensor_tensor(tmp[:], a_[:], -1.0, kab, ALU.add, mm), r=[a_.k, BV.k], w=[tmp.k])
                P.op("dve", lambda e: e.scalar_tensor_tensor(k_[:], tmp[:], 1.0, k_[:], ALU.add, mm), r=[tmp.k, k_.k], w=[k_.k])
                P.op("pool", lambda e: e.tensor_tensor(tmp[:], r_[:], k_[:], mm), r=[r_.k, k_.k], w=[tmp.k])
                P.op("pool", lambda e: e.tensor_tensor(tmp[:], tmp[:], rkb, mm), r=[tmp.k, BV.k], w=[tmp.k])
                P.op("dve", lambda e: e.tensor_reduce(s8[1][:], v3(tmp[:]), AX.X, ALU.add), r=[tmp.k], w=[s8[1].k])
                P.op("dve", lambda e: e.tensor_tensor(v3(bonus[:]), v3(v_[:]), s8[1][:, :].unsqueeze(2).to_broadcast([64, 8, 64]), mm),
                     r=[v_.k, s8[1].k], w=[bonus.k])
                yield "F"
                pcl, pcx, pca, ppc = psb(), psb(), psb(), psb()
                for pb, n in ((pcl, 0), (pcx, 1), (pca, 2)):
                    P.op("pe", lambda e: e.matmul(pb[0:64, :], lhsT=tri[:, n, :], rhs=sg[:], start=True, stop=True), r=[tri.k, sg.k], w=[pb.k])
                for h in range(8):
                    P.op("pe", lambda e: e.matmul(ppc[0:64, h:h + 1], lhsT=sg[:, h * 64:(h + 1) * 64], rhs=ncol[:], start=True, stop=True),
                         r=[sg.k, ncol.k], w=[ppc.k], inc=(h == 7))
                P.op("act", lambda e: e.activation(out=Ep[:], in_=pcl[0:64, :], func=AF.Exp), r=[pcl.k], w=[Ep.k])
                P.op("act", lambda e: e.activation(out=Em[:], in_=pcl[0:64, :], func=AF.Exp, scale=-1.0), r=[pcl.k], w=[Em.k])
                P.op("act", lambda e: e.activation(out=Ex[:], in_=pcx[0:64, :], func=AF.Exp), r=[pcx.k], w=[Ex.k])
                P.op("act", lambda e: e.activation(out=Ee[:], in_=pca[0:64, :], func=AF.Exp), r=[pca.k], w=[Ee.k])
                P.op("act", lambda e: e.activation(out=PC[:], in_=ppc[0:64, 0:8], func=AF.Exp), r=[ppc.k], w=[PC.k])
                P.op("dve", lambda e: e.tensor_tensor(r_[:], r_[:], Ep[:], mm), r=[r_.k, Ep.k], w=[r_.k])
                P.op("pool", lambda e: e.tensor_tensor(kk[:], kk[:], Ex[:], mm), r=[kk.k, Ex.k], w=[kk.k])
                P.op("dve", lambda e: e.tensor_tensor(Bi[:], be[:], Em[:], mm), r=[be.k, Em.k], w=[Bi.k])
                P.op("pool", lambda e: e.tensor_tensor(Ki[:], k_[:], Em[:], mm), r=[k_.k, Em.k], w=[Ki.k])
                P.op("dve", lambda e: e.tensor_tensor(Ke16[:], k_[:], Ee[:], mm), r=[k_.k, Ee.k], w=[Ke16.k])
                P.op("pool", lambda e: e.tensor_tensor(Be16[:], be[:], Ee[:], mm), r=[be.k, Ee.k], w=[Be16.k])
                yield "F"
                for src, dst, off, en in ((kk, KR, 0, "act"), (r_, KR, 64, "dve"), (Bi, BiT, 0, "act"), (Ki, KiT, 0, "dve")):
                    pb = psb()
                    for h in range(8):
                        P.op("pe", lambda e: e.transpose(pb[0:64, h * 64:(h + 1) * 64], src[:, h * 64:(h + 1) * 64], id64),
                             r=[src.k, C.ident_tok], w=[pb.k], inc=(h == 7))
                    d_ = dst[:, :, off:off + 64]
                    s_ = v3(pb[0:64, :])
                    if en == "act":
                        P.op("act", lambda e: e.copy(d_, s_), r=[pb.k], w=[dst.k])
                    else:
                        P.op("dve", lambda e: e.tensor_copy(d_, s_), r=[pb.k], w=[dst.k])
                yield "F"
                pma = [psb(), psb()]
                pbb = [psb(), psb()]
                pnt = psb()
                for h in range(8):
                    hb, hh = h // 4, h % 4
                    P.op("pe", lambda e: e.matmul(pma[hb][0:64, hh * 128:(hh + 1) * 128], lhsT=BiT[:, h, :], rhs=KR[:, h, :], start=True, stop=True),
                         r=[BiT.k, KR.k], w=[pma[hb].k], inc=(hh == 3))
                for h in range(8):
                    hb, hh = h // 4, h % 4
                    P.op("pe", lambda e: e.matmul(pbb[hb][0:64, hh * 128:(hh + 1) * 128], lhsT=KiT[:, h, :], rhs=KR[:, h, :], start=True, stop=True),
                         r=[KiT.k, KR.k], w=[pbb[hb].k], inc=(hh == 3))
                for h in range(8):
                    P.op("pe", lambda e: e.matmul(pnt[0:64, h * 64:(h + 1) * 64], lhsT=KR[:, h, 0:64], rhs=BiT[:, h, :], start=True, stop=True),
                         r=[BiT.k, KR.k], w=[pnt.k], inc=(h == 7))
                for hb in range(2):
                    P.op("dve", lambda e: e.tensor_tensor(MA[:, hb * 4:(hb + 1) * 4, :], hv(pma[hb][0:64, :], 4), mMA[:, hb * 4:(hb + 1) * 4, :], mm),
                         r=[pma[hb].k, mMA.k], w=[MA.k])
                    P.op("dve", lambda e: e.tensor_tensor(BB[:, hb * 4:(hb + 1) * 4, :], hv(pbb[hb][0:64, :], 4), mBB[:, hb * 4:(hb + 1) * 4, :], mm),
                         r=[pbb[hb].k, mBB.k], w=[BB.k])
                X, XT, Q = Xb[0], XTb[0], Qb[0]
                P.op("dve", lambda e: e.tensor_tensor(XT[:], v3(pnt[0:64, :]), mNT[:], mm), r=[pnt.k, mNT.k], w=[XT.k])
                P.op("pool", lambda e: e.tensor_copy(X[:], MA[:, :, 0:64]), r=[MA.k], w=[X.k])
                P.op("pool", lambda e: e.tensor_tensor(Q[:], MA[:, :, 0:64], id8[:], ALU.add), r=[MA.k, id8.k], w=[Q.k])
                for lvl in range(5):
                    Xn, XTn, Qn = Xb[(lvl + 1) % 2], XTb[(lvl + 1) % 2], Qb[(lvl + 1) % 2]
                    pxt = psb()
                    for h in range(8):
                        P.op("pe", lambda e: e.matmul(pxt[0:64, h * 64:(h + 1) * 64], lhsT=X[:, h, :], rhs=XT[:, h, :], start=True, stop=True),
                             r=[X.k, XT.k], w=[pxt.k], inc=(h == 7))
                    if lvl < 4:
                        px = psb()
                        for h in range(8):
                            P.op("pe", lambda e: e.matmul(px[0:64, h * 64:(h + 1) * 64], lhsT=XT[:, h, :], rhs=X[:, h, :], start=True, stop=True),
                                 r=[X.k, XT.k], w=[px.k], inc=(h == 7))
                    P.op("act", lambda e: e.copy(XTn[:], v3(pxt[0:64, :])), r=[pxt.k], w=[XTn.k])
                    if lvl < 4:
                        P.op("dve", lambda e: e.tensor_copy(Xn[:], v3(px[0:64, :])), r=[px.k], w=[Xn.k])
                    pq = psb()
                    for h in range(8):
                        P.op("pe", lambda e: e.matmul(pq[0:64, h * 64:(h + 1) * 64], lhsT=XTn[:, h, :], rhs=Q[:, h, :], start=True, stop=True),
                             r=[XTn.k, Q.k], w=[pq.k], inc=(h == 7))
                    P.op("dve", lambda e: e.tensor_tensor(Qn[:], Q[:], v3(pq[0:64, :]), ALU.add), r=[Q.k, pq.k], w=[Qn.k])
                    X, XT, Q = Xn, XTn, Qn
                    yield "F"
                yield "END_FRONT"
                H, Hn = Hs[g % 2], Hs[(g + 1) % 2]
                Hb, Hbn = Hbs[g % 2], Hbs[(g + 1) % 2]
                pxs = psb()
                for h in range(8):
                    P.op("pe", lambda e: e.matmul(pxs[0:64, h * 64:(h + 1) * 64], lhsT=KR[:, h, 0:64], rhs=Hb[:, h, :], start=True, stop=False),
                         r=[KR.k, Hb.k], w=[pxs.k], inc=False)
                    P.op("pe", lambda e: e.matmul(pxs[0:64, h * 64:(h + 1) * 64], lhsT=BB[:, h, 0:64], rhs=vb[:, h * 64:(h + 1) * 64], start=False, stop=True),
                         r=[BB.k, vb.k], w=[pxs.k], inc=(h == 7))
                P.op("act", lambda e: e.copy(Xs[:], v3(pxs[0:64, :])), r=[pxs.k], w=[Xs.k])
                yield "B"
                pu = psb()
                for h in range(8):
                    P.op("pe", lambda e: e.matmul(pu[0:64, h * 64:(h + 1) * 64], lhsT=Q[:, h, :], rhs=Xs[:, h, :], start=True, stop=True),
                         r=[Q.k, Xs.k], w=[pu.k], inc=(h == 7))
                P.op("act", lambda e: e.mul(nU[:], v3(pu[0:64, :]), -1.0), r=[pu.k], w=[nU.k])
                yield "B"
                py, ph = psb(), psb()
                for h in range(8):
                    sl = slice(h * 64, (h + 1) * 64)
                    P.op("pe", lambda e: e.matmul(py[0:64, sl], lhsT=KR[:, h, 64:128], rhs=Hb[:, h, :], start=True, stop=False), r=[KR.k, Hb.k], w=[py.k], inc=False)
                    P.op("pe", lambda e: e.matmul(py[0:64, sl], lhsT=BB[:, h, 64:128], rhs=vb[:, sl], start=False, stop=False), r=[BB.k, vb.k], w=[py.k], inc=False)
                    P.op("pe", lambda e: e.matmul(py[0:64, sl], lhsT=MA[:, h, 64:128], rhs=nU[:, h, :], start=False, stop=True), r=[MA.k, nU.k], w=[py.k], inc=(h == 7))
                for h in range(8):
                    sl = slice(h * 64, (h + 1) * 64)
                    P.op("pe", lambda e: e.matmul(ph[0:64, sl], lhsT=Ke16[:, sl], rhs=vb[:, sl], start=True, stop=False), r=[Ke16.k, vb.k], w=[ph.k], inc=False)
                    P.op("pe", lambda e: e.matmul(ph[0:64, sl], lhsT=Be16[:, sl], rhs=nU[:, h, :], start=False, stop=True), r=[Be16.k, nU.k], w=[ph.k], inc=(h == 7))
                P.op("pool", lambda e: e.tensor_tensor(Hn[:], H[:], PC[:, :].unsqueeze(2).to_broadcast([64, 8, 64]), mm), r=[H.k, PC.k], w=[Hn.k])
                P.op("dve", lambda e: e.tensor_tensor(Hn[:], Hn[:], v3(ph[0:64, :]), ALU.add), r=[Hn.k, ph.k], w=[Hn.k])
                P.op("act", lambda e: e.copy(Hbn[:], Hn[:]), r=[Hn.k], w=[Hbn.k])
                yield "B"
                P.op("act", lambda e: e.copy(Y[:], py[0:64, :]), r=[py.k], w=[Y.k])
                P.op("dve", lambda e: e.tensor_reduce(s8[2][:], v3(Y[:]), AX.X, ALU.add), r=[Y.k], w=[s8[2].k])
                P.op("dve", lambda e: e.tensor_scalar(s8[2][:], s8[2][:], 1.0 / 64, None, mm), r=[s8[2].k], w=[s8[2].k])
                P.op("dve", lambda e: e.tensor_tensor(v3(Y[:]), v3(Y[:]), s8[2][:, :].unsqueeze(2).to_broadcast([64, 8, 64]), ALU.subtract),
                     r=[Y.k, s8[2].k], w=[Y.k])
                yield "B"
                P.op("pool", lambda e: e.tensor_tensor(tmpb[:], Y[:], Y[:], mm), r=[Y.k], w=[tmpb.k])
                P.op("dve", lambda e: e.tensor_reduce(s8[3][:], v3(tmpb[:]), AX.X, ALU.add), r=[tmpb.k], w=[s8[3].k])
                P.op("act", lambda e: e.activation(out=s8[3][:], in_=s8[3][:], func=AF.Ln, bias=64e-5, scale=1.0 / 64), r=[s8[3].k], w=[s8[3].k])
                P.op("act", lambda e: e.activation(out=s8[3][:], in_=s8[3][:], func=AF.Exp, scale=-0.5), r=[s8[3].k], w=[s8[3].k])
                P.op("dve", lambda e: e.tensor_tensor(v3(Y[:]), v3(Y[:]), s8[3][:, :].unsqueeze(2).to_broadcast([64, 8, 64]), mm),
                     r=[Y.k, s8[3].k], w=[Y.k])
                P.op("pool", lambda e: e.tensor_tensor(Y[:], Y[:], gngb, mm), r=[Y.k, BV.k], w=[Y.k])
                P.op("pool", lambda e: e.tensor_tensor(Y[:], Y[:], gnbb, ALU.add), r=[Y.k, BV.k], w=[Y.k])
                P.op("dve", lambda e: e.tensor_tensor(Y[:], Y[:], bonus[:], ALU.add), r=[Y.k, bonus.k], w=[Y.k])
                P.op("dve", lambda e: e.tensor_tensor(Y[:], Y[:], g_[:], mm), r=[Y.k, g_.k], w=[Y.k])
                if "dbg_ya" in C.dbg:
                    P.dma("sp", C.dbg["dbg_ya"][g * 64:(g + 1) * 64, :], Y[:], r=[Y.k])
                yield "B"
                pb = psb()
                for q in range(4):
                    P.op("pe", lambda e: e.transpose(pb[:, q * 64:(q + 1) * 64], Y[:, q * 128:(q + 1) * 128], id64), r=[Y.k, C.ident_tok], w=[pb.k], inc=(q == 3))
                P.op("act", lambda e: e.copy(ys[:, :, t0:t0 + 64], hv(pb[:, 0:256], 4)), r=[pb.k], w=[ys.k])
                if ci == 7:
                    P.dma("sp", XTv(C.YT)[:, 0:4, s * 512:(s + 1) * 512], ys[:], r=[ys.k], w=[C.YT_tok[0][s]])

        makers = [(lambda s=s, ci=ci: chunk(s, ci)) for s in range(C.NS) for ci in range(8)]
        if RWKV_INTERLEAVE:
            pipeline2(makers, interleave=True)
        else:
            def to_end_front(g_):
                while next(g_) != "END_FRONT":
                    pass
            g = makers[0]()
            to_end_front(g)
            for n in range(len(makers)):
                for _ in range(3):
                    next(g)
                g2 = None
                if n + 1 < len(makers):
                    g2 = makers[n + 1]()
                    next(g2)
                for _ in g:
                    pass
                if g2 is not None:
                    to_end_front(g2)
                g = g2
        P.barrier()


def pipeline2(makers, interleave=True):
    if not interleave:
        for mk_ in makers:
            for _ in mk_():
                pass
        return
    prevB = None
    for mk_ in makers:
        g = mk_()
        while True:
            r = next(g)
            if prevB is not None:
                try:
                    next(prevB)
                except StopIteration:
                    prevB = None
            if r == "END_FRONT":
                break
        if prevB is not None:
            for _ in prevB:
                pass
        prevB = g
    if prevB is not None:
        for _ in prevB:
            pass


def psb(C):
    t, k = C.bank()
    return TL(t, k)


def phase_gla(C):
    nc, P, T, I = C.nc, C.P, C.T, C.I
    mm = ALU.mult
    with ExitStack() as es:
        WB = mk(P, [128, 8, B_COLS], BF16, "WB", es)
        for c in range(8):
            P.dma("pool", WB[:, c, :], I["w_in_even"][c * 128:(c + 1) * 128, A_COLS:EVEN_COLS], w=[WB.k])
        GW2 = mk(P, [16, 256], BF16, "GW2", es)
        P.dma("pool", GW2[:], I["b_gate_w2"], w=[GW2.k])
        gbb = mk(P, [128, 256], F32, "gbb", es)
        ngb = mk(P, [128, 512], F32, "ngb", es)
        bcast_load(C, gbb[:], I["b_gate_b"], 128, gbb.k)
        bcast_load(C, ngb[:], I["b_norm_g"], 128, ngb.k)
        tri = mk(P, [128, 2, 128], F32, "tri128", es)
        ncol = mk(P, [128, 1], F32, "ncol128", es)
        iu = mk(P, [128, 4, 128], F32, "iu128", es)
        for tl, nm in ((tri, "c_tri128"), (ncol, "c_ncol128"), (iu, "c_iu128")):
            P.dma("sp", tl[:], I[nm], w=[tl.k])
        id64 = C.ident[0:64, 0:64]

        def wt(name, shape, dt=F32):
            return mk(P, list(shape), dt, name, es)
        ATs = [wt("ATg", (128, 8, 512), BF16) for _ in range(2)]
        AL = wt("AL", (16, 512), BF16)
        l_ = wt("l", (128, 256))
        Eq, Ei, Ee = wt("Eq", (128, 256)), wt("Ei", (128, 256)), wt("Ee", (128, 256))
        PCg = wt("PCg", (64, 4))
        qd, ki, ke = wt("qd", (128, 256)), wt("ki", (128, 256)), wt("ke", (128, 256))
        v_ = wt("vg", (128, 512))
        qdT, kiT = wt("qdT", (64, 4, 128)), wt("kiT", (64, 4, 128))
        attT = wt("attT", (128, 4, 128))
        Ss = [wt("S%d" % n, (64, 4, 128)) for n in range(2)]
        o_ = wt("o", (128, 512))
        sq = wt("sqg", (128, 512))
        sl_ = wt("silu", (128, 512))
        m4 = wt("m4", (128, 4))
        yst = [wt("ystg", (128, 4, 512), BF16) for _ in range(2)]
        P.op("dve", lambda e: e.memset(Ss[0][:], 0.0), w=[Ss[0].k])
        for s in range(C.NS):
            at = ATs[s % 2]
            P.dma("sp", at[:], XTv(C.XT0)[:, :, 1 + s * 512:1 + (s + 1) * 512], r=[C.XT0_tok[s]], w=[at.k])
            pb = psb(C)
            for c in range(8):
                P.op("pe", lambda e: e.matmul(pb[0:16, :], lhsT=WB[:, c, 1536:1552], rhs=at[:, c, :], start=(c == 0), stop=(c == 7)),
                     r=[WB.k, at.k], w=[pb.k], inc=(c == 7))
            P.op("act", lambda e: e.copy(AL[:], pb[0:16, :]), r=[pb.k], w=[AL.k])
            ys = yst[s % 2]
            for ci in range(4):
                g = s * 4 + ci
                t0 = ci * 128
                pqk, pv, pg = psb(C), psb(C), psb(C)
                for pb, c0 in ((pqk, 0), (pv, 512), (pg, 1024)):
                    for c in range(8):
                        P.op("pe", lambda e: e.matmul(pb[:, :], lhsT=at[:, c, t0:t0 + 128], rhs=WB[:, c, c0:c0 + 512], start=(c == 0), stop=(c == 7)),
                             r=[WB.k, at.k], w=[pb.k], inc=(c == 7))
                pla = psb(C)
                P.op("pe", lambda e: e.matmul(pla[:, 0:256], lhsT=AL[:, t0:t0 + 128], rhs=GW2[:], start=True, stop=True), r=[AL.k, GW2.k], w=[pla.k])
                P.op("dve", lambda e: e.tensor_tensor(l_[:], pla[:, 0:256], gbb[:], ALU.add), r=[pla.k, gbb.k], w=[l_.k])
                P.op("act", lambda e: e.activation(out=l_[:], in_=l_[:], func=AF.Exp, scale=-1.0), r=[l_.k], w=[l_.k])
                P.op("act", lambda e: e.activation(out=l_[:], in_=l_[:], func=AF.Ln, bias=1.0), r=[l_.k], w=[l_.k])
                pbc, pba, ppc = psb(C), psb(C), psb(C)
                P.op("pe", lambda e: e.matmul(pbc[:, 0:256], lhsT=tri[:, 0, :], rhs=l_[:], start=True, stop=True), r=[tri.k, l_.k], w=[pbc.k])
                P.op("pe", lambda e: e.matmul(pba[:, 0:256], lhsT=tri[:, 1, :], rhs=l_[:], start=True, stop=True), r=[tri.k, l_.k], w=[pba.k])
                for h in range(4):
                    P.op("pe", lambda e: e.matmul(ppc[0:64, h:h + 1], lhsT=l_[:, h * 64:(h + 1) * 64], rhs=ncol[:], start=True, stop=True),
                         r=[l_.k, ncol.k], w=[ppc.k], inc=(h == 3))
                P.op("act", lambda e: e.activation(out=Eq[:], in_=pbc[:, 0:256], func=AF.Exp), r=[pbc.k], w=[Eq.k])
                P.op("act", lambda e: e.activation(out=Ei[:], in_=pbc[:, 0:256], func=AF.Exp, scale=-1.0), r=[pbc.k], w=[Ei.k])
                P.op("act", lambda e: e.activation(out=Ee[:], in_=pba[:, 0:256], func=AF.Exp), r=[pba.k], w=[Ee.k])
                P.op("act", lambda e: e.activation(out=PCg[:], in_=ppc[0:64, 0:4], func=AF.Exp), r=[ppc.k], w=[PCg.k])
                P.op("dve", lambda e: e.scalar_tensor_tensor(qd[:], pqk[:, 0:256], 0.125, Eq[:], mm, mm), r=[pqk.k, Eq.k], w=[qd.k])
                P.op("dve", lambda e: e.tensor_tensor(ki[:], pqk[:, 256:512], Ei[:], mm), r=[pqk.k, Ei.k], w=[ki.k])
                P.op("dve", lambda e: e.tensor_tensor(ke[:], pqk[:, 256:512], Ee[:], mm), r=[pqk.k, Ee.k], w=[ke.k])
                P.op("act", lambda e: e.copy(v_[:], pv[:, :]), r=[pv.k], w=[v_.k])
                P.op("act", lambda e: e.activation(out=sl_[:], in_=pg[:, :], func=AF.Silu), r=[pg.k], w=[sl_.k])
                for src, dst, en in ((qd, qdT, "act"), (ki, kiT, "dve")):
                    pb = psb(C)
                    for h in range(4):
                        P.op("pe", lambda e: e.transpose(pb[0:64, h * 128:(h + 1) * 128], src[:, h * 64:(h + 1) * 64], C.ident[:]),
                             r=[src.k, C.ident_tok], w=[pb.k], inc=(h == 3))
                    if en == "act":
                        P.op("act", lambda e: e.copy(dst[:], hv(pb[0:64, :], 4)), r=[pb.k], w=[dst.k])
                    else:
                        P.op("dve", lambda e: e.tensor_copy(dst[:], hv(pb[0:64, :], 4)), r=[pb.k], w=[dst.k])
                patt = psb(C)
                for h in range(4):
                    P.op("pe", lambda e: e.matmul(patt[:, h * 128:(h + 1) * 128], lhsT=kiT[:, h, :], rhs=qdT[:, h, :], start=True, stop=True),
                         r=[kiT.k, qdT.k], w=[patt.k], inc=(h == 3))
                P.op("dve", lambda e: e.tensor_tensor(attT[:], hv(patt[:, :], 4), iu[:], mm), r=[patt.k, iu.k], w=[attT.k])
                S, Sn = Ss[g % 2], Ss[(g + 1) % 2]
                po, pS = psb(C), psb(C)
                for h in range(4):
                    sl = slice(h * 128, (h + 1) * 128)
                    P.op("pe", lambda e: e.matmul(po[:, sl], lhsT=attT[:, h, :], rhs=v_[:, sl], start=True, stop=False), r=[attT.k, v_.k], w=[po.k], inc=False)
                    P.op("pe", lambda e: e.matmul(po[:, sl], lhsT=qdT[:, h, :], rhs=S[:, h, :], start=False, stop=True), r=[qdT.k, S.k], w=[po.k], inc=(h == 3))
                for h in range(4):
                    sl = slice(h * 128, (h + 1) * 128)
                    P.op("pe", lambda e: e.matmul(pS[0:64, sl], lhsT=ke[:, h * 64:(h + 1) * 64], rhs=v_[:, sl], start=True, stop=True), r=[ke.k, v_.k], w=[pS.k], inc=(h == 3))
                P.op("pool", lambda e: e.tensor_tensor(Sn[:], S[:], PCg[:, :].unsqueeze(2).to_broadcast([64, 4, 128]), mm), r=[S.k, PCg.k], w=[Sn.k])
                P.op("dve", lambda e: e.tensor_tensor(Sn[:], Sn[:], hv(pS[0:64, :], 4), ALU.add), r=[Sn.k, pS.k], w=[Sn.k])
                P.op("act", lambda e: e.copy(o_[:], po[:, :]), r=[po.k], w=[o_.k])
                P.op("pool", lambda e: e.tensor_tensor(sq[:], o_[:], o_[:], mm), r=[o_.k], w=[sq.k])
                P.op("dve", lambda e: e.tensor_reduce(m4[:], hv(sq[:], 4), AX.X, ALU.add), r=[sq.k], w=[m4.k])
                P.op("act", lambda e: e.activation(out=m4[:], in_=m4[:], func=AF.Ln, bias=1e-5, scale=1.0 / 128), r=[m4.k], w=[m4.k])
                P.op("act", lambda e: e.activation(out=m4[:], in_=m4[:], func=AF.Exp, scale=-0.5), r=[m4.k], w=[m4.k])
                P.op("dve", lambda e: e.tensor_tensor(hv(o_[:], 4), hv(o_[:], 4), m4[:, :].unsqueeze(2).to_broadcast([128, 4, 128]), mm), r=[o_.k, m4.k], w=[o_.k])
                P.op("pool", lambda e: e.tensor_tensor(o_[:], o_[:], ngb[:], mm), r=[o_.k, ngb.k], w=[o_.k])
                P.op("dve", lambda e: e.tensor_tensor(o_[:], o_[:], sl_[:], mm), r=[o_.k, sl_.k], w=[o_.k])
                if "dbg_yb" in C.dbg:
                    P.dma("sp", C.dbg["dbg_yb"][g * 128:(g + 1) * 128, :], o_[:], r=[o_.k])
                pb = psb(C)
                for q in range(4):
                    P.op("pe", lambda e: e.transpose(pb[:, q * 128:(q + 1) * 128], o_[:, q * 128:(q + 1) * 128], C.ident[:]), r=[o_.k, C.ident_tok], w=[pb.k], inc=(q == 3))
                P.op("act", lambda e: e.copy(ys[:, :, t0:t0 + 128], hv(pb[:, :], 4)), r=[pb.k], w=[ys.k])
            P.dma("sp", XTv(C.YT)[:, 4:8, s * 512:(s + 1) * 512], ys[:], r=[ys.k], w=[C.YT_tok[1][s]])
        P.barrier()


def ln_inplace(C, xt, gb, bb, st, junk):
    P = C.P
    P.op("dve", lambda e: e.tensor_reduce(st[:, 0:1], xt[:], AX.X, ALU.add), r=[xt.k], w=[st.k])
    P.op("dve", lambda e: e.tensor_scalar(st[:, 0:1], st[:, 0:1], 1.0 / D, None, ALU.mult), r=[st.k], w=[st.k])
    P.op("dve", lambda e: e.tensor_scalar(xt[:], xt[:], st[:, 0:1], None, ALU.subtract), r=[xt.k, st.k], w=[xt.k])
    P.op("act", lambda e: e.activation(out=junk[:], in_=xt[:], func=AF.Square, accum_out=st[:, 1:2]), r=[xt.k], w=[junk.k, st.k])
    P.op("act", lambda e: e.activation(out=st[:, 1:2], in_=st[:, 1:2], func=AF.Sqrt, bias=LN_EPS, scale=1.0 / D), r=[st.k], w=[st.k])
    P.op("dve", lambda e: e.reciprocal(st[:, 1:2], st[:, 1:2]), r=[st.k], w=[st.k])
    P.op("dve", lambda e: e.scalar_tensor_tensor(xt[:], xt[:], st[:, 1:2], gb[:], ALU.mult, ALU.mult), r=[xt.k, st.k, gb.k], w=[xt.k])
    P.op("pool", lambda e: e.tensor_tensor(xt[:], xt[:], bb[:], ALU.add), r=[xt.k, bb.k], w=[xt.k])


def ln_lockstep(C, xs, gb, bb, sts, junk):
    P = C.P
    n = len(xs)
    for k in range(n):
        xt, st = xs[k], sts[k]
        P.op("dve", lambda e: e.tensor_reduce(st[:, 0:1], xt[:], AX.X, ALU.add), r=[xt.k], w=[st.k])
    for k in range(n):
        xt, st = xs[k], sts[k]
        P.op("dve", lambda e: e.tensor_scalar(st[:, 0:1], st[:, 0:1], 1.0 / D, None, ALU.mult), r=[st.k], w=[st.k])
    for k in range(n):
        xt, st = xs[k], sts[k]
        P.op("dve", lambda e: e.tensor_scalar(xt[:], xt[:], st[:, 0:1], None, ALU.subtract), r=[xt.k, st.k], w=[xt.k])
        P.op("act", lambda e: e.activation(out=junk[:], in_=xt[:], func=AF.Square, accum_out=st[:, 1:2]), r=[xt.k], w=[junk.k, st.k])
    for k in range(n):
        xt, st = xs[k], sts[k]
        P.op("act", lambda e: e.activation(out=st[:, 1:2], in_=st[:, 1:2], func=AF.Sqrt, bias=LN_EPS, scale=1.0 / D), r=[st.k], w=[st.k])
    for k in range(n):
        xt, st = xs[k], sts[k]
        P.op("dve", lambda e: e.reciprocal(st[:, 1:2], st[:, 1:2]), r=[st.k], w=[st.k])
    for k in range(n):
        xt, st = xs[k], sts[k]
        P.op("dve", lambda e: e.scalar_tensor_tensor(xt[:], xt[:], st[:, 1:2], gb[:], ALU.mult, ALU.mult), r=[xt.k, st.k, gb.k], w=[xt.k])
        P.op("pool", lambda e: e.tensor_tensor(xt[:], xt[:], bb[:], ALU.add), r=[xt.k, bb.k], w=[xt.k])


def tile_to_stage(C, xt, stage, j):
    P = C.P
    for half in range(2):
        pb = psb(C)
        for c4 in range(4):
            c = half * 4 + c4
            P.op("pe", lambda e: e.transpose(pb[:, c4 * 128:(c4 + 1) * 128], xt[:, c * 128:(c + 1) * 128], C.ident[:]),
                 r=[xt.k, C.ident_tok], w=[pb.k], inc=(c4 == 3))
        dst = stage[:, half * 4:(half + 1) * 4, j * 128:(j + 1) * 128]
        src = hv(pb[:, :], 4)
        if half == 0:
            P.op("act", lambda e: e.copy(dst, src), r=[pb.k], w=[stage.k])
        else:
            P.op("dve", lambda e: e.tensor_copy(dst, src), r=[pb.k], w=[stage.k])


def phase_outproj(C, w_out, srcYT, srcYT_toks, resid, resid_toks, lng, lnb, dstH, dstH_tok, dstHT, dstHT_tok, dbgname=None):
    nc, P, T, I = C.nc, C.P, C.T, C.I
    with ExitStack() as es:
        WO = mk(P, [128, 8, D], BF16, "WO", es)
        for c in range(8):
            P.dma("pool", WO[:, c, :], w_out[c * 128:(c + 1) * 128, :], w=[WO.k])
        gb = mk(P, [128, D], F32, "lng", es)
        bb = mk(P, [128, D], F32, "lnb", es)
        bcast_load(C, gb[:], lng, 128, gb.k)
        bcast_load(C, bb[:], lnb, 128, bb.k)
        yts = [mk(P, [128, 8, 512], BF16, "yt", es) for _ in range(2)]
        xts = [mk(P, [128, D], F32, "xres", es) for _ in range(8)]
        sts = [mk(P, [128, 2], F32, "lnst", es) for _ in range(8)]
        junk = mk(P, [128, D], BF16, "junk", es)
        stg = [mk(P, [128, 8, 512], BF16, "hstg", es) for _ in range(2)]
        pend = None

        def loads(s):
            P.dma("sp", yts[s % 2][:], XTv(srcYT)[:, :, s * 512:(s + 1) * 512], r=srcYT_toks(s), w=[yts[s % 2].k])
            for j in range(4):
                i = s * 4 + j
                xt = xts[(s % 2) * 4 + j]
                P.dma("sp", xt[:], resid[i * 128:(i + 1) * 128, :], r=resid_toks(i), w=[xt.k])

        loads(0)
        for s in range(C.NS):
            yt = yts[s % 2]
            sg_ = stg[s % 2]
            X = xts[(s % 2) * 4:(s % 2) * 4 + 4]
            S = sts[(s % 2) * 4:(s % 2) * 4 + 4]
            for j in range(4):
                xt = X[j]
                for half in range(2):
                    pb = psb(C)
                    for c in range(8):
                        P.op("pe", lambda e: e.matmul(pb[:, :], lhsT=yt[:, c, j * 128:(j + 1) * 128], rhs=WO[:, c, half * 512:(half + 1) * 512], start=(c == 0), stop=(c == 7)),
                             r=[yt.k, WO.k], w=[pb.k], inc=(c == 7))
                    P.op("dve", lambda e: e.scalar_tensor_tensor(xt[:, half * 512:(half + 1) * 512], xt[:, half * 512:(half + 1) * 512], DN_ALPHA, pb[:, :], ALU.mult, ALU.add),
                         r=[xt.k, pb.k], w=[xt.k])
            if pend is not None:
                pend()
            if s + 1 < C.NS:
                loads(s + 1)
            ln_lockstep(C, X, gb, bb, S, junk)
            for j in range(4):
                i = s * 4 + j
                P.dma("sp", dstH[i * 128:(i + 1) * 128, :], X[j][:], r=[X[j].k], w=[dstH_tok[i]])

            def pend(s=s, X=X, sg_=sg_):
                for j in range(4):
                    tile_to_stage(C, X[j], sg_, j)
                P.dma("sp", XTv(dstHT)[:, :, s * 512:(s + 1) * 512], sg_[:], r=[sg_.k], w=[dstHT_tok[s]])
        pend()
        P.barrier()


def phase_moe(C, l, srcH, srcH_tok, srcHT, srcHT_tok, dstX, dstX_tok, dstXT, dstXT_tok):
    nc, P, T, I = C.nc, C.P, C.T, C.I
    mm = ALU.mult
    with ExitStack() as es:
        WD = mk(P, [128, 32, D], BF16, "WD", es)
        for c4 in range(4):
            P.dma("sp", WD[:, c4 * 8:(c4 + 1) * 8, :], C.WD16[l].rearrange("p (c d) -> p c d", c=32)[:, c4 * 8:(c4 + 1) * 8, :], r=[C.WD16_tok[l]], w=[WD.k])
        RW = mk(P, [128, 8, NE], BF16, "RW", es)
        P.dma("pool", RW[:], I["router_w"].rearrange("(c p) e -> p c e", p=128), w=[RW.k])
        rbb = mk(P, [128, NE], F32, "rbb", es)
        bcast_load(C, rbb[:], I["router_bias"], 128, rbb.k)
        SEL = mk(P, [16, 16, 128], BF16, "SEL", es)
        P.dma("pool", SEL[:], I["c_sel"], w=[SEL.k])
        gb = mk(P, [128, D], F32, "lng", es)
        bb = mk(P, [128, D], F32, "lnb", es)
        bcast_load(C, gb[:], I["ln2_g"][l:l + 1, :], 128, gb.k)
        bcast_load(C, bb[:], I["ln2_b"][l:l + 1, :], 128, bb.k)
        hts = [mk(P, [128, 8, 512], BF16, "hT", es) for _ in range(2)]
        xts = [mk(P, [128, D], F32, "hres", es) for _ in range(4)]
        sts = [mk(P, [128, 2], F32, "lnst", es) for _ in range(4)]
        junk = mk(P, [128, D], BF16, "junk", es)
        stg = [mk(P, [128, 8, 512], BF16, "xstg", es) for _ in range(2)]
        actT = mk(P, [128, 32, 512], BF16, "actT", es)
        combT = mk(P, [16, 512], BF16, "combT", es)
        WGUs = [mk(P, [128, 2, 8, DE], BF16, "WGU", es) for _ in range(3)]
        sgl = [mk(P, [128, 512], F32, "sgl", es) for _ in range(2)]
        R4 = range(4)
        s_l = [mk(P, [128, NE], F32, "rs", es) for _ in R4]
        sel_l = [mk(P, [128, NE], F32, "rsel", es) for _ in R4]
        pr_l = [mk(P, [128, 4, 6], F32, "rpr", es) for _ in R4]
        gs_l = [mk(P, [128, 4], F32, "rgs", es) for _ in R4]
        t1_l = [mk(P, [128, 4], F32, "rt1", es) for _ in R4]
        m1_l = [mk(P, [128, 2], F32, "rm1", es) for _ in R4]
        selm_l = [mk(P, [128, NE], F32, "rselm", es) for _ in R4]
        sel2_l = [mk(P, [128, NE], F32, "rsel2", es) for _ in R4]
        comb_l = [mk(P, [128, NE], F32, "rcomb", es) for _ in R4]
        nwl = [0]

        def load_w(e):
            b = nwl[0] % 3
            nwl[0] += 1
            P.dma("sp", WGUs[b][:], C.WGU16[l][e].rearrange("p (t c f) -> p t c f", t=2, c=8), r=[C.WGU16_tok[l][e]], w=[WGUs[b].k])
            return WGUs[b]

        def g4(t):
            return t[:, :].rearrange("p (g e) -> p g e", g=4)

        def load_hT(s):
            P.dma("sp", hts[s % 2][:], XTv(srcHT)[:, :, s * 512:(s + 1) * 512], r=[srcHT_tok[s]], w=[hts[s % 2].k])

        def router_front(s):
            hT = hts[s % 2]
            for j in R4:
                plg = psb(C)
                s_ = s_l[j]
                for c in range(8):
                    P.op("pe", lambda e: e.matmul(plg[:, 0:NE], lhsT=hT[:, c, j * 128:(j + 1) * 128], rhs=RW[:, c, :], start=(c == 0), stop=(c == 7)),
                         r=[hT.k, RW.k], w=[plg.k], inc=(c == 7))
                P.op("act", lambda e: e.activation(out=s_[:], in_=plg[:, 0:NE], func=AF.Sigmoid), r=[plg.k], w=[s_.k])

            def step(fn):
                for j in R4:
                    fn(s_l[j], sel_l[j], pr_l[j], gs_l[j], t1_l[j], m1_l[j], selm_l[j], sel2_l[j], comb_l[j])
            step(lambda s_, sel, pr, gs, t1, m1, selm, sel2, comb: P.op("dve", lambda e: e.tensor_tensor(sel[:], s_[:], rbb[:], ALU.add), r=[s_.k, rbb.k], w=[sel.k]))
            step(lambda s_, sel, pr, gs, t1, m1, selm, sel2, comb: P.op("dve", lambda e: e.tensor_tensor(pr[:, :, 0:3], g4(sel)[:, :, 0:3], g4(sel)[:, :, 1:4], ALU.add), r=[sel.k], w=[pr.k]))
            step(lambda s_, sel, pr, gs, t1, m1, selm, sel2, comb: P.op("dve", lambda e: e.tensor_tensor(pr[:, :, 3:5], g4(sel)[:, :, 0:2], g4(sel)[:, :, 2:4], ALU.add), r=[sel.k], w=[pr.k]))
            step(lambda s_, sel, pr, gs, t1, m1, selm, sel2, comb: P.op("dve", lambda e: e.tensor_tensor(pr[:, :, 5:6], g4(sel)[:, :, 0:1], g4(sel)[:, :, 3:4], ALU.add), r=[sel.k], w=[pr.k]))
            step(lambda s_, sel, pr, gs, t1, m1, selm, sel2, comb: P.op("dve", lambda e: e.tensor_reduce(gs[:], pr[:], AX.X, ALU.max), r=[pr.k], w=[gs.k]))
            step(lambda s_, sel, pr, gs, t1, m1, selm, sel2, comb: P.op("dve", lambda e: e.tensor_reduce(m1[:, 0:1], gs[:], AX.X, ALU.max), r=[gs.k], w=[m1.k]))
            step(lambda s_, sel, pr, gs, t1, m1, selm, sel2, comb: P.op("dve", lambda e: e.tensor_scalar(gs[:], gs[:], m1[:, 0:1], None, ALU.is_ge), r=[gs.k, m1.k], w=[gs.k]))
            step(lambda s_, sel, pr, gs, t1, m1, selm, sel2, comb: P.op("dve", lambda e: e.tensor_scalar(t1[:], gs[:], -1.0, 1e30, ALU.add, ALU.mult), r=[gs.k], w=[t1.k]))
            step(lambda s_, sel, pr, gs, t1, m1, selm, sel2, comb: P.op("dve", lambda e: e.tensor_tensor(g4(selm), g4(sel), gs[:, :].unsqueeze(2).to_broadcast([128, 4, 4]), mm), r=[sel.k, gs.k], w=[selm.k]))
            step(lambda s_, sel, pr, gs, t1, m1, selm, sel2, comb: P.op("dve", lambda e: e.tensor_tensor(g4(selm), g4(selm), t1[:, :].unsqueeze(2).to_broadcast([128, 4, 4]), ALU.add), r=[selm.k, t1.k], w=[selm.k]))
            step(lambda s_, sel, pr, gs, t1, m1, selm, sel2, comb: P.op("dve", lambda e: e.tensor_reduce(m1[:, 0:1], selm[:], AX.X, ALU.max), r=[selm.k], w=[m1.k]))
            step(lambda s_, sel, pr, gs, t1, m1, selm, sel2, comb: P.op("dve", lambda e: e.tensor_scalar(sel2[:], selm[:], m1[:, 0:1], None, ALU.is_ge), r=[selm.k, m1.k], w=[sel2.k]))
            step(lambda s_, sel, pr, gs, t1, m1, selm, sel2, comb: P.op("dve", lambda e: e.scalar_tensor_tensor(sel2[:], sel2[:], -1e30, selm[:], mm, ALU.add), r=[sel2.k, selm.k], w=[sel2.k]))
            step(lambda s_, sel, pr, gs, t1, m1, selm, sel2, comb: P.op("dve", lambda e: e.tensor_reduce(m1[:, 1:2], sel2[:], AX.X, ALU.max), r=[sel2.k], w=[m1.k]))
            step(lambda s_, sel, pr, gs, t1, m1, selm, sel2, comb: P.op("dve", lambda e: e.tensor_scalar(sel2[:], selm[:], m1[:, 1:2], None, ALU.is_ge), r=[selm.k, m1.k], w=[sel2.k]))
            step(lambda s_, sel, pr, gs, t1, m1, selm, sel2, comb: P.op("dve", lambda e: e.tensor_tensor(comb[:], s_[:], sel2[:], mm), r=[s_.k, sel2.k], w=[comb.k]))
            step(lambda s_, sel, pr, gs, t1, m1, selm, sel2, comb: P.op("dve", lambda e: e.tensor_reduce(m1[:, 0:1], comb[:], AX.X, ALU.add), r=[comb.k], w=[m1.k]))
            step(lambda s_, sel, pr, gs, t1, m1, selm, sel2, comb: P.op("dve", lambda e: e.reciprocal(m1[:, 0:1], m1[:, 0:1]), r=[m1.k], w=[m1.k]))
            step(lambda s_, sel, pr, gs, t1, m1, selm, sel2, comb: P.op("dve", lambda e: e.tensor_scalar(comb[:], comb[:], m1[:, 0:1], None, mm), r=[comb.k, m1.k], w=[comb.k]))

        def router_back(s):
            for j in R4:
                comb = comb_l[j]
                pct = psb(C)
                P.op("pe", lambda e: e.transpose(pct[0:16, 0:128], comb[:, :], C.ident[:]), r=[comb.k, C.ident_tok], w=[pct.k])
                P.op("act", lambda e: e.copy(combT[:, j * 128:(j + 1) * 128], pct[0:16, 0:128]), r=[pct.k], w=[combT.k])

        def load_x(s):
            for j in R4:
                i = s * 4 + j
                P.dma("sp", xts[j][:], srcH[i * 128:(i + 1) * 128, :], r=[srcH_tok[i]], w=[xts[j].k])

        def make_pend(s):
            xs_ = stg[s % 2]

            def pend():
                for j in R4:
                    tile_to_stage(C, xts[j], xs_, j)
                P.dma("sp", XTv(dstXT)[:, :, s * 512:(s + 1) * 512], xs_[:], r=[xs_.k], w=[dstXT_tok[s]])
            return pend

        pend = None
        pre = []
        load_hT(0)
        router_front(0)
        router_back(0)
        for s in range(C.NS):
            hT = hts[s % 2]
            if s + 1 < C.NS:
                load_hT(s + 1)
            for ex in range(NE):
                WGU = pre.pop(0) if pre else load_w(ex)
                pcb = psb(C)
                P.op("pe", lambda e: e.matmul(pcb[:, :], lhsT=SEL[:, ex, :], rhs=combT[:, :], start=True, stop=True), r=[SEL.k, combT.k], w=[pcb.k])
                for f in range(2):
                    pG, pU = psb(C), psb(C)
                    for pb, ti in ((pG, 0), (pU, 1)):
                        for c in range(8):
                            P.op("pe", lambda e: e.matmul(pb[:, :], lhsT=WGU[:, ti, c, f * 128:(f + 1) * 128], rhs=hT[:, c, :], start=(c == 0), stop=(c == 7)),
                                 r=[WGU.k, hT.k], w=[pb.k], inc=(c == 7))
                    sg_ = sgl[(ex * 2 + f) % 2]
                    P.op("act", lambda e: e.activation(out=sg_[:], in_=pG[:, :], func=AF.Silu), r=[pG.k], w=[sg_.k])
                    P.op("dve", lambda e: e.tensor_tensor(sg_[:], sg_[:], pU[:, :], mm), r=[sg_.k, pU.k], w=[sg_.k])
                    P.op("dve", lambda e: e.tensor_tensor(actT[:, ex * 2 + f, :], sg_[:], pcb[:, :], mm), r=[sg_.k, pcb.k], w=[actT.k])
                if ex == 3:
                    if pend is not None:
                        pend()
                        pend = None
                    load_x(s)
            if s + 1 < C.NS:
                pre.extend(load_w(e_) for e_ in range(3))
            if s + 1 < C.NS:
                router_front(s + 1)
            for j in R4:
                xt = xts[j]
                for half in range(2):
                    pb = psb(C)
                    for c in range(32):
                        P.op("pe", lambda e: e.matmul(pb[:, :], lhsT=actT[:, c, j * 128:(j + 1) * 128], rhs=WD[:, c, half * 512:(half + 1) * 512], start=(c == 0), stop=(c == 31)),
                             r=[actT.k, WD.k], w=[pb.k], inc=(c == 31))
                    P.op("dve", lambda e: e.scalar_tensor_tensor(xt[:, half * 512:(half + 1) * 512], xt[:, half * 512:(half + 1) * 512], DN_ALPHA, pb[:, :], ALU.mult, ALU.add),
                         r=[xt.k, pb.k], w=[xt.k])
            if s + 1 < C.NS:
                router_back(s + 1)
            ln_lockstep(C, xts, gb, bb, sts, junk)
            for j in R4:
                i = s * 4 + j
                P.dma("sp", dstX[i * 128:(i + 1) * 128, :], xts[j][:], r=[xts[j].k], w=[dstX_tok[i]])
            if dstXT is not None:
                pend = make_pend(s)
        if pend is not None:
            pend()
        P.barrier()


def rope_consts(T):
    pos = np.arange(T, dtype=np.float64)
    c = {}
    for name, half in (("k", 64), ("i", 32)):
        inv = 10000.0 ** (-np.arange(half, dtype=np.float64) / half)
        ang = (pos.astype(np.float32)[:, None] * inv.astype(np.float32)[None, :]).astype(np.float32).astype(np.float64)
        cs = np.cos(ang).astype(np.float32).reshape(T // 128, 128, half).transpose(1, 0, 2)
        sn = np.sin(ang).astype(np.float32).reshape(T // 128, 128, half).transpose(1, 0, 2)
        c["cos_" + name] = np.ascontiguousarray(cs)
        c["sin_" + name] = np.ascontiguousarray(sn)
    q = np.arange(128)[:, None]
    s = np.arange(128)[None, :]
    c["cbias"] = np.where(s <= q, 0.0, -1e30).astype(np.float32)
    c["halfpow"] = (0.5 ** np.arange(1, 33, dtype=np.float64)).astype(np.float32).reshape(1, 32)
    c["tiebias"] = (-1e-6 * np.arange(T, dtype=np.float64)).astype(np.float32).reshape(1, T)
    return c


def rope_tm(C, dst_ap, dst_k, src, src_k, cosb, sinb, nh, half, ta_ap, ta_k, tb_ap, tb_k, rdeps):
    P = C.P
    mm = ALU.mult
    n = nh * 2
    s3 = src.rearrange("p (n f) -> p n f", n=n)
    cb = cosb.unsqueeze(1).to_broadcast([128, n, half])
    sb_ = sinb.unsqueeze(1).to_broadcast([128, n, half])
    a3 = ta_ap.rearrange("p (n f) -> p n f", n=n)
    b3 = tb_ap.rearrange("p (n f) -> p n f", n=n)
    P.op("dve", lambda e: e.tensor_tensor(a3, s3, cb, mm), r=[src_k] + rdeps, w=[ta_k])
    P.op("dve", lambda e: e.tensor_tensor(b3, s3, sb_, mm), r=[src_k] + rdeps, w=[tb_k])
    a4 = ta_ap.rearrange("p (h t f) -> p h t f", h=nh, t=2)
    b4 = tb_ap.rearrange("p (h t f) -> p h t f", h=nh, t=2)
    d4 = dst_ap.rearrange("p (h t f) -> p h t f", h=nh, t=2)
    P.op("pool", lambda e: e.tensor_tensor(d4[:, :, 0, :], a4[:, :, 0, :], b4[:, :, 1, :], ALU.subtract), r=[ta_k, tb_k], w=[dst_k])
    P.op("pool", lambda e: e.tensor_tensor(d4[:, :, 1, :], a4[:, :, 1, :], b4[:, :, 0, :], ALU.add), r=[ta_k, tb_k], w=[dst_k])


def phase_dsa(C):
    nc, P, T, I = C.nc, C.P, C.T, C.I
    mm = ALU.mult
    KT = min(256, T // 4)
    NIT = 25
    SCALE = 128 ** -0.5
    C.bank_pool = [0, 1, 2, 3, 4]
    with ExitStack() as es:
        def wt(name, shape, dt=F32):
            return mk(P, list(shape), dt, name, es)
        WQ = wt("WQ", (128, 8, 1024), BF16)
        WR = wt("WR", (128, 8, 580), BF16)
        for c in range(8):
            P.dma("pool", WQ[:, c, :], I["w_in_odd"][c * 128:(c + 1) * 128, 0:1024], w=[WQ.k])
            P.dma("pool", WR[:, c, :], I["w_in_odd"][c * 128:(c + 1) * 128, 1024:1604], w=[WR.k])
        RTs = [[wt("CK", (128, 4, 64)), wt("SK", (128, 4, 64)), wt("CI", (128, 4, 32)), wt("SI", (128, 4, 32))] for _ in range(2)]

        def load_tables(s):
            tl = RTs[s % 2]
            for t_, nm in zip(tl, ("c_cos_k", "c_sin_k", "c_cos_i", "c_sin_i")):
                P.dma("sp", t_[:], I[nm][:, s * 4:(s + 1) * 4, :], w=[t_.k])
            return tl
        BIAS = wt("BIAS", (128, T))
        bcast_load(C, BIAS[:], I["c_tiebias"], 128, BIAS.k)
        CB = wt("CB", (128, 128))
        P.dma("sp", CB[:], I["c_cbias"], w=[CB.k])
        ikg, ikb = wt("ikg", (128, 64)), wt("ikb", (128, 64))
        bcast_load(C, ikg[:], I["c_ik_ln_g"], 128, ikg.k)
        bcast_load(C, ikb[:], I["c_ik_ln_b"], 128, ikb.k)
        kT = wt("kT", (128, T), BF16)
        ikT = wt("ikT", (128, T), BF16)
        Vx = wt("Vx", (128, C.NT, 129), BF16)
        P.op("dve", lambda e: e.memset(Vx[:, :, 128:129], 1.0), w=[Vx.k])
        xTs = [wt("xTd", (128, 8, 512), BF16) for _ in range(2)]
        ta, tb = wt("ropeA", (128, 512)), wt("ropeB", (128, 512))
        kr = wt("kr", (128, 128))
        ikr = wt("ikr", (128, 128))
        st2 = wt("st2", (128, 2))
        for s in range(C.NS):
            xT = xTs[s % 2]
            P.dma("sp", xT[:], XTv(C.XT1)[:, :, s * 512:(s + 1) * 512], r=[C.XT1_tok[s]], w=[xT.k])
            CK, SK, CI, SI = load_tables(s)
            for j in range(4):
                i = s * 4 + j
                pb = psb(C)
                for c in range(8):
                    P.op("pe", lambda e: e.matmul(pb[:, 0:256], lhsT=xT[:, c, j * 128:(j + 1) * 128], rhs=WR[:, c, 0:256], start=(c == 0), stop=(c == 7)),
                         r=[xT.k, WR.k], w=[pb.k], inc=False)
                for c in range(8):
                    P.op("pe", lambda e: e.matmul(pb[:, 256:320], lhsT=xT[:, c, j * 128:(j + 1) * 128], rhs=WR[:, c, 512:576], start=(c == 0), stop=(c == 7)),
                         r=[xT.k, WR.k], w=[pb.k], inc=(c == 7))
                rope_tm(C, kr[:, :], kr.k, pb[:, 0:128], pb.k, CK[:, j, :], SK[:, j, :], 1, 64, ta[:, 0:128], ta.k, tb[:, 0:128], tb.k, [CK.k, SK.k])
                P.op("act", lambda e: e.copy(Vx[:, i, 0:128], pb[:, 128:256]), r=[pb.k], w=[Vx.k])
                P.op("dve", lambda e: e.tensor_reduce(st2[:, 0:1], pb[:, 256:320], AX.X, ALU.add), r=[pb.k], w=[st2.k])
                P.op("dve", lambda e: e.tensor_scalar(st2[:, 0:1], st2[:, 0:1], 1.0 / 64, None, mm), r=[st2.k], w=[st2.k])
                P.op("dve", lambda e: e.tensor_scalar(ikr[:, 0:64], pb[:, 256:320], st2[:, 0:1], None, ALU.subtract), r=[pb.k, st2.k], w=[ikr.k])
                P.op("act", lambda e: e.activation(out=ikr[:, 64:128], in_=ikr[:, 0:64], func=AF.Square, accum_out=st2[:, 1:2]), r=[ikr.k], w=[ikr.k, st2.k])
                P.op("act", lambda e: e.activation(out=st2[:, 1:2], in_=st2[:, 1:2], func=AF.Sqrt, bias=LN_EPS, scale=1.0 / 64), r=[st2.k], w=[st2.k])
                P.op("dve", lambda e: e.reciprocal(st2[:, 1:2], st2[:, 1:2]), r=[st2.k], w=[st2.k])
                P.op("dve", lambda e: e.scalar_tensor_tensor(ikr[:, 0:64], ikr[:, 0:64], st2[:, 1:2], ikg[:], mm, mm), r=[ikr.k, st2.k, ikg.k], w=[ikr.k])
                P.op("dve", lambda e: e.tensor_tensor(ikr[:, 64:128], ikr[:, 0:64], ikb[:], ALU.add), r=[ikr.k, ikb.k], w=[ikr.k])
                ikn = TL(ikr.t, ikr.k)
                rope_src = ikr[:, 64:128]
                n = 2
                s3 = rope_src.rearrange("p (n f) -> p n f", n=n)
                cb = CI[:, j, :].unsqueeze(1).to_broadcast([128, n, 32])
                sb_ = SI[:, j, :].unsqueeze(1).to_broadcast([128, n, 32])
                a3 = ta[:, 0:64].rearrange("p (n f) -> p n f", n=n)
                b3 = tb[:, 0:64].rearrange("p (n f) -> p n f", n=n)
                P.op("dve", lambda e: e.tensor_tensor(a3, s3, cb, mm), r=[ikr.k, CI.k], w=[ta.k])
                P.op("dve", lambda e: e.tensor_tensor(b3, s3, sb_, mm), r=[ikr.k, SI.k], w=[tb.k])
                P.op("pool", lambda e: e.tensor_tensor(ikr[:, 0:32], ta[:, 0:32], tb[:, 32:64], ALU.subtract), r=[ta.k, tb.k], w=[ikr.k])
                P.op("pool", lambda e: e.tensor_tensor(ikr[:, 32:64], ta[:, 32:64], tb[:, 0:32], ALU.add), r=[ta.k, tb.k], w=[ikr.k])
                P.op("pool", lambda e: e.tensor_copy(ikr[:, 64:128], ikr[:, 0:64]), r=[ikr.k], w=[ikr.k])
                pt = psb(C)
                P.op("pe", lambda e: e.transpose(pt[:, 0:128], kr[:, :], C.ident[:]), r=[kr.k, C.ident_tok], w=[pt.k], inc=False)
                P.op("pe", lambda e: e.transpose(pt[:, 128:256], ikr[:, :], C.ident[:]), r=[ikr.k, C.ident_tok], w=[pt.k])
                P.op("act", lambda e: e.copy(kT[:, i * 128:(i + 1) * 128], pt[:, 0:128]), r=[pt.k], w=[kT.k])
                P.op("act", lambda e: e.mul(ikT[:, i * 128:(i + 1) * 128], pt[:, 128:256], 0.125), r=[pt.k], w=[ikT.k])
        SC = wt("SC", (128, T))
        MASKs = [wt("MASK", (128, T)) for _ in range(2)]
        junk = wt("junkd", (128, T), BF16)
        qr = wt("qr", (128, 1024))
        iqr = wt("iqr", (128, 256))
        qTs = [wt("qT", (128, 8, 128), BF16) for _ in range(3)]
        iqT = wt("iqT", (128, 2, 128), BF16)
        iws = wt("iws", (128, 4))
        rl = [wt("rl%d" % n, (128, 512)) for n in range(2)]
        bs = wt("bs", (128, 8))
        Dk = wt("Dk", (128, NIT))
        HK = wt("HK", (128, NIT))
        bcast_load(C, HK[:], I["c_halfpow"][0:1, 0:NIT], 128, HK.k)
        V2 = [wt("V2_%d" % n, (128, 2)) for n in range(2)]
        W2 = wt("W2s", (128, 2))
        Ek = wt("Ek", (128, NIT, 2))
        P.op("dve", lambda e: e.memset(Ek[:], 0.0), w=[Ek.k])
        mT4 = [wt("mT4_%d" % n, (128, 4, 128), BF16) for n in range(2)]
        pTs = [wt("pT%d" % n, (128, 4, 128), BF16) for n in range(4)]
        o_ = wt("od", (128, 1024))
        rs8 = wt("rs8", (128, 8))
        ostg = [wt("ostg", (128, 8, 512), BF16) for _ in range(2)]
        accb = [TL(*C.banks[b]) for b in (5, 6, 7)]
        MBIG = 30000.0
        identb = wt("identb", (128, 128), BF16)
        P.op("dve", lambda e: e.tensor_copy(identb[:], C.ident[:]), r=[C.ident_tok], w=[identb.k])
        acc_of = [(0, 0), (0, 1), (0, 2), (1, 0), (1, 1), (1, 2), (2, 0), (2, 1)]
        def qproj(s, j):
            i = s * 4 + j
            xT = xTs[s % 2]
            qT = qTs[i % 3]
            if j == 0:
                P.dma("sp", xT[:], XTv(C.XT1)[:, :, s * 512:(s + 1) * 512], r=[C.XT1_tok[s]], w=[xT.k])
                load_tables(s)
            CK, SK, CI, SI = RTs[s % 2]
            pq = [psb(C), psb(C)]
            for half in range(2):
                for c in range(8):
                    P.op("pe", lambda e: e.matmul(pq[half][:, :], lhsT=xT[:, c, j * 128:(j + 1) * 128], rhs=WQ[:, c, half * 512:(half + 1) * 512], start=(c == 0), stop=(c == 7)),
                         r=[xT.k, WQ.k], w=[pq[half].k], inc=(c == 7))
            piq = psb(C)
            for c in range(8):
                P.op("pe", lambda e: e.matmul(piq[:, 0:256], lhsT=xT[:, c, j * 128:(j + 1) * 128], rhs=WR[:, c, 256:512], start=(c == 0), stop=(c == 7)),
                     r=[xT.k, WR.k], w=[piq.k], inc=False)
            for c in range(8):
                P.op("pe", lambda e: e.matmul(piq[:, 256:260], lhsT=xT[:, c, j * 128:(j + 1) * 128], rhs=WR[:, c, 576:580], start=(c == 0), stop=(c == 7)),
                     r=[xT.k, WR.k], w=[piq.k], inc=(c == 7))
            for half in range(2):
                hs = slice(half * 512, (half + 1) * 512)
                rope_tm(C, qr[:, hs], qr.k, pq[half][:, :], pq[half].k, CK[:, j, :], SK[:, j, :], 4, 64,
                        ta[:, 0:512], ta.k, tb[:, 0:512], tb.k, [CK.k, SK.k])
            rope_tm(C, iqr[:, :], iqr.k, piq[:, 0:256], piq.k, CI[:, j, :], SI[:, j, :], 4, 32, ta[:, 0:256], ta.k, tb[:, 0:256], tb.k, [CI.k, SI.k])
            P.op("act", lambda e: e.mul(iws[:], piq[:, 256:260], 0.5), r=[piq.k], w=[iws.k])
            for half in range(2):
                pb = psb(C)
                for c4 in range(4):
                    h = half * 4 + c4
                    P.op("pe", lambda e: e.transpose(pb[:, c4 * 128:(c4 + 1) * 128], qr[:, h * 128:(h + 1) * 128], C.ident[:]), r=[qr.k, C.ident_tok], w=[pb.k], inc=(c4 == 3))
                P.op("act", lambda e: e.copy(qT[:, half * 4:(half + 1) * 4, :], hv(pb[:, :], 4)), r=[pb.k], w=[qT.k])
            pb = psb(C)
            for c2 in range(2):
                P.op("pe", lambda e: e.transpose(pb[:, c2 * 128:(c2 + 1) * 128], iqr[:, c2 * 128:(c2 + 1) * 128], C.ident[:]), r=[iqr.k, C.ident_tok], w=[pb.k], inc=(c2 == 1))
            P.op("act", lambda e: e.copy(iqT[:], hv(pb[:, 0:256], 2)), r=[pb.k], w=[iqT.k])

        NB = C.NS * 4

        def qproj_next(i):
            if i + 1 < NB:
                qproj((i + 1) // 4, (i + 1) % 4)

        def qblock(s, j):
            if True:
                i = s * 4 + j
                L = (i + 1) * 128
                og = ostg[s % 2]
                qT = qTs[i % 3]
                MASK = MASKs[i % 2]
                cast_some(C, 1, 2)
                if i == 0:
                    qproj(0, 0)
                yield "F"
                for k0 in range(0, L, 512):
                    kw = min(512, L - k0)
                    for h in range(4):
                        ph = psb(C)
                        pl = (h % 2) * 64
                        P.op("pe", lambda e: e.matmul(ph[:, 0:kw], lhsT=iqT[pl:pl + 64, h // 2, :], rhs=ikT[pl:pl + 64, k0:k0 + kw], start=True, stop=True),
                             r=[iqT.k, ikT.k], w=[ph.k])
                        r_ = rl[h % 2]
                        P.op("act", lambda e: e.activation(out=r_[:, 0:kw], in_=ph[:, 0:kw], func=AF.Relu), r=[ph.k], w=[r_.k])
                        if h == 0:
                            P.op("dve", lambda e: e.scalar_tensor_tensor(SC[:, k0:k0 + kw], r_[:, 0:kw], iws[:, 0:1], BIAS[:, k0:k0 + kw], mm, ALU.add), r=[r_.k, iws.k, BIAS.k], w=[SC.k])
                        else:
                            P.op("dve", lambda e: e.scalar_tensor_tensor(SC[:, k0:k0 + kw], r_[:, 0:kw], iws[:, h:h + 1], SC[:, k0:k0 + kw], mm, ALU.add),
                                 r=[r_.k, iws.k, SC.k], w=[SC.k])
                    yield "F"
                if L > KT:
                    P.op("dve", lambda e: e.tensor_reduce(bs[:, 1:2], SC[:, 0:L], AX.X, ALU.max, apply_absolute_value=True), r=[SC.k], w=[bs.k])
                    P.op("dve", lambda e: e.tensor_scalar(bs[:, 0:1], bs[:, 1:2], -1.0, -1.0, mm, ALU.add), r=[bs.k], w=[bs.k])
                    P.op("dve", lambda e: e.tensor_scalar(bs[:, 1:2], bs[:, 1:2], 2.0, 2.0, mm, ALU.add), r=[bs.k], w=[bs.k])
                    P.op("dve", lambda e: e.tensor_scalar(Dk[:], HK[:], bs[:, 1:2], None, mm), r=[bs.k, HK.k], w=[Dk.k])
                P.op("pool", lambda e: e.tensor_tensor(SC[:, i * 128:L], SC[:, i * 128:L], CB[:], ALU.add), r=[SC.k, CB.k], w=[SC.k])
                if L > KT:
                    P.op("dve", lambda e: e.tensor_copy(Ek[:, 0:NIT - 1, 1:2], Dk[:, 1:NIT].unsqueeze(2)), r=[Dk.k], w=[Ek.k])
                    P.op("dve", lambda e: e.tensor_copy(V2[0][:, 0:1], bs[:, 0:1]), r=[bs.k], w=[V2[0].k])
                    P.op("dve", lambda e: e.tensor_tensor(V2[0][:, 1:2], bs[:, 0:1], Dk[:, 0:1], ALU.add), r=[bs.k, Dk.k], w=[V2[0].k])
                    for it in range(NIT):
                        Vc, Vn = V2[it % 2], V2[(it + 1) % 2]
                        P.op("dve", lambda e: e.tensor_scalar(junk[:, 0:L], SC[:, 0:L], Vc[:, 1:2], None, ALU.is_gt, ALU.add, accum_out=bs[:, 3:4]),
                             r=[SC.k, Vc.k], w=[junk.k, bs.k])
                        P.op("dve", lambda e: e.scalar_tensor_tensor(W2[:], bs[:, 3:4].to_broadcast([128, 2]), float(KT) - 0.5, Dk[:, it:it + 1].to_broadcast([128, 2]), ALU.is_gt, mm),
                             r=[bs.k, Dk.k], w=[W2.k])
                        P.op("dve", lambda e: e.scalar_tensor_tensor(Vn[:], W2[:], Vc[:, 0:1], Ek[:, it, :], ALU.add, ALU.add), r=[W2.k, Vc.k, Ek.k], w=[Vn.k])
                        if it == 4:
                            qproj_next(i)
                        yield "F"
                    Vf = V2[NIT % 2]
                    P.op("dve", lambda e: e.tensor_scalar(MASK[:, 0:L], SC[:, 0:L], Vf[:, 0:1], None, ALU.is_le), r=[SC.k, Vf.k], w=[MASK.k])
                else:
                    P.op("dve", lambda e: e.tensor_scalar(MASK[:, 0:L], SC[:, 0:L], -1e29, None, ALU.is_le), r=[SC.k], w=[MASK.k])
                    qproj_next(i)
                if "dbg_mask" in C.dbg:
                    P.dma("sp", C.dbg["dbg_mask"][i * 128:(i + 1) * 128, 0:L], MASK[:, 0:L], r=[MASK.k])
                    P.dma("sp", C.dbg["dbg_sc"][i * 128:(i + 1) * 128, 0:L], SC[:, 0:L], r=[SC.k])
                yield "END_FRONT"
                units = [(st, hg) for st in range(i + 1) for hg in range(2)]

                def stage1(st, hg):
                    if hg == 0 and st % 4 == 0:
                        n4 = min(4, i + 1 - st)
                        m4 = mT4[(st // 4) % 2]
                        pb = psb(C)
                        for u in range(n4):
                            P.op("pe", lambda e: e.transpose(pb[:, u * 128:(u + 1) * 128], MASK[:, (st + u) * 128:(st + u + 1) * 128], C.ident[:]),
                                 r=[MASK.k, C.ident_tok], w=[pb.k], inc=(u == n4 - 1))
                        P.op("act", lambda e: e.mul(m4[:, 0:n4, :], hv(pb[:, :], 4)[:, 0:n4, :], -MBIG), r=[pb.k], w=[m4.k])
                    m4 = mT4[(st // 4) % 2]
                    pl_ = psb(C)
                    P.op("pe", lambda e: e.matmul(pl_[:, :], lhsT=identb[:, :], rhs=m4[:, st % 4, :].unsqueeze(1).to_broadcast([128, 4, 128]), start=True, stop=False),
                         r=[identb.k, m4.k], w=[pl_.k], inc=False)
                    P.op("pe", lambda e: e.matmul(pl_[:, :], lhsT=kT[:, st * 128:(st + 1) * 128], rhs=qT[:, hg * 4:(hg + 1) * 4, :].rearrange("p h q -> p (h q)"), start=False, stop=True),
                         r=[kT.k, qT.k], w=[pl_.k])
                    pT = pTs[hg * 2 + st % 2]
                    P.op("act", lambda e: e.activation(out=pT[:], in_=hv(pl_[:, :], 4), func=AF.Exp, scale=SCALE), r=[pl_.k], w=[pT.k])

                def stage2(st, hg):
                    pT = pTs[hg * 2 + st % 2]
                    for hh in range(4):
                        h = hg * 4 + hh
                        ab, slot = acc_of[h]
                        P.op("pe", lambda e: e.matmul(accb[ab][:, slot * 129:(slot + 1) * 129], lhsT=pT[:, hh, :], rhs=Vx[:, st, :], start=(st == 0 and slot == 0), stop=(st == i), skip_group_check=True),
                             r=[pT.k, Vx.k], w=[accb[ab].k], inc=(hh == 3))

                stage1(*units[0])
                for n in range(len(units)):
                    if n + 1 < len(units):
                        stage1(*units[n + 1])
                    stage2(*units[n])
                    if units[n][1] == 1:
                        yield "B"
                for h in range(8):
                    ab, slot = acc_of[h]
                    P.op("dve", lambda e: e.reciprocal(rs8[:, h:h + 1], accb[ab][:, slot * 129 + 128:slot * 129 + 129]), r=[accb[ab].k], w=[rs8.k])
                    P.op("act", lambda e: e.activation(out=o_[:, h * 128:(h + 1) * 128], in_=accb[ab][:, slot * 129:slot * 129 + 128], func=AF.Copy, scale=rs8[:, h:h + 1]),
                         r=[accb[ab].k, rs8.k], w=[o_.k])
                if "dbg_dsa" in C.dbg:
                    P.dma("sp", C.dbg["dbg_dsa"][i * 128:(i + 1) * 128, :], o_[:], r=[o_.k])
                tile_to_stage(C, o_, og, j)
                if j == 3:
                    P.dma("sp", XTv(C.YT)[:, :, s * 512:(s + 1) * 512], og[:], r=[og.k], w=[C.YT_tok[0][s], C.YT_tok[1][s]])

        pipeline2([(lambda s=s, j=j: qblock(s, j)) for s in range(C.NS) for j in range(4)], interleave=C.dsa_interleave)
        P.barrier()
    C.bank_pool = list(range(8))


class _View:
    def __init__(self, tl, sl):
        self.t = _Sl(tl.t, sl)
        self.k = tl.k

    def __getitem__(self, idx):
        return self.t[idx]


class _Sl:
    def __init__(self, t, sl):
        self.base = t
        self.sl = sl

    def __getitem__(self, idx):
        rows, cols = idx
        assert cols == slice(None)
        return self.base[rows, self.sl]


_NC_CACHE = {}


def _in_map(inputs, b, T, consts):
    m = {"x": np.ascontiguousarray(inputs["x"][b, :T], dtype=np.float32)}
    for k, a in inputs.items():
        if k == "x":
            continue
        a = np.asarray(a, dtype=np.float32)
        if k in ("router_w", "exp_w_gate", "exp_w_up", "exp_w_down", "ln1_g", "ln1_b", "ln2_g", "ln2_b"):
            m[k] = np.ascontiguousarray(a)
        elif k == "router_bias":
            m[k] = np.ascontiguousarray(a.reshape(1, -1))
        elif k == "a_r_k":
            m[k] = np.ascontiguousarray(a.reshape(1, 512))
        elif a.ndim == 3:
            m[k] = np.ascontiguousarray(a[0])
        elif a.ndim == 2:
            m[k] = np.ascontiguousarray(a[0:1])
    m.update(consts)
    return m


def kernel(**inputs):
    x = np.asarray(inputs["x"])
    B, T, _ = x.shape
    if T not in _NC_CACHE:
        _NC_CACHE[T] = build(T)
    nc = _NC_CACHE[T]
    consts = {"c_" + k: v for k, v in host_consts(T).items()}
    consts.update({"c_" + k: v for k, v in rope_consts(T).items()})
    in_maps = [_in_map(inputs, b, T, consts) for b in range(B)]
    res = run_bass_kernel_spmd(nc, in_maps, core_ids=list(range(B)))
    out = np.stack([np.asarray(res.results[b]["out"], dtype=np.float32) for b in range(B)], 0)
    return out
```

```python
import numpy as np
import ml_dtypes
from contextlib import ExitStack
import concourse.bass as bass
import concourse.mybir as mybir
from concourse.bass_utils import run_bass_kernel_spmd

F32 = mybir.dt.float32
BF16 = mybir.dt.bfloat16
AF = mybir.ActivationFunctionType
ALU = mybir.AluOpType
AX = mybir.AxisListType

D = 1024
A_COLS = 1792
B_COLS = 1552
EVEN_COLS = 3344
ODD_COLS = 1604
NE = 16
DE = 256
DN_ALPHA = 4 ** 0.25
LN_EPS = 1e-5
DEC = 0.6065306597126334
DSA_INTERLEAVE = True
RWKV_INTERLEAVE = False


class Tok:
    __slots__ = ("w", "r")

    def __init__(self):
        self.w = None
        self.r = {}


class Eng:
    def __init__(self, name, h, sem):
        self.name = name
        self.h = h
        self.sem = sem
        self.cnt = 0
        self.waited = {}


class Prog:
    NSLOT = 8

    def __init__(self, nc, es):
        self.nc = nc
        self.es = es
        self.E = {}
        for name, h in (("pe", nc.tensor), ("act", nc.scalar), ("dve", nc.vector),
                        ("pool", nc.gpsimd), ("sp", nc.sync)):
            sem = es.enter_context(nc.semaphore("sem_" + name))
            self.E[name] = Eng(name, h, sem)
        self.slots = {}
        self.dn = {}
        for q in ("sp", "pool", "act"):
            self.slots[q] = [[es.enter_context(nc.semaphore("dq_%s%d" % (q, i))), 0] for i in range(self.NSLOT)]
            self.dn[q] = 0
        self.nalloc = 0

    def sb(self, shape, dt=F32, name=None, es=None):
        self.nalloc += 1
        t = (es or self.es).enter_context(self.nc.sbuf_tensor("%s_%d" % (name or "t", self.nalloc), list(shape), dt))
        return t

    def _wait(self, eng, ev):
        sem, val = ev
        key = sem.num
        if eng.waited.get(key, 0) >= val:
            return
        eng.h.wait_ge(sem, val)
        eng.waited[key] = val

    def _deps(self, en, r, w):
        eng = self.E[en]
        for t in r:
            if t.w is not None:
                yield t.w
        for t in w:
            if t.w is not None:
                yield t.w
            for ev in t.r.values():
                yield ev

    def op(self, en, fn, r=(), w=(), inc=True):
        eng = self.E[en]
        for ev in list(self._deps(en, r, w)):
            if en == "pe" and ev[0] is eng.sem:
                continue
            self._wait(eng, ev)
        ins = fn(eng.h)
        myev = (eng.sem, eng.cnt + 1)
        if inc:
            ins.then_inc(eng.sem, 1)
            eng.cnt += 1
        for t in r:
            t.r[en] = myev
        for t in w:
            t.w = myev
            t.r = {}
        return ins

    def dma(self, qn, out, in_, r=(), w=(), **kw):
        q = self.E[qn]
        for ev in list(self._deps(qn, r, w)):
            self._wait(q, ev)
        slot = self.slots[qn][self.dn[qn] % self.NSLOT]
        self.dn[qn] += 1
        if slot[1] > 0:
            self._wait(q, (slot[0], slot[1]))
        ins = q.h.dma_start(out=out, in_=in_, **kw)
        slot[1] += 16
        ins.then_inc(slot[0], 16)
        ev = (slot[0], slot[1])
        key = "d%d" % slot[0].num
        for t in r:
            t.r[key] = ev
        for t in w:
            t.w = ev
            t.r = {}

    def barrier(self):
        evs = []
        for q in self.slots:
            for sem, val in self.slots[q]:
                if val > 0:
                    evs.append((sem, val))
        for name, e in self.E.items():
            if e.cnt > 0:
                evs.append((e.sem, e.cnt))
        for name, e in self.E.items():
            for ev in evs:
                if ev[0] is e.sem and name == "pe":
                    continue
                self._wait(e, ev)

    def finish(self):
        sp = self.E["sp"]
        for q in self.slots:
            for sem, val in self.slots[q]:
                if val > 0:
                    self._wait(sp, (sem, val))
        for name, e in self.E.items():
            if name != "sp" and e.cnt > 0:
                self._wait(sp, (e.sem, e.cnt))


def host_consts(T):
    c = {}
    c["ident"] = np.eye(128, dtype=np.float32)
    j = np.arange(64)[:, None]
    i = np.arange(64)[None, :]
    c["tri64"] = np.stack([(-DEC) * (j <= i), (-DEC) * (j < i), (-DEC) * (j > i)], 1).astype(np.float32)
    c["ncol64"] = np.full((64, 1), -DEC, np.float32)
    su = (j < i).astype(np.float32)
    iu = (j <= i).astype(np.float32)
    sl = (j > i).astype(np.float32)
    mMA = np.concatenate([-su, iu], 1)
    mBB = np.concatenate([su, iu], 1)
    c["mMA"] = np.tile(mMA[:, None, :], (1, 8, 1)).astype(np.float32)
    c["mBB"] = np.tile(mBB[:, None, :], (1, 8, 1)).astype(np.float32)
    c["mNT"] = np.tile((-sl)[:, None, :], (1, 8, 1)).astype(np.float32)
    c["id8"] = np.tile(np.eye(64, dtype=np.float32)[:, None, :], (1, 8, 1))
    j = np.arange(128)[:, None]
    i = np.arange(128)[None, :]
    c["tri128"] = np.stack([(-1 / 16) * (j <= i), (-1 / 16) * (j > i)], 1).astype(np.float32)
    c["ncol128"] = np.full((128, 1), -1 / 16, np.float32)
    c["sel"] = (np.arange(16)[:, None, None] == np.arange(16)[None, :, None]).astype(np.float32) * np.ones((1, 1, 128), np.float32)
    c["iu128"] = np.tile((j <= i).astype(np.float32)[:, None, :], (1, 4, 1))
    return c


class Ctx:
    pass


def build(T, dbg=(), stages=("A", "R", "G", "O0", "M0", "S1", "O1", "M1")):
    nc = bass.Bass("TRN2", target_bir_lowering=False)
    es = ExitStack()
    P = Prog(nc, es)
    NT = T // 128
    NS = T // 512
    C = Ctx()
    C.nc, C.P, C.T, C.NT, C.NS = nc, P, T, NT, NS
    C.dbg = {}
    C.dsa_interleave = DSA_INTERLEAVE

    def din(name, shape, dt=F32):
        return nc.dram_tensor(name, list(shape), dt, kind="ExternalInput").ap()

    def dscr(name, shape, dt=F32, out=False):
        kind = "ExternalOutput" if (out or name in dbg) else "Internal"
        return nc.dram_tensor(name, list(shape), dt, kind=kind).ap()

    I = {}
    I["x"] = din("x", [T, D])
    for name, shape in (("w_in_even", [D, EVEN_COLS]), ("a_mu", [1, A_COLS]), ("a_w0", [1, 512]), ("a_w2", [64, 512]),
                        ("a_a0", [1, 512]), ("a_a2", [64, 512]), ("a_g2", [128, 512]), ("a_kk_scale", [1, 512]),
                        ("a_ka_scale", [1, 512]), ("a_r_k", [1, 512]), ("a_gn_g", [1, 512]), ("a_gn_b", [1, 512]),
                        ("b_gate_w2", [16, 256]), ("b_gate_b", [1, 256]), ("b_norm_g", [1, 512]),
                        ("w_out_even", [D, D]), ("w_in_odd", [D, ODD_COLS]), ("c_ik_ln_g", [1, 64]),
                        ("c_ik_ln_b", [1, 64]), ("w_out_odd", [D, D]), ("ln1_g", [2, D]), ("ln1_b", [2, D]),
                        ("ln2_g", [2, D]), ("ln2_b", [2, D]), ("router_w", [D, NE]), ("router_bias", [1, NE]),
                        ("exp_w_gate", [2, NE, D, DE]), ("exp_w_up", [2, NE, D, DE]), ("exp_w_down", [2, NE, DE, D])):
        I[name] = din(name, shape)
    hc = host_consts(T)
    hc.update(rope_consts(T))
    for k, v in hc.items():
        I["c_" + k] = din("c_" + k, list(v.shape), F32 if v.dtype == np.float32 else BF16)
    C.I = I
    out = dscr("out", [T, D], out=True)
    C.XT0 = dscr("XT0", [D, T + 1], BF16)
    C.XT0_tok = [Tok() for _ in range(NS)]
    C.XT0_z = Tok()
    C.YT = dscr("YT", [D, T], BF16)
    C.YT_tok = [[Tok() for _ in range(NS)] for _ in range(2)]
    C.H0 = dscr("H0", [T, D])
    C.H0_tok = [Tok() for _ in range(NT)]
    C.HT0 = dscr("HT0", [D, T], BF16)
    C.HT0_tok = [Tok() for _ in range(NS)]
    C.X1 = dscr("X1", [T, D])
    C.X1_tok = [Tok() for _ in range(NT)]
    C.XT1 = dscr("XT1", [D, T], BF16)
    C.XT1_tok = [Tok() for _ in range(NS)]

    for nm, shp in (("dbg_ya", [T, 512]), ("dbg_yb", [T, 512]), ("dbg_dsa", [T, 1024]), ("dbg_mask", [T, T]), ("dbg_sc", [T, T])):
        if nm in dbg:
            C.dbg[nm] = dscr(nm, shp, out=True)
    C.WGU16 = [dscr("WGU16_%d" % l, [NE, 128, 2 * 8 * DE], BF16) for l in range(2)]
    C.WGU16_tok = [[Tok() for _ in range(NE)] for l in range(2)]
    C.WD16 = [dscr("WD16_%d" % l, [128, 32 * D], BF16) for l in range(2)]
    C.WD16_tok = [Tok() for l in range(2)]
    C.banks = []
    for b in range(8):
        t = es.enter_context(nc.psum_tensor("psb%d" % b, [128, 512], F32))
        C.banks.append((t, Tok()))
    C.bi = 0

    C.bank_pool = list(range(8))

    def bank():
        b = C.banks[C.bank_pool[C.bi % len(C.bank_pool)]]
        C.bi += 1
        return b
    C.bank = bank

    C.ident = P.sb([128, 128], F32, "ident")
    C.ident_tok = Tok()
    P.dma("sp", C.ident[:], I["c_ident"], w=[C.ident_tok])

    C.cast_todo = {}
    if "M0" in stages:
        cast_weights(C, 0)
    if "A" in stages:
        phase_A(C)
    if "R" in stages:
        phase_rwkv(C)
    if "G" in stages:
        phase_gla(C)
    if "M1" in stages:
        cast_weights(C, 1)
    if "O0" in stages:
        phase_outproj(C, I["w_out_even"], C.YT, lambda s: [C.YT_tok[0][s], C.YT_tok[1][s]], I["x"], lambda i: [],
                      I["ln1_g"][0:1, :], I["ln1_b"][0:1, :], C.H0, C.H0_tok, C.HT0, C.HT0_tok)
    if "M0" in stages:
        cast_some(C, 0, 999)
        phase_moe(C, 0, C.H0, C.H0_tok, C.HT0, C.HT0_tok, C.X1, C.X1_tok, C.XT1, C.XT1_tok)
    if "S1" in stages:
        phase_dsa(C)
    if "O1" in stages:
        C.H1 = dscr("H1", [T, D])
        C.H1_tok = [Tok() for _ in range(NT)]
        C.HT1 = dscr("HT1", [D, T], BF16)
        C.HT1_tok = [Tok() for _ in range(NS)]
        phase_outproj(C, I["w_out_odd"], C.YT, lambda s: [C.YT_tok[0][s], C.YT_tok[1][s]], C.X1, lambda i: [C.X1_tok[i]],
                      I["ln1_g"][1:2, :], I["ln1_b"][1:2, :], C.H1, C.H1_tok, C.HT1, C.HT1_tok)
    if "M1" in stages:
        cast_some(C, 1, 999)
        out_tok = [Tok() for _ in range(NT)]
        phase_moe(C, 1, C.H1, C.H1_tok, C.HT1, C.HT1_tok, out, out_tok, None, None)
    P.finish()
    es.close()
    return nc


def XTv(ap):
    return ap.rearrange("(c p) t -> p c t", p=128)


def cast_weights(C, l):
    P, I = C.P, C.I
    th = []
    for e in range(NE):
        dst = C.WGU16[l][e].rearrange("p (t c f) -> p t c f", t=2, c=8)
        th.append(lambda e=e, dst=dst: P.dma("pool", dst[:, 0, :, :], I["exp_w_gate"][l, e].rearrange("(c p) f -> p c f", p=128), w=[C.WGU16_tok[l][e]]))
        th.append(lambda e=e, dst=dst: P.dma("pool", dst[:, 1, :, :], I["exp_w_up"][l, e].rearrange("(c p) f -> p c f", p=128), w=[C.WGU16_tok[l][e]]))
    wd_flat = I["exp_w_down"][l].rearrange("e f d -> (e f) d")
    dstd = C.WD16[l].rearrange("p (c d) -> p c d", c=32)
    for c4 in range(8):
        th.append(lambda c4=c4: P.dma("pool", dstd[:, c4 * 4:(c4 + 1) * 4, :], wd_flat[c4 * 512:(c4 + 1) * 512, :].rearrange("(c p) d -> p c d", p=128), w=[C.WD16_tok[l]]))
    C.cast_todo[l] = th


def cast_some(C, l, n):
    th = C.cast_todo.get(l, [])
    for _ in range(min(n, len(th))):
        th.pop(0)()


def phase_A(C):
    nc, P, T, I = C.nc, C.P, C.T, C.I
    with ExitStack() as es:
        xin = [P.sb([128, D], F32, "xin", es) for _ in range(2)]
        xin_tok = [Tok(), Tok()]
        st = [P.sb([128, 8, 512], BF16, "ast", es) for _ in range(2)]
        st_tok = [Tok(), Tok()]
        z = P.sb([128, 8, 1], BF16, "zc", es)
        zt = Tok()
        P.op("dve", lambda e: e.memset(z[:], 0.0), w=[zt])
        P.dma("sp", XTv(C.XT0)[:, :, 0:1], z[:], r=[zt], w=[C.XT0_z], allow_slow_non_contiguous=True)
        for s in range(C.NS):
            sb_ = st[s % 2]
            for j in range(4):
                i = s * 4 + j
                xb = xin[i % 2]
                xt = xin_tok[i % 2]
                P.dma("sp", xb[:], I["x"][i * 128:(i + 1) * 128, :], w=[xt])
                for half in range(2):
                    bt, bk = C.bank()
                    for c4 in range(4):
                        c = half * 4 + c4
                        P.op("pe", lambda e: e.transpose(bt[:, c4 * 128:(c4 + 1) * 128], xb[:, c * 128:(c + 1) * 128], C.ident[:]),
                             r=[xt, C.ident_tok], w=[bk], inc=(c4 == 3))
                    en = "act" if half == 0 else "dve"
                    src = bt[:, :].rearrange("p (c t) -> p c t", c=4)
                    dst = sb_[:, half * 4:(half + 1) * 4, j * 128:(j + 1) * 128]
                    if en == "act":
                        P.op("act", lambda e: e.copy(dst, src), r=[bk], w=[st_tok[s % 2]])
                    else:
                        P.op("dve", lambda e: e.tensor_copy(dst, src), r=[bk], w=[st_tok[s % 2]])
            P.dma("sp", XTv(C.XT0)[:, :, 1 + s * 512:1 + (s + 1) * 512], sb_[:], r=[st_tok[s % 2]], w=[C.XT0_tok[s]])
        P.barrier()


class TL:
    def __init__(self, t, k=None):
        self.t = t
        self.k = k or Tok()

    def __getitem__(self, idx):
        return self.t[idx]


def mk(P, shape, dt=F32, name=None, es=None):
    return TL(P.sb(shape, dt, name, es))


def bcast_load(C, dst_ap, src_row, np_, tok, q="sp"):
    C.P.dma(q, dst_ap, src_row.partition_broadcast(np_), w=[tok])


def hv(ap, h):
    return ap.rearrange("p (h v) -> p h v", h=h)


def phase_rwkv(C):
    nc, P, T, I = C.nc, C.P, C.T, C.I
    mm = ALU.mult
    with ExitStack() as es:
        W1 = mk(P, [128, 8, A_COLS], BF16, "W1", es)
        W2 = mk(P, [128, 8, A_COLS], BF16, "W2", es)
        with ExitStack() as es2:
            mub = mk(P, [128, A_COLS], F32, "mub", es2)
            omu = mk(P, [128, A_COLS], F32, "omu", es2)
            stg = [mk(P, [128, A_COLS], F32, "wstg", es2) for _ in range(2)]
            bcast_load(C, mub[:], I["a_mu"], 128, mub.k)
            P.op("dve", lambda e: e.tensor_scalar(omu[:], mub[:], -1.0, 1.0, ALU.mult, ALU.add), r=[mub.k], w=[omu.k])
            for c in range(8):
                s_ = stg[c % 2]
                P.dma("sp", s_[:], I["w_in_even"][c * 128:(c + 1) * 128, 0:A_COLS], w=[s_.k])
                P.op("dve", lambda e: e.tensor_tensor(W1[:, c, :], s_[:], omu[:], mm), r=[s_.k, omu.k], w=[W1.k])
                P.op("pool", lambda e: e.tensor_tensor(W2[:, c, :], s_[:], mub[:], mm), r=[s_.k, mub.k], w=[W2.k])
            P.barrier()
        LW = mk(P, [128, 512], BF16, "LW", es)
        G2 = mk(P, [128, 512], BF16, "G2", es)
        P.dma("pool", LW[0:64, :], I["a_w2"], w=[LW.k])
        P.dma("pool", LW[64:128, :], I["a_a2"], w=[LW.k])
        P.dma("pool", G2[:], I["a_g2"], w=[G2.k])
        BV = mk(P, [64, 7, 512], F32, "BV", es)
        for n, name in enumerate(("a_w0", "a_a0", "a_kk_scale", "a_ka_scale", "a_r_k", "a_gn_g", "a_gn_b")):
            bcast_load(C, BV[:, n, :], I[name], 64, BV.k)
        w0b, a0b, kksb, kab, rkb, gngb, gnbb = [BV[:, n, :] for n in range(7)]
        tri = mk(P, [64, 3, 64], F32, "tri", es)
        ncol = mk(P, [64, 1], F32, "ncol", es)
        mMA = mk(P, [64, 8, 128], F32, "mMA", es)
        mBB = mk(P, [64, 8, 128], F32, "mBB", es)
        mNT = mk(P, [64, 8, 64], F32, "mNT", es)
        id8 = mk(P, [64, 8, 64], F32, "id8", es)
        for tl, nm in ((tri, "c_tri64"), (ncol, "c_ncol64"), (mMA, "c_mMA"), (mBB, "c_mBB"), (mNT, "c_mNT"), (id8, "c_id8")):
            P.dma("sp", tl[:], I[nm], w=[tl.k])
        id64 = C.ident[0:64, 0:64]

        def wt(name, shape=(64, 512), dt=F32):
            return mk(P, list(shape), dt, name, es)
        ATs = [mk(P, [128, 8, 513], BF16, "ATs", es) for _ in range(2)]
        TX = wt("TX", (128, 512), BF16)
        SG = wt("SG", (128, 512), BF16)
        r_, k_, v_, sg, a_, kk, be, Bi, Ki, tmp = [wt(n) for n in ("r", "k", "v", "sg", "a", "kk", "be", "Bi", "Ki", "tmp")]
        Ep, Em, Ex, Ee = [wt(n) for n in ("Ep", "Em", "Ex", "Ee")]
        s8 = [wt("s8_%d" % n, (64, 8)) for n in range(4)]
        KRs = [wt("KR", (64, 8, 128), BF16) for _ in range(2)]
        BiT = wt("BiT", (64, 8, 64), BF16)
        KiT = wt("KiT", (64, 8, 64), BF16)
        MAs = [wt("MA", (64, 8, 128), BF16) for _ in range(2)]
        BBs = [wt("BB", (64, 8, 128), BF16) for _ in range(2)]
        Xb = [wt("X%d" % n, (64, 8, 64), BF16) for n in range(2)]
        XTb = [wt("XT%d" % n, (64, 8, 64), BF16) for n in range(2)]
        Qbs = [[wt("Q%d" % n, (64, 8, 64), BF16) for n in range(2)] for _ in range(2)]
        Xs = wt("Xs", (64, 8, 64), BF16)
        nU = wt("nU", (64, 8, 64), BF16)
        Y = wt("Y")
        tmpb = wt("tmpb")
        Hs = [wt("H%d" % n, (64, 8, 64)) for n in range(2)]
        Hbs = [wt("Hb%d" % n, (64, 8, 64), BF16) for n in range(2)]
        vbs = [wt("vb", (64, 512), BF16) for _ in range(2)]
        Ke16s = [wt("Ke16", (64, 512), BF16) for _ in range(2)]
        Be16s = [wt("Be16", (64, 512), BF16) for _ in range(2)]
        PCs = [wt("PC", (64, 8)) for _ in range(2)]
        gs_ = [wt("g", (64, 512)) for _ in range(2)]
        bonuss = [wt("bonus", (64, 512)) for _ in range(2)]
        P.op("pool", lambda e: e.memset(Hbs[0][:], 0.0), w=[Hbs[0].k])
        yst = [mk(P, [128, 4, 512], BF16, "yst", es) for _ in range(2)]
        P.op("dve", lambda e: e.memset(Hs[0][:], 0.0), w=[Hs[0].k])

        def psb():
            t, k = C.bank()
            return TL(t, k)

        def v3(tl_or_ap, h=8):
            return hv(tl_or_ap, h)

        def chunk(s, ci):
          at = ATs[s % 2]
          ys = yst[s % 2]
          cast_some(C, 0, 1)
          if ci == 0:
            rd = [C.XT0_tok[s]] + ([C.XT0_tok[s - 1]] if s > 0 else [C.XT0_z])
            P.dma("sp", at[:], XTv(C.XT0)[:, :, s * 512:s * 512 + 513], r=rd, w=[at.k])
            for which in range(2):
                pb = psb()
                c0 = 1536 + which * 128
                for c in range(8):
                    P.op("pe", lambda e: e.matmul(pb[:, :], lhsT=W1[:, c, c0:c0 + 128], rhs=at[:, c, 1:513], start=(c == 0), stop=False),
                         r=[W1.k, at.k], w=[pb.k], inc=False)
                for c in range(8):
                    P.op("pe", lambda e: e.matmul(pb[:, :], lhsT=W2[:, c, c0:c0 + 128], rhs=at[:, c, 0:512], start=False, stop=(c == 7)),
                         r=[W2.k, at.k], w=[pb.k], inc=(c == 7))
                if which == 0:
                    P.op("act", lambda e: e.activation(out=TX[0:64, :], in_=pb[0:64, :], func=AF.Tanh), r=[pb.k], w=[TX.k])
                    P.op("act", lambda e: e.copy(TX[64:128, :], pb[64:128, :]), r=[pb.k], w=[TX.k])
                else:
                    P.op("act", lambda e: e.activation(out=SG[:, :], in_=pb[:, :], func=AF.Sigmoid), r=[pb.k], w=[SG.k])
          if True:
            if True:
                g = s * 8 + ci
                t0 = ci * 64
                KR, MA, BB, Qb = KRs[g % 2], MAs[g % 2], BBs[g % 2], Qbs[g % 2]
                vb, Ke16, Be16, PC, g_, bonus = vbs[g % 2], Ke16s[g % 2], Be16s[g % 2], PCs[g % 2], gs_[g % 2], bonuss[g % 2]
                pr, pk, pv = psb(), psb(), psb()
                for pb, c0 in ((pr, 0), (pk, 512), (pv, 1024)):
                    for c in range(8):
                        P.op("pe", lambda e: e.matmul(pb[0:64, :], lhsT=at[:, c, 1 + t0:1 + t0 + 64], rhs=W1[:, c, c0:c0 + 512], start=(c == 0), stop=False),
                             r=[W1.k, at.k], w=[pb.k], inc=False)
                    for c in range(8):
                        P.op("pe", lambda e: e.matmul(pb[0:64, :], lhsT=at[:, c, t0:t0 + 64], rhs=W2[:, c, c0:c0 + 512], start=False, stop=(c == 7)),
                             r=[W2.k, at.k], w=[pb.k], inc=(c == 7))
                yield "F"
                pz, pza, pg = psb(), psb(), psb()
                P.op("pe", lambda e: e.matmul(pz[0:64, :], lhsT=TX[0:64, t0:t0 + 64], rhs=LW[0:64, :], start=True, stop=True), r=[TX.k, LW.k], w=[pz.k])
                P.op("pe", lambda e: e.matmul(pza[0:64, :], lhsT=TX[64:128, t0:t0 + 64], rhs=LW[64:128, :], start=True, stop=True), r=[TX.k, LW.k], w=[pza.k])
                P.op("pe", lambda e: e.matmul(pg[0:64, :], lhsT=SG[:, t0:t0 + 64], rhs=G2[:, :], start=True, stop=True), r=[SG.k, G2.k], w=[pg.k])
                P.op("act", lambda e: e.copy(r_[:], pr[0:64, :]), r=[pr.k], w=[r_.k])
                P.op("act", lambda e: e.copy(v_[:], pv[0:64, :]), r=[pv.k], w=[v_.k])
                P.op("act", lambda e: e.copy(vb[:], pv[0:64, :]), r=[pv.k], w=[vb.k])
                P.op("act", lambda e: e.copy(g_[:], pg[0:64, :]), r=[pg.k], w=[g_.k])
                P.op("dve", lambda e: e.tensor_copy(k_[:], pk[0:64, :]), r=[pk.k], w=[k_.k])
                P.op("dve", lambda e: e.tensor_tensor(sg[:], pz[0:64, :], w0b, ALU.add), r=[pz.k, BV.k], w=[sg.k])
                P.op("act", lambda e: e.activation(out=sg[:], in_=sg[:], func=AF.Sigmoid), r=[sg.k], w=[sg.k])
                P.op("dve", lambda e: e.tensor_tensor(a_[:], pza[0:64, :], a0b, ALU.add), r=[pza.k, BV.k], w=[a_.k])
                P.op("act", lambda e: e.activation(out=a_[:], in_=a_[:], func=AF.Sigmoid), r=[a_.k], w=[a_.k])
                yield "F"
                P.op("pool", lambda e: e.tensor_tensor(kk[:], k_[:], kksb, mm), r=[k_.k, BV.k], w=[kk.k])
                P.op("pool", lambda e: e.tensor_tensor(tmp[:], kk[:], kk[:], mm), r=[kk.k], w=[tmp.k])
                P.op("dve", lambda e: e.tensor_reduce(s8[0][:], v3(tmp[:]), AX.X, ALU.add), r=[tmp.k], w=[s8[0].k])
                P.op("dve", lambda e: e.tensor_scalar(s8[0][:], s8[0][:], 1e-24, None, ALU.max), r=[s8[0].k], w=[s8[0].k])
                P.op("act", lambda e: e.activation(out=s8[0][:], in_=s8[0][:], func=AF.Ln), r=[s8[0].k], w=[s8[0].k])
                P.op("act", lambda e: e.activation(out=s8[0][:], in_=s8[0][:], func=AF.Exp, scale=-0.5), r=[s8[0].k], w=[s8[0].k])
                P.op("dve", lambda e: e.tensor_tensor(v3(kk[:]), v3(kk[:]), s8[0][:, :].unsqueeze(2).to_broadcast([64, 8, 64]), mm),
                     r=[kk.k, s8[0].k], w=[kk.k])
                P.op("pool", lambda e: e.tensor_tensor(be[:], kk[:], a_[:], mm), r=[kk.k, a_.k], w=[be.k])
                P.op("dve", lambda e: e.scalar_tensor_tensor(tmp[:], a_[:], -1.0, kab, ALU.add, mm), r=[a_.k, BV.k], w=[tmp.k])
                P.op("dve", lambda e: e.scalar_tensor_tensor(k_[:], tmp[:], 1.0, k_[:], ALU.add, mm), r=[tmp.k, k_.k], w=[k_.k])
                P.op("pool", lambda e: e.tensor_tensor(tmp[:], r_[:], k_[:], mm), r=[r_.k, k_.k], w=[tmp.k])
                P.op("pool", lambda e: e.tensor_tensor(tmp[:], tmp[:], rkb, mm), r=[tmp.k, BV.k], w=[tmp.k])
                P.op("dve", lambda e: e.tensor_reduce(s8[1][:], v3(tmp[:]), AX.X, ALU.add), r=[tmp.k], w=[s8[1].k])
                P.op("dve", lambda e: e.tensor_tensor(v3(bonus[:]), v3(v_[:]), s8[1][:, :].unsqueeze(2).to_broadcast([64, 8, 64]), mm),
                     r=[v_.k, s8[1].k], w=[bonus.k])
                yield "F"
                pcl, pcx, pca, ppc = psb(), psb(), psb(), psb()
                for pb, n in ((pcl, 0), (pcx, 1), (pca, 2)):
                    P.op("pe", lambda e: e.matmul(pb[0:64, :], lhsT=tri[:, n, :], rhs=sg[:], start=True, stop=True), r=[tri.k, sg.k], w=[pb.k])
                for h in range(8):
                    P.op("pe", lambda e: e.matmul(ppc[0:64, h:h + 1], lhsT=sg[:, h * 64:(h + 1) * 64], rhs=ncol[:], start=True, stop=True),
                         r=[sg.k, ncol.k], w=[ppc.k], inc=(h == 7))
                P.op("act", lambda e: e.activation(out=Ep[:], in_=pcl[0:64, :], func=AF.Exp), r=[pcl.k], w=[Ep.k])
                P.op("act", lambda e: e.activation(out=Em[:], in_=pcl[0:64, :], func=AF.Exp, scale=-1.0), r=[pcl.k], w=[Em.k])
                P.op("act", lambda e: e.activation(out=Ex[:], in_=pcx[0:64, :], func=AF.Exp), r=[pcx.k], w=[Ex.k])
                P.op("act", lambda e: e.activation(out=Ee[:], in_=pca[0:64, :], func=AF.Exp), r=[pca.k], w=[Ee.k])
                P.op("act", lambda e: e.activation(out=PC[:], in_=ppc[0:64, 0:8], func=AF.Exp), r=[ppc.k], w=[PC.k])
                P.op("dve", lambda e: e.tensor_tensor(r_[:], r_[:], Ep[:], mm), r=[r_.k, Ep.k], w=[r_.k])
                P.op("pool", lambda e: e.tensor_tensor(kk[:], kk[:], Ex[:], mm), r=[kk.k, Ex.k], w=[kk.k])
                P.op("dve", lambda e: e.tensor_tensor(Bi[:], be[:], Em[:], mm), r=[be.k, Em.k], w=[Bi.k])
                P.op("pool", lambda e: e.tensor_tensor(Ki[:], k_[:], Em[:], mm), r=[k_.k, Em.k], w=[Ki.k])
                P.op("dve", lambda e: e.tensor_tensor(Ke16[:], k_[:], Ee[:], mm), r=[k_.k, Ee.k], w=[Ke16.k])
                P.op("pool", lambda e: e.tensor_tensor(Be16[:], be[:], Ee[:], mm), r=[be.k, Ee.k], w=[Be16.k])
                yield "F"
                for src, dst, off, en in ((kk, KR, 0, "act"), (r_, KR, 64, "dve"), (Bi, BiT, 0, "act"), (Ki, KiT, 0, "dve")):
                    pb = psb()
                    for h in range(8):
                        P.op("pe", lambda e: e.transpose(pb[0:64, h * 64:(h + 1) * 64], src[:, h * 64:(h + 1) * 64], id64),
                             r=[src.k, C.ident_tok], w=[pb.k], inc=(h == 7))
                    d_ = dst[:, :, off:off + 64]
                    s_ = v3(pb[0:64, :])
                    if en == "act":
                        P.op("act", lambda e: e.copy(d_, s_), r=[pb.k], w=[dst.k])
                    else:
                        P.op("dve", lambda e: e.tensor_copy(d_, s_), r=[pb.k], w=[dst.k])
                yield "F"
                pma = [psb(), psb()]
                pbb = [psb(), psb()]
                pnt = psb()
                for h in range(8):
                    hb, hh = h // 4, h % 4
                    P.op("pe", lambda e: e.matmul(pma[hb][0:64, hh * 128:(hh + 1) * 128], lhsT=BiT[:, h, :], rhs=KR[:, h, :], start=True, stop=True),
                         r=[BiT.k, KR.k], w=[pma[hb].k], inc=(hh == 3))
                for h in range(8):
                    hb, hh = h // 4, h % 4
                    P.op("pe", lambda e: e.matmul(pbb[hb][0:64, hh * 128:(hh + 1) * 128], lhsT=KiT[:, h, :], rhs=KR[:, h, :], start=True, stop=True),
                         r=[KiT.k, KR.k], w=[pbb[hb].k], inc=(hh == 3))
                for h in range(8):
                    P.op("pe", lambda e: e.matmul(pnt[0:64, h * 64:(h + 1) * 64], lhsT=KR[:, h, 0:64], rhs=BiT[:, h, :], start=True, stop=True),
                         r=[BiT.k, KR.k], w=[pnt.k], inc=(h == 7))
                for hb in range(2):
                    P.op("dve", lambda e: e.tensor_tensor(MA[:, hb * 4:(hb + 1) * 4, :], hv(pma[hb][0:64, :], 4), mMA[:, hb * 4:(hb + 1) * 4, :], mm),
                         r=[pma[hb].k, mMA.k], w=[MA.k])
                    P.op("dve", lambda e: e.tensor_tensor(BB[:, hb * 4:(hb + 1) * 4, :], hv(pbb[hb][0:64, :], 4), mBB[:, hb * 4:(hb + 1) * 4, :], mm),
                         r=[pbb[hb].k, mBB.k], w=[BB.k])
                X, XT, Q = Xb[0], XTb[0], Qb[0]
                P.op("dve", lambda e: e.tensor_tensor(XT[:], v3(pnt[0:64, :]), mNT[:], mm), r=[pnt.k, mNT.k], w=[XT.k])
                P.op("pool", lambda e: e.tensor_copy(X[:], MA[:, :, 0:64]), r=[MA.k], w=[X.k])
                P.op("pool", lambda e: e.tensor_tensor(Q[:], MA[:, :, 0:64], id8[:], ALU.add), r=[MA.k, id8.k], w=[Q.k])
                for lvl in range(5):
                    Xn, XTn, Qn = Xb[(lvl + 1) % 2], XTb[(lvl + 1) % 2], Qb[(lvl + 1) % 2]
                    pxt = psb()
                    for h in range(8):
                        P.op("pe", lambda e: e.matmul(pxt[0:64, h * 64:(h + 1) * 64], lhsT=X[:, h, :], rhs=XT[:, h, :], start=True, stop=True),
                             r=[X.k, XT.k], w=[pxt.k], inc=(h == 7))
                    if lvl < 4:
                        px = psb()
                        for h in range(8):
                            P.op("pe", lambda e: e.matmul(px[0:64, h * 64:(h + 1) * 64], lhsT=XT[:, h, :], rhs=X[:, h, :], start=True, stop=True),
                                 r=[X.k, XT.k], w=[px.k], inc=(h == 7))
                    P.op("act", lambda e: e.copy(XTn[:], v3(pxt[0:64, :])), r=[pxt.k], w=[XTn.k])
                    if lvl < 4:
                        P.op("dve", lambda e: e.tensor_copy(Xn[:], v3(px[0:64, :])), r=[px.k], w=[Xn.k])
                    pq = psb()
                    for h in range(8):
                        P.op("pe", lambda e: e.matmul(pq[0:64, h * 64:(h + 1) * 64], lhsT=XTn[:, h, :], rhs=Q[:, h, :], start=True, stop=True),
                             r=[XTn.k, Q.k], w=[pq.k], inc=(h == 7))
                    P.op("dve", lambda e: e.tensor_tensor(Qn[:], Q[:], v3(pq[0:64, :]), ALU.add), r=[Q.k, pq.k], w=[Qn.k])
                    X, XT, Q = Xn, XTn, Qn
                    yield "F"
                yield "END_FRONT"
                H, Hn = Hs[g % 2], Hs[(g + 1) % 2]
                Hb, Hbn = Hbs[g % 2], Hbs[(g + 1) % 2]
                pxs = psb()
                for h in range(8):
                    P.op("pe", lambda e: e.matmul(pxs[0:64, h * 64:(h + 1) * 64], lhsT=KR[:, h, 0:64], rhs=Hb[:, h, :], start=True, stop=False),
                         r=[KR.k, Hb.k], w=[pxs.k], inc=False)
                    P.op("pe", lambda e: e.matmul(pxs[0:64, h * 64:(h + 1) * 64], lhsT=BB[:, h, 0:64], rhs=vb[:, h * 64:(h + 1) * 64], start=False, stop=True),
                         r=[BB.k, vb.k], w=[pxs.k], inc=(h == 7))
                P.op("act", lambda e: e.copy(Xs[:], v3(pxs[0:64, :])), r=[pxs.k], w=[Xs.k])
                yield "B"
                pu = psb()
                for h in range(8):
                    P.op("pe", lambda e: e.matmul(pu[0:64, h * 64:(h + 1) * 64], lhsT=Q[:, h, :], rhs=Xs[:, h, :], start=True, stop=True),
                         r=[Q.k, Xs.k], w=[pu.k], inc=(h == 7))
                P.op("act", lambda e: e.mul(nU[:], v3(pu[0:64, :]), -1.0), r=[pu.k], w=[nU.k])
                yield "B"
                py, ph = psb(), psb()
                for h in range(8):
                    sl = slice(h * 64, (h + 1) * 64)
                    P.op("pe", lambda e: e.matmul(py[0:64, sl], lhsT=KR[:, h, 64:128], rhs=Hb[:, h, :], start=True, stop=False), r=[KR.k, Hb.k], w=[py.k], inc=False)
                    P.op("pe", lambda e: e.matmul(py[0:64, sl], lhsT=BB[:, h, 64:128], rhs=vb[:, sl], start=False, stop=False), r=[BB.k, vb.k], w=[py.k], inc=False)
                    P.op("pe", lambda e: e.matmul(py[0:64, sl], lhsT=MA[:, h, 64:128], rhs=nU[:, h, :], start=False, stop=True), r=[MA.k, nU.k], w=[py.k], inc=(h == 7))
                for h in range(8):
                    sl = slice(h * 64, (h + 1) * 64)
                    P.op("pe", lambda e: e.matmul(ph[0:64, sl], lhsT=Ke16[:, sl], rhs=vb[:, sl], start=True, stop=False), r=[Ke16.k, vb.k], w=[ph.k], inc=False)
                    P.op("pe", lambda e: e.matmul(ph[0:64, sl], lhsT=Be16[:, sl], rhs=nU[:, h, :], start=False, stop=True), r=[Be16.k, nU.k], w=[ph.k], inc=(h == 7))
                P.op("pool", lambda e: e.tensor_tensor(Hn[:], H[:], PC[:, :].unsqueeze(2).to_broadcast([64, 8, 64]), mm), r=[H.k, PC.k], w=[Hn.k])
                P.op("dve", lambda e: e.tensor_tensor(Hn[:], Hn[:], v3(ph[0:64, :]), ALU.add), r=[Hn.k, ph.k], w=[Hn.k])
                P.op("act", lambda e: e.copy(Hbn[:], Hn[:]), r=[Hn.k], w=[Hbn.k])
                yield "B"
                P.op("act", lambda e: e.copy(Y[:], py[0:64, :]), r=[py.k], w=[Y.k])
                P.op("dve", lambda e: e.tensor_reduce(s8[2][:], v3(Y[:]), AX.X, ALU.add), r=[Y.k], w=[s8[2].k])
                P.op("dve", lambda e: e.tensor_scalar(s8[2][:], s8[2][:], 1.0 / 64, None, mm), r=[s8[2].k], w=[s8[2].k])
                P.op("dve", lambda e: e.tensor_tensor(v3(Y[:]), v3(Y[:]), s8[2][:, :].unsqueeze(2).to_broadcast([64, 8, 64]), ALU.subtract),
                     r=[Y.k, s8[2].k], w=[Y.k])
                yield "B"
                P.op("pool", lambda e: e.tensor_tensor(tmpb[:], Y[:], Y[:], mm), r=[Y.k], w=[tmpb.k])
                P.op("dve", lambda e: e.tensor_reduce(s8[3][:], v3(tmpb[:]), AX.X, ALU.add), r=[tmpb.k], w=[s8[3].k])
                P.op("act", lambda e: e.activation(out=s8[3][:], in_=s8[3][:], func=AF.Ln, bias=64e-5, scale=1.0 / 64), r=[s8[3].k], w=[s8[3].k])
                P.op("act", lambda e: e.activation(out=s8[3][:], in_=s8[3][:], func=AF.Exp, scale=-0.5), r=[s8[3].k], w=[s8[3].k])
                P.op("dve", lambda e: e.tensor_tensor(v3(Y[:]), v3(Y[:]), s8[3][:, :].unsqueeze(2).to_broadcast([64, 8, 64]), mm),
                     r=[Y.k, s8[3].k], w=[Y.k])
                P.op("pool", lambda e: e.tensor_tensor(Y[:], Y[:], gngb, mm), r=[Y.k, BV.k], w=[Y.k])
                P.op("pool", lambda e: e.tensor_tensor(Y[:], Y[:], gnbb, ALU.add), r=[Y.k, BV.k], w=[Y.k])
                P.op("dve", lambda e: e.tensor_tensor(Y[:], Y[:], bonus[:], ALU.add), r=[Y.k, bonus.k], w=[Y.k])
                P.op("dve", lambda e: e.tensor_tensor(Y[:], Y[:], g_[:], mm), r=[Y.k, g_.k], w=[Y.k])
                if "dbg_ya" in C.dbg:
                    P.dma("sp", C.dbg["dbg_ya"][g * 64:(g + 1) * 64, :], Y[:], r=[Y.k])
                yield "B"
                pb = psb()
                for q in range(4):
                    P.op("pe", lambda e: e.transpose(pb[:, q * 64:(q + 1) * 64], Y[:, q * 128:(q + 1) * 128], id64), r=[Y.k, C.ident_tok], w=[pb.k], inc=(q == 3))
                P.op("act", lambda e: e.copy(ys[:, :, t0:t0 + 64], hv(pb[:, 0:256], 4)), r=[pb.k], w=[ys.k])
                if ci == 7:
                    P.dma("sp", XTv(C.YT)[:, 0:4, s * 512:(s + 1) * 512], ys[:], r=[ys.k], w=[C.YT_tok[0][s]])

        makers = [(lambda s=s, ci=ci: chunk(s, ci)) for s in range(C.NS) for ci in range(8)]
        if RWKV_INTERLEAVE:
            pipeline2(makers, interleave=True)
        else:
            def to_end_front(g_):
                while next(g_) != "END_FRONT":
                    pass
            g = makers[0]()
            to_end_front(g)
            for n in range(len(makers)):
                for _ in range(3):
                    next(g)
                g2 = None
                if n + 1 < len(makers):
                    g2 = makers[n + 1]()
                    next(g2)
                for _ in g:
                    pass
                if g2 is not None:
                    to_end_front(g2)
                g = g2
        P.barrier()


def pipeline2(makers, interleave=True):
    if not interleave:
        for mk_ in makers:
            for _ in mk_():
                pass
        return
    prevB = None
    for mk_ in makers:
        g = mk_()
        while True:
            r = next(g)
            if prevB is not None:
                try:
                    next(prevB)
                except StopIteration:
                    prevB = None
            if r == "END_FRONT":
                break
        if prevB is not None:
            for _ in prevB:
                pass
        prevB = g
    if prevB is not None:
        for _ in prevB:
            pass


def psb(C):
    t, k = C.bank()
    return TL(t, k)


def phase_gla(C):
    nc, P, T, I = C.nc, C.P, C.T, C.I
    mm = ALU.mult
    with ExitStack() as es:
        WB = mk(P, [128, 8, B_COLS], BF16, "WB", es)
        for c in range(8):
            P.dma("pool", WB[:, c, :], I["w_in_even"][c * 128:(c + 1) * 128, A_COLS:EVEN_COLS], w=[WB.k])
        GW2 = mk(P, [16, 256], BF16, "GW2", es)
        P.dma("pool", GW2[:], I["b_gate_w2"], w=[GW2.k])
        gbb = mk(P, [128, 256], F32, "gbb", es)
        ngb = mk(P, [128, 512], F32, "ngb", es)
        bcast_load(C, gbb[:], I["b_gate_b"], 128, gbb.k)
        bcast_load(C, ngb[:], I["b_norm_g"], 128, ngb.k)
        tri = mk(P, [128, 2, 128], F32, "tri128", es)
        ncol = mk(P, [128, 1], F32, "ncol128", es)
        iu = mk(P, [128, 4, 128], F32, "iu128", es)
        for tl, nm in ((tri, "c_tri128"), (ncol, "c_ncol128"), (iu, "c_iu128")):
            P.dma("sp", tl[:], I[nm], w=[tl.k])
        id64 = C.ident[0:64, 0:64]

        def wt(name, shape, dt=F32):
            return mk(P, list(shape), dt, name, es)
        ATs = [wt("ATg", (128, 8, 512), BF16) for _ in range(2)]
        AL = wt("AL", (16, 512), BF16)
        l_ = wt("l", (128, 256))
        Eq, Ei, Ee = wt("Eq", (128, 256)), wt("Ei", (128, 256)), wt("Ee", (128, 256))
        PCg = wt("PCg", (64, 4))
        qd, ki, ke = wt("qd", (128, 256)), wt("ki", (128, 256)), wt("ke", (128, 256))
        v_ = wt("vg", (128, 512))
        qdT, kiT = wt("qdT", (64, 4, 128)), wt("kiT", (64, 4, 128))
        attT = wt("attT", (128, 4, 128))
        Ss = [wt("S%d" % n, (64, 4, 128)) for n in range(2)]
        o_ = wt("o", (128, 512))
        sq = wt("sqg", (128, 512))
        sl_ = wt("silu", (128, 512))
        m4 = wt("m4", (128, 4))
        yst = [wt("ystg", (128, 4, 512), BF16) for _ in range(2)]
        P.op("dve", lambda e: e.memset(Ss[0][:], 0.0), w=[Ss[0].k])
        def gproj(s, ci):
            at = ATs[s % 2]
            t0 = ci * 128
            if ci == 0:
                P.dma("sp", at[:], XTv(C.XT0)[:, :, 1 + s * 512:1 + (s + 1) * 512], r=[C.XT0_tok[s]], w=[at.k])
                pb = psb(C)
                for c in range(8):
                    P.op("pe", lambda e: e.matmul(pb[0:16, :], lhsT=WB[:, c, 1536:1552], rhs=at[:, c, :], start=(c == 0), stop=(c == 7)),
                         r=[WB.k, at.k], w=[pb.k], inc=(c == 7))
                P.op("act", lambda e: e.copy(AL[:], pb[0:16, :]), r=[pb.k], w=[AL.k])
            pqk, pv, pg = psb(C), psb(C), psb(C)
            for pb, c0 in ((pqk, 0), (pv, 512), (pg, 1024)):
                for c in range(8):
                    P.op("pe", lambda e: e.matmul(pb[:, :], lhsT=at[:, c, t0:t0 + 128], rhs=WB[:, c, c0:c0 + 512], start=(c == 0), stop=(c == 7)),
                         r=[WB.k, at.k], w=[pb.k], inc=(c == 7))
            pla = psb(C)
            P.op("pe", lambda e: e.matmul(pla[:, 0:256], lhsT=AL[:, t0:t0 + 128], rhs=GW2[:], start=True, stop=True), r=[AL.k, GW2.k], w=[pla.k])
            return pqk, pv, pg, pla

        nxt = gproj(0, 0)
        for s in range(C.NS):
            ys = yst[s % 2]
            for ci in range(4):
                g = s * 4 + ci
                t0 = ci * 128
                pqk, pv, pg, pla = nxt
                P.op("dve", lambda e: e.tensor_tensor(l_[:], pla[:, 0:256], gbb[:], ALU.add), r=[pla.k, gbb.k], w=[l_.k])
                P.op("act", lambda e: e.activation(out=l_[:], in_=l_[:], func=AF.Exp, scale=-1.0), r=[l_.k], w=[l_.k])
                P.op("act", lambda e: e.activation(out=l_[:], in_=l_[:], func=AF.Ln, bias=1.0), r=[l_.k], w=[l_.k])
                pbc, pba, ppc = psb(C), psb(C), psb(C)
                P.op("pe", lambda e: e.matmul(pbc[:, 0:256], lhsT=tri[:, 0, :], rhs=l_[:], start=True, stop=True), r=[tri.k, l_.k], w=[pbc.k])
                P.op("pe", lambda e: e.matmul(pba[:, 0:256], lhsT=tri[:, 1, :], rhs=l_[:], start=True, stop=True), r=[tri.k, l_.k], w=[pba.k])
                for h in range(4):
                    P.op("pe", lambda e: e.matmul(ppc[0:64, h:h + 1], lhsT=l_[:, h * 64:(h + 1) * 64], rhs=ncol[:], start=True, stop=True),
                         r=[l_.k, ncol.k], w=[ppc.k], inc=(h == 3))
                P.op("act", lambda e: e.activation(out=Eq[:], in_=pbc[:, 0:256], func=AF.Exp), r=[pbc.k], w=[Eq.k])
                P.op("act", lambda e: e.activation(out=Ei[:], in_=pbc[:, 0:256], func=AF.Exp, scale=-1.0), r=[pbc.k], w=[Ei.k])
                P.op("act", lambda e: e.activation(out=Ee[:], in_=pba[:, 0:256], func=AF.Exp), r=[pba.k], w=[Ee.k])
                P.op("act", lambda e: e.activation(out=PCg[:], in_=ppc[0:64, 0:4], func=AF.Exp), r=[ppc.k], w=[PCg.k])
                P.op("dve", lambda e: e.scalar_tensor_tensor(qd[:], pqk[:, 0:256], 0.125, Eq[:], mm, mm), r=[pqk.k, Eq.k], w=[qd.k])
                P.op("dve", lambda e: e.tensor_tensor(ki[:], pqk[:, 256:512], Ei[:], mm), r=[pqk.k, Ei.k], w=[ki.k])
                P.op("dve", lambda e: e.tensor_tensor(ke[:], pqk[:, 256:512], Ee[:], mm), r=[pqk.k, Ee.k], w=[ke.k])
                P.op("act", lambda e: e.copy(v_[:], pv[:, :]), r=[pv.k], w=[v_.k])
                P.op("act", lambda e: e.activation(out=sl_[:], in_=pg[:, :], func=AF.Silu), r=[pg.k], w=[sl_.k])
                for src, dst, en in ((qd, qdT, "act"), (ki, kiT, "dve")):
                    pb = psb(C)
                    for h in range(4):
                        P.op("pe", lambda e: e.transpose(pb[0:64, h * 128:(h + 1) * 128], src[:, h * 64:(h + 1) * 64], C.ident[:]),
                             r=[src.k, C.ident_tok], w=[pb.k], inc=(h == 3))
                    if en == "act":
                        P.op("act", lambda e: e.copy(dst[:], hv(pb[0:64, :], 4)), r=[pb.k], w=[dst.k])
                    else:
                        P.op("dve", lambda e: e.tensor_copy(dst[:], hv(pb[0:64, :], 4)), r=[pb.k], w=[dst.k])
                patt = psb(C)
                for h in range(4):
                    P.op("pe", lambda e: e.matmul(patt[:, h * 128:(h + 1) * 128], lhsT=kiT[:, h, :], rhs=qdT[:, h, :], start=True, stop=True),
                         r=[kiT.k, qdT.k], w=[patt.k], inc=(h == 3))
                P.op("dve", lambda e: e.tensor_tensor(attT[:], hv(patt[:, :], 4), iu[:], mm), r=[patt.k, iu.k], w=[attT.k])
                S, Sn = Ss[g % 2], Ss[(g + 1) % 2]
                po, pS = psb(C), psb(C)
                for h in range(4):
                    sl = slice(h * 128, (h + 1) * 128)
                    P.op("pe", lambda e: e.matmul(po[:, sl], lhsT=attT[:, h, :], rhs=v_[:, sl], start=True, stop=False), r=[attT.k, v_.k], w=[po.k], inc=False)
                    P.op("pe", lambda e: e.matmul(po[:, sl], lhsT=qdT[:, h, :], rhs=S[:, h, :], start=False, stop=True), r=[qdT.k, S.k], w=[po.k], inc=(h == 3))
                for h in range(4):
                    sl = slice(h * 128, (h + 1) * 128)
                    P.op("pe", lambda e: e.matmul(pS[0:64, sl], lhsT=ke[:, h * 64:(h + 1) * 64], rhs=v_[:, sl], start=True, stop=True), r=[ke.k, v_.k], w=[pS.k], inc=(h == 3))
                P.op("pool", lambda e: e.tensor_tensor(Sn[:], S[:], PCg[:, :].unsqueeze(2).to_broadcast([64, 4, 128]), mm), r=[S.k, PCg.k], w=[Sn.k])
                P.op("dve", lambda e: e.tensor_tensor(Sn[:], Sn[:], hv(pS[0:64, :], 4), ALU.add), r=[Sn.k, pS.k], w=[Sn.k])
                if g + 1 < C.NS * 4:
                    nxt = gproj((g + 1) // 4, (g + 1) % 4)
                P.op("act", lambda e: e.copy(o_[:], po[:, :]), r=[po.k], w=[o_.k])
                P.op("pool", lambda e: e.tensor_tensor(sq[:], o_[:], o_[:], mm), r=[o_.k], w=[sq.k])
                P.op("dve", lambda e: e.tensor_reduce(m4[:], hv(sq[:], 4), AX.X, ALU.add), r=[sq.k], w=[m4.k])
                P.op("act", lambda e: e.activation(out=m4[:], in_=m4[:], func=AF.Ln, bias=1e-5, scale=1.0 / 128), r=[m4.k], w=[m4.k])
                P.op("act", lambda e: e.activation(out=m4[:], in_=m4[:], func=AF.Exp, scale=-0.5), r=[m4.k], w=[m4.k])
                P.op("dve", lambda e: e.tensor_tensor(hv(o_[:], 4), hv(o_[:], 4), m4[:, :].unsqueeze(2).to_broadcast([128, 4, 128]), mm), r=[o_.k, m4.k], w=[o_.k])
                P.op("pool", lambda e: e.tensor_tensor(o_[:], o_[:], ngb[:], mm), r=[o_.k, ngb.k], w=[o_.k])
                P.op("dve", lambda e: e.tensor_tensor(o_[:], o_[:], sl_[:], mm), r=[o_.k, sl_.k], w=[o_.k])
                if "dbg_yb" in C.dbg:
                    P.dma("sp", C.dbg["dbg_yb"][g * 128:(g + 1) * 128, :], o_[:], r=[o_.k])
                pb = psb(C)
                for q in range(4):
                    P.op("pe", lambda e: e.transpose(pb[:, q * 128:(q + 1) * 128], o_[:, q * 128:(q + 1) * 128], C.ident[:]), r=[o_.k, C.ident_tok], w=[pb.k], inc=(q == 3))
                P.op("act", lambda e: e.copy(ys[:, :, t0:t0 + 128], hv(pb[:, :], 4)), r=[pb.k], w=[ys.k])
            P.dma("sp", XTv(C.YT)[:, 4:8, s * 512:(s + 1) * 512], ys[:], r=[ys.k], w=[C.YT_tok[1][s]])
        P.barrier()


def ln_inplace(C, xt, gb, bb, st, junk):
    P = C.P
    P.op("dve", lambda e: e.tensor_reduce(st[:, 0:1], xt[:], AX.X, ALU.add), r=[xt.k], w=[st.k])
    P.op("dve", lambda e: e.tensor_scalar(st[:, 0:1], st[:, 0:1], 1.0 / D, None, ALU.mult), r=[st.k], w=[st.k])
    P.op("dve", lambda e: e.tensor_scalar(xt[:], xt[:], st[:, 0:1], None, ALU.subtract), r=[xt.k, st.k], w=[xt.k])
    P.op("act", lambda e: e.activation(out=junk[:], in_=xt[:], func=AF.Square, accum_out=st[:, 1:2]), r=[xt.k], w=[junk.k, st.k])
    P.op("act", lambda e: e.activation(out=st[:, 1:2], in_=st[:, 1:2], func=AF.Sqrt, bias=LN_EPS, scale=1.0 / D), r=[st.k], w=[st.k])
    P.op("dve", lambda e: e.reciprocal(st[:, 1:2], st[:, 1:2]), r=[st.k], w=[st.k])
    P.op("dve", lambda e: e.scalar_tensor_tensor(xt[:], xt[:], st[:, 1:2], gb[:], ALU.mult, ALU.mult), r=[xt.k, st.k, gb.k], w=[xt.k])
    P.op("pool", lambda e: e.tensor_tensor(xt[:], xt[:], bb[:], ALU.add), r=[xt.k, bb.k], w=[xt.k])


def ln_lockstep(C, xs, gb, bb, sts, junk):
    P = C.P
    n = len(xs)
    for k in range(n):
        xt, st = xs[k], sts[k]
        P.op("dve", lambda e: e.tensor_reduce(st[:, 0:1], xt[:], AX.X, ALU.add), r=[xt.k], w=[st.k])
    for k in range(n):
        xt, st = xs[k], sts[k]
        P.op("dve", lambda e: e.tensor_scalar(st[:, 0:1], st[:, 0:1], 1.0 / D, None, ALU.mult), r=[st.k], w=[st.k])
    for k in range(n):
        xt, st = xs[k], sts[k]
        P.op("dve", lambda e: e.tensor_scalar(xt[:], xt[:], st[:, 0:1], None, ALU.subtract), r=[xt.k, st.k], w=[xt.k])
        P.op("act", lambda e: e.activation(out=junk[:], in_=xt[:], func=AF.Square, accum_out=st[:, 1:2]), r=[xt.k], w=[junk.k, st.k])
    for k in range(n):
        xt, st = xs[k], sts[k]
        P.op("act", lambda e: e.activation(out=st[:, 1:2], in_=st[:, 1:2], func=AF.Sqrt, bias=LN_EPS, scale=1.0 / D), r=[st.k], w=[st.k])
    for k in range(n):
        xt, st = xs[k], sts[k]
        P.op("dve", lambda e: e.reciprocal(st[:, 1:2], st[:, 1:2]), r=[st.k], w=[st.k])
    for k in range(n):
        xt, st = xs[k], sts[k]
        P.op("dve", lambda e: e.scalar_tensor_tensor(xt[:], xt[:], st[:, 1:2], gb[:], ALU.mult, ALU.mult), r=[xt.k, st.k, gb.k], w=[xt.k])
        P.op("pool", lambda e: e.tensor_tensor(xt[:], xt[:], bb[:], ALU.add), r=[xt.k, bb.k], w=[xt.k])


def tile_to_stage(C, xt, stage, j):
    P = C.P
    for half in range(2):
        pb = psb(C)
        for c4 in range(4):
            c = half * 4 + c4
            P.op("pe", lambda e: e.transpose(pb[:, c4 * 128:(c4 + 1) * 128], xt[:, c * 128:(c + 1) * 128], C.ident[:]),
                 r=[xt.k, C.ident_tok], w=[pb.k], inc=(c4 == 3))
        dst = stage[:, half * 4:(half + 1) * 4, j * 128:(j + 1) * 128]
        src = hv(pb[:, :], 4)
        if half == 0:
            P.op("act", lambda e: e.copy(dst, src), r=[pb.k], w=[stage.k])
        else:
            P.op("dve", lambda e: e.tensor_copy(dst, src), r=[pb.k], w=[stage.k])


def phase_outproj(C, w_out, srcYT, srcYT_toks, resid, resid_toks, lng, lnb, dstH, dstH_tok, dstHT, dstHT_tok, dbgname=None):
    nc, P, T, I = C.nc, C.P, C.T, C.I
    with ExitStack() as es:
        WO = mk(P, [128, 8, D], BF16, "WO", es)
        for c in range(8):
            P.dma("pool", WO[:, c, :], w_out[c * 128:(c + 1) * 128, :], w=[WO.k])
        gb = mk(P, [128, D], F32, "lng", es)
        bb = mk(P, [128, D], F32, "lnb", es)
        bcast_load(C, gb[:], lng, 128, gb.k)
        bcast_load(C, bb[:], lnb, 128, bb.k)
        yts = [mk(P, [128, 8, 512], BF16, "yt", es) for _ in range(2)]
        xts = [mk(P, [128, D], F32, "xres", es) for _ in range(8)]
        sts = [mk(P, [128, 2], F32, "lnst", es) for _ in range(8)]
        junk = mk(P, [128, D], BF16, "junk", es)
        stg = [mk(P, [128, 8, 512], BF16, "hstg", es) for _ in range(2)]
        pend = None

        def loads(s):
            P.dma("sp", yts[s % 2][:], XTv(srcYT)[:, :, s * 512:(s + 1) * 512], r=srcYT_toks(s), w=[yts[s % 2].k])
            for j in range(4):
                i = s * 4 + j
                xt = xts[(s % 2) * 4 + j]
                P.dma("sp", xt[:], resid[i * 128:(i + 1) * 128, :], r=resid_toks(i), w=[xt.k])

        loads(0)
        for s in range(C.NS):
            yt = yts[s % 2]
            sg_ = stg[s % 2]
            X = xts[(s % 2) * 4:(s % 2) * 4 + 4]
            S = sts[(s % 2) * 4:(s % 2) * 4 + 4]
            for j in range(4):
                xt = X[j]
                for half in range(2):
                    pb = psb(C)
                    for c in range(8):
                        P.op("pe", lambda e: e.matmul(pb[:, :], lhsT=yt[:, c, j * 128:(j + 1) * 128], rhs=WO[:, c, half * 512:(half + 1) * 512], start=(c == 0), stop=(c == 7)),
                             r=[yt.k, WO.k], w=[pb.k], inc=(c == 7))
                    P.op("dve", lambda e: e.scalar_tensor_tensor(xt[:, half * 512:(half + 1) * 512], xt[:, half * 512:(half + 1) * 512], DN_ALPHA, pb[:, :], ALU.mult, ALU.add),
                         r=[xt.k, pb.k], w=[xt.k])
            if pend is not None:
                pend()
            if s + 1 < C.NS:
                loads(s + 1)
            ln_lockstep(C, X, gb, bb, S, junk)
            for j in range(4):
                i = s * 4 + j
                P.dma("sp", dstH[i * 128:(i + 1) * 128, :], X[j][:], r=[X[j].k], w=[dstH_tok[i]])

            def pend(s=s, X=X, sg_=sg_):
                for j in range(4):
                    tile_to_stage(C, X[j], sg_, j)
                P.dma("sp", XTv(dstHT)[:, :, s * 512:(s + 1) * 512], sg_[:], r=[sg_.k], w=[dstHT_tok[s]])
        pend()
        P.barrier()


def phase_moe(C, l, srcH, srcH_tok, srcHT, srcHT_tok, dstX, dstX_tok, dstXT, dstXT_tok):
    nc, P, T, I = C.nc, C.P, C.T, C.I
    mm = ALU.mult
    with ExitStack() as es:
        WD = mk(P, [128, 32, D], BF16, "WD", es)
        for c4 in range(4):
            P.dma("sp", WD[:, c4 * 8:(c4 + 1) * 8, :], C.WD16[l].rearrange("p (c d) -> p c d", c=32)[:, c4 * 8:(c4 + 1) * 8, :], r=[C.WD16_tok[l]], w=[WD.k])
        RW = mk(P, [128, 8, NE], BF16, "RW", es)
        P.dma("pool", RW[:], I["router_w"].rearrange("(c p) e -> p c e", p=128), w=[RW.k])
        rbb = mk(P, [128, NE], F32, "rbb", es)
        bcast_load(C, rbb[:], I["router_bias"], 128, rbb.k)
        SEL = mk(P, [16, 16, 128], BF16, "SEL", es)
        P.dma("pool", SEL[:], I["c_sel"], w=[SEL.k])
        gb = mk(P, [128, D], F32, "lng", es)
        bb = mk(P, [128, D], F32, "lnb", es)
        bcast_load(C, gb[:], I["ln2_g"][l:l + 1, :], 128, gb.k)
        bcast_load(C, bb[:], I["ln2_b"][l:l + 1, :], 128, bb.k)
        hts = [mk(P, [128, 8, 512], BF16, "hT", es) for _ in range(2)]
        xts = [mk(P, [128, D], F32, "hres", es) for _ in range(4)]
        sts = [mk(P, [128, 2], F32, "lnst", es) for _ in range(4)]
        junk = mk(P, [128, D], BF16, "junk", es)
        stg = [mk(P, [128, 8, 512], BF16, "xstg", es) for _ in range(2)]
        actT = mk(P, [128, 32, 512], BF16, "actT", es)
        combT = mk(P, [16, 512], BF16, "combT", es)
        WGUs = [mk(P, [128, 2, 8, DE], BF16, "WGU", es) for _ in range(3)]
        sgl = [mk(P, [128, 512], F32, "sgl", es) for _ in range(2)]
        R4 = range(4)
        s_l = [mk(P, [128, NE], F32, "rs", es) for _ in R4]
        sel_l = [mk(P, [128, NE], F32, "rsel", es) for _ in R4]
        pr_l = [mk(P, [128, 4, 6], F32, "rpr", es) for _ in R4]
        gs_l = [mk(P, [128, 4], F32, "rgs", es) for _ in R4]
        t1_l = [mk(P, [128, 4], F32, "rt1", es) for _ in R4]
        m1_l = [mk(P, [128, 2], F32, "rm1", es) for _ in R4]
        selm_l = [mk(P, [128, NE], F32, "rselm", es) for _ in R4]
        sel2_l = [mk(P, [128, NE], F32, "rsel2", es) for _ in R4]
        comb_l = [mk(P, [128, NE], F32, "rcomb", es) for _ in R4]
        nwl = [0]

        def load_w(e):
            b = nwl[0] % 3
            nwl[0] += 1
            P.dma("sp", WGUs[b][:], C.WGU16[l][e].rearrange("p (t c f) -> p t c f", t=2, c=8), r=[C.WGU16_tok[l][e]], w=[WGUs[b].k])
            return WGUs[b]

        def g4(t):
            return t[:, :].rearrange("p (g e) -> p g e", g=4)

        def load_hT(s):
            P.dma("sp", hts[s % 2][:], XTv(srcHT)[:, :, s * 512:(s + 1) * 512], r=[srcHT_tok[s]], w=[hts[s % 2].k])

        def router_front(s):
            hT = hts[s % 2]
            for j in R4:
                plg = psb(C)
                s_ = s_l[j]
                for c in range(8):
                    P.op("pe", lambda e: e.matmul(plg[:, 0:NE], lhsT=hT[:, c, j * 128:(j + 1) * 128], rhs=RW[:, c, :], start=(c == 0), stop=(c == 7)),
                         r=[hT.k, RW.k], w=[plg.k], inc=(c == 7))
                P.op("act", lambda e: e.activation(out=s_[:], in_=plg[:, 0:NE], func=AF.Sigmoid), r=[plg.k], w=[s_.k])

            def step(fn):
                for j in R4:
                    fn(s_l[j], sel_l[j], pr_l[j], gs_l[j], t1_l[j], m1_l[j], selm_l[j], sel2_l[j], comb_l[j])
            step(lambda s_, sel, pr, gs, t1, m1, selm, sel2, comb: P.op("dve", lambda e: e.tensor_tensor(sel[:], s_[:], rbb[:], ALU.add), r=[s_.k, rbb.k], w=[sel.k]))
            step(lambda s_, sel, pr, gs, t1, m1, selm, sel2, comb: P.op("dve", lambda e: e.tensor_tensor(pr[:, :, 0:3], g4(sel)[:, :, 0:3], g4(sel)[:, :, 1:4], ALU.add), r=[sel.k], w=[pr.k]))
            step(lambda s_, sel, pr, gs, t1, m1, selm, sel2, comb: P.op("dve", lambda e: e.tensor_tensor(pr[:, :, 3:5], g4(sel)[:, :, 0:2], g4(sel)[:, :, 2:4], ALU.add), r=[sel.k], w=[pr.k]))
            step(lambda s_, sel, pr, gs, t1, m1, selm, sel2, comb: P.op("dve", lambda e: e.tensor_tensor(pr[:, :, 5:6], g4(sel)[:, :, 0:1], g4(sel)[:, :, 3:4], ALU.add), r=[sel.k], w=[pr.k]))
            step(lambda s_, sel, pr, gs, t1, m1, selm, sel2, comb: P.op("dve", lambda e: e.tensor_reduce(gs[:], pr[:], AX.X, ALU.max), r=[pr.k], w=[gs.k]))
            step(lambda s_, sel, pr, gs, t1, m1, selm, sel2, comb: P.op("dve", lambda e: e.tensor_reduce(m1[:, 0:1], gs[:], AX.X, ALU.max), r=[gs.k], w=[m1.k]))
            step(lambda s_, sel, pr, gs, t1, m1, selm, sel2, comb: P.op("dve", lambda e: e.tensor_scalar(gs[:], gs[:], m1[:, 0:1], None, ALU.is_ge), r=[gs.k, m1.k], w=[gs.k]))
            step(lambda s_, sel, pr, gs, t1, m1, selm, sel2, comb: P.op("dve", lambda e: e.tensor_scalar(t1[:], gs[:], -1.0, 1e30, ALU.add, ALU.mult), r=[gs.k], w=[t1.k]))
            step(lambda s_, sel, pr, gs, t1, m1, selm, sel2, comb: P.op("dve", lambda e: e.tensor_tensor(g4(selm), g4(sel), gs[:, :].unsqueeze(2).to_broadcast([128, 4, 4]), mm), r=[sel.k, gs.k], w=[selm.k]))
            step(lambda s_, sel, pr, gs, t1, m1, selm, sel2, comb: P.op("dve", lambda e: e.tensor_tensor(g4(selm), g4(selm), t1[:, :].unsqueeze(2).to_broadcast([128, 4, 4]), ALU.add), r=[selm.k, t1.k], w=[selm.k]))
            step(lambda s_, sel, pr, gs, t1, m1, selm, sel2, comb: P.op("dve", lambda e: e.tensor_reduce(m1[:, 0:1], selm[:], AX.X, ALU.max), r=[selm.k], w=[m1.k]))
            step(lambda s_, sel, pr, gs, t1, m1, selm, sel2, comb: P.op("dve", lambda e: e.tensor_scalar(sel2[:], selm[:], m1[:, 0:1], None, ALU.is_ge), r=[selm.k, m1.k], w=[sel2.k]))
            step(lambda s_, sel, pr, gs, t1, m1, selm, sel2, comb: P.op("dve", lambda e: e.scalar_tensor_tensor(sel2[:], sel2[:], -1e30, selm[:], mm, ALU.add), r=[sel2.k, selm.k], w=[sel2.k]))
            step(lambda s_, sel, pr, gs, t1, m1, selm, sel2, comb: P.op("dve", lambda e: e.tensor_reduce(m1[:, 1:2], sel2[:], AX.X, ALU.max), r=[sel2.k], w=[m1.k]))
            step(lambda s_, sel, pr, gs, t1, m1, selm, sel2, comb: P.op("dve", lambda e: e.tensor_scalar(sel2[:], selm[:], m1[:, 1:2], None, ALU.is_ge), r=[selm.k, m1.k], w=[sel2.k]))
            step(lambda s_, sel, pr, gs, t1, m1, selm, sel2, comb: P.op("dve", lambda e: e.tensor_tensor(comb[:], s_[:], sel2[:], mm), r=[s_.k, sel2.k], w=[comb.k]))
            step(lambda s_, sel, pr, gs, t1, m1, selm, sel2, comb: P.op("dve", lambda e: e.tensor_reduce(m1[:, 0:1], comb[:], AX.X, ALU.add), r=[comb.k], w=[m1.k]))
            step(lambda s_, sel, pr, gs, t1, m1, selm, sel2, comb: P.op("dve", lambda e: e.reciprocal(m1[:, 0:1], m1[:, 0:1]), r=[m1.k], w=[m1.k]))
            step(lambda s_, sel, pr, gs, t1, m1, selm, sel2, comb: P.op("dve", lambda e: e.tensor_scalar(comb[:], comb[:], m1[:, 0:1], None, mm), r=[comb.k, m1.k], w=[comb.k]))

        def router_back(s):
            for j in R4:
                comb = comb_l[j]
                pct = psb(C)
                P.op("pe", lambda e: e.transpose(pct[0:16, 0:128], comb[:, :], C.ident[:]), r=[comb.k, C.ident_tok], w=[pct.k])
                P.op("act", lambda e: e.copy(combT[:, j * 128:(j + 1) * 128], pct[0:16, 0:128]), r=[pct.k], w=[combT.k])

        def load_x(s):
            for j in R4:
                i = s * 4 + j
                P.dma("sp", xts[j][:], srcH[i * 128:(i + 1) * 128, :], r=[srcH_tok[i]], w=[xts[j].k])

        def make_pend(s):
            xs_ = stg[s % 2]

            def pend():
                for j in R4:
                    tile_to_stage(C, xts[j], xs_, j)
                P.dma("sp", XTv(dstXT)[:, :, s * 512:(s + 1) * 512], xs_[:], r=[xs_.k], w=[dstXT_tok[s]])
            return pend

        pend = None
        pre = []
        load_hT(0)
        router_front(0)
        router_back(0)
        for s in range(C.NS):
            hT = hts[s % 2]
            if s + 1 < C.NS:
                load_hT(s + 1)
            for ex in range(NE):
                WGU = pre.pop(0) if pre else load_w(ex)
                pcb = psb(C)
                P.op("pe", lambda e: e.matmul(pcb[:, :], lhsT=SEL[:, ex, :], rhs=combT[:, :], start=True, stop=True), r=[SEL.k, combT.k], w=[pcb.k])
                for f in range(2):
                    pG, pU = psb(C), psb(C)
                    for pb, ti in ((pG, 0), (pU, 1)):
                        for c in range(8):
                            P.op("pe", lambda e: e.matmul(pb[:, :], lhsT=WGU[:, ti, c, f * 128:(f + 1) * 128], rhs=hT[:, c, :], start=(c == 0), stop=(c == 7)),
                                 r=[WGU.k, hT.k], w=[pb.k], inc=(c == 7))
                    sg_ = sgl[(ex * 2 + f) % 2]
                    P.op("act", lambda e: e.activation(out=sg_[:], in_=pG[:, :], func=AF.Silu), r=[pG.k], w=[sg_.k])
                    P.op("dve", lambda e: e.tensor_tensor(sg_[:], sg_[:], pU[:, :], mm), r=[sg_.k, pU.k], w=[sg_.k])
                    P.op("dve", lambda e: e.tensor_tensor(actT[:, ex * 2 + f, :], sg_[:], pcb[:, :], mm), r=[sg_.k, pcb.k], w=[actT.k])
                if ex == 3:
                    if pend is not None:
                        pend()
                        pend = None
                    load_x(s)
            if s + 1 < C.NS:
                pre.extend(load_w(e_) for e_ in range(3))
            if s + 1 < C.NS:
                router_front(s + 1)
            for j in R4:
                xt = xts[j]
                for half in range(2):
                    pb = psb(C)
                    for c in range(32):
                        P.op("pe", lambda e: e.matmul(pb[:, :], lhsT=actT[:, c, j * 128:(j + 1) * 128], rhs=WD[:, c, half * 512:(half + 1) * 512], start=(c == 0), stop=(c == 31)),
                             r=[actT.k, WD.k], w=[pb.k], inc=(c == 31))
                    P.op("dve", lambda e: e.scalar_tensor_tensor(xt[:, half * 512:(half + 1) * 512], xt[:, half * 512:(half + 1) * 512], DN_ALPHA, pb[:, :], ALU.mult, ALU.add),
                         r=[xt.k, pb.k], w=[xt.k])
            if s + 1 < C.NS:
                router_back(s + 1)
            ln_lockstep(C, xts, gb, bb, sts, junk)
            for j in R4:
                i = s * 4 + j
                P.dma("sp", dstX[i * 128:(i + 1) * 128, :], xts[j][:], r=[xts[j].k], w=[dstX_tok[i]])
            if dstXT is not None:
                pend = make_pend(s)
        if pend is not None:
            pend()
        P.barrier()


def rope_consts(T):
    pos = np.arange(T, dtype=np.float64)
    c = {}
    for name, half in (("k", 64), ("i", 32)):
        inv = 10000.0 ** (-np.arange(half, dtype=np.float64) / half)
        ang = (pos.astype(np.float32)[:, None] * inv.astype(np.float32)[None, :]).astype(np.float32).astype(np.float64)
        cs = np.cos(ang).astype(np.float32).reshape(T // 128, 128, half).transpose(1, 0, 2)
        sn = np.sin(ang).astype(np.float32).reshape(T // 128, 128, half).transpose(1, 0, 2)
        c["cos_" + name] = np.ascontiguousarray(cs)
        c["sin_" + name] = np.ascontiguousarray(sn)
    q = np.arange(128)[:, None]
    s = np.arange(128)[None, :]
    c["cbias"] = np.where(s <= q, 0.0, -1e30).astype(np.float32)
    c["halfpow"] = (0.5 ** np.arange(1, 33, dtype=np.float64)).astype(np.float32).reshape(1, 32)
    c["tiebias"] = (-1e-6 * np.arange(T, dtype=np.float64)).astype(np.float32).reshape(1, T)
    return c


def rope_tm(C, dst_ap, dst_k, src, src_k, cosb, sinb, nh, half, ta_ap, ta_k, tb_ap, tb_k, rdeps):
    P = C.P
    mm = ALU.mult
    n = nh * 2
    s3 = src.rearrange("p (n f) -> p n f", n=n)
    cb = cosb.unsqueeze(1).to_broadcast([128, n, half])
    sb_ = sinb.unsqueeze(1).to_broadcast([128, n, half])
    a3 = ta_ap.rearrange("p (n f) -> p n f", n=n)
    b3 = tb_ap.rearrange("p (n f) -> p n f", n=n)
    P.op("dve", lambda e: e.tensor_tensor(a3, s3, cb, mm), r=[src_k] + rdeps, w=[ta_k])
    P.op("dve", lambda e: e.tensor_tensor(b3, s3, sb_, mm), r=[src_k] + rdeps, w=[tb_k])
    a4 = ta_ap.rearrange("p (h t f) -> p h t f", h=nh, t=2)
    b4 = tb_ap.rearrange("p (h t f) -> p h t f", h=nh, t=2)
    d4 = dst_ap.rearrange("p (h t f) -> p h t f", h=nh, t=2)
    P.op("pool", lambda e: e.tensor_tensor(d4[:, :, 0, :], a4[:, :, 0, :], b4[:, :, 1, :], ALU.subtract), r=[ta_k, tb_k], w=[dst_k])
    P.op("pool", lambda e: e.tensor_tensor(d4[:, :, 1, :], a4[:, :, 1, :], b4[:, :, 0, :], ALU.add), r=[ta_k, tb_k], w=[dst_k])


def phase_dsa(C):
    nc, P, T, I = C.nc, C.P, C.T, C.I
    mm = ALU.mult
    KT = min(256, T // 4)
    NIT = 25
    SCALE = 128 ** -0.5
    C.bank_pool = [0, 1, 2, 3, 4]
    with ExitStack() as es:
        def wt(name, shape, dt=F32):
            return mk(P, list(shape), dt, name, es)
        WQ = wt("WQ", (128, 8, 1024), BF16)
        WR = wt("WR", (128, 8, 580), BF16)
        for c in range(8):
            P.dma("pool", WQ[:, c, :], I["w_in_odd"][c * 128:(c + 1) * 128, 0:1024], w=[WQ.k])
            P.dma("pool", WR[:, c, :], I["w_in_odd"][c * 128:(c + 1) * 128, 1024:1604], w=[WR.k])
        RTs = [[wt("CK", (128, 4, 64)), wt("SK", (128, 4, 64)), wt("CI", (128, 4, 32)), wt("SI", (128, 4, 32))] for _ in range(2)]

        def load_tables(s):
            tl = RTs[s % 2]
            for t_, nm in zip(tl, ("c_cos_k", "c_sin_k", "c_cos_i", "c_sin_i")):
                P.dma("sp", t_[:], I[nm][:, s * 4:(s + 1) * 4, :], w=[t_.k])
            return tl
        BIAS = wt("BIAS", (128, T))
        bcast_load(C, BIAS[:], I["c_tiebias"], 128, BIAS.k)
        CB = wt("CB", (128, 128))
        P.dma("sp", CB[:], I["c_cbias"], w=[CB.k])
        ikg, ikb = wt("ikg", (128, 64)), wt("ikb", (128, 64))
        bcast_load(C, ikg[:], I["c_ik_ln_g"], 128, ikg.k)
        bcast_load(C, ikb[:], I["c_ik_ln_b"], 128, ikb.k)
        kT = wt("kT", (128, T), BF16)
        ikT = wt("ikT", (128, T), BF16)
        Vx = wt("Vx", (128, C.NT, 129), BF16)
        P.op("dve", lambda e: e.memset(Vx[:, :, 128:129], 1.0), w=[Vx.k])
        xTs = [wt("xTd", (128, 8, 512), BF16) for _ in range(2)]
        ta, tb = wt("ropeA", (128, 512)), wt("ropeB", (128, 512))
        kr = wt("kr", (128, 128))
        ikr = wt("ikr", (128, 128))
        st2 = wt("st2", (128, 2))
        for s in range(C.NS):
            xT = xTs[s % 2]
            P.dma("sp", xT[:], XTv(C.XT1)[:, :, s * 512:(s + 1) * 512], r=[C.XT1_tok[s]], w=[xT.k])
            CK, SK, CI, SI = load_tables(s)
            for j in range(4):
                i = s * 4 + j
                pb = psb(C)
                for c in range(8):
                    P.op("pe", lambda e: e.matmul(pb[:, 0:256], lhsT=xT[:, c, j * 128:(j + 1) * 128], rhs=WR[:, c, 0:256], start=(c == 0), stop=(c == 7)),
                         r=[xT.k, WR.k], w=[pb.k], inc=False)
                for c in range(8):
                    P.op("pe", lambda e: e.matmul(pb[:, 256:320], lhsT=xT[:, c, j * 128:(j + 1) * 128], rhs=WR[:, c, 512:576], start=(c == 0), stop=(c == 7)),
                         r=[xT.k, WR.k], w=[pb.k], inc=(c == 7))
                rope_tm(C, kr[:, :], kr.k, pb[:, 0:128], pb.k, CK[:, j, :], SK[:, j, :], 1, 64, ta[:, 0:128], ta.k, tb[:, 0:128], tb.k, [CK.k, SK.k])
                P.op("act", lambda e: e.copy(Vx[:, i, 0:128], pb[:, 128:256]), r=[pb.k], w=[Vx.k])
                P.op("dve", lambda e: e.tensor_reduce(st2[:, 0:1], pb[:, 256:320], AX.X, ALU.add), r=[pb.k], w=[st2.k])
                P.op("dve", lambda e: e.tensor_scalar(st2[:, 0:1], st2[:, 0:1], 1.0 / 64, None, mm), r=[st2.k], w=[st2.k])
                P.op("dve", lambda e: e.tensor_scalar(ikr[:, 0:64], pb[:, 256:320], st2[:, 0:1], None, ALU.subtract), r=[pb.k, st2.k], w=[ikr.k])
                P.op("act", lambda e: e.activation(out=ikr[:, 64:128], in_=ikr[:, 0:64], func=AF.Square, accum_out=st2[:, 1:2]), r=[ikr.k], w=[ikr.k, st2.k])
                P.op("act", lambda e: e.activation(out=st2[:, 1:2], in_=st2[:, 1:2], func=AF.Sqrt, bias=LN_EPS, scale=1.0 / 64), r=[st2.k], w=[st2.k])
                P.op("dve", lambda e: e.reciprocal(st2[:, 1:2], st2[:, 1:2]), r=[st2.k], w=[st2.k])
                P.op("dve", lambda e: e.scalar_tensor_tensor(ikr[:, 0:64], ikr[:, 0:64], st2[:, 1:2], ikg[:], mm, mm), r=[ikr.k, st2.k, ikg.k], w=[ikr.k])
                P.op("dve", lambda e: e.tensor_tensor(ikr[:, 64:128], ikr[:, 0:64], ikb[:], ALU.add), r=[ikr.k, ikb.k], w=[ikr.k])
                ikn = TL(ikr.t, ikr.k)
                rope_src = ikr[:, 64:128]
                n = 2
                s3 = rope_src.rearrange("p (n f) -> p n f", n=n)
                cb = CI[:, j, :].unsqueeze(1).to_broadcast([128, n, 32])
                sb_ = SI[:, j, :].unsqueeze(1).to_broadcast([128, n, 32])
                a3 = ta[:, 0:64].rearrange("p (n f) -> p n f", n=n)
                b3 = tb[:, 0:64].rearrange("p (n f) -> p n f", n=n)
                P.op("dve", lambda e: e.tensor_tensor(a3, s3, cb, mm), r=[ikr.k, CI.k], w=[ta.k])
                P.op("dve", lambda e: e.tensor_tensor(b3, s3, sb_, mm), r=[ikr.k, SI.k], w=[tb.k])
                P.op("pool", lambda e: e.tensor_tensor(ikr[:, 0:32], ta[:, 0:32], tb[:, 32:64], ALU.subtract), r=[ta.k, tb.k], w=[ikr.k])
                P.op("pool", lambda e: e.tensor_tensor(ikr[:, 32:64], ta[:, 32:64], tb[:, 0:32], ALU.add), r=[ta.k, tb.k], w=[ikr.k])
                P.op("pool", lambda e: e.tensor_copy(ikr[:, 64:128], ikr[:, 0:64]), r=[ikr.k], w=[ikr.k])
                pt = psb(C)
                P.op("pe", lambda e: e.transpose(pt[:, 0:128], kr[:, :], C.ident[:]), r=[kr.k, C.ident_tok], w=[pt.k], inc=False)
                P.op("pe", lambda e: e.transpose(pt[:, 128:256], ikr[:, :], C.ident[:]), r=[ikr.k, C.ident_tok], w=[pt.k])
                P.op("act", lambda e: e.copy(kT[:, i * 128:(i + 1) * 128], pt[:, 0:128]), r=[pt.k], w=[kT.k])
                P.op("act", lambda e: e.mul(ikT[:, i * 128:(i + 1) * 128], pt[:, 128:256], 0.125), r=[pt.k], w=[ikT.k])
        SC = wt("SC", (128, T))
        MASKs = [wt("MASK", (128, T)) for _ in range(2)]
        junk = wt("junkd", (128, T), BF16)
        qr = wt("qr", (128, 1024))
        iqr = wt("iqr", (128, 256))
        qTs = [wt("qT", (128, 8, 128), BF16) for _ in range(3)]
        iqT = wt("iqT", (128, 2, 128), BF16)
        iws = wt("iws", (128, 4))
        rl = [wt("rl%d" % n, (128, 512)) for n in range(2)]
        bs = wt("bs", (128, 8))
        Dk = wt("Dk", (128, NIT))
        HK = wt("HK", (128, NIT))
        bcast_load(C, HK[:], I["c_halfpow"][0:1, 0:NIT], 128, HK.k)
        V2 = [wt("V2_%d" % n, (128, 2)) for n in range(2)]
        W2 = wt("W2s", (128, 2))
        Ek = wt("Ek", (128, NIT, 2))
        P.op("dve", lambda e: e.memset(Ek[:], 0.0), w=[Ek.k])
        mT4 = [wt("mT4_%d" % n, (128, 4, 128), BF16) for n in range(2)]
        pTs = [wt("pT%d" % n, (128, 4, 128), BF16) for n in range(4)]
        o_ = wt("od", (128, 1024))
        rs8 = wt("rs8", (128, 8))
        ostg = [wt("ostg", (128, 8, 512), BF16) for _ in range(2)]
        accb = [TL(*C.banks[b]) for b in (5, 6, 7)]
        MBIG = 30000.0
        identb = wt("identb", (128, 128), BF16)
        P.op("dve", lambda e: e.tensor_copy(identb[:], C.ident[:]), r=[C.ident_tok], w=[identb.k])
        acc_of = [(0, 0), (0, 1), (0, 2), (1, 0), (1, 1), (1, 2), (2, 0), (2, 1)]
        def qproj(s, j):
            i = s * 4 + j
            xT = xTs[s % 2]
            qT = qTs[i % 3]
            if j == 0:
                P.dma("sp", xT[:], XTv(C.XT1)[:, :, s * 512:(s + 1) * 512], r=[C.XT1_tok[s]], w=[xT.k])
                load_tables(s)
            CK, SK, CI, SI = RTs[s % 2]
            pq = [psb(C), psb(C)]
            for half in range(2):
                for c in range(8):
                    P.op("pe", lambda e: e.matmul(pq[half][:, :], lhsT=xT[:, c, j * 128:(j + 1) * 128], rhs=WQ[:, c, half * 512:(half + 1) * 512], start=(c == 0), stop=(c == 7)),
                         r=[xT.k, WQ.k], w=[pq[half].k], inc=(c == 7))
            piq = psb(C)
            for c in range(8):
                P.op("pe", lambda e: e.matmul(piq[:, 0:256], lhsT=xT[:, c, j * 128:(j + 1) * 128], rhs=WR[:, c, 256:512], start=(c == 0), stop=(c == 7)),
                     r=[xT.k, WR.k], w=[piq.k], inc=False)
            for c in range(8):
                P.op("pe", lambda e: e.matmul(piq[:, 256:260], lhsT=xT[:, c, j * 128:(j + 1) * 128], rhs=WR[:, c, 576:580], start=(c == 0), stop=(c == 7)),
                     r=[xT.k, WR.k], w=[piq.k], inc=(c == 7))
            for half in range(2):
                hs = slice(half * 512, (half + 1) * 512)
                rope_tm(C, qr[:, hs], qr.k, pq[half][:, :], pq[half].k, CK[:, j, :], SK[:, j, :], 4, 64,
                        ta[:, 0:512], ta.k, tb[:, 0:512], tb.k, [CK.k, SK.k])
            rope_tm(C, iqr[:, :], iqr.k, piq[:, 0:256], piq.k, CI[:, j, :], SI[:, j, :], 4, 32, ta[:, 0:256], ta.k, tb[:, 0:256], tb.k, [CI.k, SI.k])
            P.op("act", lambda e: e.mul(iws[:], piq[:, 256:260], 0.5), r=[piq.k], w=[iws.k])
            for half in range(2):
                pb = psb(C)
                for c4 in range(4):
                    h = half * 4 + c4
                    P.op("pe", lambda e: e.transpose(pb[:, c4 * 128:(c4 + 1) * 128], qr[:, h * 128:(h + 1) * 128], C.ident[:]), r=[qr.k, C.ident_tok], w=[pb.k], inc=(c4 == 3))
                P.op("act", lambda e: e.copy(qT[:, half * 4:(half + 1) * 4, :], hv(pb[:, :], 4)), r=[pb.k], w=[qT.k])
            pb = psb(C)
            for c2 in range(2):
                P.op("pe", lambda e: e.transpose(pb[:, c2 * 128:(c2 + 1) * 128], iqr[:, c2 * 128:(c2 + 1) * 128], C.ident[:]), r=[iqr.k, C.ident_tok], w=[pb.k], inc=(c2 == 1))
            P.op("act", lambda e: e.copy(iqT[:], hv(pb[:, 0:256], 2)), r=[pb.k], w=[iqT.k])

        NB = C.NS * 4

        def qproj_next(i):
            if i + 1 < NB:
                qproj((i + 1) // 4, (i + 1) % 4)

        def qblock(s, j):
            if True:
                i = s * 4 + j
                L = (i + 1) * 128
                og = ostg[s % 2]
                qT = qTs[i % 3]
                MASK = MASKs[i % 2]
                cast_some(C, 1, 2)
                if i == 0:
                    qproj(0, 0)
                yield "F"
                for k0 in range(0, L, 512):
                    kw = min(512, L - k0)
                    for h in range(4):
                        ph = psb(C)
                        pl = (h % 2) * 64
                        P.op("pe", lambda e: e.matmul(ph[:, 0:kw], lhsT=iqT[pl:pl + 64, h // 2, :], rhs=ikT[pl:pl + 64, k0:k0 + kw], start=True, stop=True),
                             r=[iqT.k, ikT.k], w=[ph.k])
                        r_ = rl[h % 2]
                        P.op("act", lambda e: e.activation(out=r_[:, 0:kw], in_=ph[:, 0:kw], func=AF.Relu), r=[ph.k], w=[r_.k])
                        if h == 0:
                            P.op("dve", lambda e: e.scalar_tensor_tensor(SC[:, k0:k0 + kw], r_[:, 0:kw], iws[:, 0:1], BIAS[:, k0:k0 + kw], mm, ALU.add), r=[r_.k, iws.k, BIAS.k], w=[SC.k])
                        else:
                            P.op("dve", lambda e: e.scalar_tensor_tensor(SC[:, k0:k0 + kw], r_[:, 0:kw], iws[:, h:h + 1], SC[:, k0:k0 + kw], mm, ALU.add),
                                 r=[r_.k, iws.k, SC.k], w=[SC.k])
                    yield "F"
                if L > KT:
                    P.op("dve", lambda e: e.tensor_reduce(bs[:, 1:2], SC[:, 0:L], AX.X, ALU.max, apply_absolute_value=True), r=[SC.k], w=[bs.k])
                    P.op("dve", lambda e: e.tensor_scalar(bs[:, 0:1], bs[:, 1:2], -1.0, -1.0, mm, ALU.add), r=[bs.k], w=[bs.k])
                    P.op("dve", lambda e: e.tensor_scalar(bs[:, 1:2], bs[:, 1:2], 2.0, 2.0, mm, ALU.add), r=[bs.k], w=[bs.k])
                    P.op("dve", lambda e: e.tensor_scalar(Dk[:], HK[:], bs[:, 1:2], None, mm), r=[bs.k, HK.k], w=[Dk.k])
                P.op("pool", lambda e: e.tensor_tensor(SC[:, i * 128:L], SC[:, i * 128:L], CB[:], ALU.add), r=[SC.k, CB.k], w=[SC.k])
                if L > KT:
                    P.op("dve", lambda e: e.tensor_copy(Ek[:, 0:NIT - 1, 1:2], Dk[:, 1:NIT].unsqueeze(2)), r=[Dk.k], w=[Ek.k])
                    P.op("dve", lambda e: e.tensor_copy(V2[0][:, 0:1], bs[:, 0:1]), r=[bs.k], w=[V2[0].k])
                    P.op("dve", lambda e: e.tensor_tensor(V2[0][:, 1:2], bs[:, 0:1], Dk[:, 0:1], ALU.add), r=[bs.k, Dk.k], w=[V2[0].k])
                    for it in range(NIT):
                        Vc, Vn = V2[it % 2], V2[(it + 1) % 2]
                        P.op("dve", lambda e: e.tensor_scalar(junk[:, 0:L], SC[:, 0:L], Vc[:, 1:2], None, ALU.is_gt, ALU.add, accum_out=bs[:, 3:4]),
                             r=[SC.k, Vc.k], w=[junk.k, bs.k])
                        P.op("dve", lambda e: e.scalar_tensor_tensor(W2[:], bs[:, 3:4].to_broadcast([128, 2]), float(KT) - 0.5, Dk[:, it:it + 1].to_broadcast([128, 2]), ALU.is_gt, mm),
                             r=[bs.k, Dk.k], w=[W2.k])
                        P.op("dve", lambda e: e.scalar_tensor_tensor(Vn[:], W2[:], Vc[:, 0:1], Ek[:, it, :], ALU.add, ALU.add), r=[W2.k, Vc.k, Ek.k], w=[Vn.k])
                        if it == 4:
                            qproj_next(i)
                        yield "F"
                    Vf = V2[NIT % 2]
                    P.op("dve", lambda e: e.tensor_scalar(MASK[:, 0:L], SC[:, 0:L], Vf[:, 0:1], None, ALU.is_le), r=[SC.k, Vf.k], w=[MASK.k])
                else:
                    P.op("dve", lambda e: e.tensor_scalar(MASK[:, 0:L], SC[:, 0:L], -1e29, None, ALU.is_le), r=[SC.k], w=[MASK.k])
                    qproj_next(i)
                if "dbg_mask" in C.dbg:
                    P.dma("sp", C.dbg["dbg_mask"][i * 128:(i + 1) * 128, 0:L], MASK[:, 0:L], r=[MASK.k])
                    P.dma("sp", C.dbg["dbg_sc"][i * 128:(i + 1) * 128, 0:L], SC[:, 0:L], r=[SC.k])
                yield "END_FRONT"
                units = [(st, hg) for st in range(i + 1) for hg in range(2)]

                def stage1(st, hg):
                    if hg == 0 and st % 4 == 0:
                        n4 = min(4, i + 1 - st)
                        m4 = mT4[(st // 4) % 2]
                        pb = psb(C)
                        for u in range(n4):
                            P.op("pe", lambda e: e.transpose(pb[:, u * 128:(u + 1) * 128], MASK[:, (st + u) * 128:(st + u + 1) * 128], C.ident[:]),
                                 r=[MASK.k, C.ident_tok], w=[pb.k], inc=(u == n4 - 1))
                        P.op("act", lambda e: e.mul(m4[:, 0:n4, :], hv(pb[:, :], 4)[:, 0:n4, :], -MBIG), r=[pb.k], w=[m4.k])
                    m4 = mT4[(st // 4) % 2]
                    pl_ = psb(C)
                    P.op("pe", lambda e: e.matmul(pl_[:, :], lhsT=identb[:, :], rhs=m4[:, st % 4, :].unsqueeze(1).to_broadcast([128, 4, 128]), start=True, stop=False),
                         r=[identb.k, m4.k], w=[pl_.k], inc=False)
                    P.op("pe", lambda e: e.matmul(pl_[:, :], lhsT=kT[:, st * 128:(st + 1) * 128], rhs=qT[:, hg * 4:(hg + 1) * 4, :].rearrange("p h q -> p (h q)"), start=False, stop=True),
                         r=[kT.k, qT.k], w=[pl_.k])
                    pT = pTs[hg * 2 + st % 2]
                    P.op("act", lambda e: e.activation(out=pT[:], in_=hv(pl_[:, :], 4), func=AF.Exp, scale=SCALE), r=[pl_.k], w=[pT.k])

                def stage2(st, hg):
                    pT = pTs[hg * 2 + st % 2]
                    for hh in range(4):
                        h = hg * 4 + hh
                        ab, slot = acc_of[h]
                        P.op("pe", lambda e: e.matmul(accb[ab][:, slot * 129:(slot + 1) * 129], lhsT=pT[:, hh, :], rhs=Vx[:, st, :], start=(st == 0 and slot == 0), stop=(st == i), skip_group_check=True),
                             r=[pT.k, Vx.k], w=[accb[ab].k], inc=(hh == 3))

                stage1(*units[0])
                for n in range(len(units)):
                    if n + 1 < len(units):
                        stage1(*units[n + 1])
                    stage2(*units[n])
                    if units[n][1] == 1:
                        yield "B"
                for h in range(8):
                    ab, slot = acc_of[h]
                    P.op("dve", lambda e: e.reciprocal(rs8[:, h:h + 1], accb[ab][:, slot * 129 + 128:slot * 129 + 129]), r=[accb[ab].k], w=[rs8.k])
                    P.op("act", lambda e: e.activation(out=o_[:, h * 128:(h + 1) * 128], in_=accb[ab][:, slot * 129:slot * 129 + 128], func=AF.Copy, scale=rs8[:, h:h + 1]),
                         r=[accb[ab].k, rs8.k], w=[o_.k])
                if "dbg_dsa" in C.dbg:
                    P.dma("sp", C.dbg["dbg_dsa"][i * 128:(i + 1) * 128, :], o_[:], r=[o_.k])
                tile_to_stage(C, o_, og, j)
                if j == 3:
                    P.dma("sp", XTv(C.YT)[:, :, s * 512:(s + 1) * 512], og[:], r=[og.k], w=[C.YT_tok[0][s], C.YT_tok[1][s]])

        pipeline2([(lambda s=s, j=j: qblock(s, j)) for s in range(C.NS) for j in range(4)], interleave=C.dsa_interleave)
        P.barrier()
    C.bank_pool = list(range(8))


class _View:
    def __init__(self, tl, sl):
        self.t = _Sl(tl.t, sl)
        self.k = tl.k

    def __getitem__(self, idx):
        return self.t[idx]


class _Sl:
    def __init__(self, t, sl):
        self.base = t
        self.sl = sl

    def __getitem__(self, idx):
        rows, cols = idx
        assert cols == slice(None)
        return self.base[rows, self.sl]


_NC_CACHE = {}


def _in_map(inputs, b, T, consts):
    m = {"x": np.ascontiguousarray(inputs["x"][b, :T], dtype=np.float32)}
    for k, a in inputs.items():
        if k == "x":
            continue
        a = np.asarray(a, dtype=np.float32)
        if k in ("router_w", "exp_w_gate", "exp_w_up", "exp_w_down", "ln1_g", "ln1_b", "ln2_g", "ln2_b"):
            m[k] = np.ascontiguousarray(a)
        elif k == "router_bias":
            m[k] = np.ascontiguousarray(a.reshape(1, -1))
        elif k == "a_r_k":
            m[k] = np.ascontiguousarray(a.reshape(1, 512))
        elif a.ndim == 3:
            m[k] = np.ascontiguousarray(a[0])
        elif a.ndim == 2:
            m[k] = np.ascontiguousarray(a[0:1])
    m.update(consts)
    return m


def kernel(**inputs):
    x = np.asarray(inputs["x"])
    B, T, _ = x.shape
    if T not in _NC_CACHE:
        _NC_CACHE[T] = build(T)
    nc = _NC_CACHE[T]
    consts = {"c_" + k: v for k, v in host_consts(T).items()}
    consts.update({"c_" + k: v for k, v in rope_consts(T).items()})
    in_maps = [_in_map(inputs, b, T, consts) for b in range(B)]
    res = run_bass_kernel_spmd(nc, in_maps, core_ids=list(range(B)))
    out = np.stack([np.asarray(res.results[b]["out"], dtype=np.float32) for b in range(B)], 0)
    return out
```

```python
import numpy as np
import ml_dtypes
from contextlib import ExitStack
import concourse.bass as bass
import concourse.mybir as mybir
from concourse.bass_utils import run_bass_kernel_spmd

F32 = mybir.dt.float32
BF16 = mybir.dt.bfloat16
AF = mybir.ActivationFunctionType
ALU = mybir.AluOpType
AX = mybir.AxisListType

D = 1024
A_COLS = 1792
B_COLS = 1552
EVEN_COLS = 3344
ODD_COLS = 1604
NE = 16
DE = 256
DN_ALPHA = 4 ** 0.25
LN_EPS = 1e-5
DEC = 0.6065306597126334
DSA_INTERLEAVE = True
RWKV_INTERLEAVE = False


class Tok:
    __slots__ = ("w", "r")

    def __init__(self):
        self.w = None
        self.r = {}


class Eng:
    def __init__(self, name, h, sem):
        self.name = name
        self.h = h
        self.sem = sem
        self.cnt = 0
        self.waited = {}


class Prog:
    NSLOT = 8

    def __init__(self, nc, es):
        self.nc = nc
        self.es = es
        self.E = {}
        for name, h in (("pe", nc.tensor), ("act", nc.scalar), ("dve", nc.vector),
                        ("pool", nc.gpsimd), ("sp", nc.sync)):
            sem = es.enter_context(nc.semaphore("sem_" + name))
            self.E[name] = Eng(name, h, sem)
        self.slots = {}
        self.dn = {}
        for q in ("sp", "pool", "act"):
            self.slots[q] = [[es.enter_context(nc.semaphore("dq_%s%d" % (q, i))), 0] for i in range(self.NSLOT)]
            self.dn[q] = 0
        self.nalloc = 0

    def sb(self, shape, dt=F32, name=None, es=None):
        self.nalloc += 1
        t = (es or self.es).enter_context(self.nc.sbuf_tensor("%s_%d" % (name or "t", self.nalloc), list(shape), dt))
        return t

    def _wait(self, eng, ev):
        sem, val = ev
        key = sem.num
        if eng.waited.get(key, 0) >= val:
            return
        eng.h.wait_ge(sem, val)
        eng.waited[key] = val

    def _deps(self, en, r, w):
        eng = self.E[en]
        for t in r:
            if t.w is not None:
                yield t.w
        for t in w:
            if t.w is not None:
                yield t.w
            for ev in t.r.values():
                yield ev

    def op(self, en, fn, r=(), w=(), inc=True):
        eng = self.E[en]
        for ev in list(self._deps(en, r, w)):
            if en == "pe" and ev[0] is eng.sem:
                continue
            self._wait(eng, ev)
        ins = fn(eng.h)
        myev = (eng.sem, eng.cnt + 1)
        if inc:
            ins.then_inc(eng.sem, 1)
            eng.cnt += 1
        for t in r:
            t.r[en] = myev
        for t in w:
            t.w = myev
            t.r = {}
        return ins

    def dma(self, qn, out, in_, r=(), w=(), **kw):
        q = self.E[qn]
        for ev in list(self._deps(qn, r, w)):
            self._wait(q, ev)
        slot = self.slots[qn][self.dn[qn] % self.NSLOT]
        self.dn[qn] += 1
        if slot[1] > 0:
            self._wait(q, (slot[0], slot[1]))
        ins = q.h.dma_start(out=out, in_=in_, **kw)
        slot[1] += 16
        ins.then_inc(slot[0], 16)
        ev = (slot[0], slot[1])
        key = "d%d" % slot[0].num
        for t in r:
            t.r[key] = ev
        for t in w:
            t.w = ev
            t.r = {}

    def barrier(self):
        evs = []
        for q in self.slots:
            for sem, val in self.slots[q]:
                if val > 0:
                    evs.append((sem, val))
        for name, e in self.E.items():
            if e.cnt > 0:
                evs.append((e.sem, e.cnt))
        for name, e in self.E.items():
            for ev in evs:
                if ev[0] is e.sem and name == "pe":
                    continue
                self._wait(e, ev)

    def finish(self):
        sp = self.E["sp"]
        for q in self.slots:
            for sem, val in self.slots[q]:
                if val > 0:
                    self._wait(sp, (sem, val))
        for name, e in self.E.items():
            if name != "sp" and e.cnt > 0:
                self._wait(sp, (e.sem, e.cnt))


def host_consts(T):
    c = {}
    c["ident"] = np.eye(128, dtype=np.float32)
    j = np.arange(64)[:, None]
    i = np.arange(64)[None, :]
    c["tri64"] = np.stack([(-DEC) * (j <= i), (-DEC) * (j < i), (-DEC) * (j > i)], 1).astype(np.float32)
    c["ncol64"] = np.full((64, 1), -DEC, np.float32)
    su = (j < i).astype(np.float32)
    iu = (j <= i).astype(np.float32)
    sl = (j > i).astype(np.float32)
    mMA = np.concatenate([-su, iu], 1)
    mBB = np.concatenate([su, iu], 1)
    c["mMA"] = np.tile(mMA[:, None, :], (1, 8, 1)).astype(np.float32)
    c["mBB"] = np.tile(mBB[:, None, :], (1, 8, 1)).astype(np.float32)
    c["mNT"] = np.tile((-sl)[:, None, :], (1, 8, 1)).astype(np.float32)
    c["id8"] = np.tile(np.eye(64, dtype=np.float32)[:, None, :], (1, 8, 1))
    j = np.arange(128)[:, None]
    i = np.arange(128)[None, :]
    c["tri128"] = np.stack([(-1 / 16) * (j <= i), (-1 / 16) * (j > i)], 1).astype(np.float32)
    c["ncol128"] = np.full((128, 1), -1 / 16, np.float32)
    c["sel"] = (np.arange(16)[:, None, None] == np.arange(16)[None, :, None]).astype(np.float32) * np.ones((1, 1, 128), np.float32)
    c["iu128"] = np.tile((j <= i).astype(np.float32)[:, None, :], (1, 4, 1))
    return c


class Ctx:
    pass


def build(T, dbg=(), stages=("A", "R", "G", "O0", "M0", "S1", "O1", "M1")):
    nc = bass.Bass("TRN2", target_bir_lowering=False)
    es = ExitStack()
    P = Prog(nc, es)
    NT = T // 128
    NS = T // 512
    C = Ctx()
    C.nc, C.P, C.T, C.NT, C.NS = nc, P, T, NT, NS
    C.dbg = {}
    C.dsa_interleave = DSA_INTERLEAVE

    def din(name, shape, dt=F32):
        return nc.dram_tensor(name, list(shape), dt, kind="ExternalInput").ap()

    def dscr(name, shape, dt=F32, out=False):
        kind = "ExternalOutput" if (out or name in dbg) else "Internal"
        return nc.dram_tensor(name, list(shape), dt, kind=kind).ap()

    I = {}
    I["x"] = din("x", [T, D])
    for name, shape in (("w_in_even", [D, EVEN_COLS]), ("a_mu", [1, A_COLS]), ("a_w0", [1, 512]), ("a_w2", [64, 512]),
                        ("a_a0", [1, 512]), ("a_a2", [64, 512]), ("a_g2", [128, 512]), ("a_kk_scale", [1, 512]),
                        ("a_ka_scale", [1, 512]), ("a_r_k", [1, 512]), ("a_gn_g", [1, 512]), ("a_gn_b", [1, 512]),
                        ("b_gate_w2", [16, 256]), ("b_gate_b", [1, 256]), ("b_norm_g", [1, 512]),
                        ("w_out_even", [D, D]), ("w_in_odd", [D, ODD_COLS]), ("c_ik_ln_g", [1, 64]),
                        ("c_ik_ln_b", [1, 64]), ("w_out_odd", [D, D]), ("ln1_g", [2, D]), ("ln1_b", [2, D]),
                        ("ln2_g", [2, D]), ("ln2_b", [2, D]), ("router_w", [D, NE]), ("router_bias", [1, NE]),
                        ("exp_w_gate", [2, NE, D, DE]), ("exp_w_up", [2, NE, D, DE]), ("exp_w_down", [2, NE, DE, D])):
        I[name] = din(name, shape)
    hc = host_consts(T)
    hc.update(rope_consts(T))
    for k, v in hc.items():
        I["c_" + k] = din("c_" + k, list(v.shape), F32 if v.dtype == np.float32 else BF16)
    C.I = I
    out = dscr("out", [T, D], out=True)
    C.XT0 = dscr("XT0", [D, T + 1], BF16)
    C.XT0_tok = [Tok() for _ in range(NS)]
    C.XT0_z = Tok()
    C.YT = dscr("YT", [D, T], BF16)
    C.YT_tok = [[Tok() for _ in range(NS)] for _ in range(2)]
    C.H0 = dscr("H0", [T, D])
    C.H0_tok = [Tok() for _ in range(NT)]
    C.HT0 = dscr("HT0", [D, T], BF16)
    C.HT0_tok = [Tok() for _ in range(NS)]
    C.X1 = dscr("X1", [T, D])
    C.X1_tok = [Tok() for _ in range(NT)]
    C.XT1 = dscr("XT1", [D, T], BF16)
    C.XT1_tok = [Tok() for _ in range(NS)]

    for nm, shp in (("dbg_ya", [T, 512]), ("dbg_yb", [T, 512]), ("dbg_dsa", [T, 1024]), ("dbg_mask", [T, T]), ("dbg_sc", [T, T])):
        if nm in dbg:
            C.dbg[nm] = dscr(nm, shp, out=True)
    C.WGU16 = [dscr("WGU16_%d" % l, [NE, 128, 2 * 8 * DE], BF16) for l in range(2)]
    C.WGU16_tok = [[Tok() for _ in range(NE)] for l in range(2)]
    C.WD16 = [dscr("WD16_%d" % l, [128, 32 * D], BF16) for l in range(2)]
    C.WD16_tok = [Tok() for l in range(2)]
    C.banks = []
    for b in range(8):
        t = es.enter_context(nc.psum_tensor("psb%d" % b, [128, 512], F32))
        C.banks.append((t, Tok()))
    C.bi = 0

    C.bank_pool = list(range(8))

    def bank():
        b = C.banks[C.bank_pool[C.bi % len(C.bank_pool)]]
        C.bi += 1
        return b
    C.bank = bank

    C.ident = P.sb([128, 128], F32, "ident")
    C.ident_tok = Tok()
    P.dma("sp", C.ident[:], I["c_ident"], w=[C.ident_tok])

    C.cast_todo = {}
    if "M0" in stages:
        cast_weights(C, 0)
    if "A" in stages:
        phase_A(C)
    if "R" in stages:
        phase_rwkv(C)
    if "G" in stages:
        phase_gla(C)
    if "M1" in stages:
        cast_weights(C, 1)
    if "O0" in stages:
        phase_outproj(C, I["w_out_even"], C.YT, lambda s: [C.YT_tok[0][s], C.YT_tok[1][s]], I["x"], lambda i: [],
                      I["ln1_g"][0:1, :], I["ln1_b"][0:1, :], C.H0, C.H0_tok, C.HT0, C.HT0_tok)
    if "M0" in stages:
        cast_some(C, 0, 999)
        phase_moe(C, 0, C.H0, C.H0_tok, C.HT0, C.HT0_tok, C.X1, C.X1_tok, C.XT1, C.XT1_tok)
    if "S1" in stages:
        phase_dsa(C)
    if "O1" in stages:
        C.H1 = dscr("H1", [T, D])
        C.H1_tok = [Tok() for _ in range(NT)]
        C.HT1 = dscr("HT1", [D, T], BF16)
        C.HT1_tok = [Tok() for _ in range(NS)]
        phase_outproj(C, I["w_out_odd"], C.YT, lambda s: [C.YT_tok[0][s], C.YT_tok[1][s]], C.X1, lambda i: [C.X1_tok[i]],
                      I["ln1_g"][1:2, :], I["ln1_b"][1:2, :], C.H1, C.H1_tok, C.HT1, C.HT1_tok)
    if "M1" in stages:
        cast_some(C, 1, 999)
        out_tok = [Tok() for _ in range(NT)]
        phase_moe(C, 1, C.H1, C.H1_tok, C.HT1, C.HT1_tok, out, out_tok, None, None)
    P.finish()
    es.close()
    return nc


def XTv(ap):
    return ap.rearrange("(c p) t -> p c t", p=128)


def cast_weights(C, l):
    P, I = C.P, C.I
    th = []
    for e in range(NE):
        dst = C.WGU16[l][e].rearrange("p (t c f) -> p t c f", t=2, c=8)
        th.append(lambda e=e, dst=dst: P.dma("pool", dst[:, 0, :, :], I["exp_w_gate"][l, e].rearrange("(c p) f -> p c f", p=128), w=[C.WGU16_tok[l][e]]))
        th.append(lambda e=e, dst=dst: P.dma("pool", dst[:, 1, :, :], I["exp_w_up"][l, e].rearrange("(c p) f -> p c f", p=128), w=[C.WGU16_tok[l][e]]))
    wd_flat = I["exp_w_down"][l].rearrange("e f d -> (e f) d")
    dstd = C.WD16[l].rearrange("p (c d) -> p c d", c=32)
    for c4 in range(8):
        th.append(lambda c4=c4: P.dma("pool", dstd[:, c4 * 4:(c4 + 1) * 4, :], wd_flat[c4 * 512:(c4 + 1) * 512, :].rearrange("(c p) d -> p c d", p=128), w=[C.WD16_tok[l]]))
    C.cast_todo[l] = th


def cast_some(C, l, n):
    th = C.cast_todo.get(l, [])
    for _ in range(min(n, len(th))):
        th.pop(0)()


def phase_A(C):
    nc, P, T, I = C.nc, C.P, C.T, C.I
    with ExitStack() as es:
        xin = [P.sb([128, D], F32, "xin", es) for _ in range(2)]
        xin_tok = [Tok(), Tok()]
        st = [P.sb([128, 8, 512], BF16, "ast", es) for _ in range(2)]
        st_tok = [Tok(), Tok()]
        z = P.sb([128, 8, 1], BF16, "zc", es)
        zt = Tok()
        P.op("dve", lambda e: e.memset(z[:], 0.0), w=[zt])
        P.dma("sp", XTv(C.XT0)[:, :, 0:1], z[:], r=[zt], w=[C.XT0_z], allow_slow_non_contiguous=True)
        for s in range(C.NS):
            sb_ = st[s % 2]
            for j in range(4):
                i = s * 4 + j
                xb = xin[i % 2]
                xt = xin_tok[i % 2]
                P.dma("sp", xb[:], I["x"][i * 128:(i + 1) * 128, :], w=[xt])
                for half in range(2):
                    bt, bk = C.bank()
                    for c4 in range(4):
                        c = half * 4 + c4
                        P.op("pe", lambda e: e.transpose(bt[:, c4 * 128:(c4 + 1) * 128], xb[:, c * 128:(c + 1) * 128], C.ident[:]),
                             r=[xt, C.ident_tok], w=[bk], inc=(c4 == 3))
                    en = "act" if half == 0 else "dve"
                    src = bt[:, :].rearrange("p (c t) -> p c t", c=4)
                    dst = sb_[:, half * 4:(half + 1) * 4, j * 128:(j + 1) * 128]
                    if en == "act":
                        P.op("act", lambda e: e.copy(dst, src), r=[bk], w=[st_tok[s % 2]])
                    else:
                        P.op("dve", lambda e: e.tensor_copy(dst, src), r=[bk], w=[st_tok[s % 2]])
            P.dma("sp", XTv(C.XT0)[:, :, 1 + s * 512:1 + (s + 1) * 512], sb_[:], r=[st_tok[s % 2]], w=[C.XT0_tok[s]])
        P.barrier()


class TL:
    def __init__(self, t, k=None):
        self.t = t
        self.k = k or Tok()

    def __getitem__(self, idx):
        return self.t[idx]


def mk(P, shape, dt=F32, name=None, es=None):
    return TL(P.sb(shape, dt, name, es))


def bcast_load(C, dst_ap, src_row, np_, tok, q="sp"):
    C.P.dma(q, dst_ap, src_row.partition_broadcast(np_), w=[tok])


def hv(ap, h):
    return ap.rearrange("p (h v) -> p h v", h=h)


def phase_rwkv(C):
    nc, P, T, I = C.nc, C.P, C.T, C.I
    mm = ALU.mult
    with ExitStack() as es:
        W1 = mk(P, [128, 8, A_COLS], BF16, "W1", es)
        W2 = mk(P, [128, 8, A_COLS], BF16, "W2", es)
        with ExitStack() as es2:
            mub = mk(P, [128, A_COLS], F32, "mub", es2)
            omu = mk(P, [128, A_COLS], F32, "omu", es2)
            stg = [mk(P, [128, A_COLS], F32, "wstg", es2) for _ in range(2)]
            bcast_load(C, mub[:], I["a_mu"], 128, mub.k)
            P.op("dve", lambda e: e.tensor_scalar(omu[:], mub[:], -1.0, 1.0, ALU.mult, ALU.add), r=[mub.k], w=[omu.k])
            for c in range(8):
                s_ = stg[c % 2]
                P.dma("sp", s_[:], I["w_in_even"][c * 128:(c + 1) * 128, 0:A_COLS], w=[s_.k])
                P.op("dve", lambda e: e.tensor_tensor(W1[:, c, :], s_[:], omu[:], mm), r=[s_.k, omu.k], w=[W1.k])
                P.op("pool", lambda e: e.tensor_tensor(W2[:, c, :], s_[:], mub[:], mm), r=[s_.k, mub.k], w=[W2.k])
            P.barrier()
        LW = mk(P, [128, 512], BF16, "LW", es)
        G2 = mk(P, [128, 512], BF16, "G2", es)
        P.dma("pool", LW[0:64, :], I["a_w2"], w=[LW.k])
        P.dma("pool", LW[64:128, :], I["a_a2"], w=[LW.k])
        P.dma("pool", G2[:], I["a_g2"], w=[G2.k])
        BV = mk(P, [64, 7, 512], F32, "BV", es)
        for n, name in enumerate(("a_w0", "a_a0", "a_kk_scale", "a_ka_scale", "a_r_k", "a_gn_g", "a_gn_b")):
            bcast_load(C, BV[:, n, :], I[name], 64, BV.k)
        w0b, a0b, kksb, kab, rkb, gngb, gnbb = [BV[:, n, :] for n in range(7)]
        tri = mk(P, [64, 3, 64], F32, "tri", es)
        ncol = mk(P, [64, 1], F32, "ncol", es)
        mMA = mk(P, [64, 8, 128], F32, "mMA", es)
        mBB = mk(P, [64, 8, 128], F32, "mBB", es)
        mNT = mk(P, [64, 8, 64], F32, "mNT", es)
        id8 = mk(P, [64, 8, 64], F32, "id8", es)
        for tl, nm in ((tri, "c_tri64"), (ncol, "c_ncol64"), (mMA, "c_mMA"), (mBB, "c_mBB"), (mNT, "c_mNT"), (id8, "c_id8")):
            P.dma("sp", tl[:], I[nm], w=[tl.k])
        id64 = C.ident[0:64, 0:64]

        def wt(name, shape=(64, 512), dt=F32):
            return mk(P, list(shape), dt, name, es)
        ATs = [mk(P, [128, 8, 513], BF16, "ATs", es) for _ in range(2)]
        TX = wt("TX", (128, 512), BF16)
        SG = wt("SG", (128, 512), BF16)
        r_, k_, v_, sg, a_, kk, be, Bi, Ki, tmp = [wt(n) for n in ("r", "k", "v", "sg", "a", "kk", "be", "Bi", "Ki", "tmp")]
        Ep, Em, Ex, Ee = [wt(n) for n in ("Ep", "Em", "Ex", "Ee")]
        s8 = [wt("s8_%d" % n, (64, 8)) for n in range(4)]
        KRs = [wt("KR", (64, 8, 128), BF16) for _ in range(2)]
        BiT = wt("BiT", (64, 8, 64), BF16)
        KiT = wt("KiT", (64, 8, 64), BF16)
        MAs = [wt("MA", (64, 8, 128), BF16) for _ in range(2)]
        BBs = [wt("BB", (64, 8, 128), BF16) for _ in range(2)]
        Xb = [wt("X%d" % n, (64, 8, 64), BF16) for n in range(2)]
        XTb = [wt("XT%d" % n, (64, 8, 64), BF16) for n in range(2)]
        Qbs = [[wt("Q%d" % n, (64, 8, 64), BF16) for n in range(2)] for _ in range(2)]
        Xs = wt("Xs", (64, 8, 64), BF16)
        nU = wt("nU", (64, 8, 64), BF16)
        Y = wt("Y")
        tmpb = wt("tmpb")
        Hs = [wt("H%d" % n, (64, 8, 64)) for n in range(2)]
        Hbs = [wt("Hb%d" % n, (64, 8, 64), BF16) for n in range(2)]
        vbs = [wt("vb", (64, 512), BF16) for _ in range(2)]
        Ke16s = [wt("Ke16", (64, 512), BF16) for _ in range(2)]
        Be16s = [wt("Be16", (64, 512), BF16) for _ in range(2)]
        PCs = [wt("PC", (64, 8)) for _ in range(2)]
        gs_ = [wt("g", (64, 512)) for _ in range(2)]
        bonuss = [wt("bonus", (64, 512)) for _ in range(2)]
        P.op("pool", lambda e: e.memset(Hbs[0][:], 0.0), w=[Hbs[0].k])
        yst = [mk(P, [128, 4, 512], BF16, "yst", es) for _ in range(2)]
        P.op("dve", lambda e: e.memset(Hs[0][:], 0.0), w=[Hs[0].k])

        def psb():
            t, k = C.bank()
            return TL(t, k)

        def v3(tl_or_ap, h=8):
            return hv(tl_or_ap, h)

        def chunk(s, ci):
          at = ATs[s % 2]
          ys = yst[s % 2]
          cast_some(C, 0, 1)
          def load_at(s_):
            rd = [C.XT0_tok[s_]] + ([C.XT0_tok[s_ - 1]] if s_ > 0 else [C.XT0_z])
            P.dma("sp", ATs[s_ % 2][:], XTv(C.XT0)[:, :, s_ * 512:s_ * 512 + 513], r=rd, w=[ATs[s_ % 2].k])
          if ci == 7 and s + 1 < C.NS:
            load_at(s + 1)
          if ci == 0:
            if s == 0:
                load_at(0)
            for which in range(2):
                pb = psb()
                c0 = 1536 + which * 128
                for c in range(8):
                    P.op("pe", lambda e: e.matmul(pb[:, :], lhsT=W1[:, c, c0:c0 + 128], rhs=at[:, c, 1:513], start=(c == 0), stop=False),
                         r=[W1.k, at.k], w=[pb.k], inc=False)
                for c in range(8):
                    P.op("pe", lambda e: e.matmul(pb[:, :], lhsT=W2[:, c, c0:c0 + 128], rhs=at[:, c, 0:512], start=False, stop=(c == 7)),
                         r=[W2.k, at.k], w=[pb.k], inc=(c == 7))
                if which == 0:
                    P.op("act", lambda e: e.activation(out=TX[0:64, :], in_=pb[0:64, :], func=AF.Tanh), r=[pb.k], w=[TX.k])
                    P.op("act", lambda e: e.copy(TX[64:128, :], pb[64:128, :]), r=[pb.k], w=[TX.k])
                else:
                    P.op("act", lambda e: e.activation(out=SG[:, :], in_=pb[:, :], func=AF.Sigmoid), r=[pb.k], w=[SG.k])
          if True:
            if True:
                g = s * 8 + ci
                t0 = ci * 64
                KR, MA, BB, Qb = KRs[g % 2], MAs[g % 2], BBs[g % 2], Qbs[g % 2]
                vb, Ke16, Be16, PC, g_, bonus = vbs[g % 2], Ke16s[g % 2], Be16s[g % 2], PCs[g % 2], gs_[g % 2], bonuss[g % 2]
                pr, pk, pv = psb(), psb(), psb()
                for pb, c0 in ((pr, 0), (pk, 512), (pv, 1024)):
                    for c in range(8):
                        P.op("pe", lambda e: e.matmul(pb[0:64, :], lhsT=at[:, c, 1 + t0:1 + t0 + 64], rhs=W1[:, c, c0:c0 + 512], start=(c == 0), stop=False),
                             r=[W1.k, at.k], w=[pb.k], inc=False)
                    for c in range(8):
                        P.op("pe", lambda e: e.matmul(pb[0:64, :], lhsT=at[:, c, t0:t0 + 64], rhs=W2[:, c, c0:c0 + 512], start=False, stop=(c == 7)),
                             r=[W2.k, at.k], w=[pb.k], inc=(c == 7))
                yield "F"
                pz, pza, pg = psb(), psb(), psb()
                P.op("pe", lambda e: e.matmul(pz[0:64, :], lhsT=TX[0:64, t0:t0 + 64], rhs=LW[0:64, :], start=True, stop=True), r=[TX.k, LW.k], w=[pz.k])
                P.op("pe", lambda e: e.matmul(pza[0:64, :], lhsT=TX[64:128, t0:t0 + 64], rhs=LW[64:128, :], start=True, stop=True), r=[TX.k, LW.k], w=[pza.k])
                P.op("pe", lambda e: e.matmul(pg[0:64, :], lhsT=SG[:, t0:t0 + 64], rhs=G2[:, :], start=True, stop=True), r=[SG.k, G2.k], w=[pg.k])
                P.op("act", lambda e: e.copy(r_[:], pr[0:64, :]), r=[pr.k], w=[r_.k])
                P.op("act", lambda e: e.copy(v_[:], pv[0:64, :]), r=[pv.k], w=[v_.k])
                P.op("act", lambda e: e.copy(vb[:], pv[0:64, :]), r=[pv.k], w=[vb.k])
                P.op("act", lambda e: e.copy(g_[:], pg[0:64, :]), r=[pg.k], w=[g_.k])
                P.op("dve", lambda e: e.tensor_copy(k_[:], pk[0:64, :]), r=[pk.k], w=[k_.k])
                P.op("dve", lambda e: e.tensor_tensor(sg[:], pz[0:64, :], w0b, ALU.add), r=[pz.k, BV.k], w=[sg.k])
                P.op("act", lambda e: e.activation(out=sg[:], in_=sg[:], func=AF.Sigmoid), r=[sg.k], w=[sg.k])
                P.op("dve", lambda e: e.tensor_tensor(a_[:], pza[0:64, :], a0b, ALU.add), r=[pza.k, BV.k], w=[a_.k])
                P.op("act", lambda e: e.activation(out=a_[:], in_=a_[:], func=AF.Sigmoid), r=[a_.k], w=[a_.k])
                yield "F"
                P.op("pool", lambda e: e.tensor_tensor(kk[:], k_[:], kksb, mm), r=[k_.k, BV.k], w=[kk.k])
                P.op("pool", lambda e: e.tensor_tensor(tmp[:], kk[:], kk[:], mm), r=[kk.k], w=[tmp.k])
                P.op("dve", lambda e: e.tensor_reduce(s8[0][:], v3(tmp[:]), AX.X, ALU.add), r=[tmp.k], w=[s8[0].k])
                P.op("dve", lambda e: e.tensor_scalar(s8[0][:], s8[0][:], 1e-24, None, ALU.max), r=[s8[0].k], w=[s8[0].k])
                P.op("act", lambda e: e.activation(out=s8[0][:], in_=s8[0][:], func=AF.Ln), r=[s8[0].k], w=[s8[0].k])
                P.op("act", lambda e: e.activation(out=s8[0][:], in_=s8[0][:], func=AF.Exp, scale=-0.5), r=[s8[0].k], w=[s8[0].k])
                P.op("dve", lambda e: e.tensor_tensor(v3(kk[:]), v3(kk[:]), s8[0][:, :].unsqueeze(2).to_broadcast([64, 8, 64]), mm),
                     r=[kk.k, s8[0].k], w=[kk.k])
                P.op("pool", lambda e: e.tensor_tensor(be[:], kk[:], a_[:], mm), r=[kk.k, a_.k], w=[be.k])
                P.op("dve", lambda e: e.scalar_tensor_tensor(tmp[:], a_[:], -1.0, kab, ALU.add, mm), r=[a_.k, BV.k], w=[tmp.k])
                P.op("dve", lambda e: e.scalar_tensor_tensor(k_[:], tmp[:], 1.0, k_[:], ALU.add, mm), r=[tmp.k, k_.k], w=[k_.k])
                P.op("pool", lambda e: e.tensor_tensor(tmp[:], r_[:], k_[:], mm), r=[r_.k, k_.k], w=[tmp.k])
                P.op("pool", lambda e: e.tensor_tensor(tmp[:], tmp[:], rkb, mm), r=[tmp.k, BV.k], w=[tmp.k])
                P.op("dve", lambda e: e.tensor_reduce(s8[1][:], v3(tmp[:]), AX.X, ALU.add), r=[tmp.k], w=[s8[1].k])
                P.op("dve", lambda e: e.tensor_tensor(v3(bonus[:]), v3(v_[:]), s8[1][:, :].unsqueeze(2).to_broadcast([64, 8, 64]), mm),
                     r=[v_.k, s8[1].k], w=[bonus.k])
                yield "F"
                pcl, pcx, pca, ppc = psb(), psb(), psb(), psb()
                for pb, n in ((pcl, 0), (pcx, 1), (pca, 2)):
                    P.op("pe", lambda e: e.matmul(pb[0:64, :], lhsT=tri[:, n, :], rhs=sg[:], start=True, stop=True), r=[tri.k, sg.k], w=[pb.k])
                for h in range(8):
                    P.op("pe", lambda e: e.matmul(ppc[0:64, h:h + 1], lhsT=sg[:, h * 64:(h + 1) * 64], rhs=ncol[:], start=True, stop=True),
                         r=[sg.k, ncol.k], w=[ppc.k], inc=(h == 7))
                P.op("act", lambda e: e.activation(out=Ep[:], in_=pcl[0:64, :], func=AF.Exp), r=[pcl.k], w=[Ep.k])
                P.op("act", lambda e: e.activation(out=Em[:], in_=pcl[0:64, :], func=AF.Exp, scale=-1.0), r=[pcl.k], w=[Em.k])
                P.op("act", lambda e: e.activation(out=Ex[:], in_=pcx[0:64, :], func=AF.Exp), r=[pcx.k], w=[Ex.k])
                P.op("act", lambda e: e.activation(out=Ee[:], in_=pca[0:64, :], func=AF.Exp), r=[pca.k], w=[Ee.k])
                P.op("act", lambda e: e.activation(out=PC[:], in_=ppc[0:64, 0:8], func=AF.Exp), r=[ppc.k], w=[PC.k])
                P.op("dve", lambda e: e.tensor_tensor(r_[:], r_[:], Ep[:], mm), r=[r_.k, Ep.k], w=[r_.k])
                P.op("pool", lambda e: e.tensor_tensor(kk[:], kk[:], Ex[:], mm), r=[kk.k, Ex.k], w=[kk.k])
                P.op("dve", lambda e: e.tensor_tensor(Bi[:], be[:], Em[:], mm), r=[be.k, Em.k], w=[Bi.k])
                P.op("pool", lambda e: e.tensor_tensor(Ki[:], k_[:], Em[:], mm), r=[k_.k, Em.k], w=[Ki.k])
                P.op("dve", lambda e: e.tensor_tensor(Ke16[:], k_[:], Ee[:], mm), r=[k_.k, Ee.k], w=[Ke16.k])
                P.op("pool", lambda e: e.tensor_tensor(Be16[:], be[:], Ee[:], mm), r=[be.k, Ee.k], w=[Be16.k])
                yield "F"
                for src, dst, off, en in ((kk, KR, 0, "act"), (r_, KR, 64, "dve"), (Bi, BiT, 0, "act"), (Ki, KiT, 0, "dve")):
                    pb = psb()
                    for h in range(8):
                        P.op("pe", lambda e: e.transpose(pb[0:64, h * 64:(h + 1) * 64], src[:, h * 64:(h + 1) * 64], id64),
                             r=[src.k, C.ident_tok], w=[pb.k], inc=(h == 7))
                    d_ = dst[:, :, off:off + 64]
                    s_ = v3(pb[0:64, :])
                    if en == "act":
                        P.op("act", lambda e: e.copy(d_, s_), r=[pb.k], w=[dst.k])
                    else:
                        P.op("dve", lambda e: e.tensor_copy(d_, s_), r=[pb.k], w=[dst.k])
                yield "F"
                pma = [psb(), psb()]
                pbb = [psb(), psb()]
                pnt = psb()
                for h in range(8):
                    hb, hh = h // 4, h % 4
                    P.op("pe", lambda e: e.matmul(pma[hb][0:64, hh * 128:(hh + 1) * 128], lhsT=BiT[:, h, :], rhs=KR[:, h, :], start=True, stop=True),
                         r=[BiT.k, KR.k], w=[pma[hb].k], inc=(hh == 3))
                for h in range(8):
                    hb, hh = h // 4, h % 4
                    P.op("pe", lambda e: e.matmul(pbb[hb][0:64, hh * 128:(hh + 1) * 128], lhsT=KiT[:, h, :], rhs=KR[:, h, :], start=True, stop=True),
                         r=[KiT.k, KR.k], w=[pbb[hb].k], inc=(hh == 3))
                for h in range(8):
                    P.op("pe", lambda e: e.matmul(pnt[0:64, h * 64:(h + 1) * 64], lhsT=KR[:, h, 0:64], rhs=BiT[:, h, :], start=True, stop=True),
                         r=[BiT.k, KR.k], w=[pnt.k], inc=(h == 7))
                for hb in range(2):
                    P.op("dve", lambda e: e.tensor_tensor(MA[:, hb * 4:(hb + 1) * 4, :], hv(pma[hb][0:64, :], 4), mMA[:, hb * 4:(hb + 1) * 4, :], mm),
                         r=[pma[hb].k, mMA.k], w=[MA.k])
                    P.op("dve", lambda e: e.tensor_tensor(BB[:, hb * 4:(hb + 1) * 4, :], hv(pbb[hb][0:64, :], 4), mBB[:, hb * 4:(hb + 1) * 4, :], mm),
                         r=[pbb[hb].k, mBB.k], w=[BB.k])
                X, XT, Q = Xb[0], XTb[0], Qb[0]
                P.op("dve", lambda e: e.tensor_tensor(XT[:], v3(pnt[0:64, :]), mNT[:], mm), r=[pnt.k, mNT.k], w=[XT.k])
                P.op("pool", lambda e: e.tensor_copy(X[:], MA[:, :, 0:64]), r=[MA.k], w=[X.k])
                P.op("pool", lambda e: e.tensor_tensor(Q[:], MA[:, :, 0:64], id8[:], ALU.add), r=[MA.k, id8.k], w=[Q.k])
                for lvl in range(5):
                    Xn, XTn, Qn = Xb[(lvl + 1) % 2], XTb[(lvl + 1) % 2], Qb[(lvl + 1) % 2]
                    pxt = psb()
                    for h in range(8):
                        P.op("pe", lambda e: e.matmul(pxt[0:64, h * 64:(h + 1) * 64], lhsT=X[:, h, :], rhs=XT[:, h, :], start=True, stop=True),
                             r=[X.k, XT.k], w=[pxt.k], inc=(h == 7))
                    if lvl < 4:
                        px = psb()
                        for h in range(8):
                            P.op("pe", lambda e: e.matmul(px[0:64, h * 64:(h + 1) * 64], lhsT=XT[:, h, :], rhs=X[:, h, :], start=True, stop=True),
                                 r=[X.k, XT.k], w=[px.k], inc=(h == 7))
                    P.op("act", lambda e: e.copy(XTn[:], v3(pxt[0:64, :])), r=[pxt.k], w=[XTn.k])
                    if lvl < 4:
                        P.op("dve", lambda e: e.tensor_copy(Xn[:], v3(px[0:64, :])), r=[px.k], w=[Xn.k])
                    pq = psb()
                    for h in range(8):
                        P.op("pe", lambda e: e.matmul(pq[0:64, h * 64:(h + 1) * 64], lhsT=XTn[:, h, :], rhs=Q[:, h, :], start=True, stop=True),
                             r=[XTn.k, Q.k], w=[pq.k], inc=(h == 7))
                    P.op("dve", lambda e: e.tensor_tensor(Qn[:], Q[:], v3(pq[0:64, :]), ALU.add), r=[Q.k, pq.k], w=[Qn.k])
                    X, XT, Q = Xn, XTn, Qn
                    yield "F"
                yield "END_FRONT"
                H, Hn = Hs[g % 2], Hs[(g + 1) % 2]
                Hb, Hbn = Hbs[g % 2], Hbs[(g + 1) % 2]
                pxs = psb()
                for h in range(8):
                    P.op("pe", lambda e: e.matmul(pxs[0:64, h * 64:(h + 1) * 64], lhsT=KR[:, h, 0:64], rhs=Hb[:, h, :], start=True, stop=False),
                         r=[KR.k, Hb.k], w=[pxs.k], inc=False)
                    P.op("pe", lambda e: e.matmul(pxs[0:64, h * 64:(h + 1) * 64], lhsT=BB[:, h, 0:64], rhs=vb[:, h * 64:(h + 1) * 64], start=False, stop=True),
                         r=[BB.k, vb.k], w=[pxs.k], inc=(h == 7))
                P.op("act", lambda e: e.copy(Xs[:], v3(pxs[0:64, :])), r=[pxs.k], w=[Xs.k])
                yield "B"
                pu = psb()
                for h in range(8):
                    P.op("pe", lambda e: e.matmul(pu[0:64, h * 64:(h + 1) * 64], lhsT=Q[:, h, :], rhs=Xs[:, h, :], start=True, stop=True),
                         r=[Q.k, Xs.k], w=[pu.k], inc=(h == 7))
                P.op("act", lambda e: e.mul(nU[:], v3(pu[0:64, :]), -1.0), r=[pu.k], w=[nU.k])
                yield "B"
                py, ph = psb(), psb()
                for h in range(8):
                    sl = slice(h * 64, (h + 1) * 64)
                    P.op("pe", lambda e: e.matmul(py[0:64, sl], lhsT=KR[:, h, 64:128], rhs=Hb[:, h, :], start=True, stop=False), r=[KR.k, Hb.k], w=[py.k], inc=False)
                    P.op("pe", lambda e: e.matmul(py[0:64, sl], lhsT=BB[:, h, 64:128], rhs=vb[:, sl], start=False, stop=False), r=[BB.k, vb.k], w=[py.k], inc=False)
                    P.op("pe", lambda e: e.matmul(py[0:64, sl], lhsT=MA[:, h, 64:128], rhs=nU[:, h, :], start=False, stop=True), r=[MA.k, nU.k], w=[py.k], inc=(h == 7))
                for h in range(8):
                    sl = slice(h * 64, (h + 1) * 64)
                    P.op("pe", lambda e: e.matmul(ph[0:64, sl], lhsT=Ke16[:, sl], rhs=vb[:, sl], start=True, stop=False), r=[Ke16.k, vb.k], w=[ph.k], inc=False)
                    P.op("pe", lambda e: e.matmul(ph[0:64, sl], lhsT=Be16[:, sl], rhs=nU[:, h, :], start=False, stop=True), r=[Be16.k, nU.k], w=[ph.k], inc=(h == 7))
                P.op("pool", lambda e: e.tensor_tensor(Hn[:], H[:], PC[:, :].unsqueeze(2).to_broadcast([64, 8, 64]), mm), r=[H.k, PC.k], w=[Hn.k])
                P.op("dve", lambda e: e.tensor_tensor(Hn[:], Hn[:], v3(ph[0:64, :]), ALU.add), r=[Hn.k, ph.k], w=[Hn.k])
                P.op("act", lambda e: e.copy(Hbn[:], Hn[:]), r=[Hn.k], w=[Hbn.k])
                yield "B"
                P.op("act", lambda e: e.copy(Y[:], py[0:64, :]), r=[py.k], w=[Y.k])
                P.op("dve", lambda e: e.tensor_reduce(s8[2][:], v3(Y[:]), AX.X, ALU.add), r=[Y.k], w=[s8[2].k])
                P.op("dve", lambda e: e.tensor_scalar(s8[2][:], s8[2][:], 1.0 / 64, None, mm), r=[s8[2].k], w=[s8[2].k])
                P.op("dve", lambda e: e.tensor_tensor(v3(Y[:]), v3(Y[:]), s8[2][:, :].unsqueeze(2).to_broadcast([64, 8, 64]), ALU.subtract),
                     r=[Y.k, s8[2].k], w=[Y.k])
                yield "B"
                P.op("pool", lambda e: e.tensor_tensor(tmpb[:], Y[:], Y[:], mm), r=[Y.k], w=[tmpb.k])
                P.op("dve", lambda e: e.tensor_reduce(s8[3][:], v3(tmpb[:]), AX.X, ALU.add), r=[tmpb.k], w=[s8[3].k])
                P.op("act", lambda e: e.activation(out=s8[3][:], in_=s8[3][:], func=AF.Ln, bias=64e-5, scale=1.0 / 64), r=[s8[3].k], w=[s8[3].k])
                P.op("act", lambda e: e.activation(out=s8[3][:], in_=s8[3][:], func=AF.Exp, scale=-0.5), r=[s8[3].k], w=[s8[3].k])
                P.op("dve", lambda e: e.tensor_tensor(v3(Y[:]), v3(Y[:]), s8[3][:, :].unsqueeze(2).to_broadcast([64, 8, 64]), mm),
                     r=[Y.k, s8[3].k], w=[Y.k])
                P.op("pool", lambda e: e.tensor_tensor(Y[:], Y[:], gngb, mm), r=[Y.k, BV.k], w=[Y.k])
                P.op("pool", lambda e: e.tensor_tensor(Y[:], Y[:], gnbb, ALU.add), r=[Y.k, BV.k], w=[Y.k])
                P.op("dve", lambda e: e.tensor_tensor(Y[:], Y[:], bonus[:], ALU.add), r=[Y.k, bonus.k], w=[Y.k])
                P.op("dve", lambda e: e.tensor_tensor(Y[:], Y[:], g_[:], mm), r=[Y.k, g_.k], w=[Y.k])
                if "dbg_ya" in C.dbg:
                    P.dma("sp", C.dbg["dbg_ya"][g * 64:(g + 1) * 64, :], Y[:], r=[Y.k])
                yield "B"
                pb = psb()
                for q in range(4):
                    P.op("pe", lambda e: e.transpose(pb[:, q * 64:(q + 1) * 64], Y[:, q * 128:(q + 1) * 128], id64), r=[Y.k, C.ident_tok], w=[pb.k], inc=(q == 3))
                P.op("act", lambda e: e.copy(ys[:, :, t0:t0 + 64], hv(pb[:, 0:256], 4)), r=[pb.k], w=[ys.k])
                if ci == 7:
                    P.dma("sp", XTv(C.YT)[:, 0:4, s * 512:(s + 1) * 512], ys[:], r=[ys.k], w=[C.YT_tok[0][s]])

        makers = [(lambda s=s, ci=ci: chunk(s, ci)) for s in range(C.NS) for ci in range(8)]
        if RWKV_INTERLEAVE:
            pipeline2(makers, interleave=True)
        else:
            def to_end_front(g_):
                while next(g_) != "END_FRONT":
                    pass
            g = makers[0]()
            to_end_front(g)
            for n in range(len(makers)):
                for _ in range(3):
                    next(g)
                g2 = None
                if n + 1 < len(makers):
                    g2 = makers[n + 1]()
                    next(g2)
                for _ in g:
                    pass
                if g2 is not None:
                    to_end_front(g2)
                g = g2
        P.barrier()


def pipeline2(makers, interleave=True):
    if not interleave:
        for mk_ in makers:
            for _ in mk_():
                pass
        return
    prevB = None
    for mk_ in makers:
        g = mk_()
        while True:
            r = next(g)
            if prevB is not None:
                try:
                    next(prevB)
                except StopIteration:
                    prevB = None
            if r == "END_FRONT":
                break
        if prevB is not None:
            for _ in prevB:
                pass
        prevB = g
    if prevB is not None:
        for _ in prevB:
            pass


def psb(C):
    t, k = C.bank()
    return TL(t, k)


def phase_gla(C):
    nc, P, T, I = C.nc, C.P, C.T, C.I
    mm = ALU.mult
    with ExitStack() as es:
        WB = mk(P, [128, 8, B_COLS], BF16, "WB", es)
        for c in range(8):
            P.dma("pool", WB[:, c, :], I["w_in_even"][c * 128:(c + 1) * 128, A_COLS:EVEN_COLS], w=[WB.k])
        GW2 = mk(P, [16, 256], BF16, "GW2", es)
        P.dma("pool", GW2[:], I["b_gate_w2"], w=[GW2.k])
        gbb = mk(P, [128, 256], F32, "gbb", es)
        ngb = mk(P, [128, 512], F32, "ngb", es)
        bcast_load(C, gbb[:], I["b_gate_b"], 128, gbb.k)
        bcast_load(C, ngb[:], I["b_norm_g"], 128, ngb.k)
        tri = mk(P, [128, 2, 128], F32, "tri128", es)
        ncol = mk(P, [128, 1], F32, "ncol128", es)
        iu = mk(P, [128, 4, 128], F32, "iu128", es)
        for tl, nm in ((tri, "c_tri128"), (ncol, "c_ncol128"), (iu, "c_iu128")):
            P.dma("sp", tl[:], I[nm], w=[tl.k])
        id64 = C.ident[0:64, 0:64]

        def wt(name, shape, dt=F32):
            return mk(P, list(shape), dt, name, es)
        ATs = [wt("ATg", (128, 8, 512), BF16) for _ in range(2)]
        AL = wt("AL", (16, 512), BF16)
        l_ = wt("l", (128, 256))
        Eq, Ei, Ee = wt("Eq", (128, 256)), wt("Ei", (128, 256)), wt("Ee", (128, 256))
        PCg = wt("PCg", (64, 4))
        qd, ki, ke = wt("qd", (128, 256)), wt("ki", (128, 256)), wt("ke", (128, 256))
        v_ = wt("vg", (128, 512))
        qdT, kiT = wt("qdT", (64, 4, 128)), wt("kiT", (64, 4, 128))
        attT = wt("attT", (128, 4, 128))
        Ss = [wt("S%d" % n, (64, 4, 128)) for n in range(2)]
        o_ = wt("o", (128, 512))
        sq = wt("sqg", (128, 512))
        sl_ = wt("silu", (128, 512))
        m4 = wt("m4", (128, 4))
        yst = [wt("ystg", (128, 4, 512), BF16) for _ in range(2)]
        P.op("dve", lambda e: e.memset(Ss[0][:], 0.0), w=[Ss[0].k])
        def gproj(s, ci):
            at = ATs[s % 2]
            t0 = ci * 128
            if ci == 0:
                P.dma("sp", at[:], XTv(C.XT0)[:, :, 1 + s * 512:1 + (s + 1) * 512], r=[C.XT0_tok[s]], w=[at.k])
                pb = psb(C)
                for c in range(8):
                    P.op("pe", lambda e: e.matmul(pb[0:16, :], lhsT=WB[:, c, 1536:1552], rhs=at[:, c, :], start=(c == 0), stop=(c == 7)),
                         r=[WB.k, at.k], w=[pb.k], inc=(c == 7))
                P.op("act", lambda e: e.copy(AL[:], pb[0:16, :]), r=[pb.k], w=[AL.k])
            pqk, pv, pg = psb(C), psb(C), psb(C)
            for pb, c0 in ((pqk, 0), (pv, 512), (pg, 1024)):
                for c in range(8):
                    P.op("pe", lambda e: e.matmul(pb[:, :], lhsT=at[:, c, t0:t0 + 128], rhs=WB[:, c, c0:c0 + 512], start=(c == 0), stop=(c == 7)),
                         r=[WB.k, at.k], w=[pb.k], inc=(c == 7))
            pla = psb(C)
            P.op("pe", lambda e: e.matmul(pla[:, 0:256], lhsT=AL[:, t0:t0 + 128], rhs=GW2[:], start=True, stop=True), r=[AL.k, GW2.k], w=[pla.k])
            return pqk, pv, pg, pla

        nxt = gproj(0, 0)
        for s in range(C.NS):
            ys = yst[s % 2]
            for ci in range(4):
                g = s * 4 + ci
                t0 = ci * 128
                pqk, pv, pg, pla = nxt
                P.op("dve", lambda e: e.tensor_tensor(l_[:], pla[:, 0:256], gbb[:], ALU.add), r=[pla.k, gbb.k], w=[l_.k])
                P.op("act", lambda e: e.activation(out=l_[:], in_=l_[:], func=AF.Exp, scale=-1.0), r=[l_.k], w=[l_.k])
                P.op("act", lambda e: e.activation(out=l_[:], in_=l_[:], func=AF.Ln, bias=1.0), r=[l_.k], w=[l_.k])
                pbc, pba, ppc = psb(C), psb(C), psb(C)
                P.op("pe", lambda e: e.matmul(pbc[:, 0:256], lhsT=tri[:, 0, :], rhs=l_[:], start=True, stop=True), r=[tri.k, l_.k], w=[pbc.k])
                P.op("pe", lambda e: e.matmul(pba[:, 0:256], lhsT=tri[:, 1, :], rhs=l_[:], start=True, stop=True), r=[tri.k, l_.k], w=[pba.k])
                for h in range(4):
                    P.op("pe", lambda e: e.matmul(ppc[0:64, h:h + 1], lhsT=l_[:, h * 64:(h + 1) * 64], rhs=ncol[:], start=True, stop=True),
                         r=[l_.k, ncol.k], w=[ppc.k], inc=(h == 3))
                P.op("act", lambda e: e.activation(out=Eq[:], in_=pbc[:, 0:256], func=AF.Exp), r=[pbc.k], w=[Eq.k])
                P.op("act", lambda e: e.activation(out=Ei[:], in_=pbc[:, 0:256], func=AF.Exp, scale=-1.0), r=[pbc.k], w=[Ei.k])
                P.op("act", lambda e: e.activation(out=Ee[:], in_=pba[:, 0:256], func=AF.Exp), r=[pba.k], w=[Ee.k])
                P.op("act", lambda e: e.activation(out=PCg[:], in_=ppc[0:64, 0:4], func=AF.Exp), r=[ppc.k], w=[PCg.k])
                P.op("dve", lambda e: e.scalar_tensor_tensor(qd[:], pqk[:, 0:256], 0.125, Eq[:], mm, mm), r=[pqk.k, Eq.k], w=[qd.k])
                P.op("dve", lambda e: e.tensor_tensor(ki[:], pqk[:, 256:512], Ei[:], mm), r=[pqk.k, Ei.k], w=[ki.k])
                P.op("dve", lambda e: e.tensor_tensor(ke[:], pqk[:, 256:512], Ee[:], mm), r=[pqk.k, Ee.k], w=[ke.k])
                P.op("act", lambda e: e.copy(v_[:], pv[:, :]), r=[pv.k], w=[v_.k])
                P.op("act", lambda e: e.activation(out=sl_[:], in_=pg[:, :], func=AF.Silu), r=[pg.k], w=[sl_.k])
                for src, dst, en in ((qd, qdT, "act"), (ki, kiT, "dve")):
                    pb = psb(C)
                    for h in range(4):
                        P.op("pe", lambda e: e.transpose(pb[0:64, h * 128:(h + 1) * 128], src[:, h * 64:(h + 1) * 64], C.ident[:]),
                             r=[src.k, C.ident_tok], w=[pb.k], inc=(h == 3))
                    if en == "act":
                        P.op("act", lambda e: e.copy(dst[:], hv(pb[0:64, :], 4)), r=[pb.k], w=[dst.k])
                    else:
                        P.op("dve", lambda e: e.tensor_copy(dst[:], hv(pb[0:64, :], 4)), r=[pb.k], w=[dst.k])
                patt = psb(C)
                for h in range(4):
                    P.op("pe", lambda e: e.matmul(patt[:, h * 128:(h + 1) * 128], lhsT=kiT[:, h, :], rhs=qdT[:, h, :], start=True, stop=True),
                         r=[kiT.k, qdT.k], w=[patt.k], inc=(h == 3))
                P.op("dve", lambda e: e.tensor_tensor(attT[:], hv(patt[:, :], 4), iu[:], mm), r=[patt.k, iu.k], w=[attT.k])
                S, Sn = Ss[g % 2], Ss[(g + 1) % 2]
                po, pS = psb(C), psb(C)
                for h in range(4):
                    sl = slice(h * 128, (h + 1) * 128)
                    P.op("pe", lambda e: e.matmul(po[:, sl], lhsT=attT[:, h, :], rhs=v_[:, sl], start=True, stop=False), r=[attT.k, v_.k], w=[po.k], inc=False)
                    P.op("pe", lambda e: e.matmul(po[:, sl], lhsT=qdT[:, h, :], rhs=S[:, h, :], start=False, stop=True), r=[qdT.k, S.k], w=[po.k], inc=(h == 3))
                for h in range(4):
                    sl = slice(h * 128, (h + 1) * 128)
                    P.op("pe", lambda e: e.matmul(pS[0:64, sl], lhsT=ke[:, h * 64:(h + 1) * 64], rhs=v_[:, sl], start=True, stop=True), r=[ke.k, v_.k], w=[pS.k], inc=(h == 3))
                P.op("pool", lambda e: e.tensor_tensor(Sn[:], S[:], PCg[:, :].unsqueeze(2).to_broadcast([64, 4, 128]), mm), r=[S.k, PCg.k], w=[Sn.k])
                P.op("dve", lambda e: e.tensor_tensor(Sn[:], Sn[:], hv(pS[0:64, :], 4), ALU.add), r=[Sn.k, pS.k], w=[Sn.k])
                if g + 1 < C.NS * 4:
                    nxt = gproj((g + 1) // 4, (g + 1) % 4)
                P.op("act", lambda e: e.copy(o_[:], po[:, :]), r=[po.k], w=[o_.k])
                P.op("pool", lambda e: e.tensor_tensor(sq[:], o_[:], o_[:], mm), r=[o_.k], w=[sq.k])
                P.op("dve", lambda e: e.tensor_reduce(m4[:], hv(sq[:], 4), AX.X, ALU.add), r=[sq.k], w=[m4.k])
                P.op("act", lambda e: e.activation(out=m4[:], in_=m4[:], func=AF.Ln, bias=1e-5, scale=1.0 / 128), r=[m4.k], w=[m4.k])
                P.op("act", lambda e: e.activation(out=m4[:], in_=m4[:], func=AF.Exp, scale=-0.5), r=[m4.k], w=[m4.k])
                P.op("dve", lambda e: e.tensor_tensor(hv(o_[:], 4), hv(o_[:], 4), m4[:, :].unsqueeze(2).to_broadcast([128, 4, 128]), mm), r=[o_.k, m4.k], w=[o_.k])
                P.op("pool", lambda e: e.tensor_tensor(o_[:], o_[:], ngb[:], mm), r=[o_.k, ngb.k], w=[o_.k])
                P.op("dve", lambda e: e.tensor_tensor(o_[:], o_[:], sl_[:], mm), r=[o_.k, sl_.k], w=[o_.k])
                if "dbg_yb" in C.dbg:
                    P.dma("sp", C.dbg["dbg_yb"][g * 128:(g + 1) * 128, :], o_[:], r=[o_.k])
                pb = psb(C)
                for q in range(4):
                    P.op("pe", lambda e: e.transpose(pb[:, q * 128:(q + 1) * 128], o_[:, q * 128:(q + 1) * 128], C.ident[:]), r=[o_.k, C.ident_tok], w=[pb.k], inc=(q == 3))
                P.op("act", lambda e: e.copy(ys[:, :, t0:t0 + 128], hv(pb[:, :], 4)), r=[pb.k], w=[ys.k])
            P.dma("sp", XTv(C.YT)[:, 4:8, s * 512:(s + 1) * 512], ys[:], r=[ys.k], w=[C.YT_tok[1][s]])
        P.barrier()


def ln_inplace(C, xt, gb, bb, st, junk):
    P = C.P
    P.op("dve", lambda e: e.tensor_reduce(st[:, 0:1], xt[:], AX.X, ALU.add), r=[xt.k], w=[st.k])
    P.op("dve", lambda e: e.tensor_scalar(st[:, 0:1], st[:, 0:1], 1.0 / D, None, ALU.mult), r=[st.k], w=[st.k])
    P.op("dve", lambda e: e.tensor_scalar(xt[:], xt[:], st[:, 0:1], None, ALU.subtract), r=[xt.k, st.k], w=[xt.k])
    P.op("act", lambda e: e.activation(out=junk[:], in_=xt[:], func=AF.Square, accum_out=st[:, 1:2]), r=[xt.k], w=[junk.k, st.k])
    P.op("act", lambda e: e.activation(out=st[:, 1:2], in_=st[:, 1:2], func=AF.Sqrt, bias=LN_EPS, scale=1.0 / D), r=[st.k], w=[st.k])
    P.op("dve", lambda e: e.reciprocal(st[:, 1:2], st[:, 1:2]), r=[st.k], w=[st.k])
    P.op("dve", lambda e: e.scalar_tensor_tensor(xt[:], xt[:], st[:, 1:2], gb[:], ALU.mult, ALU.mult), r=[xt.k, st.k, gb.k], w=[xt.k])
    P.op("pool", lambda e: e.tensor_tensor(xt[:], xt[:], bb[:], ALU.add), r=[xt.k, bb.k], w=[xt.k])


def ln_lockstep(C, xs, gb, bb, sts, junk):
    P = C.P
    n = len(xs)
    for k in range(n):
        xt, st = xs[k], sts[k]
        P.op("dve", lambda e: e.tensor_reduce(st[:, 0:1], xt[:], AX.X, ALU.add), r=[xt.k], w=[st.k])
    for k in range(n):
        xt, st = xs[k], sts[k]
        P.op("dve", lambda e: e.tensor_scalar(st[:, 0:1], st[:, 0:1], 1.0 / D, None, ALU.mult), r=[st.k], w=[st.k])
    for k in range(n):
        xt, st = xs[k], sts[k]
        P.op("dve", lambda e: e.tensor_scalar(xt[:], xt[:], st[:, 0:1], None, ALU.subtract), r=[xt.k, st.k], w=[xt.k])
        P.op("act", lambda e: e.activation(out=junk[:], in_=xt[:], func=AF.Square, accum_out=st[:, 1:2]), r=[xt.k], w=[junk.k, st.k])
    for k in range(n):
        xt, st = xs[k], sts[k]
        P.op("act", lambda e: e.activation(out=st[:, 1:2], in_=st[:, 1:2], func=AF.Sqrt, bias=LN_EPS, scale=1.0 / D), r=[st.k], w=[st.k])
    for k in range(n):
        xt, st = xs[k], sts[k]
        P.op("dve", lambda e: e.reciprocal(st[:, 1:2], st[:, 1:2]), r=[st.k], w=[st.k])
    for k in range(n):
        xt, st = xs[k], sts[k]
        P.op("dve", lambda e: e.scalar_tensor_tensor(xt[:], xt[:], st[:, 1:2], gb[:], ALU.mult, ALU.mult), r=[xt.k, st.k, gb.k], w=[xt.k])
        P.op("pool", lambda e: e.tensor_tensor(xt[:], xt[:], bb[:], ALU.add), r=[xt.k, bb.k], w=[xt.k])


def tile_to_stage(C, xt, stage, j):
    P = C.P
    for half in range(2):
        pb = psb(C)
        for c4 in range(4):
            c = half * 4 + c4
            P.op("pe", lambda e: e.transpose(pb[:, c4 * 128:(c4 + 1) * 128], xt[:, c * 128:(c + 1) * 128], C.ident[:]),
                 r=[xt.k, C.ident_tok], w=[pb.k], inc=(c4 == 3))
        dst = stage[:, half * 4:(half + 1) * 4, j * 128:(j + 1) * 128]
        src = hv(pb[:, :], 4)
        if half == 0:
            P.op("act", lambda e: e.copy(dst, src), r=[pb.k], w=[stage.k])
        else:
            P.op("dve", lambda e: e.tensor_copy(dst, src), r=[pb.k], w=[stage.k])


def phase_outproj(C, w_out, srcYT, srcYT_toks, resid, resid_toks, lng, lnb, dstH, dstH_tok, dstHT, dstHT_tok, dbgname=None):
    nc, P, T, I = C.nc, C.P, C.T, C.I
    with ExitStack() as es:
        WO = mk(P, [128, 8, D], BF16, "WO", es)
        for c in range(8):
            P.dma("pool", WO[:, c, :], w_out[c * 128:(c + 1) * 128, :], w=[WO.k])
        gb = mk(P, [128, D], F32, "lng", es)
        bb = mk(P, [128, D], F32, "lnb", es)
        bcast_load(C, gb[:], lng, 128, gb.k)
        bcast_load(C, bb[:], lnb, 128, bb.k)
        yts = [mk(P, [128, 8, 512], BF16, "yt", es) for _ in range(2)]
        xts = [mk(P, [128, D], F32, "xres", es) for _ in range(8)]
        sts = [mk(P, [128, 2], F32, "lnst", es) for _ in range(8)]
        junk = mk(P, [128, D], BF16, "junk", es)
        stg = [mk(P, [128, 8, 512], BF16, "hstg", es) for _ in range(2)]
        pend = None

        def loads(s):
            P.dma("sp", yts[s % 2][:], XTv(srcYT)[:, :, s * 512:(s + 1) * 512], r=srcYT_toks(s), w=[yts[s % 2].k])
            for j in range(4):
                i = s * 4 + j
                xt = xts[(s % 2) * 4 + j]
                P.dma("sp", xt[:], resid[i * 128:(i + 1) * 128, :], r=resid_toks(i), w=[xt.k])

        loads(0)
        for s in range(C.NS):
            yt = yts[s % 2]
            sg_ = stg[s % 2]
            X = xts[(s % 2) * 4:(s % 2) * 4 + 4]
            S = sts[(s % 2) * 4:(s % 2) * 4 + 4]
            for j in range(4):
                xt = X[j]
                for half in range(2):
                    pb = psb(C)
                    for c in range(8):
                        P.op("pe", lambda e: e.matmul(pb[:, :], lhsT=yt[:, c, j * 128:(j + 1) * 128], rhs=WO[:, c, half * 512:(half + 1) * 512], start=(c == 0), stop=(c == 7)),
                             r=[yt.k, WO.k], w=[pb.k], inc=(c == 7))
                    P.op("dve", lambda e: e.scalar_tensor_tensor(xt[:, half * 512:(half + 1) * 512], xt[:, half * 512:(half + 1) * 512], DN_ALPHA, pb[:, :], ALU.mult, ALU.add),
                         r=[xt.k, pb.k], w=[xt.k])
            if pend is not None:
                pend()
            if s + 1 < C.NS:
                loads(s + 1)
            ln_lockstep(C, X, gb, bb, S, junk)
            for j in range(4):
                i = s * 4 + j
                P.dma("sp", dstH[i * 128:(i + 1) * 128, :], X[j][:], r=[X[j].k], w=[dstH_tok[i]])

            def pend(s=s, X=X, sg_=sg_):
                for j in range(4):
                    tile_to_stage(C, X[j], sg_, j)
                P.dma("sp", XTv(dstHT)[:, :, s * 512:(s + 1) * 512], sg_[:], r=[sg_.k], w=[dstHT_tok[s]])
        pend()
        P.barrier()


def phase_moe(C, l, srcH, srcH_tok, srcHT, srcHT_tok, dstX, dstX_tok, dstXT, dstXT_tok):
    nc, P, T, I = C.nc, C.P, C.T, C.I
    mm = ALU.mult
    with ExitStack() as es:
        WD = mk(P, [128, 32, D], BF16, "WD", es)
        for c4 in range(4):
            P.dma("sp", WD[:, c4 * 8:(c4 + 1) * 8, :], C.WD16[l].rearrange("p (c d) -> p c d", c=32)[:, c4 * 8:(c4 + 1) * 8, :], r=[C.WD16_tok[l]], w=[WD.k])
        RW = mk(P, [128, 8, NE], BF16, "RW", es)
        P.dma("pool", RW[:], I["router_w"].rearrange("(c p) e -> p c e", p=128), w=[RW.k])
        rbb = mk(P, [128, NE], F32, "rbb", es)
        bcast_load(C, rbb[:], I["router_bias"], 128, rbb.k)
        SEL = mk(P, [16, 16, 128], BF16, "SEL", es)
        P.dma("pool", SEL[:], I["c_sel"], w=[SEL.k])
        gb = mk(P, [128, D], F32, "lng", es)
        bb = mk(P, [128, D], F32, "lnb", es)
        bcast_load(C, gb[:], I["ln2_g"][l:l + 1, :], 128, gb.k)
        bcast_load(C, bb[:], I["ln2_b"][l:l + 1, :], 128, bb.k)
        hts = [mk(P, [128, 8, 512], BF16, "hT", es) for _ in range(2)]
        xts = [mk(P, [128, D], F32, "hres", es) for _ in range(4)]
        sts = [mk(P, [128, 2], F32, "lnst", es) for _ in range(4)]
        junk = mk(P, [128, D], BF16, "junk", es)
        stg = [mk(P, [128, 8, 512], BF16, "xstg", es) for _ in range(2)]
        actT = mk(P, [128, 32, 512], BF16, "actT", es)
        combT = mk(P, [16, 512], BF16, "combT", es)
        WGUs = [mk(P, [128, 2, 8, DE], BF16, "WGU", es) for _ in range(3)]
        sgl = [mk(P, [128, 512], F32, "sgl", es) for _ in range(2)]
        R4 = range(4)
        s_l = [mk(P, [128, NE], F32, "rs", es) for _ in R4]
        sel_l = [mk(P, [128, NE], F32, "rsel", es) for _ in R4]
        pr_l = [mk(P, [128, 4, 6], F32, "rpr", es) for _ in R4]
        gs_l = [mk(P, [128, 4], F32, "rgs", es) for _ in R4]
        t1_l = [mk(P, [128, 4], F32, "rt1", es) for _ in R4]
        m1_l = [mk(P, [128, 2], F32, "rm1", es) for _ in R4]
        selm_l = [mk(P, [128, NE], F32, "rselm", es) for _ in R4]
        sel2_l = [mk(P, [128, NE], F32, "rsel2", es) for _ in R4]
        comb_l = [mk(P, [128, NE], F32, "rcomb", es) for _ in R4]
        nwl = [0]

        def load_w(e):
            b = nwl[0] % 3
            nwl[0] += 1
            P.dma("sp", WGUs[b][:], C.WGU16[l][e].rearrange("p (t c f) -> p t c f", t=2, c=8), r=[C.WGU16_tok[l][e]], w=[WGUs[b].k])
            return WGUs[b]

        def g4(t):
            return t[:, :].rearrange("p (g e) -> p g e", g=4)

        def load_hT(s):
            P.dma("sp", hts[s % 2][:], XTv(srcHT)[:, :, s * 512:(s + 1) * 512], r=[srcHT_tok[s]], w=[hts[s % 2].k])

        def router_front(s):
            hT = hts[s % 2]
            for j in R4:
                plg = psb(C)
                s_ = s_l[j]
                for c in range(8):
                    P.op("pe", lambda e: e.matmul(plg[:, 0:NE], lhsT=hT[:, c, j * 128:(j + 1) * 128], rhs=RW[:, c, :], start=(c == 0), stop=(c == 7)),
                         r=[hT.k, RW.k], w=[plg.k], inc=(c == 7))
                P.op("act", lambda e: e.activation(out=s_[:], in_=plg[:, 0:NE], func=AF.Sigmoid), r=[plg.k], w=[s_.k])

            def step(fn):
                for j in R4:
                    fn(s_l[j], sel_l[j], pr_l[j], gs_l[j], t1_l[j], m1_l[j], selm_l[j], sel2_l[j], comb_l[j])
            step(lambda s_, sel, pr, gs, t1, m1, selm, sel2, comb: P.op("dve", lambda e: e.tensor_tensor(sel[:], s_[:], rbb[:], ALU.add), r=[s_.k, rbb.k], w=[sel.k]))
            step(lambda s_, sel, pr, gs, t1, m1, selm, sel2, comb: P.op("dve", lambda e: e.tensor_tensor(pr[:, :, 0:3], g4(sel)[:, :, 0:3], g4(sel)[:, :, 1:4], ALU.add), r=[sel.k], w=[pr.k]))
            step(lambda s_, sel, pr, gs, t1, m1, selm, sel2, comb: P.op("dve", lambda e: e.tensor_tensor(pr[:, :, 3:5], g4(sel)[:, :, 0:2], g4(sel)[:, :, 2:4], ALU.add), r=[sel.k], w=[pr.k]))
            step(lambda s_, sel, pr, gs, t1, m1, selm, sel2, comb: P.op("dve", lambda e: e.tensor_tensor(pr[:, :, 5:6], g4(sel)[:, :, 0:1], g4(sel)[:, :, 3:4], ALU.add), r=[sel.k], w=[pr.k]))
            step(lambda s_, sel, pr, gs, t1, m1, selm, sel2, comb: P.op("dve", lambda e: e.tensor_reduce(gs[:], pr[:], AX.X, ALU.max), r=[pr.k], w=[gs.k]))
            step(lambda s_, sel, pr, gs, t1, m1, selm, sel2, comb: P.op("dve", lambda e: e.tensor_reduce(m1[:, 0:1], gs[:], AX.X, ALU.max), r=[gs.k], w=[m1.k]))
            step(lambda s_, sel, pr, gs, t1, m1, selm, sel2, comb: P.op("dve", lambda e: e.tensor_scalar(gs[:], gs[:], m1[:, 0:1], None, ALU.is_ge), r=[gs.k, m1.k], w=[gs.k]))
            step(lambda s_, sel, pr, gs, t1, m1, selm, sel2, comb: P.op("dve", lambda e: e.tensor_scalar(t1[:], gs[:], -1.0, 1e30, ALU.add, ALU.mult), r=[gs.k], w=[t1.k]))
            step(lambda s_, sel, pr, gs, t1, m1, selm, sel2, comb: P.op("dve", lambda e: e.tensor_tensor(g4(selm), g4(sel), gs[:, :].unsqueeze(2).to_broadcast([128, 4, 4]), mm), r=[sel.k, gs.k], w=[selm.k]))
            step(lambda s_, sel, pr, gs, t1, m1, selm, sel2, comb: P.op("dve", lambda e: e.tensor_tensor(g4(selm), g4(selm), t1[:, :].unsqueeze(2).to_broadcast([128, 4, 4]), ALU.add), r=[selm.k, t1.k], w=[selm.k]))
            step(lambda s_, sel, pr, gs, t1, m1, selm, sel2, comb: P.op("dve", lambda e: e.tensor_reduce(m1[:, 0:1], selm[:], AX.X, ALU.max), r=[selm.k], w=[m1.k]))
            step(lambda s_, sel, pr, gs, t1, m1, selm, sel2, comb: P.op("dve", lambda e: e.tensor_scalar(sel2[:], selm[:], m1[:, 0:1], None, ALU.is_ge), r=[selm.k, m1.k], w=[sel2.k]))
            step(lambda s_, sel, pr, gs, t1, m1, selm, sel2, comb: P.op("dve", lambda e: e.scalar_tensor_tensor(sel2[:], sel2[:], -1e30, selm[:], mm, ALU.add), r=[sel2.k, selm.k], w=[sel2.k]))
            step(lambda s_, sel, pr, gs, t1, m1, selm, sel2, comb: P.op("dve", lambda e: e.tensor_reduce(m1[:, 1:2], sel2[:], AX.X, ALU.max), r=[sel2.k], w=[m1.k]))
            step(lambda s_, sel, pr, gs, t1, m1, selm, sel2, comb: P.op("dve", lambda e: e.tensor_scalar(sel2[:], selm[:], m1[:, 1:2], None, ALU.is_ge), r=[selm.k, m1.k], w=[sel2.k]))
            step(lambda s_, sel, pr, gs, t1, m1, selm, sel2, comb: P.op("dve", lambda e: e.tensor_tensor(comb[:], s_[:], sel2[:], mm), r=[s_.k, sel2.k], w=[comb.k]))
            step(lambda s_, sel, pr, gs, t1, m1, selm, sel2, comb: P.op("dve", lambda e: e.tensor_reduce(m1[:, 0:1], comb[:], AX.X, ALU.add), r=[comb.k], w=[m1.k]))
            step(lambda s_, sel, pr, gs, t1, m1, selm, sel2, comb: P.op("dve", lambda e: e.reciprocal(m1[:, 0:1], m1[:, 0:1]), r=[m1.k], w=[m1.k]))
            step(lambda s_, sel, pr, gs, t1, m1, selm, sel2, comb: P.op("dve", lambda e: e.tensor_scalar(comb[:], comb[:], m1[:, 0:1], None, mm), r=[comb.k, m1.k], w=[comb.k]))

        def router_back(s):
            for j in R4:
                comb = comb_l[j]
                pct = psb(C)
                P.op("pe", lambda e: e.transpose(pct[0:16, 0:128], comb[:, :], C.ident[:]), r=[comb.k, C.ident_tok], w=[pct.k])
                P.op("act", lambda e: e.copy(combT[:, j * 128:(j + 1) * 128], pct[0:16, 0:128]), r=[pct.k], w=[combT.k])

        def load_x(s):
            for j in R4:
                i = s * 4 + j
                P.dma("sp", xts[j][:], srcH[i * 128:(i + 1) * 128, :], r=[srcH_tok[i]], w=[xts[j].k])

        def make_pend(s):
            xs_ = stg[s % 2]

            def pend():
                for j in R4:
                    tile_to_stage(C, xts[j], xs_, j)
                P.dma("sp", XTv(dstXT)[:, :, s * 512:(s + 1) * 512], xs_[:], r=[xs_.k], w=[dstXT_tok[s]])
            return pend

        pend = None
        pre = []
        load_hT(0)
        router_front(0)
        router_back(0)
        for s in range(C.NS):
            hT = hts[s % 2]
            if s + 1 < C.NS:
                load_hT(s + 1)
            for ex in range(NE):
                WGU = pre.pop(0) if pre else load_w(ex)
                pcb = psb(C)
                P.op("pe", lambda e: e.matmul(pcb[:, :], lhsT=SEL[:, ex, :], rhs=combT[:, :], start=True, stop=True), r=[SEL.k, combT.k], w=[pcb.k])
                for f in range(2):
                    pG, pU = psb(C), psb(C)
                    for pb, ti in ((pG, 0), (pU, 1)):
                        for c in range(8):
                            P.op("pe", lambda e: e.matmul(pb[:, :], lhsT=WGU[:, ti, c, f * 128:(f + 1) * 128], rhs=hT[:, c, :], start=(c == 0), stop=(c == 7)),
                                 r=[WGU.k, hT.k], w=[pb.k], inc=(c == 7))
                    sg_ = sgl[(ex * 2 + f) % 2]
                    P.op("act", lambda e: e.activation(out=sg_[:], in_=pG[:, :], func=AF.Silu), r=[pG.k], w=[sg_.k])
                    P.op("dve", lambda e: e.tensor_tensor(sg_[:], sg_[:], pU[:, :], mm), r=[sg_.k, pU.k], w=[sg_.k])
                    P.op("dve", lambda e: e.tensor_tensor(actT[:, ex * 2 + f, :], sg_[:], pcb[:, :], mm), r=[sg_.k, pcb.k], w=[actT.k])
                if ex == 3:
                    if pend is not None:
                        pend()
                        pend = None
                    load_x(s)
            if s + 1 < C.NS:
                pre.extend(load_w(e_) for e_ in range(3))
            if s + 1 < C.NS:
                router_front(s + 1)
            for j in R4:
                xt = xts[j]
                for half in range(2):
                    pb = psb(C)
                    for c in range(32):
                        P.op("pe", lambda e: e.matmul(pb[:, :], lhsT=actT[:, c, j * 128:(j + 1) * 128], rhs=WD[:, c, half * 512:(half + 1) * 512], start=(c == 0), stop=(c == 31)),
                             r=[actT.k, WD.k], w=[pb.k], inc=(c == 31))
                    P.op("dve", lambda e: e.scalar_tensor_tensor(xt[:, half * 512:(half + 1) * 512], xt[:, half * 512:(half + 1) * 512], DN_ALPHA, pb[:, :], ALU.mult, ALU.add),
                         r=[xt.k, pb.k], w=[xt.k])
            if s + 1 < C.NS:
                router_back(s + 1)
            ln_lockstep(C, xts, gb, bb, sts, junk)
            for j in R4:
                i = s * 4 + j
                P.dma("sp", dstX[i * 128:(i + 1) * 128, :], xts[j][:], r=[xts[j].k], w=[dstX_tok[i]])
            if dstXT is not None:
                pend = make_pend(s)
        if pend is not None:
            pend()
        P.barrier()


def rope_consts(T):
    pos = np.arange(T, dtype=np.float64)
    c = {}
    for name, half in (("k", 64), ("i", 32)):
        inv = 10000.0 ** (-np.arange(half, dtype=np.float64) / half)
        ang = (pos.astype(np.float32)[:, None] * inv.astype(np.float32)[None, :]).astype(np.float32).astype(np.float64)
        cs = np.cos(ang).astype(np.float32).reshape(T // 128, 128, half).transpose(1, 0, 2)
        sn = np.sin(ang).astype(np.float32).reshape(T // 128, 128, half).transpose(1, 0, 2)
        c["cos_" + name] = np.ascontiguousarray(cs)
        c["sin_" + name] = np.ascontiguousarray(sn)
    q = np.arange(128)[:, None]
    s = np.arange(128)[None, :]
    c["cbias"] = np.where(s <= q, 0.0, -1e30).astype(np.float32)
    c["halfpow"] = (0.5 ** np.arange(1, 33, dtype=np.float64)).astype(np.float32).reshape(1, 32)
    c["tiebias"] = (-1e-6 * np.arange(T, dtype=np.float64)).astype(np.float32).reshape(1, T)
    return c


def rope_tm(C, dst_ap, dst_k, src, src_k, cosb, sinb, nh, half, ta_ap, ta_k, tb_ap, tb_k, rdeps):
    P = C.P
    mm = ALU.mult
    n = nh * 2
    s3 = src.rearrange("p (n f) -> p n f", n=n)
    cb = cosb.unsqueeze(1).to_broadcast([128, n, half])
    sb_ = sinb.unsqueeze(1).to_broadcast([128, n, half])
    a3 = ta_ap.rearrange("p (n f) -> p n f", n=n)
    b3 = tb_ap.rearrange("p (n f) -> p n f", n=n)
    P.op("dve", lambda e: e.tensor_tensor(a3, s3, cb, mm), r=[src_k] + rdeps, w=[ta_k])
    P.op("dve", lambda e: e.tensor_tensor(b3, s3, sb_, mm), r=[src_k] + rdeps, w=[tb_k])
    a4 = ta_ap.rearrange("p (h t f) -> p h t f", h=nh, t=2)
    b4 = tb_ap.rearrange("p (h t f) -> p h t f", h=nh, t=2)
    d4 = dst_ap.rearrange("p (h t f) -> p h t f", h=nh, t=2)
    P.op("pool", lambda e: e.tensor_tensor(d4[:, :, 0, :], a4[:, :, 0, :], b4[:, :, 1, :], ALU.subtract), r=[ta_k, tb_k], w=[dst_k])
    P.op("pool", lambda e: e.tensor_tensor(d4[:, :, 1, :], a4[:, :, 1, :], b4[:, :, 0, :], ALU.add), r=[ta_k, tb_k], w=[dst_k])


def phase_dsa(C):
    nc, P, T, I = C.nc, C.P, C.T, C.I
    mm = ALU.mult
    KT = min(256, T // 4)
    NIT = 25
    SCALE = 128 ** -0.5
    C.bank_pool = [0, 1, 2, 3, 4]
    with ExitStack() as es:
        def wt(name, shape, dt=F32):
            return mk(P, list(shape), dt, name, es)
        WQ = wt("WQ", (128, 8, 1024), BF16)
        WR = wt("WR", (128, 8, 580), BF16)
        for c in range(8):
            P.dma("pool", WQ[:, c, :], I["w_in_odd"][c * 128:(c + 1) * 128, 0:1024], w=[WQ.k])
            P.dma("pool", WR[:, c, :], I["w_in_odd"][c * 128:(c + 1) * 128, 1024:1604], w=[WR.k])
        RTs = [[wt("CK", (128, 4, 64)), wt("SK", (128, 4, 64)), wt("CI", (128, 4, 32)), wt("SI", (128, 4, 32))] for _ in range(2)]

        def load_tables(s):
            tl = RTs[s % 2]
            for t_, nm in zip(tl, ("c_cos_k", "c_sin_k", "c_cos_i", "c_sin_i")):
                P.dma("sp", t_[:], I[nm][:, s * 4:(s + 1) * 4, :], w=[t_.k])
            return tl
        BIAS = wt("BIAS", (128, T))
        bcast_load(C, BIAS[:], I["c_tiebias"], 128, BIAS.k)
        CB = wt("CB", (128, 128))
        P.dma("sp", CB[:], I["c_cbias"], w=[CB.k])
        ikg, ikb = wt("ikg", (128, 64)), wt("ikb", (128, 64))
        bcast_load(C, ikg[:], I["c_ik_ln_g"], 128, ikg.k)
        bcast_load(C, ikb[:], I["c_ik_ln_b"], 128, ikb.k)
        kT = wt("kT", (128, T), BF16)
        ikT = wt("ikT", (128, T), BF16)
        Vx = wt("Vx", (128, C.NT, 129), BF16)
        P.op("dve", lambda e: e.memset(Vx[:, :, 128:129], 1.0), w=[Vx.k])
        xTs = [wt("xTd", (128, 8, 512), BF16) for _ in range(2)]
        ta, tb = wt("ropeA", (128, 512)), wt("ropeB", (128, 512))
        kr = wt("kr", (128, 128))
        ikr = wt("ikr", (128, 128))
        st2 = wt("st2", (128, 2))
        for s in range(C.NS):
            xT = xTs[s % 2]
            P.dma("sp", xT[:], XTv(C.XT1)[:, :, s * 512:(s + 1) * 512], r=[C.XT1_tok[s]], w=[xT.k])
            CK, SK, CI, SI = load_tables(s)
            for j in range(4):
                i = s * 4 + j
                pb = psb(C)
                for c in range(8):
                    P.op("pe", lambda e: e.matmul(pb[:, 0:256], lhsT=xT[:, c, j * 128:(j + 1) * 128], rhs=WR[:, c, 0:256], start=(c == 0), stop=(c == 7)),
                         r=[xT.k, WR.k], w=[pb.k], inc=False)
                for c in range(8):
                    P.op("pe", lambda e: e.matmul(pb[:, 256:320], lhsT=xT[:, c, j * 128:(j + 1) * 128], rhs=WR[:, c, 512:576], start=(c == 0), stop=(c == 7)),
                         r=[xT.k, WR.k], w=[pb.k], inc=(c == 7))
                rope_tm(C, kr[:, :], kr.k, pb[:, 0:128], pb.k, CK[:, j, :], SK[:, j, :], 1, 64, ta[:, 0:128], ta.k, tb[:, 0:128], tb.k, [CK.k, SK.k])
                P.op("act", lambda e: e.copy(Vx[:, i, 0:128], pb[:, 128:256]), r=[pb.k], w=[Vx.k])
                P.op("dve", lambda e: e.tensor_reduce(st2[:, 0:1], pb[:, 256:320], AX.X, ALU.add), r=[pb.k], w=[st2.k])
                P.op("dve", lambda e: e.tensor_scalar(st2[:, 0:1], st2[:, 0:1], 1.0 / 64, None, mm), r=[st2.k], w=[st2.k])
                P.op("dve", lambda e: e.tensor_scalar(ikr[:, 0:64], pb[:, 256:320], st2[:, 0:1], None, ALU.subtract), r=[pb.k, st2.k], w=[ikr.k])
                P.op("act", lambda e: e.activation(out=ikr[:, 64:128], in_=ikr[:, 0:64], func=AF.Square, accum_out=st2[:, 1:2]), r=[ikr.k], w=[ikr.k, st2.k])
                P.op("act", lambda e: e.activation(out=st2[:, 1:2], in_=st2[:, 1:2], func=AF.Sqrt, bias=LN_EPS, scale=1.0 / 64), r=[st2.k], w=[st2.k])
                P.op("dve", lambda e: e.reciprocal(st2[:, 1:2], st2[:, 1:2]), r=[st2.k], w=[st2.k])
                P.op("dve", lambda e: e.scalar_tensor_tensor(ikr[:, 0:64], ikr[:, 0:64], st2[:, 1:2], ikg[:], mm, mm), r=[ikr.k, st2.k, ikg.k], w=[ikr.k])
                P.op("dve", lambda e: e.tensor_tensor(ikr[:, 64:128], ikr[:, 0:64], ikb[:], ALU.add), r=[ikr.k, ikb.k], w=[ikr.k])
                ikn = TL(ikr.t, ikr.k)
                rope_src = ikr[:, 64:128]
                n = 2
                s3 = rope_src.rearrange("p (n f) -> p n f", n=n)
                cb = CI[:, j, :].unsqueeze(1).to_broadcast([128, n, 32])
                sb_ = SI[:, j, :].unsqueeze(1).to_broadcast([128, n, 32])
                a3 = ta[:, 0:64].rearrange("p (n f) -> p n f", n=n)
                b3 = tb[:, 0:64].rearrange("p (n f) -> p n f", n=n)
                P.op("dve", lambda e: e.tensor_tensor(a3, s3, cb, mm), r=[ikr.k, CI.k], w=[ta.k])
                P.op("dve", lambda e: e.tensor_tensor(b3, s3, sb_, mm), r=[ikr.k, SI.k], w=[tb.k])
                P.op("pool", lambda e: e.tensor_tensor(ikr[:, 0:32], ta[:, 0:32], tb[:, 32:64], ALU.subtract), r=[ta.k, tb.k], w=[ikr.k])
                P.op("pool", lambda e: e.tensor_tensor(ikr[:, 32:64], ta[:, 32:64], tb[:, 0:32], ALU.add), r=[ta.k, tb.k], w=[ikr.k])
                P.op("pool", lambda e: e.tensor_copy(ikr[:, 64:128], ikr[:, 0:64]), r=[ikr.k], w=[ikr.k])
                pt = psb(C)
                P.op("pe", lambda e: e.transpose(pt[:, 0:128], kr[:, :], C.ident[:]), r=[kr.k, C.ident_tok], w=[pt.k], inc=False)
                P.op("pe", lambda e: e.transpose(pt[:, 128:256], ikr[:, :], C.ident[:]), r=[ikr.k, C.ident_tok], w=[pt.k])
                P.op("act", lambda e: e.copy(kT[:, i * 128:(i + 1) * 128], pt[:, 0:128]), r=[pt.k], w=[kT.k])
                P.op("act", lambda e: e.mul(ikT[:, i * 128:(i + 1) * 128], pt[:, 128:256], 0.125), r=[pt.k], w=[ikT.k])
        SC = wt("SC", (128, T))
        MASKs = [wt("MASK", (128, T)) for _ in range(2)]
        junk = wt("junkd", (128, T), BF16)
        qr = wt("qr", (128, 1024))
        iqr = wt("iqr", (128, 256))
        qTs = [wt("qT", (128, 8, 128), BF16) for _ in range(3)]
        iqT = wt("iqT", (128, 2, 128), BF16)
        iws = wt("iws", (128, 4))
        rl = [wt("rl%d" % n, (128, 512)) for n in range(2)]
        bs = wt("bs", (128, 8))
        Dk = wt("Dk", (128, NIT))
        HK = wt("HK", (128, NIT))
        bcast_load(C, HK[:], I["c_halfpow"][0:1, 0:NIT], 128, HK.k)
        V2 = [wt("V2_%d" % n, (128, 2)) for n in range(2)]
        W2 = wt("W2s", (128, 2))
        Ek = wt("Ek", (128, NIT, 2))
        P.op("dve", lambda e: e.memset(Ek[:], 0.0), w=[Ek.k])
        mT4 = [wt("mT4_%d" % n, (128, 4, 128), BF16) for n in range(2)]
        pTs = [wt("pT%d" % n, (128, 4, 128), BF16) for n in range(4)]
        o_ = wt("od", (128, 1024))
        rs8 = wt("rs8", (128, 8))
        ostg = [wt("ostg", (128, 8, 512), BF16) for _ in range(2)]
        accb = [TL(*C.banks[b]) for b in (5, 6, 7)]
        MBIG = 30000.0
        identb = wt("identb", (128, 128), BF16)
        P.op("dve", lambda e: e.tensor_copy(identb[:], C.ident[:]), r=[C.ident_tok], w=[identb.k])
        acc_of = [(0, 0), (0, 1), (0, 2), (1, 0), (1, 1), (1, 2), (2, 0), (2, 1)]
        def qproj(s, j):
            i = s * 4 + j
            xT = xTs[s % 2]
            qT = qTs[i % 3]
            if j == 0:
                P.dma("sp", xT[:], XTv(C.XT1)[:, :, s * 512:(s + 1) * 512], r=[C.XT1_tok[s]], w=[xT.k])
                load_tables(s)
            CK, SK, CI, SI = RTs[s % 2]
            pq = [psb(C), psb(C)]
            for half in range(2):
                for c in range(8):
                    P.op("pe", lambda e: e.matmul(pq[half][:, :], lhsT=xT[:, c, j * 128:(j + 1) * 128], rhs=WQ[:, c, half * 512:(half + 1) * 512], start=(c == 0), stop=(c == 7)),
                         r=[xT.k, WQ.k], w=[pq[half].k], inc=(c == 7))
            piq = psb(C)
            for c in range(8):
                P.op("pe", lambda e: e.matmul(piq[:, 0:256], lhsT=xT[:, c, j * 128:(j + 1) * 128], rhs=WR[:, c, 256:512], start=(c == 0), stop=(c == 7)),
                     r=[xT.k, WR.k], w=[piq.k], inc=False)
            for c in range(8):
                P.op("pe", lambda e: e.matmul(piq[:, 256:260], lhsT=xT[:, c, j * 128:(j + 1) * 128], rhs=WR[:, c, 576:580], start=(c == 0), stop=(c == 7)),
                     r=[xT.k, WR.k], w=[piq.k], inc=(c == 7))
            for half in range(2):
                hs = slice(half * 512, (half + 1) * 512)
                rope_tm(C, qr[:, hs], qr.k, pq[half][:, :], pq[half].k, CK[:, j, :], SK[:, j, :], 4, 64,
                        ta[:, 0:512], ta.k, tb[:, 0:512], tb.k, [CK.k, SK.k])
            rope_tm(C, iqr[:, :], iqr.k, piq[:, 0:256], piq.k, CI[:, j, :], SI[:, j, :], 4, 32, ta[:, 0:256], ta.k, tb[:, 0:256], tb.k, [CI.k, SI.k])
            P.op("act", lambda e: e.mul(iws[:], piq[:, 256:260], 0.5), r=[piq.k], w=[iws.k])
            for half in range(2):
                pb = psb(C)
                for c4 in range(4):
                    h = half * 4 + c4
                    P.op("pe", lambda e: e.transpose(pb[:, c4 * 128:(c4 + 1) * 128], qr[:, h * 128:(h + 1) * 128], C.ident[:]), r=[qr.k, C.ident_tok], w=[pb.k], inc=(c4 == 3))
                P.op("act", lambda e: e.copy(qT[:, half * 4:(half + 1) * 4, :], hv(pb[:, :], 4)), r=[pb.k], w=[qT.k])
            pb = psb(C)
            for c2 in range(2):
                P.op("pe", lambda e: e.transpose(pb[:, c2 * 128:(c2 + 1) * 128], iqr[:, c2 * 128:(c2 + 1) * 128], C.ident[:]), r=[iqr.k, C.ident_tok], w=[pb.k], inc=(c2 == 1))
            P.op("act", lambda e: e.copy(iqT[:], hv(pb[:, 0:256], 2)), r=[pb.k], w=[iqT.k])

        NB = C.NS * 4

        def qproj_next(i):
            if i + 1 < NB:
                qproj((i + 1) // 4, (i + 1) % 4)

        def qblock(s, j):
            if True:
                i = s * 4 + j
                L = (i + 1) * 128
                og = ostg[s % 2]
                qT = qTs[i % 3]
                MASK = MASKs[i % 2]
                cast_some(C, 1, 2)
                if i == 0:
                    qproj(0, 0)
                yield "F"
                for k0 in range(0, L, 512):
                    kw = min(512, L - k0)
                    for h in range(4):
                        ph = psb(C)
                        pl = (h % 2) * 64
                        P.op("pe", lambda e: e.matmul(ph[:, 0:kw], lhsT=iqT[pl:pl + 64, h // 2, :], rhs=ikT[pl:pl + 64, k0:k0 + kw], start=True, stop=True),
                             r=[iqT.k, ikT.k], w=[ph.k])
                        r_ = rl[h % 2]
                        P.op("act", lambda e: e.activation(out=r_[:, 0:kw], in_=ph[:, 0:kw], func=AF.Relu), r=[ph.k], w=[r_.k])
                        if h == 0:
                            P.op("dve", lambda e: e.scalar_tensor_tensor(SC[:, k0:k0 + kw], r_[:, 0:kw], iws[:, 0:1], BIAS[:, k0:k0 + kw], mm, ALU.add), r=[r_.k, iws.k, BIAS.k], w=[SC.k])
                        else:
                            P.op("dve", lambda e: e.scalar_tensor_tensor(SC[:, k0:k0 + kw], r_[:, 0:kw], iws[:, h:h + 1], SC[:, k0:k0 + kw], mm, ALU.add),
                                 r=[r_.k, iws.k, SC.k], w=[SC.k])
                    yield "F"
                if L > KT:
                    P.op("dve", lambda e: e.tensor_reduce(bs[:, 1:2], SC[:, 0:L], AX.X, ALU.max, apply_absolute_value=True), r=[SC.k], w=[bs.k])
                    P.op("dve", lambda e: e.tensor_scalar(bs[:, 0:1], bs[:, 1:2], -1.0, -1.0, mm, ALU.add), r=[bs.k], w=[bs.k])
                    P.op("dve", lambda e: e.tensor_scalar(bs[:, 1:2], bs[:, 1:2], 2.0, 2.0, mm, ALU.add), r=[bs.k], w=[bs.k])
                    P.op("dve", lambda e: e.tensor_scalar(Dk[:], HK[:], bs[:, 1:2], None, mm), r=[bs.k, HK.k], w=[Dk.k])
                P.op("pool", lambda e: e.tensor_tensor(SC[:, i * 128:L], SC[:, i * 128:L], CB[:], ALU.add), r=[SC.k, CB.k], w=[SC.k])
                if L > KT:
                    P.op("dve", lambda e: e.tensor_copy(Ek[:, 0:NIT - 1, 1:2], Dk[:, 1:NIT].unsqueeze(2)), r=[Dk.k], w=[Ek.k])
                    P.op("dve", lambda e: e.tensor_copy(V2[0][:, 0:1], bs[:, 0:1]), r=[bs.k], w=[V2[0].k])
                    P.op("dve", lambda e: e.tensor_tensor(V2[0][:, 1:2], bs[:, 0:1], Dk[:, 0:1], ALU.add), r=[bs.k, Dk.k], w=[V2[0].k])
                    for it in range(NIT):
                        Vc, Vn = V2[it % 2], V2[(it + 1) % 2]
                        P.op("dve", lambda e: e.tensor_scalar(junk[:, 0:L], SC[:, 0:L], Vc[:, 1:2], None, ALU.is_gt, ALU.add, accum_out=bs[:, 3:4]),
                             r=[SC.k, Vc.k], w=[junk.k, bs.k])
                        P.op("dve", lambda e: e.scalar_tensor_tensor(W2[:], bs[:, 3:4].to_broadcast([128, 2]), float(KT) - 0.5, Dk[:, it:it + 1].to_broadcast([128, 2]), ALU.is_gt, mm),
                             r=[bs.k, Dk.k], w=[W2.k])
                        P.op("dve", lambda e: e.scalar_tensor_tensor(Vn[:], W2[:], Vc[:, 0:1], Ek[:, it, :], ALU.add, ALU.add), r=[W2.k, Vc.k, Ek.k], w=[Vn.k])
                        if it == 4:
                            qproj_next(i)
                        yield "F"
                    Vf = V2[NIT % 2]
                    P.op("dve", lambda e: e.tensor_scalar(MASK[:, 0:L], SC[:, 0:L], Vf[:, 0:1], None, ALU.is_le), r=[SC.k, Vf.k], w=[MASK.k])
                else:
                    P.op("dve", lambda e: e.tensor_scalar(MASK[:, 0:L], SC[:, 0:L], -1e29, None, ALU.is_le), r=[SC.k], w=[MASK.k])
                    qproj_next(i)
                if "dbg_mask" in C.dbg:
                    P.dma("sp", C.dbg["dbg_mask"][i * 128:(i + 1) * 128, 0:L], MASK[:, 0:L], r=[MASK.k])
                    P.dma("sp", C.dbg["dbg_sc"][i * 128:(i + 1) * 128, 0:L], SC[:, 0:L], r=[SC.k])
                yield "END_FRONT"
                units = [(st, hg) for st in range(i + 1) for hg in range(2)]

                def stage1(st, hg):
                    if hg == 0 and st % 4 == 0:
                        n4 = min(4, i + 1 - st)
                        m4 = mT4[(st // 4) % 2]
                        pb = psb(C)
                        for u in range(n4):
                            P.op("pe", lambda e: e.transpose(pb[:, u * 128:(u + 1) * 128], MASK[:, (st + u) * 128:(st + u + 1) * 128], C.ident[:]),
                                 r=[MASK.k, C.ident_tok], w=[pb.k], inc=(u == n4 - 1))
                        P.op("act", lambda e: e.mul(m4[:, 0:n4, :], hv(pb[:, :], 4)[:, 0:n4, :], -MBIG), r=[pb.k], w=[m4.k])
                    m4 = mT4[(st // 4) % 2]
                    pl_ = psb(C)
                    P.op("pe", lambda e: e.matmul(pl_[:, :], lhsT=identb[:, :], rhs=m4[:, st % 4, :].unsqueeze(1).to_broadcast([128, 4, 128]), start=True, stop=False),
                         r=[identb.k, m4.k], w=[pl_.k], inc=False)
                    P.op("pe", lambda e: e.matmul(pl_[:, :], lhsT=kT[:, st * 128:(st + 1) * 128], rhs=qT[:, hg * 4:(hg + 1) * 4, :].rearrange("p h q -> p (h q)"), start=False, stop=True),
                         r=[kT.k, qT.k], w=[pl_.k])
                    pT = pTs[hg * 2 + st % 2]
                    P.op("act", lambda e: e.activation(out=pT[:], in_=hv(pl_[:, :], 4), func=AF.Exp, scale=SCALE), r=[pl_.k], w=[pT.k])

                def stage2(st, hg):
                    pT = pTs[hg * 2 + st % 2]
                    for hh in range(4):
                        h = hg * 4 + hh
                        ab, slot = acc_of[h]
                        P.op("pe", lambda e: e.matmul(accb[ab][:, slot * 129:(slot + 1) * 129], lhsT=pT[:, hh, :], rhs=Vx[:, st, :], start=(st == 0 and slot == 0), stop=(st == i), skip_group_check=True),
                             r=[pT.k, Vx.k], w=[accb[ab].k], inc=(hh == 3))

                stage1(*units[0])
                for n in range(len(units)):
                    if n + 1 < len(units):
                        stage1(*units[n + 1])
                    stage2(*units[n])
                    if units[n][1] == 1:
                        yield "B"
                for h in range(8):
                    ab, slot = acc_of[h]
                    P.op("dve", lambda e: e.reciprocal(rs8[:, h:h + 1], accb[ab][:, slot * 129 + 128:slot * 129 + 129]), r=[accb[ab].k], w=[rs8.k])
                    P.op("act", lambda e: e.activation(out=o_[:, h * 128:(h + 1) * 128], in_=accb[ab][:, slot * 129:slot * 129 + 128], func=AF.Copy, scale=rs8[:, h:h + 1]),
                         r=[accb[ab].k, rs8.k], w=[o_.k])
                if "dbg_dsa" in C.dbg:
                    P.dma("sp", C.dbg["dbg_dsa"][i * 128:(i + 1) * 128, :], o_[:], r=[o_.k])
                tile_to_stage(C, o_, og, j)
                if j == 3:
                    P.dma("sp", XTv(C.YT)[:, :, s * 512:(s + 1) * 512], og[:], r=[og.k], w=[C.YT_tok[0][s], C.YT_tok[1][s]])

        pipeline2([(lambda s=s, j=j: qblock(s, j)) for s in range(C.NS) for j in range(4)], interleave=C.dsa_interleave)
        P.barrier()
    C.bank_pool = list(range(8))


class _View:
    def __init__(self, tl, sl):
        self.t = _Sl(tl.t, sl)
        self.k = tl.k

    def __getitem__(self, idx):
        return self.t[idx]


class _Sl:
    def __init__(self, t, sl):
        self.base = t
        self.sl = sl

    def __getitem__(self, idx):
        rows, cols = idx
        assert cols == slice(None)
        return self.base[rows, self.sl]


_NC_CACHE = {}


def _in_map(inputs, b, T, consts):
    m = {"x": np.ascontiguousarray(inputs["x"][b, :T], dtype=np.float32)}
    for k, a in inputs.items():
        if k == "x":
            continue
        a = np.asarray(a, dtype=np.float32)
        if k in ("router_w", "exp_w_gate", "exp_w_up", "exp_w_down", "ln1_g", "ln1_b", "ln2_g", "ln2_b"):
            m[k] = np.ascontiguousarray(a)
        elif k == "router_bias":
            m[k] = np.ascontiguousarray(a.reshape(1, -1))
        elif k == "a_r_k":
            m[k] = np.ascontiguousarray(a.reshape(1, 512))
        elif a.ndim == 3:
            m[k] = np.ascontiguousarray(a[0])
        elif a.ndim == 2:
            m[k] = np.ascontiguousarray(a[0:1])
    m.update(consts)
    return m


def kernel(**inputs):
    x = np.asarray(inputs["x"])
    B, T, _ = x.shape
    if T not in _NC_CACHE:
        _NC_CACHE[T] = build(T)
    nc = _NC_CACHE[T]
    consts = {"c_" + k: v for k, v in host_consts(T).items()}
    consts.update({"c_" + k: v for k, v in rope_consts(T).items()})
    in_maps = [_in_map(inputs, b, T, consts) for b in range(B)]
    res = run_bass_kernel_spmd(nc, in_maps, core_ids=list(range(B)))
    out = np.stack([np.asarray(res.results[b]["out"], dtype=np.float32) for b in range(B)], 0)
    return out
```
